# Optimizing a Trainium2 kernel written in Bass

```python
import math
import jax
import jax.numpy as jnp
from jax import lax
import numpy as np

D_MODEL = 1024
BATCH = 8
SEQ = 4096
DEPTH = 1

F32 = jnp.float32
RMS_EPS = 1e-6
N_ATTN_HEADS = 8
ATTN_HEAD_DIM = 64
ATTN_W = N_ATTN_HEADS * ATTN_HEAD_DIM
IDX_HEADS = 16
IDX_DIM = 32
IDX_Q = IDX_HEADS * IDX_DIM
TOPK_MAX = 256
Q_BLOCK = 64
N_BUCKETS = 32
MAX_DISTANCE = 128
RWKV_HEADS = 8
RWKV_HEAD = 64
RWKV_W = RWKV_HEADS * RWKV_HEAD
DECAY_LORA = 64
ICLR_LORA = 64
GATE_LORA = 128
RWKV_IN = 3 * RWKV_W + DECAY_LORA + ICLR_LORA + GATE_LORA
GN_EPS = 64e-5
RWKV_START = 3 * ATTN_W + IDX_Q + IDX_DIM + IDX_HEADS
N_IN = RWKV_START + RWKV_IN + 2 * D_MODEL
IN_SPLITS = (ATTN_W, 2 * ATTN_W, 3 * ATTN_W, 3 * ATTN_W + IDX_Q,
             3 * ATTN_W + IDX_Q + IDX_DIM, RWKV_START,
             RWKV_START + RWKV_IN, RWKV_START + RWKV_IN + D_MODEL)
RWKV_SPLITS = (RWKV_W, 2 * RWKV_W, 3 * RWKV_W, 3 * RWKV_W + DECAY_LORA,
               3 * RWKV_W + DECAY_LORA + ICLR_LORA)
N_EXPERTS = 64
N_GROUPS = 8
TOPK_GROUPS = 4
MOE_TOPK = 8
EXPERT_FF = 256
ROUTED_SCALE = 2.5
EXPERT_BLOCK = 128

kernel_name = 'hybrid_dsa_rwkv7_moe_block'


def rms_norm(x, g):
    xf = x.astype(F32)
    y = xf * lax.rsqrt(jnp.mean(xf * xf, axis=-1, keepdims=True) + RMS_EPS)
    return (y * g.astype(F32)).astype(x.dtype)


def t5_bucket(rel):
    n = jnp.maximum(rel, 0)
    max_exact = N_BUCKETS // 2
    nf = jnp.maximum(n, 1).astype(F32)
    large = max_exact + (jnp.log(nf / max_exact) / math.log(MAX_DISTANCE / max_exact)
                         * (N_BUCKETS - max_exact)).astype(jnp.int32)
    large = jnp.minimum(large, N_BUCKETS - 1)
    return jnp.where(n < max_exact, n, large)


def dsa_sparse_attention(q, k, v, q_idx, k_idx, w_idx, rel_bias):
    B, S = q.shape[0], q.shape[1]
    top_k = min(TOPK_MAX, S // 4)
    nb = S // Q_BLOCK
    k_flat = k.reshape(B, S, ATTN_W)
    v_flat = v.reshape(B, S, ATTN_W)
    key_pos = jnp.arange(S, dtype=jnp.int32)
    idx_scale = (IDX_HEADS ** -0.5) * (IDX_DIM ** -0.5)

    def to_blocks(t):
        return jnp.moveaxis(t.reshape((B, nb, Q_BLOCK) + t.shape[2:]), 1, 0)

    def block(args):
        qb, qib, wib, start = args
        q_pos = start + jnp.arange(Q_BLOCK, dtype=jnp.int32)
        dots = jnp.einsum('bqhd,bsd->bqhs', qib, k_idx, preferred_element_type=F32)
        idx_score = jnp.einsum('bqhs,bqh->bqs', jax.nn.relu(dots), wib.astype(F32)) * idx_scale
        causal = key_pos[None, :] <= q_pos[:, None]
        idx_score = jnp.where(causal[None], idx_score, -jnp.inf)
        _, sel = lax.top_k(idx_score, top_k)
        kg = jax.vmap(lambda kf, i: kf[i])(k_flat, sel).reshape(B, Q_BLOCK, top_k, N_ATTN_HEADS, ATTN_HEAD_DIM)
        vg = jax.vmap(lambda vf, i: vf[i])(v_flat, sel).reshape(B, Q_BLOCK, top_k, N_ATTN_HEADS, ATTN_HEAD_DIM)
        logits = jnp.einsum('bqhd,bqkhd->bqhk', qb, kg, preferred_element_type=F32) * (ATTN_HEAD_DIM ** -0.5)
        rel = q_pos[None, :, None] - sel
        bias = rel_bias[t5_bucket(rel)].astype(F32)
        logits = logits + jnp.moveaxis(bias, -1, 2)
        valid = sel <= q_pos[None, :, None]
        logits = jnp.where(valid[:, :, None, :], logits, -jnp.inf)
        p = jax.nn.softmax(logits, axis=-1).astype(vg.dtype)
        return jnp.einsum('bqhk,bqkhd->bqhd', p, vg)

    starts = jnp.arange(nb, dtype=jnp.int32) * Q_BLOCK
    out = lax.map(block, (to_blocks(q), to_blocks(q_idx), to_blocks(w_idx), starts))
    return jnp.moveaxis(out, 0, 1).reshape(B, S, ATTN_W)


def wkv7_scan(r, decay, k, v, a, b):
    B, S, H, N = r.shape

    def step(state, inp):
        r_t, w_t, k_t, v_t, a_t, b_t = inp
        sa = jnp.einsum('bhij,bhj->bhi', state, a_t)
        state = state * w_t[:, :, None, :] + sa[..., None] * b_t[:, :, None, :] + v_t[..., None] * k_t[:, :, None, :]
        y = jnp.einsum('bhij,bhj->bhi', state, r_t)
        return state, y

    xs = tuple(jnp.moveaxis(t, 1, 0) for t in (r, decay, k, v, a, b))
    _, ys = lax.scan(step, jnp.zeros((B, H, N, N), F32), xs)
    return jnp.moveaxis(ys, 0, 1)


def rwkv7_branch(z, tshift_mu, decay_w0, decay_up, iclr_a0, iclr_up, gate_up, k_k, k_a, r_k, lnx_g, lnx_b):
    B, S = z.shape[0], z.shape[1]
    z_prev = jnp.pad(z[:, :-1], ((0, 0), (1, 0), (0, 0)))
    z = (z + tshift_mu * (z_prev - z)).astype(F32)
    r, k, v, wd, ad, gd = jnp.split(z, RWKV_SPLITS, axis=-1)
    w_log = -jax.nn.softplus(-(decay_w0.astype(F32) + jnp.tanh(wd) @ decay_up.astype(F32))) - 0.5
    decay = jnp.exp(-jnp.exp(w_log))
    a = jax.nn.sigmoid(iclr_a0.astype(F32) + ad @ iclr_up.astype(F32))
    g = jax.nn.sigmoid(gd) @ gate_up.astype(F32)
    heads = lambda t: t.reshape(B, S, RWKV_HEADS, RWKV_HEAD)
    kk = heads(k * k_k.astype(F32))
    kk = kk / jnp.maximum(jnp.sqrt(jnp.sum(kk * kk, axis=-1, keepdims=True)), 1e-12)
    k = k * (1.0 + (a - 1.0) * k_a.astype(F32))
    r, k, v, decay, a = heads(r), heads(k), heads(v), heads(decay), heads(a)
    y = wkv7_scan(r, decay, k, v, -kk, kk * a)
    mu = jnp.mean(y, axis=-1, keepdims=True)
    var = jnp.mean(jnp.square(y - mu), axis=-1, keepdims=True)
    yn = ((y - mu) * lax.rsqrt(var + GN_EPS)).reshape(B, S, RWKV_W)
    yn = yn * lnx_g.astype(F32) + lnx_b.astype(F32)
    bonus = (jnp.sum(r * k * r_k.astype(F32), axis=-1, keepdims=True) * v).reshape(B, S, RWKV_W)
    return (yn + bonus) * g


def swiglu(h, wg, wu, wd):
    return (jax.nn.silu(h @ wg) * (h @ wu)) @ wd


def moe_ffn(h, router_w, router_bias, exp_gate, exp_up, exp_down, sh_gate, sh_up, sh_down):
    B, S, D = h.shape
    T = B * S
    hf = h.reshape(T, D)
    scores = jax.nn.sigmoid(jnp.einsum('td,de->te', hf, router_w, preferred_element_type=F32))
    choice = scores + router_bias.astype(F32)
    grp_score = lax.top_k(choice.reshape(T, N_GROUPS, N_EXPERTS // N_GROUPS), 2)[0].sum(-1)
    _, top_g = lax.top_k(grp_score, TOPK_GROUPS)
    gmask = jnp.any(top_g[:, :, None] == jnp.arange(N_GROUPS)[None, None, :], axis=1)
    emask = jnp.repeat(gmask, N_EXPERTS // N_GROUPS, axis=1)
    _, top_e = lax.top_k(jnp.where(emask, choice, -jnp.inf), MOE_TOPK)
    gw = jnp.take_along_axis(scores, top_e, axis=1)
    gw = gw / jnp.sum(gw, axis=-1, keepdims=True) * ROUTED_SCALE
    A = T * MOE_TOPK
    flat_e = top_e.reshape(A)
    order = jnp.argsort(flat_e)
    se = flat_e[order]
    counts = jnp.bincount(flat_e, length=N_EXPERTS)
    padded = (counts + EXPERT_BLOCK - 1) // EXPERT_BLOCK * EXPERT_BLOCK
    start = jnp.cumsum(counts) - counts
    pend = jnp.cumsum(padded)
    pstart = pend - padded
    dest = pstart[se] + jnp.arange(A, dtype=jnp.int32) - start[se]
    n_blocks = -(-A // EXPERT_BLOCK) + N_EXPERTS
    P = n_blocks * EXPERT_BLOCK
    slot_tok = jnp.zeros((P,), jnp.int32).at[dest].set((order // MOE_TOPK).astype(jnp.int32))
    slot_w = jnp.zeros((P,), F32).at[dest].set(gw.reshape(A)[order])
    block_start = jnp.arange(n_blocks, dtype=jnp.int32) * EXPERT_BLOCK
    block_e = jnp.minimum(jnp.sum(pend[None, :] <= block_start[:, None], axis=1), N_EXPERTS - 1)

    def step(y, blk):
        tok, wt, e = blk
        out = swiglu(hf[tok], exp_gate[e], exp_up[e], exp_down[e])
        return y.at[tok].add(out.astype(F32) * wt[:, None]), None

    y, _ = lax.scan(step, jnp.zeros((T, D), F32),
                    (slot_tok.reshape(n_blocks, EXPERT_BLOCK), slot_w.reshape(n_blocks, EXPERT_BLOCK), block_e))
    shared = swiglu(hf, sh_gate, sh_up, sh_down)
    return (y.astype(h.dtype) + shared).reshape(B, S, D)


def hybrid_layer(x, c, ada_w, ada_b, norm1_g, w_in, rel_bias, tshift_mu, decay_w0, decay_up,
                 iclr_a0, iclr_up, gate_up, k_k, k_a, r_k, lnx_g, lnx_b, w_attn_br, w_rwkv_br,
                 w_out, norm2_g, router_w, router_bias, exp_gate, exp_up, exp_down,
                 sh_gate, sh_up, sh_down):
    B, S, D = x.shape
    mod = (jax.nn.silu(c.astype(F32)) @ ada_w.astype(F32) + ada_b.astype(F32)).astype(x.dtype)
    sh1, sc1, g1, sh2, sc2, g2 = [m[:, None, :] for m in jnp.split(mod, 6, axis=-1)]
    h = rms_norm(x, norm1_g) * (1 + sc1) + sh1
    z = h @ w_in
    q, k, v, iq, ik, iw, zr, ga, gr = jnp.split(z, IN_SPLITS, axis=-1)
    hs = lambda t: t.reshape(B, S, N_ATTN_HEADS, ATTN_HEAD_DIM)
    attn = dsa_sparse_attention(hs(q), hs(k), hs(v), iq.reshape(B, S, IDX_HEADS, IDX_DIM), ik, iw, rel_bias)
    rw = rwkv7_branch(zr, tshift_mu, decay_w0, decay_up, iclr_a0, iclr_up, gate_up,
                      k_k, k_a, r_k, lnx_g, lnx_b).astype(x.dtype)
    mixed = jax.nn.sigmoid(ga) * (attn @ w_attn_br) + jax.nn.sigmoid(gr) * (rw @ w_rwkv_br)
    x = x + g1 * (mixed @ w_out)
    h2 = rms_norm(x, norm2_g) * (1 + sc2) + sh2
    x = x + g2 * moe_ffn(h2, router_w, router_bias, exp_gate, exp_up, exp_down, sh_gate, sh_up, sh_down)
    return x


def setup_inputs(seed: int = 0) -> dict:
    key = jax.random.key(seed)
    ks = iter(jax.random.split(key, 40))
    L, D = DEPTH, D_MODEL

    def nrm(shape, scale):
        return jax.random.normal(next(ks), shape, F32) * scale

    def uni(shape, lo, hi):
        return jax.random.uniform(next(ks), shape, F32, lo, hi)

    return {
        'x': nrm((BATCH, SEQ, D), 1.0),
        'c': nrm((BATCH, D), 1.0),
        'ada_w': nrm((L, D, 6 * D), 0.5 * D ** -0.5),
        'ada_b': nrm((L, 6 * D), 0.02),
        'norm1_g': 1.0 + nrm((L, D), 0.02),
        'w_in': nrm((L, D, N_IN), D ** -0.5),
        'rel_bias': nrm((N_BUCKETS, N_ATTN_HEADS), 0.5),
        'tshift_mu': uni((L, RWKV_IN), 0.0, 1.0),
        'decay_w0': uni((L, RWKV_W), -3.0, 1.0),
        'decay_up': nrm((L, DECAY_LORA, RWKV_W), 0.1),
        'iclr_a0': nrm((L, RWKV_W), 0.5),
        'iclr_up': nrm((L, ICLR_LORA, RWKV_W), ICLR_LORA ** -0.5),
        'gate_up': nrm((L, GATE_LORA, RWKV_W), GATE_LORA ** -0.5),
        'k_k': 0.85 + nrm((L, RWKV_W), 0.02),
        'k_a': 1.0 + nrm((L, RWKV_W), 0.02),
        'r_k': nrm((L, RWKV_HEADS, RWKV_HEAD), 0.1),
        'lnx_g': 1.0 + nrm((L, RWKV_W), 0.02),
        'lnx_b': nrm((L, RWKV_W), 0.02),
        'w_attn_br': nrm((L, ATTN_W, D), ATTN_W ** -0.5),
        'w_rwkv_br': nrm((L, RWKV_W, D), RWKV_W ** -0.5),
        'w_out': nrm((L, D, D), D ** -0.5),
        'norm2_g': 1.0 + nrm((L, D), 0.02),
        'router_w': nrm((L, D, N_EXPERTS), D ** -0.5),
        'router_bias': nrm((L, N_EXPERTS), 0.01),
        'exp_gate': nrm((L, N_EXPERTS, D, EXPERT_FF), D ** -0.5),
        'exp_up': nrm((L, N_EXPERTS, D, EXPERT_FF), D ** -0.5),
        'exp_down': nrm((L, N_EXPERTS, EXPERT_FF, D), EXPERT_FF ** -0.5),
        'sh_gate': nrm((L, D, EXPERT_FF), D ** -0.5),
        'sh_up': nrm((L, D, EXPERT_FF), D ** -0.5),
        'sh_down': nrm((L, EXPERT_FF, D), EXPERT_FF ** -0.5),
        'final_g': 1.0 + nrm((D,), 0.02),
    }


def reference(x, c, ada_w, ada_b, norm1_g, w_in, rel_bias, tshift_mu, decay_w0, decay_up,
              iclr_a0, iclr_up, gate_up, k_k, k_a, r_k, lnx_g, lnx_b, w_attn_br, w_rwkv_br,
              w_out, norm2_g, router_w, router_bias, exp_gate, exp_up, exp_down,
              sh_gate, sh_up, sh_down, final_g):
    for l in range(DEPTH):
        x = hybrid_layer(x, c, ada_w[l], ada_b[l], norm1_g[l], w_in[l], rel_bias, tshift_mu[l],
                         decay_w0[l], decay_up[l], iclr_a0[l], iclr_up[l], gate_up[l], k_k[l],
                         k_a[l], r_k[l], lnx_g[l], lnx_b[l], w_attn_br[l], w_rwkv_br[l], w_out[l],
                         norm2_g[l], router_w[l], router_bias[l], exp_gate[l], exp_up[l],
                         exp_down[l], sh_gate[l], sh_up[l], sh_down[l])
    return rms_norm(x, final_g)
```

```python
import contextlib
import numpy as np
import concourse.bass as bass
import concourse.mybir as mybir

F32 = mybir.dt.float32
BF16 = mybir.dt.bfloat16
I32 = mybir.dt.int32
U32 = mybir.dt.uint32
AF = mybir.ActivationFunctionType
ALU = mybir.AluOpType
AX = mybir.AxisListType

N_DMA_SEMS = 6


class Prog:
    ENGS = ('pe', 'act', 'dve', 'pool', 'sp')

    def __init__(self, nc):
        self.nc = nc
        self.ops = {e: [] for e in self.ENGS}
        self.cnt = {e: 0 for e in self.ENGS}
        self.waited = {e: {} for e in self.ENGS}
        self.res = {}
        self.dma_tot = [0] * N_DMA_SEMS
        self.dma_rr = 0
        self.stack = contextlib.ExitStack()
        self.gstack = contextlib.ExitStack()
        self.nsb = 0
        self.nphase = 0
        self.sems = None

    def _new_sems(self):
        nc = self.nc
        self.sems = {}
        for e in self.ENGS:
            self.sems[e] = self.gstack.enter_context(nc.semaphore(f"s_{e}_{self.nphase}"))
        for i in range(N_DMA_SEMS):
            self.sems[('dma', i)] = self.gstack.enter_context(nc.semaphore(f"s_dma{i}_{self.nphase}"))
        self.cnt = {e: 0 for e in self.ENGS}
        self.waited = {e: {} for e in self.ENGS}
        self.dma_tot = [0] * N_DMA_SEMS
        self.dma_rr = 0

    def gsb(self, shape, dt, name=None):
        self.nsb += 1
        return self.gstack.enter_context(self.nc.sbuf_tensor(name or f"gsb{self.nsb}", list(shape), dt))

    def sb(self, shape, dt, name=None):
        self.nsb += 1
        return self.stack.enter_context(self.nc.sbuf_tensor(name or f"sb{self.nsb}", list(shape), dt))

    def ps(self, shape, dt, name=None):
        self.nsb += 1
        return self.stack.enter_context(self.nc.psum_tensor(name or f"ps{self.nsb}", list(shape), dt))

    def _deps(self, reads, writes):
        deps = {}
        def add(d):
            if d is None:
                return
            k, v = d
            if deps.get(k, 0) < v:
                deps[k] = v
        for k in reads:
            r = self.res.get(k)
            if r:
                add(r['w'])
        for k in writes:
            r = self.res.get(k)
            if r:
                add(r['w'])
                for d in r['r']:
                    add(d)
        return deps

    def _emit_waits(self, eng, deps):
        w = self.waited[eng]
        for k, v in deps.items():
            if w.get(k, 0) < v:
                w[k] = v
                self.ops[eng].append(('wait', k, v))

    def _update(self, dep, reads, writes):
        for k in reads:
            r = self.res.setdefault(k, {'w': None, 'r': []})
            r['r'] = [d for d in r['r'] if d[0] != dep[0]] + [dep]
        for k in writes:
            self.res[k] = {'w': dep, 'r': []}

    def op(self, eng, fn, reads=(), writes=()):
        if self.sems is None:
            self._new_sems()
        deps = self._deps(reads, writes)
        if eng == 'pe':
            deps.pop('pe', None)
        self._emit_waits(eng, deps)
        self.cnt[eng] += 1
        dep = (eng, self.cnt[eng])
        self.ops[eng].append(('op', fn))
        self._update(dep, reads, writes)
        return dep

    def dma(self, q, fn, reads=(), writes=()):
        if self.sems is None:
            self._new_sems()
        deps = self._deps(reads, writes)
        s = self.dma_rr
        self.dma_rr = (self.dma_rr + 1) % N_DMA_SEMS
        key = ('dma', s)
        if self.dma_tot[s] > 0:
            if deps.get(key, 0) < self.dma_tot[s]:
                deps[key] = self.dma_tot[s]
        self._emit_waits(q, deps)
        self.dma_tot[s] += 16
        dep = (key, self.dma_tot[s])
        self.ops[q].append(('dma', fn, s))
        self._update(dep, reads, writes)
        return dep

    def emit(self):
        nc = self.nc
        with contextlib.ExitStack() as st:
            sems = self.sems
            self.nphase += 1
            block = st.enter_context(nc.Block(f"ph{self.nphase}"))
            engobj = {'pe': nc.tensor, 'act': nc.scalar, 'dve': nc.vector, 'pool': nc.gpsimd, 'sp': nc.sync}
            fin = {}
            for e in self.ENGS:
                if e != 'sp' and self.cnt[e] > 0:
                    fin[e] = self.cnt[e]
            for i in range(N_DMA_SEMS):
                if self.dma_tot[i] > 0:
                    fin[('dma', i)] = self.dma_tot[i]
            self._emit_waits('sp', fin)

            def run(e):
                eo = engobj[e]
                for item in self.ops[e]:
                    if item[0] == 'wait':
                        eo.wait_ge(sems[item[1]], item[2])
                    elif item[0] == 'op':
                        item[1](eo).then_inc(sems[e], 1)
                    else:
                        item[1](eo).then_inc(sems[('dma', item[2])], 16)

            @block.tensor
            def _(t):
                run('pe')

            @block.scalar
            def _(t):
                run('act')

            @block.vector
            def _(t):
                run('dve')

            @block.gpsimd
            def _(t):
                run('pool')

            @block.sync
            def _(t):
                run('sp')
        self.stack.close()
        self.stack = contextlib.ExitStack()
        self.ops = {e: [] for e in self.ENGS}
        self.res = {}
        self.sems = None

    def finish(self):
        self.gstack.close()

from concourse.bass_utils import run_bass_kernel_spmd
import ml_dtypes

S = 4096
D = 1024
NT = S // 128
NIN = 5936


class Rot:
    def __init__(self, P, n, shape, dt, name, psum=False):
        self.tiles = [(P.ps(shape, dt) if psum else P.sb(shape, dt)) for _ in range(n)]
        self.name = name
        self.i = 0

    def next(self):
        t = self.tiles[self.i % len(self.tiles)]
        k = f"{self.name}{self.i % len(self.tiles)}"
        self.i += 1
        return t, k


def mm(P, out, lhsT, rhs, start, stop, reads, writes):
    P.op('pe', lambda e: e.matmul(out, lhsT=lhsT, rhs=rhs, start=start, stop=stop), reads=reads, writes=writes)


def phase_A(P, T, G):
    mod_bc = P.sb([128, 6 * D], F32)
    ccol = P.sb([128, 8], F32)
    scol = P.sb([128, 8], F32)
    adab = P.sb([1, 6144], F32)
    modrow = P.sb([1, 6144], F32)
    ngb = P.sb([128, 1024], F32)
    P.dma('sp', lambda e: e.dma_start(out=ccol[:], in_=T['c_col']), writes=['ccol'])
    P.dma('sp', lambda e: e.dma_start(out=adab[:], in_=T['ada_b']), writes=['adab'])
    P.op('act', lambda e: e.activation(out=scol[:], in_=ccol[:], func=AF.Silu), reads=['ccol'], writes=['scol'])
    wrot = Rot(P, 2, [128, 8, 512], F32, 'aw')
    psr = Rot(P, 2, [1, 512], F32, 'psr', psum=True)
    psb = Rot(P, 2, [128, 512], F32, 'psb', psum=True)
    adaw = T['ada_w'].rearrange("(k p) n -> p k n", p=128)
    for n in range(12):
        wb, wk = wrot.next()
        P.dma('sp' if n % 2 == 0 else 'act',
              lambda e, wb=wb, n=n: e.dma_start(out=wb[:], in_=adaw[:, :, n * 512:(n + 1) * 512]), writes=[wk])
        pr, pk = psr.next()
        for k in range(8):
            mm(P, pr[:], scol[:, k:k + 1], wb[:, k, :], k == 0, k == 7, [wk, 'scol'], [pk])
        sl = slice(n * 512, (n + 1) * 512)
        P.op('dve', lambda e, pr=pr, sl=sl: e.tensor_tensor(out=modrow[0:1, sl], in0=pr[:], in1=adab[0:1, sl], op=ALU.add),
             reads=[pk, 'adab'], writes=[f'modrow{n}'])
        pb, pbk = psb.next()
        mm(P, pb[:], G['ones_row'][:], modrow[0:1, sl], True, True, [f'modrow{n}'], [pbk])
        P.op('act', lambda e, pb=pb, sl=sl: e.activation(out=mod_bc[:, sl], in_=pb[:], func=AF.Copy),
             reads=[pbk], writes=[f'mod{n}'])
    for (gname, c0, deps) in (('norm1_g', 1024, ['mod2', 'mod3']), ('norm2_g', 4096, ['mod8', 'mod9'])):
        P.dma('sp', lambda e, gname=gname: e.dma_start(out=ngb[:], in_=T[gname].partition_broadcast(128)), writes=['ngb'])
        P.op('dve', lambda e, c0=c0: e.scalar_tensor_tensor(out=mod_bc[:, c0:c0 + 1024], in0=mod_bc[:, c0:c0 + 1024],
                                                            scalar=1.0, in1=ngb[:], op0=ALU.add, op1=ALU.mult),
             reads=deps + ['ngb'], writes=deps)
    P.dma('sp', lambda e: e.dma_start(out=T['modd'], in_=mod_bc[:]), reads=[f'mod{n}' for n in range(12)])


def rms_rstd(P, xt, xk, junk, ss, rs, tag):
    P.op('act', lambda e: e.activation(out=junk[:], in_=xt[:], func=AF.Square, accum_out=ss[:]),
         reads=[xk], writes=['junk' + tag, 'ss' + tag])
    P.op('act', lambda e: e.activation(out=ss[:], in_=ss[:], func=AF.Sqrt, scale=1.0 / D, bias=G_EPS[0][:, 0:1]),
         reads=['ss' + tag], writes=['ss' + tag])
    P.op('dve', lambda e: e.reciprocal(out=rs[:], in_=ss[:]), reads=['ss' + tag], writes=['rs' + tag])


G_EPS = [None]


def norm_mod_transpose(P, G, xt, xk, hT, i, g_sl, sh_sl, W):
    mod_bc = W['mod']
    junk, ss, rs, t1, hb, pt = W['junk'], W['ss'], W['rs'], W['t1'], W['hb'], W['pt']
    rms_rstd(P, xt, xk, junk, ss, rs, '')
    P.op('dve', lambda e: e.scalar_tensor_tensor(out=t1[:], in0=xt[:], scalar=rs[:, 0:1], in1=mod_bc[:, g_sl],
                                                 op0=ALU.mult, op1=ALU.mult), reads=[xk, 'rs', 'modl'], writes=['t1'])
    P.op('pool', lambda e: e.tensor_tensor(out=hb[:], in0=t1[:], in1=mod_bc[:, sh_sl], op=ALU.add),
         reads=['t1', 'modl'], writes=['hb'])
    for k in range(8):
        P.op('pe', lambda e, k=k: e.transpose(out=pt[:, k, :], in_=hb[:, k * 128:(k + 1) * 128], identity=G['identb'][:]),
             reads=['hb'], writes=['pt'])
    P.op('act', lambda e: e.activation(out=hT[:, :, i * 128:(i + 1) * 128], in_=pt[:], func=AF.Copy),
         reads=['pt'], writes=[f'hT{i // 4}'])


def phase_BC(P, T, G):
    hT = P.sb([128, 8, S], BF16)
    W = dict(junk=P.sb([128, D], F32), ss=P.sb([128, 1], F32), rs=P.sb([128, 1], F32), t1=P.sb([128, D], F32),
             hb=P.sb([128, D], BF16), pt=P.ps([128, 8, 128], BF16))
    W['mod'] = P.sb([128, 2048], F32)
    P.dma('sp', lambda e: e.dma_start(out=W['mod'][:], in_=T['modd'][:, 0:2048]), writes=['modl'])
    xrot = Rot(P, 2, [128, D], F32, 'x')
    for i in range(NT):
        xt, xk = xrot.next()
        P.dma('sp', lambda e, xt=xt, i=i: e.dma_start(out=xt[:], in_=T['x'][i * 128:(i + 1) * 128, :]), writes=[xk])
        norm_mod_transpose(P, G, xt, xk, hT, i, slice(1024, 2048), slice(0, 1024), W)
    win = T['w_in'].rearrange("(k p) n -> p k n", p=128)
    segs = [('zq', 0, 512, BF16), ('zk', 512, 512, BF16), ('ziq', 1536, 512, BF16), ('zik', 2048, 32, BF16),
            ('zr', 2096, 1792, F32), ('zga', 3888, 1024, BF16), ('zgr', 4912, 1024, BF16)]
    wrot = Rot(P, 2, [128, 8, 128], BF16, 'w')
    psrot = Rot(P, 3, [128, 512], F32, 'ps', psum=True)
    strot = {BF16: Rot(P, 3, [128, 512], BF16, 'stb'), F32: Rot(P, 3, [128, 512], F32, 'stf')}
    ev = 0
    for (name, c0, n, dt) in segs:
        for m0 in range(0, n, 128):
            M = min(128, n - m0)
            wt, wk = wrot.next()
            P.dma('pool', lambda e, wt=wt, M=M, a=c0 + m0: e.dma_start(out=wt[:, :, :M], in_=win[:, :, a:a + M]), writes=[wk])
            for tg in range(8):
                ps, pk = psrot.next()
                for k in range(8):
                    mm(P, ps[:M, :], wt[:, k, :M], hT[:, k, tg * 512:(tg + 1) * 512], k == 0, k == 7, [wk, f'hT{tg}'], [pk])
                st, sk = strot[dt].next()
                if ev % 2 == 0:
                    P.op('act', lambda e, st=st, ps=ps, M=M: e.activation(out=st[:M, :], in_=ps[:M, :], func=AF.Copy),
                         reads=[pk], writes=[sk])
                else:
                    P.op('dve', lambda e, st=st, ps=ps, M=M: e.tensor_copy(out=st[:M, :], in_=ps[:M, :]),
                         reads=[pk], writes=[sk])
                ev += 1
                P.dma('sp', lambda e, st=st, M=M, name=name, m0=m0, tg=tg:
                      e.dma_start(out=T[name][m0:m0 + M, tg * 512:(tg + 1) * 512], in_=st[:M, :]), reads=[sk])
    wv = P.sb([128, 8, 528], BF16)
    P.dma('pool', lambda e: e.dma_start(out=wv[:, :, 0:512], in_=win[:, :, 1024:1536]), writes=['wv'])
    P.dma('pool', lambda e: e.dma_start(out=wv[:, :, 512:528], in_=win[:, :, 2080:2096]), writes=['wv2'])
    ps2rot = Rot(P, 2, [128, 16], F32, 'ps2', psum=True)
    st2rot = Rot(P, 2, [128, 16], F32, 'st2')
    for i in range(NT):
        ps, pk = psrot.next()
        ps2, pk2 = ps2rot.next()
        for k in range(8):
            mm(P, ps[:], hT[:, k, i * 128:(i + 1) * 128], wv[:, k, 0:512], k == 0, k == 7, ['wv', f'hT{i // 4}'], [pk])
        for k in range(8):
            mm(P, ps2[:], hT[:, k, i * 128:(i + 1) * 128], wv[:, k, 512:528], k == 0, k == 7, ['wv2', f'hT{i // 4}'], [pk2])
        st, sk = strot[BF16].next()
        st2, sk2 = st2rot.next()
        P.op('act', lambda e, st=st, ps=ps: e.activation(out=st[:], in_=ps[:], func=AF.Copy), reads=[pk], writes=[sk])
        P.op('dve', lambda e, st2=st2, ps2=ps2: e.tensor_copy(out=st2[:], in_=ps2[:]), reads=[pk2], writes=[sk2])
        P.dma('sp', lambda e, st=st, i=i: e.dma_start(out=T['zv'][i * 128:(i + 1) * 128, :], in_=st[:]), reads=[sk])
        P.dma('sp', lambda e, st2=st2, i=i: e.dma_start(out=T['ziw'][i * 128:(i + 1) * 128, :], in_=st2[:]), reads=[sk2])


def t5_lo_bounds():
    n = np.arange(256)
    nf = np.maximum(n, 1).astype(np.float32)
    large = 16 + (np.log(nf / np.float32(16)) / np.float32(np.log(8.0)) * np.float32(16)).astype(np.int32)
    large = np.minimum(large, 31)
    bk = np.where(n < 16, n, large)
    return [int(np.min(np.nonzero(bk >= b)[0])) for b in range(1, 32)]


def phase_D0(P, T, G):
    relb = P.sb([128, 256], F32)
    diff = P.sb([128, 248], F32)
    base = P.sb([128, 8], F32)
    relidx = P.sb([128, 256], F32)
    P.dma('sp', lambda e: e.dma_start(out=relb[:], in_=T['rel_bias'].partition_broadcast(128)), writes=['relb'])
    P.dma('sp', lambda e: e.dma_start(out=relidx[:], in_=T['k_rel']), writes=['relidx'])
    P.op('dve', lambda e: e.tensor_tensor(out=diff[:], in0=relb[:, 8:256], in1=relb[:, 0:248], op=ALU.subtract),
         reads=['relb'], writes=['diff'])
    P.op('dve', lambda e: e.tensor_tensor(out=base[:], in0=relb[:, 0:8], in1=relb[:, 248:256], op=ALU.subtract),
         reads=['relb'], writes=['base'])
    P.op('dve', lambda e: e.tensor_copy(out=G['b31'][:], in_=relb[:, 248:256]), reads=['relb'], writes=['b31'])
    E = G['E']
    irot = Rot(P, 2, [128, 256], F32, 'ind')
    los = t5_lo_bounds()
    for b in range(1, 32):
        ind, ik = irot.next()
        P.op('pool', lambda e, ind=ind, lo=float(los[b - 1]): e.tensor_scalar(out=ind[:], in0=relidx[:], scalar1=lo, scalar2=None,
                                                                              op0=ALU.is_ge), reads=['relidx'], writes=[ik])
        for h in range(8):
            if b == 1:
                P.op('dve', lambda e, ind=ind, h=h: e.tensor_scalar(out=E[:, h, :], in0=ind[:], scalar1=diff[:, h:h + 1],
                                                                    scalar2=base[:, h:h + 1], op0=ALU.mult, op1=ALU.add),
                     reads=[ik, 'diff', 'base'], writes=[f'E{h}'])
            else:
                c = (b - 1) * 8 + h
                P.op('dve', lambda e, ind=ind, h=h, c=c: e.scalar_tensor_tensor(out=E[:, h, :], in0=ind[:], scalar=diff[:, c:c + 1],
                                                                               in1=E[:, h, :], op0=ALU.mult, op1=ALU.add),
                     reads=[ik, 'diff', f'E{h}'], writes=[f'E{h}'])
    P.op('act', lambda e: e.activation(out=E[:], in_=E[:], func=AF.Exp), reads=[f'E{h}' for h in range(8)],
         writes=[f'E{h}' for h in range(8)])


def phase_D(P, T, G):
    KT = P.sb([64, 8, S], BF16)
    V = P.sb([128, NT, 512], BF16)
    ik4 = P.sb([128, S], BF16)
    iw = P.sb([128, NT, 16], F32)
    ones64 = P.sb([128, 64], BF16)
    sc = P.sb([128, S], F32)
    maskb = P.sb([128, S], BF16)
    maskT = P.sb([128, NT, 128], BF16)
    lo, dd, mid, cnt, ge = [P.sb([128, 1], F32) for _ in range(5)]
    P.dma('sp', lambda e: e.dma_start(out=KT[:], in_=T['zk'].rearrange("(h p) t -> p h t", p=64)), writes=['KT'])
    P.dma('act', lambda e: e.dma_start(out=V[:], in_=T['zv'].rearrange("(i p) f -> p i f", p=128)), writes=['V'])
    for i in range(3):
        P.dma('sp', lambda e, i=i: e.dma_start(out=ik4[32 * i:32 * i + 32, :], in_=T['zik']), writes=[f'ik4{i}'])
    P.dma('sp', lambda e: e.dma_start(out=iw[:], in_=T['ziw'].rearrange("(i p) f -> p i f", p=128)), writes=['iw'])
    P.op('dve', lambda e: e.memset(ones64[:], 1.0), writes=['ones64'])
    zq = T['zq'].rearrange("(h p) t -> p h t", p=64)
    ziq = T['ziq'][0:480, :].rearrange("(j p) t -> p j t", p=96)
    attnT = T['attnT'].rearrange("(h p) t -> p h t", p=64)
    qrot = Rot(P, 2, [64, 8, 128], BF16, 'q')
    iqrot = Rot(P, 2, [96, 6, 128], BF16, 'iq')
    rrot = Rot(P, 3, [128, 512], BF16, 'r')
    psi = Rot(P, 2, [128, 512], F32, 'psi', psum=True)
    pss = Rot(P, 2, [128, 4, 128], F32, 'pss', psum=True)
    ptm = Rot(P, 1, [128, 4, 128], BF16, 'ptm', psum=True)
    pod = Rot(P, 1, [64, 512], F32, 'pod', psum=True)
    pdd = Rot(P, 1, [64, 512], F32, 'pdd', psum=True)
    pTrot = Rot(P, 3, [128, 4, 128], BF16, 'pT')
    atrot = Rot(P, 2, [64, 8, 128], BF16, 'at')
    rdrot = Rot(P, 2, [64, 128], F32, 'rd')
    E, b31 = G['E'], G['b31']
    for qi in range(NT):
        n = 128 * (qi + 1)
        nkt = qi + 1
        tsl = slice(qi * 128, (qi + 1) * 128)
        qt, qk = qrot.next()
        iqt, iqk = iqrot.next()
        P.dma('sp', lambda e, qt=qt, tsl=tsl: e.dma_start(out=qt[:], in_=zq[:, :, tsl]), writes=[qk])
        P.dma('act', lambda e, iqt=iqt, tsl=tsl: e.dma_start(out=iqt[:, 0:5, :], in_=ziq[:, :, tsl]), writes=[iqk])
        P.dma('act', lambda e, iqt=iqt, tsl=tsl: e.dma_start(out=iqt[0:32, 5, :], in_=T['ziq'][480:512, tsl]), writes=[iqk + 'b'])
        for ch in range((n + 511) // 512):
            c0 = ch * 512
            nc_ = min(512, n - c0)
            for h in range(16):
                j, i = divmod(h, 3)
                ps, pk = psi.next()
                mm(P, ps[:, :nc_], iqt[32 * i:32 * i + 32, j, :], ik4[32 * i:32 * i + 32, c0:c0 + nc_], True, True,
                   [iqk, iqk + 'b', f'ik4{i}'], [pk])
                r, rk = rrot.next()
                P.op('act', lambda e, r=r, ps=ps, nc_=nc_: e.activation(out=r[:, :nc_], in_=ps[:, :nc_], func=AF.Relu),
                     reads=[pk], writes=[rk])
                if h == 0:
                    P.op('dve', lambda e, r=r, nc_=nc_, c0=c0, qi=qi: e.tensor_scalar(
                        out=sc[:, c0:c0 + nc_], in0=r[:, :nc_], scalar1=iw[:, qi, 0:1], scalar2=None, op0=ALU.mult),
                        reads=[rk, 'iw'], writes=['sc'])
                else:
                    P.op('dve', lambda e, r=r, nc_=nc_, c0=c0, qi=qi, h=h: e.scalar_tensor_tensor(
                        out=sc[:, c0:c0 + nc_], in0=r[:, :nc_], scalar=iw[:, qi, h:h + 1], in1=sc[:, c0:c0 + nc_],
                        op0=ALU.mult, op1=ALU.add), reads=[rk, 'iw', 'sc'], writes=['sc'])
        P.op('pool', lambda e, tsl=tsl: e.affine_select(out=sc[:, tsl], in_=sc[:, tsl], pattern=[[-1, 128]],
                                                         compare_op=ALU.is_ge, fill=-1e30, base=0, channel_multiplier=1),
             reads=['sc'], writes=['sc'])
        if n <= 256:
            P.op('dve', lambda e: e.memset(lo[:], -1e29), reads=['lo'], writes=['lo'])
        else:
            nv = 128 * qi
            P.op('dve', lambda e, nv=nv: e.tensor_reduce(out=lo[:], in_=sc[:, :nv], axis=AX.X, op=ALU.min),
                 reads=['sc', 'lo'], writes=['lo'])
            P.op('dve', lambda e, n=n: e.tensor_reduce(out=mid[:], in_=sc[:, :n], axis=AX.X, op=ALU.max),
                 reads=['sc', 'mid'], writes=['mid'])
            P.op('dve', lambda e: e.tensor_tensor(out=dd[:], in0=mid[:], in1=lo[:], op=ALU.subtract),
                 reads=['mid', 'lo', 'dd'], writes=['dd'])
            for it in range(20):
                P.op('dve', lambda e: e.tensor_scalar(out=dd[:], in0=dd[:], scalar1=0.5, scalar2=None, op0=ALU.mult),
                     reads=['dd'], writes=['dd'])
                P.op('dve', lambda e: e.tensor_tensor(out=mid[:], in0=lo[:], in1=dd[:], op=ALU.add),
                     reads=['lo', 'dd', 'mid'], writes=['mid'])
                P.op('dve', lambda e, n=n: e.tensor_scalar(out=maskb[:, :n], in0=sc[:, :n], scalar1=mid[:, 0:1], scalar2=None,
                                                          op0=ALU.is_ge, op1=ALU.add, accum_out=cnt[:]),
                     reads=['sc', 'mid', 'cnt', 'maskb'], writes=['maskb', 'cnt'])
                P.op('dve', lambda e: e.tensor_scalar(out=ge[:], in0=cnt[:], scalar1=255.5, scalar2=None, op0=ALU.is_ge),
                     reads=['cnt', 'ge'], writes=['ge'])
                P.op('dve', lambda e: e.scalar_tensor_tensor(out=lo[:], in0=dd[:], scalar=ge[:, 0:1], in1=lo[:],
                                                             op0=ALU.mult, op1=ALU.add),
                     reads=['dd', 'ge', 'lo'], writes=['lo'])
        P.op('dve', lambda e, n=n: e.tensor_scalar(out=maskb[:, :n], in0=sc[:, :n], scalar1=lo[:, 0:1], scalar2=None,
                                                  op0=ALU.is_ge), reads=['sc', 'lo', 'maskb'], writes=['maskb'])
        for c4 in range((nkt + 3) // 4):
            kts = list(range(4 * c4, min(4 * c4 + 4, nkt)))
            pm, pmk = ptm.next()
            for kt in kts:
                P.op('pe', lambda e, pm=pm, kt=kt: e.transpose(out=pm[:, kt % 4, :], in_=maskb[:, kt * 128:(kt + 1) * 128],
                                                                identity=G['identb'][:]), reads=['maskb'], writes=[pmk])
            P.op('act', lambda e, pm=pm, kts=kts: e.activation(out=maskT[:, kts[0]:kts[-1] + 1, :], in_=pm[:, :len(kts), :],
                                                               func=AF.Copy), reads=[pmk], writes=['maskT'])
        at, atk = atrot.next()
        for h in range(8):
            po, pok = pod.next()
            pd, pdk = pdd.next()
            for c4 in range((nkt + 3) // 4):
                kts = list(range(4 * c4, min(4 * c4 + 4, nkt)))
                nk = len(kts)
                ps, pk = pss.next()
                for kt in kts:
                    mm(P, ps[:, kt % 4, :], KT[:, h, kt * 128:(kt + 1) * 128], qt[:, h, :], True, True, ['KT', qk], [pk])
                pT, pTk = pTrot.next()
                P.op('act', lambda e, pT=pT, ps=ps, nk=nk, h=h: e.activation(out=pT[:, :nk, :], in_=ps[:, :nk, :], func=AF.Exp,
                                                                             scale=0.125, bias=b31[:, h:h + 1]),
                     reads=[pk], writes=[pTk])
                P.op('dve', lambda e, pT=pT, nk=nk, kts=kts: e.tensor_tensor(out=pT[:, :nk, :], in0=pT[:, :nk, :],
                                                                             in1=maskT[:, kts[0]:kts[-1] + 1, :], op=ALU.mult),
                     reads=[pTk, 'maskT'], writes=[pTk])
                for kt in kts:
                    dl = qi - kt
                    if dl <= 1:
                        P.op('dve', lambda e, pT=pT, kt=kt, dl=dl, h=h: e.tensor_tensor(
                            out=pT[:, kt % 4, :], in0=pT[:, kt % 4, :], in1=E[:, h, dl * 128:(dl + 1) * 128], op=ALU.mult),
                            reads=[pTk], writes=[pTk])
                for kt in kts:
                    mm(P, po[:, 0:128], V[:, kt, h * 64:(h + 1) * 64], pT[:, kt % 4, :], kt == 0, kt == nkt - 1, ['V', pTk], [pok])
                    mm(P, pd[:, 0:128], ones64[:], pT[:, kt % 4, :], kt == 0, kt == nkt - 1, ['ones64', pTk], [pdk])
            rd, rdk = rdrot.next()
            P.op('dve', lambda e, rd=rd, pd=pd: e.reciprocal(out=rd[:], in_=pd[:, 0:128]), reads=[pdk], writes=[rdk])
            P.op('dve', lambda e, rd=rd, po=po, at=at, h=h: e.tensor_tensor(out=at[:, h, :], in0=po[:, 0:128], in1=rd[:], op=ALU.mult),
                 reads=[pok, rdk], writes=[atk])
        P.dma('sp', lambda e, at=at, tsl=tsl: e.dma_start(out=attnT[:, :, tsl], in_=at[:]), reads=[atk])


def phase_E(P, T, G):
    LD = 0.6065306597126334
    ident = G['identb']
    zr = T['zr']
    def colload(name, n, key):
        t = P.sb([64, n], F32)
        P.dma('sp', lambda e: e.dma_start(out=t[:], in_=T[name].rearrange("(h p) -> p h", p=64), allow_slow_non_contiguous=True), writes=[key])
        return t
    mu_rkv = P.sb([64, 24], F32)
    P.dma('sp', lambda e: e.dma_start(out=mu_rkv[:], in_=T['tshift_mu'][0:1536].rearrange("(h p) -> p h", p=64), allow_slow_non_contiguous=True), writes=['mu'])
    mu_wa = P.sb([64, 2], F32)
    P.dma('sp', lambda e: e.dma_start(out=mu_wa[:], in_=T['tshift_mu'][1536:1664].rearrange("(h p) -> p h", p=64), allow_slow_non_contiguous=True), writes=['mu'])
    mu_g = P.sb([128, 1], F32)
    P.dma('sp', lambda e: e.dma_start(out=mu_g[:], in_=T['tshift_mu'][1664:1792].rearrange("(h p) -> p h", p=128), allow_slow_non_contiguous=True), writes=['mu'])
    om_rkv, om_wa, om_g = P.sb([64, 24], F32), P.sb([64, 2], F32), P.sb([128, 1], F32)
    for (o, m) in ((om_rkv, mu_rkv), (om_wa, mu_wa), (om_g, mu_g)):
        P.op('dve', lambda e, o=o, m=m: e.tensor_scalar(out=o[:], in0=m[:], scalar1=-1.0, scalar2=1.0, op0=ALU.mult, op1=ALU.add),
             reads=['mu'], writes=['om'])
    w0c = colload('decay_w0', 8, 'par'); a0c = colload('iclr_a0', 8, 'par'); kkc = colload('k_k', 8, 'par')
    kac = colload('k_a', 8, 'par'); rkc = colload('r_k', 8, 'par'); lgc = colload('lnx_g', 8, 'par'); lbc = colload('lnx_b', 8, 'par')
    omka = P.sb([64, 8], F32)
    P.op('dve', lambda e: e.tensor_scalar(out=omka[:], in0=kac[:], scalar1=-1.0, scalar2=1.0, op0=ALU.mult, op1=ALU.add),
         reads=['par'], writes=['omka'])
    dup, iup, gup = P.sb([64, 512], BF16), P.sb([64, 512], BF16), P.sb([128, 512], BF16)
    P.dma('pool', lambda e: e.dma_start(out=dup[:], in_=T['decay_up']), writes=['wts'])
    P.dma('pool', lambda e: e.dma_start(out=iup[:], in_=T['iclr_up']), writes=['wts'])
    P.dma('pool', lambda e: e.dma_start(out=gup[:], in_=T['gate_up']), writes=['wts'])
    onesf = P.sb([64, 64], F32)
    onesm = P.sb([64, 64], F32)
    gneps = P.sb([64, 1], F32)
    P.op('dve', lambda e: e.memset(onesf[:], 1.0), writes=['onesf'])
    P.op('dve', lambda e: e.memset(onesm[:], 1.0 / 64), writes=['onesm'])
    P.op('dve', lambda e: e.memset(gneps[:], 64e-5), writes=['gneps'])
    km = P.sb([128, 896], F32)
    P.dma('sp', lambda e: e.dma_start(out=km[:], in_=T['k_masks']), writes=['km'])
    mask4 = P.sb([128, 512], BF16)
    maskL = P.sb([128, 128], BF16)
    P.op('dve', lambda e: e.tensor_copy(out=mask4[:], in_=km[:, 0:512]), reads=['km'], writes=['mask4'])
    P.op('dve', lambda e: e.tensor_copy(out=maskL[:], in_=km[:, 512:640]), reads=['km'], writes=['maskL'])
    rst = P.sb([64, 512], F32)
    P.dma('sp', lambda e: e.dma_start(out=rst[:], in_=T['k_reset']), writes=['rst'])
    Tst = P.sb([64, 8, 64], BF16)
    P.op('dve', lambda e: e.memset(Tst[:], 0.0), writes=[f'T{h}' for h in range(8)])
    zrot = Rot(P, 2, [64, 3, 513], F32, 'z')
    wa_in = P.sb([64, 2, 513], F32)
    gd_in = P.sb([128, 513], F32)
    tmpr = Rot(P, 2, [128, 512], F32, 'tmp')
    twb, adb, sgb = P.sb([64, 512], BF16), P.sb([64, 512], BF16), P.sb([128, 512], BF16)
    AR = P.sb([64, 8, 4, 256], BF16)
    BK = P.sb([64, 8, 4, 256], BF16)
    tok3 = P.sb([128, 8, 4, 3, 64], BF16)
    pC = P.sb([64, 8, 4], F32)
    bon = P.sb([64, 8, 512], F32)
    gg = P.sb([64, 8, 512], BF16)
    yT = P.sb([64, 8, 512], F32)
    RW = P.sb([64, 8, 512], BF16)
    hb = {n: P.sb([64, 512], F32) for n in ('sig', 'cs', 'ep', 'em', 'epv', 'kk', 'kkn', 'a', 't', 'kp', 'b', 'u1')}
    vb = P.sb([64, 512], BF16)
    Gm = P.sb([128, 8, 512], BF16)
    XY = [P.sb([128, 8, 256], BF16) for _ in range(2)]
    Nm = P.sb([128, 8, 128], BF16)
    Wsb, Usb = P.sb([128, 8, 64], BF16), P.sb([128, 8, 64], BF16)
    pg = Rot(P, 4, [128, 512], F32, 'pg', psum=True)
    pl = Rot(P, 2, [64, 512], F32, 'pl', psum=True)
    ptr = Rot(P, 1, [128, 3, 64], BF16, 'ptr', psum=True)
    psm = Rot(P, 1, [128, 512], F32, 'psm', psum=True)
    rwT = T['rwT'].rearrange("(h p) t -> p h t", p=64)

    def dve(fn, reads, writes):
        P.op('dve', fn, reads=reads, writes=writes)

    for tg in range(8):
        t0 = tg * 512
        def load_halo(dst, rows, key, q, tg=tg, t0=t0):
            if tg == 0:
                src = rows(t0, t0 + 512)
                P.op('pool', lambda e: e.memset(dst[:, 0:1] if len(dst.shape) == 2 else dst[:, :, 0:1], 0.0), reads=[key], writes=[key])
                P.dma(q, lambda e: e.dma_start(out=(dst[:, 1:513] if len(dst.shape) == 2 else dst[:, :, 1:513]), in_=src), writes=[key + 'b'])
            else:
                src = rows(t0 - 1, t0 + 512)
                P.dma(q, lambda e: e.dma_start(out=dst[:], in_=src), reads=[key + 'b'], writes=[key])
        load_halo(wa_in, lambda a, b: zr[1536:1664, a:b].rearrange("(h p) t -> p h t", p=64), 'wa', 'sp')
        load_halo(gd_in, lambda a, b: zr[1664:1792, a:b], 'gd', 'act')

        def tshift(src_prev, src_cur, mu_ap, om_ap, np_, keys):
            tm, tk = tmpr.next()
            P.op('pool', lambda e: e.tensor_scalar(out=tm[:np_, :], in0=src_prev, scalar1=mu_ap, scalar2=None, op0=ALU.mult),
                 reads=keys + ['mu'], writes=[tk])
            dve(lambda e: e.scalar_tensor_tensor(out=src_cur, in0=src_cur, scalar=om_ap, in1=tm[:np_, :], op0=ALU.mult, op1=ALU.add),
                keys + [tk, 'om'], keys)
        for i in range(2):
            tshift(wa_in[:, i, 0:512], wa_in[:, i, 1:513], mu_wa[:, i:i + 1], om_wa[:, i:i + 1], 64, ['wa', 'wab'])
        tshift(gd_in[:, 0:512], gd_in[:, 1:513], mu_g[:, 0:1], om_g[:, 0:1], 128, ['gd', 'gdb'])
        P.op('act', lambda e: e.activation(out=twb[:], in_=wa_in[:, 0, 1:513], func=AF.Tanh), reads=['wa', 'wab'], writes=['twb'])
        P.op('act', lambda e: e.activation(out=sgb[:], in_=gd_in[:, 1:513], func=AF.Sigmoid), reads=['gd', 'gdb'], writes=['sgb'])
        dve(lambda e: e.tensor_copy(out=adb[:], in_=wa_in[:, 1, 1:513]), ['wa', 'wab'], ['adb'])
        def prep_head(h, z, zk):
            def zrows(a, b, h=h):
                return zr[0:1536, a:b].rearrange("(s hh p) t -> hh p s t", s=3, p=64)[h]
            load_halo(z, zrows, zk, 'sp' if h % 2 == 0 else 'act')
            for s_ in range(3):
                tshift(z[:, s_, 0:512], z[:, s_, 1:513], mu_rkv[:, s_ * 8 + h:s_ * 8 + h + 1], om_rkv[:, s_ * 8 + h:s_ * 8 + h + 1], 64, [zk, zk + 'b'])
            r_, k_, v_ = z[:, 0, 1:513], z[:, 1, 1:513], z[:, 2, 1:513]
            zkeys = [zk, zk + 'b']
            sig, cs, ep, em, epv, kk, kkn, a_, t_, kp, b_, u1 = [hb[n] for n in ('sig', 'cs', 'ep', 'em', 'epv', 'kk', 'kkn', 'a', 't', 'kp', 'b', 'u1')]
            hs = slice(h * 64, (h + 1) * 64)
            p1, p1k = pl.next()
            mm(P, p1[:], dup[:, hs], twb[:], True, True, ['wts', 'twb'], [p1k])
            P.op('act', lambda e, p1=p1, h=h: e.activation(out=sig[:], in_=p1[:], func=AF.Sigmoid, bias=w0c[:, h:h + 1]),
                 reads=[p1k, 'par'], writes=['sig'])
            dve(lambda e: e.tensor_tensor_scan(out=cs[:], data0=rst[:], data1=sig[:], initial=0.0, op0=ALU.mult, op1=ALU.add),
                ['rst', 'sig'], ['cs'])
            P.op('act', lambda e: e.activation(out=ep[:], in_=cs[:], func=AF.Exp, scale=-LD), reads=['cs'], writes=['ep'])
            P.op('act', lambda e: e.activation(out=em[:], in_=cs[:], func=AF.Exp, scale=LD), reads=['cs'], writes=['em'])
            dve(lambda e: e.tensor_tensor(out=u1[:], in0=cs[:], in1=sig[:], op=ALU.subtract), ['cs', 'sig'], ['u1'])
            P.op('act', lambda e: e.activation(out=epv[:], in_=u1[:], func=AF.Exp, scale=-LD), reads=['u1'], writes=['epv'])
            dve(lambda e, h=h: e.tensor_copy(out=pC[:, h, :], in_=ep[:, 127:512:128]), ['ep'], ['pC'])
            p2, p2k = pl.next()
            mm(P, p2[:], iup[:, hs], adb[:], True, True, ['wts', 'adb'], [p2k])
            P.op('act', lambda e, p2=p2, h=h: e.activation(out=a_[:], in_=p2[:], func=AF.Sigmoid, bias=a0c[:, h:h + 1]),
                 reads=[p2k, 'par'], writes=['a'])
            p3, p3k = pl.next()
            mm(P, p3[:], gup[:, hs], sgb[:], True, True, ['wts', 'sgb'], [p3k])
            P.op('act', lambda e, p3=p3, h=h: e.activation(out=gg[:, h, :], in_=p3[:], func=AF.Copy), reads=[p3k], writes=[f'gg{h}'])
            dve(lambda e, h=h: e.tensor_scalar(out=kk[:], in0=k_, scalar1=kkc[:, h:h + 1], scalar2=None, op0=ALU.mult), zkeys + ['par'], ['kk'])
            P.op('act', lambda e: e.activation(out=u1[:], in_=kk[:], func=AF.Square), reads=['kk', 'u1'], writes=['u1'])
            p4, p4k = pl.next()
            mm(P, p4[:], onesf[:], u1[:], True, True, ['onesf', 'u1'], [p4k])
            P.op('act', lambda e, p4=p4: e.activation(out=kkn[:], in_=p4[:], func=AF.Sqrt), reads=[p4k], writes=['kkn'])
            dve(lambda e: e.tensor_scalar(out=kkn[:], in0=kkn[:], scalar1=1e-12, scalar2=None, op0=ALU.max), ['kkn'], ['kkn'])
            dve(lambda e: e.reciprocal(out=kkn[:], in_=kkn[:]), ['kkn'], ['kkn'])
            dve(lambda e: e.tensor_tensor(out=kkn[:], in0=kkn[:], in1=kk[:], op=ALU.mult), ['kkn', 'kk'], ['kkn'])
            dve(lambda e, h=h: e.tensor_scalar(out=t_[:], in0=a_[:], scalar1=kac[:, h:h + 1], scalar2=omka[:, h:h + 1], op0=ALU.mult, op1=ALU.add),
                ['a', 'par', 'omka'], ['t'])
            dve(lambda e: e.tensor_tensor(out=kp[:], in0=t_[:], in1=k_, op=ALU.mult), ['t'] + zkeys, ['kp'])
            dve(lambda e: e.tensor_tensor(out=b_[:], in0=kkn[:], in1=a_[:], op=ALU.mult), ['kkn', 'a'], ['b'])
            c4 = lambda ap: ap.rearrange("p (c t) -> p c t", c=4)
            dve(lambda e, h=h: e.tensor_tensor(out=AR[:, h, :, 128:256], in0=c4(r_), in1=c4(ep[:]), op=ALU.mult), zkeys + ['ep'], [f'AR{h}'])
            dve(lambda e, h=h: e.scalar_tensor_tensor(out=AR[:, h, :, 0:128], in0=c4(kkn[:]), scalar=-1.0, in1=c4(epv[:]), op0=ALU.mult, op1=ALU.mult),
                ['kkn', 'epv'], [f'AR{h}'])
            dve(lambda e, h=h: e.tensor_tensor(out=BK[:, h, :, 0:128], in0=c4(b_[:]), in1=c4(em[:]), op=ALU.mult), ['b', 'em'], [f'BK{h}'])
            dve(lambda e, h=h: e.tensor_tensor(out=BK[:, h, :, 128:256], in0=c4(kp[:]), in1=c4(em[:]), op=ALU.mult), ['kp', 'em'], [f'BK{h}'])
            dve(lambda e, h=h: e.scalar_tensor_tensor(out=u1[:], in0=r_, scalar=rkc[:, h:h + 1], in1=kp[:], op0=ALU.mult, op1=ALU.mult),
                zkeys + ['kp', 'par', 'u1'], ['u1'])
            p5, p5k = pl.next()
            mm(P, p5[:], onesf[:], u1[:], True, True, ['onesf', 'u1'], [p5k])
            dve(lambda e, p5=p5, h=h: e.tensor_tensor(out=bon[:, h, :], in0=p5[:], in1=v_, op=ALU.mult), [p5k] + zkeys, [f'bon{h}'])
            P.op('pool', lambda e: e.tensor_copy(out=vb[:], in_=v_), reads=zkeys, writes=['vb'])
            for c in range(4):
                pt_, ptk = ptr.next()
                cs_ = slice(c * 128, (c + 1) * 128)
                P.op('pe', lambda e, pt_=pt_, cs_=cs_: e.transpose(out=pt_[:, 0, :], in_=vb[:, cs_], identity=ident[0:64, 0:64]), reads=['vb'], writes=[ptk])
                P.op('pe', lambda e, pt_=pt_, h=h, c=c: e.transpose(out=pt_[:, 1, :], in_=BK[:, h, c, 0:128], identity=ident[0:64, 0:64]), reads=[f'BK{h}'], writes=[ptk])
                P.op('pe', lambda e, pt_=pt_, h=h, c=c: e.transpose(out=pt_[:, 2, :], in_=BK[:, h, c, 128:256], identity=ident[0:64, 0:64]), reads=[f'BK{h}'], writes=[ptk])
                P.op('act', lambda e, pt_=pt_, h=h, c=c: e.activation(out=tok3[:, h, c, :, :], in_=pt_[:], func=AF.Copy), reads=[ptk], writes=[f'tok{h}'])
        for h in range(8):
            z, zk = zrot.next()
            prep_head(h, z, zk)
        for c in range(4):
            for h in range(8):
                p_, pk = pg.next()
                mm(P, p_[:, 0:256], BK[:, h, c, 0:128], AR[:, h, c, :], True, True, [f'BK{h}', f'AR{h}'], [pk])
                mm(P, p_[:, 256:512], BK[:, h, c, 128:256], AR[:, h, c, :], True, True, [f'BK{h}', f'AR{h}'], [pk])
                dve(lambda e, p_=p_, h=h: e.tensor_tensor(out=Gm[:, h, :], in0=p_[:], in1=mask4[:], op=ALU.mult), [pk, 'mask4'], [f'Gm{h}'])
                p2_, p2k = pg.next()
                mm(P, p2_[:, 0:128], AR[:, h, c, 0:128], BK[:, h, c, 0:128], True, True, [f'BK{h}', f'AR{h}'], [p2k])
                dve(lambda e, p2_=p2_, h=h: e.tensor_tensor(out=XY[0][:, h, 128:256], in0=p2_[:, 0:128], in1=maskL[:], op=ALU.mult),
                    [p2k, 'maskL'], [f'XY0{h}'])
                P.op('pool', lambda e, h=h: e.tensor_copy(out=XY[0][:, h, 0:128], in_=Gm[:, h, 0:128]), reads=[f'Gm{h}'], writes=[f'XY0{h}x'])
                P.op('pool', lambda e, h=h: e.tensor_tensor(out=Nm[:, h, :], in0=Gm[:, h, 0:128], in1=ident[:], op=ALU.add),
                     reads=[f'Gm{h}'], writes=[f'N{h}'])
            for j in range(6):
                cur, nxt = XY[j % 2], XY[(j + 1) % 2]
                ck, nk_ = f'XY{j % 2}', f'XY{(j + 1) % 2}'
                for h in range(8):
                    p_, pk = pg.next()
                    rk_ = [ck + f'{h}', ck + f'{h}x']
                    mm(P, p_[:, 0:128], cur[:, h, 128:256], cur[:, h, 0:128], True, True, rk_, [pk])
                    mm(P, p_[:, 128:256], cur[:, h, 0:128], cur[:, h, 128:256], True, True, rk_, [pk])
                    P.op('act', lambda e, p_=p_, nxt=nxt, h=h: e.activation(out=nxt[:, h, :], in_=p_[:, 0:256], func=AF.Copy),
                         reads=[pk], writes=[nk_ + f'{h}', nk_ + f'{h}x'])
                for h in range(8):
                    p_, pk = pg.next()
                    mm(P, p_[:, 0:128], nxt[:, h, 128:256], Nm[:, h, :], True, True, [nk_ + f'{h}', nk_ + f'{h}x', f'N{h}'], [pk])
                    dve(lambda e, p_=p_, h=h: e.tensor_tensor(out=Nm[:, h, :], in0=p_[:, 0:128], in1=Nm[:, h, :], op=ALU.add), [pk, f'N{h}'], [f'N{h}'])
            for h in range(8):
                p_, pk = psm.next()
                Th = Tst[:, h, :]
                mm(P, p_[:, 0:64], AR[:, h, c, 0:128], Th, True, False, [f'AR{h}', f'T{h}'], [pk])
                mm(P, p_[:, 0:64], Gm[:, h, 256:384], tok3[:, h, c, 0, :], False, True, [f'Gm{h}', f'tok{h}'], [pk])
                P.op('act', lambda e, p_=p_, h=h: e.activation(out=Wsb[:, h, :], in_=p_[:, 0:64], func=AF.Copy), reads=[pk], writes=[f'W{h}'])
                p_, pk = psm.next()
                mm(P, p_[:, 0:64], Nm[:, h, :], Wsb[:, h, :], True, True, [f'N{h}', f'W{h}'], [pk])
                P.op('act', lambda e, p_=p_, h=h: e.activation(out=Usb[:, h, :], in_=p_[:, 0:64], func=AF.Copy), reads=[pk], writes=[f'U{h}'])
                p_, pk = psm.next()
                mm(P, p_[0:64, 0:128], Th, AR[:, h, c, 128:256], True, False, [f'AR{h}', f'T{h}'], [pk])
                mm(P, p_[0:64, 0:128], Usb[:, h, :], Gm[:, h, 128:256], False, False, [f'U{h}', f'Gm{h}'], [pk])
                mm(P, p_[0:64, 0:128], tok3[:, h, c, 0, :], Gm[:, h, 384:512], False, True, [f'tok{h}', f'Gm{h}'], [pk])
                P.op('act', lambda e, p_=p_, h=h, c=c: e.activation(out=yT[:, h, c * 128:(c + 1) * 128], in_=p_[0:64, 0:128], func=AF.Copy),
                     reads=[pk], writes=[f'yT{h}'])
                p_, pk = psm.next()
                mm(P, p_[0:64, 0:64], ident[0:64, 0:64], Th, True, False, [f'T{h}'], [pk])
                mm(P, p_[0:64, 0:64], tok3[:, h, c, 1, :], Usb[:, h, :], False, False, [f'tok{h}', f'U{h}'], [pk])
                mm(P, p_[0:64, 0:64], tok3[:, h, c, 2, :], tok3[:, h, c, 0, :], False, True, [f'tok{h}'], [pk])
                dve(lambda e, p_=p_, h=h, c=c: e.tensor_scalar(out=Tst[:, h, :], in0=p_[0:64, 0:64], scalar1=pC[:, h, c:c + 1], scalar2=None, op0=ALU.mult),
                    [pk, 'pC', f'T{h}'], [f'T{h}'])
        for h in range(8):
            u1, u2 = hb['u1'], hb['t']
            p1, p1k = pl.next()
            mm(P, p1[:], onesm[:], yT[:, h, :], True, True, ['onesm', f'yT{h}'], [p1k])
            dve(lambda e, p1=p1, h=h: e.tensor_tensor(out=u1[:], in0=yT[:, h, :], in1=p1[:], op=ALU.subtract), [p1k, f'yT{h}', 'u1'], ['u1'])
            P.op('act', lambda e: e.activation(out=u2[:], in_=u1[:], func=AF.Square), reads=['u1', 't'], writes=['t'])
            p2, p2k = pl.next()
            mm(P, p2[:], onesm[:], u2[:], True, True, ['onesm', 't'], [p2k])
            P.op('act', lambda e, p2=p2: e.activation(out=u2[:], in_=p2[:], func=AF.Sqrt, bias=gneps[:, 0:1]), reads=[p2k, 'gneps', 't'], writes=['t'])
            dve(lambda e: e.reciprocal(out=u2[:], in_=u2[:]), ['t'], ['t'])
            dve(lambda e: e.tensor_tensor(out=u1[:], in0=u1[:], in1=u2[:], op=ALU.mult), ['u1', 't'], ['u1'])
            dve(lambda e, h=h: e.tensor_scalar(out=u1[:], in0=u1[:], scalar1=lgc[:, h:h + 1], scalar2=lbc[:, h:h + 1], op0=ALU.mult, op1=ALU.add),
                ['u1', 'par'], ['u1'])
            dve(lambda e, h=h: e.tensor_tensor(out=u1[:], in0=u1[:], in1=bon[:, h, :], op=ALU.add), ['u1', f'bon{h}'], ['u1'])
            dve(lambda e, h=h: e.tensor_tensor(out=RW[:, h, :], in0=u1[:], in1=gg[:, h, :], op=ALU.mult), ['u1', f'gg{h}'], ['RW'])
        P.dma('sp', lambda e, t0=t0: e.dma_start(out=rwT[:, :, t0:t0 + 512], in_=RW[:]), reads=['RW'])
        if tg == 0 and 'dbg_y' in T:
            P.dma('sp', lambda e: e.dma_start(out=T['dbg_y'], in_=yT[:]), reads=[f'yT{h}' for h in range(8)])
            P.dma('sp', lambda e: e.dma_start(out=T['dbg_bon'], in_=bon[:]), reads=[f'bon{h}' for h in range(8)])
            P.dma('sp', lambda e: e.dma_start(out=T['dbg_g'], in_=gg[:]), reads=[f'gg{h}' for h in range(8)])
            P.dma('sp', lambda e: e.dma_start(out=T['dbg_AR'], in_=AR[:]), reads=[f'AR{h}' for h in range(8)])
            P.dma('sp', lambda e: e.dma_start(out=T['dbg_BK'], in_=BK[:]), reads=[f'BK{h}' for h in range(8)])


def phase_F(P, T, G):
    wa, wr, wo = P.sb([128, 4, D], BF16), P.sb([128, 4, D], BF16), P.sb([128, 8, D], BF16)
    P.dma('pool', lambda e: e.dma_start(out=wa[:], in_=T['w_attn_br'].rearrange("(j p) d -> p j d", p=128)), writes=['wa'])
    P.dma('pool', lambda e: e.dma_start(out=wr[:], in_=T['w_rwkv_br'].rearrange("(j p) d -> p j d", p=128)), writes=['wr'])
    P.dma('pool', lambda e: e.dma_start(out=wo[:], in_=T['w_out'].rearrange("(j p) d -> p j d", p=128)), writes=['wo'])
    W = dict(junk=P.sb([128, D], F32), ss=P.sb([128, 1], F32), rs=P.sb([128, 1], F32), t1=P.sb([128, D], F32),
             hb=P.sb([128, D], BF16), pt=P.ps([128, 8, 128], BF16))
    W['mod'] = P.sb([128, 3072], F32)
    P.dma('sp', lambda e: e.dma_start(out=W['mod'][:], in_=T['modd'][:, 2048:5120]), writes=['modl'])
    xrot = Rot(P, 2, [128, D], F32, 'x')
    atr, rtr = Rot(P, 2, [128, 4, 128], BF16, 'at'), Rot(P, 2, [128, 4, 128], BF16, 'rt')
    gar, grr = Rot(P, 2, [128, 8, 128], BF16, 'ga'), Rot(P, 2, [128, 8, 128], BF16, 'gr')
    sga, sgr = P.sb([128, 8, 128], F32), P.sb([128, 8, 128], F32)
    m1, m2 = P.sb([128, 4, 128], F32), P.sb([128, 4, 128], F32)
    mixT = P.sb([128, 8, 128], BF16)
    x1t = P.sb([128, D], F32)
    h2t = P.sb([128, 8, 128], BF16)
    pA = Rot(P, 1, [128, 4, 128], F32, 'pA', psum=True)
    pR = Rot(P, 1, [128, 4, 128], F32, 'pR', psum=True)
    po = Rot(P, 2, [128, 512], F32, 'po', psum=True)
    aT = T['attnT'].rearrange("(j p) t -> p j t", p=128)
    rT = T['rwT'].rearrange("(j p) t -> p j t", p=128)
    gaT = T['zga'].rearrange("(j p) t -> p j t", p=128)
    grT = T['zgr'].rearrange("(j p) t -> p j t", p=128)
    h2T = T['h2T'].rearrange("(k p) t -> p k t", p=128)
    for i in range(NT):
        tsl = slice(i * 128, (i + 1) * 128)
        xt, xk = xrot.next()
        at, atk = atr.next(); rt, rtk = rtr.next(); ga, gak = gar.next(); gr, grk = grr.next()
        P.dma('sp', lambda e, xt=xt, tsl=tsl: e.dma_start(out=xt[:], in_=T['x'][tsl, :]), writes=[xk])
        P.dma('act', lambda e, at=at, tsl=tsl: e.dma_start(out=at[:], in_=aT[:, :, tsl]), writes=[atk])
        P.dma('act', lambda e, rt=rt, tsl=tsl: e.dma_start(out=rt[:], in_=rT[:, :, tsl]), writes=[rtk])
        P.dma('sp', lambda e, ga=ga, tsl=tsl: e.dma_start(out=ga[:], in_=gaT[:, :, tsl]), writes=[gak])
        P.dma('sp', lambda e, gr=gr, tsl=tsl: e.dma_start(out=gr[:], in_=grT[:, :, tsl]), writes=[grk])
        P.op('act', lambda e, ga=ga: e.activation(out=sga[:], in_=ga[:], func=AF.Sigmoid), reads=[gak], writes=['sga'])
        P.op('act', lambda e, gr=gr: e.activation(out=sgr[:], in_=gr[:], func=AF.Sigmoid), reads=[grk], writes=['sgr'])
        for half in range(2):
            pa, pak = pA.next(); pr, prk = pR.next()
            for s_ in range(4):
                dt = half * 4 + s_
                for j in range(4):
                    mm(P, pa[:, s_, :], wa[:, j, dt * 128:(dt + 1) * 128], at[:, j, :], j == 0, j == 3, ['wa', atk], [pak])
            for s_ in range(4):
                dt = half * 4 + s_
                for j in range(4):
                    mm(P, pr[:, s_, :], wr[:, j, dt * 128:(dt + 1) * 128], rt[:, j, :], j == 0, j == 3, ['wr', rtk], [prk])
            hs = slice(half * 4, half * 4 + 4)
            P.op('dve', lambda e, pa=pa, hs=hs: e.tensor_tensor(out=m1[:], in0=pa[:], in1=sga[:, hs, :], op=ALU.mult), reads=[pak, 'sga'], writes=['m1'])
            P.op('dve', lambda e, pr=pr, hs=hs: e.tensor_tensor(out=m2[:], in0=pr[:], in1=sgr[:, hs, :], op=ALU.mult), reads=[prk, 'sgr'], writes=['m2'])
            P.op('pool', lambda e, hs=hs: e.tensor_tensor(out=mixT[:, hs, :], in0=m1[:], in1=m2[:], op=ALU.add), reads=['m1', 'm2'], writes=['mixT'])
        for half in range(2):
            p_, pk = po.next()
            cs_ = slice(half * 512, (half + 1) * 512)
            for dt in range(8):
                mm(P, p_[:], mixT[:, dt, :], wo[:, dt, cs_], dt == 0, dt == 7, ['mixT', 'wo'], [pk])
            P.op('dve', lambda e, p_=p_, cs_=cs_: e.tensor_tensor(out=x1t[:, cs_], in0=p_[:], in1=W['mod'][:, cs_], op=ALU.mult),
                 reads=[pk, 'modl'], writes=['x1t'])
        P.op('pool', lambda e, xt=xt: e.tensor_tensor(out=x1t[:], in0=x1t[:], in1=xt[:], op=ALU.add), reads=['x1t', xk], writes=['x1t'])
        P.dma('sp', lambda e, tsl=tsl: e.dma_start(out=T['x1'][tsl, :], in_=x1t[:]), reads=['x1t'])
        norm_mod_transpose(P, G, x1t, 'x1t', h2t, 0, slice(2048, 3072), slice(1024, 2048), W)
        P.dma('act', lambda e, tsl=tsl: e.dma_start(out=h2T[:, :, tsl], in_=h2t[:]), reads=['hT0'])


def phase_G0(P, T, G):
    for e in range(64):
        for (src, dst) in (('exp_gate', 'wg16'), ('exp_up', 'wu16'), ('exp_down', 'wd16')):
            P.dma('pool', lambda e_, e=e, src=src, dst=dst: e_.dma_start(
                out=T[dst][e].rearrange("(a b) -> a b", b=2048), in_=T[src][e].rearrange("r c -> (r c)").rearrange("(a b) -> a b", b=2048)))
    for (src, dst) in (('sh_gate', 'wg16'), ('sh_up', 'wu16'), ('sh_down', 'wd16')):
        P.dma('pool', lambda e_, src=src, dst=dst: e_.dma_start(
            out=T[dst][64].rearrange("(a b) -> a b", b=2048), in_=T[src].rearrange("r c -> (r c)").rearrange("(a b) -> a b", b=2048)))


def phase_G(P, T, G):
    ident = G['identb']
    h2T = T['h2T'].rearrange("(k p) t -> p k t", p=128)
    yacc = G['yacc']
    gwT = P.sb([64, S], BF16)
    rwt = P.sb([128, 8, 64], BF16)
    rbias = P.sb([128, 64], F32)
    P.dma('pool', lambda e: e.dma_start(out=rwt[:], in_=T['router_w'].rearrange("(k p) n -> p k n", p=128)), writes=['rwt'])
    P.dma('sp', lambda e: e.dma_start(out=rbias[:], in_=T['router_bias'].partition_broadcast(128)), writes=['rbias'])
    ones128 = P.sb([64, 128], BF16)
    P.op('dve', lambda e: e.memset(ones128[:], 1.0), writes=['ones128'])
    hrot = Rot(P, 2, [128, 8, 256], BF16, 'h2g')
    pmisc = P.ps([128, 512], F32)
    ptb = P.ps([64, 128], BF16)
    emb = P.sb([128, 64], BF16)
    sc_, ch, tmp, cm, em = [P.sb([128, 64], F32) for _ in range(5)]
    m1, m2, grp, s8, gmask, pen, den = [P.sb([128, 8], F32) for _ in range(7)]
    dve = lambda fn, r, w: P.op('dve', fn, reads=r, writes=w)
    for tgp in range(16):
        hg, hk = hrot.next()
        P.dma('sp', lambda e, hg=hg, tgp=tgp: e.dma_start(out=hg[:], in_=h2T[:, :, tgp * 256:(tgp + 1) * 256]), writes=[hk])
        for tt in range(2):
            i = tgp * 2 + tt
            p_, pk = pmisc[:, 0:64], 'pm_a'
            for k in range(8):
                mm(P, p_, hg[:, k, tt * 128:(tt + 1) * 128], rwt[:, k, :], k == 0, k == 7, [hk, 'rwt'], [pk])
            P.op('act', lambda e, p_=p_: e.activation(out=sc_[:], in_=p_, func=AF.Sigmoid), reads=[pk], writes=['sc'])
            dve(lambda e: e.tensor_tensor(out=ch[:], in0=sc_[:], in1=rbias[:], op=ALU.add), ['sc', 'rbias'], ['ch'])
            ch3 = ch[:].rearrange("p (g e) -> p g e", g=8)
            dve(lambda e, ch3=ch3: e.tensor_reduce(out=m1[:], in_=ch3, axis=AX.X, op=ALU.max), ['ch'], ['m1'])
            for g in range(8):
                dve(lambda e, g=g: e.tensor_scalar(out=tmp[:, g * 8:(g + 1) * 8], in0=ch[:, g * 8:(g + 1) * 8], scalar1=m1[:, g:g + 1],
                                                  scalar2=-1e9, op0=ALU.is_equal, op1=ALU.mult), ['ch', 'm1', 'tmp'], ['tmp'])
            dve(lambda e: e.tensor_tensor(out=tmp[:], in0=tmp[:], in1=ch[:], op=ALU.add), ['tmp', 'ch'], ['tmp'])
            dve(lambda e: e.tensor_reduce(out=m2[:], in_=tmp[:].rearrange("p (g e) -> p g e", g=8), axis=AX.X, op=ALU.max), ['tmp'], ['m2'])
            dve(lambda e: e.tensor_tensor(out=grp[:], in0=m1[:], in1=m2[:], op=ALU.add), ['m1', 'm2'], ['grp'])
            dve(lambda e: e.max(out=s8[:], in_=grp[:]), ['grp'], ['s8'])
            dve(lambda e: e.tensor_scalar(out=gmask[:], in0=grp[:], scalar1=s8[:, 3:4], scalar2=None, op0=ALU.is_ge), ['grp', 's8'], ['gmask'])
            dve(lambda e: e.tensor_scalar(out=pen[:], in0=gmask[:], scalar1=-1.0, scalar2=1e9, op0=ALU.add, op1=ALU.mult), ['gmask'], ['pen'])
            for g in range(8):
                dve(lambda e, g=g: e.tensor_scalar(out=cm[:, g * 8:(g + 1) * 8], in0=ch[:, g * 8:(g + 1) * 8], scalar1=pen[:, g:g + 1],
                                                  scalar2=None, op0=ALU.add), ['ch', 'pen', 'cm'], ['cm'])
            dve(lambda e: e.max(out=s8[:], in_=cm[:]), ['cm', 's8'], ['s8'])
            dve(lambda e: e.tensor_scalar(out=em[:], in0=cm[:], scalar1=s8[:, 7:8], scalar2=None, op0=ALU.is_ge), ['cm', 's8'], ['em'])
            dve(lambda e: e.tensor_tensor(out=em[:], in0=em[:], in1=sc_[:], op=ALU.mult), ['em', 'sc'], ['em'])
            dve(lambda e: e.tensor_reduce(out=den[:, 0:1], in_=em[:], axis=AX.X, op=ALU.add), ['em'], ['den'])
            dve(lambda e: e.reciprocal(out=den[:, 1:2], in_=den[:, 0:1]), ['den'], ['den'])
            dve(lambda e: e.tensor_scalar(out=em[:], in0=em[:], scalar1=den[:, 1:2], scalar2=2.5, op0=ALU.mult, op1=ALU.mult), ['em', 'den'], ['em'])
            pt_, ptk = ptb[:], 'pm_b'
            dve(lambda e: e.tensor_copy(out=emb[:], in_=em[:]), ['em', 'emb'], ['emb'])
            P.op('pe', lambda e, pt_=pt_: e.transpose(out=pt_, in_=emb[:], identity=G['identb'][:]), reads=['emb'], writes=[ptk])
            P.op('act', lambda e, pt_=pt_, i=i: e.activation(out=gwT[:, i * 128:(i + 1) * 128], in_=pt_, func=AF.Copy), reads=[ptk], writes=['gwT'])
    if G.get('gstop') == 'router':
        return
    wgr = Rot(P, 2, [128, 8, 256], BF16, 'wg')
    wur = Rot(P, 2, [128, 8, 256], BF16, 'wu')
    wdr = Rot(P, 2, [128, 2, D], BF16, 'wd')
    selr = Rot(P, 2, [64, 128], BF16, 'sel')
    pgu = Rot(P, 2, [128, 4, 256], F32, 'pgu', psum=True)
    py = Rot(P, 2, [128, 512], F32, 'py', psum=True)
    sgr_ = Rot(P, 2, [128, 2, 256], F32, 'sg')
    tr_ = Rot(P, 2, [128, 2, 256], F32, 'tt')
    actr = Rot(P, 2, [128, 2, 256], BF16, 'act')
    for e_ in range(G.get('nexp', 65)):
        wg, wgk = wgr.next(); wu, wuk = wur.next(); wd, wdk = wdr.next()
        if e_ < 64:
            sg_, su_, sd_ = T['exp_gate'][e_], T['exp_up'][e_], T['exp_down'][e_]
        else:
            sg_, su_, sd_ = T['sh_gate'], T['sh_up'], T['sh_down']
        P.dma('pool', lambda e, wg=wg, sg_=sg_: e.dma_start(out=wg[:], in_=sg_.rearrange("(k p) f -> p k f", p=128)), writes=[wgk])
        P.dma('pool', lambda e, wu=wu, su_=su_: e.dma_start(out=wu[:], in_=su_.rearrange("(k p) f -> p k f", p=128)), writes=[wuk])
        P.dma('pool', lambda e, wd=wd, sd_=sd_: e.dma_start(out=wd[:], in_=sd_.rearrange("(k p) f -> p k f", p=128)), writes=[wdk])
        if e_ < 64:
            sel, selk = selr.next()
            P.op('pool', lambda e, sel=sel, e_=e_: e.tensor_scalar(out=sel[:], in0=ones128[:], scalar1=G['identf'][0:64, e_:e_ + 1], scalar2=None, op0=ALU.mult),
                 reads=['ones128'], writes=[selk])
        for tgp in range(16):
            hg, hk = hrot.next()
            P.dma('sp' if tgp % 2 == 0 else 'act', lambda e, hg=hg, tgp=tgp: e.dma_start(out=hg[:], in_=h2T[:, :, tgp * 256:(tgp + 1) * 256]), writes=[hk])
            p_, pk = pgu.next()
            for s_, (w_, wk_) in enumerate(((wg, wgk), (wg, wgk), (wu, wuk), (wu, wuk))):
                ft = s_ % 2
                for k in range(8):
                    mm(P, p_[:, s_, :], w_[:, k, ft * 128:(ft + 1) * 128], hg[:, k, :], k == 0, k == 7, [wk_, hk], [pk])
            sg, sgk = sgr_.next(); t_, tk = tr_.next(); ac, ack = actr.next()
            P.op('act', lambda e, sg=sg, p_=p_: e.activation(out=sg[:], in_=p_[:, 0:2, :], func=AF.Silu), reads=[pk], writes=[sgk])
            P.op('dve', lambda e, sg=sg, p_=p_, t_=t_: e.tensor_tensor(out=t_[:], in0=p_[:, 2:4, :], in1=sg[:], op=ALU.mult), reads=[pk, sgk], writes=[tk])
            if e_ < 64:
                pw, pwk = pmisc[:, 256:512], 'pm_c'
                mm(P, pw, sel[:], gwT[:, tgp * 256:(tgp + 1) * 256], True, True, [selk, 'gwT'], [pwk])
                for ft in range(2):
                    P.op('dve', lambda e, ac=ac, t_=t_, pw=pw, ft=ft: e.tensor_tensor(out=ac[:, ft, :], in0=pw, in1=t_[:, ft, :], op=ALU.mult),
                         reads=[tk, pwk], writes=[ack])
            else:
                P.op('pool', lambda e, ac=ac, t_=t_: e.tensor_copy(out=ac[:], in_=t_[:]), reads=[tk], writes=[ack])
            for tt in range(2):
                i = tgp * 2 + tt
                for half in range(2):
                    q_, qk = py.next()
                    cs_ = slice(half * 512, (half + 1) * 512)
                    for ft in range(2):
                        mm(P, q_[:], ac[:, ft, tt * 128:(tt + 1) * 128], wd[:, ft, cs_], ft == 0, ft == 1, [ack, wdk], [qk])
                    if e_ == 0:
                        P.op('act', lambda e, q_=q_, i=i, cs_=cs_: e.activation(out=yacc[:, i, cs_], in_=q_[:], func=AF.Copy), reads=[qk], writes=[f'y{i}'])
                    else:
                        P.op('dve', lambda e, q_=q_, i=i, cs_=cs_: e.tensor_tensor(out=yacc[:, i, cs_], in0=q_[:], in1=yacc[:, i, cs_], op=ALU.add),
                             reads=[qk, f'y{i}'], writes=[f'y{i}'])


def phase_H(P, T, G):
    yacc = G['yacc']
    dve = lambda fn, r, w: P.op('dve', fn, reads=r, writes=w)
    g2b = P.sb([128, D], F32)
    fing = P.sb([128, D], F32)
    P.dma('sp', lambda e: e.dma_start(out=g2b[:], in_=T['modd'][:, 5120:6144]), writes=['g2b'])
    P.dma('sp', lambda e: e.dma_start(out=fing[:], in_=T['final_g'].partition_broadcast(128)), writes=['fing'])
    xr = Rot(P, 2, [128, D], F32, 'x1')
    junk, ss, rs = P.sb([128, D], F32), P.sb([128, 1], F32), P.sb([128, 1], F32)
    for i in range(NT):
        tsl = slice(i * 128, (i + 1) * 128)
        xt, xk = xr.next()
        P.dma('sp', lambda e, xt=xt, tsl=tsl: e.dma_start(out=xt[:], in_=T['x1'][tsl, :]), writes=[xk])
        dve(lambda e, i=i: e.tensor_tensor(out=yacc[:, i, :], in0=yacc[:, i, :], in1=g2b[:], op=ALU.mult), [f'y{i}', 'g2b'], [f'y{i}'])
        P.op('pool', lambda e, i=i, xt=xt: e.tensor_tensor(out=xt[:], in0=xt[:], in1=yacc[:, i, :], op=ALU.add), reads=[f'y{i}', xk], writes=[xk])
        rms_rstd(P, xt, xk, junk, ss, rs, 'f')
        dve(lambda e, xt=xt: e.scalar_tensor_tensor(out=xt[:], in0=xt[:], scalar=rs[:, 0:1], in1=fing[:], op0=ALU.mult, op1=ALU.mult),
            [xk, 'rsf', 'fing'], [xk])
        P.dma('sp', lambda e, xt=xt, tsl=tsl: e.dma_start(out=T['out'][tsl, :], in_=xt[:]), reads=[xk])


SCRATCH = [
    ('zq', [512, S], BF16), ('zk', [512, S], BF16), ('zv', [S, 512], BF16), ('ziq', [512, S], BF16),
    ('zik', [32, S], BF16), ('ziw', [S, 16], F32), ('zr', [1792, S], F32), ('zga', [1024, S], BF16),
    ('zgr', [1024, S], BF16), ('modd', [128, 6 * D], F32), ('attnT', [512, S], BF16), ('rwT', [512, S], BF16),
    ('x1', [S, D], F32), ('h2T', [D, S], BF16),
]

INPUT_SHAPES = [
    ('x', [S, D]), ('c_col', [128, 8]), ('ada_w', [D, 6 * D]), ('ada_b', [1, 6 * D]), ('norm1_g', [D]),
    ('w_in', [D, NIN]), ('rel_bias', [256]), ('tshift_mu', [1792]), ('decay_w0', [512]), ('decay_up', [64, 512]),
    ('iclr_a0', [512]), ('iclr_up', [64, 512]), ('gate_up', [128, 512]), ('k_k', [512]), ('k_a', [512]),
    ('r_k', [512]), ('lnx_g', [512]), ('lnx_b', [512]), ('w_attn_br', [512, D]), ('w_rwkv_br', [512, D]),
    ('w_out', [D, D]), ('norm2_g', [D]), ('router_w', [D, 64]), ('router_bias', [64]),
    ('exp_gate', [64, D, 256]), ('exp_up', [64, D, 256]), ('exp_down', [64, 256, D]),
    ('sh_gate', [D, 256]), ('sh_up', [D, 256]), ('sh_down', [256, D]), ('final_g', [D]),
    ('k_ident', [128, 128]), ('k_rel', [128, 256]), ('k_masks', [128, 896]), ('k_reset', [64, 512]),
]


def build(debug_outs=(), stop_after=None):
    nc = bass.Bass("TRN2", target_bir_lowering=False)
    T = {}
    for name, shp in INPUT_SHAPES:
        T[name] = nc.dram_tensor(name, shp, F32, kind="ExternalInput").ap()
    for name, shp, dt in SCRATCH:
        kind = "ExternalOutput" if name in debug_outs else "Internal"
        T[name] = nc.dram_tensor(name, shp, dt, kind=kind).ap()
    T['out'] = nc.dram_tensor('out', [S, D], F32, kind="ExternalOutput").ap()
    if 'dbg_y' in debug_outs:
        T['dbg_y'] = nc.dram_tensor('dbg_y', [64, 8, 512], F32, kind="ExternalOutput").ap()
        T['dbg_bon'] = nc.dram_tensor('dbg_bon', [64, 8, 512], F32, kind="ExternalOutput").ap()
        T['dbg_g'] = nc.dram_tensor('dbg_g', [64, 8, 512], BF16, kind="ExternalOutput").ap()
        T['dbg_AR'] = nc.dram_tensor('dbg_AR', [64, 8, 4, 256], BF16, kind="ExternalOutput").ap()
        T['dbg_BK'] = nc.dram_tensor('dbg_BK', [64, 8, 4, 256], BF16, kind="ExternalOutput").ap()
    P = Prog(nc)
    G = {}
    G['E'] = P.gsb([128, 8, 256], F32)
    G['b31'] = P.gsb([128, 8], F32)
    G['ones_row'] = P.gsb([1, 128], F32)
    G['identf'] = P.gsb([128, 128], F32)
    G['identb'] = P.gsb([128, 128], BF16)
    G['eps'] = P.gsb([128, 1], F32)
    G_EPS[0] = G['eps']
    P.op('dve', lambda e: e.memset(G['ones_row'][:], 1.0), writes=['ones_row'])
    P.op('dve', lambda e: e.memset(G['eps'][:], 1e-6), writes=['eps'])
    P.dma('sp', lambda e: e.dma_start(out=G['identf'][:], in_=T['k_ident']), writes=['identf'])
    P.op('dve', lambda e: e.tensor_copy(out=G['identb'][:], in_=G['identf'][:]), reads=['identf'], writes=['identb'])
    phase_A(P, T, G)
    P.emit()
    if stop_after == 'A':
        P.emit(); P.finish(); return nc
    phase_BC(P, T, G)
    P.emit()
    if stop_after == 'C':
        P.finish(); return nc
    phase_D0(P, T, G)
    P.emit()
    phase_D(P, T, G)
    P.emit()
    if stop_after == 'D':
        P.finish(); return nc
    phase_E(P, T, G)
    P.emit()
    if stop_after == 'E':
        P.finish(); return nc
    phase_F(P, T, G)
    P.emit()
    if stop_after == 'F':
        P.finish(); return nc
    G['yacc'] = P.gsb([128, NT, D], F32)
    if stop_after in ('router', 'exp1'):
        G['gstop'] = stop_after
        G['nexp'] = 1
    if stop_after == 'router':
        phase_G(P, T, G); P.emit(); P.finish(); return nc
    if stop_after == 'exp1':
        phase_G(P, T, G); P.emit(); P.finish(); return nc
    phase_G(P, T, G)
    P.emit()
    phase_H(P, T, G)
    P.emit()
    P.finish()
    return nc


def host_inputs(inputs, b):
    m = {}
    f = lambda a: np.ascontiguousarray(np.asarray(a, dtype=np.float32))
    m['x'] = f(inputs['x'][b])
    m['c_col'] = f(np.asarray(inputs['c'][b]).reshape(8, 128).T)
    for name, shp in INPUT_SHAPES:
        if name in ('x', 'c_col', 'k_ident', 'k_rel', 'k_masks', 'k_reset'):
            continue
        a = np.asarray(inputs[name])
        m[name] = f(a.reshape(shp))
    m['k_ident'] = np.eye(128, dtype=np.float32)
    ii = np.arange(128)
    su = (ii[:, None] < ii[None, :]).astype(np.float32)
    iu = (ii[:, None] <= ii[None, :]).astype(np.float32)
    sl = (ii[:, None] > ii[None, :]).astype(np.float32)
    m['k_masks'] = np.ascontiguousarray(np.concatenate([su, iu, su, iu, sl, np.zeros((128, 256), np.float32)], axis=1))
    rs_ = np.ones((64, 512), np.float32); rs_[:, ::128] = 0.0
    m['k_reset'] = rs_
    m['k_rel'] = (np.arange(256, dtype=np.float32)[None, :] - np.arange(128, dtype=np.float32)[:, None])
    return m


def kernel(**inputs):
    nc = build()
    in_maps = [host_inputs(inputs, b) for b in range(8)]
    res = run_bass_kernel_spmd(nc, in_maps, core_ids=list(range(8)))
    return np.stack([np.asarray(r['out']) for r in res.results], axis=0).astype(np.float32)
```

```python
import contextlib
import numpy as np
import concourse.bass as bass
import concourse.mybir as mybir

F32 = mybir.dt.float32
BF16 = mybir.dt.bfloat16
I32 = mybir.dt.int32
U32 = mybir.dt.uint32
AF = mybir.ActivationFunctionType
ALU = mybir.AluOpType
AX = mybir.AxisListType

N_DMA_SEMS = 6


class Prog:
    ENGS = ('pe', 'act', 'dve', 'pool', 'sp')

    def __init__(self, nc):
        self.nc = nc
        self.ops = {e: [] for e in self.ENGS}
        self.cnt = {e: 0 for e in self.ENGS}
        self.waited = {e: {} for e in self.ENGS}
        self.res = {}
        self.dma_tot = [0] * N_DMA_SEMS
        self.dma_rr = 0
        self.stack = contextlib.ExitStack()
        self.gstack = contextlib.ExitStack()
        self.nsb = 0
        self.nphase = 0
        self.sems = None

    def _new_sems(self):
        nc = self.nc
        self.sems = {}
        for e in self.ENGS:
            self.sems[e] = self.gstack.enter_context(nc.semaphore(f"s_{e}_{self.nphase}"))
        for i in range(N_DMA_SEMS):
            self.sems[('dma', i)] = self.gstack.enter_context(nc.semaphore(f"s_dma{i}_{self.nphase}"))
        self.cnt = {e: 0 for e in self.ENGS}
        self.waited = {e: {} for e in self.ENGS}
        self.dma_tot = [0] * N_DMA_SEMS
        self.dma_rr = 0

    def gsb(self, shape, dt, name=None):
        self.nsb += 1
        return self.gstack.enter_context(self.nc.sbuf_tensor(name or f"gsb{self.nsb}", list(shape), dt))

    def sb(self, shape, dt, name=None):
        self.nsb += 1
        return self.stack.enter_context(self.nc.sbuf_tensor(name or f"sb{self.nsb}", list(shape), dt))

    def ps(self, shape, dt, name=None):
        self.nsb += 1
        return self.stack.enter_context(self.nc.psum_tensor(name or f"ps{self.nsb}", list(shape), dt))

    def _deps(self, reads, writes):
        deps = {}
        def add(d):
            if d is None:
                return
            k, v = d
            if deps.get(k, 0) < v:
                deps[k] = v
        for k in reads:
            r = self.res.get(k)
            if r:
                add(r['w'])
        for k in writes:
            r = self.res.get(k)
            if r:
                add(r['w'])
                for d in r['r']:
                    add(d)
        return deps

    def _emit_waits(self, eng, deps):
        w = self.waited[eng]
        for k, v in deps.items():
            if w.get(k, 0) < v:
                w[k] = v
                self.ops[eng].append(('wait', k, v))

    def _update(self, dep, reads, writes):
        for k in reads:
            r = self.res.setdefault(k, {'w': None, 'r': []})
            r['r'] = [d for d in r['r'] if d[0] != dep[0]] + [dep]
        for k in writes:
            self.res[k] = {'w': dep, 'r': []}

    def op(self, eng, fn, reads=(), writes=()):
        if self.sems is None:
            self._new_sems()
        deps = self._deps(reads, writes)
        if eng == 'pe':
            deps.pop('pe', None)
        self._emit_waits(eng, deps)
        self.cnt[eng] += 1
        dep = (eng, self.cnt[eng])
        self.ops[eng].append(('op', fn))
        self._update(dep, reads, writes)
        return dep

    def dma(self, q, fn, reads=(), writes=()):
        if self.sems is None:
            self._new_sems()
        deps = self._deps(reads, writes)
        s = self.dma_rr
        self.dma_rr = (self.dma_rr + 1) % N_DMA_SEMS
        key = ('dma', s)
        if self.dma_tot[s] > 0:
            if deps.get(key, 0) < self.dma_tot[s]:
                deps[key] = self.dma_tot[s]
        self._emit_waits(q, deps)
        self.dma_tot[s] += 16
        dep = (key, self.dma_tot[s])
        self.ops[q].append(('dma', fn, s))
        self._update(dep, reads, writes)
        return dep

    def emit(self):
        nc = self.nc
        with contextlib.ExitStack() as st:
            sems = self.sems
            self.nphase += 1
            block = st.enter_context(nc.Block(f"ph{self.nphase}"))
            engobj = {'pe': nc.tensor, 'act': nc.scalar, 'dve': nc.vector, 'pool': nc.gpsimd, 'sp': nc.sync}
            fin = {}
            for e in self.ENGS:
                if e != 'sp' and self.cnt[e] > 0:
                    fin[e] = self.cnt[e]
            for i in range(N_DMA_SEMS):
                if self.dma_tot[i] > 0:
                    fin[('dma', i)] = self.dma_tot[i]
            self._emit_waits('sp', fin)

            def run(e):
                eo = engobj[e]
                for item in self.ops[e]:
                    if item[0] == 'wait':
                        eo.wait_ge(sems[item[1]], item[2])
                    elif item[0] == 'op':
                        item[1](eo).then_inc(sems[e], 1)
                    else:
                        item[1](eo).then_inc(sems[('dma', item[2])], 16)

            @block.tensor
            def _(t):
                run('pe')

            @block.scalar
            def _(t):
                run('act')

            @block.vector
            def _(t):
                run('dve')

            @block.gpsimd
            def _(t):
                run('pool')

            @block.sync
            def _(t):
                run('sp')
        self.stack.close()
        self.stack = contextlib.ExitStack()
        self.ops = {e: [] for e in self.ENGS}
        self.res = {}
        self.sems = None

    def finish(self):
        self.gstack.close()

from concourse.bass_utils import run_bass_kernel_spmd
import ml_dtypes

S = 4096
D = 1024
NT = S // 128
NIN = 5936


class Rot:
    def __init__(self, P, n, shape, dt, name, psum=False):
        self.tiles = [(P.ps(shape, dt) if psum else P.sb(shape, dt)) for _ in range(n)]
        self.name = name
        self.i = 0

    def next(self):
        t = self.tiles[self.i % len(self.tiles)]
        k = f"{self.name}{self.i % len(self.tiles)}"
        self.i += 1
        return t, k


def mm(P, out, lhsT, rhs, start, stop, reads, writes):
    P.op('pe', lambda e: e.matmul(out, lhsT=lhsT, rhs=rhs, start=start, stop=stop), reads=reads, writes=writes)


def phase_A(P, T, G):
    mod_bc = P.sb([128, 6 * D], F32)
    ccol = P.sb([128, 8], F32)
    scol = P.sb([128, 8], F32)
    adab = P.sb([1, 6144], F32)
    modrow = P.sb([1, 6144], F32)
    ngb = P.sb([128, 1024], F32)
    P.dma('sp', lambda e: e.dma_start(out=ccol[:], in_=T['c_col']), writes=['ccol'])
    P.dma('sp', lambda e: e.dma_start(out=adab[:], in_=T['ada_b']), writes=['adab'])
    P.op('act', lambda e: e.activation(out=scol[:], in_=ccol[:], func=AF.Silu), reads=['ccol'], writes=['scol'])
    wrot = Rot(P, 2, [128, 8, 512], F32, 'aw')
    psr = Rot(P, 2, [1, 512], F32, 'psr', psum=True)
    psb = Rot(P, 2, [128, 512], F32, 'psb', psum=True)
    adaw = T['ada_w'].rearrange("(k p) n -> p k n", p=128)
    for n in range(12):
        wb, wk = wrot.next()
        P.dma('sp' if n % 2 == 0 else 'act',
              lambda e, wb=wb, n=n: e.dma_start(out=wb[:], in_=adaw[:, :, n * 512:(n + 1) * 512]), writes=[wk])
        pr, pk = psr.next()
        for k in range(8):
            mm(P, pr[:], scol[:, k:k + 1], wb[:, k, :], k == 0, k == 7, [wk, 'scol'], [pk])
        sl = slice(n * 512, (n + 1) * 512)
        P.op('dve', lambda e, pr=pr, sl=sl: e.tensor_tensor(out=modrow[0:1, sl], in0=pr[:], in1=adab[0:1, sl], op=ALU.add),
             reads=[pk, 'adab'], writes=[f'modrow{n}'])
        pb, pbk = psb.next()
        mm(P, pb[:], G['ones_row'][:], modrow[0:1, sl], True, True, [f'modrow{n}'], [pbk])
        P.op('act', lambda e, pb=pb, sl=sl: e.activation(out=mod_bc[:, sl], in_=pb[:], func=AF.Copy),
             reads=[pbk], writes=[f'mod{n}'])
    for (gname, c0, deps) in (('norm1_g', 1024, ['mod2', 'mod3']), ('norm2_g', 4096, ['mod8', 'mod9'])):
        P.dma('sp', lambda e, gname=gname: e.dma_start(out=ngb[:], in_=T[gname].partition_broadcast(128)), writes=['ngb'])
        P.op('dve', lambda e, c0=c0: e.scalar_tensor_tensor(out=mod_bc[:, c0:c0 + 1024], in0=mod_bc[:, c0:c0 + 1024],
                                                            scalar=1.0, in1=ngb[:], op0=ALU.add, op1=ALU.mult),
             reads=deps + ['ngb'], writes=deps)
    P.dma('sp', lambda e: e.dma_start(out=T['modd'], in_=mod_bc[:]), reads=[f'mod{n}' for n in range(12)])


def rms_rstd(P, xt, xk, junk, ss, rs, tag):
    P.op('act', lambda e: e.activation(out=junk[:], in_=xt[:], func=AF.Square, accum_out=ss[:]),
         reads=[xk], writes=['junk' + tag, 'ss' + tag])
    P.op('act', lambda e: e.activation(out=ss[:], in_=ss[:], func=AF.Sqrt, scale=1.0 / D, bias=G_EPS[0][:, 0:1]),
         reads=['ss' + tag], writes=['ss' + tag])
    P.op('dve', lambda e: e.reciprocal(out=rs[:], in_=ss[:]), reads=['ss' + tag], writes=['rs' + tag])


G_EPS = [None]


def norm_mod_transpose(P, G, xt, xk, hT, i, g_sl, sh_sl, W):
    mod_bc = W['mod']
    junk, ss, rs, t1, hb, pt = W['junk'], W['ss'], W['rs'], W['t1'], W['hb'], W['pt']
    rms_rstd(P, xt, xk, junk, ss, rs, '')
    P.op('dve', lambda e: e.scalar_tensor_tensor(out=t1[:], in0=xt[:], scalar=rs[:, 0:1], in1=mod_bc[:, g_sl],
                                                 op0=ALU.mult, op1=ALU.mult), reads=[xk, 'rs', 'modl'], writes=['t1'])
    P.op('pool', lambda e: e.tensor_tensor(out=hb[:], in0=t1[:], in1=mod_bc[:, sh_sl], op=ALU.add),
         reads=['t1', 'modl'], writes=['hb'])
    for k in range(8):
        P.op('pe', lambda e, k=k: e.transpose(out=pt[:, k, :], in_=hb[:, k * 128:(k + 1) * 128], identity=G['identb'][:]),
             reads=['hb'], writes=['pt'])
    P.op('act', lambda e: e.activation(out=hT[:, :, i * 128:(i + 1) * 128], in_=pt[:], func=AF.Copy),
         reads=['pt'], writes=[f'hT{i // 4}'])


def phase_BC(P, T, G):
    hT = P.sb([128, 8, S], BF16)
    W = dict(junk=P.sb([128, D], F32), ss=P.sb([128, 1], F32), rs=P.sb([128, 1], F32), t1=P.sb([128, D], F32),
             hb=P.sb([128, D], BF16), pt=P.ps([128, 8, 128], BF16))
    W['mod'] = P.sb([128, 2048], F32)
    P.dma('sp', lambda e: e.dma_start(out=W['mod'][:], in_=T['modd'][:, 0:2048]), writes=['modl'])
    xrot = Rot(P, 2, [128, D], F32, 'x')
    for i in range(NT):
        xt, xk = xrot.next()
        P.dma('sp', lambda e, xt=xt, i=i: e.dma_start(out=xt[:], in_=T['x'][i * 128:(i + 1) * 128, :]), writes=[xk])
        norm_mod_transpose(P, G, xt, xk, hT, i, slice(1024, 2048), slice(0, 1024), W)
    win = T['w_in'].rearrange("(k p) n -> p k n", p=128)
    segs = [('zq', 0, 512, BF16), ('zk', 512, 512, BF16), ('ziq', 1536, 512, BF16), ('zik', 2048, 32, BF16),
            ('zr', 2096, 1792, F32), ('zga', 3888, 1024, BF16), ('zgr', 4912, 1024, BF16)]
    wrot = Rot(P, 2, [128, 8, 128], BF16, 'w')
    psrot = Rot(P, 3, [128, 512], F32, 'ps', psum=True)
    strot = {BF16: Rot(P, 3, [128, 512], BF16, 'stb'), F32: Rot(P, 3, [128, 512], F32, 'stf')}
    ev = 0
    for (name, c0, n, dt) in segs:
        for m0 in range(0, n, 128):
            M = min(128, n - m0)
            wt, wk = wrot.next()
            P.dma('pool', lambda e, wt=wt, M=M, a=c0 + m0: e.dma_start(out=wt[:, :, :M], in_=win[:, :, a:a + M]), writes=[wk])
            for tg in range(8):
                ps, pk = psrot.next()
                for k in range(8):
                    mm(P, ps[:M, :], wt[:, k, :M], hT[:, k, tg * 512:(tg + 1) * 512], k == 0, k == 7, [wk, f'hT{tg}'], [pk])
                st, sk = strot[dt].next()
                if ev % 2 == 0:
                    P.op('act', lambda e, st=st, ps=ps, M=M: e.activation(out=st[:M, :], in_=ps[:M, :], func=AF.Copy),
                         reads=[pk], writes=[sk])
                else:
                    P.op('dve', lambda e, st=st, ps=ps, M=M: e.tensor_copy(out=st[:M, :], in_=ps[:M, :]),
                         reads=[pk], writes=[sk])
                ev += 1
                P.dma('sp', lambda e, st=st, M=M, name=name, m0=m0, tg=tg:
                      e.dma_start(out=T[name][m0:m0 + M, tg * 512:(tg + 1) * 512], in_=st[:M, :]), reads=[sk])
    wv = P.sb([128, 8, 528], BF16)
    P.dma('pool', lambda e: e.dma_start(out=wv[:, :, 0:512], in_=win[:, :, 1024:1536]), writes=['wv'])
    P.dma('pool', lambda e: e.dma_start(out=wv[:, :, 512:528], in_=win[:, :, 2080:2096]), writes=['wv2'])
    ps2rot = Rot(P, 2, [128, 16], F32, 'ps2', psum=True)
    st2rot = Rot(P, 2, [128, 16], F32, 'st2')
    for i in range(NT):
        ps, pk = psrot.next()
        ps2, pk2 = ps2rot.next()
        for k in range(8):
            mm(P, ps[:], hT[:, k, i * 128:(i + 1) * 128], wv[:, k, 0:512], k == 0, k == 7, ['wv', f'hT{i // 4}'], [pk])
        for k in range(8):
            mm(P, ps2[:], hT[:, k, i * 128:(i + 1) * 128], wv[:, k, 512:528], k == 0, k == 7, ['wv2', f'hT{i // 4}'], [pk2])
        st, sk = strot[BF16].next()
        st2, sk2 = st2rot.next()
        P.op('act', lambda e, st=st, ps=ps: e.activation(out=st[:], in_=ps[:], func=AF.Copy), reads=[pk], writes=[sk])
        P.op('dve', lambda e, st2=st2, ps2=ps2: e.tensor_copy(out=st2[:], in_=ps2[:]), reads=[pk2], writes=[sk2])
        P.dma('sp', lambda e, st=st, i=i: e.dma_start(out=T['zv'][i * 128:(i + 1) * 128, :], in_=st[:]), reads=[sk])
        P.dma('sp', lambda e, st2=st2, i=i: e.dma_start(out=T['ziw'][i * 128:(i + 1) * 128, :], in_=st2[:]), reads=[sk2])


def t5_lo_bounds():
    n = np.arange(256)
    nf = np.maximum(n, 1).astype(np.float32)
    large = 16 + (np.log(nf / np.float32(16)) / np.float32(np.log(8.0)) * np.float32(16)).astype(np.int32)
    large = np.minimum(large, 31)
    bk = np.where(n < 16, n, large)
    return [int(np.min(np.nonzero(bk >= b)[0])) for b in range(1, 32)]


def phase_D0(P, T, G):
    relb = P.sb([128, 256], F32)
    diff = P.sb([128, 248], F32)
    base = P.sb([128, 8], F32)
    relidx = P.sb([128, 256], F32)
    P.dma('sp', lambda e: e.dma_start(out=relb[:], in_=T['rel_bias'].partition_broadcast(128)), writes=['relb'])
    P.dma('sp', lambda e: e.dma_start(out=relidx[:], in_=T['k_rel']), writes=['relidx'])
    P.op('dve', lambda e: e.tensor_tensor(out=diff[:], in0=relb[:, 8:256], in1=relb[:, 0:248], op=ALU.subtract),
         reads=['relb'], writes=['diff'])
    P.op('dve', lambda e: e.tensor_tensor(out=base[:], in0=relb[:, 0:8], in1=relb[:, 248:256], op=ALU.subtract),
         reads=['relb'], writes=['base'])
    P.op('dve', lambda e: e.tensor_copy(out=G['b31'][:], in_=relb[:, 248:256]), reads=['relb'], writes=['b31'])
    E = G['E']
    irot = Rot(P, 2, [128, 256], F32, 'ind')
    los = t5_lo_bounds()
    for b in range(1, 32):
        ind, ik = irot.next()
        P.op('dve', lambda e, ind=ind, lo=float(los[b - 1]): e.tensor_scalar(out=ind[:], in0=relidx[:], scalar1=lo, scalar2=None,
                                                                              op0=ALU.is_ge), reads=['relidx'], writes=[ik])
        for h in range(8):
            if b == 1:
                P.op('dve', lambda e, ind=ind, h=h: e.tensor_scalar(out=E[:, h, :], in0=ind[:], scalar1=diff[:, h:h + 1],
                                                                    scalar2=base[:, h:h + 1], op0=ALU.mult, op1=ALU.add),
                     reads=[ik, 'diff', 'base'], writes=[f'E{h}'])
            else:
                c = (b - 1) * 8 + h
                P.op('dve', lambda e, ind=ind, h=h, c=c: e.scalar_tensor_tensor(out=E[:, h, :], in0=ind[:], scalar=diff[:, c:c + 1],
                                                                               in1=E[:, h, :], op0=ALU.mult, op1=ALU.add),
                     reads=[ik, 'diff', f'E{h}'], writes=[f'E{h}'])
    P.op('act', lambda e: e.activation(out=E[:], in_=E[:], func=AF.Exp), reads=[f'E{h}' for h in range(8)],
         writes=[f'E{h}' for h in range(8)])


def phase_D(P, T, G):
    NIT = 16
    KT = P.sb([64, 8, S], BF16)
    V = P.sb([128, NT, 512], BF16)
    ik4 = P.sb([128, S], BF16)
    iw = P.sb([128, NT, 16], F32)
    ones64 = P.sb([128, 64], BF16)
    scs = [P.sb([128, S], F32), P.sb([128, S], F32)]
    maskbs = [P.sb([128, S], BF16), P.sb([128, S], BF16)]
    maskT = P.sb([128, NT, 128], BF16)
    lo, hi, mid, cnt, tmp, thr = [P.sb([128, 1], F32) for _ in range(6)]
    cvec = P.sb([128, NIT + 1], F32)
    dk = P.sb([128, NIT + 1], F32)
    for k in range(NIT + 1):
        P.op('pool', lambda e, k=k: e.memset(cvec[:, k:k + 1], 2.0 ** -(k + 1)), reads=['cvec'], writes=['cvec'])
    P.dma('sp', lambda e: e.dma_start(out=KT[:], in_=T['zk'].rearrange("(h p) t -> p h t", p=64)), writes=['KT'])
    P.dma('act', lambda e: e.dma_start(out=V[:], in_=T['zv'].rearrange("(i p) f -> p i f", p=128)), writes=['V'])
    for i in range(3):
        P.dma('sp', lambda e, i=i: e.dma_start(out=ik4[32 * i:32 * i + 32, :], in_=T['zik']), writes=[f'ik4{i}'])
    P.dma('sp', lambda e: e.dma_start(out=iw[:], in_=T['ziw'].rearrange("(i p) f -> p i f", p=128)), writes=['iw'])
    P.op('dve', lambda e: e.memset(ones64[:], 1.0), writes=['ones64'])
    zq = T['zq'].rearrange("(h p) t -> p h t", p=64)
    ziq = T['ziq'][0:480, :].rearrange("(j p) t -> p j t", p=96)
    attnT = T['attnT'].rearrange("(h p) t -> p h t", p=64)
    qrot = Rot(P, 2, [64, 8, 128], BF16, 'q')
    iqrot = Rot(P, 2, [96, 6, 128], BF16, 'iq')
    dgrot = Rot(P, 2, [128, 16, 128], BF16, 'dg')
    rrot = Rot(P, 4, [128, 512], BF16, 'r')
    psi = Rot(P, 2, [128, 512], F32, 'psi', psum=True)
    pacc = Rot(P, 1, [128, 512], F32, 'pacc', psum=True)
    pss = Rot(P, 3, [128, 4, 128], F32, 'pss', psum=True)
    ptm = Rot(P, 1, [128, 4, 128], BF16, 'ptm', psum=True)
    pod = Rot(P, 1, [64, 512], F32, 'pod', psum=True)
    pTrot = Rot(P, 4, [128, 4, 128], BF16, 'pT')
    atrot = Rot(P, 2, [64, 8, 128], BF16, 'at')
    rdrot = Rot(P, 2, [64, 128], F32, 'rd')
    E, b31 = G['E'], G['b31']
    identb = G['identb']

    def indexer(qi):
        sc, sck = scs[qi % 2], f'sc{qi % 2}'
        n = 128 * (qi + 1)
        tsl = slice(qi * 128, (qi + 1) * 128)
        iqt, iqk = iqrot.next()
        P.dma('act', lambda e: e.dma_start(out=iqt[:, 0:5, :], in_=ziq[:, :, tsl]), writes=[iqk])
        P.dma('act', lambda e: e.dma_start(out=iqt[0:32, 5, :], in_=T['ziq'][480:512, tsl]), writes=[iqk + 'b'])
        dg, dgk = dgrot.next()
        for h in range(16):
            P.op('pool', lambda e, h=h: e.tensor_scalar(out=dg[:, h, :], in0=identb[:], scalar1=iw[:, qi, h:h + 1], scalar2=0.0, op0=ALU.mult, op1=ALU.add),
                 reads=['iw'], writes=[dgk])
        for ch in range((n + 511) // 512):
            c0 = ch * 512
            nc_ = min(512, n - c0)
            pa, pak = pacc.next()
            pend = []

            def acc(h, r, rk):
                mm(P, pa[:, :nc_], dg[:, h, :], r[:, :nc_], h == 0, h == 15, [dgk, rk], [pak])
            for h in range(16):
                j, i = divmod(h, 3)
                ps, pk = psi.next()
                mm(P, ps[:, :nc_], iqt[32 * i:32 * i + 32, j, :], ik4[32 * i:32 * i + 32, c0:c0 + nc_], True, True,
                   [iqk, iqk + 'b', f'ik4{i}'], [pk])
                r, rk = rrot.next()
                P.op('act', lambda e, r=r, ps=ps, nc_=nc_: e.activation(out=r[:, :nc_], in_=ps[:, :nc_], func=AF.Relu), reads=[pk], writes=[rk])
                pend.append((h, r, rk))
                if len(pend) > 1:
                    acc(*pend.pop(0))
                yield
            while pend:
                acc(*pend.pop(0))
            P.op('dve', lambda e, pa=pa, c0=c0, nc_=nc_: e.tensor_copy(out=sc[:, c0:c0 + nc_], in_=pa[:, :nc_]), reads=[pak], writes=[sck])
        P.op('pool', lambda e: e.affine_select(out=sc[:, tsl], in_=sc[:, tsl], pattern=[[-1, 128]], compare_op=ALU.is_ge, fill=-1e30,
                                               base=0, channel_multiplier=1), reads=[sck], writes=[sck])

    def threshold(qi):
        sc, sck = scs[qi % 2], f'sc{qi % 2}'
        n = 128 * (qi + 1)
        maskb, mbk = maskbs[qi % 2], f'maskb{qi % 2}'
        dve = lambda fn, r, w: P.op('dve', fn, reads=r, writes=w)
        if n <= 256:
            dve(lambda e: e.memset(thr[:], -1e29), ['thr'], ['thr'])
        else:
            nv = 128 * qi
            dve(lambda e: e.tensor_reduce(out=lo[:], in_=sc[:, :nv], axis=AX.X, op=ALU.min), [sck, 'lo'], ['lo'])
            dve(lambda e: e.tensor_reduce(out=hi[:], in_=sc[:, :n], axis=AX.X, op=ALU.max), [sck, 'hi'], ['hi'])
            dve(lambda e: e.tensor_tensor(out=hi[:], in0=hi[:], in1=lo[:], op=ALU.subtract), ['hi', 'lo'], ['hi'])
            dve(lambda e: e.tensor_scalar(out=dk[:], in0=cvec[:], scalar1=hi[:, 0:1], scalar2=None, op0=ALU.mult), ['cvec', 'hi', 'dk'], ['dk'])
            dve(lambda e: e.tensor_tensor(out=mid[:], in0=lo[:], in1=dk[:, 0:1], op=ALU.add), ['lo', 'dk', 'mid'], ['mid'])
            for k in range(NIT):
                dve(lambda e: e.tensor_scalar(out=maskb[:, :n], in0=sc[:, :n], scalar1=mid[:, 0:1], scalar2=None,
                                              op0=ALU.is_ge, op1=ALU.add, accum_out=cnt[:]), [sck, 'mid', 'cnt', mbk], [mbk, 'cnt'])
                dve(lambda e: e.tensor_scalar(out=tmp[:], in0=cnt[:], scalar1=255.5, scalar2=-0.5, op0=ALU.is_ge, op1=ALU.add),
                    ['cnt', 'tmp'], ['tmp'])
                dve(lambda e, k=k: e.scalar_tensor_tensor(out=mid[:], in0=tmp[:], scalar=dk[:, k:k + 1], in1=mid[:], op0=ALU.mult, op1=ALU.add),
                    ['tmp', 'dk', 'mid'], ['mid'])
                yield
            dve(lambda e: e.tensor_tensor(out=thr[:], in0=mid[:], in1=dk[:, NIT:NIT + 1], op=ALU.subtract), ['mid', 'dk', 'thr'], ['thr'])
        dve(lambda e: e.tensor_scalar(out=maskb[:, :n], in0=sc[:, :n], scalar1=thr[:, 0:1], scalar2=None, op0=ALU.is_ge),
            [sck, 'thr', mbk], [mbk])
        yield

    def attention(qi):
        LOOK = 2
        nkt = qi + 1
        nch = (nkt + 3) // 4
        tsl = slice(qi * 128, (qi + 1) * 128)
        mb = maskbs[qi % 2]
        mbk = f'maskb{qi % 2}'
        qt, qk = qrot.next()
        P.dma('sp', lambda e: e.dma_start(out=qt[:], in_=zq[:, :, tsl]), writes=[qk])
        for c4 in range(nch):
            kts = list(range(4 * c4, min(4 * c4 + 4, nkt)))
            pm, pmk = ptm.next()
            for kt in kts:
                P.op('pe', lambda e, pm=pm, kt=kt: e.transpose(out=pm[:, kt % 4, :], in_=mb[:, kt * 128:(kt + 1) * 128],
                                                                identity=identb[:]), reads=[mbk], writes=[pmk])
            P.op('act', lambda e, pm=pm, kts=kts: e.activation(out=maskT[:, kts[0]:kts[-1] + 1, :], in_=pm[:, :len(kts), :],
                                                               func=AF.Copy), reads=[pmk], writes=['maskT'])
        at, atk = atrot.next()
        items = [(h, c4) for h in range(8) for c4 in range(nch)]
        qkd = {}
        hst = {}

        def emit_qk(h, c4):
            kts = list(range(4 * c4, min(4 * c4 + 4, nkt)))
            ps, pk = pss.next()
            for kt in kts:
                mm(P, ps[:, kt % 4, :], KT[:, h, kt * 128:(kt + 1) * 128], qt[:, h, :], True, True, ['KT', qk], [pk])
            qkd[(h, c4)] = (ps, pk)

        def emit_rest(h, c4):
            kts = list(range(4 * c4, min(4 * c4 + 4, nkt)))
            nk = len(kts)
            ps, pk = qkd.pop((h, c4))
            if c4 == 0:
                hst[h] = pod.next()
            po, pok = hst[h]
            pT, pTk = pTrot.next()
            P.op('act', lambda e: e.activation(out=pT[:, :nk, :], in_=ps[:, :nk, :], func=AF.Exp, scale=0.125, bias=b31[:, h:h + 1]),
                 reads=[pk], writes=[pTk])
            P.op('dve', lambda e: e.tensor_tensor(out=pT[:, :nk, :], in0=pT[:, :nk, :], in1=maskT[:, kts[0]:kts[-1] + 1, :], op=ALU.mult),
                 reads=[pTk, 'maskT'], writes=[pTk])
            for kt in kts:
                dl = qi - kt
                if dl <= 1:
                    P.op('dve', lambda e, kt=kt, dl=dl: e.tensor_tensor(
                        out=pT[:, kt % 4, :], in0=pT[:, kt % 4, :], in1=E[:, h, dl * 128:(dl + 1) * 128], op=ALU.mult),
                        reads=[pTk], writes=[pTk])
            for kt in kts:
                P.op('pe', lambda e, kt=kt: e.matmul(po[:, 0:128], lhsT=V[:, kt, h * 64:(h + 1) * 64], rhs=pT[:, kt % 4, :],
                                                     start=(kt == 0), stop=(kt == nkt - 1), skip_group_check=True),
                     reads=['V', pTk], writes=[pok])
                P.op('pe', lambda e, kt=kt: e.matmul(po[:, 128:256], lhsT=ones64[:], rhs=pT[:, kt % 4, :],
                                                     start=False, stop=(kt == nkt - 1), skip_group_check=True),
                     reads=['ones64', pTk], writes=[pok])
            if c4 == nch - 1:
                rd, rdk = rdrot.next()
                P.op('dve', lambda e: e.reciprocal(out=rd[:], in_=po[:, 128:256]), reads=[pok], writes=[rdk])
                P.op('dve', lambda e: e.tensor_tensor(out=at[:, h, :], in0=po[:, 0:128], in1=rd[:], op=ALU.mult),
                     reads=[pok, rdk], writes=[atk])

        for idx in range(len(items) + LOOK):
            if idx < len(items):
                emit_qk(*items[idx])
            if idx >= LOOK:
                emit_rest(*items[idx - LOOK])
                yield
        P.dma('sp', lambda e: e.dma_start(out=attnT[:, :, tsl], in_=at[:]), reads=[atk])

    def drain(g):
        for _ in g:
            pass

    def merge(gens):
        gens = [[g, max(1, n), 0.0, True] for g, n in gens]
        total = max(n for _, n, _, _ in gens)
        for step in range(total + 1):
            for it in gens:
                it[2] += it[1] / total
                while it[3] and it[2] >= 1.0:
                    it[2] -= 1.0
                    try:
                        next(it[0])
                    except StopIteration:
                        it[3] = False
        for it in gens:
            if it[3]:
                drain(it[0])

    drain(indexer(0))
    drain(threshold(0))
    for qi in range(NT):
        if qi + 1 < NT:
            drain(indexer(qi + 1))
            merge([(attention(qi), 8 * ((qi + 4) // 4)), (threshold(qi + 1), NIT + 1)])
        else:
            drain(attention(qi))


def phase_E(P, T, G):
    LD = 0.6065306597126334
    ident = G['identb']
    zr = T['zr']
    def colload(name, n, key):
        t = P.sb([64, n], F32)
        P.dma('sp', lambda e: e.dma_start(out=t[:], in_=T[name].rearrange("(h p) -> p h", p=64), allow_slow_non_contiguous=True), writes=[key])
        return t
    mu_rkv = P.sb([64, 24], F32)
    P.dma('sp', lambda e: e.dma_start(out=mu_rkv[:], in_=T['tshift_mu'][0:1536].rearrange("(h p) -> p h", p=64), allow_slow_non_contiguous=True), writes=['mu'])
    mu_wa = P.sb([64, 2], F32)
    P.dma('sp', lambda e: e.dma_start(out=mu_wa[:], in_=T['tshift_mu'][1536:1664].rearrange("(h p) -> p h", p=64), allow_slow_non_contiguous=True), writes=['mu'])
    mu_g = P.sb([128, 1], F32)
    P.dma('sp', lambda e: e.dma_start(out=mu_g[:], in_=T['tshift_mu'][1664:1792].rearrange("(h p) -> p h", p=128), allow_slow_non_contiguous=True), writes=['mu'])
    om_rkv, om_wa, om_g = P.sb([64, 24], F32), P.sb([64, 2], F32), P.sb([128, 1], F32)
    for (o, m) in ((om_rkv, mu_rkv), (om_wa, mu_wa), (om_g, mu_g)):
        P.op('dve', lambda e, o=o, m=m: e.tensor_scalar(out=o[:], in0=m[:], scalar1=-1.0, scalar2=1.0, op0=ALU.mult, op1=ALU.add),
             reads=['mu'], writes=['om'])
    w0c = colload('decay_w0', 8, 'par'); a0c = colload('iclr_a0', 8, 'par'); kkc = colload('k_k', 8, 'par')
    kac = colload('k_a', 8, 'par'); rkc = colload('r_k', 8, 'par'); lgc = colload('lnx_g', 8, 'par'); lbc = colload('lnx_b', 8, 'par')
    omka = P.sb([64, 8], F32)
    P.op('dve', lambda e: e.tensor_scalar(out=omka[:], in0=kac[:], scalar1=-1.0, scalar2=1.0, op0=ALU.mult, op1=ALU.add),
         reads=['par'], writes=['omka'])
    dup, iup, gup = P.sb([64, 512], BF16), P.sb([64, 512], BF16), P.sb([128, 512], BF16)
    P.dma('pool', lambda e: e.dma_start(out=dup[:], in_=T['decay_up']), writes=['wts'])
    P.dma('pool', lambda e: e.dma_start(out=iup[:], in_=T['iclr_up']), writes=['wts'])
    P.dma('pool', lambda e: e.dma_start(out=gup[:], in_=T['gate_up']), writes=['wts'])
    onesf = P.sb([64, 64], F32)
    onesm = P.sb([64, 64], F32)
    gneps = P.sb([64, 1], F32)
    P.op('dve', lambda e: e.memset(onesf[:], 1.0), writes=['onesf'])
    P.op('dve', lambda e: e.memset(onesm[:], 1.0 / 64), writes=['onesm'])
    P.op('dve', lambda e: e.memset(gneps[:], 64e-5), writes=['gneps'])
    km = P.sb([128, 896], F32)
    P.dma('sp', lambda e: e.dma_start(out=km[:], in_=T['k_masks']), writes=['km'])
    mask4 = P.sb([128, 512], BF16)
    maskL = P.sb([128, 128], BF16)
    P.op('dve', lambda e: e.tensor_copy(out=mask4[:], in_=km[:, 0:512]), reads=['km'], writes=['mask4'])
    P.op('dve', lambda e: e.tensor_copy(out=maskL[:], in_=km[:, 512:640]), reads=['km'], writes=['maskL'])
    rst = P.sb([64, 512], F32)
    P.dma('sp', lambda e: e.dma_start(out=rst[:], in_=T['k_reset']), writes=['rst'])
    Tst = P.sb([64, 8, 64], BF16)
    P.op('dve', lambda e: e.memset(Tst[:], 0.0), writes=[f'T{h}' for h in range(8)])
    zrot = Rot(P, 1, [64, 3, 513], F32, 'z')
    wa_in = P.sb([64, 2, 513], F32)
    gd_in = P.sb([128, 513], F32)
    tmpr = Rot(P, 1, [128, 512], F32, 'tmp')
    twb, adb, sgb = P.sb([64, 512], BF16), P.sb([64, 512], BF16), P.sb([128, 512], BF16)
    AR = P.sb([64, 8, 4, 256], BF16)
    BK = P.sb([64, 8, 4, 256], BF16)
    tok3 = P.sb([128, 8, 4, 3, 64], BF16)
    pC = P.sb([64, 8, 4], F32)
    bon = P.sb([64, 8, 512], BF16)
    gg = P.sb([64, 8, 512], BF16)
    yT = P.sb([64, 8, 512], F32)
    RW = P.sb([64, 8, 512], BF16)
    hb = {n: P.sb([64, 512], F32) for n in ('sig', 'cs', 'ep', 'em', 'epv', 'kk', 'kkn', 'a', 't', 'kp', 'b', 'u1')}
    vb = P.sb([64, 512], BF16)
    Gms = [P.sb([128, 16, 512], BF16) for _ in range(2)]
    XY = [P.sb([128, 16, 256], BF16) for _ in range(2)]
    Nms = [P.sb([128, 16, 128], BF16) for _ in range(2)]
    Wsb, Usb = P.sb([128, 8, 64], BF16), P.sb([128, 8, 64], BF16)
    pg = Rot(P, 5, [128, 512], F32, 'pg', psum=True)
    pl = Rot(P, 2, [64, 512], F32, 'pl', psum=True)
    ptr = Rot(P, 1, [128, 3, 64], BF16, 'ptr', psum=True)
    rwT = T['rwT'].rearrange("(h p) t -> p h t", p=64)

    def dve(fn, reads, writes):
        P.op('dve', fn, reads=reads, writes=writes)

    for tg in range(8):
        t0 = tg * 512
        def load_halo(dst, rows, key, q, tg=tg, t0=t0):
            if tg == 0:
                src = rows(t0, t0 + 512)
                P.op('pool', lambda e: e.memset(dst[:, 0:1] if len(dst.shape) == 2 else dst[:, :, 0:1], 0.0), reads=[key], writes=[key])
                P.dma(q, lambda e: e.dma_start(out=(dst[:, 1:513] if len(dst.shape) == 2 else dst[:, :, 1:513]), in_=src), writes=[key + 'b'])
            else:
                src = rows(t0 - 1, t0 + 512)
                P.dma(q, lambda e: e.dma_start(out=dst[:], in_=src), reads=[key + 'b'], writes=[key])
        load_halo(wa_in, lambda a, b: zr[1536:1664, a:b].rearrange("(h p) t -> p h t", p=64), 'wa', 'sp')
        load_halo(gd_in, lambda a, b: zr[1664:1792, a:b], 'gd', 'act')

        def tshift(src_prev, src_cur, mu_ap, om_ap, np_, keys):
            tm, tk = tmpr.next()
            P.op('pool', lambda e: e.tensor_scalar(out=tm[:np_, :], in0=src_prev, scalar1=mu_ap, scalar2=0.0, op0=ALU.mult, op1=ALU.add),
                 reads=keys + ['mu'], writes=[tk])
            dve(lambda e: e.scalar_tensor_tensor(out=src_cur, in0=src_cur, scalar=om_ap, in1=tm[:np_, :], op0=ALU.mult, op1=ALU.add),
                keys + [tk, 'om'], keys)
        for i in range(2):
            tshift(wa_in[:, i, 0:512], wa_in[:, i, 1:513], mu_wa[:, i:i + 1], om_wa[:, i:i + 1], 64, ['wa', 'wab'])
        tshift(gd_in[:, 0:512], gd_in[:, 1:513], mu_g[:, 0:1], om_g[:, 0:1], 128, ['gd', 'gdb'])
        P.op('act', lambda e: e.activation(out=twb[:], in_=wa_in[:, 0, 1:513], func=AF.Tanh), reads=['wa', 'wab'], writes=['twb'])
        P.op('act', lambda e: e.activation(out=sgb[:], in_=gd_in[:, 1:513], func=AF.Sigmoid), reads=['gd', 'gdb'], writes=['sgb'])
        dve(lambda e: e.tensor_copy(out=adb[:], in_=wa_in[:, 1, 1:513]), ['wa', 'wab'], ['adb'])
        def prep_head(h, z, zk):
            def zrows(a, b, h=h):
                return zr[0:1536, a:b].rearrange("(s hh p) t -> hh p s t", s=3, p=64)[h]
            load_halo(z, zrows, zk, 'sp' if h % 2 == 0 else 'act')
            for s_ in range(3):
                tshift(z[:, s_, 0:512], z[:, s_, 1:513], mu_rkv[:, s_ * 8 + h:s_ * 8 + h + 1], om_rkv[:, s_ * 8 + h:s_ * 8 + h + 1], 64, [zk, zk + 'b'])
            r_, k_, v_ = z[:, 0, 1:513], z[:, 1, 1:513], z[:, 2, 1:513]
            zkeys = [zk, zk + 'b']
            sig, cs, ep, em, epv, kk, kkn, a_, t_, kp, b_, u1 = [hb[n] for n in ('sig', 'cs', 'ep', 'em', 'epv', 'kk', 'kkn', 'a', 't', 'kp', 'b', 'u1')]
            hs = slice(h * 64, (h + 1) * 64)
            p1, p1k = pl.next()
            mm(P, p1[:], dup[:, hs], twb[:], True, True, ['wts', 'twb'], [p1k])
            P.op('act', lambda e, p1=p1, h=h: e.activation(out=sig[:], in_=p1[:], func=AF.Sigmoid, bias=w0c[:, h:h + 1]),
                 reads=[p1k, 'par'], writes=['sig'])
            dve(lambda e: e.tensor_tensor_scan(out=cs[:], data0=rst[:], data1=sig[:], initial=0.0, op0=ALU.mult, op1=ALU.add),
                ['rst', 'sig'], ['cs'])
            P.op('act', lambda e: e.activation(out=ep[:], in_=cs[:], func=AF.Exp, scale=-LD), reads=['cs'], writes=['ep'])
            P.op('act', lambda e: e.activation(out=em[:], in_=cs[:], func=AF.Exp, scale=LD), reads=['cs'], writes=['em'])
            dve(lambda e: e.tensor_tensor(out=u1[:], in0=cs[:], in1=sig[:], op=ALU.subtract), ['cs', 'sig'], ['u1'])
            P.op('act', lambda e: e.activation(out=epv[:], in_=u1[:], func=AF.Exp, scale=-LD), reads=['u1'], writes=['epv'])
            dve(lambda e, h=h: e.tensor_copy(out=pC[:, h, :], in_=ep[:, 127:512:128]), ['ep'], ['pC'])
            p2, p2k = pl.next()
            mm(P, p2[:], iup[:, hs], adb[:], True, True, ['wts', 'adb'], [p2k])
            P.op('act', lambda e, p2=p2, h=h: e.activation(out=a_[:], in_=p2[:], func=AF.Sigmoid, bias=a0c[:, h:h + 1]),
                 reads=[p2k, 'par'], writes=['a'])
            p3, p3k = pl.next()
            mm(P, p3[:], gup[:, hs], sgb[:], True, True, ['wts', 'sgb'], [p3k])
            P.op('act', lambda e, p3=p3, h=h: e.activation(out=gg[:, h, :], in_=p3[:], func=AF.Copy), reads=[p3k], writes=[f'gg{h}'])
            dve(lambda e, h=h: e.tensor_scalar(out=kk[:], in0=k_, scalar1=kkc[:, h:h + 1], scalar2=None, op0=ALU.mult), zkeys + ['par'], ['kk'])
            P.op('act', lambda e: e.activation(out=u1[:], in_=kk[:], func=AF.Square), reads=['kk', 'u1'], writes=['u1'])
            p4, p4k = pl.next()
            mm(P, p4[:], onesf[:], u1[:], True, True, ['onesf', 'u1'], [p4k])
            P.op('act', lambda e, p4=p4: e.activation(out=kkn[:], in_=p4[:], func=AF.Sqrt), reads=[p4k], writes=['kkn'])
            dve(lambda e: e.tensor_scalar(out=kkn[:], in0=kkn[:], scalar1=1e-12, scalar2=None, op0=ALU.max), ['kkn'], ['kkn'])
            dve(lambda e: e.reciprocal(out=kkn[:], in_=kkn[:]), ['kkn'], ['kkn'])
            dve(lambda e: e.tensor_tensor(out=kkn[:], in0=kkn[:], in1=kk[:], op=ALU.mult), ['kkn', 'kk'], ['kkn'])
            dve(lambda e, h=h: e.tensor_scalar(out=t_[:], in0=a_[:], scalar1=kac[:, h:h + 1], scalar2=omka[:, h:h + 1], op0=ALU.mult, op1=ALU.add),
                ['a', 'par', 'omka'], ['t'])
            dve(lambda e: e.tensor_tensor(out=kp[:], in0=t_[:], in1=k_, op=ALU.mult), ['t'] + zkeys, ['kp'])
            dve(lambda e: e.tensor_tensor(out=b_[:], in0=kkn[:], in1=a_[:], op=ALU.mult), ['kkn', 'a'], ['b'])
            c4 = lambda ap: ap.rearrange("p (c t) -> p c t", c=4)
            dve(lambda e, h=h: e.tensor_tensor(out=AR[:, h, :, 128:256], in0=c4(r_), in1=c4(ep[:]), op=ALU.mult), zkeys + ['ep'], [f'AR{h}'])
            dve(lambda e, h=h: e.scalar_tensor_tensor(out=AR[:, h, :, 0:128], in0=c4(kkn[:]), scalar=-1.0, in1=c4(epv[:]), op0=ALU.mult, op1=ALU.mult),
                ['kkn', 'epv'], [f'AR{h}'])
            dve(lambda e, h=h: e.tensor_tensor(out=BK[:, h, :, 0:128], in0=c4(b_[:]), in1=c4(em[:]), op=ALU.mult), ['b', 'em'], [f'BK{h}'])
            dve(lambda e, h=h: e.tensor_tensor(out=BK[:, h, :, 128:256], in0=c4(kp[:]), in1=c4(em[:]), op=ALU.mult), ['kp', 'em'], [f'BK{h}'])
            dve(lambda e, h=h: e.scalar_tensor_tensor(out=u1[:], in0=r_, scalar=rkc[:, h:h + 1], in1=kp[:], op0=ALU.mult, op1=ALU.mult),
                zkeys + ['kp', 'par', 'u1'], ['u1'])
            p5, p5k = pl.next()
            mm(P, p5[:], onesf[:], u1[:], True, True, ['onesf', 'u1'], [p5k])
            dve(lambda e, p5=p5, h=h: e.tensor_tensor(out=bon[:, h, :], in0=p5[:], in1=v_, op=ALU.mult), [p5k] + zkeys, [f'bon{h}'])
            P.op('pool', lambda e: e.tensor_copy(out=vb[:], in_=v_), reads=zkeys, writes=['vb'])
            for c in range(4):
                pt_, ptk = ptr.next()
                cs_ = slice(c * 128, (c + 1) * 128)
                P.op('pe', lambda e, pt_=pt_, cs_=cs_: e.transpose(out=pt_[:, 0, :], in_=vb[:, cs_], identity=ident[0:64, 0:64]), reads=['vb'], writes=[ptk])
                P.op('pe', lambda e, pt_=pt_, h=h, c=c: e.transpose(out=pt_[:, 1, :], in_=BK[:, h, c, 0:128], identity=ident[0:64, 0:64]), reads=[f'BK{h}'], writes=[ptk])
                P.op('pe', lambda e, pt_=pt_, h=h, c=c: e.transpose(out=pt_[:, 2, :], in_=BK[:, h, c, 128:256], identity=ident[0:64, 0:64]), reads=[f'BK{h}'], writes=[ptk])
                P.op('act', lambda e, pt_=pt_, h=h, c=c: e.activation(out=tok3[:, h, c, :, :], in_=pt_[:], func=AF.Copy), reads=[ptk], writes=[f'tok{h}'])
        for h in range(8):
            z, zk = zrot.next()
            prep_head(h, z, zk)
        def stage1(cs, tg=tg):
            sl = (cs[0] // 2) % 2
            Gm, Nm = Gms[sl], Nms[sl]
            probs = [(ci, c, h) for ci, c in enumerate(cs) for h in range(8)]
            for (ci, c, h) in probs:
                q = ci * 8 + h
                p_, pk = pg.next()
                mm(P, p_[:, 0:256], BK[:, h, c, 0:128], AR[:, h, c, :], True, True, [f'BK{h}', f'AR{h}'], [pk])
                mm(P, p_[:, 256:512], BK[:, h, c, 128:256], AR[:, h, c, :], True, True, [f'BK{h}', f'AR{h}'], [pk])
                dve(lambda e, p_=p_, q=q: e.tensor_tensor(out=Gm[:, q, :], in0=p_[:], in1=mask4[:], op=ALU.mult), [pk, 'mask4'], [f'Gm{sl}_{q}'])
                p2_, p2k = pg.next()
                mm(P, p2_[:, 0:128], AR[:, h, c, 0:128], BK[:, h, c, 0:128], True, True, [f'BK{h}', f'AR{h}'], [p2k])
                dve(lambda e, p2_=p2_, q=q: e.tensor_tensor(out=XY[0][:, q, 128:256], in0=p2_[:, 0:128], in1=maskL[:], op=ALU.mult),
                    [p2k, 'maskL'], [f'XY0{q}'])
                P.op('pool', lambda e, q=q: e.tensor_copy(out=XY[0][:, q, 0:128], in_=Gm[:, q, 0:128]), reads=[f'Gm{sl}_{q}'], writes=[f'XY0{q}x'])
                P.op('pool', lambda e, q=q: e.tensor_tensor(out=Nm[:, q, :], in0=Gm[:, q, 0:128], in1=ident[:], op=ALU.add),
                     reads=[f'Gm{sl}_{q}'], writes=[f'N{sl}_{q}'])
            yield
            for j in range(6):
                cur, nxt = XY[j % 2], XY[(j + 1) % 2]
                ck, nk_ = f'XY{j % 2}', f'XY{(j + 1) % 2}'
                for q in range(len(probs)):
                    p_, pk = pg.next()
                    rk_ = [ck + f'{q}', ck + f'{q}x']
                    mm(P, p_[:, 0:128], cur[:, q, 128:256], cur[:, q, 0:128], True, True, rk_, [pk])
                    mm(P, p_[:, 128:256], cur[:, q, 0:128], cur[:, q, 128:256], True, True, rk_, [pk])
                    P.op('act', lambda e, p_=p_, nxt=nxt, q=q: e.activation(out=nxt[:, q, :], in_=p_[:, 0:256], func=AF.Copy),
                         reads=[pk], writes=[nk_ + f'{q}', nk_ + f'{q}x'])
                    if q % 4 == 3:
                        yield
                for q in range(len(probs)):
                    p_, pk = pg.next()
                    mm(P, p_[:, 0:128], nxt[:, q, 128:256], Nm[:, q, :], True, True, [nk_ + f'{q}', nk_ + f'{q}x', f'N{sl}_{q}'], [pk])
                    dve(lambda e, p_=p_, q=q: e.tensor_tensor(out=Nm[:, q, :], in0=p_[:, 0:128], in1=Nm[:, q, :], op=ALU.add),
                        [pk, f'N{sl}_{q}'], [f'N{sl}_{q}'])
                    if q % 4 == 3:
                        yield

        def stage2(c, tg=tg):
            sl = (c // 2) % 2
            Gm, Nm = Gms[sl], Nms[sl]
            ci = c % 2
            pw, pwk = pg.next()
            for h in range(8):
                q = ci * 8 + h
                mm(P, pw[:, h * 64:(h + 1) * 64], AR[:, h, c, 0:128], Tst[:, h, :], True, False, [f'AR{h}', f'T{h}'], [pwk])
                mm(P, pw[:, h * 64:(h + 1) * 64], Gm[:, q, 256:384], tok3[:, h, c, 0, :], False, True, [f'Gm{sl}_{q}', f'tok{h}'], [pwk])
            P.op('act', lambda e: e.activation(out=Wsb[:].rearrange("p h v -> p (h v)"), in_=pw[:], func=AF.Copy), reads=[pwk], writes=['W'])
            yield
            pu, puk = pg.next()
            for h in range(8):
                q = ci * 8 + h
                mm(P, pu[:, h * 64:(h + 1) * 64], Nm[:, q, :], Wsb[:, h, :], True, True, [f'N{sl}_{q}', 'W'], [puk])
            P.op('act', lambda e: e.activation(out=Usb[:].rearrange("p h v -> p (h v)"), in_=pu[:], func=AF.Copy), reads=[puk], writes=['U'])
            yield
            pt_, ptk = pg.next()
            for h in range(8):
                q = ci * 8 + h
                o_ = pt_[0:64, h * 64:(h + 1) * 64]
                mm(P, o_, ident[0:64, 0:64], Tst[:, h, :], True, False, [f'T{h}'], [ptk])
                mm(P, o_, tok3[:, h, c, 1, :], Usb[:, h, :], False, False, [f'tok{h}', 'U'], [ptk])
                mm(P, o_, tok3[:, h, c, 2, :], tok3[:, h, c, 0, :], False, True, [f'tok{h}'], [ptk])
            for half in range(2):
                py_, pyk = pg.next()
                for hh in range(4):
                    h = half * 4 + hh
                    q = ci * 8 + h
                    o_ = py_[0:64, hh * 128:(hh + 1) * 128]
                    mm(P, o_, Tst[:, h, :], AR[:, h, c, 128:256], True, False, [f'AR{h}', f'T{h}'], [pyk])
                    mm(P, o_, Usb[:, h, :], Gm[:, q, 128:256], False, False, ['U', f'Gm{sl}_{q}'], [pyk])
                    mm(P, o_, tok3[:, h, c, 0, :], Gm[:, q, 384:512], False, True, [f'tok{h}', f'Gm{sl}_{q}'], [pyk])
                P.op('act', lambda e, py_=py_, half=half: e.activation(
                    out=yT[:, half * 4:half * 4 + 4, c * 128:(c + 1) * 128], in_=py_[0:64, :].rearrange("p (h t) -> p h t", h=4), func=AF.Copy),
                    reads=[pyk], writes=[f'yT{half}'])
            for h in range(8):
                dve(lambda e, h=h: e.tensor_scalar(out=Tst[:, h, :], in0=pt_[0:64, h * 64:(h + 1) * 64], scalar1=pC[:, h, c:c + 1], scalar2=None, op0=ALU.mult),
                    [ptk, 'pC', f'T{h}'], [f'T{h}'])
            yield

        def chain(*gs):
            for g in gs:
                yield from g

        def rr(g1, g2):
            a = b = True
            while a or b:
                if a:
                    try:
                        next(g1)
                    except StopIteration:
                        a = False
                if b:
                    try:
                        next(g2)
                    except StopIteration:
                        b = False
        for _ in stage1([0, 1]):
            pass
        rr(stage1([2, 3]), chain(stage2(0), stage2(1)))
        for _ in chain(stage2(2), stage2(3)):
            pass
        for h in range(8):
            u1, u2 = hb['u1'], hb['t']
            p1, p1k = pl.next()
            mm(P, p1[:], onesm[:], yT[:, h, :], True, True, ['onesm', f'yT{h // 4}'], [p1k])
            dve(lambda e, p1=p1, h=h: e.tensor_tensor(out=u1[:], in0=yT[:, h, :], in1=p1[:], op=ALU.subtract), [p1k, f'yT{h // 4}', 'u1'], ['u1'])
            P.op('act', lambda e: e.activation(out=u2[:], in_=u1[:], func=AF.Square), reads=['u1', 't'], writes=['t'])
            p2, p2k = pl.next()
            mm(P, p2[:], onesm[:], u2[:], True, True, ['onesm', 't'], [p2k])
            P.op('act', lambda e, p2=p2: e.activation(out=u2[:], in_=p2[:], func=AF.Sqrt, bias=gneps[:, 0:1]), reads=[p2k, 'gneps', 't'], writes=['t'])
            dve(lambda e: e.reciprocal(out=u2[:], in_=u2[:]), ['t'], ['t'])
            dve(lambda e: e.tensor_tensor(out=u1[:], in0=u1[:], in1=u2[:], op=ALU.mult), ['u1', 't'], ['u1'])
            dve(lambda e, h=h: e.tensor_scalar(out=u1[:], in0=u1[:], scalar1=lgc[:, h:h + 1], scalar2=lbc[:, h:h + 1], op0=ALU.mult, op1=ALU.add),
                ['u1', 'par'], ['u1'])
            dve(lambda e, h=h: e.tensor_tensor(out=u1[:], in0=u1[:], in1=bon[:, h, :], op=ALU.add), ['u1', f'bon{h}'], ['u1'])
            dve(lambda e, h=h: e.tensor_tensor(out=RW[:, h, :], in0=u1[:], in1=gg[:, h, :], op=ALU.mult), ['u1', f'gg{h}'], ['RW'])
        P.dma('sp', lambda e, t0=t0: e.dma_start(out=rwT[:, :, t0:t0 + 512], in_=RW[:]), reads=['RW'])
        if tg == 0 and 'dbg_y' in T:
            P.dma('sp', lambda e: e.dma_start(out=T['dbg_y'], in_=yT[:]), reads=['yT0', 'yT1'])
            P.dma('sp', lambda e: e.dma_start(out=T['dbg_bon'], in_=bon[:]), reads=[f'bon{h}' for h in range(8)])
            P.dma('sp', lambda e: e.dma_start(out=T['dbg_g'], in_=gg[:]), reads=[f'gg{h}' for h in range(8)])
            P.dma('sp', lambda e: e.dma_start(out=T['dbg_AR'], in_=AR[:]), reads=[f'AR{h}' for h in range(8)])
            P.dma('sp', lambda e: e.dma_start(out=T['dbg_BK'], in_=BK[:]), reads=[f'BK{h}' for h in range(8)])


def phase_F(P, T, G):
    wa, wr, wo = P.sb([128, 4, D], BF16), P.sb([128, 4, D], BF16), P.sb([128, 8, D], BF16)
    P.dma('pool', lambda e: e.dma_start(out=wa[:], in_=T['w_attn_br'].rearrange("(j p) d -> p j d", p=128)), writes=['wa'])
    P.dma('pool', lambda e: e.dma_start(out=wr[:], in_=T['w_rwkv_br'].rearrange("(j p) d -> p j d", p=128)), writes=['wr'])
    P.dma('pool', lambda e: e.dma_start(out=wo[:], in_=T['w_out'].rearrange("(j p) d -> p j d", p=128)), writes=['wo'])
    W = dict(junk=P.sb([128, D], F32), ss=P.sb([128, 1], F32), rs=P.sb([128, 1], F32), t1=P.sb([128, D], F32),
             hb=P.sb([128, D], BF16), pt=P.ps([128, 8, 128], BF16))
    W['mod'] = P.sb([128, 3072], F32)
    P.dma('sp', lambda e: e.dma_start(out=W['mod'][:], in_=T['modd'][:, 2048:5120]), writes=['modl'])
    xrot = Rot(P, 2, [128, D], F32, 'x')
    atr, rtr = Rot(P, 2, [128, 4, 128], BF16, 'at'), Rot(P, 2, [128, 4, 128], BF16, 'rt')
    gar, grr = Rot(P, 2, [128, 8, 128], BF16, 'ga'), Rot(P, 2, [128, 8, 128], BF16, 'gr')
    sga, sgr = P.sb([128, 8, 128], F32), P.sb([128, 8, 128], F32)
    m1, m2 = P.sb([128, 4, 128], F32), P.sb([128, 4, 128], F32)
    mixT = P.sb([128, 8, 128], BF16)
    x1t = P.sb([128, D], F32)
    h2t = P.sb([128, 8, 128], BF16)
    pA = Rot(P, 1, [128, 4, 128], F32, 'pA', psum=True)
    pR = Rot(P, 1, [128, 4, 128], F32, 'pR', psum=True)
    po = Rot(P, 2, [128, 512], F32, 'po', psum=True)
    aT = T['attnT'].rearrange("(j p) t -> p j t", p=128)
    rT = T['rwT'].rearrange("(j p) t -> p j t", p=128)
    gaT = T['zga'].rearrange("(j p) t -> p j t", p=128)
    grT = T['zgr'].rearrange("(j p) t -> p j t", p=128)
    h2T = T['h2T'].rearrange("(k p) t -> p k t", p=128)
    for i in range(NT):
        tsl = slice(i * 128, (i + 1) * 128)
        xt, xk = xrot.next()
        at, atk = atr.next(); rt, rtk = rtr.next(); ga, gak = gar.next(); gr, grk = grr.next()
        P.dma('sp', lambda e, xt=xt, tsl=tsl: e.dma_start(out=xt[:], in_=T['x'][tsl, :]), writes=[xk])
        P.dma('act', lambda e, at=at, tsl=tsl: e.dma_start(out=at[:], in_=aT[:, :, tsl]), writes=[atk])
        P.dma('act', lambda e, rt=rt, tsl=tsl: e.dma_start(out=rt[:], in_=rT[:, :, tsl]), writes=[rtk])
        P.dma('sp', lambda e, ga=ga, tsl=tsl: e.dma_start(out=ga[:], in_=gaT[:, :, tsl]), writes=[gak])
        P.dma('sp', lambda e, gr=gr, tsl=tsl: e.dma_start(out=gr[:], in_=grT[:, :, tsl]), writes=[grk])
        P.op('act', lambda e, ga=ga: e.activation(out=sga[:], in_=ga[:], func=AF.Sigmoid), reads=[gak], writes=['sga'])
        P.op('act', lambda e, gr=gr: e.activation(out=sgr[:], in_=gr[:], func=AF.Sigmoid), reads=[grk], writes=['sgr'])
        for half in range(2):
            pa, pak = pA.next(); pr, prk = pR.next()
            for s_ in range(4):
                dt = half * 4 + s_
                for j in range(4):
                    mm(P, pa[:, s_, :], wa[:, j, dt * 128:(dt + 1) * 128], at[:, j, :], j == 0, j == 3, ['wa', atk], [pak])
            for s_ in range(4):
                dt = half * 4 + s_
                for j in range(4):
                    mm(P, pr[:, s_, :], wr[:, j, dt * 128:(dt + 1) * 128], rt[:, j, :], j == 0, j == 3, ['wr', rtk], [prk])
            hs = slice(half * 4, half * 4 + 4)
            P.op('dve', lambda e, pa=pa, hs=hs: e.tensor_tensor(out=m1[:], in0=pa[:], in1=sga[:, hs, :], op=ALU.mult), reads=[pak, 'sga'], writes=['m1'])
            P.op('dve', lambda e, pr=pr, hs=hs: e.tensor_tensor(out=m2[:], in0=pr[:], in1=sgr[:, hs, :], op=ALU.mult), reads=[prk, 'sgr'], writes=['m2'])
            P.op('pool', lambda e, hs=hs: e.tensor_tensor(out=mixT[:, hs, :], in0=m1[:], in1=m2[:], op=ALU.add), reads=['m1', 'm2'], writes=['mixT'])
        for half in range(2):
            p_, pk = po.next()
            cs_ = slice(half * 512, (half + 1) * 512)
            for dt in range(8):
                mm(P, p_[:], mixT[:, dt, :], wo[:, dt, cs_], dt == 0, dt == 7, ['mixT', 'wo'], [pk])
            P.op('dve', lambda e, p_=p_, cs_=cs_: e.tensor_tensor(out=x1t[:, cs_], in0=p_[:], in1=W['mod'][:, cs_], op=ALU.mult),
                 reads=[pk, 'modl'], writes=['x1t'])
        P.op('pool', lambda e, xt=xt: e.tensor_tensor(out=x1t[:], in0=x1t[:], in1=xt[:], op=ALU.add), reads=['x1t', xk], writes=['x1t'])
        P.dma('sp', lambda e, tsl=tsl: e.dma_start(out=T['x1'][tsl, :], in_=x1t[:]), reads=['x1t'])
        norm_mod_transpose(P, G, x1t, 'x1t', h2t, 0, slice(2048, 3072), slice(1024, 2048), W)
        P.dma('act', lambda e, tsl=tsl: e.dma_start(out=h2T[:, :, tsl], in_=h2t[:]), reads=['hT0'])


def phase_G0(P, T, G):
    for e in range(64):
        for (src, dst) in (('exp_gate', 'wg16'), ('exp_up', 'wu16'), ('exp_down', 'wd16')):
            P.dma('pool', lambda e_, e=e, src=src, dst=dst: e_.dma_start(
                out=T[dst][e].rearrange("(a b) -> a b", b=2048), in_=T[src][e].rearrange("r c -> (r c)").rearrange("(a b) -> a b", b=2048)))
    for (src, dst) in (('sh_gate', 'wg16'), ('sh_up', 'wu16'), ('sh_down', 'wd16')):
        P.dma('pool', lambda e_, src=src, dst=dst: e_.dma_start(
            out=T[dst][64].rearrange("(a b) -> a b", b=2048), in_=T[src].rearrange("r c -> (r c)").rearrange("(a b) -> a b", b=2048)))


def phase_G(P, T, G):
    ident = G['identb']
    h2T = T['h2T'].rearrange("(k p) t -> p k t", p=128)
    yacc = G['yacc']
    gwT = P.sb([64, S], BF16)
    rwt = P.sb([128, 8, 64], BF16)
    rbias = P.sb([128, 64], F32)
    P.dma('pool', lambda e: e.dma_start(out=rwt[:], in_=T['router_w'].rearrange("(k p) n -> p k n", p=128)), writes=['rwt'])
    P.dma('sp', lambda e: e.dma_start(out=rbias[:], in_=T['router_bias'].partition_broadcast(128)), writes=['rbias'])
    ones128 = P.sb([64, 128], BF16)
    P.op('dve', lambda e: e.memset(ones128[:], 1.0), writes=['ones128'])
    hrot = Rot(P, 2, [128, 8, 256], BF16, 'h2g')
    pmisc = P.ps([128, 512], F32)
    ptb = P.ps([64, 128], BF16)
    emb = P.sb([128, 64], BF16)
    sc_, ch, tmp, cm, em = [P.sb([128, 64], F32) for _ in range(5)]
    m1, m2, grp, s8, gmask, pen, den = [P.sb([128, 8], F32) for _ in range(7)]
    dve = lambda fn, r, w: P.op('dve', fn, reads=r, writes=w)
    for tgp in range(16):
        hg, hk = hrot.next()
        P.dma('sp', lambda e, hg=hg, tgp=tgp: e.dma_start(out=hg[:], in_=h2T[:, :, tgp * 256:(tgp + 1) * 256]), writes=[hk])
        for tt in range(2):
            i = tgp * 2 + tt
            p_, pk = pmisc[:, 0:64], 'pm_a'
            for k in range(8):
                mm(P, p_, hg[:, k, tt * 128:(tt + 1) * 128], rwt[:, k, :], k == 0, k == 7, [hk, 'rwt'], [pk])
            P.op('act', lambda e, p_=p_: e.activation(out=sc_[:], in_=p_, func=AF.Sigmoid), reads=[pk], writes=['sc'])
            dve(lambda e: e.tensor_tensor(out=ch[:], in0=sc_[:], in1=rbias[:], op=ALU.add), ['sc', 'rbias'], ['ch'])
            ch3 = ch[:].rearrange("p (g e) -> p g e", g=8)
            dve(lambda e, ch3=ch3: e.tensor_reduce(out=m1[:], in_=ch3, axis=AX.X, op=ALU.max), ['ch'], ['m1'])
            for g in range(8):
                dve(lambda e, g=g: e.tensor_scalar(out=tmp[:, g * 8:(g + 1) * 8], in0=ch[:, g * 8:(g + 1) * 8], scalar1=m1[:, g:g + 1],
                                                  scalar2=-1e9, op0=ALU.is_equal, op1=ALU.mult), ['ch', 'm1', 'tmp'], ['tmp'])
            dve(lambda e: e.tensor_tensor(out=tmp[:], in0=tmp[:], in1=ch[:], op=ALU.add), ['tmp', 'ch'], ['tmp'])
            dve(lambda e: e.tensor_reduce(out=m2[:], in_=tmp[:].rearrange("p (g e) -> p g e", g=8), axis=AX.X, op=ALU.max), ['tmp'], ['m2'])
            dve(lambda e: e.tensor_tensor(out=grp[:], in0=m1[:], in1=m2[:], op=ALU.add), ['m1', 'm2'], ['grp'])
            dve(lambda e: e.max(out=s8[:], in_=grp[:]), ['grp'], ['s8'])
            dve(lambda e: e.tensor_scalar(out=gmask[:], in0=grp[:], scalar1=s8[:, 3:4], scalar2=None, op0=ALU.is_ge), ['grp', 's8'], ['gmask'])
            dve(lambda e: e.tensor_scalar(out=pen[:], in0=gmask[:], scalar1=-1.0, scalar2=1e9, op0=ALU.add, op1=ALU.mult), ['gmask'], ['pen'])
            for g in range(8):
                dve(lambda e, g=g: e.tensor_scalar(out=cm[:, g * 8:(g + 1) * 8], in0=ch[:, g * 8:(g + 1) * 8], scalar1=pen[:, g:g + 1],
                                                  scalar2=None, op0=ALU.add), ['ch', 'pen', 'cm'], ['cm'])
            dve(lambda e: e.max(out=s8[:], in_=cm[:]), ['cm', 's8'], ['s8'])
            dve(lambda e: e.tensor_scalar(out=em[:], in0=cm[:], scalar1=s8[:, 7:8], scalar2=None, op0=ALU.is_ge), ['cm', 's8'], ['em'])
            dve(lambda e: e.tensor_tensor(out=em[:], in0=em[:], in1=sc_[:], op=ALU.mult), ['em', 'sc'], ['em'])
            dve(lambda e: e.tensor_reduce(out=den[:, 0:1], in_=em[:], axis=AX.X, op=ALU.add), ['em'], ['den'])
            dve(lambda e: e.reciprocal(out=den[:, 1:2], in_=den[:, 0:1]), ['den'], ['den'])
            dve(lambda e: e.tensor_scalar(out=em[:], in0=em[:], scalar1=den[:, 1:2], scalar2=2.5, op0=ALU.mult, op1=ALU.mult), ['em', 'den'], ['em'])
            pt_, ptk = ptb[:], 'pm_b'
            dve(lambda e: e.tensor_copy(out=emb[:], in_=em[:]), ['em', 'emb'], ['emb'])
            P.op('pe', lambda e, pt_=pt_: e.transpose(out=pt_, in_=emb[:], identity=G['identb'][:]), reads=['emb'], writes=[ptk])
            P.op('act', lambda e, pt_=pt_, i=i: e.activation(out=gwT[:, i * 128:(i + 1) * 128], in_=pt_, func=AF.Copy), reads=[ptk], writes=['gwT'])
    if G.get('gstop') == 'router':
        return
    wgr = Rot(P, 2, [128, 8, 256], BF16, 'wg')
    wur = Rot(P, 2, [128, 8, 256], BF16, 'wu')
    wdr = Rot(P, 2, [128, 2, D], BF16, 'wd')
    selr = Rot(P, 2, [64, 128], BF16, 'sel')
    pgu = Rot(P, 2, [128, 4, 256], F32, 'pgu', psum=True)
    py = Rot(P, 2, [128, 512], F32, 'py', psum=True)
    sgr_ = Rot(P, 2, [128, 2, 256], F32, 'sg')
    tr_ = Rot(P, 2, [128, 2, 256], F32, 'tt')
    actr = Rot(P, 2, [128, 2, 256], BF16, 'act')
    for e_ in range(G.get('nexp', 65)):
        wg, wgk = wgr.next(); wu, wuk = wur.next(); wd, wdk = wdr.next()
        if e_ < 64:
            sg_, su_, sd_ = T['exp_gate'][e_], T['exp_up'][e_], T['exp_down'][e_]
        else:
            sg_, su_, sd_ = T['sh_gate'], T['sh_up'], T['sh_down']
        P.dma('pool', lambda e, wg=wg, sg_=sg_: e.dma_start(out=wg[:], in_=sg_.rearrange("(k p) f -> p k f", p=128)), writes=[wgk])
        P.dma('pool', lambda e, wu=wu, su_=su_: e.dma_start(out=wu[:], in_=su_.rearrange("(k p) f -> p k f", p=128)), writes=[wuk])
        P.dma('pool', lambda e, wd=wd, sd_=sd_: e.dma_start(out=wd[:], in_=sd_.rearrange("(k p) f -> p k f", p=128)), writes=[wdk])
        if e_ < 64:
            sel, selk = selr.next()
            P.op('pool', lambda e, sel=sel, e_=e_: e.tensor_scalar(out=sel[:], in0=ones128[:], scalar1=G['identf'][0:64, e_:e_ + 1], scalar2=0.0, op0=ALU.mult, op1=ALU.add),
                 reads=['ones128'], writes=[selk])
        for tgp in range(16):
            hg, hk = hrot.next()
            P.dma('sp' if tgp % 2 == 0 else 'act', lambda e, hg=hg, tgp=tgp: e.dma_start(out=hg[:], in_=h2T[:, :, tgp * 256:(tgp + 1) * 256]), writes=[hk])
            p_, pk = pgu.next()
            for s_, (w_, wk_) in enumerate(((wg, wgk), (wg, wgk), (wu, wuk), (wu, wuk))):
                ft = s_ % 2
                for k in range(8):
                    mm(P, p_[:, s_, :], w_[:, k, ft * 128:(ft + 1) * 128], hg[:, k, :], k == 0, k == 7, [wk_, hk], [pk])
            sg, sgk = sgr_.next(); t_, tk = tr_.next(); ac, ack = actr.next()
            P.op('act', lambda e, sg=sg, p_=p_: e.activation(out=sg[:], in_=p_[:, 0:2, :], func=AF.Silu), reads=[pk], writes=[sgk])
            P.op('dve', lambda e, sg=sg, p_=p_, t_=t_: e.tensor_tensor(out=t_[:], in0=p_[:, 2:4, :], in1=sg[:], op=ALU.mult), reads=[pk, sgk], writes=[tk])
            if e_ < 64:
                pw, pwk = pmisc[:, 256:512], 'pm_c'
                mm(P, pw, sel[:], gwT[:, tgp * 256:(tgp + 1) * 256], True, True, [selk, 'gwT'], [pwk])
                for ft in range(2):
                    P.op('dve', lambda e, ac=ac, t_=t_, pw=pw, ft=ft: e.tensor_tensor(out=ac[:, ft, :], in0=pw, in1=t_[:, ft, :], op=ALU.mult),
                         reads=[tk, pwk], writes=[ack])
            else:
                P.op('pool', lambda e, ac=ac, t_=t_: e.tensor_copy(out=ac[:], in_=t_[:]), reads=[tk], writes=[ack])
            for tt in range(2):
                i = tgp * 2 + tt
                for half in range(2):
                    q_, qk = py.next()
                    cs_ = slice(half * 512, (half + 1) * 512)
                    for ft in range(2):
                        mm(P, q_[:], ac[:, ft, tt * 128:(tt + 1) * 128], wd[:, ft, cs_], ft == 0, ft == 1, [ack, wdk], [qk])
                    if e_ == 0:
                        P.op('act', lambda e, q_=q_, i=i, cs_=cs_: e.activation(out=yacc[:, i, cs_], in_=q_[:], func=AF.Copy), reads=[qk], writes=[f'y{i}'])
                    else:
                        P.op('dve', lambda e, q_=q_, i=i, cs_=cs_: e.tensor_tensor(out=yacc[:, i, cs_], in0=q_[:], in1=yacc[:, i, cs_], op=ALU.add),
                             reads=[qk, f'y{i}'], writes=[f'y{i}'])


def phase_H(P, T, G):
    yacc = G['yacc']
    dve = lambda fn, r, w: P.op('dve', fn, reads=r, writes=w)
    g2b = P.sb([128, D], F32)
    fing = P.sb([128, D], F32)
    P.dma('sp', lambda e: e.dma_start(out=g2b[:], in_=T['modd'][:, 5120:6144]), writes=['g2b'])
    P.dma('sp', lambda e: e.dma_start(out=fing[:], in_=T['final_g'].partition_broadcast(128)), writes=['fing'])
    xr = Rot(P, 2, [128, D], F32, 'x1')
    junk, ss, rs = P.sb([128, D], F32), P.sb([128, 1], F32), P.sb([128, 1], F32)
    for i in range(NT):
        tsl = slice(i * 128, (i + 1) * 128)
        xt, xk = xr.next()
        P.dma('sp', lambda e, xt=xt, tsl=tsl: e.dma_start(out=xt[:], in_=T['x1'][tsl, :]), writes=[xk])
        dve(lambda e, i=i: e.tensor_tensor(out=yacc[:, i, :], in0=yacc[:, i, :], in1=g2b[:], op=ALU.mult), [f'y{i}', 'g2b'], [f'y{i}'])
        P.op('pool', lambda e, i=i, xt=xt: e.tensor_tensor(out=xt[:], in0=xt[:], in1=yacc[:, i, :], op=ALU.add), reads=[f'y{i}', xk], writes=[xk])
        rms_rstd(P, xt, xk, junk, ss, rs, 'f')
        dve(lambda e, xt=xt: e.scalar_tensor_tensor(out=xt[:], in0=xt[:], scalar=rs[:, 0:1], in1=fing[:], op0=ALU.mult, op1=ALU.mult),
            [xk, 'rsf', 'fing'], [xk])
        P.dma('sp', lambda e, xt=xt, tsl=tsl: e.dma_start(out=T['out'][tsl, :], in_=xt[:]), reads=[xk])


SCRATCH = [
    ('zq', [512, S], BF16), ('zk', [512, S], BF16), ('zv', [S, 512], BF16), ('ziq', [512, S], BF16),
    ('zik', [32, S], BF16), ('ziw', [S, 16], F32), ('zr', [1792, S], F32), ('zga', [1024, S], BF16),
    ('zgr', [1024, S], BF16), ('modd', [128, 6 * D], F32), ('attnT', [512, S], BF16), ('rwT', [512, S], BF16),
    ('x1', [S, D], F32), ('h2T', [D, S], BF16),
]

INPUT_SHAPES = [
    ('x', [S, D]), ('c_col', [128, 8]), ('ada_w', [D, 6 * D]), ('ada_b', [1, 6 * D]), ('norm1_g', [D]),
    ('w_in', [D, NIN]), ('rel_bias', [256]), ('tshift_mu', [1792]), ('decay_w0', [512]), ('decay_up', [64, 512]),
    ('iclr_a0', [512]), ('iclr_up', [64, 512]), ('gate_up', [128, 512]), ('k_k', [512]), ('k_a', [512]),
    ('r_k', [512]), ('lnx_g', [512]), ('lnx_b', [512]), ('w_attn_br', [512, D]), ('w_rwkv_br', [512, D]),
    ('w_out', [D, D]), ('norm2_g', [D]), ('router_w', [D, 64]), ('router_bias', [64]),
    ('exp_gate', [64, D, 256]), ('exp_up', [64, D, 256]), ('exp_down', [64, 256, D]),
    ('sh_gate', [D, 256]), ('sh_up', [D, 256]), ('sh_down', [256, D]), ('final_g', [D]),
    ('k_ident', [128, 128]), ('k_rel', [128, 256]), ('k_masks', [128, 896]), ('k_reset', [64, 512]),
]


def build(debug_outs=(), stop_after=None):
    nc = bass.Bass("TRN2", target_bir_lowering=False)
    T = {}
    for name, shp in INPUT_SHAPES:
        T[name] = nc.dram_tensor(name, shp, F32, kind="ExternalInput").ap()
    for name, shp, dt in SCRATCH:
        kind = "ExternalOutput" if name in debug_outs else "Internal"
        T[name] = nc.dram_tensor(name, shp, dt, kind=kind).ap()
    T['out'] = nc.dram_tensor('out', [S, D], F32, kind="ExternalOutput").ap()
    if 'dbg_y' in debug_outs:
        T['dbg_y'] = nc.dram_tensor('dbg_y', [64, 8, 512], F32, kind="ExternalOutput").ap()
        T['dbg_bon'] = nc.dram_tensor('dbg_bon', [64, 8, 512], BF16, kind="ExternalOutput").ap()
        T['dbg_g'] = nc.dram_tensor('dbg_g', [64, 8, 512], BF16, kind="ExternalOutput").ap()
        T['dbg_AR'] = nc.dram_tensor('dbg_AR', [64, 8, 4, 256], BF16, kind="ExternalOutput").ap()
        T['dbg_BK'] = nc.dram_tensor('dbg_BK', [64, 8, 4, 256], BF16, kind="ExternalOutput").ap()
    P = Prog(nc)
    G = {}
    G['E'] = P.gsb([128, 8, 256], F32)
    G['b31'] = P.gsb([128, 8], F32)
    G['ones_row'] = P.gsb([1, 128], F32)
    G['identf'] = P.gsb([128, 128], F32)
    G['identb'] = P.gsb([128, 128], BF16)
    G['eps'] = P.gsb([128, 1], F32)
    G_EPS[0] = G['eps']
    P.op('dve', lambda e: e.memset(G['ones_row'][:], 1.0), writes=['ones_row'])
    P.op('dve', lambda e: e.memset(G['eps'][:], 1e-6), writes=['eps'])
    P.dma('sp', lambda e: e.dma_start(out=G['identf'][:], in_=T['k_ident']), writes=['identf'])
    P.op('dve', lambda e: e.tensor_copy(out=G['identb'][:], in_=G['identf'][:]), reads=['identf'], writes=['identb'])
    phase_A(P, T, G)
    P.emit()
    if stop_after == 'A':
        P.emit(); P.finish(); return nc
    phase_BC(P, T, G)
    P.emit()
    if stop_after == 'C':
        P.finish(); return nc
    phase_D0(P, T, G)
    P.emit()
    phase_D(P, T, G)
    P.emit()
    if stop_after == 'D':
        P.finish(); return nc
    phase_E(P, T, G)
    P.emit()
    if stop_after == 'E':
        P.finish(); return nc
    phase_F(P, T, G)
    P.emit()
    if stop_after == 'F':
        P.finish(); return nc
    G['yacc'] = P.gsb([128, NT, D], F32)
    if stop_after in ('router', 'exp1'):
        G['gstop'] = stop_after
        G['nexp'] = 1
    if stop_after == 'router':
        phase_G(P, T, G); P.emit(); P.finish(); return nc
    if stop_after == 'exp1':
        phase_G(P, T, G); P.emit(); P.finish(); return nc
    phase_G(P, T, G)
    P.emit()
    phase_H(P, T, G)
    P.emit()
    P.finish()
    return nc


def host_inputs(inputs, b):
    m = {}
    f = lambda a: np.ascontiguousarray(np.asarray(a, dtype=np.float32))
    m['x'] = f(inputs['x'][b])
    m['c_col'] = f(np.asarray(inputs['c'][b]).reshape(8, 128).T)
    for name, shp in INPUT_SHAPES:
        if name in ('x', 'c_col', 'k_ident', 'k_rel', 'k_masks', 'k_reset'):
            continue
        a = np.asarray(inputs[name])
        m[name] = f(a.reshape(shp))
    m['k_ident'] = np.eye(128, dtype=np.float32)
    ii = np.arange(128)
    su = (ii[:, None] < ii[None, :]).astype(np.float32)
    iu = (ii[:, None] <= ii[None, :]).astype(np.float32)
    sl = (ii[:, None] > ii[None, :]).astype(np.float32)
    m['k_masks'] = np.ascontiguousarray(np.concatenate([su, iu, su, iu, sl, np.zeros((128, 256), np.float32)], axis=1))
    rs_ = np.ones((64, 512), np.float32); rs_[:, ::128] = 0.0
    m['k_reset'] = rs_
    m['k_rel'] = (np.arange(256, dtype=np.float32)[None, :] - np.arange(128, dtype=np.float32)[:, None])
    return m


def kernel(**inputs):
    nc = build()
    in_maps = [host_inputs(inputs, b) for b in range(8)]
    res = run_bass_kernel_spmd(nc, in_maps, core_ids=list(range(8)))
    return np.stack([np.asarray(r['out']) for r in res.results], axis=0).astype(np.float32)
```

```python
import contextlib
import numpy as np
import concourse.bass as bass
import concourse.mybir as mybir

F32 = mybir.dt.float32
BF16 = mybir.dt.bfloat16
I32 = mybir.dt.int32
U32 = mybir.dt.uint32
AF = mybir.ActivationFunctionType
ALU = mybir.AluOpType
AX = mybir.AxisListType

N_DMA_SEMS = 6


class Prog:
    ENGS = ('pe', 'act', 'dve', 'pool', 'sp')

    def __init__(self, nc):
        self.nc = nc
        self.ops = {e: [] for e in self.ENGS}
        self.cnt = {e: 0 for e in self.ENGS}
        self.waited = {e: {} for e in self.ENGS}
        self.res = {}
        self.dma_tot = [0] * N_DMA_SEMS
        self.dma_rr = 0
        self.stack = contextlib.ExitStack()
        self.gstack = contextlib.ExitStack()
        self.nsb = 0
        self.nphase = 0
        self.sems = None

    def _new_sems(self):
        nc = self.nc
        self.sems = {}
        for e in self.ENGS:
            self.sems[e] = self.gstack.enter_context(nc.semaphore(f"s_{e}_{self.nphase}"))
        for i in range(N_DMA_SEMS):
            self.sems[('dma', i)] = self.gstack.enter_context(nc.semaphore(f"s_dma{i}_{self.nphase}"))
        self.cnt = {e: 0 for e in self.ENGS}
        self.waited = {e: {} for e in self.ENGS}
        self.dma_tot = [0] * N_DMA_SEMS
        self.dma_rr = 0

    def gsb(self, shape, dt, name=None):
        self.nsb += 1
        return self.gstack.enter_context(self.nc.sbuf_tensor(name or f"gsb{self.nsb}", list(shape), dt))

    def sb(self, shape, dt, name=None):
        self.nsb += 1
        return self.stack.enter_context(self.nc.sbuf_tensor(name or f"sb{self.nsb}", list(shape), dt))

    def ps(self, shape, dt, name=None):
        self.nsb += 1
        return self.stack.enter_context(self.nc.psum_tensor(name or f"ps{self.nsb}", list(shape), dt))

    def _deps(self, reads, writes):
        deps = {}
        def add(d):
            if d is None:
                return
            k, v = d
            if deps.get(k, 0) < v:
                deps[k] = v
        for k in reads:
            r = self.res.get(k)
            if r:
                add(r['w'])
        for k in writes:
            r = self.res.get(k)
            if r:
                add(r['w'])
                for d in r['r']:
                    add(d)
        return deps

    def _emit_waits(self, eng, deps):
        w = self.waited[eng]
        for k, v in deps.items():
            if w.get(k, 0) < v:
                w[k] = v
                self.ops[eng].append(('wait', k, v))

    def _update(self, dep, reads, writes):
        for k in reads:
            r = self.res.setdefault(k, {'w': None, 'r': []})
            r['r'] = [d for d in r['r'] if d[0] != dep[0]] + [dep]
        for k in writes:
            self.res[k] = {'w': dep, 'r': []}

    def op(self, eng, fn, reads=(), writes=()):
        if self.sems is None:
            self._new_sems()
        deps = self._deps(reads, writes)
        if eng == 'pe':
            deps.pop('pe', None)
        self._emit_waits(eng, deps)
        self.cnt[eng] += 1
        dep = (eng, self.cnt[eng])
        self.ops[eng].append(('op', fn))
        self._update(dep, reads, writes)
        return dep

    def dma(self, q, fn, reads=(), writes=()):
        if self.sems is None:
            self._new_sems()
        deps = self._deps(reads, writes)
        s = self.dma_rr
        self.dma_rr = (self.dma_rr + 1) % N_DMA_SEMS
        key = ('dma', s)
        if self.dma_tot[s] > 0:
            if deps.get(key, 0) < self.dma_tot[s]:
                deps[key] = self.dma_tot[s]
        self._emit_waits(q, deps)
        self.dma_tot[s] += 16
        dep = (key, self.dma_tot[s])
        self.ops[q].append(('dma', fn, s))
        self._update(dep, reads, writes)
        return dep

    def emit(self, keep=False):
        nc = self.nc
        with contextlib.ExitStack() as st:
            sems = self.sems
            self.nphase += 1
            block = st.enter_context(nc.Block(f"ph{self.nphase}"))
            engobj = {'pe': nc.tensor, 'act': nc.scalar, 'dve': nc.vector, 'pool': nc.gpsimd, 'sp': nc.sync}
            fin = {}
            for e in self.ENGS:
                if e != 'sp' and self.cnt[e] > 0:
                    fin[e] = self.cnt[e]
            for i in range(N_DMA_SEMS):
                if self.dma_tot[i] > 0:
                    fin[('dma', i)] = self.dma_tot[i]
            self._emit_waits('sp', fin)

            def run(e):
                eo = engobj[e]
                for item in self.ops[e]:
                    if item[0] == 'wait':
                        eo.wait_ge(sems[item[1]], item[2])
                    elif item[0] == 'op':
                        item[1](eo).then_inc(sems[e], 1)
                    else:
                        item[1](eo).then_inc(sems[('dma', item[2])], 16)

            @block.tensor
            def _(t):
                run('pe')

            @block.scalar
            def _(t):
                run('act')

            @block.vector
            def _(t):
                run('dve')

            @block.gpsimd
            def _(t):
                run('pool')

            @block.sync
            def _(t):
                run('sp')
        self.ops = {e: [] for e in self.ENGS}
        self.res = {}
        if keep:
            return
        self.stack.close()
        self.stack = contextlib.ExitStack()
        self.sems = None

    def finish(self):
        self.gstack.close()

from concourse.bass_utils import run_bass_kernel_spmd
import ml_dtypes

SPARSE = True
S = 4096
D = 1024
NT = S // 128
NIN = 5936


class Rot:
    def __init__(self, P, n, shape, dt, name, psum=False):
        self.tiles = [(P.ps(shape, dt) if psum else P.sb(shape, dt)) for _ in range(n)]
        self.name = name
        self.i = 0

    def next(self):
        t = self.tiles[self.i % len(self.tiles)]
        k = f"{self.name}{self.i % len(self.tiles)}"
        self.i += 1
        return t, k


class Rot2(Rot):
    def __init__(self, P, n, shape, dt, name):
        self.tiles = [(P.sb(shape, dt), P.sb(shape, dt)) for _ in range(n)]
        self.name = name
        self.i = 0


def mm(P, out, lhsT, rhs, start, stop, reads, writes):
    P.op('pe', lambda e: e.matmul(out, lhsT=lhsT, rhs=rhs, start=start, stop=stop), reads=reads, writes=writes)


def phase_A(P, T, G):
    mod_bc = P.sb([128, 6 * D], F32)
    ccol = P.sb([128, 8], F32)
    scol = P.sb([128, 8], F32)
    adab = P.sb([1, 6144], F32)
    modrow = P.sb([1, 6144], F32)
    ngb = P.sb([128, 1024], F32)
    P.dma('sp', lambda e: e.dma_start(out=ccol[:], in_=T['c_col']), writes=['ccol'])
    P.dma('sp', lambda e: e.dma_start(out=adab[:], in_=T['ada_b']), writes=['adab'])
    P.op('act', lambda e: e.activation(out=scol[:], in_=ccol[:], func=AF.Silu), reads=['ccol'], writes=['scol'])
    wrot = Rot(P, 2, [128, 8, 512], F32, 'aw')
    psr = Rot(P, 2, [1, 512], F32, 'psr', psum=True)
    psb = Rot(P, 2, [128, 512], F32, 'psb', psum=True)
    adaw = T['ada_w'].rearrange("(k p) n -> p k n", p=128)
    for n in range(12):
        wb, wk = wrot.next()
        P.dma('sp' if n % 2 == 0 else 'act',
              lambda e, wb=wb, n=n: e.dma_start(out=wb[:], in_=adaw[:, :, n * 512:(n + 1) * 512]), writes=[wk])
        pr, pk = psr.next()
        for k in range(8):
            mm(P, pr[:], scol[:, k:k + 1], wb[:, k, :], k == 0, k == 7, [wk, 'scol'], [pk])
        sl = slice(n * 512, (n + 1) * 512)
        P.op('dve', lambda e, pr=pr, sl=sl: e.tensor_tensor(out=modrow[0:1, sl], in0=pr[:], in1=adab[0:1, sl], op=ALU.add),
             reads=[pk, 'adab'], writes=[f'modrow{n}'])
        pb, pbk = psb.next()
        mm(P, pb[:], G['ones_row'][:], modrow[0:1, sl], True, True, [f'modrow{n}'], [pbk])
        P.op('act', lambda e, pb=pb, sl=sl: e.activation(out=mod_bc[:, sl], in_=pb[:], func=AF.Copy),
             reads=[pbk], writes=[f'mod{n}'])
    for (gname, c0, deps) in (('norm1_g', 1024, ['mod2', 'mod3']), ('norm2_g', 4096, ['mod8', 'mod9'])):
        P.dma('sp', lambda e, gname=gname: e.dma_start(out=ngb[:], in_=T[gname].partition_broadcast(128)), writes=['ngb'])
        P.op('dve', lambda e, c0=c0: e.scalar_tensor_tensor(out=mod_bc[:, c0:c0 + 1024], in0=mod_bc[:, c0:c0 + 1024],
                                                            scalar=1.0, in1=ngb[:], op0=ALU.add, op1=ALU.mult),
             reads=deps + ['ngb'], writes=deps)
    P.dma('sp', lambda e: e.dma_start(out=T['modd'], in_=mod_bc[:]), reads=[f'mod{n}' for n in range(12)])


def rms_rstd(P, xt, xk, junk, ss, rs, tag):
    P.op('act', lambda e: e.activation(out=junk[:], in_=xt[:], func=AF.Square, accum_out=ss[:]),
         reads=[xk], writes=['junk' + tag, 'ss' + tag])
    P.op('act', lambda e: e.activation(out=ss[:], in_=ss[:], func=AF.Sqrt, scale=1.0 / D, bias=G_EPS[0][:, 0:1]),
         reads=['ss' + tag], writes=['ss' + tag])
    P.op('dve', lambda e: e.reciprocal(out=rs[:], in_=ss[:]), reads=['ss' + tag], writes=['rs' + tag])


G_EPS = [None]


def norm_mod_transpose(P, G, xt, xk, hT, i, g_sl, sh_sl, W):
    mod_bc = W['mod']
    junk, ss, rs, t1, hb, pt = W['junk'], W['ss'], W['rs'], W['t1'], W['hb'], W['pt']
    rms_rstd(P, xt, xk, junk, ss, rs, '')
    P.op('dve', lambda e: e.scalar_tensor_tensor(out=t1[:], in0=xt[:], scalar=rs[:, 0:1], in1=mod_bc[:, g_sl],
                                                 op0=ALU.mult, op1=ALU.mult), reads=[xk, 'rs', 'modl'], writes=['t1'])
    P.op('pool', lambda e: e.tensor_tensor(out=hb[:], in0=t1[:], in1=mod_bc[:, sh_sl], op=ALU.add),
         reads=['t1', 'modl'], writes=['hb'])
    for k in range(8):
        P.op('pe', lambda e, k=k: e.transpose(out=pt[:, k, :], in_=hb[:, k * 128:(k + 1) * 128], identity=G['identb'][:]),
             reads=['hb'], writes=['pt'])
    P.op('act', lambda e: e.activation(out=hT[:, :, i * 128:(i + 1) * 128], in_=pt[:], func=AF.Copy),
         reads=['pt'], writes=[f'hT{i // 4}'])


def phase_BC(P, T, G):
    hT = P.sb([128, 8, S], BF16)
    W = dict(junk=P.sb([128, D], F32), ss=P.sb([128, 1], F32), rs=P.sb([128, 1], F32), t1=P.sb([128, D], F32),
             hb=P.sb([128, D], BF16), pt=P.ps([128, 8, 128], BF16))
    W['mod'] = P.sb([128, 2048], F32)
    P.dma('sp', lambda e: e.dma_start(out=W['mod'][:], in_=T['modd'][:, 0:2048]), writes=['modl'])
    xrot = Rot(P, 2, [128, D], F32, 'x')
    for i in range(NT):
        xt, xk = xrot.next()
        P.dma('sp', lambda e, xt=xt, i=i: e.dma_start(out=xt[:], in_=T['x'][i * 128:(i + 1) * 128, :]), writes=[xk])
        norm_mod_transpose(P, G, xt, xk, hT, i, slice(1024, 2048), slice(0, 1024), W)
    win = T['w_in'].rearrange("(k p) n -> p k n", p=128)
    segs = [('zq', 0, 512, BF16), ('zk', 512, 512, BF16), ('ziq', 1536, 512, BF16), ('zik', 2048, 32, BF16),
            ('zr', 2096, 1792, F32), ('zga', 3888, 1024, BF16), ('zgr', 4912, 1024, BF16)]
    wrot = Rot(P, 2, [128, 8, 128], BF16, 'w')
    psrot = Rot(P, 3, [128, 512], F32, 'ps', psum=True)
    strot = {BF16: Rot(P, 3, [128, 512], BF16, 'stb'), F32: Rot(P, 3, [128, 512], F32, 'stf')}
    ev = 0
    for (name, c0, n, dt) in segs:
        for m0 in range(0, n, 128):
            M = min(128, n - m0)
            wt, wk = wrot.next()
            P.dma('pool', lambda e, wt=wt, M=M, a=c0 + m0: e.dma_start(out=wt[:, :, :M], in_=win[:, :, a:a + M]), writes=[wk])
            for tg in range(8):
                ps, pk = psrot.next()
                for k in range(8):
                    mm(P, ps[:M, :], wt[:, k, :M], hT[:, k, tg * 512:(tg + 1) * 512], k == 0, k == 7, [wk, f'hT{tg}'], [pk])
                st, sk = strot[dt].next()
                if ev % 2 == 0:
                    P.op('act', lambda e, st=st, ps=ps, M=M: e.activation(out=st[:M, :], in_=ps[:M, :], func=AF.Copy),
                         reads=[pk], writes=[sk])
                else:
                    P.op('dve', lambda e, st=st, ps=ps, M=M: e.tensor_copy(out=st[:M, :], in_=ps[:M, :]),
                         reads=[pk], writes=[sk])
                ev += 1
                P.dma('sp', lambda e, st=st, M=M, name=name, m0=m0, tg=tg:
                      e.dma_start(out=T[name][m0:m0 + M, tg * 512:(tg + 1) * 512], in_=st[:M, :]), reads=[sk])
    wv = P.sb([128, 8, 528], BF16)
    P.dma('pool', lambda e: e.dma_start(out=wv[:, :, 0:512], in_=win[:, :, 1024:1536]), writes=['wv'])
    P.dma('pool', lambda e: e.dma_start(out=wv[:, :, 512:528], in_=win[:, :, 2080:2096]), writes=['wv2'])
    ps2rot = Rot(P, 2, [128, 16], F32, 'ps2', psum=True)
    st2rot = Rot(P, 2, [128, 16], F32, 'st2')
    for i in range(NT):
        ps, pk = psrot.next()
        ps2, pk2 = ps2rot.next()
        for k in range(8):
            mm(P, ps[:], hT[:, k, i * 128:(i + 1) * 128], wv[:, k, 0:512], k == 0, k == 7, ['wv', f'hT{i // 4}'], [pk])
        for k in range(8):
            mm(P, ps2[:], hT[:, k, i * 128:(i + 1) * 128], wv[:, k, 512:528], k == 0, k == 7, ['wv2', f'hT{i // 4}'], [pk2])
        st, sk = strot[BF16].next()
        st2, sk2 = st2rot.next()
        P.op('act', lambda e, st=st, ps=ps: e.activation(out=st[:], in_=ps[:], func=AF.Copy), reads=[pk], writes=[sk])
        P.op('dve', lambda e, st2=st2, ps2=ps2: e.tensor_copy(out=st2[:], in_=ps2[:]), reads=[pk2], writes=[sk2])
        P.dma('sp', lambda e, st=st, i=i: e.dma_start(out=T['zv'][i * 128:(i + 1) * 128, :], in_=st[:]), reads=[sk])
        P.dma('sp', lambda e, st2=st2, i=i: e.dma_start(out=T['ziw'][i * 128:(i + 1) * 128, :], in_=st2[:]), reads=[sk2])


def t5_lo_bounds():
    n = np.arange(256)
    nf = np.maximum(n, 1).astype(np.float32)
    large = 16 + (np.log(nf / np.float32(16)) / np.float32(np.log(8.0)) * np.float32(16)).astype(np.int32)
    large = np.minimum(large, 31)
    bk = np.where(n < 16, n, large)
    return [int(np.min(np.nonzero(bk >= b)[0])) for b in range(1, 32)]


def phase_D0(P, T, G):
    relb = P.sb([128, 256], F32)
    diff = P.sb([128, 248], F32)
    base = P.sb([128, 8], F32)
    relidx = P.sb([128, 256], F32)
    P.dma('sp', lambda e: e.dma_start(out=relb[:], in_=T['rel_bias'].partition_broadcast(128)), writes=['relb'])
    P.dma('sp', lambda e: e.dma_start(out=relidx[:], in_=T['k_rel']), writes=['relidx'])
    P.op('dve', lambda e: e.tensor_tensor(out=diff[:], in0=relb[:, 8:256], in1=relb[:, 0:248], op=ALU.subtract),
         reads=['relb'], writes=['diff'])
    P.op('dve', lambda e: e.tensor_tensor(out=base[:], in0=relb[:, 0:8], in1=relb[:, 248:256], op=ALU.subtract),
         reads=['relb'], writes=['base'])
    P.op('dve', lambda e: e.tensor_copy(out=G['b31'][:], in_=relb[:, 248:256]), reads=['relb'], writes=['b31'])
    E = G['E']
    irot = Rot(P, 2, [128, 256], F32, 'ind')
    los = t5_lo_bounds()
    for b in range(1, 32):
        ind, ik = irot.next()
        P.op('dve', lambda e, ind=ind, lo=float(los[b - 1]): e.tensor_scalar(out=ind[:], in0=relidx[:], scalar1=lo, scalar2=None,
                                                                              op0=ALU.is_ge), reads=['relidx'], writes=[ik])
        for h in range(8):
            if b == 1:
                P.op('dve', lambda e, ind=ind, h=h: e.tensor_scalar(out=E[:, h, :], in0=ind[:], scalar1=diff[:, h:h + 1],
                                                                    scalar2=base[:, h:h + 1], op0=ALU.mult, op1=ALU.add),
                     reads=[ik, 'diff', 'base'], writes=[f'E{h}'])
            else:
                c = (b - 1) * 8 + h
                P.op('dve', lambda e, ind=ind, h=h, c=c: e.scalar_tensor_tensor(out=E[:, h, :], in0=ind[:], scalar=diff[:, c:c + 1],
                                                                               in1=E[:, h, :], op0=ALU.mult, op1=ALU.add),
                     reads=[ik, 'diff', f'E{h}'], writes=[f'E{h}'])
    P.op('act', lambda e: e.activation(out=E[:], in_=E[:], func=AF.Exp), reads=[f'E{h}' for h in range(8)],
         writes=[f'E{h}' for h in range(8)])


def phase_D(P, T, G):
    NIT = 16
    KT = P.sb([64, 8, S], BF16)
    V = P.sb([128, NT, 512], BF16)
    ik4 = P.sb([128, S], BF16)
    iw = P.sb([128, NT, 16], F32)
    ones64 = P.sb([128, 64], BF16)
    scs = [P.sb([128, S], F32), P.sb([128, S], F32)]
    maskbs = [P.sb([128, S], BF16), P.sb([128, S], BF16)]
    maskT = P.sb([128, NT, 128], BF16)
    lo, hi, mid, cnt, tmp, thr = [P.sb([128, 1], F32) for _ in range(6)]
    cvec = P.sb([128, NIT + 1], F32)
    dk = P.sb([128, NIT + 1], F32)
    for k in range(NIT + 1):
        P.op('pool', lambda e, k=k: e.memset(cvec[:, k:k + 1], 2.0 ** -(k + 1)), reads=['cvec'], writes=['cvec'])
    P.dma('sp', lambda e: e.dma_start(out=KT[:], in_=T['zk'].rearrange("(h p) t -> p h t", p=64)), writes=['KT'])
    P.dma('act', lambda e: e.dma_start(out=V[:], in_=T['zv'].rearrange("(i p) f -> p i f", p=128)), writes=['V'])
    for i in range(3):
        P.dma('sp', lambda e, i=i: e.dma_start(out=ik4[32 * i:32 * i + 32, :], in_=T['zik']), writes=[f'ik4{i}'])
    P.dma('sp', lambda e: e.dma_start(out=iw[:], in_=T['ziw'].rearrange("(i p) f -> p i f", p=128)), writes=['iw'])
    P.op('dve', lambda e: e.memset(ones64[:], 1.0), writes=['ones64'])
    zq = T['zq'].rearrange("(h p) t -> p h t", p=64)
    ziq = T['ziq'][0:480, :].rearrange("(j p) t -> p j t", p=96)
    attnT = T['attnT'].rearrange("(h p) t -> p h t", p=64)
    qrot = Rot(P, 2, [64, 8, 128], BF16, 'q')
    iqrot = Rot(P, 2, [96, 6, 128], BF16, 'iq')
    dgrot = Rot(P, 2, [128, 16, 128], BF16, 'dg')
    rrot = Rot(P, 4, [128, 512], BF16, 'r')
    psi = Rot(P, 2, [128, 512], F32, 'psi', psum=True)
    pacc = Rot(P, 1, [128, 512], F32, 'pacc', psum=True)
    pss = Rot(P, 3, [128, 4, 128], F32, 'pss', psum=True)
    ptm = Rot(P, 1, [128, 4, 128], BF16, 'ptm', psum=True)
    pod = Rot(P, 1, [64, 512], F32, 'pod', psum=True)
    pTrot = Rot(P, 4, [128, 4, 128], BF16, 'pT')
    atrot = Rot(P, 2, [64, 8, 128], BF16, 'at')
    rdrot = Rot(P, 2, [64, 128], F32, 'rd')
    E, b31 = G['E'], G['b31']
    identb = G['identb']

    def indexer(qi):
        sc, sck = scs[qi % 2], f'sc{qi % 2}'
        n = 128 * (qi + 1)
        tsl = slice(qi * 128, (qi + 1) * 128)
        iqt, iqk = iqrot.next()
        P.dma('act', lambda e: e.dma_start(out=iqt[:, 0:5, :], in_=ziq[:, :, tsl]), writes=[iqk])
        P.dma('act', lambda e: e.dma_start(out=iqt[0:32, 5, :], in_=T['ziq'][480:512, tsl]), writes=[iqk + 'b'])
        dg, dgk = dgrot.next()
        for h in range(16):
            P.op('pool', lambda e, h=h: e.tensor_scalar(out=dg[:, h, :], in0=identb[:], scalar1=iw[:, qi, h:h + 1], scalar2=0.0, op0=ALU.mult, op1=ALU.add),
                 reads=['iw'], writes=[dgk])
        for ch in range((n + 511) // 512):
            c0 = ch * 512
            nc_ = min(512, n - c0)
            pa, pak = pacc.next()
            pend = []

            def acc(h, r, rk):
                mm(P, pa[:, :nc_], dg[:, h, :], r[:, :nc_], h == 0, h == 15, [dgk, rk], [pak])
            for h in range(16):
                j, i = divmod(h, 3)
                ps, pk = psi.next()
                mm(P, ps[:, :nc_], iqt[32 * i:32 * i + 32, j, :], ik4[32 * i:32 * i + 32, c0:c0 + nc_], True, True,
                   [iqk, iqk + 'b', f'ik4{i}'], [pk])
                r, rk = rrot.next()
                P.op('act', lambda e, r=r, ps=ps, nc_=nc_: e.activation(out=r[:, :nc_], in_=ps[:, :nc_], func=AF.Relu), reads=[pk], writes=[rk])
                pend.append((h, r, rk))
                if len(pend) > 1:
                    acc(*pend.pop(0))
                yield
            while pend:
                acc(*pend.pop(0))
            P.op('dve', lambda e, pa=pa, c0=c0, nc_=nc_: e.tensor_copy(out=sc[:, c0:c0 + nc_], in_=pa[:, :nc_]), reads=[pak], writes=[sck])
        P.op('pool', lambda e: e.affine_select(out=sc[:, tsl], in_=sc[:, tsl], pattern=[[-1, 128]], compare_op=ALU.is_ge, fill=-1e30,
                                               base=0, channel_multiplier=1), reads=[sck], writes=[sck])

    def threshold(qi):
        sc, sck = scs[qi % 2], f'sc{qi % 2}'
        n = 128 * (qi + 1)
        maskb, mbk = maskbs[qi % 2], f'maskb{qi % 2}'
        dve = lambda fn, r, w: P.op('dve', fn, reads=r, writes=w)
        if n <= 256:
            dve(lambda e: e.memset(thr[:], -1e29), ['thr'], ['thr'])
        else:
            nv = 128 * qi
            dve(lambda e: e.tensor_reduce(out=lo[:], in_=sc[:, :nv], axis=AX.X, op=ALU.min), [sck, 'lo'], ['lo'])
            dve(lambda e: e.tensor_reduce(out=hi[:], in_=sc[:, :n], axis=AX.X, op=ALU.max), [sck, 'hi'], ['hi'])
            dve(lambda e: e.tensor_tensor(out=hi[:], in0=hi[:], in1=lo[:], op=ALU.subtract), ['hi', 'lo'], ['hi'])
            dve(lambda e: e.tensor_scalar(out=dk[:], in0=cvec[:], scalar1=hi[:, 0:1], scalar2=None, op0=ALU.mult), ['cvec', 'hi', 'dk'], ['dk'])
            dve(lambda e: e.tensor_tensor(out=mid[:], in0=lo[:], in1=dk[:, 0:1], op=ALU.add), ['lo', 'dk', 'mid'], ['mid'])
            for k in range(NIT):
                dve(lambda e: e.tensor_scalar(out=maskb[:, :n], in0=sc[:, :n], scalar1=mid[:, 0:1], scalar2=None,
                                              op0=ALU.is_ge, op1=ALU.add, accum_out=cnt[:]), [sck, 'mid', 'cnt', mbk], [mbk, 'cnt'])
                dve(lambda e: e.tensor_scalar(out=tmp[:], in0=cnt[:], scalar1=255.5, scalar2=-0.5, op0=ALU.is_ge, op1=ALU.add),
                    ['cnt', 'tmp'], ['tmp'])
                dve(lambda e, k=k: e.scalar_tensor_tensor(out=mid[:], in0=tmp[:], scalar=dk[:, k:k + 1], in1=mid[:], op0=ALU.mult, op1=ALU.add),
                    ['tmp', 'dk', 'mid'], ['mid'])
                yield
            dve(lambda e: e.tensor_tensor(out=thr[:], in0=mid[:], in1=dk[:, NIT:NIT + 1], op=ALU.subtract), ['mid', 'dk', 'thr'], ['thr'])
        dve(lambda e: e.tensor_scalar(out=maskb[:, :n], in0=sc[:, :n], scalar1=thr[:, 0:1], scalar2=None, op0=ALU.is_ge),
            [sck, 'thr', mbk], [mbk])
        yield

    def attention(qi):
        LOOK = 2
        nkt = qi + 1
        nch = (nkt + 3) // 4
        tsl = slice(qi * 128, (qi + 1) * 128)
        mb = maskbs[qi % 2]
        mbk = f'maskb{qi % 2}'
        qt, qk = qrot.next()
        P.dma('sp', lambda e: e.dma_start(out=qt[:], in_=zq[:, :, tsl]), writes=[qk])
        for c4 in range(nch):
            kts = list(range(4 * c4, min(4 * c4 + 4, nkt)))
            pm, pmk = ptm.next()
            for kt in kts:
                P.op('pe', lambda e, pm=pm, kt=kt: e.transpose(out=pm[:, kt % 4, :], in_=mb[:, kt * 128:(kt + 1) * 128],
                                                                identity=identb[:]), reads=[mbk], writes=[pmk])
            P.op('act', lambda e, pm=pm, kts=kts: e.activation(out=maskT[:, kts[0]:kts[-1] + 1, :], in_=pm[:, :len(kts), :],
                                                               func=AF.Copy), reads=[pmk], writes=['maskT'])
        at, atk = atrot.next()
        items = [(h, c4) for h in range(8) for c4 in range(nch)]
        qkd = {}
        hst = {}

        def emit_qk(h, c4):
            kts = list(range(4 * c4, min(4 * c4 + 4, nkt)))
            ps, pk = pss.next()
            for kt in kts:
                mm(P, ps[:, kt % 4, :], KT[:, h, kt * 128:(kt + 1) * 128], qt[:, h, :], True, True, ['KT', qk], [pk])
            qkd[(h, c4)] = (ps, pk)

        def emit_rest(h, c4):
            kts = list(range(4 * c4, min(4 * c4 + 4, nkt)))
            nk = len(kts)
            ps, pk = qkd.pop((h, c4))
            if c4 == 0:
                hst[h] = pod.next()
            po, pok = hst[h]
            pT, pTk = pTrot.next()
            P.op('act', lambda e: e.activation(out=pT[:, :nk, :], in_=ps[:, :nk, :], func=AF.Exp, scale=0.125, bias=b31[:, h:h + 1]),
                 reads=[pk], writes=[pTk])
            P.op('dve', lambda e: e.tensor_tensor(out=pT[:, :nk, :], in0=pT[:, :nk, :], in1=maskT[:, kts[0]:kts[-1] + 1, :], op=ALU.mult),
                 reads=[pTk, 'maskT'], writes=[pTk])
            for kt in kts:
                dl = qi - kt
                if dl <= 1:
                    P.op('dve', lambda e, kt=kt, dl=dl: e.tensor_tensor(
                        out=pT[:, kt % 4, :], in0=pT[:, kt % 4, :], in1=E[:, h, dl * 128:(dl + 1) * 128], op=ALU.mult),
                        reads=[pTk], writes=[pTk])
            for kt in kts:
                P.op('pe', lambda e, kt=kt: e.matmul(po[:, 0:128], lhsT=V[:, kt, h * 64:(h + 1) * 64], rhs=pT[:, kt % 4, :],
                                                     start=(kt == 0), stop=(kt == nkt - 1), skip_group_check=True),
                     reads=['V', pTk], writes=[pok])
                P.op('pe', lambda e, kt=kt: e.matmul(po[:, 128:256], lhsT=ones64[:], rhs=pT[:, kt % 4, :],
                                                     start=False, stop=(kt == nkt - 1), skip_group_check=True),
                     reads=['ones64', pTk], writes=[pok])
            if c4 == nch - 1:
                rd, rdk = rdrot.next()
                P.op('dve', lambda e: e.reciprocal(out=rd[:], in_=po[:, 128:256]), reads=[pok], writes=[rdk])
                P.op('dve', lambda e: e.tensor_tensor(out=at[:, h, :], in0=po[:, 0:128], in1=rd[:], op=ALU.mult),
                     reads=[pok, rdk], writes=[atk])

        for idx in range(len(items) + LOOK):
            if idx < len(items):
                emit_qk(*items[idx])
            if idx >= LOOK:
                emit_rest(*items[idx - LOOK])
                yield
        P.dma('sp', lambda e: e.dma_start(out=attnT[:, :, tsl], in_=at[:]), reads=[atk])

    def drain(g):
        for _ in g:
            pass

    def merge(gens):
        gens = [[g, max(1, n), 0.0, True] for g, n in gens]
        total = max(n for _, n, _, _ in gens)
        for step in range(total + 1):
            for it in gens:
                it[2] += it[1] / total
                while it[3] and it[2] >= 1.0:
                    it[2] -= 1.0
                    try:
                        next(it[0])
                    except StopIteration:
                        it[3] = False
        for it in gens:
            if it[3]:
                drain(it[0])

    drain(indexer(0))
    drain(threshold(0))
    for qi in range(NT):
        if qi + 1 < NT:
            drain(indexer(qi + 1))
            merge([(attention(qi), 8 * ((qi + 4) // 4)), (threshold(qi + 1), NIT + 1)])
        else:
            drain(attention(qi))


def phase_E(P, T, G):
    LD = 0.6065306597126334
    ident = G['identb']
    zr = T['zr']
    def colload(name, n, key):
        t = P.sb([64, n], F32)
        P.dma('sp', lambda e: e.dma_start(out=t[:], in_=T[name].rearrange("(h p) -> p h", p=64), allow_slow_non_contiguous=True), writes=[key])
        return t
    mu_rkv = P.sb([64, 24], F32)
    P.dma('sp', lambda e: e.dma_start(out=mu_rkv[:], in_=T['tshift_mu'][0:1536].rearrange("(h p) -> p h", p=64), allow_slow_non_contiguous=True), writes=['mu'])
    mu_wa = P.sb([64, 2], F32)
    P.dma('sp', lambda e: e.dma_start(out=mu_wa[:], in_=T['tshift_mu'][1536:1664].rearrange("(h p) -> p h", p=64), allow_slow_non_contiguous=True), writes=['mu'])
    mu_g = P.sb([128, 1], F32)
    P.dma('sp', lambda e: e.dma_start(out=mu_g[:], in_=T['tshift_mu'][1664:1792].rearrange("(h p) -> p h", p=128), allow_slow_non_contiguous=True), writes=['mu'])
    om_rkv, om_wa, om_g = P.sb([64, 24], F32), P.sb([64, 2], F32), P.sb([128, 1], F32)
    for (o, m) in ((om_rkv, mu_rkv), (om_wa, mu_wa), (om_g, mu_g)):
        P.op('dve', lambda e, o=o, m=m: e.tensor_scalar(out=o[:], in0=m[:], scalar1=-1.0, scalar2=1.0, op0=ALU.mult, op1=ALU.add),
             reads=['mu'], writes=['om'])
    w0c = colload('decay_w0', 8, 'par'); a0c = colload('iclr_a0', 8, 'par'); kkc = colload('k_k', 8, 'par')
    kac = colload('k_a', 8, 'par'); rkc = colload('r_k', 8, 'par'); lgc = colload('lnx_g', 8, 'par'); lbc = colload('lnx_b', 8, 'par')
    omka = P.sb([64, 8], F32)
    P.op('dve', lambda e: e.tensor_scalar(out=omka[:], in0=kac[:], scalar1=-1.0, scalar2=1.0, op0=ALU.mult, op1=ALU.add),
         reads=['par'], writes=['omka'])
    dup, iup, gup = P.sb([64, 512], BF16), P.sb([64, 512], BF16), P.sb([128, 512], BF16)
    P.dma('pool', lambda e: e.dma_start(out=dup[:], in_=T['decay_up']), writes=['wts'])
    P.dma('pool', lambda e: e.dma_start(out=iup[:], in_=T['iclr_up']), writes=['wts'])
    P.dma('pool', lambda e: e.dma_start(out=gup[:], in_=T['gate_up']), writes=['wts'])
    onesf = P.sb([64, 64], F32)
    onesm = P.sb([64, 64], F32)
    gneps = P.sb([64, 1], F32)
    P.op('dve', lambda e: e.memset(onesf[:], 1.0), writes=['onesf'])
    P.op('dve', lambda e: e.memset(onesm[:], 1.0 / 64), writes=['onesm'])
    P.op('dve', lambda e: e.memset(gneps[:], 64e-5), writes=['gneps'])
    km = P.sb([128, 896], F32)
    P.dma('sp', lambda e: e.dma_start(out=km[:], in_=T['k_masks']), writes=['km'])
    mask4 = P.sb([128, 512], BF16)
    maskL = P.sb([128, 128], BF16)
    P.op('dve', lambda e: e.tensor_copy(out=mask4[:], in_=km[:, 0:512]), reads=['km'], writes=['mask4'])
    P.op('dve', lambda e: e.tensor_copy(out=maskL[:], in_=km[:, 512:640]), reads=['km'], writes=['maskL'])
    rst = P.sb([64, 512], F32)
    P.dma('sp', lambda e: e.dma_start(out=rst[:], in_=T['k_reset']), writes=['rst'])
    Tst = P.sb([64, 8, 64], BF16)
    P.op('dve', lambda e: e.memset(Tst[:], 0.0), writes=[f'T{h}' for h in range(8)])
    zrot = Rot(P, 1, [64, 3, 513], F32, 'z')
    wa_in = P.sb([64, 2, 513], F32)
    gd_in = P.sb([128, 513], F32)
    tmpr = Rot(P, 1, [128, 512], F32, 'tmp')
    twb, adb, sgb = P.sb([64, 512], BF16), P.sb([64, 512], BF16), P.sb([128, 512], BF16)
    AR = P.sb([64, 8, 4, 256], BF16)
    BK = P.sb([64, 8, 4, 256], BF16)
    tok3 = P.sb([128, 8, 4, 3, 64], BF16)
    pC = P.sb([64, 8, 4], F32)
    bon = P.sb([64, 8, 512], BF16)
    gg = P.sb([64, 8, 512], BF16)
    yT = P.sb([64, 8, 512], F32)
    RW = P.sb([64, 8, 512], BF16)
    hb = {n: P.sb([64, 512], F32) for n in ('sig', 'cs', 'ep', 'em', 'epv', 'kk', 'kkn', 'a', 't', 'kp', 'b', 'u1')}
    vb = P.sb([64, 512], BF16)
    Gms = [P.sb([128, 16, 512], BF16) for _ in range(2)]
    XY = [P.sb([128, 16, 256], BF16) for _ in range(2)]
    Nms = [P.sb([128, 16, 128], BF16) for _ in range(2)]
    Wsb, Usb = P.sb([128, 8, 64], BF16), P.sb([128, 8, 64], BF16)
    pg = Rot(P, 5, [128, 512], F32, 'pg', psum=True)
    pl = Rot(P, 2, [64, 512], F32, 'pl', psum=True)
    ptr = Rot(P, 1, [128, 3, 64], BF16, 'ptr', psum=True)
    rwT = T['rwT'].rearrange("(h p) t -> p h t", p=64)

    def dve(fn, reads, writes):
        P.op('dve', fn, reads=reads, writes=writes)

    for tg in range(8):
        t0 = tg * 512
        def load_halo(dst, rows, key, q, tg=tg, t0=t0):
            if tg == 0:
                src = rows(t0, t0 + 512)
                P.op('pool', lambda e: e.memset(dst[:, 0:1] if len(dst.shape) == 2 else dst[:, :, 0:1], 0.0), reads=[key], writes=[key])
                P.dma(q, lambda e: e.dma_start(out=(dst[:, 1:513] if len(dst.shape) == 2 else dst[:, :, 1:513]), in_=src), writes=[key + 'b'])
            else:
                src = rows(t0 - 1, t0 + 512)
                P.dma(q, lambda e: e.dma_start(out=dst[:], in_=src), reads=[key + 'b'], writes=[key])
        load_halo(wa_in, lambda a, b: zr[1536:1664, a:b].rearrange("(h p) t -> p h t", p=64), 'wa', 'sp')
        load_halo(gd_in, lambda a, b: zr[1664:1792, a:b], 'gd', 'act')

        def tshift(src_prev, src_cur, mu_ap, om_ap, np_, keys):
            tm, tk = tmpr.next()
            P.op('pool', lambda e: e.tensor_scalar(out=tm[:np_, :], in0=src_prev, scalar1=mu_ap, scalar2=0.0, op0=ALU.mult, op1=ALU.add),
                 reads=keys + ['mu'], writes=[tk])
            dve(lambda e: e.scalar_tensor_tensor(out=src_cur, in0=src_cur, scalar=om_ap, in1=tm[:np_, :], op0=ALU.mult, op1=ALU.add),
                keys + [tk, 'om'], keys)
        for i in range(2):
            tshift(wa_in[:, i, 0:512], wa_in[:, i, 1:513], mu_wa[:, i:i + 1], om_wa[:, i:i + 1], 64, ['wa', 'wab'])
        tshift(gd_in[:, 0:512], gd_in[:, 1:513], mu_g[:, 0:1], om_g[:, 0:1], 128, ['gd', 'gdb'])
        P.op('act', lambda e: e.activation(out=twb[:], in_=wa_in[:, 0, 1:513], func=AF.Tanh), reads=['wa', 'wab'], writes=['twb'])
        P.op('act', lambda e: e.activation(out=sgb[:], in_=gd_in[:, 1:513], func=AF.Sigmoid), reads=['gd', 'gdb'], writes=['sgb'])
        dve(lambda e: e.tensor_copy(out=adb[:], in_=wa_in[:, 1, 1:513]), ['wa', 'wab'], ['adb'])
        def prep_head(h, z, zk):
            def zrows(a, b, h=h):
                return zr[0:1536, a:b].rearrange("(s hh p) t -> hh p s t", s=3, p=64)[h]
            load_halo(z, zrows, zk, 'sp' if h % 2 == 0 else 'act')
            for s_ in range(3):
                tshift(z[:, s_, 0:512], z[:, s_, 1:513], mu_rkv[:, s_ * 8 + h:s_ * 8 + h + 1], om_rkv[:, s_ * 8 + h:s_ * 8 + h + 1], 64, [zk, zk + 'b'])
            r_, k_, v_ = z[:, 0, 1:513], z[:, 1, 1:513], z[:, 2, 1:513]
            zkeys = [zk, zk + 'b']
            sig, cs, ep, em, epv, kk, kkn, a_, t_, kp, b_, u1 = [hb[n] for n in ('sig', 'cs', 'ep', 'em', 'epv', 'kk', 'kkn', 'a', 't', 'kp', 'b', 'u1')]
            hs = slice(h * 64, (h + 1) * 64)
            p1, p1k = pl.next()
            mm(P, p1[:], dup[:, hs], twb[:], True, True, ['wts', 'twb'], [p1k])
            P.op('act', lambda e, p1=p1, h=h: e.activation(out=sig[:], in_=p1[:], func=AF.Sigmoid, bias=w0c[:, h:h + 1]),
                 reads=[p1k, 'par'], writes=['sig'])
            dve(lambda e: e.tensor_tensor_scan(out=cs[:], data0=rst[:], data1=sig[:], initial=0.0, op0=ALU.mult, op1=ALU.add),
                ['rst', 'sig'], ['cs'])
            P.op('act', lambda e: e.activation(out=ep[:], in_=cs[:], func=AF.Exp, scale=-LD), reads=['cs'], writes=['ep'])
            P.op('act', lambda e: e.activation(out=em[:], in_=cs[:], func=AF.Exp, scale=LD), reads=['cs'], writes=['em'])
            dve(lambda e: e.tensor_tensor(out=u1[:], in0=cs[:], in1=sig[:], op=ALU.subtract), ['cs', 'sig'], ['u1'])
            P.op('act', lambda e: e.activation(out=epv[:], in_=u1[:], func=AF.Exp, scale=-LD), reads=['u1'], writes=['epv'])
            dve(lambda e, h=h: e.tensor_copy(out=pC[:, h, :], in_=ep[:, 127:512:128]), ['ep'], ['pC'])
            p2, p2k = pl.next()
            mm(P, p2[:], iup[:, hs], adb[:], True, True, ['wts', 'adb'], [p2k])
            P.op('act', lambda e, p2=p2, h=h: e.activation(out=a_[:], in_=p2[:], func=AF.Sigmoid, bias=a0c[:, h:h + 1]),
                 reads=[p2k, 'par'], writes=['a'])
            p3, p3k = pl.next()
            mm(P, p3[:], gup[:, hs], sgb[:], True, True, ['wts', 'sgb'], [p3k])
            P.op('act', lambda e, p3=p3, h=h: e.activation(out=gg[:, h, :], in_=p3[:], func=AF.Copy), reads=[p3k], writes=[f'gg{h}'])
            dve(lambda e, h=h: e.tensor_scalar(out=kk[:], in0=k_, scalar1=kkc[:, h:h + 1], scalar2=None, op0=ALU.mult), zkeys + ['par'], ['kk'])
            P.op('act', lambda e: e.activation(out=u1[:], in_=kk[:], func=AF.Square), reads=['kk', 'u1'], writes=['u1'])
            p4, p4k = pl.next()
            mm(P, p4[:], onesf[:], u1[:], True, True, ['onesf', 'u1'], [p4k])
            P.op('act', lambda e, p4=p4: e.activation(out=kkn[:], in_=p4[:], func=AF.Sqrt), reads=[p4k], writes=['kkn'])
            dve(lambda e: e.tensor_scalar(out=kkn[:], in0=kkn[:], scalar1=1e-12, scalar2=None, op0=ALU.max), ['kkn'], ['kkn'])
            dve(lambda e: e.reciprocal(out=kkn[:], in_=kkn[:]), ['kkn'], ['kkn'])
            dve(lambda e: e.tensor_tensor(out=kkn[:], in0=kkn[:], in1=kk[:], op=ALU.mult), ['kkn', 'kk'], ['kkn'])
            dve(lambda e, h=h: e.tensor_scalar(out=t_[:], in0=a_[:], scalar1=kac[:, h:h + 1], scalar2=omka[:, h:h + 1], op0=ALU.mult, op1=ALU.add),
                ['a', 'par', 'omka'], ['t'])
            dve(lambda e: e.tensor_tensor(out=kp[:], in0=t_[:], in1=k_, op=ALU.mult), ['t'] + zkeys, ['kp'])
            dve(lambda e: e.tensor_tensor(out=b_[:], in0=kkn[:], in1=a_[:], op=ALU.mult), ['kkn', 'a'], ['b'])
            c4 = lambda ap: ap.rearrange("p (c t) -> p c t", c=4)
            dve(lambda e, h=h: e.tensor_tensor(out=AR[:, h, :, 128:256], in0=c4(r_), in1=c4(ep[:]), op=ALU.mult), zkeys + ['ep'], [f'AR{h}'])
            dve(lambda e, h=h: e.scalar_tensor_tensor(out=AR[:, h, :, 0:128], in0=c4(kkn[:]), scalar=-1.0, in1=c4(epv[:]), op0=ALU.mult, op1=ALU.mult),
                ['kkn', 'epv'], [f'AR{h}'])
            dve(lambda e, h=h: e.tensor_tensor(out=BK[:, h, :, 0:128], in0=c4(b_[:]), in1=c4(em[:]), op=ALU.mult), ['b', 'em'], [f'BK{h}'])
            dve(lambda e, h=h: e.tensor_tensor(out=BK[:, h, :, 128:256], in0=c4(kp[:]), in1=c4(em[:]), op=ALU.mult), ['kp', 'em'], [f'BK{h}'])
            dve(lambda e, h=h: e.scalar_tensor_tensor(out=u1[:], in0=r_, scalar=rkc[:, h:h + 1], in1=kp[:], op0=ALU.mult, op1=ALU.mult),
                zkeys + ['kp', 'par', 'u1'], ['u1'])
            p5, p5k = pl.next()
            mm(P, p5[:], onesf[:], u1[:], True, True, ['onesf', 'u1'], [p5k])
            dve(lambda e, p5=p5, h=h: e.tensor_tensor(out=bon[:, h, :], in0=p5[:], in1=v_, op=ALU.mult), [p5k] + zkeys, [f'bon{h}'])
            P.op('pool', lambda e: e.tensor_copy(out=vb[:], in_=v_), reads=zkeys, writes=['vb'])
            for c in range(4):
                pt_, ptk = ptr.next()
                cs_ = slice(c * 128, (c + 1) * 128)
                P.op('pe', lambda e, pt_=pt_, cs_=cs_: e.transpose(out=pt_[:, 0, :], in_=vb[:, cs_], identity=ident[0:64, 0:64]), reads=['vb'], writes=[ptk])
                P.op('pe', lambda e, pt_=pt_, h=h, c=c: e.transpose(out=pt_[:, 1, :], in_=BK[:, h, c, 0:128], identity=ident[0:64, 0:64]), reads=[f'BK{h}'], writes=[ptk])
                P.op('pe', lambda e, pt_=pt_, h=h, c=c: e.transpose(out=pt_[:, 2, :], in_=BK[:, h, c, 128:256], identity=ident[0:64, 0:64]), reads=[f'BK{h}'], writes=[ptk])
                P.op('act', lambda e, pt_=pt_, h=h, c=c: e.activation(out=tok3[:, h, c, :, :], in_=pt_[:], func=AF.Copy), reads=[ptk], writes=[f'tok{h}'])
        for h in range(8):
            z, zk = zrot.next()
            prep_head(h, z, zk)
        def stage1(cs, tg=tg):
            sl = (cs[0] // 2) % 2
            Gm, Nm = Gms[sl], Nms[sl]
            probs = [(ci, c, h) for ci, c in enumerate(cs) for h in range(8)]
            for (ci, c, h) in probs:
                q = ci * 8 + h
                p_, pk = pg.next()
                mm(P, p_[:, 0:256], BK[:, h, c, 0:128], AR[:, h, c, :], True, True, [f'BK{h}', f'AR{h}'], [pk])
                mm(P, p_[:, 256:512], BK[:, h, c, 128:256], AR[:, h, c, :], True, True, [f'BK{h}', f'AR{h}'], [pk])
                dve(lambda e, p_=p_, q=q: e.tensor_tensor(out=Gm[:, q, :], in0=p_[:], in1=mask4[:], op=ALU.mult), [pk, 'mask4'], [f'Gm{sl}_{q}'])
                p2_, p2k = pg.next()
                mm(P, p2_[:, 0:128], AR[:, h, c, 0:128], BK[:, h, c, 0:128], True, True, [f'BK{h}', f'AR{h}'], [p2k])
                dve(lambda e, p2_=p2_, q=q: e.tensor_tensor(out=XY[0][:, q, 128:256], in0=p2_[:, 0:128], in1=maskL[:], op=ALU.mult),
                    [p2k, 'maskL'], [f'XY0{q}'])
                P.op('pool', lambda e, q=q: e.tensor_copy(out=XY[0][:, q, 0:128], in_=Gm[:, q, 0:128]), reads=[f'Gm{sl}_{q}'], writes=[f'XY0{q}x'])
                P.op('pool', lambda e, q=q: e.tensor_tensor(out=Nm[:, q, :], in0=Gm[:, q, 0:128], in1=ident[:], op=ALU.add),
                     reads=[f'Gm{sl}_{q}'], writes=[f'N{sl}_{q}'])
            yield
            for j in range(6):
                cur, nxt = XY[j % 2], XY[(j + 1) % 2]
                ck, nk_ = f'XY{j % 2}', f'XY{(j + 1) % 2}'
                for q in range(len(probs)):
                    p_, pk = pg.next()
                    rk_ = [ck + f'{q}', ck + f'{q}x']
                    mm(P, p_[:, 0:128], cur[:, q, 128:256], cur[:, q, 0:128], True, True, rk_, [pk])
                    mm(P, p_[:, 128:256], cur[:, q, 0:128], cur[:, q, 128:256], True, True, rk_, [pk])
                    P.op('act', lambda e, p_=p_, nxt=nxt, q=q: e.activation(out=nxt[:, q, :], in_=p_[:, 0:256], func=AF.Copy),
                         reads=[pk], writes=[nk_ + f'{q}', nk_ + f'{q}x'])
                    if q % 4 == 3:
                        yield
                for q in range(len(probs)):
                    p_, pk = pg.next()
                    mm(P, p_[:, 0:128], nxt[:, q, 128:256], Nm[:, q, :], True, True, [nk_ + f'{q}', nk_ + f'{q}x', f'N{sl}_{q}'], [pk])
                    dve(lambda e, p_=p_, q=q: e.tensor_tensor(out=Nm[:, q, :], in0=p_[:, 0:128], in1=Nm[:, q, :], op=ALU.add),
                        [pk, f'N{sl}_{q}'], [f'N{sl}_{q}'])
                    if q % 4 == 3:
                        yield

        def stage2(c, tg=tg):
            sl = (c // 2) % 2
            Gm, Nm = Gms[sl], Nms[sl]
            ci = c % 2
            pw, pwk = pg.next()
            for h in range(8):
                q = ci * 8 + h
                mm(P, pw[:, h * 64:(h + 1) * 64], AR[:, h, c, 0:128], Tst[:, h, :], True, False, [f'AR{h}', f'T{h}'], [pwk])
                mm(P, pw[:, h * 64:(h + 1) * 64], Gm[:, q, 256:384], tok3[:, h, c, 0, :], False, True, [f'Gm{sl}_{q}', f'tok{h}'], [pwk])
            P.op('act', lambda e: e.activation(out=Wsb[:].rearrange("p h v -> p (h v)"), in_=pw[:], func=AF.Copy), reads=[pwk], writes=['W'])
            yield
            pu, puk = pg.next()
            for h in range(8):
                q = ci * 8 + h
                mm(P, pu[:, h * 64:(h + 1) * 64], Nm[:, q, :], Wsb[:, h, :], True, True, [f'N{sl}_{q}', 'W'], [puk])
            P.op('act', lambda e: e.activation(out=Usb[:].rearrange("p h v -> p (h v)"), in_=pu[:], func=AF.Copy), reads=[puk], writes=['U'])
            yield
            pt_, ptk = pg.next()
            for h in range(8):
                q = ci * 8 + h
                o_ = pt_[0:64, h * 64:(h + 1) * 64]
                mm(P, o_, ident[0:64, 0:64], Tst[:, h, :], True, False, [f'T{h}'], [ptk])
                mm(P, o_, tok3[:, h, c, 1, :], Usb[:, h, :], False, False, [f'tok{h}', 'U'], [ptk])
                mm(P, o_, tok3[:, h, c, 2, :], tok3[:, h, c, 0, :], False, True, [f'tok{h}'], [ptk])
            for half in range(2):
                py_, pyk = pg.next()
                for hh in range(4):
                    h = half * 4 + hh
                    q = ci * 8 + h
                    o_ = py_[0:64, hh * 128:(hh + 1) * 128]
                    mm(P, o_, Tst[:, h, :], AR[:, h, c, 128:256], True, False, [f'AR{h}', f'T{h}'], [pyk])
                    mm(P, o_, Usb[:, h, :], Gm[:, q, 128:256], False, False, ['U', f'Gm{sl}_{q}'], [pyk])
                    mm(P, o_, tok3[:, h, c, 0, :], Gm[:, q, 384:512], False, True, [f'tok{h}', f'Gm{sl}_{q}'], [pyk])
                P.op('act', lambda e, py_=py_, half=half: e.activation(
                    out=yT[:, half * 4:half * 4 + 4, c * 128:(c + 1) * 128], in_=py_[0:64, :].rearrange("p (h t) -> p h t", h=4), func=AF.Copy),
                    reads=[pyk], writes=[f'yT{half}'])
            for h in range(8):
                dve(lambda e, h=h: e.tensor_scalar(out=Tst[:, h, :], in0=pt_[0:64, h * 64:(h + 1) * 64], scalar1=pC[:, h, c:c + 1], scalar2=None, op0=ALU.mult),
                    [ptk, 'pC', f'T{h}'], [f'T{h}'])
            yield

        def chain(*gs):
            for g in gs:
                yield from g

        def rr(g1, g2):
            a = b = True
            while a or b:
                if a:
                    try:
                        next(g1)
                    except StopIteration:
                        a = False
                if b:
                    try:
                        next(g2)
                    except StopIteration:
                        b = False
        for _ in stage1([0, 1]):
            pass
        rr(stage1([2, 3]), chain(stage2(0), stage2(1)))
        for _ in chain(stage2(2), stage2(3)):
            pass
        for h in range(8):
            u1, u2 = hb['u1'], hb['t']
            p1, p1k = pl.next()
            mm(P, p1[:], onesm[:], yT[:, h, :], True, True, ['onesm', f'yT{h // 4}'], [p1k])
            dve(lambda e, p1=p1, h=h: e.tensor_tensor(out=u1[:], in0=yT[:, h, :], in1=p1[:], op=ALU.subtract), [p1k, f'yT{h // 4}', 'u1'], ['u1'])
            P.op('act', lambda e: e.activation(out=u2[:], in_=u1[:], func=AF.Square), reads=['u1', 't'], writes=['t'])
            p2, p2k = pl.next()
            mm(P, p2[:], onesm[:], u2[:], True, True, ['onesm', 't'], [p2k])
            P.op('act', lambda e, p2=p2: e.activation(out=u2[:], in_=p2[:], func=AF.Sqrt, bias=gneps[:, 0:1]), reads=[p2k, 'gneps', 't'], writes=['t'])
            dve(lambda e: e.reciprocal(out=u2[:], in_=u2[:]), ['t'], ['t'])
            dve(lambda e: e.tensor_tensor(out=u1[:], in0=u1[:], in1=u2[:], op=ALU.mult), ['u1', 't'], ['u1'])
            dve(lambda e, h=h: e.tensor_scalar(out=u1[:], in0=u1[:], scalar1=lgc[:, h:h + 1], scalar2=lbc[:, h:h + 1], op0=ALU.mult, op1=ALU.add),
                ['u1', 'par'], ['u1'])
            dve(lambda e, h=h: e.tensor_tensor(out=u1[:], in0=u1[:], in1=bon[:, h, :], op=ALU.add), ['u1', f'bon{h}'], ['u1'])
            dve(lambda e, h=h: e.tensor_tensor(out=RW[:, h, :], in0=u1[:], in1=gg[:, h, :], op=ALU.mult), ['u1', f'gg{h}'], ['RW'])
        P.dma('sp', lambda e, t0=t0: e.dma_start(out=rwT[:, :, t0:t0 + 512], in_=RW[:]), reads=['RW'])
        if tg == 0 and 'dbg_y' in T:
            P.dma('sp', lambda e: e.dma_start(out=T['dbg_y'], in_=yT[:]), reads=['yT0', 'yT1'])
            P.dma('sp', lambda e: e.dma_start(out=T['dbg_bon'], in_=bon[:]), reads=[f'bon{h}' for h in range(8)])
            P.dma('sp', lambda e: e.dma_start(out=T['dbg_g'], in_=gg[:]), reads=[f'gg{h}' for h in range(8)])
            P.dma('sp', lambda e: e.dma_start(out=T['dbg_AR'], in_=AR[:]), reads=[f'AR{h}' for h in range(8)])
            P.dma('sp', lambda e: e.dma_start(out=T['dbg_BK'], in_=BK[:]), reads=[f'BK{h}' for h in range(8)])


def phase_F(P, T, G):
    wa, wr, wo = P.sb([128, 4, D], BF16), P.sb([128, 4, D], BF16), P.sb([128, 8, D], BF16)
    P.dma('pool', lambda e: e.dma_start(out=wa[:], in_=T['w_attn_br'].rearrange("(j p) d -> p j d", p=128)), writes=['wa'])
    P.dma('pool', lambda e: e.dma_start(out=wr[:], in_=T['w_rwkv_br'].rearrange("(j p) d -> p j d", p=128)), writes=['wr'])
    P.dma('pool', lambda e: e.dma_start(out=wo[:], in_=T['w_out'].rearrange("(j p) d -> p j d", p=128)), writes=['wo'])
    W = dict(junk=P.sb([128, D], F32), ss=P.sb([128, 1], F32), rs=P.sb([128, 1], F32), t1=P.sb([128, D], F32),
             hb=P.sb([128, D], BF16), pt=P.ps([128, 8, 128], BF16))
    W['mod'] = P.sb([128, 3072], F32)
    P.dma('sp', lambda e: e.dma_start(out=W['mod'][:], in_=T['modd'][:, 2048:5120]), writes=['modl'])
    xrot = Rot(P, 2, [128, D], F32, 'x')
    atr, rtr = Rot(P, 2, [128, 4, 128], BF16, 'at'), Rot(P, 2, [128, 4, 128], BF16, 'rt')
    gar, grr = Rot(P, 2, [128, 8, 128], BF16, 'ga'), Rot(P, 2, [128, 8, 128], BF16, 'gr')
    sga, sgr = P.sb([128, 8, 128], F32), P.sb([128, 8, 128], F32)
    m1, m2 = P.sb([128, 4, 128], F32), P.sb([128, 4, 128], F32)
    mixT = P.sb([128, 8, 128], BF16)
    x1t = P.sb([128, D], F32)
    h2t = P.sb([128, 8, 128], BF16)
    pA = Rot(P, 1, [128, 4, 128], F32, 'pA', psum=True)
    pR = Rot(P, 1, [128, 4, 128], F32, 'pR', psum=True)
    po = Rot(P, 2, [128, 512], F32, 'po', psum=True)
    aT = T['attnT'].rearrange("(j p) t -> p j t", p=128)
    rT = T['rwT'].rearrange("(j p) t -> p j t", p=128)
    gaT = T['zga'].rearrange("(j p) t -> p j t", p=128)
    grT = T['zgr'].rearrange("(j p) t -> p j t", p=128)
    h2T = T['h2T'].rearrange("(k p) t -> p k t", p=128)
    R = router_setup(P, T, G) if G.get('sparse') else None
    for i in range(NT):
        tsl = slice(i * 128, (i + 1) * 128)
        xt, xk = xrot.next()
        at, atk = atr.next(); rt, rtk = rtr.next(); ga, gak = gar.next(); gr, grk = grr.next()
        P.dma('sp', lambda e, xt=xt, tsl=tsl: e.dma_start(out=xt[:], in_=T['x'][tsl, :]), writes=[xk])
        P.dma('act', lambda e, at=at, tsl=tsl: e.dma_start(out=at[:], in_=aT[:, :, tsl]), writes=[atk])
        P.dma('act', lambda e, rt=rt, tsl=tsl: e.dma_start(out=rt[:], in_=rT[:, :, tsl]), writes=[rtk])
        P.dma('sp', lambda e, ga=ga, tsl=tsl: e.dma_start(out=ga[:], in_=gaT[:, :, tsl]), writes=[gak])
        P.dma('sp', lambda e, gr=gr, tsl=tsl: e.dma_start(out=gr[:], in_=grT[:, :, tsl]), writes=[grk])
        P.op('act', lambda e, ga=ga: e.activation(out=sga[:], in_=ga[:], func=AF.Sigmoid), reads=[gak], writes=['sga'])
        P.op('act', lambda e, gr=gr: e.activation(out=sgr[:], in_=gr[:], func=AF.Sigmoid), reads=[grk], writes=['sgr'])
        for half in range(2):
            pa, pak = pA.next(); pr, prk = pR.next()
            for s_ in range(4):
                dt = half * 4 + s_
                for j in range(4):
                    mm(P, pa[:, s_, :], wa[:, j, dt * 128:(dt + 1) * 128], at[:, j, :], j == 0, j == 3, ['wa', atk], [pak])
            for s_ in range(4):
                dt = half * 4 + s_
                for j in range(4):
                    mm(P, pr[:, s_, :], wr[:, j, dt * 128:(dt + 1) * 128], rt[:, j, :], j == 0, j == 3, ['wr', rtk], [prk])
            hs = slice(half * 4, half * 4 + 4)
            P.op('dve', lambda e, pa=pa, hs=hs: e.tensor_tensor(out=m1[:], in0=pa[:], in1=sga[:, hs, :], op=ALU.mult), reads=[pak, 'sga'], writes=['m1'])
            P.op('dve', lambda e, pr=pr, hs=hs: e.tensor_tensor(out=m2[:], in0=pr[:], in1=sgr[:, hs, :], op=ALU.mult), reads=[prk, 'sgr'], writes=['m2'])
            P.op('pool', lambda e, hs=hs: e.tensor_tensor(out=mixT[:, hs, :], in0=m1[:], in1=m2[:], op=ALU.add), reads=['m1', 'm2'], writes=['mixT'])
        for half in range(2):
            p_, pk = po.next()
            cs_ = slice(half * 512, (half + 1) * 512)
            for dt in range(8):
                mm(P, p_[:], mixT[:, dt, :], wo[:, dt, cs_], dt == 0, dt == 7, ['mixT', 'wo'], [pk])
            P.op('dve', lambda e, p_=p_, cs_=cs_: e.tensor_tensor(out=x1t[:, cs_], in0=p_[:], in1=W['mod'][:, cs_], op=ALU.mult),
                 reads=[pk, 'modl'], writes=['x1t'])
        P.op('pool', lambda e, xt=xt: e.tensor_tensor(out=x1t[:], in0=x1t[:], in1=xt[:], op=ALU.add), reads=['x1t', xk], writes=['x1t'])
        P.dma('sp', lambda e, tsl=tsl: e.dma_start(out=T['x1'][tsl, :], in_=x1t[:]), reads=['x1t'])
        norm_mod_transpose(P, G, x1t, 'x1t', h2t, 0, slice(2048, 3072), slice(1024, 2048), W)
        P.dma('act', lambda e, tsl=tsl: e.dma_start(out=h2T[:, :, tsl], in_=h2t[:]), reads=['hT0'])
        if R is not None:
            P.dma('act', lambda e, tsl=tsl: e.dma_start(out=T['h2tok'][tsl, :], in_=W['hb'][:]), reads=['hb'])
            router_tile(P, T, R, h2t, 'hT0', i)
    if R is not None:
        P.dma('sp', lambda e: e.dma_start(out=T['cntd'], in_=R['cnt'][:]), reads=['cnt'])


def phase_G0(P, T, G):
    for e in range(64):
        for (src, dst) in (('exp_gate', 'wg16'), ('exp_up', 'wu16'), ('exp_down', 'wd16')):
            P.dma('pool', lambda e_, e=e, src=src, dst=dst: e_.dma_start(
                out=T[dst][e].rearrange("k p f -> (k p f)").rearrange("(a b) -> a b", b=2048), in_=T[src][e].rearrange("r c -> (r c)").rearrange("(a b) -> a b", b=2048)))
    for (src, dst) in (('sh_gate', 'wg16'), ('sh_up', 'wu16'), ('sh_down', 'wd16')):
        P.dma('pool', lambda e_, src=src, dst=dst: e_.dma_start(
            out=T[dst][64].rearrange("k p f -> (k p f)").rearrange("(a b) -> a b", b=2048), in_=T[src].rearrange("r c -> (r c)").rearrange("(a b) -> a b", b=2048)))


def phase_G(P, T, G):
    ident = G['identb']
    h2T = T['h2T'].rearrange("(k p) t -> p k t", p=128)
    yacc = G['yacc']
    gwT = P.sb([64, S], BF16)
    rwt = P.sb([128, 8, 64], BF16)
    rbias = P.sb([128, 64], F32)
    P.dma('pool', lambda e: e.dma_start(out=rwt[:], in_=T['router_w'].rearrange("(k p) n -> p k n", p=128)), writes=['rwt'])
    P.dma('sp', lambda e: e.dma_start(out=rbias[:], in_=T['router_bias'].partition_broadcast(128)), writes=['rbias'])
    ones128 = P.sb([64, 128], BF16)
    P.op('dve', lambda e: e.memset(ones128[:], 1.0), writes=['ones128'])
    hrot = Rot(P, 2, [128, 8, 256], BF16, 'h2g')
    pmisc = P.ps([128, 512], F32)
    ptb = P.ps([64, 128], BF16)
    emb = P.sb([128, 64], BF16)
    sc_, ch, tmp, cm, em = [P.sb([128, 64], F32) for _ in range(5)]
    m1, m2, grp, s8, gmask, pen, den = [P.sb([128, 8], F32) for _ in range(7)]
    dve = lambda fn, r, w: P.op('dve', fn, reads=r, writes=w)
    for tgp in range(16):
        hg, hk = hrot.next()
        P.dma('sp', lambda e, hg=hg, tgp=tgp: e.dma_start(out=hg[:], in_=h2T[:, :, tgp * 256:(tgp + 1) * 256]), writes=[hk])
        for tt in range(2):
            i = tgp * 2 + tt
            p_, pk = pmisc[:, 0:64], 'pm_a'
            for k in range(8):
                mm(P, p_, hg[:, k, tt * 128:(tt + 1) * 128], rwt[:, k, :], k == 0, k == 7, [hk, 'rwt'], [pk])
            P.op('act', lambda e, p_=p_: e.activation(out=sc_[:], in_=p_, func=AF.Sigmoid), reads=[pk], writes=['sc'])
            dve(lambda e: e.tensor_tensor(out=ch[:], in0=sc_[:], in1=rbias[:], op=ALU.add), ['sc', 'rbias'], ['ch'])
            ch3 = ch[:].rearrange("p (g e) -> p g e", g=8)
            dve(lambda e, ch3=ch3: e.tensor_reduce(out=m1[:], in_=ch3, axis=AX.X, op=ALU.max), ['ch'], ['m1'])
            for g in range(8):
                dve(lambda e, g=g: e.tensor_scalar(out=tmp[:, g * 8:(g + 1) * 8], in0=ch[:, g * 8:(g + 1) * 8], scalar1=m1[:, g:g + 1],
                                                  scalar2=-1e9, op0=ALU.is_equal, op1=ALU.mult), ['ch', 'm1', 'tmp'], ['tmp'])
            dve(lambda e: e.tensor_tensor(out=tmp[:], in0=tmp[:], in1=ch[:], op=ALU.add), ['tmp', 'ch'], ['tmp'])
            dve(lambda e: e.tensor_reduce(out=m2[:], in_=tmp[:].rearrange("p (g e) -> p g e", g=8), axis=AX.X, op=ALU.max), ['tmp'], ['m2'])
            dve(lambda e: e.tensor_tensor(out=grp[:], in0=m1[:], in1=m2[:], op=ALU.add), ['m1', 'm2'], ['grp'])
            dve(lambda e: e.max(out=s8[:], in_=grp[:]), ['grp'], ['s8'])
            dve(lambda e: e.tensor_scalar(out=gmask[:], in0=grp[:], scalar1=s8[:, 3:4], scalar2=None, op0=ALU.is_ge), ['grp', 's8'], ['gmask'])
            dve(lambda e: e.tensor_scalar(out=pen[:], in0=gmask[:], scalar1=-1.0, scalar2=1e9, op0=ALU.add, op1=ALU.mult), ['gmask'], ['pen'])
            for g in range(8):
                dve(lambda e, g=g: e.tensor_scalar(out=cm[:, g * 8:(g + 1) * 8], in0=ch[:, g * 8:(g + 1) * 8], scalar1=pen[:, g:g + 1],
                                                  scalar2=None, op0=ALU.add), ['ch', 'pen', 'cm'], ['cm'])
            dve(lambda e: e.max(out=s8[:], in_=cm[:]), ['cm', 's8'], ['s8'])
            dve(lambda e: e.tensor_scalar(out=em[:], in0=cm[:], scalar1=s8[:, 7:8], scalar2=None, op0=ALU.is_ge), ['cm', 's8'], ['em'])
            dve(lambda e: e.tensor_tensor(out=em[:], in0=em[:], in1=sc_[:], op=ALU.mult), ['em', 'sc'], ['em'])
            dve(lambda e: e.tensor_reduce(out=den[:, 0:1], in_=em[:], axis=AX.X, op=ALU.add), ['em'], ['den'])
            dve(lambda e: e.reciprocal(out=den[:, 1:2], in_=den[:, 0:1]), ['den'], ['den'])
            dve(lambda e: e.tensor_scalar(out=em[:], in0=em[:], scalar1=den[:, 1:2], scalar2=2.5, op0=ALU.mult, op1=ALU.mult), ['em', 'den'], ['em'])
            pt_, ptk = ptb[:], 'pm_b'
            dve(lambda e: e.tensor_copy(out=emb[:], in_=em[:]), ['em', 'emb'], ['emb'])
            P.op('pe', lambda e, pt_=pt_: e.transpose(out=pt_, in_=emb[:], identity=G['identb'][:]), reads=['emb'], writes=[ptk])
            P.op('act', lambda e, pt_=pt_, i=i: e.activation(out=gwT[:, i * 128:(i + 1) * 128], in_=pt_, func=AF.Copy), reads=[ptk], writes=['gwT'])
    if G.get('gstop') == 'router':
        return
    wgr = Rot(P, 2, [128, 8, 256], BF16, 'wg')
    wur = Rot(P, 2, [128, 8, 256], BF16, 'wu')
    wdr = Rot(P, 2, [128, 2, D], BF16, 'wd')
    selr = Rot(P, 2, [64, 128], BF16, 'sel')
    pgu = Rot(P, 2, [128, 4, 256], F32, 'pgu', psum=True)
    py = Rot(P, 2, [128, 512], F32, 'py', psum=True)
    sgr_ = Rot(P, 2, [128, 2, 256], F32, 'sg')
    tr_ = Rot(P, 2, [128, 2, 256], F32, 'tt')
    actr = Rot(P, 2, [128, 2, 256], BF16, 'act')
    for e_ in range(G.get('nexp', 65)):
        wg, wgk = wgr.next(); wu, wuk = wur.next(); wd, wdk = wdr.next()
        if e_ < 64:
            sg_, su_, sd_ = T['exp_gate'][e_], T['exp_up'][e_], T['exp_down'][e_]
        else:
            sg_, su_, sd_ = T['sh_gate'], T['sh_up'], T['sh_down']
        P.dma('pool', lambda e, wg=wg, sg_=sg_: e.dma_start(out=wg[:], in_=sg_.rearrange("(k p) f -> p k f", p=128)), writes=[wgk])
        P.dma('pool', lambda e, wu=wu, su_=su_: e.dma_start(out=wu[:], in_=su_.rearrange("(k p) f -> p k f", p=128)), writes=[wuk])
        P.dma('pool', lambda e, wd=wd, sd_=sd_: e.dma_start(out=wd[:], in_=sd_.rearrange("(k p) f -> p k f", p=128)), writes=[wdk])
        if e_ < 64:
            sel, selk = selr.next()
            P.op('pool', lambda e, sel=sel, e_=e_: e.tensor_scalar(out=sel[:], in0=ones128[:], scalar1=G['identf'][0:64, e_:e_ + 1], scalar2=0.0, op0=ALU.mult, op1=ALU.add),
                 reads=['ones128'], writes=[selk])
        for tgp in range(16):
            hg, hk = hrot.next()
            P.dma('sp' if tgp % 2 == 0 else 'act', lambda e, hg=hg, tgp=tgp: e.dma_start(out=hg[:], in_=h2T[:, :, tgp * 256:(tgp + 1) * 256]), writes=[hk])
            p_, pk = pgu.next()
            for s_, (w_, wk_) in enumerate(((wg, wgk), (wg, wgk), (wu, wuk), (wu, wuk))):
                ft = s_ % 2
                for k in range(8):
                    mm(P, p_[:, s_, :], w_[:, k, ft * 128:(ft + 1) * 128], hg[:, k, :], k == 0, k == 7, [wk_, hk], [pk])
            sg, sgk = sgr_.next(); t_, tk = tr_.next(); ac, ack = actr.next()
            P.op('act', lambda e, sg=sg, p_=p_: e.activation(out=sg[:], in_=p_[:, 0:2, :], func=AF.Silu), reads=[pk], writes=[sgk])
            P.op('dve', lambda e, sg=sg, p_=p_, t_=t_: e.tensor_tensor(out=t_[:], in0=p_[:, 2:4, :], in1=sg[:], op=ALU.mult), reads=[pk, sgk], writes=[tk])
            if e_ < 64:
                pw, pwk = pmisc[:, 256:512], 'pm_c'
                mm(P, pw, sel[:], gwT[:, tgp * 256:(tgp + 1) * 256], True, True, [selk, 'gwT'], [pwk])
                for ft in range(2):
                    P.op('dve', lambda e, ac=ac, t_=t_, pw=pw, ft=ft: e.tensor_tensor(out=ac[:, ft, :], in0=pw, in1=t_[:, ft, :], op=ALU.mult),
                         reads=[tk, pwk], writes=[ack])
            else:
                P.op('pool', lambda e, ac=ac, t_=t_: e.tensor_copy(out=ac[:], in_=t_[:]), reads=[tk], writes=[ack])
            for tt in range(2):
                i = tgp * 2 + tt
                for half in range(2):
                    q_, qk = py.next()
                    cs_ = slice(half * 512, (half + 1) * 512)
                    for ft in range(2):
                        mm(P, q_[:], ac[:, ft, tt * 128:(tt + 1) * 128], wd[:, ft, cs_], ft == 0, ft == 1, [ack, wdk], [qk])
                    if e_ == 0:
                        P.op('act', lambda e, q_=q_, i=i, cs_=cs_: e.activation(out=yacc[:, i, cs_], in_=q_[:], func=AF.Copy), reads=[qk], writes=[f'y{i}'])
                    else:
                        P.op('dve', lambda e, q_=q_, i=i, cs_=cs_: e.tensor_tensor(out=yacc[:, i, cs_], in0=q_[:], in1=yacc[:, i, cs_], op=ALU.add),
                             reads=[qk, f'y{i}'], writes=[f'y{i}'])


def phase_H(P, T, G):
    yacc = G['yacc']
    dve = lambda fn, r, w: P.op('dve', fn, reads=r, writes=w)
    g2b = P.sb([128, D], F32)
    fing = P.sb([128, D], F32)
    P.dma('sp', lambda e: e.dma_start(out=g2b[:], in_=T['modd'][:, 5120:6144]), writes=['g2b'])
    P.dma('sp', lambda e: e.dma_start(out=fing[:], in_=T['final_g'].partition_broadcast(128)), writes=['fing'])
    xr = Rot(P, 2, [128, D], F32, 'x1')
    junk, ss, rs = P.sb([128, D], F32), P.sb([128, 1], F32), P.sb([128, 1], F32)
    for i in range(NT):
        tsl = slice(i * 128, (i + 1) * 128)
        xt, xk = xr.next()
        P.dma('sp', lambda e, xt=xt, tsl=tsl: e.dma_start(out=xt[:], in_=T['x1'][tsl, :]), writes=[xk])
        dve(lambda e, i=i: e.tensor_tensor(out=yacc[:, i, :], in0=yacc[:, i, :], in1=g2b[:], op=ALU.mult), [f'y{i}', 'g2b'], [f'y{i}'])
        P.op('pool', lambda e, i=i, xt=xt: e.tensor_tensor(out=xt[:], in0=xt[:], in1=yacc[:, i, :], op=ALU.add), reads=[f'y{i}', xk], writes=[xk])
        rms_rstd(P, xt, xk, junk, ss, rs, 'f')
        dve(lambda e, xt=xt: e.scalar_tensor_tensor(out=xt[:], in0=xt[:], scalar=rs[:, 0:1], in1=fing[:], op0=ALU.mult, op1=ALU.mult),
            [xk, 'rsf', 'fing'], [xk])
        P.dma('sp', lambda e, xt=xt, tsl=tsl: e.dma_start(out=T['out'][tsl, :], in_=xt[:]), reads=[xk])


NBLK = 320


def router_setup(P, T, G):
    R = {}
    R['rwt'] = P.sb([128, 8, 64], BF16)
    R['rbias'] = P.sb([128, 64], F32)
    R['iota'] = P.sb([128, 64], F32)
    R['su'] = P.sb([128, 128], BF16)
    R['onesb'] = P.sb([128, 128], BF16)
    R['cnt'] = P.sb([128, 64], F32)
    R['kmf'] = P.sb([128, 128], F32)
    P.dma('pool', lambda e: e.dma_start(out=R['rwt'][:], in_=T['router_w'].rearrange("(k p) n -> p k n", p=128)), writes=['rwt'])
    P.dma('sp', lambda e: e.dma_start(out=R['rbias'][:], in_=T['router_bias'].partition_broadcast(128)), writes=['rbias'])
    P.dma('sp', lambda e: e.dma_start(out=R['iota'][:], in_=T['k_rel'][0:1, 0:64].rearrange("o f -> (o f)").partition_broadcast(128)), writes=['iota'])
    P.dma('sp', lambda e: e.dma_start(out=R['kmf'][:], in_=T['k_masks'][:, 0:128]), writes=['kmf'])
    P.op('dve', lambda e: e.tensor_copy(out=R['su'][:], in_=R['kmf'][:]), reads=['kmf'], writes=['su'])
    P.op('dve', lambda e: e.memset(R['onesb'][:], 1.0), writes=['onesb'])
    P.op('dve', lambda e: e.memset(R['cnt'][:], 0.0), writes=['cnt'])
    R['pm'] = P.ps([128, 512], F32)
    for n in ('sc', 'ch', 'tmp', 'cm', 'em', 'mk', 'oh', 'rk', 'jk'):
        R[n] = P.sb([128, 64], F32)
    R['mkb'] = P.sb([128, 64], BF16)
    for n in ('m1', 'm2', 'grp', 's8', 'gmask', 'pen', 'den', 'i8f'):
        R[n] = P.sb([128, 8], F32)
    R['i8u'] = P.sb([128, 8], U32)
    R['meta'] = P.sb([128, 24], F32)
    return R


def router_tile(P, T, R, h2t, h2k, i):
    dve = lambda fn, r, w: P.op('dve', fn, reads=r, writes=w)
    pm = R['pm']
    sc_, ch, tmp, cm, em, mk, oh, rk, jk, mkb = [R[n] for n in ('sc', 'ch', 'tmp', 'cm', 'em', 'mk', 'oh', 'rk', 'jk', 'mkb')]
    m1, m2, grp, s8, gmask, pen, den, i8f, i8u, meta = [R[n] for n in ('m1', 'm2', 'grp', 's8', 'gmask', 'pen', 'den', 'i8f', 'i8u', 'meta')]
    for k in range(8):
        mm(P, pm[:, 0:64], h2t[:, k, :], R['rwt'][:, k, :], k == 0, k == 7, [h2k, 'rwt'], ['pm_a'])
    P.op('act', lambda e: e.activation(out=sc_[:], in_=pm[:, 0:64], func=AF.Sigmoid), reads=['pm_a'], writes=['sc'])
    dve(lambda e: e.tensor_tensor(out=ch[:], in0=sc_[:], in1=R['rbias'][:], op=ALU.add), ['sc', 'rbias'], ['ch'])
    dve(lambda e: e.tensor_reduce(out=m1[:], in_=ch[:].rearrange("p (g e) -> p g e", g=8), axis=AX.X, op=ALU.max), ['ch'], ['m1'])
    for g in range(8):
        dve(lambda e, g=g: e.tensor_scalar(out=tmp[:, g * 8:(g + 1) * 8], in0=ch[:, g * 8:(g + 1) * 8], scalar1=m1[:, g:g + 1],
                                          scalar2=-1e9, op0=ALU.is_equal, op1=ALU.mult), ['ch', 'm1', 'tmp'], ['tmp'])
    dve(lambda e: e.tensor_tensor(out=tmp[:], in0=tmp[:], in1=ch[:], op=ALU.add), ['tmp', 'ch'], ['tmp'])
    dve(lambda e: e.tensor_reduce(out=m2[:], in_=tmp[:].rearrange("p (g e) -> p g e", g=8), axis=AX.X, op=ALU.max), ['tmp'], ['m2'])
    dve(lambda e: e.tensor_tensor(out=grp[:], in0=m1[:], in1=m2[:], op=ALU.add), ['m1', 'm2'], ['grp'])
    dve(lambda e: e.max(out=s8[:], in_=grp[:]), ['grp'], ['s8'])
    dve(lambda e: e.tensor_scalar(out=gmask[:], in0=grp[:], scalar1=s8[:, 3:4], scalar2=None, op0=ALU.is_ge), ['grp', 's8'], ['gmask'])
    dve(lambda e: e.tensor_scalar(out=pen[:], in0=gmask[:], scalar1=-1.0, scalar2=1e9, op0=ALU.add, op1=ALU.mult), ['gmask'], ['pen'])
    for g in range(8):
        dve(lambda e, g=g: e.tensor_scalar(out=cm[:, g * 8:(g + 1) * 8], in0=ch[:, g * 8:(g + 1) * 8], scalar1=pen[:, g:g + 1],
                                          scalar2=None, op0=ALU.add), ['ch', 'pen', 'cm'], ['cm'])
    dve(lambda e: e.max(out=s8[:], in_=cm[:]), ['cm', 's8'], ['s8'])
    dve(lambda e: e.max_index(out=i8u[:], in_max=s8[:], in_values=cm[:]), ['cm', 's8', 'i8u'], ['i8u'])
    dve(lambda e: e.tensor_copy(out=meta[:, 0:8], in_=i8u[:]), ['i8u', 'meta'], ['meta'])
    dve(lambda e: e.tensor_scalar(out=mk[:], in0=cm[:], scalar1=s8[:, 7:8], scalar2=None, op0=ALU.is_ge), ['cm', 's8'], ['mk'])
    dve(lambda e: e.tensor_copy(out=mkb[:], in_=mk[:]), ['mk', 'mkb'], ['mkb'])
    dve(lambda e: e.tensor_tensor(out=em[:], in0=mk[:], in1=sc_[:], op=ALU.mult), ['mk', 'sc'], ['em'])
    dve(lambda e: e.tensor_reduce(out=den[:, 0:1], in_=em[:], axis=AX.X, op=ALU.add), ['em'], ['den'])
    dve(lambda e: e.reciprocal(out=den[:, 1:2], in_=den[:, 0:1]), ['den'], ['den'])
    dve(lambda e: e.tensor_scalar(out=em[:], in0=em[:], scalar1=den[:, 1:2], scalar2=2.5, op0=ALU.mult, op1=ALU.mult), ['em', 'den'], ['em'])
    mm(P, pm[:, 64:128], R['su'][:], mkb[:], True, True, ['su', 'mkb'], ['pm_b'])
    mm(P, pm[:, 128:192], R['onesb'][:], mkb[:], True, True, ['onesb', 'mkb'], ['pm_c'])
    dve(lambda e: e.tensor_tensor(out=rk[:], in0=pm[:, 64:128], in1=R['cnt'][:], op=ALU.add), ['pm_b', 'cnt', 'rk'], ['rk'])
    dve(lambda e: e.tensor_tensor(out=R['cnt'][:], in0=pm[:, 128:192], in1=R['cnt'][:], op=ALU.add), ['pm_c', 'cnt'], ['cnt'])
    for k in range(8):
        dve(lambda e, k=k: e.tensor_scalar(out=oh[:], in0=R['iota'][:], scalar1=meta[:, k:k + 1], scalar2=None, op0=ALU.is_equal),
            ['iota', 'meta', 'oh'], ['oh'])
        dve(lambda e, k=k: e.scalar_tensor_tensor(out=jk[:], in0=oh[:], scalar=1.0, in1=rk[:], op0=ALU.mult, op1=ALU.mult, accum_out=meta[:, 8 + k:9 + k]), ['oh', 'rk', 'jk', 'meta'], ['jk', 'meta'])
        dve(lambda e, k=k: e.scalar_tensor_tensor(out=jk[:], in0=oh[:], scalar=1.0, in1=em[:], op0=ALU.mult, op1=ALU.mult, accum_out=meta[:, 16 + k:17 + k]), ['oh', 'em', 'jk', 'meta'], ['jk', 'meta'])
    P.dma('sp', lambda e: e.dma_start(out=T['meta'][i * 128:(i + 1) * 128, :], in_=meta[:]), reads=['meta'])


def phase_S0(P, T, G):
    st32 = Rot(P, 3, [128, 2048], F32, 's32')
    st16 = Rot(P, 3, [128, 2048], BF16, 's16')
    n = 0
    for e in range(65):
        for (src, shsrc, dst) in (('exp_gate', 'sh_gate', 'wg16'), ('exp_up', 'sh_up', 'wu16'), ('exp_down', 'sh_down', 'wd16')):
            s_ap = T[src][e] if e < 64 else T[shsrc]
            a, ak = st32.next()
            c, ck = st16.next()
            f = 256 if dst != 'wd16' else D
            q = 'sp' if n % 2 == 0 else 'act'
            P.dma(q, lambda e_, a=a, s_ap=s_ap, f=f: e_.dma_start(out=a[:].rearrange("p (k f) -> p k f", f=f), in_=s_ap.rearrange("(k p) f -> p k f", p=128)), writes=[ak])
            eng = ('act', 'dve', 'pool')[n % 3]
            if eng == 'act':
                P.op('act', lambda e_, a=a, c=c: e_.activation(out=c[:], in_=a[:], func=AF.Copy), reads=[ak], writes=[ck])
            else:
                P.op(eng, lambda e_, a=a, c=c: e_.tensor_copy(out=c[:], in_=a[:]), reads=[ak], writes=[ck])
            P.dma('act' if n % 2 == 0 else 'sp', lambda e_, c=c, dst=dst, e=e: e_.dma_start(out=T[dst][e * 128:(e + 1) * 128, :], in_=c[:]), reads=[ck])
            n += 1


def phase_S(P, T, G):
    identb = G['identb']
    dve = lambda fn, r, w: P.op('dve', fn, reads=r, writes=w)
    cnt = P.sb([128, 64], F32)
    ci = P.sb([128, 64], I32)
    pad = P.sb([128, 64], F32)
    pend = P.sb([128, 64], F32)
    pst = P.sb([128, 64], F32)
    ones64f = P.sb([128, 64], F32)
    iota = P.sb([128, 64], F32)
    bst = P.sb([128, NBLK], F32)
    bef = P.sb([128, NBLK], F32)
    bei = P.sb([128, NBLK], I32)
    P.dma('sp', lambda e: e.dma_start(out=cnt[:], in_=T['cntd']), writes=['cnt'])
    P.dma('sp', lambda e: e.dma_start(out=iota[:], in_=T['k_rel'][0:1, 0:64].rearrange("o f -> (o f)").partition_broadcast(128)), writes=['iota'])
    P.dma('sp', lambda e: e.dma_start(out=bst[:], in_=T['k_bst'].partition_broadcast(128)), writes=['bst'])
    dve(lambda e: e.memset(ones64f[:], 1.0), [], ['ones64f'])
    dve(lambda e: e.tensor_scalar(out=pad[:], in0=cnt[:], scalar1=127.0, scalar2=None, op0=ALU.add), ['cnt'], ['pad'])
    dve(lambda e: e.tensor_copy(out=ci[:], in_=pad[:]), ['pad'], ['ci'])
    dve(lambda e: e.tensor_scalar(out=ci[:], in0=ci[:], scalar1=7, scalar2=None, op0=ALU.arith_shift_right), ['ci'], ['ci'])
    dve(lambda e: e.tensor_scalar(out=ci[:], in0=ci[:], scalar1=7, scalar2=None, op0=ALU.logical_shift_left), ['ci'], ['ci'])
    dve(lambda e: e.tensor_copy(out=pad[:], in_=ci[:]), ['ci', 'pad'], ['pad'])
    dve(lambda e: e.tensor_tensor_scan(out=pend[:], data0=ones64f[:], data1=pad[:], initial=0.0, op0=ALU.mult, op1=ALU.add),
        ['ones64f', 'pad'], ['pend'])
    dve(lambda e: e.tensor_tensor(out=pst[:], in0=pend[:], in1=pad[:], op=ALU.subtract), ['pend', 'pad'], ['pst'])
    for ex in range(64):
        if ex == 0:
            dve(lambda e: e.tensor_scalar(out=bef[:], in0=bst[:], scalar1=pend[:, 0:1], scalar2=None, op0=ALU.is_ge), ['bst', 'pend'], ['bef'])
        else:
            dve(lambda e, ex=ex: e.scalar_tensor_tensor(out=bef[:], in0=bst[:], scalar=pend[:, ex:ex + 1], in1=bef[:], op0=ALU.is_ge, op1=ALU.add),
                ['bst', 'pend', 'bef'], ['bef'])
    dve(lambda e: e.tensor_scalar(out=bef[:], in0=bef[:], scalar1=63.0, scalar2=None, op0=ALU.min), ['bef'], ['bef'])
    pcol = P.sb([128, 1], F32)
    widxf = P.sb([128, NBLK], F32)
    widx = P.sb([128, NBLK], I32)
    P.dma('sp', lambda e: e.dma_start(out=pcol[:], in_=T['k_rel'][:, 0:1], allow_slow_non_contiguous=True), writes=['pcol'])
    dve(lambda e: e.tensor_scalar(out=pcol[:], in0=pcol[:], scalar1=-1.0, scalar2=None, op0=ALU.mult), ['pcol'], ['pcol'])
    dve(lambda e: e.tensor_scalar(out=widxf[:], in0=bef[:], scalar1=128.0, scalar2=pcol[:, 0:1], op0=ALU.mult, op1=ALU.add), ['bef', 'pcol'], ['widxf'])
    dve(lambda e: e.tensor_copy(out=widx[:], in_=widxf[:]), ['widxf'], ['widx'])
    d8i = P.sb([128, NT, 8], I32)
    gw8 = P.sb([128, NT, 8], F32)
    mrot = Rot(P, 2, [128, 24], F32, 'meta')
    hrot = Rot(P, 2, [128, D], BF16, 'htok')
    oh, jk = P.sb([128, 64], F32), P.sb([128, 64], F32)
    d8f = P.sb([128, 8], F32)
    for i in range(NT):
        tsl = slice(i * 128, (i + 1) * 128)
        mt, mtk = mrot.next()
        ht, htk = hrot.next()
        P.dma('sp', lambda e, mt=mt, tsl=tsl: e.dma_start(out=mt[:], in_=T['meta'][tsl, :]), writes=[mtk])
        P.dma('act', lambda e, ht=ht, tsl=tsl: e.dma_start(out=ht[:], in_=T['h2tok'][tsl, :]), writes=[htk])
        for k in range(8):
            dve(lambda e, mt=mt, k=k: e.tensor_scalar(out=oh[:], in0=iota[:], scalar1=mt[:, k:k + 1], scalar2=None, op0=ALU.is_equal),
                ['iota', mtk, 'oh'], ['oh'])
            dve(lambda e, k=k: e.scalar_tensor_tensor(out=jk[:], in0=oh[:], scalar=1.0, in1=pst[:], op0=ALU.mult, op1=ALU.mult, accum_out=d8f[:, k:k + 1]), ['oh', 'pst', 'jk', 'd8f'], ['jk', 'd8f'])
        dve(lambda e, mt=mt: e.tensor_tensor(out=d8f[:], in0=d8f[:], in1=mt[:, 8:16], op=ALU.add), ['d8f', mtk], ['d8f'])
        dve(lambda e, i=i: e.tensor_copy(out=d8i[:, i, :], in_=d8f[:]), ['d8f'], [f'd8i{i}'])
        dve(lambda e, mt=mt, i=i: e.tensor_copy(out=gw8[:, i, :], in_=mt[:, 16:24]), [mtk], [f'gw{i}'])
        for k in range(8):
            P.dma('pool', lambda e, ht=ht, i=i, k=k: e.indirect_dma_start(
                out=T['Xs'], out_offset=bass.IndirectOffsetOnAxis(ap=d8i[:, i, k:k + 1], axis=0), in_=ht[:], in_offset=None),
                reads=[htk, f'd8i{i}'], writes=['Xs'])
    wgu = Rot2(P, 3, [128, 2048], BF16, 'wgu')
    wdr = Rot(P, 3, [128, 2, D], BF16, 'wd')
    xbr = Rot(P, 3, [128, D], BF16, 'xb')
    xTr = Rot(P, 2, [128, 8, 128], BF16, 'xT')
    sgr_ = Rot(P, 2, [128, 256], F32, 'sg')
    acr = Rot(P, 2, [128, 256], BF16, 'ac')
    aTr = Rot(P, 2, [128, 2, 128], BF16, 'aT')
    ybr = Rot(P, 2, [128, D], BF16, 'yb')
    ptx = Rot(P, 1, [128, 8, 128], BF16, 'ptx', psum=True)
    pgu = Rot(P, 2, [128, 512], F32, 'pgu', psum=True)
    pta = Rot(P, 1, [128, 2, 128], BF16, 'pta', psum=True)
    pyd = Rot(P, 3, [128, 512], F32, 'pyd', psum=True)
    wg16 = (T['wg16'], 256)
    wu16 = (T['wu16'], 256)
    wd16 = (T['wd16'], D)
    st64 = lambda w: w[0][64 * 128:65 * 128, :]
    regn = [0]

    def dyn_load(q, dst, src4, b, key, rkeys):
        P.dma('pool', lambda e: e.indirect_dma_start(out=dst, out_offset=None, in_=src4[0],
                                                     in_offset=bass.IndirectOffsetOnAxis(ap=widx[:, b:b + 1], axis=0)),
              reads=['widx'] + rkeys, writes=[key])

    blocks = [('r', b) for b in range(NBLK)] + [('s', i) for i in range(NT)]
    st = {}

    def stageA(kind, b):
        (wga, wua), wk = wgu.next(); wd, wdk = wdr.next(); xb, xk = xbr.next()
        if kind == 'r':
            dyn_load('sp', wga[:], wg16, b, wk, [])
            dyn_load('act', wua[:], wu16, b, wk + 'u', [])
            dyn_load('sp', wd[:].rearrange("p k f -> p (k f)"), wd16, b, wdk, [])
            P.dma('act', lambda e: e.dma_start(out=xb[:], in_=T['Xs'][b * 128:(b + 1) * 128, :]), reads=['Xs'], writes=[xk])
        else:
            P.dma('sp', lambda e: e.dma_start(out=wga[:], in_=st64(wg16)), writes=[wk])
            P.dma('act', lambda e: e.dma_start(out=wua[:], in_=st64(wu16)), writes=[wk + 'u'])
            P.dma('sp', lambda e: e.dma_start(out=wd[:].rearrange("p k f -> p (k f)"), in_=st64(wd16)), writes=[wdk])
            P.dma('act', lambda e: e.dma_start(out=xb[:], in_=T['h2tok'][b * 128:(b + 1) * 128, :]), writes=[xk])
        px, pxk = ptx.next()
        for k in range(8):
            P.op('pe', lambda e, k=k: e.transpose(out=px[:, k, :], in_=xb[:, k * 128:(k + 1) * 128], identity=identb[:]), reads=[xk], writes=[pxk])
        xT, xTk = xTr.next()
        P.op('act', lambda e: e.activation(out=xT[:], in_=px[:], func=AF.Copy), reads=[pxk], writes=[xTk])
        pg_, pgk = pgu.next()
        for k in range(8):
            mm(P, pg_[:, 0:256], xT[:, k, :], wga[:, k * 256:(k + 1) * 256], k == 0, False, [xTk, wk], [pgk])
        for k in range(8):
            P.op('pe', lambda e, k=k: e.matmul(pg_[:, 256:512], lhsT=xT[:, k, :], rhs=wua[:, k * 256:(k + 1) * 256], start=False, stop=(k == 7), skip_group_check=True), reads=[xTk, wk + 'u'], writes=[pgk])
        st[(kind, b)] = (pg_, pgk, wd, wdk)

    def stageB(kind, b):
        pg_, pgk, wd, wdk = st.pop((kind, b))
        sg, sgk = sgr_.next(); ac, ack = acr.next()
        P.op('act', lambda e: e.activation(out=sg[:], in_=pg_[:, 0:256], func=AF.Silu), reads=[pgk], writes=[sgk])
        dve(lambda e: e.tensor_tensor(out=ac[:], in0=pg_[:, 256:512], in1=sg[:], op=ALU.mult), [pgk, sgk], [ack])
        pa, pak = pta.next()
        for ft in range(2):
            P.op('pe', lambda e, ft=ft: e.transpose(out=pa[:, ft, :], in_=ac[:, ft * 128:(ft + 1) * 128], identity=identb[:]), reads=[ack], writes=[pak])
        aT, aTk = aTr.next()
        dve(lambda e: e.tensor_copy(out=aT[:], in_=pa[:]), [pak], [aTk])
        yb, ybk = ybr.next()
        for half in range(2):
            py_, pyk = pyd.next()
            cs_ = slice(half * 512, (half + 1) * 512)
            for ft in range(2):
                mm(P, py_[:], aT[:, ft, :], wd[:, ft, cs_], ft == 0, ft == 1, [aTk, wdk], [pyk])
            if half == 0:
                P.op('act', lambda e, py_=py_, cs_=cs_: e.activation(out=yb[:, cs_], in_=py_[:], func=AF.Copy), reads=[pyk], writes=[ybk])
            else:
                dve(lambda e, py_=py_, cs_=cs_: e.tensor_copy(out=yb[:, cs_], in_=py_[:]), [pyk], [ybk])
        dst = T['Ys'] if kind == 'r' else T['Ysh']
        P.dma('sp', lambda e: e.dma_start(out=dst[b * 128:(b + 1) * 128, :], in_=yb[:]), reads=[ybk], writes=['Ys' if kind == 'r' else 'Ysh'])

    stageA(*blocks[0])
    for bi in range(len(blocks)):
        if bi + 1 < len(blocks):
            stageA(*blocks[bi + 1])
        stageB(*blocks[bi])
    g2b = P.sb([128, D], F32)
    fing = P.sb([128, D], F32)
    P.dma('sp', lambda e: e.dma_start(out=g2b[:], in_=T['modd'][:, 5120:6144]), writes=['g2b'])
    P.dma('sp', lambda e: e.dma_start(out=fing[:], in_=T['final_g'].partition_broadcast(128)), writes=['fing'])
    xr = Rot(P, 2, [128, D], F32, 'x1')
    grot = Rot(P, 4, [128, D], BF16, 'gat')
    shr = Rot(P, 2, [128, D], BF16, 'shr')
    acc = P.sb([128, D], F32)
    junk, ss, rs = P.sb([128, D], F32), P.sb([128, 1], F32), P.sb([128, 1], F32)
    for i in range(NT):
        tsl = slice(i * 128, (i + 1) * 128)
        xt, xk = xr.next()
        sh, shk = shr.next()
        P.dma('sp', lambda e, xt=xt, tsl=tsl: e.dma_start(out=xt[:], in_=T['x1'][tsl, :]), writes=[xk])
        P.dma('act', lambda e, sh=sh, tsl=tsl: e.dma_start(out=sh[:], in_=T['Ysh'][tsl, :]), reads=['Ysh'], writes=[shk])
        for k in range(8):
            gt, gtk = grot.next()
            P.dma('pool', lambda e, gt=gt, i=i, k=k: e.indirect_dma_start(
                out=gt[:], out_offset=None, in_=T['Ys'], in_offset=bass.IndirectOffsetOnAxis(ap=d8i[:, i, k:k + 1], axis=0)),
                reads=['Ys', f'd8i{i}'], writes=[gtk])
            if k == 0:
                dve(lambda e, gt=gt, i=i, sh=sh: e.scalar_tensor_tensor(out=acc[:], in0=gt[:], scalar=gw8[:, i, 0:1], in1=sh[:], op0=ALU.mult, op1=ALU.add),
                    [gtk, f'gw{i}', shk, 'acc'], ['acc'])
            else:
                dve(lambda e, gt=gt, i=i, k=k: e.scalar_tensor_tensor(out=acc[:], in0=gt[:], scalar=gw8[:, i, k:k + 1], in1=acc[:], op0=ALU.mult, op1=ALU.add),
                    [gtk, f'gw{i}', 'acc'], ['acc'])
        dve(lambda e: e.tensor_tensor(out=acc[:], in0=acc[:], in1=g2b[:], op=ALU.mult), ['acc', 'g2b'], ['acc'])
        P.op('pool', lambda e, xt=xt: e.tensor_tensor(out=xt[:], in0=xt[:], in1=acc[:], op=ALU.add), reads=['acc', xk], writes=[xk])
        rms_rstd(P, xt, xk, junk, ss, rs, 'f')
        dve(lambda e, xt=xt: e.scalar_tensor_tensor(out=xt[:], in0=xt[:], scalar=rs[:, 0:1], in1=fing[:], op0=ALU.mult, op1=ALU.mult),
            [xk, 'rsf', 'fing'], [xk])
        P.dma('sp', lambda e, xt=xt, tsl=tsl: e.dma_start(out=T['out'][tsl, :], in_=xt[:]), reads=[xk])


SCRATCH = [
    ('zq', [512, S], BF16), ('zk', [512, S], BF16), ('zv', [S, 512], BF16), ('ziq', [512, S], BF16),
    ('zik', [32, S], BF16), ('ziw', [S, 16], F32), ('zr', [1792, S], F32), ('zga', [1024, S], BF16),
    ('zgr', [1024, S], BF16), ('modd', [128, 6 * D], F32), ('attnT', [512, S], BF16), ('rwT', [512, S], BF16),
    ('x1', [S, D], F32), ('h2T', [D, S], BF16), ('h2tok', [S, D], BF16), ('meta', [S, 24], F32), ('cntd', [128, 64], F32),
    ('Xs', [NBLK * 128, D], BF16), ('Ys', [NBLK * 128, D], BF16), ('Ysh', [S, D], BF16),
    ('wg16', [65 * 128, 2048], BF16), ('wu16', [65 * 128, 2048], BF16), ('wd16', [65 * 128, 2048], BF16),
]

INPUT_SHAPES = [
    ('x', [S, D]), ('c_col', [128, 8]), ('ada_w', [D, 6 * D]), ('ada_b', [1, 6 * D]), ('norm1_g', [D]),
    ('w_in', [D, NIN]), ('rel_bias', [256]), ('tshift_mu', [1792]), ('decay_w0', [512]), ('decay_up', [64, 512]),
    ('iclr_a0', [512]), ('iclr_up', [64, 512]), ('gate_up', [128, 512]), ('k_k', [512]), ('k_a', [512]),
    ('r_k', [512]), ('lnx_g', [512]), ('lnx_b', [512]), ('w_attn_br', [512, D]), ('w_rwkv_br', [512, D]),
    ('w_out', [D, D]), ('norm2_g', [D]), ('router_w', [D, 64]), ('router_bias', [64]),
    ('exp_gate', [64, D, 256]), ('exp_up', [64, D, 256]), ('exp_down', [64, 256, D]),
    ('sh_gate', [D, 256]), ('sh_up', [D, 256]), ('sh_down', [256, D]), ('final_g', [D]),
    ('k_ident', [128, 128]), ('k_rel', [128, 256]), ('k_masks', [128, 896]), ('k_reset', [64, 512]), ('k_bst', [NBLK]),
]


def build(debug_outs=(), stop_after=None):
    nc = bass.Bass("TRN2", target_bir_lowering=False)
    T = {}
    for name, shp in INPUT_SHAPES:
        T[name] = nc.dram_tensor(name, shp, F32, kind="ExternalInput").ap()
    for name, shp, dt in SCRATCH:
        kind = "ExternalOutput" if name in debug_outs else "Internal"
        T[name] = nc.dram_tensor(name, shp, dt, kind=kind).ap()
    T['out'] = nc.dram_tensor('out', [S, D], F32, kind="ExternalOutput").ap()
    if 'dbg_y' in debug_outs:
        T['dbg_y'] = nc.dram_tensor('dbg_y', [64, 8, 512], F32, kind="ExternalOutput").ap()
        T['dbg_bon'] = nc.dram_tensor('dbg_bon', [64, 8, 512], BF16, kind="ExternalOutput").ap()
        T['dbg_g'] = nc.dram_tensor('dbg_g', [64, 8, 512], BF16, kind="ExternalOutput").ap()
        T['dbg_AR'] = nc.dram_tensor('dbg_AR', [64, 8, 4, 256], BF16, kind="ExternalOutput").ap()
        T['dbg_BK'] = nc.dram_tensor('dbg_BK', [64, 8, 4, 256], BF16, kind="ExternalOutput").ap()
    P = Prog(nc)
    G = {}
    G['E'] = P.gsb([128, 8, 256], F32)
    G['b31'] = P.gsb([128, 8], F32)
    G['ones_row'] = P.gsb([1, 128], F32)
    G['identf'] = P.gsb([128, 128], F32)
    G['identb'] = P.gsb([128, 128], BF16)
    G['eps'] = P.gsb([128, 1], F32)
    G_EPS[0] = G['eps']
    P.op('dve', lambda e: e.memset(G['ones_row'][:], 1.0), writes=['ones_row'])
    P.op('dve', lambda e: e.memset(G['eps'][:], 1e-6), writes=['eps'])
    P.dma('sp', lambda e: e.dma_start(out=G['identf'][:], in_=T['k_ident']), writes=['identf'])
    P.op('dve', lambda e: e.tensor_copy(out=G['identb'][:], in_=G['identf'][:]), reads=['identf'], writes=['identb'])
    phase_A(P, T, G)
    P.emit()
    if stop_after == 'A':
        P.emit(); P.finish(); return nc
    phase_BC(P, T, G)
    P.emit()
    if stop_after == 'C':
        P.finish(); return nc
    phase_D0(P, T, G)
    P.emit()
    phase_D(P, T, G)
    P.emit()
    if stop_after == 'D':
        P.finish(); return nc
    phase_E(P, T, G)
    P.emit()
    if stop_after == 'E':
        P.finish(); return nc
    G['sparse'] = SPARSE
    if SPARSE:
        phase_S0(P, T, G)
        P.emit()
    phase_F(P, T, G)
    P.emit()
    if stop_after == 'F':
        P.finish(); return nc
    if SPARSE:
        phase_S(P, T, G)
        P.emit()
        P.finish()
        return nc
    G['yacc'] = P.gsb([128, NT, D], F32)
    if stop_after in ('router', 'exp1'):
        G['gstop'] = stop_after
        G['nexp'] = 1
    if stop_after == 'router':
        phase_G(P, T, G); P.emit(); P.finish(); return nc
    if stop_after == 'exp1':
        phase_G(P, T, G); P.emit(); P.finish(); return nc
    phase_G(P, T, G)
    P.emit()
    phase_H(P, T, G)
    P.emit()
    P.finish()
    return nc


def host_inputs(inputs, b):
    m = {}
    f = lambda a: np.ascontiguousarray(np.asarray(a, dtype=np.float32))
    m['x'] = f(inputs['x'][b])
    m['c_col'] = f(np.asarray(inputs['c'][b]).reshape(8, 128).T)
    for name, shp in INPUT_SHAPES:
        if name in ('x', 'c_col', 'k_ident', 'k_rel', 'k_masks', 'k_reset', 'k_bst'):
            continue
        a = np.asarray(inputs[name])
        m[name] = f(a.reshape(shp))
    m['k_ident'] = np.eye(128, dtype=np.float32)
    ii = np.arange(128)
    su = (ii[:, None] < ii[None, :]).astype(np.float32)
    iu = (ii[:, None] <= ii[None, :]).astype(np.float32)
    sl = (ii[:, None] > ii[None, :]).astype(np.float32)
    m['k_masks'] = np.ascontiguousarray(np.concatenate([su, iu, su, iu, sl, np.zeros((128, 256), np.float32)], axis=1))
    rs_ = np.ones((64, 512), np.float32); rs_[:, ::128] = 0.0
    m['k_reset'] = rs_
    m['k_bst'] = (np.arange(NBLK, dtype=np.float32) * 128.0)
    m['k_rel'] = (np.arange(256, dtype=np.float32)[None, :] - np.arange(128, dtype=np.float32)[:, None])
    return m


def kernel(**inputs):
    nc = build()
    in_maps = [host_inputs(inputs, b) for b in range(8)]
    res = run_bass_kernel_spmd(nc, in_maps, core_ids=list(range(8)))
    return np.stack([np.asarray(r['out']) for r in res.results], axis=0).astype(np.float32)
```

```python
import contextlib
import numpy as np
import concourse.bass as bass
import concourse.mybir as mybir

F32 = mybir.dt.float32
BF16 = mybir.dt.bfloat16
I32 = mybir.dt.int32
U32 = mybir.dt.uint32
AF = mybir.ActivationFunctionType
ALU = mybir.AluOpType
AX = mybir.AxisListType

N_DMA_SEMS = 6


class Prog:
    ENGS = ('pe', 'act', 'dve', 'pool', 'sp')

    def __init__(self, nc):
        self.nc = nc
        self.ops = {e: [] for e in self.ENGS}
        self.cnt = {e: 0 for e in self.ENGS}
        self.waited = {e: {} for e in self.ENGS}
        self.res = {}
        self.dma_tot = [0] * N_DMA_SEMS
        self.dma_rr = 0
        self.stack = contextlib.ExitStack()
        self.gstack = contextlib.ExitStack()
        self.nsb = 0
        self.nphase = 0
        self.sems = None

    def _new_sems(self):
        nc = self.nc
        self.sems = {}
        for e in self.ENGS:
            self.sems[e] = self.gstack.enter_context(nc.semaphore(f"s_{e}_{self.nphase}"))
        for i in range(N_DMA_SEMS):
            self.sems[('dma', i)] = self.gstack.enter_context(nc.semaphore(f"s_dma{i}_{self.nphase}"))
        self.cnt = {e: 0 for e in self.ENGS}
        self.waited = {e: {} for e in self.ENGS}
        self.dma_tot = [0] * N_DMA_SEMS
        self.dma_rr = 0

    def gsb(self, shape, dt, name=None):
        self.nsb += 1
        return self.gstack.enter_context(self.nc.sbuf_tensor(name or f"gsb{self.nsb}", list(shape), dt))

    def sb(self, shape, dt, name=None):
        self.nsb += 1
        return self.stack.enter_context(self.nc.sbuf_tensor(name or f"sb{self.nsb}", list(shape), dt))

    def ps(self, shape, dt, name=None):
        self.nsb += 1
        return self.stack.enter_context(self.nc.psum_tensor(name or f"ps{self.nsb}", list(shape), dt))

    def _deps(self, reads, writes):
        deps = {}
        def add(d):
            if d is None:
                return
            k, v = d
            if deps.get(k, 0) < v:
                deps[k] = v
        for k in reads:
            r = self.res.get(k)
            if r:
                add(r['w'])
        for k in writes:
            r = self.res.get(k)
            if r:
                add(r['w'])
                for d in r['r']:
                    add(d)
        return deps

    def _emit_waits(self, eng, deps):
        w = self.waited[eng]
        for k, v in deps.items():
            if w.get(k, 0) < v:
                w[k] = v
                self.ops[eng].append(('wait', k, v))

    def _update(self, dep, reads, writes):
        for k in reads:
            r = self.res.setdefault(k, {'w': None, 'r': []})
            r['r'] = [d for d in r['r'] if d[0] != dep[0]] + [dep]
        for k in writes:
            self.res[k] = {'w': dep, 'r': []}

    def op(self, eng, fn, reads=(), writes=()):
        if self.sems is None:
            self._new_sems()
        deps = self._deps(reads, writes)
        if eng == 'pe':
            deps.pop('pe', None)
        self._emit_waits(eng, deps)
        self.cnt[eng] += 1
        dep = (eng, self.cnt[eng])
        self.ops[eng].append(('op', fn))
        self._update(dep, reads, writes)
        return dep

    def dma(self, q, fn, reads=(), writes=()):
        if self.sems is None:
            self._new_sems()
        deps = self._deps(reads, writes)
        s = self.dma_rr
        self.dma_rr = (self.dma_rr + 1) % N_DMA_SEMS
        key = ('dma', s)
        if self.dma_tot[s] > 0:
            if deps.get(key, 0) < self.dma_tot[s]:
                deps[key] = self.dma_tot[s]
        self._emit_waits(q, deps)
        self.dma_tot[s] += 16
        dep = (key, self.dma_tot[s])
        self.ops[q].append(('dma', fn, s))
        self._update(dep, reads, writes)
        return dep

    def emit(self, keep=False):
        nc = self.nc
        with contextlib.ExitStack() as st:
            sems = self.sems
            self.nphase += 1
            block = st.enter_context(nc.Block(f"ph{self.nphase}"))
            engobj = {'pe': nc.tensor, 'act': nc.scalar, 'dve': nc.vector, 'pool': nc.gpsimd, 'sp': nc.sync}
            fin = {}
            for e in self.ENGS:
                if e != 'sp' and self.cnt[e] > 0:
                    fin[e] = self.cnt[e]
            for i in range(N_DMA_SEMS):
                if self.dma_tot[i] > 0:
                    fin[('dma', i)] = self.dma_tot[i]
            self._emit_waits('sp', fin)

            def run(e):
                eo = engobj[e]
                for item in self.ops[e]:
                    if item[0] == 'wait':
                        eo.wait_ge(sems[item[1]], item[2])
                    elif item[0] == 'op':
                        item[1](eo).then_inc(sems[e], 1)
                    else:
                        item[1](eo).then_inc(sems[('dma', item[2])], 16)

            @block.tensor
            def _(t):
                run('pe')

            @block.scalar
            def _(t):
                run('act')

            @block.vector
            def _(t):
                run('dve')

            @block.gpsimd
            def _(t):
                run('pool')

            @block.sync
            def _(t):
                run('sp')
        self.ops = {e: [] for e in self.ENGS}
        self.res = {}
        if keep:
            return
        self.stack.close()
        self.stack = contextlib.ExitStack()
        self.sems = None

    def finish(self):
        self.gstack.close()

from concourse.bass_utils import run_bass_kernel_spmd
import ml_dtypes

SPARSE = True
S = 4096
D = 1024
NT = S // 128
NIN = 5936


class Rot:
    def __init__(self, P, n, shape, dt, name, psum=False):
        self.tiles = [(P.ps(shape, dt) if psum else P.sb(shape, dt)) for _ in range(n)]
        self.name = name
        self.i = 0

    def next(self):
        t = self.tiles[self.i % len(self.tiles)]
        k = f"{self.name}{self.i % len(self.tiles)}"
        self.i += 1
        return t, k


class Rot2(Rot):
    def __init__(self, P, n, shape, dt, name):
        self.tiles = [(P.sb(shape, dt), P.sb(shape, dt)) for _ in range(n)]
        self.name = name
        self.i = 0


def mm(P, out, lhsT, rhs, start, stop, reads, writes):
    P.op('pe', lambda e: e.matmul(out, lhsT=lhsT, rhs=rhs, start=start, stop=stop), reads=reads, writes=writes)


def phase_A(P, T, G):
    mod_bc = P.sb([128, 6 * D], F32)
    ccol = P.sb([128, 8], F32)
    scol = P.sb([128, 8], F32)
    adab = P.sb([1, 6144], F32)
    modrow = P.sb([1, 6144], F32)
    ngb = P.sb([128, 1024], F32)
    P.dma('sp', lambda e: e.dma_start(out=ccol[:], in_=T['c_col']), writes=['ccol'])
    P.dma('sp', lambda e: e.dma_start(out=adab[:], in_=T['ada_b']), writes=['adab'])
    P.op('act', lambda e: e.activation(out=scol[:], in_=ccol[:], func=AF.Silu), reads=['ccol'], writes=['scol'])
    wrot = Rot(P, 2, [128, 8, 512], F32, 'aw')
    psr = Rot(P, 2, [1, 512], F32, 'psr', psum=True)
    psb = Rot(P, 2, [128, 512], F32, 'psb', psum=True)
    adaw = T['ada_w'].rearrange("(k p) n -> p k n", p=128)
    for n in range(12):
        wb, wk = wrot.next()
        P.dma('sp' if n % 2 == 0 else 'act',
              lambda e, wb=wb, n=n: e.dma_start(out=wb[:], in_=adaw[:, :, n * 512:(n + 1) * 512]), writes=[wk])
        pr, pk = psr.next()
        for k in range(8):
            mm(P, pr[:], scol[:, k:k + 1], wb[:, k, :], k == 0, k == 7, [wk, 'scol'], [pk])
        sl = slice(n * 512, (n + 1) * 512)
        P.op('dve', lambda e, pr=pr, sl=sl: e.tensor_tensor(out=modrow[0:1, sl], in0=pr[:], in1=adab[0:1, sl], op=ALU.add),
             reads=[pk, 'adab'], writes=[f'modrow{n}'])
        pb, pbk = psb.next()
        mm(P, pb[:], G['ones_row'][:], modrow[0:1, sl], True, True, [f'modrow{n}'], [pbk])
        P.op('act', lambda e, pb=pb, sl=sl: e.activation(out=mod_bc[:, sl], in_=pb[:], func=AF.Copy),
             reads=[pbk], writes=[f'mod{n}'])
    for (gname, c0, deps) in (('norm1_g', 1024, ['mod2', 'mod3']), ('norm2_g', 4096, ['mod8', 'mod9'])):
        P.dma('sp', lambda e, gname=gname: e.dma_start(out=ngb[:], in_=T[gname].partition_broadcast(128)), writes=['ngb'])
        P.op('dve', lambda e, c0=c0: e.scalar_tensor_tensor(out=mod_bc[:, c0:c0 + 1024], in0=mod_bc[:, c0:c0 + 1024],
                                                            scalar=1.0, in1=ngb[:], op0=ALU.add, op1=ALU.mult),
             reads=deps + ['ngb'], writes=deps)
    P.dma('sp', lambda e: e.dma_start(out=T['modd'], in_=mod_bc[:]), reads=[f'mod{n}' for n in range(12)])
    if G.get('sparse'):
        zt = P.sb([128, D], BF16)
        P.op('pool', lambda e: e.memset(zt[:], 0.0), writes=['zt'])
        for bz in range(NBLK):
            P.dma('act' if bz % 2 else 'sp', lambda e, bz=bz: e.dma_start(out=T['Xs'][bz * 128:(bz + 1) * 128, :], in_=zt[:]), reads=['zt'])


def rms_rstd(P, xt, xk, junk, ss, rs, tag):
    P.op('act', lambda e: e.activation(out=junk[:], in_=xt[:], func=AF.Square, accum_out=ss[:]),
         reads=[xk], writes=['junk' + tag, 'ss' + tag])
    P.op('act', lambda e: e.activation(out=ss[:], in_=ss[:], func=AF.Sqrt, scale=1.0 / D, bias=G_EPS[0][:, 0:1]),
         reads=['ss' + tag], writes=['ss' + tag])
    P.op('dve', lambda e: e.reciprocal(out=rs[:], in_=ss[:]), reads=['ss' + tag], writes=['rs' + tag])


G_EPS = [None]


def norm_mod_transpose(P, G, xt, xk, hT, i, g_sl, sh_sl, W):
    mod_bc = W['mod']
    junk, ss, rs, t1, hb, pt = W['junk'], W['ss'], W['rs'], W['t1'], W['hb'], W['pt']
    rms_rstd(P, xt, xk, junk, ss, rs, '')
    P.op('dve', lambda e: e.scalar_tensor_tensor(out=t1[:], in0=xt[:], scalar=rs[:, 0:1], in1=mod_bc[:, g_sl],
                                                 op0=ALU.mult, op1=ALU.mult), reads=[xk, 'rs', 'modl'], writes=['t1'])
    P.op('pool', lambda e: e.tensor_tensor(out=hb[:], in0=t1[:], in1=mod_bc[:, sh_sl], op=ALU.add),
         reads=['t1', 'modl'], writes=['hb'])
    for k in range(8):
        P.op('pe', lambda e, k=k: e.transpose(out=pt[:, k, :], in_=hb[:, k * 128:(k + 1) * 128], identity=G['identb'][:]),
             reads=['hb'], writes=['pt'])
    P.op('act', lambda e: e.activation(out=hT[:, :, i * 128:(i + 1) * 128], in_=pt[:], func=AF.Copy),
         reads=['pt'], writes=[f'hT{i // 4}'])


def phase_BC(P, T, G):
    hT = P.sb([128, 8, S], BF16)
    W = dict(junk=P.sb([128, D], F32), ss=P.sb([128, 1], F32), rs=P.sb([128, 1], F32), t1=P.sb([128, D], F32),
             hb=P.sb([128, D], BF16), pt=P.ps([128, 8, 128], BF16))
    W['mod'] = P.sb([128, 2048], F32)
    P.dma('sp', lambda e: e.dma_start(out=W['mod'][:], in_=T['modd'][:, 0:2048]), writes=['modl'])
    xrot = Rot(P, 2, [128, D], F32, 'x')
    s0 = phase_S0(P, T, G) if G.get('sparse') else iter(())

    def s0step(n=1):
        for _ in range(n):
            try:
                next(s0)
            except StopIteration:
                return
    for i in range(NT):
        xt, xk = xrot.next()
        P.dma('sp', lambda e, xt=xt, i=i: e.dma_start(out=xt[:], in_=T['x'][i * 128:(i + 1) * 128, :]), writes=[xk])
        norm_mod_transpose(P, G, xt, xk, hT, i, slice(1024, 2048), slice(0, 1024), W)
        s0step()
    win = T['w_in'].rearrange("(k p) n -> p k n", p=128)
    segs = [('zq', 0, 512, BF16), ('zk', 512, 512, BF16), ('ziq', 1536, 512, BF16), ('zik', 2048, 32, BF16),
            ('zr', 2096, 1792, F32), ('zga', 3888, 1024, BF16), ('zgr', 4912, 1024, BF16)]
    wrot = Rot(P, 2, [128, 8, 128], BF16, 'w')
    psrot = Rot(P, 3, [128, 512], F32, 'ps', psum=True)
    strot = {BF16: Rot(P, 3, [128, 512], BF16, 'stb'), F32: Rot(P, 3, [128, 512], F32, 'stf')}
    ev = 0
    for (name, c0, n, dt) in segs:
        for m0 in range(0, n, 128):
            M = min(128, n - m0)
            wt, wk = wrot.next()
            P.dma('pool', lambda e, wt=wt, M=M, a=c0 + m0: e.dma_start(out=wt[:, :, :M], in_=win[:, :, a:a + M]), writes=[wk])
            for tg in range(8):
                ps, pk = psrot.next()
                for k in range(8):
                    mm(P, ps[:M, :], wt[:, k, :M], hT[:, k, tg * 512:(tg + 1) * 512], k == 0, k == 7, [wk, f'hT{tg}'], [pk])
                st, sk = strot[dt].next()
                if ev % 2 == 0:
                    P.op('act', lambda e, st=st, ps=ps, M=M: e.activation(out=st[:M, :], in_=ps[:M, :], func=AF.Copy),
                         reads=[pk], writes=[sk])
                else:
                    P.op('dve', lambda e, st=st, ps=ps, M=M: e.tensor_copy(out=st[:M, :], in_=ps[:M, :]),
                         reads=[pk], writes=[sk])
                ev += 1
                if ev % 2 == 0:
                    s0step()
                P.dma('sp', lambda e, st=st, M=M, name=name, m0=m0, tg=tg:
                      e.dma_start(out=T[name][m0:m0 + M, tg * 512:(tg + 1) * 512], in_=st[:M, :]), reads=[sk])
    wv = P.sb([128, 8, 528], BF16)
    P.dma('pool', lambda e: e.dma_start(out=wv[:, :, 0:512], in_=win[:, :, 1024:1536]), writes=['wv'])
    P.dma('pool', lambda e: e.dma_start(out=wv[:, :, 512:528], in_=win[:, :, 2080:2096]), writes=['wv2'])
    ps2rot = Rot(P, 2, [128, 16], F32, 'ps2', psum=True)
    st2rot = Rot(P, 2, [128, 16], F32, 'st2')
    for i in range(NT):
        ps, pk = psrot.next()
        ps2, pk2 = ps2rot.next()
        for k in range(8):
            mm(P, ps[:], hT[:, k, i * 128:(i + 1) * 128], wv[:, k, 0:512], k == 0, k == 7, ['wv', f'hT{i // 4}'], [pk])
        for k in range(8):
            mm(P, ps2[:], hT[:, k, i * 128:(i + 1) * 128], wv[:, k, 512:528], k == 0, k == 7, ['wv2', f'hT{i // 4}'], [pk2])
        st, sk = strot[BF16].next()
        st2, sk2 = st2rot.next()
        P.op('act', lambda e, st=st, ps=ps: e.activation(out=st[:], in_=ps[:], func=AF.Copy), reads=[pk], writes=[sk])
        P.op('dve', lambda e, st2=st2, ps2=ps2: e.tensor_copy(out=st2[:], in_=ps2[:]), reads=[pk2], writes=[sk2])
        P.dma('sp', lambda e, st=st, i=i: e.dma_start(out=T['zv'][i * 128:(i + 1) * 128, :], in_=st[:]), reads=[sk])
        P.dma('sp', lambda e, st2=st2, i=i: e.dma_start(out=T['ziw'][i * 128:(i + 1) * 128, :], in_=st2[:]), reads=[sk2])
    s0step(1000)


def t5_lo_bounds():
    n = np.arange(256)
    nf = np.maximum(n, 1).astype(np.float32)
    large = 16 + (np.log(nf / np.float32(16)) / np.float32(np.log(8.0)) * np.float32(16)).astype(np.int32)
    large = np.minimum(large, 31)
    bk = np.where(n < 16, n, large)
    return [int(np.min(np.nonzero(bk >= b)[0])) for b in range(1, 32)]


def phase_D0(P, T, G):
    relb = P.sb([128, 256], F32)
    diff = P.sb([128, 248], F32)
    base = P.sb([128, 8], F32)
    relidx = P.sb([128, 256], F32)
    P.dma('sp', lambda e: e.dma_start(out=relb[:], in_=T['rel_bias'].partition_broadcast(128)), writes=['relb'])
    P.dma('sp', lambda e: e.dma_start(out=relidx[:], in_=T['k_rel']), writes=['relidx'])
    P.op('dve', lambda e: e.tensor_tensor(out=diff[:], in0=relb[:, 8:256], in1=relb[:, 0:248], op=ALU.subtract),
         reads=['relb'], writes=['diff'])
    P.op('dve', lambda e: e.tensor_tensor(out=base[:], in0=relb[:, 0:8], in1=relb[:, 248:256], op=ALU.subtract),
         reads=['relb'], writes=['base'])
    P.op('dve', lambda e: e.tensor_copy(out=G['b31'][:], in_=relb[:, 248:256]), reads=['relb'], writes=['b31'])
    E = G['E']
    irot = Rot(P, 2, [128, 256], F32, 'ind')
    los = t5_lo_bounds()
    for b in range(1, 32):
        ind, ik = irot.next()
        P.op('dve', lambda e, ind=ind, lo=float(los[b - 1]): e.tensor_scalar(out=ind[:], in0=relidx[:], scalar1=lo, scalar2=None,
                                                                              op0=ALU.is_ge), reads=['relidx'], writes=[ik])
        for h in range(8):
            if b == 1:
                P.op('dve', lambda e, ind=ind, h=h: e.tensor_scalar(out=E[:, h, :], in0=ind[:], scalar1=diff[:, h:h + 1],
                                                                    scalar2=base[:, h:h + 1], op0=ALU.mult, op1=ALU.add),
                     reads=[ik, 'diff', 'base'], writes=[f'E{h}'])
            else:
                c = (b - 1) * 8 + h
                P.op('dve', lambda e, ind=ind, h=h, c=c: e.scalar_tensor_tensor(out=E[:, h, :], in0=ind[:], scalar=diff[:, c:c + 1],
                                                                               in1=E[:, h, :], op0=ALU.mult, op1=ALU.add),
                     reads=[ik, 'diff', f'E{h}'], writes=[f'E{h}'])
    P.op('act', lambda e: e.activation(out=E[:], in_=E[:], func=AF.Exp), reads=[f'E{h}' for h in range(8)],
         writes=[f'E{h}' for h in range(8)])


def phase_D(P, T, G):
    NIT = 16
    KT = P.sb([64, 8, S], BF16)
    V = P.sb([128, NT, 512], BF16)
    ik4 = P.sb([128, S], BF16)
    iw = P.sb([128, NT, 16], F32)
    ones64 = P.sb([128, 64], BF16)
    scs = [P.sb([128, S], F32), P.sb([128, S], F32)]
    maskbs = [P.sb([128, S], BF16), P.sb([128, S], BF16)]
    maskT = P.sb([128, NT, 128], BF16)
    lo, hi, mid, cnt, tmp, thr = [P.sb([128, 1], F32) for _ in range(6)]
    cvec = P.sb([128, NIT + 1], F32)
    dk = P.sb([128, NIT + 1], F32)
    for k in range(NIT + 1):
        P.op('pool', lambda e, k=k: e.memset(cvec[:, k:k + 1], 2.0 ** -(k + 1)), reads=['cvec'], writes=['cvec'])
    P.dma('sp', lambda e: e.dma_start(out=KT[:], in_=T['zk'].rearrange("(h p) t -> p h t", p=64)), writes=['KT'])
    P.dma('act', lambda e: e.dma_start(out=V[:], in_=T['zv'].rearrange("(i p) f -> p i f", p=128)), writes=['V'])
    for i in range(3):
        P.dma('sp', lambda e, i=i: e.dma_start(out=ik4[32 * i:32 * i + 32, :], in_=T['zik']), writes=[f'ik4{i}'])
    P.dma('sp', lambda e: e.dma_start(out=iw[:], in_=T['ziw'].rearrange("(i p) f -> p i f", p=128)), writes=['iw'])
    P.op('dve', lambda e: e.memset(ones64[:], 1.0), writes=['ones64'])
    zq = T['zq'].rearrange("(h p) t -> p h t", p=64)
    ziq = T['ziq'][0:480, :].rearrange("(j p) t -> p j t", p=96)
    attnT = T['attnT'].rearrange("(h p) t -> p h t", p=64)
    qrot = Rot(P, 2, [64, 8, 128], BF16, 'q')
    iqrot = Rot(P, 2, [96, 6, 128], BF16, 'iq')
    dgrot = Rot(P, 2, [128, 16, 128], BF16, 'dg')
    rrot = Rot(P, 4, [128, 512], BF16, 'r')
    psi = Rot(P, 2, [128, 512], F32, 'psi', psum=True)
    pacc = Rot(P, 1, [128, 512], F32, 'pacc', psum=True)
    pss = Rot(P, 3, [128, 4, 128], F32, 'pss', psum=True)
    ptm = Rot(P, 1, [128, 4, 128], BF16, 'ptm', psum=True)
    pod = Rot(P, 1, [64, 512], F32, 'pod', psum=True)
    pTrot = Rot(P, 4, [128, 4, 128], BF16, 'pT')
    atrot = Rot(P, 2, [64, 8, 128], BF16, 'at')
    rdrot = Rot(P, 2, [64, 128], F32, 'rd')
    E, b31 = G['E'], G['b31']
    identb = G['identb']

    def indexer(qi):
        sc, sck = scs[qi % 2], f'sc{qi % 2}'
        n = 128 * (qi + 1)
        tsl = slice(qi * 128, (qi + 1) * 128)
        iqt, iqk = iqrot.next()
        P.dma('act', lambda e: e.dma_start(out=iqt[:, 0:5, :], in_=ziq[:, :, tsl]), writes=[iqk])
        P.dma('act', lambda e: e.dma_start(out=iqt[0:32, 5, :], in_=T['ziq'][480:512, tsl]), writes=[iqk + 'b'])
        dg, dgk = dgrot.next()
        for h in range(16):
            P.op('pool', lambda e, h=h: e.tensor_scalar(out=dg[:, h, :], in0=identb[:], scalar1=iw[:, qi, h:h + 1], scalar2=0.0, op0=ALU.mult, op1=ALU.add),
                 reads=['iw'], writes=[dgk])
        for ch in range((n + 511) // 512):
            c0 = ch * 512
            nc_ = min(512, n - c0)
            pa, pak = pacc.next()
            pend = []

            def acc(h, r, rk):
                mm(P, pa[:, :nc_], dg[:, h, :], r[:, :nc_], h == 0, h == 15, [dgk, rk], [pak])
            for h in range(16):
                j, i = divmod(h, 3)
                ps, pk = psi.next()
                mm(P, ps[:, :nc_], iqt[32 * i:32 * i + 32, j, :], ik4[32 * i:32 * i + 32, c0:c0 + nc_], True, True,
                   [iqk, iqk + 'b', f'ik4{i}'], [pk])
                r, rk = rrot.next()
                P.op('act', lambda e, r=r, ps=ps, nc_=nc_: e.activation(out=r[:, :nc_], in_=ps[:, :nc_], func=AF.Relu), reads=[pk], writes=[rk])
                pend.append((h, r, rk))
                if len(pend) > 1:
                    acc(*pend.pop(0))
                yield
            while pend:
                acc(*pend.pop(0))
            P.op('dve', lambda e, pa=pa, c0=c0, nc_=nc_: e.tensor_copy(out=sc[:, c0:c0 + nc_], in_=pa[:, :nc_]), reads=[pak], writes=[sck])
        P.op('pool', lambda e: e.affine_select(out=sc[:, tsl], in_=sc[:, tsl], pattern=[[-1, 128]], compare_op=ALU.is_ge, fill=-1e30,
                                               base=0, channel_multiplier=1), reads=[sck], writes=[sck])

    def threshold(qi):
        sc, sck = scs[qi % 2], f'sc{qi % 2}'
        n = 128 * (qi + 1)
        maskb, mbk = maskbs[qi % 2], f'maskb{qi % 2}'
        dve = lambda fn, r, w: P.op('dve', fn, reads=r, writes=w)
        if n <= 256:
            dve(lambda e: e.memset(thr[:], -1e29), ['thr'], ['thr'])
        else:
            nv = 128 * qi
            dve(lambda e: e.tensor_reduce(out=lo[:], in_=sc[:, :nv], axis=AX.X, op=ALU.min), [sck, 'lo'], ['lo'])
            dve(lambda e: e.tensor_reduce(out=hi[:], in_=sc[:, :n], axis=AX.X, op=ALU.max), [sck, 'hi'], ['hi'])
            dve(lambda e: e.tensor_tensor(out=hi[:], in0=hi[:], in1=lo[:], op=ALU.subtract), ['hi', 'lo'], ['hi'])
            dve(lambda e: e.tensor_scalar(out=dk[:], in0=cvec[:], scalar1=hi[:, 0:1], scalar2=None, op0=ALU.mult), ['cvec', 'hi', 'dk'], ['dk'])
            dve(lambda e: e.tensor_tensor(out=mid[:], in0=lo[:], in1=dk[:, 0:1], op=ALU.add), ['lo', 'dk', 'mid'], ['mid'])
            for k in range(NIT):
                dve(lambda e: e.tensor_scalar(out=maskb[:, :n], in0=sc[:, :n], scalar1=mid[:, 0:1], scalar2=None,
                                              op0=ALU.is_ge, op1=ALU.add, accum_out=cnt[:]), [sck, 'mid', 'cnt', mbk], [mbk, 'cnt'])
                dve(lambda e: e.tensor_scalar(out=tmp[:], in0=cnt[:], scalar1=255.5, scalar2=-0.5, op0=ALU.is_ge, op1=ALU.add),
                    ['cnt', 'tmp'], ['tmp'])
                dve(lambda e, k=k: e.scalar_tensor_tensor(out=mid[:], in0=tmp[:], scalar=dk[:, k:k + 1], in1=mid[:], op0=ALU.mult, op1=ALU.add),
                    ['tmp', 'dk', 'mid'], ['mid'])
                yield
            dve(lambda e: e.tensor_tensor(out=thr[:], in0=mid[:], in1=dk[:, NIT:NIT + 1], op=ALU.subtract), ['mid', 'dk', 'thr'], ['thr'])
        dve(lambda e: e.tensor_scalar(out=maskb[:, :n], in0=sc[:, :n], scalar1=thr[:, 0:1], scalar2=None, op0=ALU.is_ge),
            [sck, 'thr', mbk], [mbk])
        yield

    def attention(qi):
        LOOK = 2
        nkt = qi + 1
        nch = (nkt + 3) // 4
        tsl = slice(qi * 128, (qi + 1) * 128)
        mb = maskbs[qi % 2]
        mbk = f'maskb{qi % 2}'
        qt, qk = qrot.next()
        P.dma('sp', lambda e: e.dma_start(out=qt[:], in_=zq[:, :, tsl]), writes=[qk])
        for c4 in range(nch):
            kts = list(range(4 * c4, min(4 * c4 + 4, nkt)))
            pm, pmk = ptm.next()
            for kt in kts:
                P.op('pe', lambda e, pm=pm, kt=kt: e.transpose(out=pm[:, kt % 4, :], in_=mb[:, kt * 128:(kt + 1) * 128],
                                                                identity=identb[:]), reads=[mbk], writes=[pmk])
            P.op('act', lambda e, pm=pm, kts=kts: e.activation(out=maskT[:, kts[0]:kts[-1] + 1, :], in_=pm[:, :len(kts), :],
                                                               func=AF.Copy), reads=[pmk], writes=['maskT'])
        at, atk = atrot.next()
        items = [(h, c4) for h in range(8) for c4 in range(nch)]
        qkd = {}
        hst = {}

        def emit_qk(h, c4):
            kts = list(range(4 * c4, min(4 * c4 + 4, nkt)))
            ps, pk = pss.next()
            for kt in kts:
                mm(P, ps[:, kt % 4, :], KT[:, h, kt * 128:(kt + 1) * 128], qt[:, h, :], True, True, ['KT', qk], [pk])
            qkd[(h, c4)] = (ps, pk)

        def emit_rest(h, c4):
            kts = list(range(4 * c4, min(4 * c4 + 4, nkt)))
            nk = len(kts)
            ps, pk = qkd.pop((h, c4))
            if c4 == 0:
                hst[h] = pod.next()
            po, pok = hst[h]
            pT, pTk = pTrot.next()
            P.op('act', lambda e: e.activation(out=pT[:, :nk, :], in_=ps[:, :nk, :], func=AF.Exp, scale=0.125, bias=b31[:, h:h + 1]),
                 reads=[pk], writes=[pTk])
            P.op('dve', lambda e: e.tensor_tensor(out=pT[:, :nk, :], in0=pT[:, :nk, :], in1=maskT[:, kts[0]:kts[-1] + 1, :], op=ALU.mult),
                 reads=[pTk, 'maskT'], writes=[pTk])
            for kt in kts:
                dl = qi - kt
                if dl <= 1:
                    P.op('dve', lambda e, kt=kt, dl=dl: e.tensor_tensor(
                        out=pT[:, kt % 4, :], in0=pT[:, kt % 4, :], in1=E[:, h, dl * 128:(dl + 1) * 128], op=ALU.mult),
                        reads=[pTk], writes=[pTk])
            for kt in kts:
                P.op('pe', lambda e, kt=kt: e.matmul(po[:, 0:128], lhsT=V[:, kt, h * 64:(h + 1) * 64], rhs=pT[:, kt % 4, :],
                                                     start=(kt == 0), stop=(kt == nkt - 1), skip_group_check=True),
                     reads=['V', pTk], writes=[pok])
                P.op('pe', lambda e, kt=kt: e.matmul(po[:, 128:256], lhsT=ones64[:], rhs=pT[:, kt % 4, :],
                                                     start=False, stop=(kt == nkt - 1), skip_group_check=True),
                     reads=['ones64', pTk], writes=[pok])
            if c4 == nch - 1:
                rd, rdk = rdrot.next()
                P.op('dve', lambda e: e.reciprocal(out=rd[:], in_=po[:, 128:256]), reads=[pok], writes=[rdk])
                P.op('dve', lambda e: e.tensor_tensor(out=at[:, h, :], in0=po[:, 0:128], in1=rd[:], op=ALU.mult),
                     reads=[pok, rdk], writes=[atk])

        for idx in range(len(items) + LOOK):
            if idx < len(items):
                emit_qk(*items[idx])
            if idx >= LOOK:
                emit_rest(*items[idx - LOOK])
                yield
        P.dma('sp', lambda e: e.dma_start(out=attnT[:, :, tsl], in_=at[:]), reads=[atk])

    def drain(g):
        for _ in g:
            pass

    def merge(gens):
        gens = [[g, max(1, n), 0.0, True] for g, n in gens]
        total = max(n for _, n, _, _ in gens)
        for step in range(total + 1):
            for it in gens:
                it[2] += it[1] / total
                while it[3] and it[2] >= 1.0:
                    it[2] -= 1.0
                    try:
                        next(it[0])
                    except StopIteration:
                        it[3] = False
        for it in gens:
            if it[3]:
                drain(it[0])

    drain(indexer(0))
    drain(threshold(0))
    for qi in range(NT):
        if qi + 1 < NT:
            drain(indexer(qi + 1))
            merge([(attention(qi), 8 * ((qi + 4) // 4)), (threshold(qi + 1), NIT + 1)])
        else:
            drain(attention(qi))


def phase_E(P, T, G):
    LD = 0.6065306597126334
    ident = G['identb']
    zr = T['zr']
    def colload(name, n, key):
        t = P.sb([64, n], F32)
        P.dma('sp', lambda e: e.dma_start(out=t[:], in_=T[name].rearrange("(h p) -> p h", p=64), allow_slow_non_contiguous=True), writes=[key])
        return t
    mu_rkv = P.sb([64, 24], F32)
    P.dma('sp', lambda e: e.dma_start(out=mu_rkv[:], in_=T['tshift_mu'][0:1536].rearrange("(h p) -> p h", p=64), allow_slow_non_contiguous=True), writes=['mu'])
    mu_wa = P.sb([64, 2], F32)
    P.dma('sp', lambda e: e.dma_start(out=mu_wa[:], in_=T['tshift_mu'][1536:1664].rearrange("(h p) -> p h", p=64), allow_slow_non_contiguous=True), writes=['mu'])
    mu_g = P.sb([128, 1], F32)
    P.dma('sp', lambda e: e.dma_start(out=mu_g[:], in_=T['tshift_mu'][1664:1792].rearrange("(h p) -> p h", p=128), allow_slow_non_contiguous=True), writes=['mu'])
    om_rkv, om_wa, om_g = P.sb([64, 24], F32), P.sb([64, 2], F32), P.sb([128, 1], F32)
    for (o, m) in ((om_rkv, mu_rkv), (om_wa, mu_wa), (om_g, mu_g)):
        P.op('dve', lambda e, o=o, m=m: e.tensor_scalar(out=o[:], in0=m[:], scalar1=-1.0, scalar2=1.0, op0=ALU.mult, op1=ALU.add),
             reads=['mu'], writes=['om'])
    w0c = colload('decay_w0', 8, 'par'); a0c = colload('iclr_a0', 8, 'par'); kkc = colload('k_k', 8, 'par')
    kac = colload('k_a', 8, 'par'); rkc = colload('r_k', 8, 'par'); lgc = colload('lnx_g', 8, 'par'); lbc = colload('lnx_b', 8, 'par')
    omka = P.sb([64, 8], F32)
    P.op('dve', lambda e: e.tensor_scalar(out=omka[:], in0=kac[:], scalar1=-1.0, scalar2=1.0, op0=ALU.mult, op1=ALU.add),
         reads=['par'], writes=['omka'])
    dup, iup, gup = P.sb([64, 512], BF16), P.sb([64, 512], BF16), P.sb([128, 512], BF16)
    P.dma('pool', lambda e: e.dma_start(out=dup[:], in_=T['decay_up']), writes=['wts'])
    P.dma('pool', lambda e: e.dma_start(out=iup[:], in_=T['iclr_up']), writes=['wts'])
    P.dma('pool', lambda e: e.dma_start(out=gup[:], in_=T['gate_up']), writes=['wts'])
    onesf = P.sb([64, 64], F32)
    onesm = P.sb([64, 64], F32)
    gneps = P.sb([64, 1], F32)
    P.op('dve', lambda e: e.memset(onesf[:], 1.0), writes=['onesf'])
    P.op('dve', lambda e: e.memset(onesm[:], 1.0 / 64), writes=['onesm'])
    P.op('dve', lambda e: e.memset(gneps[:], 64e-5), writes=['gneps'])
    km = P.sb([128, 896], F32)
    P.dma('sp', lambda e: e.dma_start(out=km[:], in_=T['k_masks']), writes=['km'])
    mask4 = P.sb([128, 512], BF16)
    maskL = P.sb([128, 128], BF16)
    P.op('dve', lambda e: e.tensor_copy(out=mask4[:], in_=km[:, 0:512]), reads=['km'], writes=['mask4'])
    P.op('dve', lambda e: e.tensor_copy(out=maskL[:], in_=km[:, 512:640]), reads=['km'], writes=['maskL'])
    rst = P.sb([64, 512], F32)
    P.dma('sp', lambda e: e.dma_start(out=rst[:], in_=T['k_reset']), writes=['rst'])
    Tst = P.sb([64, 8, 64], BF16)
    P.op('dve', lambda e: e.memset(Tst[:], 0.0), writes=[f'T{h}' for h in range(8)])
    zrot = Rot(P, 1, [64, 3, 513], F32, 'z')
    wa_in = P.sb([64, 2, 513], F32)
    gd_in = P.sb([128, 513], F32)
    tmpr = Rot(P, 1, [128, 512], F32, 'tmp')
    twb, adb, sgb = P.sb([64, 512], BF16), P.sb([64, 512], BF16), P.sb([128, 512], BF16)
    AR = P.sb([64, 8, 4, 256], BF16)
    BK = P.sb([64, 8, 4, 256], BF16)
    tok3 = P.sb([128, 8, 4, 3, 64], BF16)
    pC = P.sb([64, 8, 4], F32)
    bon = P.sb([64, 8, 512], BF16)
    gg = P.sb([64, 8, 512], BF16)
    yT = P.sb([64, 8, 512], F32)
    RW = P.sb([64, 8, 512], BF16)
    hb = {n: P.sb([64, 512], F32) for n in ('sig', 'cs', 'ep', 'em', 'epv', 'kk', 'kkn', 'a', 't', 'kp', 'b', 'u1')}
    vb = P.sb([64, 512], BF16)
    Gms = [P.sb([128, 16, 512], BF16) for _ in range(2)]
    XY = [P.sb([128, 16, 256], BF16) for _ in range(2)]
    Nms = [P.sb([128, 16, 128], BF16) for _ in range(2)]
    Wsb, Usb = P.sb([128, 8, 64], BF16), P.sb([128, 8, 64], BF16)
    pg = Rot(P, 5, [128, 512], F32, 'pg', psum=True)
    pl = Rot(P, 2, [64, 512], F32, 'pl', psum=True)
    ptr = Rot(P, 1, [128, 3, 64], BF16, 'ptr', psum=True)
    rwT = T['rwT'].rearrange("(h p) t -> p h t", p=64)

    def dve(fn, reads, writes):
        P.op('dve', fn, reads=reads, writes=writes)

    for tg in range(8):
        t0 = tg * 512
        def load_halo(dst, rows, key, q, tg=tg, t0=t0):
            if tg == 0:
                src = rows(t0, t0 + 512)
                P.op('pool', lambda e: e.memset(dst[:, 0:1] if len(dst.shape) == 2 else dst[:, :, 0:1], 0.0), reads=[key], writes=[key])
                P.dma(q, lambda e: e.dma_start(out=(dst[:, 1:513] if len(dst.shape) == 2 else dst[:, :, 1:513]), in_=src), writes=[key + 'b'])
            else:
                src = rows(t0 - 1, t0 + 512)
                P.dma(q, lambda e: e.dma_start(out=dst[:], in_=src), reads=[key + 'b'], writes=[key])
        load_halo(wa_in, lambda a, b: zr[1536:1664, a:b].rearrange("(h p) t -> p h t", p=64), 'wa', 'sp')
        load_halo(gd_in, lambda a, b: zr[1664:1792, a:b], 'gd', 'act')

        def tshift(src_prev, src_cur, mu_ap, om_ap, np_, keys):
            tm, tk = tmpr.next()
            P.op('pool', lambda e: e.tensor_scalar(out=tm[:np_, :], in0=src_prev, scalar1=mu_ap, scalar2=0.0, op0=ALU.mult, op1=ALU.add),
                 reads=keys + ['mu'], writes=[tk])
            dve(lambda e: e.scalar_tensor_tensor(out=src_cur, in0=src_cur, scalar=om_ap, in1=tm[:np_, :], op0=ALU.mult, op1=ALU.add),
                keys + [tk, 'om'], keys)
        for i in range(2):
            tshift(wa_in[:, i, 0:512], wa_in[:, i, 1:513], mu_wa[:, i:i + 1], om_wa[:, i:i + 1], 64, ['wa', 'wab'])
        tshift(gd_in[:, 0:512], gd_in[:, 1:513], mu_g[:, 0:1], om_g[:, 0:1], 128, ['gd', 'gdb'])
        P.op('act', lambda e: e.activation(out=twb[:], in_=wa_in[:, 0, 1:513], func=AF.Tanh), reads=['wa', 'wab'], writes=['twb'])
        P.op('act', lambda e: e.activation(out=sgb[:], in_=gd_in[:, 1:513], func=AF.Sigmoid), reads=['gd', 'gdb'], writes=['sgb'])
        dve(lambda e: e.tensor_copy(out=adb[:], in_=wa_in[:, 1, 1:513]), ['wa', 'wab'], ['adb'])
        def prep_head(h, z, zk):
            def zrows(a, b, h=h):
                return zr[0:1536, a:b].rearrange("(s hh p) t -> hh p s t", s=3, p=64)[h]
            load_halo(z, zrows, zk, 'sp' if h % 2 == 0 else 'act')
            for s_ in range(3):
                tshift(z[:, s_, 0:512], z[:, s_, 1:513], mu_rkv[:, s_ * 8 + h:s_ * 8 + h + 1], om_rkv[:, s_ * 8 + h:s_ * 8 + h + 1], 64, [zk, zk + 'b'])
            r_, k_, v_ = z[:, 0, 1:513], z[:, 1, 1:513], z[:, 2, 1:513]
            zkeys = [zk, zk + 'b']
            sig, cs, ep, em, epv, kk, kkn, a_, t_, kp, b_, u1 = [hb[n] for n in ('sig', 'cs', 'ep', 'em', 'epv', 'kk', 'kkn', 'a', 't', 'kp', 'b', 'u1')]
            hs = slice(h * 64, (h + 1) * 64)
            p1, p1k = pl.next()
            mm(P, p1[:], dup[:, hs], twb[:], True, True, ['wts', 'twb'], [p1k])
            P.op('act', lambda e, p1=p1, h=h: e.activation(out=sig[:], in_=p1[:], func=AF.Sigmoid, bias=w0c[:, h:h + 1]),
                 reads=[p1k, 'par'], writes=['sig'])
            dve(lambda e: e.tensor_tensor_scan(out=cs[:], data0=rst[:], data1=sig[:], initial=0.0, op0=ALU.mult, op1=ALU.add),
                ['rst', 'sig'], ['cs'])
            P.op('act', lambda e: e.activation(out=ep[:], in_=cs[:], func=AF.Exp, scale=-LD), reads=['cs'], writes=['ep'])
            P.op('act', lambda e: e.activation(out=em[:], in_=cs[:], func=AF.Exp, scale=LD), reads=['cs'], writes=['em'])
            dve(lambda e: e.tensor_tensor(out=u1[:], in0=cs[:], in1=sig[:], op=ALU.subtract), ['cs', 'sig'], ['u1'])
            P.op('act', lambda e: e.activation(out=epv[:], in_=u1[:], func=AF.Exp, scale=-LD), reads=['u1'], writes=['epv'])
            dve(lambda e, h=h: e.tensor_copy(out=pC[:, h, :], in_=ep[:, 127:512:128]), ['ep'], ['pC'])
            p2, p2k = pl.next()
            mm(P, p2[:], iup[:, hs], adb[:], True, True, ['wts', 'adb'], [p2k])
            P.op('act', lambda e, p2=p2, h=h: e.activation(out=a_[:], in_=p2[:], func=AF.Sigmoid, bias=a0c[:, h:h + 1]),
                 reads=[p2k, 'par'], writes=['a'])
            p3, p3k = pl.next()
            mm(P, p3[:], gup[:, hs], sgb[:], True, True, ['wts', 'sgb'], [p3k])
            P.op('act', lambda e, p3=p3, h=h: e.activation(out=gg[:, h, :], in_=p3[:], func=AF.Copy), reads=[p3k], writes=[f'gg{h}'])
            dve(lambda e, h=h: e.tensor_scalar(out=kk[:], in0=k_, scalar1=kkc[:, h:h + 1], scalar2=None, op0=ALU.mult), zkeys + ['par'], ['kk'])
            P.op('act', lambda e: e.activation(out=u1[:], in_=kk[:], func=AF.Square), reads=['kk', 'u1'], writes=['u1'])
            p4, p4k = pl.next()
            mm(P, p4[:], onesf[:], u1[:], True, True, ['onesf', 'u1'], [p4k])
            P.op('act', lambda e, p4=p4: e.activation(out=kkn[:], in_=p4[:], func=AF.Sqrt), reads=[p4k], writes=['kkn'])
            dve(lambda e: e.tensor_scalar(out=kkn[:], in0=kkn[:], scalar1=1e-12, scalar2=None, op0=ALU.max), ['kkn'], ['kkn'])
            dve(lambda e: e.reciprocal(out=kkn[:], in_=kkn[:]), ['kkn'], ['kkn'])
            dve(lambda e: e.tensor_tensor(out=kkn[:], in0=kkn[:], in1=kk[:], op=ALU.mult), ['kkn', 'kk'], ['kkn'])
            dve(lambda e, h=h: e.tensor_scalar(out=t_[:], in0=a_[:], scalar1=kac[:, h:h + 1], scalar2=omka[:, h:h + 1], op0=ALU.mult, op1=ALU.add),
                ['a', 'par', 'omka'], ['t'])
            dve(lambda e: e.tensor_tensor(out=kp[:], in0=t_[:], in1=k_, op=ALU.mult), ['t'] + zkeys, ['kp'])
            dve(lambda e: e.tensor_tensor(out=b_[:], in0=kkn[:], in1=a_[:], op=ALU.mult), ['kkn', 'a'], ['b'])
            c4 = lambda ap: ap.rearrange("p (c t) -> p c t", c=4)
            dve(lambda e, h=h: e.tensor_tensor(out=AR[:, h, :, 128:256], in0=c4(r_), in1=c4(ep[:]), op=ALU.mult), zkeys + ['ep'], [f'AR{h}'])
            dve(lambda e, h=h: e.scalar_tensor_tensor(out=AR[:, h, :, 0:128], in0=c4(kkn[:]), scalar=-1.0, in1=c4(epv[:]), op0=ALU.mult, op1=ALU.mult),
                ['kkn', 'epv'], [f'AR{h}'])
            dve(lambda e, h=h: e.tensor_tensor(out=BK[:, h, :, 0:128], in0=c4(b_[:]), in1=c4(em[:]), op=ALU.mult), ['b', 'em'], [f'BK{h}'])
            dve(lambda e, h=h: e.tensor_tensor(out=BK[:, h, :, 128:256], in0=c4(kp[:]), in1=c4(em[:]), op=ALU.mult), ['kp', 'em'], [f'BK{h}'])
            dve(lambda e, h=h: e.scalar_tensor_tensor(out=u1[:], in0=r_, scalar=rkc[:, h:h + 1], in1=kp[:], op0=ALU.mult, op1=ALU.mult),
                zkeys + ['kp', 'par', 'u1'], ['u1'])
            p5, p5k = pl.next()
            mm(P, p5[:], onesf[:], u1[:], True, True, ['onesf', 'u1'], [p5k])
            dve(lambda e, p5=p5, h=h: e.tensor_tensor(out=bon[:, h, :], in0=p5[:], in1=v_, op=ALU.mult), [p5k] + zkeys, [f'bon{h}'])
            P.op('pool', lambda e: e.tensor_copy(out=vb[:], in_=v_), reads=zkeys, writes=['vb'])
            for c in range(4):
                pt_, ptk = ptr.next()
                cs_ = slice(c * 128, (c + 1) * 128)
                P.op('pe', lambda e, pt_=pt_, cs_=cs_: e.transpose(out=pt_[:, 0, :], in_=vb[:, cs_], identity=ident[0:64, 0:64]), reads=['vb'], writes=[ptk])
                P.op('pe', lambda e, pt_=pt_, h=h, c=c: e.transpose(out=pt_[:, 1, :], in_=BK[:, h, c, 0:128], identity=ident[0:64, 0:64]), reads=[f'BK{h}'], writes=[ptk])
                P.op('pe', lambda e, pt_=pt_, h=h, c=c: e.transpose(out=pt_[:, 2, :], in_=BK[:, h, c, 128:256], identity=ident[0:64, 0:64]), reads=[f'BK{h}'], writes=[ptk])
                P.op('act', lambda e, pt_=pt_, h=h, c=c: e.activation(out=tok3[:, h, c, :, :], in_=pt_[:], func=AF.Copy), reads=[ptk], writes=[f'tok{h}'])
        for h in range(8):
            z, zk = zrot.next()
            prep_head(h, z, zk)
        def stage1(cs, tg=tg):
            sl = (cs[0] // 2) % 2
            Gm, Nm = Gms[sl], Nms[sl]
            probs = [(ci, c, h) for ci, c in enumerate(cs) for h in range(8)]
            for (ci, c, h) in probs:
                q = ci * 8 + h
                p_, pk = pg.next()
                mm(P, p_[:, 0:256], BK[:, h, c, 0:128], AR[:, h, c, :], True, True, [f'BK{h}', f'AR{h}'], [pk])
                mm(P, p_[:, 256:512], BK[:, h, c, 128:256], AR[:, h, c, :], True, True, [f'BK{h}', f'AR{h}'], [pk])
                dve(lambda e, p_=p_, q=q: e.tensor_tensor(out=Gm[:, q, :], in0=p_[:], in1=mask4[:], op=ALU.mult), [pk, 'mask4'], [f'Gm{sl}_{q}'])
                p2_, p2k = pg.next()
                mm(P, p2_[:, 0:128], AR[:, h, c, 0:128], BK[:, h, c, 0:128], True, True, [f'BK{h}', f'AR{h}'], [p2k])
                dve(lambda e, p2_=p2_, q=q: e.tensor_tensor(out=XY[0][:, q, 128:256], in0=p2_[:, 0:128], in1=maskL[:], op=ALU.mult),
                    [p2k, 'maskL'], [f'XY0{q}'])
                P.op('pool', lambda e, q=q: e.tensor_copy(out=XY[0][:, q, 0:128], in_=Gm[:, q, 0:128]), reads=[f'Gm{sl}_{q}'], writes=[f'XY0{q}x'])
                P.op('pool', lambda e, q=q: e.tensor_tensor(out=Nm[:, q, :], in0=Gm[:, q, 0:128], in1=ident[:], op=ALU.add),
                     reads=[f'Gm{sl}_{q}'], writes=[f'N{sl}_{q}'])
            yield
            for j in range(6):
                cur, nxt = XY[j % 2], XY[(j + 1) % 2]
                ck, nk_ = f'XY{j % 2}', f'XY{(j + 1) % 2}'
                for q in range(len(probs)):
                    p_, pk = pg.next()
                    rk_ = [ck + f'{q}', ck + f'{q}x']
                    mm(P, p_[:, 0:128], cur[:, q, 128:256], cur[:, q, 0:128], True, True, rk_, [pk])
                    mm(P, p_[:, 128:256], cur[:, q, 0:128], cur[:, q, 128:256], True, True, rk_, [pk])
                    P.op('act', lambda e, p_=p_, nxt=nxt, q=q: e.activation(out=nxt[:, q, :], in_=p_[:, 0:256], func=AF.Copy),
                         reads=[pk], writes=[nk_ + f'{q}', nk_ + f'{q}x'])
                    if q % 4 == 3:
                        yield
                for q in range(len(probs)):
                    p_, pk = pg.next()
                    mm(P, p_[:, 0:128], nxt[:, q, 128:256], Nm[:, q, :], True, True, [nk_ + f'{q}', nk_ + f'{q}x', f'N{sl}_{q}'], [pk])
                    dve(lambda e, p_=p_, q=q: e.tensor_tensor(out=Nm[:, q, :], in0=p_[:, 0:128], in1=Nm[:, q, :], op=ALU.add),
                        [pk, f'N{sl}_{q}'], [f'N{sl}_{q}'])
                    if q % 4 == 3:
                        yield

        def stage2(c, tg=tg):
            sl = (c // 2) % 2
            Gm, Nm = Gms[sl], Nms[sl]
            ci = c % 2
            pw, pwk = pg.next()
            for h in range(8):
                q = ci * 8 + h
                mm(P, pw[:, h * 64:(h + 1) * 64], AR[:, h, c, 0:128], Tst[:, h, :], True, False, [f'AR{h}', f'T{h}'], [pwk])
                mm(P, pw[:, h * 64:(h + 1) * 64], Gm[:, q, 256:384], tok3[:, h, c, 0, :], False, True, [f'Gm{sl}_{q}', f'tok{h}'], [pwk])
            P.op('act', lambda e: e.activation(out=Wsb[:].rearrange("p h v -> p (h v)"), in_=pw[:], func=AF.Copy), reads=[pwk], writes=['W'])
            yield
            pu, puk = pg.next()
            for h in range(8):
                q = ci * 8 + h
                mm(P, pu[:, h * 64:(h + 1) * 64], Nm[:, q, :], Wsb[:, h, :], True, True, [f'N{sl}_{q}', 'W'], [puk])
            P.op('act', lambda e: e.activation(out=Usb[:].rearrange("p h v -> p (h v)"), in_=pu[:], func=AF.Copy), reads=[puk], writes=['U'])
            yield
            pt_, ptk = pg.next()
            for h in range(8):
                q = ci * 8 + h
                o_ = pt_[0:64, h * 64:(h + 1) * 64]
                mm(P, o_, ident[0:64, 0:64], Tst[:, h, :], True, False, [f'T{h}'], [ptk])
                mm(P, o_, tok3[:, h, c, 1, :], Usb[:, h, :], False, False, [f'tok{h}', 'U'], [ptk])
                mm(P, o_, tok3[:, h, c, 2, :], tok3[:, h, c, 0, :], False, True, [f'tok{h}'], [ptk])
            for half in range(2):
                py_, pyk = pg.next()
                for hh in range(4):
                    h = half * 4 + hh
                    q = ci * 8 + h
                    o_ = py_[0:64, hh * 128:(hh + 1) * 128]
                    mm(P, o_, Tst[:, h, :], AR[:, h, c, 128:256], True, False, [f'AR{h}', f'T{h}'], [pyk])
                    mm(P, o_, Usb[:, h, :], Gm[:, q, 128:256], False, False, ['U', f'Gm{sl}_{q}'], [pyk])
                    mm(P, o_, tok3[:, h, c, 0, :], Gm[:, q, 384:512], False, True, [f'tok{h}', f'Gm{sl}_{q}'], [pyk])
                P.op('act', lambda e, py_=py_, half=half: e.activation(
                    out=yT[:, half * 4:half * 4 + 4, c * 128:(c + 1) * 128], in_=py_[0:64, :].rearrange("p (h t) -> p h t", h=4), func=AF.Copy),
                    reads=[pyk], writes=[f'yT{half}'])
            for h in range(8):
                dve(lambda e, h=h: e.tensor_scalar(out=Tst[:, h, :], in0=pt_[0:64, h * 64:(h + 1) * 64], scalar1=pC[:, h, c:c + 1], scalar2=None, op0=ALU.mult),
                    [ptk, 'pC', f'T{h}'], [f'T{h}'])
            yield

        def chain(*gs):
            for g in gs:
                yield from g

        def rr(g1, g2):
            a = b = True
            while a or b:
                if a:
                    try:
                        next(g1)
                    except StopIteration:
                        a = False
                if b:
                    try:
                        next(g2)
                    except StopIteration:
                        b = False
        for _ in stage1([0, 1]):
            pass
        rr(stage1([2, 3]), chain(stage2(0), stage2(1)))
        for _ in chain(stage2(2), stage2(3)):
            pass
        for h in range(8):
            u1, u2 = hb['u1'], hb['t']
            p1, p1k = pl.next()
            mm(P, p1[:], onesm[:], yT[:, h, :], True, True, ['onesm', f'yT{h // 4}'], [p1k])
            dve(lambda e, p1=p1, h=h: e.tensor_tensor(out=u1[:], in0=yT[:, h, :], in1=p1[:], op=ALU.subtract), [p1k, f'yT{h // 4}', 'u1'], ['u1'])
            P.op('act', lambda e: e.activation(out=u2[:], in_=u1[:], func=AF.Square), reads=['u1', 't'], writes=['t'])
            p2, p2k = pl.next()
            mm(P, p2[:], onesm[:], u2[:], True, True, ['onesm', 't'], [p2k])
            P.op('act', lambda e, p2=p2: e.activation(out=u2[:], in_=p2[:], func=AF.Sqrt, bias=gneps[:, 0:1]), reads=[p2k, 'gneps', 't'], writes=['t'])
            dve(lambda e: e.reciprocal(out=u2[:], in_=u2[:]), ['t'], ['t'])
            dve(lambda e: e.tensor_tensor(out=u1[:], in0=u1[:], in1=u2[:], op=ALU.mult), ['u1', 't'], ['u1'])
            dve(lambda e, h=h: e.tensor_scalar(out=u1[:], in0=u1[:], scalar1=lgc[:, h:h + 1], scalar2=lbc[:, h:h + 1], op0=ALU.mult, op1=ALU.add),
                ['u1', 'par'], ['u1'])
            dve(lambda e, h=h: e.tensor_tensor(out=u1[:], in0=u1[:], in1=bon[:, h, :], op=ALU.add), ['u1', f'bon{h}'], ['u1'])
            dve(lambda e, h=h: e.tensor_tensor(out=RW[:, h, :], in0=u1[:], in1=gg[:, h, :], op=ALU.mult), ['u1', f'gg{h}'], ['RW'])
        P.dma('sp', lambda e, t0=t0: e.dma_start(out=rwT[:, :, t0:t0 + 512], in_=RW[:]), reads=['RW'])
        if tg == 0 and 'dbg_y' in T:
            P.dma('sp', lambda e: e.dma_start(out=T['dbg_y'], in_=yT[:]), reads=['yT0', 'yT1'])
            P.dma('sp', lambda e: e.dma_start(out=T['dbg_bon'], in_=bon[:]), reads=[f'bon{h}' for h in range(8)])
            P.dma('sp', lambda e: e.dma_start(out=T['dbg_g'], in_=gg[:]), reads=[f'gg{h}' for h in range(8)])
            P.dma('sp', lambda e: e.dma_start(out=T['dbg_AR'], in_=AR[:]), reads=[f'AR{h}' for h in range(8)])
            P.dma('sp', lambda e: e.dma_start(out=T['dbg_BK'], in_=BK[:]), reads=[f'BK{h}' for h in range(8)])


def phase_F(P, T, G):
    wa, wr, wo = P.sb([128, 4, D], BF16), P.sb([128, 4, D], BF16), P.sb([128, 8, D], BF16)
    P.dma('pool', lambda e: e.dma_start(out=wa[:], in_=T['w_attn_br'].rearrange("(j p) d -> p j d", p=128)), writes=['wa'])
    P.dma('pool', lambda e: e.dma_start(out=wr[:], in_=T['w_rwkv_br'].rearrange("(j p) d -> p j d", p=128)), writes=['wr'])
    P.dma('pool', lambda e: e.dma_start(out=wo[:], in_=T['w_out'].rearrange("(j p) d -> p j d", p=128)), writes=['wo'])
    W = dict(junk=P.sb([128, D], F32), ss=P.sb([128, 1], F32), rs=P.sb([128, 1], F32), t1=P.sb([128, D], F32),
             hb=P.sb([128, D], BF16), pt=P.ps([128, 8, 128], BF16))
    W['mod'] = P.sb([128, 3072], F32)
    P.dma('sp', lambda e: e.dma_start(out=W['mod'][:], in_=T['modd'][:, 2048:5120]), writes=['modl'])
    xrot = Rot(P, 2, [128, D], F32, 'x')
    atr, rtr = Rot(P, 2, [128, 4, 128], BF16, 'at'), Rot(P, 2, [128, 4, 128], BF16, 'rt')
    gar, grr = Rot(P, 2, [128, 8, 128], BF16, 'ga'), Rot(P, 2, [128, 8, 128], BF16, 'gr')
    sga, sgr = P.sb([128, 8, 128], F32), P.sb([128, 8, 128], F32)
    m1, m2 = P.sb([128, 4, 128], F32), P.sb([128, 4, 128], F32)
    mixT = P.sb([128, 8, 128], BF16)
    x1t = P.sb([128, D], F32)
    h2t = P.sb([128, 8, 128], BF16)
    pA = Rot(P, 1, [128, 4, 128], F32, 'pA', psum=True)
    pR = Rot(P, 1, [128, 4, 128], F32, 'pR', psum=True)
    po = Rot(P, 2, [128, 512], F32, 'po', psum=True)
    aT = T['attnT'].rearrange("(j p) t -> p j t", p=128)
    rT = T['rwT'].rearrange("(j p) t -> p j t", p=128)
    gaT = T['zga'].rearrange("(j p) t -> p j t", p=128)
    grT = T['zgr'].rearrange("(j p) t -> p j t", p=128)
    h2T = T['h2T'].rearrange("(k p) t -> p k t", p=128)
    R = router_setup(P, T, G) if G.get('sparse') else None
    for i in range(NT):
        tsl = slice(i * 128, (i + 1) * 128)
        xt, xk = xrot.next()
        at, atk = atr.next(); rt, rtk = rtr.next(); ga, gak = gar.next(); gr, grk = grr.next()
        P.dma('sp', lambda e, xt=xt, tsl=tsl: e.dma_start(out=xt[:], in_=T['x'][tsl, :]), writes=[xk])
        P.dma('act', lambda e, at=at, tsl=tsl: e.dma_start(out=at[:], in_=aT[:, :, tsl]), writes=[atk])
        P.dma('act', lambda e, rt=rt, tsl=tsl: e.dma_start(out=rt[:], in_=rT[:, :, tsl]), writes=[rtk])
        P.dma('sp', lambda e, ga=ga, tsl=tsl: e.dma_start(out=ga[:], in_=gaT[:, :, tsl]), writes=[gak])
        P.dma('sp', lambda e, gr=gr, tsl=tsl: e.dma_start(out=gr[:], in_=grT[:, :, tsl]), writes=[grk])
        P.op('act', lambda e, ga=ga: e.activation(out=sga[:], in_=ga[:], func=AF.Sigmoid), reads=[gak], writes=['sga'])
        P.op('act', lambda e, gr=gr: e.activation(out=sgr[:], in_=gr[:], func=AF.Sigmoid), reads=[grk], writes=['sgr'])
        for half in range(2):
            pa, pak = pA.next(); pr, prk = pR.next()
            for s_ in range(4):
                dt = half * 4 + s_
                for j in range(4):
                    mm(P, pa[:, s_, :], wa[:, j, dt * 128:(dt + 1) * 128], at[:, j, :], j == 0, j == 3, ['wa', atk], [pak])
            for s_ in range(4):
                dt = half * 4 + s_
                for j in range(4):
                    mm(P, pr[:, s_, :], wr[:, j, dt * 128:(dt + 1) * 128], rt[:, j, :], j == 0, j == 3, ['wr', rtk], [prk])
            hs = slice(half * 4, half * 4 + 4)
            P.op('dve', lambda e, pa=pa, hs=hs: e.tensor_tensor(out=m1[:], in0=pa[:], in1=sga[:, hs, :], op=ALU.mult), reads=[pak, 'sga'], writes=['m1'])
            P.op('dve', lambda e, pr=pr, hs=hs: e.tensor_tensor(out=m2[:], in0=pr[:], in1=sgr[:, hs, :], op=ALU.mult), reads=[prk, 'sgr'], writes=['m2'])
            P.op('pool', lambda e, hs=hs: e.tensor_tensor(out=mixT[:, hs, :], in0=m1[:], in1=m2[:], op=ALU.add), reads=['m1', 'm2'], writes=['mixT'])
        for half in range(2):
            p_, pk = po.next()
            cs_ = slice(half * 512, (half + 1) * 512)
            for dt in range(8):
                mm(P, p_[:], mixT[:, dt, :], wo[:, dt, cs_], dt == 0, dt == 7, ['mixT', 'wo'], [pk])
            P.op('dve', lambda e, p_=p_, cs_=cs_: e.tensor_tensor(out=x1t[:, cs_], in0=p_[:], in1=W['mod'][:, cs_], op=ALU.mult),
                 reads=[pk, 'modl'], writes=['x1t'])
        P.op('pool', lambda e, xt=xt: e.tensor_tensor(out=x1t[:], in0=x1t[:], in1=xt[:], op=ALU.add), reads=['x1t', xk], writes=['x1t'])
        P.dma('sp', lambda e, tsl=tsl: e.dma_start(out=T['x1'][tsl, :], in_=x1t[:]), reads=['x1t'])
        norm_mod_transpose(P, G, x1t, 'x1t', h2t, 0, slice(2048, 3072), slice(1024, 2048), W)
        P.dma('act', lambda e, tsl=tsl: e.dma_start(out=h2T[:, :, tsl], in_=h2t[:]), reads=['hT0'])
        if R is not None:
            P.dma('act', lambda e, tsl=tsl: e.dma_start(out=T['h2tok'][tsl, :], in_=W['hb'][:]), reads=['hb'])
            router_tile(P, T, R, h2t, 'hT0', i)
    if R is not None:
        P.dma('sp', lambda e: e.dma_start(out=T['cntd'], in_=R['cnt'][:]), reads=['cnt'])


def phase_G0(P, T, G):
    for e in range(64):
        for (src, dst) in (('exp_gate', 'wg16'), ('exp_up', 'wu16'), ('exp_down', 'wd16')):
            P.dma('pool', lambda e_, e=e, src=src, dst=dst: e_.dma_start(
                out=T[dst][e].rearrange("k p f -> (k p f)").rearrange("(a b) -> a b", b=2048), in_=T[src][e].rearrange("r c -> (r c)").rearrange("(a b) -> a b", b=2048)))
    for (src, dst) in (('sh_gate', 'wg16'), ('sh_up', 'wu16'), ('sh_down', 'wd16')):
        P.dma('pool', lambda e_, src=src, dst=dst: e_.dma_start(
            out=T[dst][64].rearrange("k p f -> (k p f)").rearrange("(a b) -> a b", b=2048), in_=T[src].rearrange("r c -> (r c)").rearrange("(a b) -> a b", b=2048)))


def phase_G(P, T, G):
    ident = G['identb']
    h2T = T['h2T'].rearrange("(k p) t -> p k t", p=128)
    yacc = G['yacc']
    gwT = P.sb([64, S], BF16)
    rwt = P.sb([128, 8, 64], BF16)
    rbias = P.sb([128, 64], F32)
    P.dma('pool', lambda e: e.dma_start(out=rwt[:], in_=T['router_w'].rearrange("(k p) n -> p k n", p=128)), writes=['rwt'])
    P.dma('sp', lambda e: e.dma_start(out=rbias[:], in_=T['router_bias'].partition_broadcast(128)), writes=['rbias'])
    ones128 = P.sb([64, 128], BF16)
    P.op('dve', lambda e: e.memset(ones128[:], 1.0), writes=['ones128'])
    hrot = Rot(P, 2, [128, 8, 256], BF16, 'h2g')
    pmisc = P.ps([128, 512], F32)
    ptb = P.ps([64, 128], BF16)
    emb = P.sb([128, 64], BF16)
    sc_, ch, tmp, cm, em = [P.sb([128, 64], F32) for _ in range(5)]
    m1, m2, grp, s8, gmask, pen, den = [P.sb([128, 8], F32) for _ in range(7)]
    dve = lambda fn, r, w: P.op('dve', fn, reads=r, writes=w)
    for tgp in range(16):
        hg, hk = hrot.next()
        P.dma('sp', lambda e, hg=hg, tgp=tgp: e.dma_start(out=hg[:], in_=h2T[:, :, tgp * 256:(tgp + 1) * 256]), writes=[hk])
        for tt in range(2):
            i = tgp * 2 + tt
            p_, pk = pmisc[:, 0:64], 'pm_a'
            for k in range(8):
                mm(P, p_, hg[:, k, tt * 128:(tt + 1) * 128], rwt[:, k, :], k == 0, k == 7, [hk, 'rwt'], [pk])
            P.op('act', lambda e, p_=p_: e.activation(out=sc_[:], in_=p_, func=AF.Sigmoid), reads=[pk], writes=['sc'])
            dve(lambda e: e.tensor_tensor(out=ch[:], in0=sc_[:], in1=rbias[:], op=ALU.add), ['sc', 'rbias'], ['ch'])
            ch3 = ch[:].rearrange("p (g e) -> p g e", g=8)
            dve(lambda e, ch3=ch3: e.tensor_reduce(out=m1[:], in_=ch3, axis=AX.X, op=ALU.max), ['ch'], ['m1'])
            for g in range(8):
                dve(lambda e, g=g: e.tensor_scalar(out=tmp[:, g * 8:(g + 1) * 8], in0=ch[:, g * 8:(g + 1) * 8], scalar1=m1[:, g:g + 1],
                                                  scalar2=-1e9, op0=ALU.is_equal, op1=ALU.mult), ['ch', 'm1', 'tmp'], ['tmp'])
            dve(lambda e: e.tensor_tensor(out=tmp[:], in0=tmp[:], in1=ch[:], op=ALU.add), ['tmp', 'ch'], ['tmp'])
            dve(lambda e: e.tensor_reduce(out=m2[:], in_=tmp[:].rearrange("p (g e) -> p g e", g=8), axis=AX.X, op=ALU.max), ['tmp'], ['m2'])
            dve(lambda e: e.tensor_tensor(out=grp[:], in0=m1[:], in1=m2[:], op=ALU.add), ['m1', 'm2'], ['grp'])
            dve(lambda e: e.max(out=s8[:], in_=grp[:]), ['grp'], ['s8'])
            dve(lambda e: e.tensor_scalar(out=gmask[:], in0=grp[:], scalar1=s8[:, 3:4], scalar2=None, op0=ALU.is_ge), ['grp', 's8'], ['gmask'])
            dve(lambda e: e.tensor_scalar(out=pen[:], in0=gmask[:], scalar1=-1.0, scalar2=1e9, op0=ALU.add, op1=ALU.mult), ['gmask'], ['pen'])
            for g in range(8):
                dve(lambda e, g=g: e.tensor_scalar(out=cm[:, g * 8:(g + 1) * 8], in0=ch[:, g * 8:(g + 1) * 8], scalar1=pen[:, g:g + 1],
                                                  scalar2=None, op0=ALU.add), ['ch', 'pen', 'cm'], ['cm'])
            dve(lambda e: e.max(out=s8[:], in_=cm[:]), ['cm', 's8'], ['s8'])
            dve(lambda e: e.tensor_scalar(out=em[:], in0=cm[:], scalar1=s8[:, 7:8], scalar2=None, op0=ALU.is_ge), ['cm', 's8'], ['em'])
            dve(lambda e: e.tensor_tensor(out=em[:], in0=em[:], in1=sc_[:], op=ALU.mult), ['em', 'sc'], ['em'])
            dve(lambda e: e.tensor_reduce(out=den[:, 0:1], in_=em[:], axis=AX.X, op=ALU.add), ['em'], ['den'])
            dve(lambda e: e.reciprocal(out=den[:, 1:2], in_=den[:, 0:1]), ['den'], ['den'])
            dve(lambda e: e.tensor_scalar(out=em[:], in0=em[:], scalar1=den[:, 1:2], scalar2=2.5, op0=ALU.mult, op1=ALU.mult), ['em', 'den'], ['em'])
            pt_, ptk = ptb[:], 'pm_b'
            dve(lambda e: e.tensor_copy(out=emb[:], in_=em[:]), ['em', 'emb'], ['emb'])
            P.op('pe', lambda e, pt_=pt_: e.transpose(out=pt_, in_=emb[:], identity=G['identb'][:]), reads=['emb'], writes=[ptk])
            P.op('act', lambda e, pt_=pt_, i=i: e.activation(out=gwT[:, i * 128:(i + 1) * 128], in_=pt_, func=AF.Copy), reads=[ptk], writes=['gwT'])
    if G.get('gstop') == 'router':
        return
    wgr = Rot(P, 2, [128, 8, 256], BF16, 'wg')
    wur = Rot(P, 2, [128, 8, 256], BF16, 'wu')
    wdr = Rot(P, 2, [128, 2, D], BF16, 'wd')
    selr = Rot(P, 2, [64, 128], BF16, 'sel')
    pgu = Rot(P, 2, [128, 4, 256], F32, 'pgu', psum=True)
    py = Rot(P, 2, [128, 512], F32, 'py', psum=True)
    sgr_ = Rot(P, 2, [128, 2, 256], F32, 'sg')
    tr_ = Rot(P, 2, [128, 2, 256], F32, 'tt')
    actr = Rot(P, 2, [128, 2, 256], BF16, 'act')
    for e_ in range(G.get('nexp', 65)):
        wg, wgk = wgr.next(); wu, wuk = wur.next(); wd, wdk = wdr.next()
        if e_ < 64:
            sg_, su_, sd_ = T['exp_gate'][e_], T['exp_up'][e_], T['exp_down'][e_]
        else:
            sg_, su_, sd_ = T['sh_gate'], T['sh_up'], T['sh_down']
        P.dma('pool', lambda e, wg=wg, sg_=sg_: e.dma_start(out=wg[:], in_=sg_.rearrange("(k p) f -> p k f", p=128)), writes=[wgk])
        P.dma('pool', lambda e, wu=wu, su_=su_: e.dma_start(out=wu[:], in_=su_.rearrange("(k p) f -> p k f", p=128)), writes=[wuk])
        P.dma('pool', lambda e, wd=wd, sd_=sd_: e.dma_start(out=wd[:], in_=sd_.rearrange("(k p) f -> p k f", p=128)), writes=[wdk])
        if e_ < 64:
            sel, selk = selr.next()
            P.op('pool', lambda e, sel=sel, e_=e_: e.tensor_scalar(out=sel[:], in0=ones128[:], scalar1=G['identf'][0:64, e_:e_ + 1], scalar2=0.0, op0=ALU.mult, op1=ALU.add),
                 reads=['ones128'], writes=[selk])
        for tgp in range(16):
            hg, hk = hrot.next()
            P.dma('sp' if tgp % 2 == 0 else 'act', lambda e, hg=hg, tgp=tgp: e.dma_start(out=hg[:], in_=h2T[:, :, tgp * 256:(tgp + 1) * 256]), writes=[hk])
            p_, pk = pgu.next()
            for s_, (w_, wk_) in enumerate(((wg, wgk), (wg, wgk), (wu, wuk), (wu, wuk))):
                ft = s_ % 2
                for k in range(8):
                    mm(P, p_[:, s_, :], w_[:, k, ft * 128:(ft + 1) * 128], hg[:, k, :], k == 0, k == 7, [wk_, hk], [pk])
            sg, sgk = sgr_.next(); t_, tk = tr_.next(); ac, ack = actr.next()
            P.op('act', lambda e, sg=sg, p_=p_: e.activation(out=sg[:], in_=p_[:, 0:2, :], func=AF.Silu), reads=[pk], writes=[sgk])
            P.op('dve', lambda e, sg=sg, p_=p_, t_=t_: e.tensor_tensor(out=t_[:], in0=p_[:, 2:4, :], in1=sg[:], op=ALU.mult), reads=[pk, sgk], writes=[tk])
            if e_ < 64:
                pw, pwk = pmisc[:, 256:512], 'pm_c'
                mm(P, pw, sel[:], gwT[:, tgp * 256:(tgp + 1) * 256], True, True, [selk, 'gwT'], [pwk])
                for ft in range(2):
                    P.op('dve', lambda e, ac=ac, t_=t_, pw=pw, ft=ft: e.tensor_tensor(out=ac[:, ft, :], in0=pw, in1=t_[:, ft, :], op=ALU.mult),
                         reads=[tk, pwk], writes=[ack])
            else:
                P.op('pool', lambda e, ac=ac, t_=t_: e.tensor_copy(out=ac[:], in_=t_[:]), reads=[tk], writes=[ack])
            for tt in range(2):
                i = tgp * 2 + tt
                for half in range(2):
                    q_, qk = py.next()
                    cs_ = slice(half * 512, (half + 1) * 512)
                    for ft in range(2):
                        mm(P, q_[:], ac[:, ft, tt * 128:(tt + 1) * 128], wd[:, ft, cs_], ft == 0, ft == 1, [ack, wdk], [qk])
                    if e_ == 0:
                        P.op('act', lambda e, q_=q_, i=i, cs_=cs_: e.activation(out=yacc[:, i, cs_], in_=q_[:], func=AF.Copy), reads=[qk], writes=[f'y{i}'])
                    else:
                        P.op('dve', lambda e, q_=q_, i=i, cs_=cs_: e.tensor_tensor(out=yacc[:, i, cs_], in0=q_[:], in1=yacc[:, i, cs_], op=ALU.add),
                             reads=[qk, f'y{i}'], writes=[f'y{i}'])


def phase_H(P, T, G):
    yacc = G['yacc']
    dve = lambda fn, r, w: P.op('dve', fn, reads=r, writes=w)
    g2b = P.sb([128, D], F32)
    fing = P.sb([128, D], F32)
    P.dma('sp', lambda e: e.dma_start(out=g2b[:], in_=T['modd'][:, 5120:6144]), writes=['g2b'])
    P.dma('sp', lambda e: e.dma_start(out=fing[:], in_=T['final_g'].partition_broadcast(128)), writes=['fing'])
    xr = Rot(P, 2, [128, D], F32, 'x1')
    junk, ss, rs = P.sb([128, D], F32), P.sb([128, 1], F32), P.sb([128, 1], F32)
    for i in range(NT):
        tsl = slice(i * 128, (i + 1) * 128)
        xt, xk = xr.next()
        P.dma('sp', lambda e, xt=xt, tsl=tsl: e.dma_start(out=xt[:], in_=T['x1'][tsl, :]), writes=[xk])
        dve(lambda e, i=i: e.tensor_tensor(out=yacc[:, i, :], in0=yacc[:, i, :], in1=g2b[:], op=ALU.mult), [f'y{i}', 'g2b'], [f'y{i}'])
        P.op('pool', lambda e, i=i, xt=xt: e.tensor_tensor(out=xt[:], in0=xt[:], in1=yacc[:, i, :], op=ALU.add), reads=[f'y{i}', xk], writes=[xk])
        rms_rstd(P, xt, xk, junk, ss, rs, 'f')
        dve(lambda e, xt=xt: e.scalar_tensor_tensor(out=xt[:], in0=xt[:], scalar=rs[:, 0:1], in1=fing[:], op0=ALU.mult, op1=ALU.mult),
            [xk, 'rsf', 'fing'], [xk])
        P.dma('sp', lambda e, xt=xt, tsl=tsl: e.dma_start(out=T['out'][tsl, :], in_=xt[:]), reads=[xk])


NBLK = 320


def router_setup(P, T, G):
    R = {}
    R['rwt'] = P.sb([128, 8, 64], BF16)
    R['rbias'] = P.sb([128, 64], F32)
    R['iota'] = P.sb([128, 64], F32)
    R['su'] = P.sb([128, 128], BF16)
    R['onesb'] = P.sb([128, 128], BF16)
    R['cnt'] = P.sb([128, 64], F32)
    R['kmf'] = P.sb([128, 128], F32)
    P.dma('pool', lambda e: e.dma_start(out=R['rwt'][:], in_=T['router_w'].rearrange("(k p) n -> p k n", p=128)), writes=['rwt'])
    P.dma('sp', lambda e: e.dma_start(out=R['rbias'][:], in_=T['router_bias'].partition_broadcast(128)), writes=['rbias'])
    P.dma('sp', lambda e: e.dma_start(out=R['iota'][:], in_=T['k_rel'][0:1, 0:64].rearrange("o f -> (o f)").partition_broadcast(128)), writes=['iota'])
    P.dma('sp', lambda e: e.dma_start(out=R['kmf'][:], in_=T['k_masks'][:, 0:128]), writes=['kmf'])
    P.op('dve', lambda e: e.tensor_copy(out=R['su'][:], in_=R['kmf'][:]), reads=['kmf'], writes=['su'])
    P.op('dve', lambda e: e.memset(R['onesb'][:], 1.0), writes=['onesb'])
    P.op('dve', lambda e: e.memset(R['cnt'][:], 0.0), writes=['cnt'])
    R['pm'] = P.ps([128, 512], F32)
    for n in ('sc', 'ch', 'tmp', 'cm', 'em', 'mk', 'oh', 'rk', 'jk'):
        R[n] = P.sb([128, 64], F32)
    R['mkb'] = P.sb([128, 64], BF16)
    for n in ('m1', 'm2', 'grp', 's8', 'gmask', 'pen', 'den', 'i8f'):
        R[n] = P.sb([128, 8], F32)
    R['i8u'] = P.sb([128, 8], U32)
    R['meta'] = P.sb([128, 24], F32)
    return R


def router_tile(P, T, R, h2t, h2k, i):
    dve = lambda fn, r, w: P.op('dve', fn, reads=r, writes=w)
    pm = R['pm']
    sc_, ch, tmp, cm, em, mk, oh, rk, jk, mkb = [R[n] for n in ('sc', 'ch', 'tmp', 'cm', 'em', 'mk', 'oh', 'rk', 'jk', 'mkb')]
    m1, m2, grp, s8, gmask, pen, den, i8f, i8u, meta = [R[n] for n in ('m1', 'm2', 'grp', 's8', 'gmask', 'pen', 'den', 'i8f', 'i8u', 'meta')]
    for k in range(8):
        mm(P, pm[:, 0:64], h2t[:, k, :], R['rwt'][:, k, :], k == 0, k == 7, [h2k, 'rwt'], ['pm_a'])
    P.op('act', lambda e: e.activation(out=sc_[:], in_=pm[:, 0:64], func=AF.Sigmoid), reads=['pm_a'], writes=['sc'])
    dve(lambda e: e.tensor_tensor(out=ch[:], in0=sc_[:], in1=R['rbias'][:], op=ALU.add), ['sc', 'rbias'], ['ch'])
    dve(lambda e: e.tensor_reduce(out=m1[:], in_=ch[:].rearrange("p (g e) -> p g e", g=8), axis=AX.X, op=ALU.max), ['ch'], ['m1'])
    for g in range(8):
        dve(lambda e, g=g: e.tensor_scalar(out=tmp[:, g * 8:(g + 1) * 8], in0=ch[:, g * 8:(g + 1) * 8], scalar1=m1[:, g:g + 1],
                                          scalar2=-1e9, op0=ALU.is_equal, op1=ALU.mult), ['ch', 'm1', 'tmp'], ['tmp'])
    dve(lambda e: e.tensor_tensor(out=tmp[:], in0=tmp[:], in1=ch[:], op=ALU.add), ['tmp', 'ch'], ['tmp'])
    dve(lambda e: e.tensor_reduce(out=m2[:], in_=tmp[:].rearrange("p (g e) -> p g e", g=8), axis=AX.X, op=ALU.max), ['tmp'], ['m2'])
    dve(lambda e: e.tensor_tensor(out=grp[:], in0=m1[:], in1=m2[:], op=ALU.add), ['m1', 'm2'], ['grp'])
    dve(lambda e: e.max(out=s8[:], in_=grp[:]), ['grp'], ['s8'])
    dve(lambda e: e.tensor_scalar(out=gmask[:], in0=grp[:], scalar1=s8[:, 3:4], scalar2=None, op0=ALU.is_ge), ['grp', 's8'], ['gmask'])
    dve(lambda e: e.tensor_scalar(out=pen[:], in0=gmask[:], scalar1=-1.0, scalar2=1e9, op0=ALU.add, op1=ALU.mult), ['gmask'], ['pen'])
    for g in range(8):
        dve(lambda e, g=g: e.tensor_scalar(out=cm[:, g * 8:(g + 1) * 8], in0=ch[:, g * 8:(g + 1) * 8], scalar1=pen[:, g:g + 1],
                                          scalar2=None, op0=ALU.add), ['ch', 'pen', 'cm'], ['cm'])
    dve(lambda e: e.max(out=s8[:], in_=cm[:]), ['cm', 's8'], ['s8'])
    dve(lambda e: e.max_index(out=i8u[:], in_max=s8[:], in_values=cm[:]), ['cm', 's8', 'i8u'], ['i8u'])
    dve(lambda e: e.tensor_copy(out=meta[:, 0:8], in_=i8u[:]), ['i8u', 'meta'], ['meta'])
    dve(lambda e: e.tensor_scalar(out=mk[:], in0=cm[:], scalar1=s8[:, 7:8], scalar2=None, op0=ALU.is_ge), ['cm', 's8'], ['mk'])
    dve(lambda e: e.tensor_copy(out=mkb[:], in_=mk[:]), ['mk', 'mkb'], ['mkb'])
    dve(lambda e: e.tensor_tensor(out=em[:], in0=mk[:], in1=sc_[:], op=ALU.mult), ['mk', 'sc'], ['em'])
    dve(lambda e: e.tensor_reduce(out=den[:, 0:1], in_=em[:], axis=AX.X, op=ALU.add), ['em'], ['den'])
    dve(lambda e: e.reciprocal(out=den[:, 1:2], in_=den[:, 0:1]), ['den'], ['den'])
    dve(lambda e: e.tensor_scalar(out=em[:], in0=em[:], scalar1=den[:, 1:2], scalar2=2.5, op0=ALU.mult, op1=ALU.mult), ['em', 'den'], ['em'])
    mm(P, pm[:, 64:128], R['su'][:], mkb[:], True, True, ['su', 'mkb'], ['pm_b'])
    mm(P, pm[:, 128:192], R['onesb'][:], mkb[:], True, True, ['onesb', 'mkb'], ['pm_c'])
    dve(lambda e: e.tensor_tensor(out=rk[:], in0=pm[:, 64:128], in1=R['cnt'][:], op=ALU.add), ['pm_b', 'cnt', 'rk'], ['rk'])
    dve(lambda e: e.tensor_tensor(out=R['cnt'][:], in0=pm[:, 128:192], in1=R['cnt'][:], op=ALU.add), ['pm_c', 'cnt'], ['cnt'])
    for k in range(8):
        dve(lambda e, k=k: e.tensor_scalar(out=oh[:], in0=R['iota'][:], scalar1=meta[:, k:k + 1], scalar2=None, op0=ALU.is_equal),
            ['iota', 'meta', 'oh'], ['oh'])
        dve(lambda e, k=k: e.scalar_tensor_tensor(out=jk[:], in0=oh[:], scalar=1.0, in1=rk[:], op0=ALU.mult, op1=ALU.mult, accum_out=meta[:, 8 + k:9 + k]), ['oh', 'rk', 'jk', 'meta'], ['jk', 'meta'])
        dve(lambda e, k=k: e.scalar_tensor_tensor(out=jk[:], in0=oh[:], scalar=1.0, in1=em[:], op0=ALU.mult, op1=ALU.mult, accum_out=meta[:, 16 + k:17 + k]), ['oh', 'em', 'jk', 'meta'], ['jk', 'meta'])
    P.dma('sp', lambda e: e.dma_start(out=T['meta'][i * 128:(i + 1) * 128, :], in_=meta[:]), reads=['meta'])


def phase_S0(P, T, G):
    st32 = Rot(P, 3, [128, 2048], F32, 's32')
    st16 = Rot(P, 3, [128, 2048], BF16, 's16')
    n = 0
    for e in range(65):
        for (src, shsrc, dst) in (('exp_gate', 'sh_gate', 'wg16'), ('exp_up', 'sh_up', 'wu16'), ('exp_down', 'sh_down', 'wd16')):
            s_ap = T[src][e] if e < 64 else T[shsrc]
            a, ak = st32.next()
            c, ck = st16.next()
            f = 256 if dst != 'wd16' else D
            q = 'sp' if n % 2 == 0 else 'act'
            P.dma(q, lambda e_, a=a, s_ap=s_ap, f=f: e_.dma_start(out=a[:].rearrange("p (k f) -> p k f", f=f), in_=s_ap.rearrange("(k p) f -> p k f", p=128)), writes=[ak])
            eng = ('act', 'dve', 'pool')[n % 3]
            if eng == 'act':
                P.op('act', lambda e_, a=a, c=c: e_.activation(out=c[:], in_=a[:], func=AF.Copy), reads=[ak], writes=[ck])
            else:
                P.op(eng, lambda e_, a=a, c=c: e_.tensor_copy(out=c[:], in_=a[:]), reads=[ak], writes=[ck])
            c0 = {'wg16': 0, 'wu16': 2048, 'wd16': 4096}[dst]
            P.dma('act' if n % 2 == 0 else 'sp', lambda e_, c=c, c0=c0, e=e: e_.dma_start(out=T['wall16'][e * 128:(e + 1) * 128, c0:c0 + 2048], in_=c[:]), reads=[ck])
            n += 1
            yield


def phase_S(P, T, G):
    identb = G['identb']
    dve = lambda fn, r, w: P.op('dve', fn, reads=r, writes=w)
    cnt = P.sb([128, 64], F32)
    ci = P.sb([128, 64], I32)
    pad = P.sb([128, 64], F32)
    pend = P.sb([128, 64], F32)
    pst = P.sb([128, 64], F32)
    ones64f = P.sb([128, 64], F32)
    iota = P.sb([128, 64], F32)
    bst = P.sb([128, NBLK], F32)
    bef = P.sb([128, NBLK], F32)
    bei = P.sb([128, NBLK], I32)
    P.dma('sp', lambda e: e.dma_start(out=cnt[:], in_=T['cntd']), writes=['cnt'])
    P.dma('sp', lambda e: e.dma_start(out=iota[:], in_=T['k_rel'][0:1, 0:64].rearrange("o f -> (o f)").partition_broadcast(128)), writes=['iota'])
    P.dma('sp', lambda e: e.dma_start(out=bst[:], in_=T['k_bst'].partition_broadcast(128)), writes=['bst'])
    dve(lambda e: e.memset(ones64f[:], 1.0), [], ['ones64f'])
    dve(lambda e: e.tensor_scalar(out=pad[:], in0=cnt[:], scalar1=127.0, scalar2=None, op0=ALU.add), ['cnt'], ['pad'])
    dve(lambda e: e.tensor_copy(out=ci[:], in_=pad[:]), ['pad'], ['ci'])
    dve(lambda e: e.tensor_scalar(out=ci[:], in0=ci[:], scalar1=7, scalar2=None, op0=ALU.arith_shift_right), ['ci'], ['ci'])
    dve(lambda e: e.tensor_scalar(out=ci[:], in0=ci[:], scalar1=7, scalar2=None, op0=ALU.logical_shift_left), ['ci'], ['ci'])
    dve(lambda e: e.tensor_copy(out=pad[:], in_=ci[:]), ['ci', 'pad'], ['pad'])
    dve(lambda e: e.tensor_tensor_scan(out=pend[:], data0=ones64f[:], data1=pad[:], initial=0.0, op0=ALU.mult, op1=ALU.add),
        ['ones64f', 'pad'], ['pend'])
    dve(lambda e: e.tensor_tensor(out=pst[:], in0=pend[:], in1=pad[:], op=ALU.subtract), ['pend', 'pad'], ['pst'])
    for ex in range(64):
        if ex == 0:
            dve(lambda e: e.tensor_scalar(out=bef[:], in0=bst[:], scalar1=pend[:, 0:1], scalar2=None, op0=ALU.is_ge), ['bst', 'pend'], ['bef'])
        else:
            dve(lambda e, ex=ex: e.scalar_tensor_tensor(out=bef[:], in0=bst[:], scalar=pend[:, ex:ex + 1], in1=bef[:], op0=ALU.is_ge, op1=ALU.add),
                ['bst', 'pend', 'bef'], ['bef'])
    dve(lambda e: e.tensor_scalar(out=bef[:], in0=bef[:], scalar1=63.0, scalar2=None, op0=ALU.min), ['bef'], ['bef'])
    pcol = P.sb([128, 1], F32)
    widxf = P.sb([128, NBLK], F32)
    widx = P.sb([128, NBLK], I32)
    P.dma('sp', lambda e: e.dma_start(out=pcol[:], in_=T['k_rel'][:, 0:1], allow_slow_non_contiguous=True), writes=['pcol'])
    dve(lambda e: e.tensor_scalar(out=pcol[:], in0=pcol[:], scalar1=-1.0, scalar2=None, op0=ALU.mult), ['pcol'], ['pcol'])
    dve(lambda e: e.tensor_scalar(out=widxf[:], in0=bef[:], scalar1=128.0, scalar2=pcol[:, 0:1], op0=ALU.mult, op1=ALU.add), ['bef', 'pcol'], ['widxf'])
    chg = P.sb([128, NBLK], F32)
    dve(lambda e: e.memset(chg[:], 1.0), [], ['chg'])
    dve(lambda e: e.tensor_tensor(out=chg[:, 3:NBLK], in0=bef[:, 3:NBLK], in1=bef[:, 0:NBLK - 3], op=ALU.not_equal), ['bef', 'chg'], ['chg'])
    dve(lambda e: e.scalar_tensor_tensor(out=widxf[:], in0=widxf[:], scalar=-1.0e6, in1=chg[:], op0=ALU.add, op1=ALU.mult), ['widxf', 'chg'], ['widxf'])
    dve(lambda e: e.tensor_scalar(out=widxf[:], in0=widxf[:], scalar1=1.0e6, scalar2=None, op0=ALU.add), ['widxf'], ['widxf'])
    dve(lambda e: e.tensor_copy(out=widx[:], in_=widxf[:]), ['widxf'], ['widx'])
    d8i = P.sb([128, NT, 8], I32)
    gw8 = P.sb([128, NT, 8], F32)
    mrot = Rot(P, 2, [128, 24], F32, 'meta')
    hrot = Rot(P, 2, [128, D], BF16, 'htok')
    oh, jk = P.sb([128, 64], F32), P.sb([128, 64], F32)
    d8f = P.sb([128, 8], F32)
    for i in range(NT):
        tsl = slice(i * 128, (i + 1) * 128)
        mt, mtk = mrot.next()
        ht, htk = hrot.next()
        P.dma('sp', lambda e, mt=mt, tsl=tsl: e.dma_start(out=mt[:], in_=T['meta'][tsl, :]), writes=[mtk])
        P.dma('act', lambda e, ht=ht, tsl=tsl: e.dma_start(out=ht[:], in_=T['h2tok'][tsl, :]), writes=[htk])
        for k in range(8):
            dve(lambda e, mt=mt, k=k: e.tensor_scalar(out=oh[:], in0=iota[:], scalar1=mt[:, k:k + 1], scalar2=None, op0=ALU.is_equal),
                ['iota', mtk, 'oh'], ['oh'])
            dve(lambda e, k=k: e.scalar_tensor_tensor(out=jk[:], in0=oh[:], scalar=1.0, in1=pst[:], op0=ALU.mult, op1=ALU.mult, accum_out=d8f[:, k:k + 1]), ['oh', 'pst', 'jk', 'd8f'], ['jk', 'd8f'])
        dve(lambda e, mt=mt: e.tensor_tensor(out=d8f[:], in0=d8f[:], in1=mt[:, 8:16], op=ALU.add), ['d8f', mtk], ['d8f'])
        dve(lambda e, i=i: e.tensor_copy(out=d8i[:, i, :], in_=d8f[:]), ['d8f'], [f'd8i{i}'])
        dve(lambda e, mt=mt, i=i: e.tensor_copy(out=gw8[:, i, :], in_=mt[:, 16:24]), [mtk], [f'gw{i}'])
        for k in range(8):
            P.dma('pool', lambda e, ht=ht, i=i, k=k: e.indirect_dma_start(
                out=T['Xs'], out_offset=bass.IndirectOffsetOnAxis(ap=d8i[:, i, k:k + 1], axis=0), in_=ht[:], in_offset=None),
                reads=[htk, f'd8i{i}'])
    wgu = Rot(P, 3, [128, 6144], BF16, 'wgu')
    wsh = P.sb([128, 6144], BF16)
    P.dma('sp', lambda e: e.dma_start(out=wsh[:], in_=T['wall16'][64 * 128:65 * 128, :]), writes=['wsh'])
    xbr = Rot(P, 4, [128, D], BF16, 'xb')
    xTr = Rot(P, 2, [128, 8, 128], BF16, 'xT')
    sgr_ = Rot(P, 2, [128, 256], F32, 'sg')
    acr = Rot(P, 2, [128, 256], BF16, 'ac')
    aTr = Rot(P, 2, [128, 2, 128], BF16, 'aT')
    ybr = Rot(P, 2, [128, D], BF16, 'yb')
    ptx = Rot(P, 1, [128, 8, 128], BF16, 'ptx', psum=True)
    pgu = Rot(P, 2, [128, 512], F32, 'pgu', psum=True)
    pta = Rot(P, 1, [128, 2, 128], BF16, 'pta', psum=True)
    pyd = Rot(P, 3, [128, 512], F32, 'pyd', psum=True)
    regn = [0]

    P.emit(keep=True)
    hold = {}
    blocks = [('r', b) for b in range(NBLK)] + [('s', i) for i in range(NT)]
    st = {}

    ld = {}

    def stageL(kind, b):
        xb, xk = xbr.next()
        if kind == 'r':
            wl, wk = wgu.next()

            def gat(e):
                if 'bc' not in hold:
                    hold['bc'] = e.alloc_register("bc_reg")
                    e.reg_mov(hold['bc'], 65 * 128 - 1)
                return e.indirect_dma_start(out=wl[:], out_offset=None, in_=T['wall16'],
                                            in_offset=bass.IndirectOffsetOnAxis(ap=widx[:, b:b + 1], axis=0),
                                            bounds_check=hold['bc'], oob_is_err=False)
            P.dma('pool', gat, reads=['widx'], writes=[wk])
            P.dma('sp', lambda e: e.dma_start(out=xb[:], in_=T['Xs'][b * 128:(b + 1) * 128, :]), writes=[xk])
        else:
            wl, wk = wsh, 'wsh'
            P.dma('sp', lambda e: e.dma_start(out=xb[:], in_=T['h2tok'][b * 128:(b + 1) * 128, :]), writes=[xk])
        ld[(kind, b)] = (xb, xk, wl, wk)

    def stageA(kind, b):
        xb, xk, wl, wk = ld.pop((kind, b))
        wga, wua = wl[:, 0:2048], wl[:, 2048:4096]
        wd, wdk = wl[:, 4096:6144].rearrange("p (k f) -> p k f", f=D), wk
        px, pxk = ptx.next()
        for k in range(8):
            P.op('pe', lambda e, k=k: e.transpose(out=px[:, k, :], in_=xb[:, k * 128:(k + 1) * 128], identity=identb[:]), reads=[xk], writes=[pxk])
        xT, xTk = xTr.next()
        P.op('act', lambda e: e.activation(out=xT[:], in_=px[:], func=AF.Copy), reads=[pxk], writes=[xTk])
        pg_, pgk = pgu.next()
        for k in range(8):
            mm(P, pg_[:, 0:256], xT[:, k, :], wga[:, k * 256:(k + 1) * 256], k == 0, False, [xTk, wk], [pgk])
        for k in range(8):
            P.op('pe', lambda e, k=k: e.matmul(pg_[:, 256:512], lhsT=xT[:, k, :], rhs=wua[:, k * 256:(k + 1) * 256], start=False, stop=(k == 7), skip_group_check=True), reads=[xTk, wk], writes=[pgk])
        st[(kind, b)] = (pg_, pgk, wd, wdk)

    def stageB(kind, b):
        pg_, pgk, wd, wdk = st.pop((kind, b))
        sg, sgk = sgr_.next(); ac, ack = acr.next()
        P.op('act', lambda e: e.activation(out=sg[:], in_=pg_[:, 0:256], func=AF.Silu), reads=[pgk], writes=[sgk])
        dve(lambda e: e.tensor_tensor(out=ac[:], in0=pg_[:, 256:512], in1=sg[:], op=ALU.mult), [pgk, sgk], [ack])
        pa, pak = pta.next()
        for ft in range(2):
            P.op('pe', lambda e, ft=ft: e.transpose(out=pa[:, ft, :], in_=ac[:, ft * 128:(ft + 1) * 128], identity=identb[:]), reads=[ack], writes=[pak])
        aT, aTk = aTr.next()
        dve(lambda e: e.tensor_copy(out=aT[:], in_=pa[:]), [pak], [aTk])
        yb, ybk = ybr.next()
        for half in range(2):
            py_, pyk = pyd.next()
            cs_ = slice(half * 512, (half + 1) * 512)
            for ft in range(2):
                mm(P, py_[:], aT[:, ft, :], wd[:, ft, cs_], ft == 0, ft == 1, [aTk, wdk], [pyk])
            if half == 0:
                P.op('act', lambda e, py_=py_, cs_=cs_: e.activation(out=yb[:, cs_], in_=py_[:], func=AF.Copy), reads=[pyk], writes=[ybk])
            else:
                dve(lambda e, py_=py_, cs_=cs_: e.tensor_copy(out=yb[:, cs_], in_=py_[:]), [pyk], [ybk])
        dst = T['Ys'] if kind == 'r' else T['Ysh']
        P.dma('sp', lambda e: e.dma_start(out=dst[b * 128:(b + 1) * 128, :], in_=yb[:]), reads=[ybk])

    stageL(*blocks[0])
    stageL(*blocks[1])
    stageA(*blocks[0])
    for bi in range(len(blocks)):
        if bi + 2 < len(blocks):
            stageL(*blocks[bi + 2])
        if bi + 1 < len(blocks):
            stageA(*blocks[bi + 1])
        stageB(*blocks[bi])
    P.emit(keep=True)
    g2b = P.sb([128, D], F32)
    fing = P.sb([128, D], F32)
    P.dma('sp', lambda e: e.dma_start(out=g2b[:], in_=T['modd'][:, 5120:6144]), writes=['g2b'])
    P.dma('sp', lambda e: e.dma_start(out=fing[:], in_=T['final_g'].partition_broadcast(128)), writes=['fing'])
    xr = Rot(P, 2, [128, D], F32, 'x1')
    grot = Rot(P, 4, [128, D], BF16, 'gat')
    shr = Rot(P, 2, [128, D], BF16, 'shr')
    acc = P.sb([128, D], F32)
    junk, ss, rs = P.sb([128, D], F32), P.sb([128, 1], F32), P.sb([128, 1], F32)
    for i in range(NT):
        tsl = slice(i * 128, (i + 1) * 128)
        xt, xk = xr.next()
        sh, shk = shr.next()
        P.dma('sp', lambda e, xt=xt, tsl=tsl: e.dma_start(out=xt[:], in_=T['x1'][tsl, :]), writes=[xk])
        P.dma('act', lambda e, sh=sh, tsl=tsl: e.dma_start(out=sh[:], in_=T['Ysh'][tsl, :]), writes=[shk])
        for k in range(8):
            gt, gtk = grot.next()
            P.dma('pool', lambda e, gt=gt, i=i, k=k: e.indirect_dma_start(
                out=gt[:], out_offset=None, in_=T['Ys'], in_offset=bass.IndirectOffsetOnAxis(ap=d8i[:, i, k:k + 1], axis=0)),
                reads=[f'd8i{i}'], writes=[gtk])
            if k == 0:
                dve(lambda e, gt=gt, i=i, sh=sh: e.scalar_tensor_tensor(out=acc[:], in0=gt[:], scalar=gw8[:, i, 0:1], in1=sh[:], op0=ALU.mult, op1=ALU.add),
                    [gtk, f'gw{i}', shk, 'acc'], ['acc'])
            else:
                dve(lambda e, gt=gt, i=i, k=k: e.scalar_tensor_tensor(out=acc[:], in0=gt[:], scalar=gw8[:, i, k:k + 1], in1=acc[:], op0=ALU.mult, op1=ALU.add),
                    [gtk, f'gw{i}', 'acc'], ['acc'])
        dve(lambda e: e.tensor_tensor(out=acc[:], in0=acc[:], in1=g2b[:], op=ALU.mult), ['acc', 'g2b'], ['acc'])
        P.op('pool', lambda e, xt=xt: e.tensor_tensor(out=xt[:], in0=xt[:], in1=acc[:], op=ALU.add), reads=['acc', xk], writes=[xk])
        rms_rstd(P, xt, xk, junk, ss, rs, 'f')
        dve(lambda e, xt=xt: e.scalar_tensor_tensor(out=xt[:], in0=xt[:], scalar=rs[:, 0:1], in1=fing[:], op0=ALU.mult, op1=ALU.mult),
            [xk, 'rsf', 'fing'], [xk])
        P.dma('sp', lambda e, xt=xt, tsl=tsl: e.dma_start(out=T['out'][tsl, :], in_=xt[:]), reads=[xk])


SCRATCH = [
    ('zq', [512, S], BF16), ('zk', [512, S], BF16), ('zv', [S, 512], BF16), ('ziq', [512, S], BF16),
    ('zik', [32, S], BF16), ('ziw', [S, 16], F32), ('zr', [1792, S], F32), ('zga', [1024, S], BF16),
    ('zgr', [1024, S], BF16), ('modd', [128, 6 * D], F32), ('attnT', [512, S], BF16), ('rwT', [512, S], BF16),
    ('x1', [S, D], F32), ('h2T', [D, S], BF16), ('h2tok', [S, D], BF16), ('meta', [S, 24], F32), ('cntd', [128, 64], F32),
    ('Xs', [NBLK * 128, D], BF16), ('Ys', [NBLK * 128, D], BF16), ('Ysh', [S, D], BF16),
    ('wall16', [65 * 128, 6144], BF16),
]

INPUT_SHAPES = [
    ('x', [S, D]), ('c_col', [128, 8]), ('ada_w', [D, 6 * D]), ('ada_b', [1, 6 * D]), ('norm1_g', [D]),
    ('w_in', [D, NIN]), ('rel_bias', [256]), ('tshift_mu', [1792]), ('decay_w0', [512]), ('decay_up', [64, 512]),
    ('iclr_a0', [512]), ('iclr_up', [64, 512]), ('gate_up', [128, 512]), ('k_k', [512]), ('k_a', [512]),
    ('r_k', [512]), ('lnx_g', [512]), ('lnx_b', [512]), ('w_attn_br', [512, D]), ('w_rwkv_br', [512, D]),
    ('w_out', [D, D]), ('norm2_g', [D]), ('router_w', [D, 64]), ('router_bias', [64]),
    ('exp_gate', [64, D, 256]), ('exp_up', [64, D, 256]), ('exp_down', [64, 256, D]),
    ('sh_gate', [D, 256]), ('sh_up', [D, 256]), ('sh_down', [256, D]), ('final_g', [D]),
    ('k_ident', [128, 128]), ('k_rel', [128, 256]), ('k_masks', [128, 896]), ('k_reset', [64, 512]), ('k_bst', [NBLK]),
]


def build(debug_outs=(), stop_after=None):
    nc = bass.Bass("TRN2", target_bir_lowering=False)
    T = {}
    for name, shp in INPUT_SHAPES:
        T[name] = nc.dram_tensor(name, shp, F32, kind="ExternalInput").ap()
    for name, shp, dt in SCRATCH:
        kind = "ExternalOutput" if name in debug_outs else "Internal"
        T[name] = nc.dram_tensor(name, shp, dt, kind=kind).ap()
    T['out'] = nc.dram_tensor('out', [S, D], F32, kind="ExternalOutput").ap()
    if 'dbg_y' in debug_outs:
        T['dbg_y'] = nc.dram_tensor('dbg_y', [64, 8, 512], F32, kind="ExternalOutput").ap()
        T['dbg_bon'] = nc.dram_tensor('dbg_bon', [64, 8, 512], BF16, kind="ExternalOutput").ap()
        T['dbg_g'] = nc.dram_tensor('dbg_g', [64, 8, 512], BF16, kind="ExternalOutput").ap()
        T['dbg_AR'] = nc.dram_tensor('dbg_AR', [64, 8, 4, 256], BF16, kind="ExternalOutput").ap()
        T['dbg_BK'] = nc.dram_tensor('dbg_BK', [64, 8, 4, 256], BF16, kind="ExternalOutput").ap()
    P = Prog(nc)
    G = {}
    G['E'] = P.gsb([128, 8, 256], F32)
    G['b31'] = P.gsb([128, 8], F32)
    G['ones_row'] = P.gsb([1, 128], F32)
    G['identf'] = P.gsb([128, 128], F32)
    G['identb'] = P.gsb([128, 128], BF16)
    G['eps'] = P.gsb([128, 1], F32)
    G_EPS[0] = G['eps']
    P.op('dve', lambda e: e.memset(G['ones_row'][:], 1.0), writes=['ones_row'])
    P.op('dve', lambda e: e.memset(G['eps'][:], 1e-6), writes=['eps'])
    P.dma('sp', lambda e: e.dma_start(out=G['identf'][:], in_=T['k_ident']), writes=['identf'])
    P.op('dve', lambda e: e.tensor_copy(out=G['identb'][:], in_=G['identf'][:]), reads=['identf'], writes=['identb'])
    G['sparse'] = SPARSE
    phase_A(P, T, G)
    P.emit()
    if stop_after == 'A':
        P.emit(); P.finish(); return nc
    G['sparse'] = SPARSE
    phase_BC(P, T, G)
    P.emit()
    if stop_after == 'C':
        P.finish(); return nc
    phase_D0(P, T, G)
    P.emit()
    phase_D(P, T, G)
    P.emit()
    if stop_after == 'D':
        P.finish(); return nc
    phase_E(P, T, G)
    P.emit()
    if stop_after == 'E':
        P.finish(); return nc
    G['sparse'] = SPARSE
    phase_F(P, T, G)
    P.emit()
    if stop_after == 'F':
        P.finish(); return nc
    if SPARSE:
        phase_S(P, T, G)
        P.emit()
        P.finish()
        return nc
    G['yacc'] = P.gsb([128, NT, D], F32)
    if stop_after in ('router', 'exp1'):
        G['gstop'] = stop_after
        G['nexp'] = 1
    if stop_after == 'router':
        phase_G(P, T, G); P.emit(); P.finish(); return nc
    if stop_after == 'exp1':
        phase_G(P, T, G); P.emit(); P.finish(); return nc
    phase_G(P, T, G)
    P.emit()
    phase_H(P, T, G)
    P.emit()
    P.finish()
    return nc


def host_inputs(inputs, b):
    m = {}
    f = lambda a: np.ascontiguousarray(np.asarray(a, dtype=np.float32))
    m['x'] = f(inputs['x'][b])
    m['c_col'] = f(np.asarray(inputs['c'][b]).reshape(8, 128).T)
    for name, shp in INPUT_SHAPES:
        if name in ('x', 'c_col', 'k_ident', 'k_rel', 'k_masks', 'k_reset', 'k_bst'):
            continue
        a = np.asarray(inputs[name])
        m[name] = f(a.reshape(shp))
    m['k_ident'] = np.eye(128, dtype=np.float32)
    ii = np.arange(128)
    su = (ii[:, None] < ii[None, :]).astype(np.float32)
    iu = (ii[:, None] <= ii[None, :]).astype(np.float32)
    sl = (ii[:, None] > ii[None, :]).astype(np.float32)
    m['k_masks'] = np.ascontiguousarray(np.concatenate([su, iu, su, iu, sl, np.zeros((128, 256), np.float32)], axis=1))
    rs_ = np.ones((64, 512), np.float32); rs_[:, ::128] = 0.0
    m['k_reset'] = rs_
    m['k_bst'] = (np.arange(NBLK, dtype=np.float32) * 128.0)
    m['k_rel'] = (np.arange(256, dtype=np.float32)[None, :] - np.arange(128, dtype=np.float32)[:, None])
    return m


def kernel(**inputs):
    nc = build()
    in_maps = [host_inputs(inputs, b) for b in range(8)]
    res = run_bass_kernel_spmd(nc, in_maps, core_ids=list(range(8)))
    return np.stack([np.asarray(r['out']) for r in res.results], axis=0).astype(np.float32)
```

```python
import contextlib
import numpy as np
import concourse.bass as bass
import concourse.mybir as mybir

F32 = mybir.dt.float32
BF16 = mybir.dt.bfloat16
I32 = mybir.dt.int32
U32 = mybir.dt.uint32
AF = mybir.ActivationFunctionType
ALU = mybir.AluOpType
AX = mybir.AxisListType

N_DMA_SEMS = 8


class Prog:
    ENGS = ('pe', 'act', 'dve', 'pool', 'sp')

    def __init__(self, nc):
        self.nc = nc
        self.ops = {e: [] for e in self.ENGS}
        self.cnt = {e: 0 for e in self.ENGS}
        self.waited = {e: {} for e in self.ENGS}
        self.res = {}
        self.dma_tot = [0] * N_DMA_SEMS
        self.dma_rr = 0
        self.stack = contextlib.ExitStack()
        self.gstack = contextlib.ExitStack()
        self.nsb = 0
        self.nphase = 0
        self.sems = None

    def _new_sems(self):
        nc = self.nc
        self.sems = {}
        for e in self.ENGS:
            self.sems[e] = self.gstack.enter_context(nc.semaphore(f"s_{e}_{self.nphase}"))
        for i in range(N_DMA_SEMS):
            self.sems[('dma', i)] = self.gstack.enter_context(nc.semaphore(f"s_dma{i}_{self.nphase}"))
        self.cnt = {e: 0 for e in self.ENGS}
        self.waited = {e: {} for e in self.ENGS}
        self.dma_tot = [0] * N_DMA_SEMS
        self.dma_rr = 0

    def gsb(self, shape, dt, name=None):
        self.nsb += 1
        return self.gstack.enter_context(self.nc.sbuf_tensor(name or f"gsb{self.nsb}", list(shape), dt))

    def sb(self, shape, dt, name=None):
        self.nsb += 1
        return self.stack.enter_context(self.nc.sbuf_tensor(name or f"sb{self.nsb}", list(shape), dt))

    def ps(self, shape, dt, name=None):
        self.nsb += 1
        return self.stack.enter_context(self.nc.psum_tensor(name or f"ps{self.nsb}", list(shape), dt))

    def _deps(self, reads, writes):
        deps = {}
        def add(d):
            if d is None:
                return
            k, v = d
            if deps.get(k, 0) < v:
                deps[k] = v
        for k in reads:
            r = self.res.get(k)
            if r:
                add(r['w'])
        for k in writes:
            r = self.res.get(k)
            if r:
                add(r['w'])
                for d in r['r']:
                    add(d)
        return deps

    def _emit_waits(self, eng, deps):
        w = self.waited[eng]
        for k, v in deps.items():
            if w.get(k, 0) < v:
                w[k] = v
                self.ops[eng].append(('wait', k, v))

    def _update(self, dep, reads, writes):
        for k in reads:
            r = self.res.setdefault(k, {'w': None, 'r': []})
            r['r'] = [d for d in r['r'] if d[0] != dep[0]] + [dep]
        for k in writes:
            self.res[k] = {'w': dep, 'r': []}

    def op(self, eng, fn, reads=(), writes=()):
        if self.sems is None:
            self._new_sems()
        deps = self._deps(reads, writes)
        if eng == 'pe':
            deps.pop('pe', None)
        self._emit_waits(eng, deps)
        self.cnt[eng] += 1
        dep = (eng, self.cnt[eng])
        self.ops[eng].append(('op', fn))
        self._update(dep, reads, writes)
        return dep

    def dma(self, q, fn, reads=(), writes=()):
        if self.sems is None:
            self._new_sems()
        deps = self._deps(reads, writes)
        s = self.dma_rr
        self.dma_rr = (self.dma_rr + 1) % N_DMA_SEMS
        key = ('dma', s)
        if self.dma_tot[s] > 0:
            if deps.get(key, 0) < self.dma_tot[s]:
                deps[key] = self.dma_tot[s]
        self._emit_waits(q, deps)
        self.dma_tot[s] += 16
        dep = (key, self.dma_tot[s])
        self.ops[q].append(('dma', fn, s))
        self._update(dep, reads, writes)
        return dep

    def emit(self, keep=False):
        nc = self.nc
        with contextlib.ExitStack() as st:
            sems = self.sems
            self.nphase += 1
            block = st.enter_context(nc.Block(f"ph{self.nphase}"))
            engobj = {'pe': nc.tensor, 'act': nc.scalar, 'dve': nc.vector, 'pool': nc.gpsimd, 'sp': nc.sync}
            fin = {}
            for e in self.ENGS:
                if e != 'sp' and self.cnt[e] > 0:
                    fin[e] = self.cnt[e]
            for i in range(N_DMA_SEMS):
                if self.dma_tot[i] > 0:
                    fin[('dma', i)] = self.dma_tot[i]
            self._emit_waits('sp', fin)

            def run(e):
                eo = engobj[e]
                for item in self.ops[e]:
                    if item[0] == 'wait':
                        eo.wait_ge(sems[item[1]], item[2])
                    elif item[0] == 'op':
                        item[1](eo).then_inc(sems[e], 1)
                    else:
                        item[1](eo).then_inc(sems[('dma', item[2])], 16)

            @block.tensor
            def _(t):
                run('pe')

            @block.scalar
            def _(t):
                run('act')

            @block.vector
            def _(t):
                run('dve')

            @block.gpsimd
            def _(t):
                run('pool')

            @block.sync
            def _(t):
                run('sp')
        self.ops = {e: [] for e in self.ENGS}
        self.res = {}
        if keep:
            return
        self.stack.close()
        self.stack = contextlib.ExitStack()
        self.sems = None

    def finish(self):
        self.gstack.close()

from concourse.bass_utils import run_bass_kernel_spmd
import ml_dtypes

SPARSE = True
S = 4096
D = 1024
NT = S // 128
NIN = 5936


class Rot:
    def __init__(self, P, n, shape, dt, name, psum=False):
        self.tiles = [(P.ps(shape, dt) if psum else P.sb(shape, dt)) for _ in range(n)]
        self.name = name
        self.i = 0

    def next(self):
        t = self.tiles[self.i % len(self.tiles)]
        k = f"{self.name}{self.i % len(self.tiles)}"
        self.i += 1
        return t, k


class Rot2(Rot):
    def __init__(self, P, n, shape, dt, name):
        self.tiles = [(P.sb(shape, dt), P.sb(shape, dt)) for _ in range(n)]
        self.name = name
        self.i = 0


def mm(P, out, lhsT, rhs, start, stop, reads, writes):
    P.op('pe', lambda e: e.matmul(out, lhsT=lhsT, rhs=rhs, start=start, stop=stop), reads=reads, writes=writes)


def phase_A(P, T, G):
    mod_bc = P.sb([128, 6 * D], F32)
    ccol = P.sb([128, 8], F32)
    scol = P.sb([128, 8], F32)
    adab = P.sb([1, 6144], F32)
    modrow = P.sb([1, 6144], F32)
    ngb = P.sb([128, 1024], F32)
    P.dma('sp', lambda e: e.dma_start(out=ccol[:], in_=T['c_col']), writes=['ccol'])
    P.dma('sp', lambda e: e.dma_start(out=adab[:], in_=T['ada_b']), writes=['adab'])
    P.op('act', lambda e: e.activation(out=scol[:], in_=ccol[:], func=AF.Silu), reads=['ccol'], writes=['scol'])
    wrot = Rot(P, 2, [128, 8, 512], F32, 'aw')
    psr = Rot(P, 2, [1, 512], F32, 'psr', psum=True)
    psb = Rot(P, 2, [128, 512], F32, 'psb', psum=True)
    adaw = T['ada_w'].rearrange("(k p) n -> p k n", p=128)
    for n in range(12):
        wb, wk = wrot.next()
        P.dma('sp' if n % 2 == 0 else 'act',
              lambda e, wb=wb, n=n: e.dma_start(out=wb[:], in_=adaw[:, :, n * 512:(n + 1) * 512]), writes=[wk])
        pr, pk = psr.next()
        for k in range(8):
            mm(P, pr[:], scol[:, k:k + 1], wb[:, k, :], k == 0, k == 7, [wk, 'scol'], [pk])
        sl = slice(n * 512, (n + 1) * 512)
        P.op('dve', lambda e, pr=pr, sl=sl: e.tensor_tensor(out=modrow[0:1, sl], in0=pr[:], in1=adab[0:1, sl], op=ALU.add),
             reads=[pk, 'adab'], writes=[f'modrow{n}'])
        pb, pbk = psb.next()
        mm(P, pb[:], G['ones_row'][:], modrow[0:1, sl], True, True, [f'modrow{n}'], [pbk])
        P.op('act', lambda e, pb=pb, sl=sl: e.activation(out=mod_bc[:, sl], in_=pb[:], func=AF.Copy),
             reads=[pbk], writes=[f'mod{n}'])
    for (gname, c0, deps) in (('norm1_g', 1024, ['mod2', 'mod3']), ('norm2_g', 4096, ['mod8', 'mod9'])):
        P.dma('sp', lambda e, gname=gname: e.dma_start(out=ngb[:], in_=T[gname].partition_broadcast(128)), writes=['ngb'])
        P.op('dve', lambda e, c0=c0: e.scalar_tensor_tensor(out=mod_bc[:, c0:c0 + 1024], in0=mod_bc[:, c0:c0 + 1024],
                                                            scalar=1.0, in1=ngb[:], op0=ALU.add, op1=ALU.mult),
             reads=deps + ['ngb'], writes=deps)
    P.dma('sp', lambda e: e.dma_start(out=T['modd'], in_=mod_bc[:]), reads=[f'mod{n}' for n in range(12)])
    if G.get('sparse'):
        zt = P.sb([128, D], BF16)
        P.op('pool', lambda e: e.memset(zt[:], 0.0), writes=['zt'])
        for bz in range(NBLK):
            P.dma('act' if bz % 2 else 'sp', lambda e, bz=bz: e.dma_start(out=T['Xs'][bz * 128:(bz + 1) * 128, :], in_=zt[:]), reads=['zt'])


def rms_rstd(P, xt, xk, junk, ss, rs, tag):
    P.op('act', lambda e: e.activation(out=junk[:], in_=xt[:], func=AF.Square, accum_out=ss[:]),
         reads=[xk], writes=['junk' + tag, 'ss' + tag])
    P.op('act', lambda e: e.activation(out=ss[:], in_=ss[:], func=AF.Sqrt, scale=1.0 / D, bias=G_EPS[0][:, 0:1]),
         reads=['ss' + tag], writes=['ss' + tag])
    P.op('dve', lambda e: e.reciprocal(out=rs[:], in_=ss[:]), reads=['ss' + tag], writes=['rs' + tag])


G_EPS = [None]


def norm_mod_transpose(P, G, xt, xk, hT, i, g_sl, sh_sl, W):
    mod_bc = W['mod']
    junk, ss, rs, t1, hb, pt = W['junk'], W['ss'], W['rs'], W['t1'], W['hb'], W['pt']
    rms_rstd(P, xt, xk, junk, ss, rs, '')
    P.op('dve', lambda e: e.scalar_tensor_tensor(out=t1[:], in0=xt[:], scalar=rs[:, 0:1], in1=mod_bc[:, g_sl],
                                                 op0=ALU.mult, op1=ALU.mult), reads=[xk, 'rs', 'modl'], writes=['t1'])
    P.op('pool', lambda e: e.tensor_tensor(out=hb[:], in0=t1[:], in1=mod_bc[:, sh_sl], op=ALU.add),
         reads=['t1', 'modl'], writes=['hb'])
    for k in range(8):
        P.op('pe', lambda e, k=k: e.transpose(out=pt[:, k, :], in_=hb[:, k * 128:(k + 1) * 128], identity=G['identb'][:]),
             reads=['hb'], writes=['pt'])
    P.op('act', lambda e: e.activation(out=hT[:, :, i * 128:(i + 1) * 128], in_=pt[:], func=AF.Copy),
         reads=['pt'], writes=[f'hT{i // 4}'])


def phase_BC(P, T, G):
    hT = P.sb([128, 8, S], BF16)
    W = dict(junk=P.sb([128, D], F32), ss=P.sb([128, 1], F32), rs=P.sb([128, 1], F32), t1=P.sb([128, D], F32),
             hb=P.sb([128, D], BF16), pt=P.ps([128, 8, 128], BF16))
    W['mod'] = P.sb([128, 2048], F32)
    P.dma('sp', lambda e: e.dma_start(out=W['mod'][:], in_=T['modd'][:, 0:2048]), writes=['modl'])
    xrot = Rot(P, 2, [128, D], F32, 'x')
    s0 = iter(())

    def s0step(n=1):
        for _ in range(n):
            try:
                next(s0)
            except StopIteration:
                return
    for i in range(NT):
        xt, xk = xrot.next()
        P.dma('sp', lambda e, xt=xt, i=i: e.dma_start(out=xt[:], in_=T['x'][i * 128:(i + 1) * 128, :]), writes=[xk])
        norm_mod_transpose(P, G, xt, xk, hT, i, slice(1024, 2048), slice(0, 1024), W)
        s0step()
    win = T['w_in'].rearrange("(k p) n -> p k n", p=128)
    segs = [('zq', 0, 512, BF16), ('zk', 512, 512, BF16), ('ziq', 1536, 512, BF16), ('zik', 2048, 32, BF16),
            ('zr', 2096, 1792, F32), ('zga', 3888, 1024, BF16), ('zgr', 4912, 1024, BF16)]
    wrot = Rot(P, 2, [128, 8, 128], BF16, 'w')
    psrot = Rot(P, 3, [128, 512], F32, 'ps', psum=True)
    strot = {BF16: Rot(P, 3, [128, 512], BF16, 'stb'), F32: Rot(P, 3, [128, 512], F32, 'stf')}
    ev = 0
    for (name, c0, n, dt) in segs:
        for m0 in range(0, n, 128):
            M = min(128, n - m0)
            wt, wk = wrot.next()
            P.dma('pool', lambda e, wt=wt, M=M, a=c0 + m0: e.dma_start(out=wt[:, :, :M], in_=win[:, :, a:a + M]), writes=[wk])
            for tg in range(8):
                ps, pk = psrot.next()
                for k in range(8):
                    mm(P, ps[:M, :], wt[:, k, :M], hT[:, k, tg * 512:(tg + 1) * 512], k == 0, k == 7, [wk, f'hT{tg}'], [pk])
                st, sk = strot[dt].next()
                if ev % 2 == 0:
                    P.op('act', lambda e, st=st, ps=ps, M=M: e.activation(out=st[:M, :], in_=ps[:M, :], func=AF.Copy),
                         reads=[pk], writes=[sk])
                else:
                    P.op('dve', lambda e, st=st, ps=ps, M=M: e.tensor_copy(out=st[:M, :], in_=ps[:M, :]),
                         reads=[pk], writes=[sk])
                ev += 1
                if ev % 2 == 0:
                    s0step()
                P.dma('sp', lambda e, st=st, M=M, name=name, m0=m0, tg=tg:
                      e.dma_start(out=T[name][m0:m0 + M, tg * 512:(tg + 1) * 512], in_=st[:M, :]), reads=[sk])
    wv = P.sb([128, 8, 528], BF16)
    P.dma('pool', lambda e: e.dma_start(out=wv[:, :, 0:512], in_=win[:, :, 1024:1536]), writes=['wv'])
    P.dma('pool', lambda e: e.dma_start(out=wv[:, :, 512:528], in_=win[:, :, 2080:2096]), writes=['wv2'])
    ps2rot = Rot(P, 2, [128, 16], F32, 'ps2', psum=True)
    st2rot = Rot(P, 2, [128, 16], F32, 'st2')
    for i in range(NT):
        ps, pk = psrot.next()
        ps2, pk2 = ps2rot.next()
        for k in range(8):
            mm(P, ps[:], hT[:, k, i * 128:(i + 1) * 128], wv[:, k, 0:512], k == 0, k == 7, ['wv', f'hT{i // 4}'], [pk])
        for k in range(8):
            mm(P, ps2[:], hT[:, k, i * 128:(i + 1) * 128], wv[:, k, 512:528], k == 0, k == 7, ['wv2', f'hT{i // 4}'], [pk2])
        st, sk = strot[BF16].next()
        st2, sk2 = st2rot.next()
        P.op('act', lambda e, st=st, ps=ps: e.activation(out=st[:], in_=ps[:], func=AF.Copy), reads=[pk], writes=[sk])
        P.op('dve', lambda e, st2=st2, ps2=ps2: e.tensor_copy(out=st2[:], in_=ps2[:]), reads=[pk2], writes=[sk2])
        P.dma('sp', lambda e, st=st, i=i: e.dma_start(out=T['zv'][i * 128:(i + 1) * 128, :], in_=st[:]), reads=[sk])
        P.dma('sp', lambda e, st2=st2, i=i: e.dma_start(out=T['ziw'][i * 128:(i + 1) * 128, :], in_=st2[:]), reads=[sk2])
    s0step(1000)


def t5_lo_bounds():
    n = np.arange(256)
    nf = np.maximum(n, 1).astype(np.float32)
    large = 16 + (np.log(nf / np.float32(16)) / np.float32(np.log(8.0)) * np.float32(16)).astype(np.int32)
    large = np.minimum(large, 31)
    bk = np.where(n < 16, n, large)
    return [int(np.min(np.nonzero(bk >= b)[0])) for b in range(1, 32)]


def phase_D0(P, T, G):
    relb = P.sb([128, 256], F32)
    diff = P.sb([128, 248], F32)
    base = P.sb([128, 8], F32)
    relidx = P.sb([128, 256], F32)
    P.dma('sp', lambda e: e.dma_start(out=relb[:], in_=T['rel_bias'].partition_broadcast(128)), writes=['relb'])
    P.dma('sp', lambda e: e.dma_start(out=relidx[:], in_=T['k_rel']), writes=['relidx'])
    P.op('dve', lambda e: e.tensor_tensor(out=diff[:], in0=relb[:, 8:256], in1=relb[:, 0:248], op=ALU.subtract),
         reads=['relb'], writes=['diff'])
    P.op('dve', lambda e: e.tensor_tensor(out=base[:], in0=relb[:, 0:8], in1=relb[:, 248:256], op=ALU.subtract),
         reads=['relb'], writes=['base'])
    P.op('dve', lambda e: e.tensor_copy(out=G['b31'][:], in_=relb[:, 248:256]), reads=['relb'], writes=['b31'])
    E = G['E']
    irot = Rot(P, 2, [128, 256], F32, 'ind')
    los = t5_lo_bounds()
    for b in range(1, 32):
        ind, ik = irot.next()
        P.op('dve', lambda e, ind=ind, lo=float(los[b - 1]): e.tensor_scalar(out=ind[:], in0=relidx[:], scalar1=lo, scalar2=None,
                                                                              op0=ALU.is_ge), reads=['relidx'], writes=[ik])
        for h in range(8):
            if b == 1:
                P.op('dve', lambda e, ind=ind, h=h: e.tensor_scalar(out=E[:, h, :], in0=ind[:], scalar1=diff[:, h:h + 1],
                                                                    scalar2=base[:, h:h + 1], op0=ALU.mult, op1=ALU.add),
                     reads=[ik, 'diff', 'base'], writes=[f'E{h}'])
            else:
                c = (b - 1) * 8 + h
                P.op('dve', lambda e, ind=ind, h=h, c=c: e.scalar_tensor_tensor(out=E[:, h, :], in0=ind[:], scalar=diff[:, c:c + 1],
                                                                               in1=E[:, h, :], op0=ALU.mult, op1=ALU.add),
                     reads=[ik, 'diff', f'E{h}'], writes=[f'E{h}'])
    P.op('act', lambda e: e.activation(out=E[:], in_=E[:], func=AF.Exp), reads=[f'E{h}' for h in range(8)],
         writes=[f'E{h}' for h in range(8)])


def phase_D(P, T, G):
    NIT = 14
    KT = P.sb([64, 8, S], BF16)
    V = P.sb([128, NT, 512], BF16)
    ik4 = P.sb([128, S], BF16)
    iw = P.sb([128, NT, 16], F32)
    ones64 = P.sb([128, 64], BF16)
    scs = [P.sb([128, S], F32), P.sb([128, S], F32)]
    maskbs = [P.sb([128, S], BF16), P.sb([128, S], BF16)]
    maskT = P.sb([128, NT, 128], BF16)
    lo, hi, mid, cnt, tmp, thr = [P.sb([128, 1], F32) for _ in range(6)]
    cvec = P.sb([128, NIT + 1], F32)
    dk = P.sb([128, NIT + 1], F32)
    for k in range(NIT + 1):
        P.op('pool', lambda e, k=k: e.memset(cvec[:, k:k + 1], 2.0 ** -(k + 1)), reads=['cvec'], writes=['cvec'])
    P.dma('sp', lambda e: e.dma_start(out=KT[:], in_=T['zk'].rearrange("(h p) t -> p h t", p=64)), writes=['KT'])
    P.dma('act', lambda e: e.dma_start(out=V[:], in_=T['zv'].rearrange("(i p) f -> p i f", p=128)), writes=['V'])
    for i in range(3):
        P.dma('sp', lambda e, i=i: e.dma_start(out=ik4[32 * i:32 * i + 32, :], in_=T['zik']), writes=[f'ik4{i}'])
    P.dma('sp', lambda e: e.dma_start(out=iw[:], in_=T['ziw'].rearrange("(i p) f -> p i f", p=128)), writes=['iw'])
    P.op('dve', lambda e: e.memset(ones64[:], 1.0), writes=['ones64'])
    zq = T['zq'].rearrange("(h p) t -> p h t", p=64)
    ziq = T['ziq'][0:480, :].rearrange("(j p) t -> p j t", p=96)
    attnT = T['attnT'].rearrange("(h p) t -> p h t", p=64)
    qrot = Rot(P, 2, [64, 8, 128], BF16, 'q')
    iqrot = Rot(P, 2, [96, 6, 128], BF16, 'iq')
    dgrot = Rot(P, 2, [128, 16, 128], BF16, 'dg')
    rrot = Rot(P, 3, [128, 512], BF16, 'r')
    psi = Rot(P, 2, [128, 512], F32, 'psi', psum=True)
    pacc = Rot(P, 1, [128, 512], F32, 'pacc', psum=True)
    pss = Rot(P, 3, [128, 4, 128], F32, 'pss', psum=True)
    ptm = Rot(P, 1, [128, 4, 128], BF16, 'ptm', psum=True)
    pod = Rot(P, 1, [64, 512], F32, 'pod', psum=True)
    pTrot = Rot(P, 3, [128, 4, 128], BF16, 'pT')
    atrot = Rot(P, 2, [64, 8, 128], BF16, 'at')
    rdrot = Rot(P, 2, [64, 128], F32, 'rd')
    E, b31 = G['E'], G['b31']
    identb = G['identb']

    def indexer(qi):
        sc, sck = scs[qi % 2], f'sc{qi % 2}'
        n = 128 * (qi + 1)
        tsl = slice(qi * 128, (qi + 1) * 128)
        iqt, iqk = iqrot.next()
        P.dma('act', lambda e: e.dma_start(out=iqt[:, 0:5, :], in_=ziq[:, :, tsl]), writes=[iqk])
        P.dma('act', lambda e: e.dma_start(out=iqt[0:32, 5, :], in_=T['ziq'][480:512, tsl]), writes=[iqk + 'b'])
        dg, dgk = dgrot.next()
        for h in range(16):
            P.op('pool', lambda e, h=h: e.tensor_scalar(out=dg[:, h, :], in0=identb[:], scalar1=iw[:, qi, h:h + 1], scalar2=0.0, op0=ALU.mult, op1=ALU.add),
                 reads=['iw'], writes=[dgk])
        for ch in range((n + 511) // 512):
            c0 = ch * 512
            nc_ = min(512, n - c0)
            pa, pak = pacc.next()
            pend = []

            def acc(h, r, rk):
                mm(P, pa[:, :nc_], dg[:, h, :], r[:, :nc_], h == 0, h == 15, [dgk, rk], [pak])
            for h in range(16):
                j, i = divmod(h, 3)
                ps, pk = psi.next()
                mm(P, ps[:, :nc_], iqt[32 * i:32 * i + 32, j, :], ik4[32 * i:32 * i + 32, c0:c0 + nc_], True, True,
                   [iqk, iqk + 'b', f'ik4{i}'], [pk])
                r, rk = rrot.next()
                P.op('act', lambda e, r=r, ps=ps, nc_=nc_: e.activation(out=r[:, :nc_], in_=ps[:, :nc_], func=AF.Relu), reads=[pk], writes=[rk])
                pend.append((h, r, rk))
                if len(pend) > 1:
                    acc(*pend.pop(0))
                yield
            while pend:
                acc(*pend.pop(0))
            P.op('dve', lambda e, pa=pa, c0=c0, nc_=nc_: e.tensor_copy(out=sc[:, c0:c0 + nc_], in_=pa[:, :nc_]), reads=[pak], writes=[sck])
        P.op('pool', lambda e: e.affine_select(out=sc[:, tsl], in_=sc[:, tsl], pattern=[[-1, 128]], compare_op=ALU.is_ge, fill=-1e30,
                                               base=0, channel_multiplier=1), reads=[sck], writes=[sck])

    def threshold(qi):
        sc, sck = scs[qi % 2], f'sc{qi % 2}'
        n = 128 * (qi + 1)
        maskb, mbk = maskbs[qi % 2], f'maskb{qi % 2}'
        dve = lambda fn, r, w: P.op('dve', fn, reads=r, writes=w)
        if n <= 256:
            dve(lambda e: e.memset(thr[:], -1e29), ['thr'], ['thr'])
        else:
            nv = 128 * qi
            dve(lambda e: e.tensor_reduce(out=lo[:], in_=sc[:, :nv], axis=AX.X, op=ALU.min), [sck, 'lo'], ['lo'])
            dve(lambda e: e.tensor_reduce(out=hi[:], in_=sc[:, :n], axis=AX.X, op=ALU.max), [sck, 'hi'], ['hi'])
            dve(lambda e: e.tensor_tensor(out=hi[:], in0=hi[:], in1=lo[:], op=ALU.subtract), ['hi', 'lo'], ['hi'])
            dve(lambda e: e.tensor_scalar(out=dk[:], in0=cvec[:], scalar1=hi[:, 0:1], scalar2=None, op0=ALU.mult), ['cvec', 'hi', 'dk'], ['dk'])
            dve(lambda e: e.tensor_tensor(out=mid[:], in0=lo[:], in1=dk[:, 0:1], op=ALU.add), ['lo', 'dk', 'mid'], ['mid'])
            for k in range(NIT):
                dve(lambda e: e.tensor_scalar(out=maskb[:, :n], in0=sc[:, :n], scalar1=mid[:, 0:1], scalar2=None,
                                              op0=ALU.is_ge, op1=ALU.add, accum_out=cnt[:]), [sck, 'mid', 'cnt', mbk], [mbk, 'cnt'])
                dve(lambda e: e.tensor_scalar(out=tmp[:], in0=cnt[:], scalar1=255.5, scalar2=-0.5, op0=ALU.is_ge, op1=ALU.add),
                    ['cnt', 'tmp'], ['tmp'])
                dve(lambda e, k=k: e.scalar_tensor_tensor(out=mid[:], in0=tmp[:], scalar=dk[:, k:k + 1], in1=mid[:], op0=ALU.mult, op1=ALU.add),
                    ['tmp', 'dk', 'mid'], ['mid'])
                yield
            dve(lambda e: e.tensor_tensor(out=thr[:], in0=mid[:], in1=dk[:, NIT:NIT + 1], op=ALU.subtract), ['mid', 'dk', 'thr'], ['thr'])
        dve(lambda e: e.tensor_scalar(out=maskb[:, :n], in0=sc[:, :n], scalar1=thr[:, 0:1], scalar2=None, op0=ALU.is_ge),
            [sck, 'thr', mbk], [mbk])
        yield

    def attention(qi):
        LOOK = 2
        nkt = qi + 1
        nch = (nkt + 3) // 4
        tsl = slice(qi * 128, (qi + 1) * 128)
        mb = maskbs[qi % 2]
        mbk = f'maskb{qi % 2}'
        qt, qk = qrot.next()
        P.dma('sp', lambda e: e.dma_start(out=qt[:], in_=zq[:, :, tsl]), writes=[qk])
        for c4 in range(nch):
            kts = list(range(4 * c4, min(4 * c4 + 4, nkt)))
            pm, pmk = ptm.next()
            for kt in kts:
                P.op('pe', lambda e, pm=pm, kt=kt: e.transpose(out=pm[:, kt % 4, :], in_=mb[:, kt * 128:(kt + 1) * 128],
                                                                identity=identb[:]), reads=[mbk], writes=[pmk])
            P.op('act', lambda e, pm=pm, kts=kts: e.activation(out=maskT[:, kts[0]:kts[-1] + 1, :], in_=pm[:, :len(kts), :],
                                                               func=AF.Copy), reads=[pmk], writes=['maskT'])
        at, atk = atrot.next()
        items = [(h, c4) for h in range(8) for c4 in range(nch)]
        qkd = {}
        hst = {}

        def emit_qk(h, c4):
            kts = list(range(4 * c4, min(4 * c4 + 4, nkt)))
            ps, pk = pss.next()
            for kt in kts:
                mm(P, ps[:, kt % 4, :], KT[:, h, kt * 128:(kt + 1) * 128], qt[:, h, :], True, True, ['KT', qk], [pk])
            qkd[(h, c4)] = (ps, pk)

        def emit_rest(h, c4):
            kts = list(range(4 * c4, min(4 * c4 + 4, nkt)))
            nk = len(kts)
            ps, pk = qkd.pop((h, c4))
            if c4 == 0:
                hst[h] = pod.next()
            po, pok = hst[h]
            pT, pTk = pTrot.next()
            P.op('act', lambda e: e.activation(out=pT[:, :nk, :], in_=ps[:, :nk, :], func=AF.Exp, scale=0.125, bias=b31[:, h:h + 1]),
                 reads=[pk], writes=[pTk])
            P.op('dve', lambda e: e.tensor_tensor(out=pT[:, :nk, :], in0=pT[:, :nk, :], in1=maskT[:, kts[0]:kts[-1] + 1, :], op=ALU.mult),
                 reads=[pTk, 'maskT'], writes=[pTk])
            for kt in kts:
                dl = qi - kt
                if dl <= 1:
                    P.op('dve', lambda e, kt=kt, dl=dl: e.tensor_tensor(
                        out=pT[:, kt % 4, :], in0=pT[:, kt % 4, :], in1=E[:, h, dl * 128:(dl + 1) * 128], op=ALU.mult),
                        reads=[pTk], writes=[pTk])
            for kt in kts:
                P.op('pe', lambda e, kt=kt: e.matmul(po[:, 0:128], lhsT=V[:, kt, h * 64:(h + 1) * 64], rhs=pT[:, kt % 4, :],
                                                     start=(kt == 0), stop=(kt == nkt - 1), skip_group_check=True),
                     reads=['V', pTk], writes=[pok])
                P.op('pe', lambda e, kt=kt: e.matmul(po[:, 128:256], lhsT=ones64[:], rhs=pT[:, kt % 4, :],
                                                     start=False, stop=(kt == nkt - 1), skip_group_check=True),
                     reads=['ones64', pTk], writes=[pok])
            if c4 == nch - 1:
                rd, rdk = rdrot.next()
                P.op('dve', lambda e: e.reciprocal(out=rd[:], in_=po[:, 128:256]), reads=[pok], writes=[rdk])
                P.op('dve', lambda e: e.tensor_tensor(out=at[:, h, :], in0=po[:, 0:128], in1=rd[:], op=ALU.mult),
                     reads=[pok, rdk], writes=[atk])

        for idx in range(len(items) + LOOK):
            if idx < len(items):
                emit_qk(*items[idx])
            if idx >= LOOK:
                emit_rest(*items[idx - LOOK])
                yield
        P.dma('sp', lambda e: e.dma_start(out=attnT[:, :, tsl], in_=at[:]), reads=[atk])

    def drain(g):
        for _ in g:
            pass

    def merge(gens):
        gens = [[g, max(1, n), 0.0, True] for g, n in gens]
        total = max(n for _, n, _, _ in gens)
        for step in range(total + 1):
            for it in gens:
                it[2] += it[1] / total
                while it[3] and it[2] >= 1.0:
                    it[2] -= 1.0
                    try:
                        next(it[0])
                    except StopIteration:
                        it[3] = False
        for it in gens:
            if it[3]:
                drain(it[0])

    s0 = phase_S0(P, T, G) if G.get('sparse') else iter(())

    def s0gen(n):
        for _ in range(n):
            try:
                next(s0)
            except StopIteration:
                return
            yield
    drain(indexer(0))
    drain(threshold(0))
    for qi in range(NT):
        ns0 = -(-390 * (qi + 1) // 528)
        if qi + 1 < NT:
            drain(indexer(qi + 1))
            merge([(attention(qi), 8 * ((qi + 4) // 4)), (threshold(qi + 1), NIT + 1), (s0gen(ns0), ns0)])
        else:
            merge([(attention(qi), 8 * ((qi + 4) // 4)), (s0gen(1000), 100)])
    drain(s0gen(1000))


def phase_E(P, T, G):
    LD = 0.6065306597126334
    ident = G['identb']
    zr = T['zr']
    def colload(name, n, key):
        t = P.sb([64, n], F32)
        P.dma('sp', lambda e: e.dma_start(out=t[:], in_=T[name].rearrange("(h p) -> p h", p=64), allow_slow_non_contiguous=True), writes=[key])
        return t
    mu_rkv = P.sb([64, 24], F32)
    P.dma('sp', lambda e: e.dma_start(out=mu_rkv[:], in_=T['tshift_mu'][0:1536].rearrange("(h p) -> p h", p=64), allow_slow_non_contiguous=True), writes=['mu'])
    mu_wa = P.sb([64, 2], F32)
    P.dma('sp', lambda e: e.dma_start(out=mu_wa[:], in_=T['tshift_mu'][1536:1664].rearrange("(h p) -> p h", p=64), allow_slow_non_contiguous=True), writes=['mu'])
    mu_g = P.sb([128, 1], F32)
    P.dma('sp', lambda e: e.dma_start(out=mu_g[:], in_=T['tshift_mu'][1664:1792].rearrange("(h p) -> p h", p=128), allow_slow_non_contiguous=True), writes=['mu'])
    om_rkv, om_wa, om_g = P.sb([64, 24], F32), P.sb([64, 2], F32), P.sb([128, 1], F32)
    for (o, m) in ((om_rkv, mu_rkv), (om_wa, mu_wa), (om_g, mu_g)):
        P.op('dve', lambda e, o=o, m=m: e.tensor_scalar(out=o[:], in0=m[:], scalar1=-1.0, scalar2=1.0, op0=ALU.mult, op1=ALU.add),
             reads=['mu'], writes=['om'])
    w0c = colload('decay_w0', 8, 'par'); a0c = colload('iclr_a0', 8, 'par'); kkc = colload('k_k', 8, 'par')
    kac = colload('k_a', 8, 'par'); rkc = colload('r_k', 8, 'par'); lgc = colload('lnx_g', 8, 'par'); lbc = colload('lnx_b', 8, 'par')
    omka = P.sb([64, 8], F32)
    P.op('dve', lambda e: e.tensor_scalar(out=omka[:], in0=kac[:], scalar1=-1.0, scalar2=1.0, op0=ALU.mult, op1=ALU.add),
         reads=['par'], writes=['omka'])
    dup, iup, gup = P.sb([64, 512], BF16), P.sb([64, 512], BF16), P.sb([128, 512], BF16)
    P.dma('pool', lambda e: e.dma_start(out=dup[:], in_=T['decay_up']), writes=['wts'])
    P.dma('pool', lambda e: e.dma_start(out=iup[:], in_=T['iclr_up']), writes=['wts'])
    P.dma('pool', lambda e: e.dma_start(out=gup[:], in_=T['gate_up']), writes=['wts'])
    onesf = P.sb([64, 64], F32)
    onesm = P.sb([64, 64], F32)
    gneps = P.sb([64, 1], F32)
    P.op('dve', lambda e: e.memset(onesf[:], 1.0), writes=['onesf'])
    P.op('dve', lambda e: e.memset(onesm[:], 1.0 / 64), writes=['onesm'])
    P.op('dve', lambda e: e.memset(gneps[:], 64e-5), writes=['gneps'])
    km = P.sb([128, 896], F32)
    P.dma('sp', lambda e: e.dma_start(out=km[:], in_=T['k_masks']), writes=['km'])
    mask4 = P.sb([128, 512], BF16)
    maskL = P.sb([128, 128], BF16)
    P.op('dve', lambda e: e.tensor_copy(out=mask4[:], in_=km[:, 0:512]), reads=['km'], writes=['mask4'])
    P.op('dve', lambda e: e.tensor_copy(out=maskL[:], in_=km[:, 512:640]), reads=['km'], writes=['maskL'])
    rst = P.sb([64, 512], F32)
    P.dma('sp', lambda e: e.dma_start(out=rst[:], in_=T['k_reset']), writes=['rst'])
    Tst = P.sb([64, 8, 64], BF16)
    P.op('dve', lambda e: e.memset(Tst[:], 0.0), writes=[f'T{h}' for h in range(8)])
    zrot = Rot(P, 1, [64, 3, 513], F32, 'z')
    wa_in = P.sb([64, 2, 513], F32)
    gd_in = P.sb([128, 513], F32)
    tmpr = Rot(P, 1, [128, 512], F32, 'tmp')
    twb, adb, sgb = P.sb([64, 512], BF16), P.sb([64, 512], BF16), P.sb([128, 512], BF16)
    AR = P.sb([64, 8, 4, 256], BF16)
    BK = P.sb([64, 8, 4, 256], BF16)
    tok3 = P.sb([128, 8, 4, 3, 64], BF16)
    pC = P.sb([64, 8, 4], F32)
    bon = P.sb([64, 8, 512], BF16)
    gg = P.sb([64, 8, 512], BF16)
    yT = P.sb([64, 8, 512], F32)
    RW = P.sb([64, 8, 512], BF16)
    hb = {n: P.sb([64, 512], F32) for n in ('sig', 'cs', 'ep', 'em', 'epv', 'kk', 'kkn', 'a', 't', 'kp', 'b', 'u1')}
    vb = P.sb([64, 512], BF16)
    Gms = [P.sb([128, 16, 512], BF16) for _ in range(2)]
    XY = [P.sb([128, 16, 256], BF16) for _ in range(2)]
    Nms = [P.sb([128, 16, 128], BF16) for _ in range(2)]
    Wsb, Usb = P.sb([128, 8, 64], BF16), P.sb([128, 8, 64], BF16)
    pg = Rot(P, 5, [128, 512], F32, 'pg', psum=True)
    pl = Rot(P, 2, [64, 512], F32, 'pl', psum=True)
    ptr = Rot(P, 1, [128, 3, 64], BF16, 'ptr', psum=True)
    rwT = T['rwT'].rearrange("(h p) t -> p h t", p=64)

    def dve(fn, reads, writes):
        P.op('dve', fn, reads=reads, writes=writes)

    for tg in range(8):
        t0 = tg * 512
        def load_halo(dst, rows, key, q, tg=tg, t0=t0):
            if tg == 0:
                src = rows(t0, t0 + 512)
                P.op('pool', lambda e: e.memset(dst[:, 0:1] if len(dst.shape) == 2 else dst[:, :, 0:1], 0.0), reads=[key], writes=[key])
                P.dma(q, lambda e: e.dma_start(out=(dst[:, 1:513] if len(dst.shape) == 2 else dst[:, :, 1:513]), in_=src), writes=[key + 'b'])
            else:
                src = rows(t0 - 1, t0 + 512)
                P.dma(q, lambda e: e.dma_start(out=dst[:], in_=src), reads=[key + 'b'], writes=[key])
        load_halo(wa_in, lambda a, b: zr[1536:1664, a:b].rearrange("(h p) t -> p h t", p=64), 'wa', 'sp')
        load_halo(gd_in, lambda a, b: zr[1664:1792, a:b], 'gd', 'act')

        def tshift(src_prev, src_cur, mu_ap, om_ap, np_, keys):
            tm, tk = tmpr.next()
            P.op('pool', lambda e: e.tensor_scalar(out=tm[:np_, :], in0=src_prev, scalar1=mu_ap, scalar2=0.0, op0=ALU.mult, op1=ALU.add),
                 reads=keys + ['mu'], writes=[tk])
            dve(lambda e: e.scalar_tensor_tensor(out=src_cur, in0=src_cur, scalar=om_ap, in1=tm[:np_, :], op0=ALU.mult, op1=ALU.add),
                keys + [tk, 'om'], keys)
        for i in range(2):
            tshift(wa_in[:, i, 0:512], wa_in[:, i, 1:513], mu_wa[:, i:i + 1], om_wa[:, i:i + 1], 64, ['wa', 'wab'])
        tshift(gd_in[:, 0:512], gd_in[:, 1:513], mu_g[:, 0:1], om_g[:, 0:1], 128, ['gd', 'gdb'])
        P.op('act', lambda e: e.activation(out=twb[:], in_=wa_in[:, 0, 1:513], func=AF.Tanh), reads=['wa', 'wab'], writes=['twb'])
        P.op('act', lambda e: e.activation(out=sgb[:], in_=gd_in[:, 1:513], func=AF.Sigmoid), reads=['gd', 'gdb'], writes=['sgb'])
        dve(lambda e: e.tensor_copy(out=adb[:], in_=wa_in[:, 1, 1:513]), ['wa', 'wab'], ['adb'])
        def prep_head(h, z, zk):
            def zrows(a, b, h=h):
                return zr[0:1536, a:b].rearrange("(s hh p) t -> hh p s t", s=3, p=64)[h]
            load_halo(z, zrows, zk, 'sp' if h % 2 == 0 else 'act')
            for s_ in range(3):
                tshift(z[:, s_, 0:512], z[:, s_, 1:513], mu_rkv[:, s_ * 8 + h:s_ * 8 + h + 1], om_rkv[:, s_ * 8 + h:s_ * 8 + h + 1], 64, [zk, zk + 'b'])
            r_, k_, v_ = z[:, 0, 1:513], z[:, 1, 1:513], z[:, 2, 1:513]
            zkeys = [zk, zk + 'b']
            sig, cs, ep, em, epv, kk, kkn, a_, t_, kp, b_, u1 = [hb[n] for n in ('sig', 'cs', 'ep', 'em', 'epv', 'kk', 'kkn', 'a', 't', 'kp', 'b', 'u1')]
            hs = slice(h * 64, (h + 1) * 64)
            p1, p1k = pl.next()
            mm(P, p1[:], dup[:, hs], twb[:], True, True, ['wts', 'twb'], [p1k])
            P.op('act', lambda e, p1=p1, h=h: e.activation(out=sig[:], in_=p1[:], func=AF.Sigmoid, bias=w0c[:, h:h + 1]),
                 reads=[p1k, 'par'], writes=['sig'])
            dve(lambda e: e.tensor_tensor_scan(out=cs[:], data0=rst[:], data1=sig[:], initial=0.0, op0=ALU.mult, op1=ALU.add),
                ['rst', 'sig'], ['cs'])
            P.op('act', lambda e: e.activation(out=ep[:], in_=cs[:], func=AF.Exp, scale=-LD), reads=['cs'], writes=['ep'])
            P.op('act', lambda e: e.activation(out=em[:], in_=cs[:], func=AF.Exp, scale=LD), reads=['cs'], writes=['em'])
            dve(lambda e: e.tensor_tensor(out=u1[:], in0=cs[:], in1=sig[:], op=ALU.subtract), ['cs', 'sig'], ['u1'])
            P.op('act', lambda e: e.activation(out=epv[:], in_=u1[:], func=AF.Exp, scale=-LD), reads=['u1'], writes=['epv'])
            dve(lambda e, h=h: e.tensor_copy(out=pC[:, h, :], in_=ep[:, 127:512:128]), ['ep'], ['pC'])
            p2, p2k = pl.next()
            mm(P, p2[:], iup[:, hs], adb[:], True, True, ['wts', 'adb'], [p2k])
            P.op('act', lambda e, p2=p2, h=h: e.activation(out=a_[:], in_=p2[:], func=AF.Sigmoid, bias=a0c[:, h:h + 1]),
                 reads=[p2k, 'par'], writes=['a'])
            p3, p3k = pl.next()
            mm(P, p3[:], gup[:, hs], sgb[:], True, True, ['wts', 'sgb'], [p3k])
            P.op('act', lambda e, p3=p3, h=h: e.activation(out=gg[:, h, :], in_=p3[:], func=AF.Copy), reads=[p3k], writes=[f'gg{h}'])
            dve(lambda e, h=h: e.tensor_scalar(out=kk[:], in0=k_, scalar1=kkc[:, h:h + 1], scalar2=None, op0=ALU.mult), zkeys + ['par'], ['kk'])
            P.op('act', lambda e: e.activation(out=u1[:], in_=kk[:], func=AF.Square), reads=['kk', 'u1'], writes=['u1'])
            p4, p4k = pl.next()
            mm(P, p4[:], onesf[:], u1[:], True, True, ['onesf', 'u1'], [p4k])
            P.op('act', lambda e, p4=p4: e.activation(out=kkn[:], in_=p4[:], func=AF.Sqrt), reads=[p4k], writes=['kkn'])
            dve(lambda e: e.tensor_scalar(out=kkn[:], in0=kkn[:], scalar1=1e-12, scalar2=None, op0=ALU.max), ['kkn'], ['kkn'])
            dve(lambda e: e.reciprocal(out=kkn[:], in_=kkn[:]), ['kkn'], ['kkn'])
            dve(lambda e: e.tensor_tensor(out=kkn[:], in0=kkn[:], in1=kk[:], op=ALU.mult), ['kkn', 'kk'], ['kkn'])
            dve(lambda e, h=h: e.tensor_scalar(out=t_[:], in0=a_[:], scalar1=kac[:, h:h + 1], scalar2=omka[:, h:h + 1], op0=ALU.mult, op1=ALU.add),
                ['a', 'par', 'omka'], ['t'])
            dve(lambda e: e.tensor_tensor(out=kp[:], in0=t_[:], in1=k_, op=ALU.mult), ['t'] + zkeys, ['kp'])
            dve(lambda e: e.tensor_tensor(out=b_[:], in0=kkn[:], in1=a_[:], op=ALU.mult), ['kkn', 'a'], ['b'])
            c4 = lambda ap: ap.rearrange("p (c t) -> p c t", c=4)
            dve(lambda e, h=h: e.tensor_tensor(out=AR[:, h, :, 128:256], in0=c4(r_), in1=c4(ep[:]), op=ALU.mult), zkeys + ['ep'], [f'AR{h}'])
            dve(lambda e, h=h: e.scalar_tensor_tensor(out=AR[:, h, :, 0:128], in0=c4(kkn[:]), scalar=-1.0, in1=c4(epv[:]), op0=ALU.mult, op1=ALU.mult),
                ['kkn', 'epv'], [f'AR{h}'])
            dve(lambda e, h=h: e.tensor_tensor(out=BK[:, h, :, 0:128], in0=c4(b_[:]), in1=c4(em[:]), op=ALU.mult), ['b', 'em'], [f'BK{h}'])
            dve(lambda e, h=h: e.tensor_tensor(out=BK[:, h, :, 128:256], in0=c4(kp[:]), in1=c4(em[:]), op=ALU.mult), ['kp', 'em'], [f'BK{h}'])
            dve(lambda e, h=h: e.scalar_tensor_tensor(out=u1[:], in0=r_, scalar=rkc[:, h:h + 1], in1=kp[:], op0=ALU.mult, op1=ALU.mult),
                zkeys + ['kp', 'par', 'u1'], ['u1'])
            p5, p5k = pl.next()
            mm(P, p5[:], onesf[:], u1[:], True, True, ['onesf', 'u1'], [p5k])
            dve(lambda e, p5=p5, h=h: e.tensor_tensor(out=bon[:, h, :], in0=p5[:], in1=v_, op=ALU.mult), [p5k] + zkeys, [f'bon{h}'])
            P.op('pool', lambda e: e.tensor_copy(out=vb[:], in_=v_), reads=zkeys, writes=['vb'])
            for c in range(4):
                pt_, ptk = ptr.next()
                cs_ = slice(c * 128, (c + 1) * 128)
                P.op('pe', lambda e, pt_=pt_, cs_=cs_: e.transpose(out=pt_[:, 0, :], in_=vb[:, cs_], identity=ident[0:64, 0:64]), reads=['vb'], writes=[ptk])
                P.op('pe', lambda e, pt_=pt_, h=h, c=c: e.transpose(out=pt_[:, 1, :], in_=BK[:, h, c, 0:128], identity=ident[0:64, 0:64]), reads=[f'BK{h}'], writes=[ptk])
                P.op('pe', lambda e, pt_=pt_, h=h, c=c: e.transpose(out=pt_[:, 2, :], in_=BK[:, h, c, 128:256], identity=ident[0:64, 0:64]), reads=[f'BK{h}'], writes=[ptk])
                P.op('act', lambda e, pt_=pt_, h=h, c=c: e.activation(out=tok3[:, h, c, :, :], in_=pt_[:], func=AF.Copy), reads=[ptk], writes=[f'tok{h}'])
        for h in range(8):
            z, zk = zrot.next()
            prep_head(h, z, zk)
        def stage1(cs, tg=tg):
            sl = (cs[0] // 2) % 2
            Gm, Nm = Gms[sl], Nms[sl]
            probs = [(ci, c, h) for ci, c in enumerate(cs) for h in range(8)]
            for (ci, c, h) in probs:
                q = ci * 8 + h
                p_, pk = pg.next()
                mm(P, p_[:, 0:256], BK[:, h, c, 0:128], AR[:, h, c, :], True, True, [f'BK{h}', f'AR{h}'], [pk])
                mm(P, p_[:, 256:512], BK[:, h, c, 128:256], AR[:, h, c, :], True, True, [f'BK{h}', f'AR{h}'], [pk])
                dve(lambda e, p_=p_, q=q: e.tensor_tensor(out=Gm[:, q, :], in0=p_[:], in1=mask4[:], op=ALU.mult), [pk, 'mask4'], [f'Gm{sl}_{q}'])
                p2_, p2k = pg.next()
                mm(P, p2_[:, 0:128], AR[:, h, c, 0:128], BK[:, h, c, 0:128], True, True, [f'BK{h}', f'AR{h}'], [p2k])
                dve(lambda e, p2_=p2_, q=q: e.tensor_tensor(out=XY[0][:, q, 128:256], in0=p2_[:, 0:128], in1=maskL[:], op=ALU.mult),
                    [p2k, 'maskL'], [f'XY0{q}'])
                P.op('pool', lambda e, q=q: e.tensor_copy(out=XY[0][:, q, 0:128], in_=Gm[:, q, 0:128]), reads=[f'Gm{sl}_{q}'], writes=[f'XY0{q}x'])
                P.op('pool', lambda e, q=q: e.tensor_tensor(out=Nm[:, q, :], in0=Gm[:, q, 0:128], in1=ident[:], op=ALU.add),
                     reads=[f'Gm{sl}_{q}'], writes=[f'N{sl}_{q}'])
            yield
            for j in range(6):
                cur, nxt = XY[j % 2], XY[(j + 1) % 2]
                ck, nk_ = f'XY{j % 2}', f'XY{(j + 1) % 2}'
                for q in range(len(probs)):
                    p_, pk = pg.next()
                    rk_ = [ck + f'{q}', ck + f'{q}x']
                    mm(P, p_[:, 0:128], cur[:, q, 128:256], cur[:, q, 0:128], True, True, rk_, [pk])
                    mm(P, p_[:, 128:256], cur[:, q, 0:128], cur[:, q, 128:256], True, True, rk_, [pk])
                    P.op('act', lambda e, p_=p_, nxt=nxt, q=q: e.activation(out=nxt[:, q, :], in_=p_[:, 0:256], func=AF.Copy),
                         reads=[pk], writes=[nk_ + f'{q}', nk_ + f'{q}x'])
                    if q % 4 == 3:
                        yield
                for q in range(len(probs)):
                    p_, pk = pg.next()
                    mm(P, p_[:, 0:128], nxt[:, q, 128:256], Nm[:, q, :], True, True, [nk_ + f'{q}', nk_ + f'{q}x', f'N{sl}_{q}'], [pk])
                    dve(lambda e, p_=p_, q=q: e.tensor_tensor(out=Nm[:, q, :], in0=p_[:, 0:128], in1=Nm[:, q, :], op=ALU.add),
                        [pk, f'N{sl}_{q}'], [f'N{sl}_{q}'])
                    if q % 4 == 3:
                        yield

        def stage2(c, tg=tg):
            sl = (c // 2) % 2
            Gm, Nm = Gms[sl], Nms[sl]
            ci = c % 2
            pw, pwk = pg.next()
            for h in range(8):
                q = ci * 8 + h
                mm(P, pw[:, h * 64:(h + 1) * 64], AR[:, h, c, 0:128], Tst[:, h, :], True, False, [f'AR{h}', f'T{h}'], [pwk])
                mm(P, pw[:, h * 64:(h + 1) * 64], Gm[:, q, 256:384], tok3[:, h, c, 0, :], False, True, [f'Gm{sl}_{q}', f'tok{h}'], [pwk])
            P.op('act', lambda e: e.activation(out=Wsb[:].rearrange("p h v -> p (h v)"), in_=pw[:], func=AF.Copy), reads=[pwk], writes=['W'])
            yield
            pu, puk = pg.next()
            for h in range(8):
                q = ci * 8 + h
                mm(P, pu[:, h * 64:(h + 1) * 64], Nm[:, q, :], Wsb[:, h, :], True, True, [f'N{sl}_{q}', 'W'], [puk])
            P.op('act', lambda e: e.activation(out=Usb[:].rearrange("p h v -> p (h v)"), in_=pu[:], func=AF.Copy), reads=[puk], writes=['U'])
            yield
            pt_, ptk = pg.next()
            for h in range(8):
                q = ci * 8 + h
                o_ = pt_[0:64, h * 64:(h + 1) * 64]
                mm(P, o_, ident[0:64, 0:64], Tst[:, h, :], True, False, [f'T{h}'], [ptk])
                mm(P, o_, tok3[:, h, c, 1, :], Usb[:, h, :], False, False, [f'tok{h}', 'U'], [ptk])
                mm(P, o_, tok3[:, h, c, 2, :], tok3[:, h, c, 0, :], False, True, [f'tok{h}'], [ptk])
            for half in range(2):
                py_, pyk = pg.next()
                for hh in range(4):
                    h = half * 4 + hh
                    q = ci * 8 + h
                    o_ = py_[0:64, hh * 128:(hh + 1) * 128]
                    mm(P, o_, Tst[:, h, :], AR[:, h, c, 128:256], True, False, [f'AR{h}', f'T{h}'], [pyk])
                    mm(P, o_, Usb[:, h, :], Gm[:, q, 128:256], False, False, ['U', f'Gm{sl}_{q}'], [pyk])
                    mm(P, o_, tok3[:, h, c, 0, :], Gm[:, q, 384:512], False, True, [f'tok{h}', f'Gm{sl}_{q}'], [pyk])
                P.op('act', lambda e, py_=py_, half=half: e.activation(
                    out=yT[:, half * 4:half * 4 + 4, c * 128:(c + 1) * 128], in_=py_[0:64, :].rearrange("p (h t) -> p h t", h=4), func=AF.Copy),
                    reads=[pyk], writes=[f'yT{half}'])
            for h in range(8):
                dve(lambda e, h=h: e.tensor_scalar(out=Tst[:, h, :], in0=pt_[0:64, h * 64:(h + 1) * 64], scalar1=pC[:, h, c:c + 1], scalar2=None, op0=ALU.mult),
                    [ptk, 'pC', f'T{h}'], [f'T{h}'])
            yield

        def chain(*gs):
            for g in gs:
                yield from g

        def rr(g1, g2):
            a = b = True
            while a or b:
                if a:
                    try:
                        next(g1)
                    except StopIteration:
                        a = False
                if b:
                    try:
                        next(g2)
                    except StopIteration:
                        b = False
        for _ in stage1([0, 1]):
            pass
        rr(stage1([2, 3]), chain(stage2(0), stage2(1)))
        for _ in chain(stage2(2), stage2(3)):
            pass
        for h in range(8):
            u1, u2 = hb['u1'], hb['t']
            p1, p1k = pl.next()
            mm(P, p1[:], onesm[:], yT[:, h, :], True, True, ['onesm', f'yT{h // 4}'], [p1k])
            dve(lambda e, p1=p1, h=h: e.tensor_tensor(out=u1[:], in0=yT[:, h, :], in1=p1[:], op=ALU.subtract), [p1k, f'yT{h // 4}', 'u1'], ['u1'])
            P.op('act', lambda e: e.activation(out=u2[:], in_=u1[:], func=AF.Square), reads=['u1', 't'], writes=['t'])
            p2, p2k = pl.next()
            mm(P, p2[:], onesm[:], u2[:], True, True, ['onesm', 't'], [p2k])
            P.op('act', lambda e, p2=p2: e.activation(out=u2[:], in_=p2[:], func=AF.Sqrt, bias=gneps[:, 0:1]), reads=[p2k, 'gneps', 't'], writes=['t'])
            dve(lambda e: e.reciprocal(out=u2[:], in_=u2[:]), ['t'], ['t'])
            dve(lambda e: e.tensor_tensor(out=u1[:], in0=u1[:], in1=u2[:], op=ALU.mult), ['u1', 't'], ['u1'])
            dve(lambda e, h=h: e.tensor_scalar(out=u1[:], in0=u1[:], scalar1=lgc[:, h:h + 1], scalar2=lbc[:, h:h + 1], op0=ALU.mult, op1=ALU.add),
                ['u1', 'par'], ['u1'])
            dve(lambda e, h=h: e.tensor_tensor(out=u1[:], in0=u1[:], in1=bon[:, h, :], op=ALU.add), ['u1', f'bon{h}'], ['u1'])
            dve(lambda e, h=h: e.tensor_tensor(out=RW[:, h, :], in0=u1[:], in1=gg[:, h, :], op=ALU.mult), ['u1', f'gg{h}'], ['RW'])
        P.dma('sp', lambda e, t0=t0: e.dma_start(out=rwT[:, :, t0:t0 + 512], in_=RW[:]), reads=['RW'])
        if tg == 0 and 'dbg_y' in T:
            P.dma('sp', lambda e: e.dma_start(out=T['dbg_y'], in_=yT[:]), reads=['yT0', 'yT1'])
            P.dma('sp', lambda e: e.dma_start(out=T['dbg_bon'], in_=bon[:]), reads=[f'bon{h}' for h in range(8)])
            P.dma('sp', lambda e: e.dma_start(out=T['dbg_g'], in_=gg[:]), reads=[f'gg{h}' for h in range(8)])
            P.dma('sp', lambda e: e.dma_start(out=T['dbg_AR'], in_=AR[:]), reads=[f'AR{h}' for h in range(8)])
            P.dma('sp', lambda e: e.dma_start(out=T['dbg_BK'], in_=BK[:]), reads=[f'BK{h}' for h in range(8)])


def phase_F(P, T, G):
    wa, wr, wo = P.sb([128, 4, D], BF16), P.sb([128, 4, D], BF16), P.sb([128, 8, D], BF16)
    P.dma('pool', lambda e: e.dma_start(out=wa[:], in_=T['w_attn_br'].rearrange("(j p) d -> p j d", p=128)), writes=['wa'])
    P.dma('pool', lambda e: e.dma_start(out=wr[:], in_=T['w_rwkv_br'].rearrange("(j p) d -> p j d", p=128)), writes=['wr'])
    P.dma('pool', lambda e: e.dma_start(out=wo[:], in_=T['w_out'].rearrange("(j p) d -> p j d", p=128)), writes=['wo'])
    W = dict(junk=P.sb([128, D], F32), ss=P.sb([128, 1], F32), rs=P.sb([128, 1], F32), t1=P.sb([128, D], F32),
             hb=P.sb([128, D], BF16), pt=P.ps([128, 8, 128], BF16))
    W['mod'] = P.sb([128, 3072], F32)
    P.dma('sp', lambda e: e.dma_start(out=W['mod'][:], in_=T['modd'][:, 2048:5120]), writes=['modl'])
    xrot = Rot(P, 2, [128, D], F32, 'x')
    atr, rtr = Rot(P, 2, [128, 4, 128], BF16, 'at'), Rot(P, 2, [128, 4, 128], BF16, 'rt')
    gar, grr = Rot(P, 2, [128, 8, 128], BF16, 'ga'), Rot(P, 2, [128, 8, 128], BF16, 'gr')
    sga, sgr = P.sb([128, 8, 128], F32), P.sb([128, 8, 128], F32)
    m1, m2 = P.sb([128, 4, 128], F32), P.sb([128, 4, 128], F32)
    mixT = P.sb([128, 8, 128], BF16)
    x1t = P.sb([128, D], F32)
    h2t = P.sb([128, 8, 128], BF16)
    pA = Rot(P, 1, [128, 4, 128], F32, 'pA', psum=True)
    pR = Rot(P, 1, [128, 4, 128], F32, 'pR', psum=True)
    po = Rot(P, 2, [128, 512], F32, 'po', psum=True)
    aT = T['attnT'].rearrange("(j p) t -> p j t", p=128)
    rT = T['rwT'].rearrange("(j p) t -> p j t", p=128)
    gaT = T['zga'].rearrange("(j p) t -> p j t", p=128)
    grT = T['zgr'].rearrange("(j p) t -> p j t", p=128)
    h2T = T['h2T'].rearrange("(k p) t -> p k t", p=128)
    R = router_setup(P, T, G) if G.get('sparse') else None
    for i in range(NT):
        tsl = slice(i * 128, (i + 1) * 128)
        xt, xk = xrot.next()
        at, atk = atr.next(); rt, rtk = rtr.next(); ga, gak = gar.next(); gr, grk = grr.next()
        P.dma('sp', lambda e, xt=xt, tsl=tsl: e.dma_start(out=xt[:], in_=T['x'][tsl, :]), writes=[xk])
        P.dma('act', lambda e, at=at, tsl=tsl: e.dma_start(out=at[:], in_=aT[:, :, tsl]), writes=[atk])
        P.dma('act', lambda e, rt=rt, tsl=tsl: e.dma_start(out=rt[:], in_=rT[:, :, tsl]), writes=[rtk])
        P.dma('sp', lambda e, ga=ga, tsl=tsl: e.dma_start(out=ga[:], in_=gaT[:, :, tsl]), writes=[gak])
        P.dma('sp', lambda e, gr=gr, tsl=tsl: e.dma_start(out=gr[:], in_=grT[:, :, tsl]), writes=[grk])
        P.op('act', lambda e, ga=ga: e.activation(out=sga[:], in_=ga[:], func=AF.Sigmoid), reads=[gak], writes=['sga'])
        P.op('act', lambda e, gr=gr: e.activation(out=sgr[:], in_=gr[:], func=AF.Sigmoid), reads=[grk], writes=['sgr'])
        for half in range(2):
            pa, pak = pA.next(); pr, prk = pR.next()
            for s_ in range(4):
                dt = half * 4 + s_
                for j in range(4):
                    mm(P, pa[:, s_, :], wa[:, j, dt * 128:(dt + 1) * 128], at[:, j, :], j == 0, j == 3, ['wa', atk], [pak])
            for s_ in range(4):
                dt = half * 4 + s_
                for j in range(4):
                    mm(P, pr[:, s_, :], wr[:, j, dt * 128:(dt + 1) * 128], rt[:, j, :], j == 0, j == 3, ['wr', rtk], [prk])
            hs = slice(half * 4, half * 4 + 4)
            P.op('dve', lambda e, pa=pa, hs=hs: e.tensor_tensor(out=m1[:], in0=pa[:], in1=sga[:, hs, :], op=ALU.mult), reads=[pak, 'sga'], writes=['m1'])
            P.op('dve', lambda e, pr=pr, hs=hs: e.tensor_tensor(out=m2[:], in0=pr[:], in1=sgr[:, hs, :], op=ALU.mult), reads=[prk, 'sgr'], writes=['m2'])
            P.op('pool', lambda e, hs=hs: e.tensor_tensor(out=mixT[:, hs, :], in0=m1[:], in1=m2[:], op=ALU.add), reads=['m1', 'm2'], writes=['mixT'])
        for half in range(2):
            p_, pk = po.next()
            cs_ = slice(half * 512, (half + 1) * 512)
            for dt in range(8):
                mm(P, p_[:], mixT[:, dt, :], wo[:, dt, cs_], dt == 0, dt == 7, ['mixT', 'wo'], [pk])
            P.op('dve', lambda e, p_=p_, cs_=cs_: e.tensor_tensor(out=x1t[:, cs_], in0=p_[:], in1=W['mod'][:, cs_], op=ALU.mult),
                 reads=[pk, 'modl'], writes=['x1t'])
        P.op('pool', lambda e, xt=xt: e.tensor_tensor(out=x1t[:], in0=x1t[:], in1=xt[:], op=ALU.add), reads=['x1t', xk], writes=['x1t'])
        P.dma('sp', lambda e, tsl=tsl: e.dma_start(out=T['x1'][tsl, :], in_=x1t[:]), reads=['x1t'])
        norm_mod_transpose(P, G, x1t, 'x1t', h2t, 0, slice(2048, 3072), slice(1024, 2048), W)
        P.dma('act', lambda e, tsl=tsl: e.dma_start(out=h2T[:, :, tsl], in_=h2t[:]), reads=['hT0'])
        if R is not None:
            P.dma('act', lambda e, tsl=tsl: e.dma_start(out=T['h2tok'][tsl, :], in_=W['hb'][:]), reads=['hb'])
            router_tile(P, T, R, h2t, 'hT0', i)
    if R is not None:
        P.dma('sp', lambda e: e.dma_start(out=T['cntd'], in_=R['cnt'][:]), reads=['cnt'])


def phase_G0(P, T, G):
    for e in range(64):
        for (src, dst) in (('exp_gate', 'wg16'), ('exp_up', 'wu16'), ('exp_down', 'wd16')):
            P.dma('pool', lambda e_, e=e, src=src, dst=dst: e_.dma_start(
                out=T[dst][e].rearrange("k p f -> (k p f)").rearrange("(a b) -> a b", b=2048), in_=T[src][e].rearrange("r c -> (r c)").rearrange("(a b) -> a b", b=2048)))
    for (src, dst) in (('sh_gate', 'wg16'), ('sh_up', 'wu16'), ('sh_down', 'wd16')):
        P.dma('pool', lambda e_, src=src, dst=dst: e_.dma_start(
            out=T[dst][64].rearrange("k p f -> (k p f)").rearrange("(a b) -> a b", b=2048), in_=T[src].rearrange("r c -> (r c)").rearrange("(a b) -> a b", b=2048)))


def phase_G(P, T, G):
    ident = G['identb']
    h2T = T['h2T'].rearrange("(k p) t -> p k t", p=128)
    yacc = G['yacc']
    gwT = P.sb([64, S], BF16)
    rwt = P.sb([128, 8, 64], BF16)
    rbias = P.sb([128, 64], F32)
    P.dma('pool', lambda e: e.dma_start(out=rwt[:], in_=T['router_w'].rearrange("(k p) n -> p k n", p=128)), writes=['rwt'])
    P.dma('sp', lambda e: e.dma_start(out=rbias[:], in_=T['router_bias'].partition_broadcast(128)), writes=['rbias'])
    ones128 = P.sb([64, 128], BF16)
    P.op('dve', lambda e: e.memset(ones128[:], 1.0), writes=['ones128'])
    hrot = Rot(P, 2, [128, 8, 256], BF16, 'h2g')
    pmisc = P.ps([128, 512], F32)
    ptb = P.ps([64, 128], BF16)
    emb = P.sb([128, 64], BF16)
    sc_, ch, tmp, cm, em = [P.sb([128, 64], F32) for _ in range(5)]
    m1, m2, grp, s8, gmask, pen, den = [P.sb([128, 8], F32) for _ in range(7)]
    dve = lambda fn, r, w: P.op('dve', fn, reads=r, writes=w)
    for tgp in range(16):
        hg, hk = hrot.next()
        P.dma('sp', lambda e, hg=hg, tgp=tgp: e.dma_start(out=hg[:], in_=h2T[:, :, tgp * 256:(tgp + 1) * 256]), writes=[hk])
        for tt in range(2):
            i = tgp * 2 + tt
            p_, pk = pmisc[:, 0:64], 'pm_a'
            for k in range(8):
                mm(P, p_, hg[:, k, tt * 128:(tt + 1) * 128], rwt[:, k, :], k == 0, k == 7, [hk, 'rwt'], [pk])
            P.op('act', lambda e, p_=p_: e.activation(out=sc_[:], in_=p_, func=AF.Sigmoid), reads=[pk], writes=['sc'])
            dve(lambda e: e.tensor_tensor(out=ch[:], in0=sc_[:], in1=rbias[:], op=ALU.add), ['sc', 'rbias'], ['ch'])
            ch3 = ch[:].rearrange("p (g e) -> p g e", g=8)
            dve(lambda e, ch3=ch3: e.tensor_reduce(out=m1[:], in_=ch3, axis=AX.X, op=ALU.max), ['ch'], ['m1'])
            for g in range(8):
                dve(lambda e, g=g: e.tensor_scalar(out=tmp[:, g * 8:(g + 1) * 8], in0=ch[:, g * 8:(g + 1) * 8], scalar1=m1[:, g:g + 1],
                                                  scalar2=-1e9, op0=ALU.is_equal, op1=ALU.mult), ['ch', 'm1', 'tmp'], ['tmp'])
            dve(lambda e: e.tensor_tensor(out=tmp[:], in0=tmp[:], in1=ch[:], op=ALU.add), ['tmp', 'ch'], ['tmp'])
            dve(lambda e: e.tensor_reduce(out=m2[:], in_=tmp[:].rearrange("p (g e) -> p g e", g=8), axis=AX.X, op=ALU.max), ['tmp'], ['m2'])
            dve(lambda e: e.tensor_tensor(out=grp[:], in0=m1[:], in1=m2[:], op=ALU.add), ['m1', 'm2'], ['grp'])
            dve(lambda e: e.max(out=s8[:], in_=grp[:]), ['grp'], ['s8'])
            dve(lambda e: e.tensor_scalar(out=gmask[:], in0=grp[:], scalar1=s8[:, 3:4], scalar2=None, op0=ALU.is_ge), ['grp', 's8'], ['gmask'])
            dve(lambda e: e.tensor_scalar(out=pen[:], in0=gmask[:], scalar1=-1.0, scalar2=1e9, op0=ALU.add, op1=ALU.mult), ['gmask'], ['pen'])
            for g in range(8):
                dve(lambda e, g=g: e.tensor_scalar(out=cm[:, g * 8:(g + 1) * 8], in0=ch[:, g * 8:(g + 1) * 8], scalar1=pen[:, g:g + 1],
                                                  scalar2=None, op0=ALU.add), ['ch', 'pen', 'cm'], ['cm'])
            dve(lambda e: e.max(out=s8[:], in_=cm[:]), ['cm', 's8'], ['s8'])
            dve(lambda e: e.tensor_scalar(out=em[:], in0=cm[:], scalar1=s8[:, 7:8], scalar2=None, op0=ALU.is_ge), ['cm', 's8'], ['em'])
            dve(lambda e: e.tensor_tensor(out=em[:], in0=em[:], in1=sc_[:], op=ALU.mult), ['em', 'sc'], ['em'])
            dve(lambda e: e.tensor_reduce(out=den[:, 0:1], in_=em[:], axis=AX.X, op=ALU.add), ['em'], ['den'])
            dve(lambda e: e.reciprocal(out=den[:, 1:2], in_=den[:, 0:1]), ['den'], ['den'])
            dve(lambda e: e.tensor_scalar(out=em[:], in0=em[:], scalar1=den[:, 1:2], scalar2=2.5, op0=ALU.mult, op1=ALU.mult), ['em', 'den'], ['em'])
            pt_, ptk = ptb[:], 'pm_b'
            dve(lambda e: e.tensor_copy(out=emb[:], in_=em[:]), ['em', 'emb'], ['emb'])
            P.op('pe', lambda e, pt_=pt_: e.transpose(out=pt_, in_=emb[:], identity=G['identb'][:]), reads=['emb'], writes=[ptk])
            P.op('act', lambda e, pt_=pt_, i=i: e.activation(out=gwT[:, i * 128:(i + 1) * 128], in_=pt_, func=AF.Copy), reads=[ptk], writes=['gwT'])
    if G.get('gstop') == 'router':
        return
    wgr = Rot(P, 2, [128, 8, 256], BF16, 'wg')
    wur = Rot(P, 2, [128, 8, 256], BF16, 'wu')
    wdr = Rot(P, 2, [128, 2, D], BF16, 'wd')
    selr = Rot(P, 2, [64, 128], BF16, 'sel')
    pgu = Rot(P, 2, [128, 4, 256], F32, 'pgu', psum=True)
    py = Rot(P, 2, [128, 512], F32, 'py', psum=True)
    sgr_ = Rot(P, 2, [128, 2, 256], F32, 'sg')
    tr_ = Rot(P, 2, [128, 2, 256], F32, 'tt')
    actr = Rot(P, 2, [128, 2, 256], BF16, 'act')
    for e_ in range(G.get('nexp', 65)):
        wg, wgk = wgr.next(); wu, wuk = wur.next(); wd, wdk = wdr.next()
        if e_ < 64:
            sg_, su_, sd_ = T['exp_gate'][e_], T['exp_up'][e_], T['exp_down'][e_]
        else:
            sg_, su_, sd_ = T['sh_gate'], T['sh_up'], T['sh_down']
        P.dma('pool', lambda e, wg=wg, sg_=sg_: e.dma_start(out=wg[:], in_=sg_.rearrange("(k p) f -> p k f", p=128)), writes=[wgk])
        P.dma('pool', lambda e, wu=wu, su_=su_: e.dma_start(out=wu[:], in_=su_.rearrange("(k p) f -> p k f", p=128)), writes=[wuk])
        P.dma('pool', lambda e, wd=wd, sd_=sd_: e.dma_start(out=wd[:], in_=sd_.rearrange("(k p) f -> p k f", p=128)), writes=[wdk])
        if e_ < 64:
            sel, selk = selr.next()
            P.op('pool', lambda e, sel=sel, e_=e_: e.tensor_scalar(out=sel[:], in0=ones128[:], scalar1=G['identf'][0:64, e_:e_ + 1], scalar2=0.0, op0=ALU.mult, op1=ALU.add),
                 reads=['ones128'], writes=[selk])
        for tgp in range(16):
            hg, hk = hrot.next()
            P.dma('sp' if tgp % 2 == 0 else 'act', lambda e, hg=hg, tgp=tgp: e.dma_start(out=hg[:], in_=h2T[:, :, tgp * 256:(tgp + 1) * 256]), writes=[hk])
            p_, pk = pgu.next()
            for s_, (w_, wk_) in enumerate(((wg, wgk), (wg, wgk), (wu, wuk), (wu, wuk))):
                ft = s_ % 2
                for k in range(8):
                    mm(P, p_[:, s_, :], w_[:, k, ft * 128:(ft + 1) * 128], hg[:, k, :], k == 0, k == 7, [wk_, hk], [pk])
            sg, sgk = sgr_.next(); t_, tk = tr_.next(); ac, ack = actr.next()
            P.op('act', lambda e, sg=sg, p_=p_: e.activation(out=sg[:], in_=p_[:, 0:2, :], func=AF.Silu), reads=[pk], writes=[sgk])
            P.op('dve', lambda e, sg=sg, p_=p_, t_=t_: e.tensor_tensor(out=t_[:], in0=p_[:, 2:4, :], in1=sg[:], op=ALU.mult), reads=[pk, sgk], writes=[tk])
            if e_ < 64:
                pw, pwk = pmisc[:, 256:512], 'pm_c'
                mm(P, pw, sel[:], gwT[:, tgp * 256:(tgp + 1) * 256], True, True, [selk, 'gwT'], [pwk])
                for ft in range(2):
                    P.op('dve', lambda e, ac=ac, t_=t_, pw=pw, ft=ft: e.tensor_tensor(out=ac[:, ft, :], in0=pw, in1=t_[:, ft, :], op=ALU.mult),
                         reads=[tk, pwk], writes=[ack])
            else:
                P.op('pool', lambda e, ac=ac, t_=t_: e.tensor_copy(out=ac[:], in_=t_[:]), reads=[tk], writes=[ack])
            for tt in range(2):
                i = tgp * 2 + tt
                for half in range(2):
                    q_, qk = py.next()
                    cs_ = slice(half * 512, (half + 1) * 512)
                    for ft in range(2):
                        mm(P, q_[:], ac[:, ft, tt * 128:(tt + 1) * 128], wd[:, ft, cs_], ft == 0, ft == 1, [ack, wdk], [qk])
                    if e_ == 0:
                        P.op('act', lambda e, q_=q_, i=i, cs_=cs_: e.activation(out=yacc[:, i, cs_], in_=q_[:], func=AF.Copy), reads=[qk], writes=[f'y{i}'])
                    else:
                        P.op('dve', lambda e, q_=q_, i=i, cs_=cs_: e.tensor_tensor(out=yacc[:, i, cs_], in0=q_[:], in1=yacc[:, i, cs_], op=ALU.add),
                             reads=[qk, f'y{i}'], writes=[f'y{i}'])


def phase_H(P, T, G):
    yacc = G['yacc']
    dve = lambda fn, r, w: P.op('dve', fn, reads=r, writes=w)
    g2b = P.sb([128, D], F32)
    fing = P.sb([128, D], F32)
    P.dma('sp', lambda e: e.dma_start(out=g2b[:], in_=T['modd'][:, 5120:6144]), writes=['g2b'])
    P.dma('sp', lambda e: e.dma_start(out=fing[:], in_=T['final_g'].partition_broadcast(128)), writes=['fing'])
    xr = Rot(P, 2, [128, D], F32, 'x1')
    junk, ss, rs = P.sb([128, D], F32), P.sb([128, 1], F32), P.sb([128, 1], F32)
    for i in range(NT):
        tsl = slice(i * 128, (i + 1) * 128)
        xt, xk = xr.next()
        P.dma('sp', lambda e, xt=xt, tsl=tsl: e.dma_start(out=xt[:], in_=T['x1'][tsl, :]), writes=[xk])
        dve(lambda e, i=i: e.tensor_tensor(out=yacc[:, i, :], in0=yacc[:, i, :], in1=g2b[:], op=ALU.mult), [f'y{i}', 'g2b'], [f'y{i}'])
        P.op('pool', lambda e, i=i, xt=xt: e.tensor_tensor(out=xt[:], in0=xt[:], in1=yacc[:, i, :], op=ALU.add), reads=[f'y{i}', xk], writes=[xk])
        rms_rstd(P, xt, xk, junk, ss, rs, 'f')
        dve(lambda e, xt=xt: e.scalar_tensor_tensor(out=xt[:], in0=xt[:], scalar=rs[:, 0:1], in1=fing[:], op0=ALU.mult, op1=ALU.mult),
            [xk, 'rsf', 'fing'], [xk])
        P.dma('sp', lambda e, xt=xt, tsl=tsl: e.dma_start(out=T['out'][tsl, :], in_=xt[:]), reads=[xk])


NBLK = 320


def router_setup(P, T, G):
    R = {}
    R['rwt'] = P.sb([128, 8, 64], BF16)
    R['rbias'] = P.sb([128, 64], F32)
    R['iota'] = P.sb([128, 64], F32)
    R['su'] = P.sb([128, 128], BF16)
    R['onesb'] = P.sb([128, 128], BF16)
    R['cnt'] = P.sb([128, 64], F32)
    R['kmf'] = P.sb([128, 128], F32)
    P.dma('pool', lambda e: e.dma_start(out=R['rwt'][:], in_=T['router_w'].rearrange("(k p) n -> p k n", p=128)), writes=['rwt'])
    P.dma('sp', lambda e: e.dma_start(out=R['rbias'][:], in_=T['router_bias'].partition_broadcast(128)), writes=['rbias'])
    P.dma('sp', lambda e: e.dma_start(out=R['iota'][:], in_=T['k_rel'][0:1, 0:64].rearrange("o f -> (o f)").partition_broadcast(128)), writes=['iota'])
    P.dma('sp', lambda e: e.dma_start(out=R['kmf'][:], in_=T['k_masks'][:, 0:128]), writes=['kmf'])
    P.op('dve', lambda e: e.tensor_copy(out=R['su'][:], in_=R['kmf'][:]), reads=['kmf'], writes=['su'])
    P.op('dve', lambda e: e.memset(R['onesb'][:], 1.0), writes=['onesb'])
    P.op('dve', lambda e: e.memset(R['cnt'][:], 0.0), writes=['cnt'])
    R['pm'] = P.ps([128, 512], F32)
    for n in ('sc', 'ch', 'tmp', 'cm', 'em', 'mk', 'oh', 'rk', 'jk'):
        R[n] = P.sb([128, 64], F32)
    R['mkb'] = P.sb([128, 64], BF16)
    for n in ('m1', 'm2', 'grp', 's8', 'gmask', 'pen', 'den', 'i8f'):
        R[n] = P.sb([128, 8], F32)
    R['i8u'] = P.sb([128, 8], U32)
    R['meta'] = P.sb([128, 24], F32)
    return R


def router_tile(P, T, R, h2t, h2k, i):
    dve = lambda fn, r, w: P.op('dve', fn, reads=r, writes=w)
    pm = R['pm']
    sc_, ch, tmp, cm, em, mk, oh, rk, jk, mkb = [R[n] for n in ('sc', 'ch', 'tmp', 'cm', 'em', 'mk', 'oh', 'rk', 'jk', 'mkb')]
    m1, m2, grp, s8, gmask, pen, den, i8f, i8u, meta = [R[n] for n in ('m1', 'm2', 'grp', 's8', 'gmask', 'pen', 'den', 'i8f', 'i8u', 'meta')]
    for k in range(8):
        mm(P, pm[:, 0:64], h2t[:, k, :], R['rwt'][:, k, :], k == 0, k == 7, [h2k, 'rwt'], ['pm_a'])
    P.op('act', lambda e: e.activation(out=sc_[:], in_=pm[:, 0:64], func=AF.Sigmoid), reads=['pm_a'], writes=['sc'])
    dve(lambda e: e.tensor_tensor(out=ch[:], in0=sc_[:], in1=R['rbias'][:], op=ALU.add), ['sc', 'rbias'], ['ch'])
    dve(lambda e: e.tensor_reduce(out=m1[:], in_=ch[:].rearrange("p (g e) -> p g e", g=8), axis=AX.X, op=ALU.max), ['ch'], ['m1'])
    for g in range(8):
        dve(lambda e, g=g: e.tensor_scalar(out=tmp[:, g * 8:(g + 1) * 8], in0=ch[:, g * 8:(g + 1) * 8], scalar1=m1[:, g:g + 1],
                                          scalar2=-1e9, op0=ALU.is_equal, op1=ALU.mult), ['ch', 'm1', 'tmp'], ['tmp'])
    dve(lambda e: e.tensor_tensor(out=tmp[:], in0=tmp[:], in1=ch[:], op=ALU.add), ['tmp', 'ch'], ['tmp'])
    dve(lambda e: e.tensor_reduce(out=m2[:], in_=tmp[:].rearrange("p (g e) -> p g e", g=8), axis=AX.X, op=ALU.max), ['tmp'], ['m2'])
    dve(lambda e: e.tensor_tensor(out=grp[:], in0=m1[:], in1=m2[:], op=ALU.add), ['m1', 'm2'], ['grp'])
    dve(lambda e: e.max(out=s8[:], in_=grp[:]), ['grp'], ['s8'])
    dve(lambda e: e.tensor_scalar(out=gmask[:], in0=grp[:], scalar1=s8[:, 3:4], scalar2=None, op0=ALU.is_ge), ['grp', 's8'], ['gmask'])
    dve(lambda e: e.tensor_scalar(out=pen[:], in0=gmask[:], scalar1=-1.0, scalar2=1e9, op0=ALU.add, op1=ALU.mult), ['gmask'], ['pen'])
    for g in range(8):
        dve(lambda e, g=g: e.tensor_scalar(out=cm[:, g * 8:(g + 1) * 8], in0=ch[:, g * 8:(g + 1) * 8], scalar1=pen[:, g:g + 1],
                                          scalar2=None, op0=ALU.add), ['ch', 'pen', 'cm'], ['cm'])
    dve(lambda e: e.max(out=s8[:], in_=cm[:]), ['cm', 's8'], ['s8'])
    dve(lambda e: e.max_index(out=i8u[:], in_max=s8[:], in_values=cm[:]), ['cm', 's8', 'i8u'], ['i8u'])
    dve(lambda e: e.tensor_copy(out=meta[:, 0:8], in_=i8u[:]), ['i8u', 'meta'], ['meta'])
    dve(lambda e: e.tensor_scalar(out=mk[:], in0=cm[:], scalar1=s8[:, 7:8], scalar2=None, op0=ALU.is_ge), ['cm', 's8'], ['mk'])
    dve(lambda e: e.tensor_copy(out=mkb[:], in_=mk[:]), ['mk', 'mkb'], ['mkb'])
    dve(lambda e: e.tensor_tensor(out=em[:], in0=mk[:], in1=sc_[:], op=ALU.mult), ['mk', 'sc'], ['em'])
    dve(lambda e: e.tensor_reduce(out=den[:, 0:1], in_=em[:], axis=AX.X, op=ALU.add), ['em'], ['den'])
    dve(lambda e: e.reciprocal(out=den[:, 1:2], in_=den[:, 0:1]), ['den'], ['den'])
    dve(lambda e: e.tensor_scalar(out=em[:], in0=em[:], scalar1=den[:, 1:2], scalar2=2.5, op0=ALU.mult, op1=ALU.mult), ['em', 'den'], ['em'])
    mm(P, pm[:, 64:128], R['su'][:], mkb[:], True, True, ['su', 'mkb'], ['pm_b'])
    mm(P, pm[:, 128:192], R['onesb'][:], mkb[:], True, True, ['onesb', 'mkb'], ['pm_c'])
    dve(lambda e: e.tensor_tensor(out=rk[:], in0=pm[:, 64:128], in1=R['cnt'][:], op=ALU.add), ['pm_b', 'cnt', 'rk'], ['rk'])
    dve(lambda e: e.tensor_tensor(out=R['cnt'][:], in0=pm[:, 128:192], in1=R['cnt'][:], op=ALU.add), ['pm_c', 'cnt'], ['cnt'])
    for k in range(8):
        dve(lambda e, k=k: e.tensor_scalar(out=oh[:], in0=R['iota'][:], scalar1=meta[:, k:k + 1], scalar2=None, op0=ALU.is_equal),
            ['iota', 'meta', 'oh'], ['oh'])
        dve(lambda e, k=k: e.scalar_tensor_tensor(out=jk[:], in0=oh[:], scalar=1.0, in1=rk[:], op0=ALU.mult, op1=ALU.mult, accum_out=meta[:, 8 + k:9 + k]), ['oh', 'rk', 'jk', 'meta'], ['jk', 'meta'])
        dve(lambda e, k=k: e.scalar_tensor_tensor(out=jk[:], in0=oh[:], scalar=1.0, in1=em[:], op0=ALU.mult, op1=ALU.mult, accum_out=meta[:, 16 + k:17 + k]), ['oh', 'em', 'jk', 'meta'], ['jk', 'meta'])
    P.dma('sp', lambda e: e.dma_start(out=T['meta'][i * 128:(i + 1) * 128, :], in_=meta[:]), reads=['meta'])


def phase_S0(P, T, G):
    st32 = Rot(P, 2, [128, 1024], F32, 's32')
    st16 = Rot(P, 1, [128, 1024], BF16, 's16')
    n = 0
    for e in range(65):
        for (src, shsrc, dst) in (('exp_gate', 'sh_gate', 'wg16'), ('exp_up', 'sh_up', 'wu16'), ('exp_down', 'sh_down', 'wd16')):
            s_ap = T[src][e] if e < 64 else T[shsrc]
            f = 256 if dst != 'wd16' else D
            rows = 512 if dst != 'wd16' else 128
            c0 = {'wg16': 0, 'wu16': 2048, 'wd16': 4096}[dst]
            for hh in range(2):
                a, ak = st32.next()
                c, ck = st16.next()
                src_ap = s_ap[hh * rows:(hh + 1) * rows, :].rearrange("(k p) f -> p k f", p=128)
                q = 'sp' if n % 2 == 0 else 'act'
                P.dma(q, lambda e_, a=a, src_ap=src_ap, f=f: e_.dma_start(out=a[:].rearrange("p (k f) -> p k f", f=f), in_=src_ap), writes=[ak])
                eng = ('dve', 'pool')[n % 2]
                P.op(eng, lambda e_, a=a, c=c: e_.tensor_copy(out=c[:], in_=a[:]), reads=[ak], writes=[ck])
                cc = c0 + hh * 1024
                P.dma('act' if n % 2 == 0 else 'sp', lambda e_, c=c, cc=cc, e=e: e_.dma_start(out=T['wall16'][e * 128:(e + 1) * 128, cc:cc + 1024], in_=c[:]), reads=[ck])
                n += 1
                yield


def phase_S(P, T, G):
    identb = G['identb']
    dve = lambda fn, r, w: P.op('dve', fn, reads=r, writes=w)
    cnt = P.sb([128, 64], F32)
    ci = P.sb([128, 64], I32)
    pad = P.sb([128, 64], F32)
    pend = P.sb([128, 64], F32)
    pst = P.sb([128, 64], F32)
    ones64f = P.sb([128, 64], F32)
    iota = P.sb([128, 64], F32)
    bst = P.sb([128, NBLK], F32)
    bef = P.sb([128, NBLK], F32)
    bei = P.sb([128, NBLK], I32)
    P.dma('sp', lambda e: e.dma_start(out=cnt[:], in_=T['cntd']), writes=['cnt'])
    P.dma('sp', lambda e: e.dma_start(out=iota[:], in_=T['k_rel'][0:1, 0:64].rearrange("o f -> (o f)").partition_broadcast(128)), writes=['iota'])
    P.dma('sp', lambda e: e.dma_start(out=bst[:], in_=T['k_bst'].partition_broadcast(128)), writes=['bst'])
    dve(lambda e: e.memset(ones64f[:], 1.0), [], ['ones64f'])
    dve(lambda e: e.tensor_scalar(out=pad[:], in0=cnt[:], scalar1=127.0, scalar2=None, op0=ALU.add), ['cnt'], ['pad'])
    dve(lambda e: e.tensor_copy(out=ci[:], in_=pad[:]), ['pad'], ['ci'])
    dve(lambda e: e.tensor_scalar(out=ci[:], in0=ci[:], scalar1=7, scalar2=None, op0=ALU.arith_shift_right), ['ci'], ['ci'])
    dve(lambda e: e.tensor_scalar(out=ci[:], in0=ci[:], scalar1=7, scalar2=None, op0=ALU.logical_shift_left), ['ci'], ['ci'])
    dve(lambda e: e.tensor_copy(out=pad[:], in_=ci[:]), ['ci', 'pad'], ['pad'])
    dve(lambda e: e.tensor_tensor_scan(out=pend[:], data0=ones64f[:], data1=pad[:], initial=0.0, op0=ALU.mult, op1=ALU.add),
        ['ones64f', 'pad'], ['pend'])
    dve(lambda e: e.tensor_tensor(out=pst[:], in0=pend[:], in1=pad[:], op=ALU.subtract), ['pend', 'pad'], ['pst'])
    for ex in range(64):
        if ex == 0:
            dve(lambda e: e.tensor_scalar(out=bef[:], in0=bst[:], scalar1=pend[:, 0:1], scalar2=None, op0=ALU.is_ge), ['bst', 'pend'], ['bef'])
        else:
            dve(lambda e, ex=ex: e.scalar_tensor_tensor(out=bef[:], in0=bst[:], scalar=pend[:, ex:ex + 1], in1=bef[:], op0=ALU.is_ge, op1=ALU.add),
                ['bst', 'pend', 'bef'], ['bef'])
    dve(lambda e: e.tensor_scalar(out=bef[:], in0=bef[:], scalar1=63.0, scalar2=None, op0=ALU.min), ['bef'], ['bef'])
    pcol = P.sb([128, 1], F32)
    widxf = P.sb([128, NBLK], F32)
    widx = P.sb([128, NBLK], I32)
    P.dma('sp', lambda e: e.dma_start(out=pcol[:], in_=T['k_rel'][:, 0:1], allow_slow_non_contiguous=True), writes=['pcol'])
    dve(lambda e: e.tensor_scalar(out=pcol[:], in0=pcol[:], scalar1=-1.0, scalar2=None, op0=ALU.mult), ['pcol'], ['pcol'])
    dve(lambda e: e.tensor_scalar(out=widxf[:], in0=bef[:], scalar1=128.0, scalar2=pcol[:, 0:1], op0=ALU.mult, op1=ALU.add), ['bef', 'pcol'], ['widxf'])
    chg = P.sb([128, NBLK], F32)
    dve(lambda e: e.memset(chg[:], 1.0), [], ['chg'])
    dve(lambda e: e.tensor_tensor(out=chg[:, 3:NBLK], in0=bef[:, 3:NBLK], in1=bef[:, 0:NBLK - 3], op=ALU.not_equal), ['bef', 'chg'], ['chg'])
    dve(lambda e: e.scalar_tensor_tensor(out=widxf[:], in0=widxf[:], scalar=-1.0e6, in1=chg[:], op0=ALU.add, op1=ALU.mult), ['widxf', 'chg'], ['widxf'])
    dve(lambda e: e.tensor_scalar(out=widxf[:], in0=widxf[:], scalar1=1.0e6, scalar2=None, op0=ALU.add), ['widxf'], ['widxf'])
    dve(lambda e: e.tensor_copy(out=widx[:], in_=widxf[:]), ['widxf'], ['widx'])
    d8i = P.sb([128, NT, 8], I32)
    gw8 = P.sb([128, NT, 8], F32)
    mrot = Rot(P, 2, [128, 24], F32, 'meta')
    hrot = Rot(P, 2, [128, D], BF16, 'htok')
    oh, jk = P.sb([128, 64], F32), P.sb([128, 64], F32)
    d8f = P.sb([128, 8], F32)
    for i in range(NT):
        tsl = slice(i * 128, (i + 1) * 128)
        mt, mtk = mrot.next()
        ht, htk = hrot.next()
        P.dma('sp', lambda e, mt=mt, tsl=tsl: e.dma_start(out=mt[:], in_=T['meta'][tsl, :]), writes=[mtk])
        P.dma('act', lambda e, ht=ht, tsl=tsl: e.dma_start(out=ht[:], in_=T['h2tok'][tsl, :]), writes=[htk])
        for k in range(8):
            dve(lambda e, mt=mt, k=k: e.tensor_scalar(out=oh[:], in0=iota[:], scalar1=mt[:, k:k + 1], scalar2=None, op0=ALU.is_equal),
                ['iota', mtk, 'oh'], ['oh'])
            dve(lambda e, k=k: e.scalar_tensor_tensor(out=jk[:], in0=oh[:], scalar=1.0, in1=pst[:], op0=ALU.mult, op1=ALU.mult, accum_out=d8f[:, k:k + 1]), ['oh', 'pst', 'jk', 'd8f'], ['jk', 'd8f'])
        dve(lambda e, mt=mt: e.tensor_tensor(out=d8f[:], in0=d8f[:], in1=mt[:, 8:16], op=ALU.add), ['d8f', mtk], ['d8f'])
        dve(lambda e, i=i: e.tensor_copy(out=d8i[:, i, :], in_=d8f[:]), ['d8f'], [f'd8i{i}'])
        dve(lambda e, mt=mt, i=i: e.tensor_copy(out=gw8[:, i, :], in_=mt[:, 16:24]), [mtk], [f'gw{i}'])
        for k in range(8):
            P.dma('pool', lambda e, ht=ht, i=i, k=k: e.indirect_dma_start(
                out=T['Xs'], out_offset=bass.IndirectOffsetOnAxis(ap=d8i[:, i, k:k + 1], axis=0), in_=ht[:], in_offset=None),
                reads=[htk, f'd8i{i}'])
    wgu = Rot(P, 3, [128, 6144], BF16, 'wgu')
    wsh = P.sb([128, 6144], BF16)
    P.dma('sp', lambda e: e.dma_start(out=wsh[:], in_=T['wall16'][64 * 128:65 * 128, :]), writes=['wsh'])
    xbr = Rot(P, 4, [128, D], BF16, 'xb')
    xTr = Rot(P, 2, [128, 8, 128], BF16, 'xT')
    sgr_ = Rot(P, 2, [128, 256], F32, 'sg')
    acr = Rot(P, 2, [128, 256], BF16, 'ac')
    aTr = Rot(P, 2, [128, 2, 128], BF16, 'aT')
    ybr = Rot(P, 2, [128, D], BF16, 'yb')
    ptx = Rot(P, 1, [128, 8, 128], BF16, 'ptx', psum=True)
    pgu = Rot(P, 2, [128, 512], F32, 'pgu', psum=True)
    pta = Rot(P, 1, [128, 2, 128], BF16, 'pta', psum=True)
    pyd = Rot(P, 3, [128, 512], F32, 'pyd', psum=True)
    regn = [0]

    P.emit(keep=True)
    hold = {}
    blocks = [('r', b) for b in range(NBLK)] + [('s', i) for i in range(NT)]
    st = {}

    ld = {}

    def stageL(kind, b):
        xb, xk = xbr.next()
        if kind == 'r':
            wl, wk = wgu.next()

            def gat(e):
                if 'bc' not in hold:
                    hold['bc'] = e.alloc_register("bc_reg")
                    e.reg_mov(hold['bc'], 65 * 128 - 1)
                return e.indirect_dma_start(out=wl[:], out_offset=None, in_=T['wall16'],
                                            in_offset=bass.IndirectOffsetOnAxis(ap=widx[:, b:b + 1], axis=0),
                                            bounds_check=hold['bc'], oob_is_err=False)
            P.dma('pool', gat, reads=['widx'], writes=[wk])
            P.dma('sp', lambda e: e.dma_start(out=xb[:], in_=T['Xs'][b * 128:(b + 1) * 128, :]), writes=[xk])
        else:
            wl, wk = wsh, 'wsh'
            P.dma('sp', lambda e: e.dma_start(out=xb[:], in_=T['h2tok'][b * 128:(b + 1) * 128, :]), writes=[xk])
        ld[(kind, b)] = (xb, xk, wl, wk)

    def stageA(kind, b):
        xb, xk, wl, wk = ld.pop((kind, b))
        wga, wua = wl[:, 0:2048], wl[:, 2048:4096]
        wd, wdk = wl[:, 4096:6144].rearrange("p (k f) -> p k f", f=D), wk
        px, pxk = ptx.next()
        for k in range(8):
            P.op('pe', lambda e, k=k: e.transpose(out=px[:, k, :], in_=xb[:, k * 128:(k + 1) * 128], identity=identb[:]), reads=[xk], writes=[pxk])
        xT, xTk = xTr.next()
        P.op('act', lambda e: e.activation(out=xT[:], in_=px[:], func=AF.Copy), reads=[pxk], writes=[xTk])
        pg_, pgk = pgu.next()
        for k in range(8):
            mm(P, pg_[:, 0:256], xT[:, k, :], wga[:, k * 256:(k + 1) * 256], k == 0, False, [xTk, wk], [pgk])
        for k in range(8):
            P.op('pe', lambda e, k=k: e.matmul(pg_[:, 256:512], lhsT=xT[:, k, :], rhs=wua[:, k * 256:(k + 1) * 256], start=False, stop=(k == 7), skip_group_check=True), reads=[xTk, wk], writes=[pgk])
        st[(kind, b)] = (pg_, pgk, wd, wdk)

    def stageB(kind, b):
        pg_, pgk, wd, wdk = st.pop((kind, b))
        sg, sgk = sgr_.next(); ac, ack = acr.next()
        P.op('act', lambda e: e.activation(out=sg[:], in_=pg_[:, 0:256], func=AF.Silu), reads=[pgk], writes=[sgk])
        dve(lambda e: e.tensor_tensor(out=ac[:], in0=pg_[:, 256:512], in1=sg[:], op=ALU.mult), [pgk, sgk], [ack])
        pa, pak = pta.next()
        for ft in range(2):
            P.op('pe', lambda e, ft=ft: e.transpose(out=pa[:, ft, :], in_=ac[:, ft * 128:(ft + 1) * 128], identity=identb[:]), reads=[ack], writes=[pak])
        aT, aTk = aTr.next()
        dve(lambda e: e.tensor_copy(out=aT[:], in_=pa[:]), [pak], [aTk])
        yb, ybk = ybr.next()
        for half in range(2):
            py_, pyk = pyd.next()
            cs_ = slice(half * 512, (half + 1) * 512)
            for ft in range(2):
                mm(P, py_[:], aT[:, ft, :], wd[:, ft, cs_], ft == 0, ft == 1, [aTk, wdk], [pyk])
            if half == 0:
                P.op('act', lambda e, py_=py_, cs_=cs_: e.activation(out=yb[:, cs_], in_=py_[:], func=AF.Copy), reads=[pyk], writes=[ybk])
            else:
                dve(lambda e, py_=py_, cs_=cs_: e.tensor_copy(out=yb[:, cs_], in_=py_[:]), [pyk], [ybk])
        dst = T['Ys'] if kind == 'r' else T['Ysh']
        P.dma('sp', lambda e: e.dma_start(out=dst[b * 128:(b + 1) * 128, :], in_=yb[:]), reads=[ybk])

    stageL(*blocks[0])
    stageL(*blocks[1])
    stageA(*blocks[0])
    for bi in range(len(blocks)):
        if bi + 2 < len(blocks):
            stageL(*blocks[bi + 2])
        if bi + 1 < len(blocks):
            stageA(*blocks[bi + 1])
        stageB(*blocks[bi])
    P.emit(keep=True)
    g2b = P.sb([128, D], F32)
    fing = P.sb([128, D], F32)
    P.dma('sp', lambda e: e.dma_start(out=g2b[:], in_=T['modd'][:, 5120:6144]), writes=['g2b'])
    P.dma('sp', lambda e: e.dma_start(out=fing[:], in_=T['final_g'].partition_broadcast(128)), writes=['fing'])
    xr = Rot(P, 2, [128, D], F32, 'x1')
    grot = Rot(P, 4, [128, D], BF16, 'gat')
    shr = Rot(P, 2, [128, D], BF16, 'shr')
    acc = P.sb([128, D], F32)
    junk, ss, rs = P.sb([128, D], F32), P.sb([128, 1], F32), P.sb([128, 1], F32)
    for i in range(NT):
        tsl = slice(i * 128, (i + 1) * 128)
        xt, xk = xr.next()
        sh, shk = shr.next()
        P.dma('sp', lambda e, xt=xt, tsl=tsl: e.dma_start(out=xt[:], in_=T['x1'][tsl, :]), writes=[xk])
        P.dma('act', lambda e, sh=sh, tsl=tsl: e.dma_start(out=sh[:], in_=T['Ysh'][tsl, :]), writes=[shk])
        for k in range(8):
            gt, gtk = grot.next()
            P.dma('pool', lambda e, gt=gt, i=i, k=k: e.indirect_dma_start(
                out=gt[:], out_offset=None, in_=T['Ys'], in_offset=bass.IndirectOffsetOnAxis(ap=d8i[:, i, k:k + 1], axis=0)),
                reads=[f'd8i{i}'], writes=[gtk])
            if k == 0:
                dve(lambda e, gt=gt, i=i, sh=sh: e.scalar_tensor_tensor(out=acc[:], in0=gt[:], scalar=gw8[:, i, 0:1], in1=sh[:], op0=ALU.mult, op1=ALU.add),
                    [gtk, f'gw{i}', shk, 'acc'], ['acc'])
            else:
                dve(lambda e, gt=gt, i=i, k=k: e.scalar_tensor_tensor(out=acc[:], in0=gt[:], scalar=gw8[:, i, k:k + 1], in1=acc[:], op0=ALU.mult, op1=ALU.add),
                    [gtk, f'gw{i}', 'acc'], ['acc'])
        dve(lambda e: e.tensor_tensor(out=acc[:], in0=acc[:], in1=g2b[:], op=ALU.mult), ['acc', 'g2b'], ['acc'])
        P.op('pool', lambda e, xt=xt: e.tensor_tensor(out=xt[:], in0=xt[:], in1=acc[:], op=ALU.add), reads=['acc', xk], writes=[xk])
        rms_rstd(P, xt, xk, junk, ss, rs, 'f')
        dve(lambda e, xt=xt: e.scalar_tensor_tensor(out=xt[:], in0=xt[:], scalar=rs[:, 0:1], in1=fing[:], op0=ALU.mult, op1=ALU.mult),
            [xk, 'rsf', 'fing'], [xk])
        P.dma('sp', lambda e, xt=xt, tsl=tsl: e.dma_start(out=T['out'][tsl, :], in_=xt[:]), reads=[xk])


SCRATCH = [
    ('zq', [512, S], BF16), ('zk', [512, S], BF16), ('zv', [S, 512], BF16), ('ziq', [512, S], BF16),
    ('zik', [32, S], BF16), ('ziw', [S, 16], F32), ('zr', [1792, S], F32), ('zga', [1024, S], BF16),
    ('zgr', [1024, S], BF16), ('modd', [128, 6 * D], F32), ('attnT', [512, S], BF16), ('rwT', [512, S], BF16),
    ('x1', [S, D], F32), ('h2T', [D, S], BF16), ('h2tok', [S, D], BF16), ('meta', [S, 24], F32), ('cntd', [128, 64], F32),
    ('Xs', [NBLK * 128, D], BF16), ('Ys', [NBLK * 128, D], BF16), ('Ysh', [S, D], BF16),
    ('wall16', [65 * 128, 6144], BF16),
]

INPUT_SHAPES = [
    ('x', [S, D]), ('c_col', [128, 8]), ('ada_w', [D, 6 * D]), ('ada_b', [1, 6 * D]), ('norm1_g', [D]),
    ('w_in', [D, NIN]), ('rel_bias', [256]), ('tshift_mu', [1792]), ('decay_w0', [512]), ('decay_up', [64, 512]),
    ('iclr_a0', [512]), ('iclr_up', [64, 512]), ('gate_up', [128, 512]), ('k_k', [512]), ('k_a', [512]),
    ('r_k', [512]), ('lnx_g', [512]), ('lnx_b', [512]), ('w_attn_br', [512, D]), ('w_rwkv_br', [512, D]),
    ('w_out', [D, D]), ('norm2_g', [D]), ('router_w', [D, 64]), ('router_bias', [64]),
    ('exp_gate', [64, D, 256]), ('exp_up', [64, D, 256]), ('exp_down', [64, 256, D]),
    ('sh_gate', [D, 256]), ('sh_up', [D, 256]), ('sh_down', [256, D]), ('final_g', [D]),
    ('k_ident', [128, 128]), ('k_rel', [128, 256]), ('k_masks', [128, 896]), ('k_reset', [64, 512]), ('k_bst', [NBLK]),
]


def build(debug_outs=(), stop_after=None):
    nc = bass.Bass("TRN2", target_bir_lowering=False)
    T = {}
    for name, shp in INPUT_SHAPES:
        T[name] = nc.dram_tensor(name, shp, F32, kind="ExternalInput").ap()
    for name, shp, dt in SCRATCH:
        kind = "ExternalOutput" if name in debug_outs else "Internal"
        T[name] = nc.dram_tensor(name, shp, dt, kind=kind).ap()
    T['out'] = nc.dram_tensor('out', [S, D], F32, kind="ExternalOutput").ap()
    if 'dbg_y' in debug_outs:
        T['dbg_y'] = nc.dram_tensor('dbg_y', [64, 8, 512], F32, kind="ExternalOutput").ap()
        T['dbg_bon'] = nc.dram_tensor('dbg_bon', [64, 8, 512], BF16, kind="ExternalOutput").ap()
        T['dbg_g'] = nc.dram_tensor('dbg_g', [64, 8, 512], BF16, kind="ExternalOutput").ap()
        T['dbg_AR'] = nc.dram_tensor('dbg_AR', [64, 8, 4, 256], BF16, kind="ExternalOutput").ap()
        T['dbg_BK'] = nc.dram_tensor('dbg_BK', [64, 8, 4, 256], BF16, kind="ExternalOutput").ap()
    P = Prog(nc)
    G = {}
    G['E'] = P.gsb([128, 8, 256], F32)
    G['b31'] = P.gsb([128, 8], F32)
    G['ones_row'] = P.gsb([1, 128], F32)
    G['identf'] = P.gsb([128, 128], F32)
    G['identb'] = P.gsb([128, 128], BF16)
    G['eps'] = P.gsb([128, 1], F32)
    G_EPS[0] = G['eps']
    P.op('dve', lambda e: e.memset(G['ones_row'][:], 1.0), writes=['ones_row'])
    P.op('dve', lambda e: e.memset(G['eps'][:], 1e-6), writes=['eps'])
    P.dma('sp', lambda e: e.dma_start(out=G['identf'][:], in_=T['k_ident']), writes=['identf'])
    P.op('dve', lambda e: e.tensor_copy(out=G['identb'][:], in_=G['identf'][:]), reads=['identf'], writes=['identb'])
    G['sparse'] = SPARSE
    phase_A(P, T, G)
    P.emit()
    if stop_after == 'A':
        P.emit(); P.finish(); return nc
    G['sparse'] = SPARSE
    phase_BC(P, T, G)
    P.emit()
    if stop_after == 'C':
        P.finish(); return nc
    phase_D0(P, T, G)
    P.emit()
    phase_D(P, T, G)
    P.emit()
    if stop_after == 'D':
        P.finish(); return nc
    phase_E(P, T, G)
    P.emit()
    if stop_after == 'E':
        P.finish(); return nc
    G['sparse'] = SPARSE
    phase_F(P, T, G)
    P.emit()
    if stop_after == 'F':
        P.finish(); return nc
    if SPARSE:
        phase_S(P, T, G)
        P.emit()
        P.finish()
        return nc
    G['yacc'] = P.gsb([128, NT, D], F32)
    if stop_after in ('router', 'exp1'):
        G['gstop'] = stop_after
        G['nexp'] = 1
    if stop_after == 'router':
        phase_G(P, T, G); P.emit(); P.finish(); return nc
    if stop_after == 'exp1':
        phase_G(P, T, G); P.emit(); P.finish(); return nc
    phase_G(P, T, G)
    P.emit()
    phase_H(P, T, G)
    P.emit()
    P.finish()
    return nc


def host_inputs(inputs, b):
    m = {}
    f = lambda a: np.ascontiguousarray(np.asarray(a, dtype=np.float32))
    m['x'] = f(inputs['x'][b])
    m['c_col'] = f(np.asarray(inputs['c'][b]).reshape(8, 128).T)
    for name, shp in INPUT_SHAPES:
        if name in ('x', 'c_col', 'k_ident', 'k_rel', 'k_masks', 'k_reset', 'k_bst'):
            continue
        a = np.asarray(inputs[name])
        m[name] = f(a.reshape(shp))
    m['k_ident'] = np.eye(128, dtype=np.float32)
    ii = np.arange(128)
    su = (ii[:, None] < ii[None, :]).astype(np.float32)
    iu = (ii[:, None] <= ii[None, :]).astype(np.float32)
    sl = (ii[:, None] > ii[None, :]).astype(np.float32)
    m['k_masks'] = np.ascontiguousarray(np.concatenate([su, iu, su, iu, sl, np.zeros((128, 256), np.float32)], axis=1))
    rs_ = np.ones((64, 512), np.float32); rs_[:, ::128] = 0.0
    m['k_reset'] = rs_
    m['k_bst'] = (np.arange(NBLK, dtype=np.float32) * 128.0)
    m['k_rel'] = (np.arange(256, dtype=np.float32)[None, :] - np.arange(128, dtype=np.float32)[:, None])
    return m


def kernel(**inputs):
    nc = build()
    in_maps = [host_inputs(inputs, b) for b in range(8)]
    res = run_bass_kernel_spmd(nc, in_maps, core_ids=list(range(8)))
    return np.stack([np.asarray(r['out']) for r in res.results], axis=0).astype(np.float32)
```

```python
import contextlib
import numpy as np
import concourse.bass as bass
import concourse.mybir as mybir

F32 = mybir.dt.float32
BF16 = mybir.dt.bfloat16
I32 = mybir.dt.int32
U32 = mybir.dt.uint32
AF = mybir.ActivationFunctionType
ALU = mybir.AluOpType
AX = mybir.AxisListType

N_DMA_SEMS = 8


class Prog:
    ENGS = ('pe', 'act', 'dve', 'pool', 'sp')

    def __init__(self, nc):
        self.nc = nc
        self.ops = {e: [] for e in self.ENGS}
        self.cnt = {e: 0 for e in self.ENGS}
        self.waited = {e: {} for e in self.ENGS}
        self.res = {}
        self.dma_tot = [0] * N_DMA_SEMS
        self.dma_rr = 0
        self.stack = contextlib.ExitStack()
        self.gstack = contextlib.ExitStack()
        self.nsb = 0
        self.nphase = 0
        self.sems = None

    def _new_sems(self):
        nc = self.nc
        self.sems = {}
        for e in self.ENGS:
            self.sems[e] = self.gstack.enter_context(nc.semaphore(f"s_{e}_{self.nphase}"))
        for i in range(N_DMA_SEMS):
            self.sems[('dma', i)] = self.gstack.enter_context(nc.semaphore(f"s_dma{i}_{self.nphase}"))
        self.cnt = {e: 0 for e in self.ENGS}
        self.waited = {e: {} for e in self.ENGS}
        self.dma_tot = [0] * N_DMA_SEMS
        self.dma_rr = 0

    def gsb(self, shape, dt, name=None):
        self.nsb += 1
        return self.gstack.enter_context(self.nc.sbuf_tensor(name or f"gsb{self.nsb}", list(shape), dt))

    def sb(self, shape, dt, name=None):
        self.nsb += 1
        return self.stack.enter_context(self.nc.sbuf_tensor(name or f"sb{self.nsb}", list(shape), dt))

    def ps(self, shape, dt, name=None):
        self.nsb += 1
        return self.stack.enter_context(self.nc.psum_tensor(name or f"ps{self.nsb}", list(shape), dt))

    def _deps(self, reads, writes):
        deps = {}
        def add(d):
            if d is None:
                return
            k, v = d
            if deps.get(k, 0) < v:
                deps[k] = v
        for k in reads:
            r = self.res.get(k)
            if r:
                add(r['w'])
        for k in writes:
            r = self.res.get(k)
            if r:
                add(r['w'])
                for d in r['r']:
                    add(d)
        return deps

    def _emit_waits(self, eng, deps):
        w = self.waited[eng]
        for k, v in deps.items():
            if w.get(k, 0) < v:
                w[k] = v
                self.ops[eng].append(('wait', k, v))

    def _update(self, dep, reads, writes):
        for k in reads:
            r = self.res.setdefault(k, {'w': None, 'r': []})
            r['r'] = [d for d in r['r'] if d[0] != dep[0]] + [dep]
        for k in writes:
            self.res[k] = {'w': dep, 'r': []}

    def op(self, eng, fn, reads=(), writes=()):
        if self.sems is None:
            self._new_sems()
        deps = self._deps(reads, writes)
        if eng == 'pe':
            deps.pop('pe', None)
        self._emit_waits(eng, deps)
        self.cnt[eng] += 1
        dep = (eng, self.cnt[eng])
        self.ops[eng].append(('op', fn))
        self._update(dep, reads, writes)
        return dep

    def dma(self, q, fn, reads=(), writes=()):
        if self.sems is None:
            self._new_sems()
        deps = self._deps(reads, writes)
        s = self.dma_rr
        self.dma_rr = (self.dma_rr + 1) % N_DMA_SEMS
        key = ('dma', s)
        if self.dma_tot[s] > 0:
            if deps.get(key, 0) < self.dma_tot[s]:
                deps[key] = self.dma_tot[s]
        self._emit_waits(q, deps)
        self.dma_tot[s] += 16
        dep = (key, self.dma_tot[s])
        self.ops[q].append(('dma', fn, s))
        self._update(dep, reads, writes)
        return dep

    def emit(self, keep=False):
        nc = self.nc
        with contextlib.ExitStack() as st:
            sems = self.sems
            self.nphase += 1
            block = st.enter_context(nc.Block(f"ph{self.nphase}"))
            engobj = {'pe': nc.tensor, 'act': nc.scalar, 'dve': nc.vector, 'pool': nc.gpsimd, 'sp': nc.sync}
            fin = {}
            for e in self.ENGS:
                if e != 'sp' and self.cnt[e] > 0:
                    fin[e] = self.cnt[e]
            for i in range(N_DMA_SEMS):
                if self.dma_tot[i] > 0:
                    fin[('dma', i)] = self.dma_tot[i]
            self._emit_waits('sp', fin)

            def run(e):
                eo = engobj[e]
                for item in self.ops[e]:
                    if item[0] == 'wait':
                        eo.wait_ge(sems[item[1]], item[2])
                    elif item[0] == 'op':
                        item[1](eo).then_inc(sems[e], 1)
                    else:
                        item[1](eo).then_inc(sems[('dma', item[2])], 16)

            @block.tensor
            def _(t):
                run('pe')

            @block.scalar
            def _(t):
                run('act')

            @block.vector
            def _(t):
                run('dve')

            @block.gpsimd
            def _(t):
                run('pool')

            @block.sync
            def _(t):
                run('sp')
        self.ops = {e: [] for e in self.ENGS}
        self.res = {}
        if keep:
            return
        self.stack.close()
        self.stack = contextlib.ExitStack()
        self.sems = None

    def finish(self):
        self.gstack.close()

from concourse.bass_utils import run_bass_kernel_spmd
import ml_dtypes

SPARSE = True
S = 4096
D = 1024
NT = S // 128
NIN = 5936


class Rot:
    def __init__(self, P, n, shape, dt, name, psum=False):
        self.tiles = [(P.ps(shape, dt) if psum else P.sb(shape, dt)) for _ in range(n)]
        self.name = name
        self.i = 0

    def next(self):
        t = self.tiles[self.i % len(self.tiles)]
        k = f"{self.name}{self.i % len(self.tiles)}"
        self.i += 1
        return t, k


class Rot2(Rot):
    def __init__(self, P, n, shape, dt, name):
        self.tiles = [(P.sb(shape, dt), P.sb(shape, dt)) for _ in range(n)]
        self.name = name
        self.i = 0


def mm(P, out, lhsT, rhs, start, stop, reads, writes):
    P.op('pe', lambda e: e.matmul(out, lhsT=lhsT, rhs=rhs, start=start, stop=stop), reads=reads, writes=writes)


def phase_A(P, T, G):
    mod_bc = P.sb([128, 6 * D], F32)
    ccol = P.sb([128, 8], F32)
    scol = P.sb([128, 8], F32)
    adab = P.sb([1, 6144], F32)
    modrow = P.sb([1, 6144], F32)
    ngb = P.sb([128, 1024], F32)
    P.dma('sp', lambda e: e.dma_start(out=ccol[:], in_=T['c_col']), writes=['ccol'])
    P.dma('sp', lambda e: e.dma_start(out=adab[:], in_=T['ada_b']), writes=['adab'])
    P.op('act', lambda e: e.activation(out=scol[:], in_=ccol[:], func=AF.Silu), reads=['ccol'], writes=['scol'])
    wrot = Rot(P, 2, [128, 8, 512], F32, 'aw')
    psr = Rot(P, 2, [1, 512], F32, 'psr', psum=True)
    psb = Rot(P, 2, [128, 512], F32, 'psb', psum=True)
    adaw = T['ada_w'].rearrange("(k p) n -> p k n", p=128)
    for n in range(12):
        wb, wk = wrot.next()
        P.dma('sp' if n % 2 == 0 else 'act',
              lambda e, wb=wb, n=n: e.dma_start(out=wb[:], in_=adaw[:, :, n * 512:(n + 1) * 512]), writes=[wk])
        pr, pk = psr.next()
        for k in range(8):
            mm(P, pr[:], scol[:, k:k + 1], wb[:, k, :], k == 0, k == 7, [wk, 'scol'], [pk])
        sl = slice(n * 512, (n + 1) * 512)
        P.op('dve', lambda e, pr=pr, sl=sl: e.tensor_tensor(out=modrow[0:1, sl], in0=pr[:], in1=adab[0:1, sl], op=ALU.add),
             reads=[pk, 'adab'], writes=[f'modrow{n}'])
        pb, pbk = psb.next()
        mm(P, pb[:], G['ones_row'][:], modrow[0:1, sl], True, True, [f'modrow{n}'], [pbk])
        P.op('act', lambda e, pb=pb, sl=sl: e.activation(out=mod_bc[:, sl], in_=pb[:], func=AF.Copy),
             reads=[pbk], writes=[f'mod{n}'])
    for (gname, c0, deps) in (('norm1_g', 1024, ['mod2', 'mod3']), ('norm2_g', 4096, ['mod8', 'mod9'])):
        P.dma('sp', lambda e, gname=gname: e.dma_start(out=ngb[:], in_=T[gname].partition_broadcast(128)), writes=['ngb'])
        P.op('dve', lambda e, c0=c0: e.scalar_tensor_tensor(out=mod_bc[:, c0:c0 + 1024], in0=mod_bc[:, c0:c0 + 1024],
                                                            scalar=1.0, in1=ngb[:], op0=ALU.add, op1=ALU.mult),
             reads=deps + ['ngb'], writes=deps)
    P.dma('sp', lambda e: e.dma_start(out=T['modd'], in_=mod_bc[:]), reads=[f'mod{n}' for n in range(12)])


def rms_rstd(P, xt, xk, junk, ss, rs, tag):
    P.op('act', lambda e: e.activation(out=junk[:], in_=xt[:], func=AF.Square, accum_out=ss[:]),
         reads=[xk], writes=['junk' + tag, 'ss' + tag])
    P.op('act', lambda e: e.activation(out=ss[:], in_=ss[:], func=AF.Sqrt, scale=1.0 / D, bias=G_EPS[0][:, 0:1]),
         reads=['ss' + tag], writes=['ss' + tag])
    P.op('dve', lambda e: e.reciprocal(out=rs[:], in_=ss[:]), reads=['ss' + tag], writes=['rs' + tag])


G_EPS = [None]


def norm_mod_transpose(P, G, xt, xk, hT, i, g_sl, sh_sl, W):
    mod_bc = W['mod']
    junk, ss, rs, t1, hb, pt = W['junk'], W['ss'], W['rs'], W['t1'], W['hb'], W['pt']
    rms_rstd(P, xt, xk, junk, ss, rs, '')
    P.op('dve', lambda e: e.scalar_tensor_tensor(out=t1[:], in0=xt[:], scalar=rs[:, 0:1], in1=mod_bc[:, g_sl],
                                                 op0=ALU.mult, op1=ALU.mult), reads=[xk, 'rs', 'modl'], writes=['t1'])
    P.op('pool', lambda e: e.tensor_tensor(out=hb[:], in0=t1[:], in1=mod_bc[:, sh_sl], op=ALU.add),
         reads=['t1', 'modl'], writes=['hb'])
    for k in range(8):
        P.op('pe', lambda e, k=k: e.transpose(out=pt[:, k, :], in_=hb[:, k * 128:(k + 1) * 128], identity=G['identb'][:]),
             reads=['hb'], writes=['pt'])
    P.op('act', lambda e: e.activation(out=hT[:, :, i * 128:(i + 1) * 128], in_=pt[:], func=AF.Copy),
         reads=['pt'], writes=[f'hT{i // 4}'])


def phase_BC(P, T, G):
    hT = P.sb([128, 8, S], BF16)
    W = dict(junk=P.sb([128, D], F32), ss=P.sb([128, 1], F32), rs=P.sb([128, 1], F32), t1=P.sb([128, D], F32),
             hb=P.sb([128, D], BF16), pt=P.ps([128, 8, 128], BF16))
    W['mod'] = P.sb([128, 2048], F32)
    P.dma('sp', lambda e: e.dma_start(out=W['mod'][:], in_=T['modd'][:, 0:2048]), writes=['modl'])
    xrot = Rot(P, 2, [128, D], F32, 'x')
    s0 = phase_D0(P, T, G)

    def s0step(n=1):
        for _ in range(n):
            try:
                next(s0)
            except StopIteration:
                return
    for i in range(NT):
        xt, xk = xrot.next()
        P.dma('sp', lambda e, xt=xt, i=i: e.dma_start(out=xt[:], in_=T['x'][i * 128:(i + 1) * 128, :]), writes=[xk])
        norm_mod_transpose(P, G, xt, xk, hT, i, slice(1024, 2048), slice(0, 1024), W)
        s0step()
    win = T['w_in'].rearrange("(k p) n -> p k n", p=128)
    segs = [('zq', 0, 512, BF16), ('zk', 512, 512, BF16), ('ziq', 1536, 512, BF16), ('zik', 2048, 32, BF16),
            ('zr', 2096, 1792, F32), ('zga', 3888, 1024, BF16), ('zgr', 4912, 1024, BF16)]
    wrot = Rot(P, 2, [128, 8, 128], BF16, 'w')
    psrot = Rot(P, 3, [128, 512], F32, 'ps', psum=True)
    strot = {BF16: Rot(P, 3, [128, 512], BF16, 'stb'), F32: Rot(P, 3, [128, 512], F32, 'stf')}
    ev = 0
    for (name, c0, n, dt) in segs:
        for m0 in range(0, n, 128):
            M = min(128, n - m0)
            wt, wk = wrot.next()
            P.dma('pool', lambda e, wt=wt, M=M, a=c0 + m0: e.dma_start(out=wt[:, :, :M], in_=win[:, :, a:a + M]), writes=[wk])
            for tg in range(8):
                ps, pk = psrot.next()
                for k in range(8):
                    mm(P, ps[:M, :], wt[:, k, :M], hT[:, k, tg * 512:(tg + 1) * 512], k == 0, k == 7, [wk, f'hT{tg}'], [pk])
                st, sk = strot[dt].next()
                if ev % 2 == 0:
                    P.op('act', lambda e, st=st, ps=ps, M=M: e.activation(out=st[:M, :], in_=ps[:M, :], func=AF.Copy),
                         reads=[pk], writes=[sk])
                else:
                    P.op('dve', lambda e, st=st, ps=ps, M=M: e.tensor_copy(out=st[:M, :], in_=ps[:M, :]),
                         reads=[pk], writes=[sk])
                ev += 1
                if ev % 2 == 0:
                    s0step()
                P.dma('sp', lambda e, st=st, M=M, name=name, m0=m0, tg=tg:
                      e.dma_start(out=T[name][m0:m0 + M, tg * 512:(tg + 1) * 512], in_=st[:M, :]), reads=[sk])
    wv = P.sb([128, 8, 528], BF16)
    P.dma('pool', lambda e: e.dma_start(out=wv[:, :, 0:512], in_=win[:, :, 1024:1536]), writes=['wv'])
    P.dma('pool', lambda e: e.dma_start(out=wv[:, :, 512:528], in_=win[:, :, 2080:2096]), writes=['wv2'])
    ps2rot = Rot(P, 2, [128, 16], F32, 'ps2', psum=True)
    st2rot = Rot(P, 2, [128, 16], F32, 'st2')
    for i in range(NT):
        ps, pk = psrot.next()
        ps2, pk2 = ps2rot.next()
        for k in range(8):
            mm(P, ps[:], hT[:, k, i * 128:(i + 1) * 128], wv[:, k, 0:512], k == 0, k == 7, ['wv', f'hT{i // 4}'], [pk])
        for k in range(8):
            mm(P, ps2[:], hT[:, k, i * 128:(i + 1) * 128], wv[:, k, 512:528], k == 0, k == 7, ['wv2', f'hT{i // 4}'], [pk2])
        st, sk = strot[BF16].next()
        st2, sk2 = st2rot.next()
        P.op('act', lambda e, st=st, ps=ps: e.activation(out=st[:], in_=ps[:], func=AF.Copy), reads=[pk], writes=[sk])
        P.op('dve', lambda e, st2=st2, ps2=ps2: e.tensor_copy(out=st2[:], in_=ps2[:]), reads=[pk2], writes=[sk2])
        P.dma('sp', lambda e, st=st, i=i: e.dma_start(out=T['zv'][i * 128:(i + 1) * 128, :], in_=st[:]), reads=[sk])
        P.dma('sp', lambda e, st2=st2, i=i: e.dma_start(out=T['ziw'][i * 128:(i + 1) * 128, :], in_=st2[:]), reads=[sk2])
    s0step(1000)


def t5_lo_bounds():
    n = np.arange(256)
    nf = np.maximum(n, 1).astype(np.float32)
    large = 16 + (np.log(nf / np.float32(16)) / np.float32(np.log(8.0)) * np.float32(16)).astype(np.int32)
    large = np.minimum(large, 31)
    bk = np.where(n < 16, n, large)
    return [int(np.min(np.nonzero(bk >= b)[0])) for b in range(1, 32)]


def phase_D0(P, T, G):
    relb = P.sb([128, 256], F32)
    diff = P.sb([128, 248], F32)
    base = P.sb([128, 8], F32)
    relidx = P.sb([128, 256], F32)
    P.dma('sp', lambda e: e.dma_start(out=relb[:], in_=T['rel_bias'].partition_broadcast(128)), writes=['relb'])
    P.dma('sp', lambda e: e.dma_start(out=relidx[:], in_=T['k_rel']), writes=['relidx'])
    P.op('dve', lambda e: e.tensor_tensor(out=diff[:], in0=relb[:, 8:256], in1=relb[:, 0:248], op=ALU.subtract),
         reads=['relb'], writes=['diff'])
    P.op('dve', lambda e: e.tensor_tensor(out=base[:], in0=relb[:, 0:8], in1=relb[:, 248:256], op=ALU.subtract),
         reads=['relb'], writes=['base'])
    P.op('dve', lambda e: e.tensor_copy(out=G['b31'][:], in_=relb[:, 248:256]), reads=['relb'], writes=['b31'])
    E = G['E']
    irot = Rot(P, 2, [128, 256], F32, 'ind')
    los = t5_lo_bounds()
    for b in range(1, 32):
        ind, ik = irot.next()
        P.op('dve', lambda e, ind=ind, lo=float(los[b - 1]): e.tensor_scalar(out=ind[:], in0=relidx[:], scalar1=lo, scalar2=None,
                                                                              op0=ALU.is_ge), reads=['relidx'], writes=[ik])
        for h in range(8):
            if b == 1:
                P.op('dve', lambda e, ind=ind, h=h: e.tensor_scalar(out=E[:, h, :], in0=ind[:], scalar1=diff[:, h:h + 1],
                                                                    scalar2=base[:, h:h + 1], op0=ALU.mult, op1=ALU.add),
                     reads=[ik, 'diff', 'base'], writes=[f'E{h}'])
            else:
                c = (b - 1) * 8 + h
                P.op('dve', lambda e, ind=ind, h=h, c=c: e.scalar_tensor_tensor(out=E[:, h, :], in0=ind[:], scalar=diff[:, c:c + 1],
                                                                               in1=E[:, h, :], op0=ALU.mult, op1=ALU.add),
                     reads=[ik, 'diff', f'E{h}'], writes=[f'E{h}'])
        yield
    P.op('act', lambda e: e.activation(out=E[:], in_=E[:], func=AF.Exp), reads=[f'E{h}' for h in range(8)],
         writes=[f'E{h}' for h in range(8)])


def phase_D(P, T, G):
    NIT = 14
    KT = P.sb([64, 8, S], BF16)
    V = P.sb([128, NT, 512], BF16)
    ik4 = P.sb([128, S], BF16)
    iw = P.sb([128, NT, 16], F32)
    ones64 = P.sb([128, 64], BF16)
    scs = [P.sb([128, S], F32), P.sb([128, S], F32)]
    maskbs = [P.sb([128, S], BF16), P.sb([128, S], BF16)]
    maskT = P.sb([128, NT, 128], BF16)
    lo, hi, mid, cnt, tmp, thr = [P.sb([128, 1], F32) for _ in range(6)]
    cvec = P.sb([128, NIT + 1], F32)
    dk = P.sb([128, NIT + 1], F32)
    for k in range(NIT + 1):
        P.op('pool', lambda e, k=k: e.memset(cvec[:, k:k + 1], 2.0 ** -(k + 1)), reads=['cvec'], writes=['cvec'])
    P.dma('sp', lambda e: e.dma_start(out=KT[:], in_=T['zk'].rearrange("(h p) t -> p h t", p=64)), writes=['KT'])
    P.dma('act', lambda e: e.dma_start(out=V[:], in_=T['zv'].rearrange("(i p) f -> p i f", p=128)), writes=['V'])
    for i in range(3):
        P.dma('sp', lambda e, i=i: e.dma_start(out=ik4[32 * i:32 * i + 32, :], in_=T['zik']), writes=[f'ik4{i}'])
    P.dma('sp', lambda e: e.dma_start(out=iw[:], in_=T['ziw'].rearrange("(i p) f -> p i f", p=128)), writes=['iw'])
    P.op('dve', lambda e: e.memset(ones64[:], 1.0), writes=['ones64'])
    zq = T['zq'].rearrange("(h p) t -> p h t", p=64)
    ziq = T['ziq'][0:480, :].rearrange("(j p) t -> p j t", p=96)
    attnT = T['attnT'].rearrange("(h p) t -> p h t", p=64)
    qrot = Rot(P, 2, [64, 8, 128], BF16, 'q')
    iqrot = Rot(P, 2, [96, 6, 128], BF16, 'iq')
    dgrot = Rot(P, 2, [128, 16, 128], BF16, 'dg')
    rrot = Rot(P, 3, [128, 512], BF16, 'r')
    psi = Rot(P, 2, [128, 512], F32, 'psi', psum=True)
    pacc = Rot(P, 1, [128, 512], F32, 'pacc', psum=True)
    pss = Rot(P, 3, [128, 4, 128], F32, 'pss', psum=True)
    ptm = Rot(P, 1, [128, 4, 128], BF16, 'ptm', psum=True)
    pod = Rot(P, 1, [64, 512], F32, 'pod', psum=True)
    pTrot = Rot(P, 3, [128, 4, 128], BF16, 'pT')
    atrot = Rot(P, 2, [64, 8, 128], BF16, 'at')
    rdrot = Rot(P, 2, [64, 128], F32, 'rd')
    E, b31 = G['E'], G['b31']
    identb = G['identb']

    def indexer(qi):
        sc, sck = scs[qi % 2], f'sc{qi % 2}'
        n = 128 * (qi + 1)
        tsl = slice(qi * 128, (qi + 1) * 128)
        iqt, iqk = iqrot.next()
        P.dma('act', lambda e: e.dma_start(out=iqt[:, 0:5, :], in_=ziq[:, :, tsl]), writes=[iqk])
        P.dma('act', lambda e: e.dma_start(out=iqt[0:32, 5, :], in_=T['ziq'][480:512, tsl]), writes=[iqk + 'b'])
        dg, dgk = dgrot.next()
        for h in range(16):
            P.op('pool', lambda e, h=h: e.tensor_scalar(out=dg[:, h, :], in0=identb[:], scalar1=iw[:, qi, h:h + 1], scalar2=0.0, op0=ALU.mult, op1=ALU.add),
                 reads=['iw'], writes=[dgk])
        for ch in range((n + 511) // 512):
            c0 = ch * 512
            nc_ = min(512, n - c0)
            pa, pak = pacc.next()
            pend = []

            def acc(h, r, rk):
                mm(P, pa[:, :nc_], dg[:, h, :], r[:, :nc_], h == 0, h == 15, [dgk, rk], [pak])
            for h in range(16):
                j, i = divmod(h, 3)
                ps, pk = psi.next()
                mm(P, ps[:, :nc_], iqt[32 * i:32 * i + 32, j, :], ik4[32 * i:32 * i + 32, c0:c0 + nc_], True, True,
                   [iqk, iqk + 'b', f'ik4{i}'], [pk])
                r, rk = rrot.next()
                P.op('act', lambda e, r=r, ps=ps, nc_=nc_: e.activation(out=r[:, :nc_], in_=ps[:, :nc_], func=AF.Relu), reads=[pk], writes=[rk])
                pend.append((h, r, rk))
                if len(pend) > 1:
                    acc(*pend.pop(0))
                yield
            while pend:
                acc(*pend.pop(0))
            P.op('dve', lambda e, pa=pa, c0=c0, nc_=nc_: e.tensor_copy(out=sc[:, c0:c0 + nc_], in_=pa[:, :nc_]), reads=[pak], writes=[sck])
        P.op('pool', lambda e: e.affine_select(out=sc[:, tsl], in_=sc[:, tsl], pattern=[[-1, 128]], compare_op=ALU.is_ge, fill=-1e30,
                                               base=0, channel_multiplier=1), reads=[sck], writes=[sck])

    def threshold(qi):
        sc, sck = scs[qi % 2], f'sc{qi % 2}'
        n = 128 * (qi + 1)
        maskb, mbk = maskbs[qi % 2], f'maskb{qi % 2}'
        dve = lambda fn, r, w: P.op('dve', fn, reads=r, writes=w)
        if n <= 256:
            dve(lambda e: e.memset(thr[:], -1e29), ['thr'], ['thr'])
        else:
            nv = 128 * qi
            dve(lambda e: e.tensor_reduce(out=lo[:], in_=sc[:, :nv], axis=AX.X, op=ALU.min), [sck, 'lo'], ['lo'])
            dve(lambda e: e.tensor_reduce(out=hi[:], in_=sc[:, :n], axis=AX.X, op=ALU.max), [sck, 'hi'], ['hi'])
            dve(lambda e: e.tensor_tensor(out=hi[:], in0=hi[:], in1=lo[:], op=ALU.subtract), ['hi', 'lo'], ['hi'])
            dve(lambda e: e.tensor_scalar(out=dk[:], in0=cvec[:], scalar1=hi[:, 0:1], scalar2=None, op0=ALU.mult), ['cvec', 'hi', 'dk'], ['dk'])
            dve(lambda e: e.tensor_tensor(out=mid[:], in0=lo[:], in1=dk[:, 0:1], op=ALU.add), ['lo', 'dk', 'mid'], ['mid'])
            for k in range(NIT):
                dve(lambda e: e.tensor_scalar(out=maskb[:, :n], in0=sc[:, :n], scalar1=mid[:, 0:1], scalar2=None,
                                              op0=ALU.is_ge, op1=ALU.add, accum_out=cnt[:]), [sck, 'mid', 'cnt', mbk], [mbk, 'cnt'])
                dve(lambda e: e.tensor_scalar(out=tmp[:], in0=cnt[:], scalar1=255.5, scalar2=-0.5, op0=ALU.is_ge, op1=ALU.add),
                    ['cnt', 'tmp'], ['tmp'])
                dve(lambda e, k=k: e.scalar_tensor_tensor(out=mid[:], in0=tmp[:], scalar=dk[:, k:k + 1], in1=mid[:], op0=ALU.mult, op1=ALU.add),
                    ['tmp', 'dk', 'mid'], ['mid'])
                yield
            dve(lambda e: e.tensor_tensor(out=thr[:], in0=mid[:], in1=dk[:, NIT:NIT + 1], op=ALU.subtract), ['mid', 'dk', 'thr'], ['thr'])
        dve(lambda e: e.tensor_scalar(out=maskb[:, :n], in0=sc[:, :n], scalar1=thr[:, 0:1], scalar2=None, op0=ALU.is_ge),
            [sck, 'thr', mbk], [mbk])
        yield

    def attention(qi):
        LOOK = 2
        nkt = qi + 1
        nch = (nkt + 3) // 4
        tsl = slice(qi * 128, (qi + 1) * 128)
        mb = maskbs[qi % 2]
        mbk = f'maskb{qi % 2}'
        qt, qk = qrot.next()
        P.dma('sp', lambda e: e.dma_start(out=qt[:], in_=zq[:, :, tsl]), writes=[qk])
        for c4 in range(nch):
            kts = list(range(4 * c4, min(4 * c4 + 4, nkt)))
            pm, pmk = ptm.next()
            for kt in kts:
                P.op('pe', lambda e, pm=pm, kt=kt: e.transpose(out=pm[:, kt % 4, :], in_=mb[:, kt * 128:(kt + 1) * 128],
                                                                identity=identb[:]), reads=[mbk], writes=[pmk])
            P.op('act', lambda e, pm=pm, kts=kts: e.activation(out=maskT[:, kts[0]:kts[-1] + 1, :], in_=pm[:, :len(kts), :],
                                                               func=AF.Copy), reads=[pmk], writes=['maskT'])
        at, atk = atrot.next()
        items = [(h, c4) for h in range(8) for c4 in range(nch)]
        qkd = {}
        hst = {}

        def emit_qk(h, c4):
            kts = list(range(4 * c4, min(4 * c4 + 4, nkt)))
            ps, pk = pss.next()
            for kt in kts:
                mm(P, ps[:, kt % 4, :], KT[:, h, kt * 128:(kt + 1) * 128], qt[:, h, :], True, True, ['KT', qk], [pk])
            qkd[(h, c4)] = (ps, pk)

        def emit_rest(h, c4):
            kts = list(range(4 * c4, min(4 * c4 + 4, nkt)))
            nk = len(kts)
            ps, pk = qkd.pop((h, c4))
            if c4 == 0:
                hst[h] = pod.next()
            po, pok = hst[h]
            pT, pTk = pTrot.next()
            P.op('act', lambda e: e.activation(out=pT[:, :nk, :], in_=ps[:, :nk, :], func=AF.Exp, scale=0.125, bias=b31[:, h:h + 1]),
                 reads=[pk], writes=[pTk])
            P.op('dve', lambda e: e.tensor_tensor(out=pT[:, :nk, :], in0=pT[:, :nk, :], in1=maskT[:, kts[0]:kts[-1] + 1, :], op=ALU.mult),
                 reads=[pTk, 'maskT'], writes=[pTk])
            for kt in kts:
                dl = qi - kt
                if dl <= 1:
                    P.op('dve', lambda e, kt=kt, dl=dl: e.tensor_tensor(
                        out=pT[:, kt % 4, :], in0=pT[:, kt % 4, :], in1=E[:, h, dl * 128:(dl + 1) * 128], op=ALU.mult),
                        reads=[pTk], writes=[pTk])
            for kt in kts:
                P.op('pe', lambda e, kt=kt: e.matmul(po[:, 0:128], lhsT=V[:, kt, h * 64:(h + 1) * 64], rhs=pT[:, kt % 4, :],
                                                     start=(kt == 0), stop=(kt == nkt - 1), skip_group_check=True),
                     reads=['V', pTk], writes=[pok])
                P.op('pe', lambda e, kt=kt: e.matmul(po[:, 128:256], lhsT=ones64[:], rhs=pT[:, kt % 4, :],
                                                     start=False, stop=(kt == nkt - 1), skip_group_check=True),
                     reads=['ones64', pTk], writes=[pok])
            if c4 == nch - 1:
                rd, rdk = rdrot.next()
                P.op('dve', lambda e: e.reciprocal(out=rd[:], in_=po[:, 128:256]), reads=[pok], writes=[rdk])
                P.op('dve', lambda e: e.tensor_tensor(out=at[:, h, :], in0=po[:, 0:128], in1=rd[:], op=ALU.mult),
                     reads=[pok, rdk], writes=[atk])

        for idx in range(len(items) + LOOK):
            if idx < len(items):
                emit_qk(*items[idx])
            if idx >= LOOK:
                emit_rest(*items[idx - LOOK])
                yield
        P.dma('sp', lambda e: e.dma_start(out=attnT[:, :, tsl], in_=at[:]), reads=[atk])

    def drain(g):
        for _ in g:
            pass

    def merge(gens):
        gens = [[g, max(1, n), 0.0, True] for g, n in gens]
        total = max(n for _, n, _, _ in gens)
        for step in range(total + 1):
            for it in gens:
                it[2] += it[1] / total
                while it[3] and it[2] >= 1.0:
                    it[2] -= 1.0
                    try:
                        next(it[0])
                    except StopIteration:
                        it[3] = False
        for it in gens:
            if it[3]:
                drain(it[0])

    s0 = phase_S0(P, T, G) if G.get('sparse') else iter(())

    def s0gen(n):
        for _ in range(n):
            try:
                next(s0)
            except StopIteration:
                return
            yield
    drain(indexer(0))
    drain(threshold(0))
    for qi in range(NT):
        ns0 = -(-390 * (qi + 1) // 528)
        if qi + 1 < NT:
            drain(indexer(qi + 1))
            merge([(attention(qi), 8 * ((qi + 4) // 4)), (threshold(qi + 1), NIT + 1), (s0gen(ns0), ns0)])
        else:
            merge([(attention(qi), 8 * ((qi + 4) // 4)), (s0gen(1000), 100)])
    drain(s0gen(1000))


def phase_E(P, T, G):
    LD = 0.6065306597126334
    ident = G['identb']
    zr = T['zr']
    def colload(name, n, key):
        t = P.sb([64, n], F32)
        P.dma('sp', lambda e: e.dma_start(out=t[:], in_=T[name].rearrange("(h p) -> p h", p=64), allow_slow_non_contiguous=True), writes=[key])
        return t
    mu_rkv = P.sb([64, 24], F32)
    P.dma('sp', lambda e: e.dma_start(out=mu_rkv[:], in_=T['tshift_mu'][0:1536].rearrange("(h p) -> p h", p=64), allow_slow_non_contiguous=True), writes=['mu'])
    mu_wa = P.sb([64, 2], F32)
    P.dma('sp', lambda e: e.dma_start(out=mu_wa[:], in_=T['tshift_mu'][1536:1664].rearrange("(h p) -> p h", p=64), allow_slow_non_contiguous=True), writes=['mu'])
    mu_g = P.sb([128, 1], F32)
    P.dma('sp', lambda e: e.dma_start(out=mu_g[:], in_=T['tshift_mu'][1664:1792].rearrange("(h p) -> p h", p=128), allow_slow_non_contiguous=True), writes=['mu'])
    om_rkv, om_wa, om_g = P.sb([64, 24], F32), P.sb([64, 2], F32), P.sb([128, 1], F32)
    for (o, m) in ((om_rkv, mu_rkv), (om_wa, mu_wa), (om_g, mu_g)):
        P.op('dve', lambda e, o=o, m=m: e.tensor_scalar(out=o[:], in0=m[:], scalar1=-1.0, scalar2=1.0, op0=ALU.mult, op1=ALU.add),
             reads=['mu'], writes=['om'])
    w0c = colload('decay_w0', 8, 'par'); a0c = colload('iclr_a0', 8, 'par'); kkc = colload('k_k', 8, 'par')
    kac = colload('k_a', 8, 'par'); rkc = colload('r_k', 8, 'par'); lgc = colload('lnx_g', 8, 'par'); lbc = colload('lnx_b', 8, 'par')
    omka = P.sb([64, 8], F32)
    P.op('dve', lambda e: e.tensor_scalar(out=omka[:], in0=kac[:], scalar1=-1.0, scalar2=1.0, op0=ALU.mult, op1=ALU.add),
         reads=['par'], writes=['omka'])
    dup, iup, gup = P.sb([64, 512], BF16), P.sb([64, 512], BF16), P.sb([128, 512], BF16)
    P.dma('pool', lambda e: e.dma_start(out=dup[:], in_=T['decay_up']), writes=['wts'])
    P.dma('pool', lambda e: e.dma_start(out=iup[:], in_=T['iclr_up']), writes=['wts'])
    P.dma('pool', lambda e: e.dma_start(out=gup[:], in_=T['gate_up']), writes=['wts'])
    onesf = P.sb([64, 64], F32)
    onesm = P.sb([64, 64], F32)
    gneps = P.sb([64, 1], F32)
    P.op('dve', lambda e: e.memset(onesf[:], 1.0), writes=['onesf'])
    P.op('dve', lambda e: e.memset(onesm[:], 1.0 / 64), writes=['onesm'])
    P.op('dve', lambda e: e.memset(gneps[:], 64e-5), writes=['gneps'])
    km = P.sb([128, 896], F32)
    P.dma('sp', lambda e: e.dma_start(out=km[:], in_=T['k_masks']), writes=['km'])
    mask4 = P.sb([128, 512], BF16)
    maskL = P.sb([128, 128], BF16)
    P.op('dve', lambda e: e.tensor_copy(out=mask4[:], in_=km[:, 0:512]), reads=['km'], writes=['mask4'])
    P.op('dve', lambda e: e.tensor_copy(out=maskL[:], in_=km[:, 512:640]), reads=['km'], writes=['maskL'])
    rst = P.sb([64, 512], F32)
    P.dma('sp', lambda e: e.dma_start(out=rst[:], in_=T['k_reset']), writes=['rst'])
    Tst = P.sb([64, 8, 64], BF16)
    P.op('dve', lambda e: e.memset(Tst[:], 0.0), writes=[f'T{h}' for h in range(8)])
    zrot = Rot(P, 1, [64, 3, 513], F32, 'z')
    wa_in = P.sb([64, 2, 513], F32)
    gd_in = P.sb([128, 513], F32)
    tmpr = Rot(P, 1, [128, 512], F32, 'tmp')
    twb, adb, sgb = P.sb([64, 512], BF16), P.sb([64, 512], BF16), P.sb([128, 512], BF16)
    AR = P.sb([64, 8, 4, 256], BF16)
    BK = P.sb([64, 8, 4, 256], BF16)
    tok3 = P.sb([128, 8, 4, 3, 64], BF16)
    pC = P.sb([64, 8, 4], F32)
    bon = P.sb([64, 8, 512], BF16)
    gg = P.sb([64, 8, 512], BF16)
    yT = P.sb([64, 8, 512], F32)
    RW = P.sb([64, 8, 512], BF16)
    hb = {n: P.sb([64, 512], F32) for n in ('sig', 'cs', 'ep', 'em', 'epv', 'kk', 'kkn', 'a', 't', 'kp', 'b', 'u1')}
    vb = P.sb([64, 512], BF16)
    Gms = [P.sb([128, 16, 512], BF16) for _ in range(2)]
    XY = [P.sb([128, 16, 256], BF16) for _ in range(2)]
    Nms = [P.sb([128, 16, 128], BF16) for _ in range(2)]
    Wsb, Usb = P.sb([128, 8, 64], BF16), P.sb([128, 8, 64], BF16)
    pg = Rot(P, 5, [128, 512], F32, 'pg', psum=True)
    pl = Rot(P, 2, [64, 512], F32, 'pl', psum=True)
    ptr = Rot(P, 1, [128, 3, 64], BF16, 'ptr', psum=True)
    rwT = T['rwT'].rearrange("(h p) t -> p h t", p=64)

    def dve(fn, reads, writes):
        P.op('dve', fn, reads=reads, writes=writes)

    for tg in range(8):
        t0 = tg * 512
        def load_halo(dst, rows, key, q, tg=tg, t0=t0):
            if tg == 0:
                src = rows(t0, t0 + 512)
                P.op('pool', lambda e: e.memset(dst[:, 0:1] if len(dst.shape) == 2 else dst[:, :, 0:1], 0.0), reads=[key], writes=[key])
                P.dma(q, lambda e: e.dma_start(out=(dst[:, 1:513] if len(dst.shape) == 2 else dst[:, :, 1:513]), in_=src), writes=[key + 'b'])
            else:
                src = rows(t0 - 1, t0 + 512)
                P.dma(q, lambda e: e.dma_start(out=dst[:], in_=src), reads=[key + 'b'], writes=[key])
        load_halo(wa_in, lambda a, b: zr[1536:1664, a:b].rearrange("(h p) t -> p h t", p=64), 'wa', 'sp')
        load_halo(gd_in, lambda a, b: zr[1664:1792, a:b], 'gd', 'act')

        def tshift(src_prev, src_cur, mu_ap, om_ap, np_, keys):
            tm, tk = tmpr.next()
            P.op('pool', lambda e: e.tensor_scalar(out=tm[:np_, :], in0=src_prev, scalar1=mu_ap, scalar2=0.0, op0=ALU.mult, op1=ALU.add),
                 reads=keys + ['mu'], writes=[tk])
            dve(lambda e: e.scalar_tensor_tensor(out=src_cur, in0=src_cur, scalar=om_ap, in1=tm[:np_, :], op0=ALU.mult, op1=ALU.add),
                keys + [tk, 'om'], keys)
        for i in range(2):
            tshift(wa_in[:, i, 0:512], wa_in[:, i, 1:513], mu_wa[:, i:i + 1], om_wa[:, i:i + 1], 64, ['wa', 'wab'])
        tshift(gd_in[:, 0:512], gd_in[:, 1:513], mu_g[:, 0:1], om_g[:, 0:1], 128, ['gd', 'gdb'])
        P.op('act', lambda e: e.activation(out=twb[:], in_=wa_in[:, 0, 1:513], func=AF.Tanh), reads=['wa', 'wab'], writes=['twb'])
        P.op('act', lambda e: e.activation(out=sgb[:], in_=gd_in[:, 1:513], func=AF.Sigmoid), reads=['gd', 'gdb'], writes=['sgb'])
        dve(lambda e: e.tensor_copy(out=adb[:], in_=wa_in[:, 1, 1:513]), ['wa', 'wab'], ['adb'])
        def prep_head(h, z, zk):
            def zrows(a, b, h=h):
                return zr[0:1536, a:b].rearrange("(s hh p) t -> hh p s t", s=3, p=64)[h]
            load_halo(z, zrows, zk, 'sp' if h % 2 == 0 else 'act')
            for s_ in range(3):
                tshift(z[:, s_, 0:512], z[:, s_, 1:513], mu_rkv[:, s_ * 8 + h:s_ * 8 + h + 1], om_rkv[:, s_ * 8 + h:s_ * 8 + h + 1], 64, [zk, zk + 'b'])
            r_, k_, v_ = z[:, 0, 1:513], z[:, 1, 1:513], z[:, 2, 1:513]
            zkeys = [zk, zk + 'b']
            sig, cs, ep, em, epv, kk, kkn, a_, t_, kp, b_, u1 = [hb[n] for n in ('sig', 'cs', 'ep', 'em', 'epv', 'kk', 'kkn', 'a', 't', 'kp', 'b', 'u1')]
            hs = slice(h * 64, (h + 1) * 64)
            p1, p1k = pl.next()
            mm(P, p1[:], dup[:, hs], twb[:], True, True, ['wts', 'twb'], [p1k])
            P.op('act', lambda e, p1=p1, h=h: e.activation(out=sig[:], in_=p1[:], func=AF.Sigmoid, bias=w0c[:, h:h + 1]),
                 reads=[p1k, 'par'], writes=['sig'])
            dve(lambda e: e.tensor_tensor_scan(out=cs[:], data0=rst[:], data1=sig[:], initial=0.0, op0=ALU.mult, op1=ALU.add),
                ['rst', 'sig'], ['cs'])
            P.op('act', lambda e: e.activation(out=ep[:], in_=cs[:], func=AF.Exp, scale=-LD), reads=['cs'], writes=['ep'])
            P.op('act', lambda e: e.activation(out=em[:], in_=cs[:], func=AF.Exp, scale=LD), reads=['cs'], writes=['em'])
            dve(lambda e: e.tensor_tensor(out=u1[:], in0=cs[:], in1=sig[:], op=ALU.subtract), ['cs', 'sig'], ['u1'])
            P.op('act', lambda e: e.activation(out=epv[:], in_=u1[:], func=AF.Exp, scale=-LD), reads=['u1'], writes=['epv'])
            dve(lambda e, h=h: e.tensor_copy(out=pC[:, h, :], in_=ep[:, 127:512:128]), ['ep'], ['pC'])
            p2, p2k = pl.next()
            mm(P, p2[:], iup[:, hs], adb[:], True, True, ['wts', 'adb'], [p2k])
            P.op('act', lambda e, p2=p2, h=h: e.activation(out=a_[:], in_=p2[:], func=AF.Sigmoid, bias=a0c[:, h:h + 1]),
                 reads=[p2k, 'par'], writes=['a'])
            p3, p3k = pl.next()
            mm(P, p3[:], gup[:, hs], sgb[:], True, True, ['wts', 'sgb'], [p3k])
            P.op('act', lambda e, p3=p3, h=h: e.activation(out=gg[:, h, :], in_=p3[:], func=AF.Copy), reads=[p3k], writes=[f'gg{h}'])
            dve(lambda e, h=h: e.tensor_scalar(out=kk[:], in0=k_, scalar1=kkc[:, h:h + 1], scalar2=None, op0=ALU.mult), zkeys + ['par'], ['kk'])
            P.op('act', lambda e: e.activation(out=u1[:], in_=kk[:], func=AF.Square), reads=['kk', 'u1'], writes=['u1'])
            p4, p4k = pl.next()
            mm(P, p4[:], onesf[:], u1[:], True, True, ['onesf', 'u1'], [p4k])
            P.op('act', lambda e, p4=p4: e.activation(out=kkn[:], in_=p4[:], func=AF.Sqrt), reads=[p4k], writes=['kkn'])
            dve(lambda e: e.tensor_scalar(out=kkn[:], in0=kkn[:], scalar1=1e-12, scalar2=None, op0=ALU.max), ['kkn'], ['kkn'])
            dve(lambda e: e.reciprocal(out=kkn[:], in_=kkn[:]), ['kkn'], ['kkn'])
            dve(lambda e: e.tensor_tensor(out=kkn[:], in0=kkn[:], in1=kk[:], op=ALU.mult), ['kkn', 'kk'], ['kkn'])
            dve(lambda e, h=h: e.tensor_scalar(out=t_[:], in0=a_[:], scalar1=kac[:, h:h + 1], scalar2=omka[:, h:h + 1], op0=ALU.mult, op1=ALU.add),
                ['a', 'par', 'omka'], ['t'])
            dve(lambda e: e.tensor_tensor(out=kp[:], in0=t_[:], in1=k_, op=ALU.mult), ['t'] + zkeys, ['kp'])
            dve(lambda e: e.tensor_tensor(out=b_[:], in0=kkn[:], in1=a_[:], op=ALU.mult), ['kkn', 'a'], ['b'])
            c4 = lambda ap: ap.rearrange("p (c t) -> p c t", c=4)
            dve(lambda e, h=h: e.tensor_tensor(out=AR[:, h, :, 128:256], in0=c4(r_), in1=c4(ep[:]), op=ALU.mult), zkeys + ['ep'], [f'AR{h}'])
            dve(lambda e, h=h: e.scalar_tensor_tensor(out=AR[:, h, :, 0:128], in0=c4(kkn[:]), scalar=-1.0, in1=c4(epv[:]), op0=ALU.mult, op1=ALU.mult),
                ['kkn', 'epv'], [f'AR{h}'])
            dve(lambda e, h=h: e.tensor_tensor(out=BK[:, h, :, 0:128], in0=c4(b_[:]), in1=c4(em[:]), op=ALU.mult), ['b', 'em'], [f'BK{h}'])
            dve(lambda e, h=h: e.tensor_tensor(out=BK[:, h, :, 128:256], in0=c4(kp[:]), in1=c4(em[:]), op=ALU.mult), ['kp', 'em'], [f'BK{h}'])
            dve(lambda e, h=h: e.scalar_tensor_tensor(out=u1[:], in0=r_, scalar=rkc[:, h:h + 1], in1=kp[:], op0=ALU.mult, op1=ALU.mult),
                zkeys + ['kp', 'par', 'u1'], ['u1'])
            p5, p5k = pl.next()
            mm(P, p5[:], onesf[:], u1[:], True, True, ['onesf', 'u1'], [p5k])
            dve(lambda e, p5=p5, h=h: e.tensor_tensor(out=bon[:, h, :], in0=p5[:], in1=v_, op=ALU.mult), [p5k] + zkeys, [f'bon{h}'])
            P.op('pool', lambda e: e.tensor_copy(out=vb[:], in_=v_), reads=zkeys, writes=['vb'])
            for c in range(4):
                pt_, ptk = ptr.next()
                cs_ = slice(c * 128, (c + 1) * 128)
                P.op('pe', lambda e, pt_=pt_, cs_=cs_: e.transpose(out=pt_[:, 0, :], in_=vb[:, cs_], identity=ident[0:64, 0:64]), reads=['vb'], writes=[ptk])
                P.op('pe', lambda e, pt_=pt_, h=h, c=c: e.transpose(out=pt_[:, 1, :], in_=BK[:, h, c, 0:128], identity=ident[0:64, 0:64]), reads=[f'BK{h}'], writes=[ptk])
                P.op('pe', lambda e, pt_=pt_, h=h, c=c: e.transpose(out=pt_[:, 2, :], in_=BK[:, h, c, 128:256], identity=ident[0:64, 0:64]), reads=[f'BK{h}'], writes=[ptk])
                P.op('act', lambda e, pt_=pt_, h=h, c=c: e.activation(out=tok3[:, h, c, :, :], in_=pt_[:], func=AF.Copy), reads=[ptk], writes=[f'tok{h}'])
        for h in range(8):
            z, zk = zrot.next()
            prep_head(h, z, zk)
        def stage1(cs, tg=tg):
            sl = (cs[0] // 2) % 2
            Gm, Nm = Gms[sl], Nms[sl]
            probs = [(ci, c, h) for ci, c in enumerate(cs) for h in range(8)]
            for (ci, c, h) in probs:
                q = ci * 8 + h
                p_, pk = pg.next()
                mm(P, p_[:, 0:256], BK[:, h, c, 0:128], AR[:, h, c, :], True, True, [f'BK{h}', f'AR{h}'], [pk])
                mm(P, p_[:, 256:512], BK[:, h, c, 128:256], AR[:, h, c, :], True, True, [f'BK{h}', f'AR{h}'], [pk])
                dve(lambda e, p_=p_, q=q: e.tensor_tensor(out=Gm[:, q, :], in0=p_[:], in1=mask4[:], op=ALU.mult), [pk, 'mask4'], [f'Gm{sl}_{q}'])
                p2_, p2k = pg.next()
                mm(P, p2_[:, 0:128], AR[:, h, c, 0:128], BK[:, h, c, 0:128], True, True, [f'BK{h}', f'AR{h}'], [p2k])
                dve(lambda e, p2_=p2_, q=q: e.tensor_tensor(out=XY[0][:, q, 128:256], in0=p2_[:, 0:128], in1=maskL[:], op=ALU.mult),
                    [p2k, 'maskL'], [f'XY0{q}'])
                P.op('pool', lambda e, q=q: e.tensor_copy(out=XY[0][:, q, 0:128], in_=Gm[:, q, 0:128]), reads=[f'Gm{sl}_{q}'], writes=[f'XY0{q}x'])
                P.op('pool', lambda e, q=q: e.tensor_tensor(out=Nm[:, q, :], in0=Gm[:, q, 0:128], in1=ident[:], op=ALU.add),
                     reads=[f'Gm{sl}_{q}'], writes=[f'N{sl}_{q}'])
            yield
            for j in range(6):
                cur, nxt = XY[j % 2], XY[(j + 1) % 2]
                ck, nk_ = f'XY{j % 2}', f'XY{(j + 1) % 2}'
                for q in range(len(probs)):
                    p_, pk = pg.next()
                    rk_ = [ck + f'{q}', ck + f'{q}x']
                    mm(P, p_[:, 0:128], cur[:, q, 128:256], cur[:, q, 0:128], True, True, rk_, [pk])
                    mm(P, p_[:, 128:256], cur[:, q, 0:128], cur[:, q, 128:256], True, True, rk_, [pk])
                    P.op('act', lambda e, p_=p_, nxt=nxt, q=q: e.activation(out=nxt[:, q, :], in_=p_[:, 0:256], func=AF.Copy),
                         reads=[pk], writes=[nk_ + f'{q}', nk_ + f'{q}x'])
                    if q % 4 == 3:
                        yield
                for q in range(len(probs)):
                    p_, pk = pg.next()
                    mm(P, p_[:, 0:128], nxt[:, q, 128:256], Nm[:, q, :], True, True, [nk_ + f'{q}', nk_ + f'{q}x', f'N{sl}_{q}'], [pk])
                    dve(lambda e, p_=p_, q=q: e.tensor_tensor(out=Nm[:, q, :], in0=p_[:, 0:128], in1=Nm[:, q, :], op=ALU.add),
                        [pk, f'N{sl}_{q}'], [f'N{sl}_{q}'])
                    if q % 4 == 3:
                        yield

        def stage2(c, tg=tg):
            sl = (c // 2) % 2
            Gm, Nm = Gms[sl], Nms[sl]
            ci = c % 2
            pw, pwk = pg.next()
            for h in range(8):
                q = ci * 8 + h
                mm(P, pw[:, h * 64:(h + 1) * 64], AR[:, h, c, 0:128], Tst[:, h, :], True, False, [f'AR{h}', f'T{h}'], [pwk])
                mm(P, pw[:, h * 64:(h + 1) * 64], Gm[:, q, 256:384], tok3[:, h, c, 0, :], False, True, [f'Gm{sl}_{q}', f'tok{h}'], [pwk])
            P.op('act', lambda e: e.activation(out=Wsb[:].rearrange("p h v -> p (h v)"), in_=pw[:], func=AF.Copy), reads=[pwk], writes=['W'])
            yield
            pu, puk = pg.next()
            for h in range(8):
                q = ci * 8 + h
                mm(P, pu[:, h * 64:(h + 1) * 64], Nm[:, q, :], Wsb[:, h, :], True, True, [f'N{sl}_{q}', 'W'], [puk])
            P.op('act', lambda e: e.activation(out=Usb[:].rearrange("p h v -> p (h v)"), in_=pu[:], func=AF.Copy), reads=[puk], writes=['U'])
            yield
            pt_, ptk = pg.next()
            for h in range(8):
                q = ci * 8 + h
                o_ = pt_[0:64, h * 64:(h + 1) * 64]
                mm(P, o_, ident[0:64, 0:64], Tst[:, h, :], True, False, [f'T{h}'], [ptk])
                mm(P, o_, tok3[:, h, c, 1, :], Usb[:, h, :], False, False, [f'tok{h}', 'U'], [ptk])
                mm(P, o_, tok3[:, h, c, 2, :], tok3[:, h, c, 0, :], False, True, [f'tok{h}'], [ptk])
            for half in range(2):
                py_, pyk = pg.next()
                for hh in range(4):
                    h = half * 4 + hh
                    q = ci * 8 + h
                    o_ = py_[0:64, hh * 128:(hh + 1) * 128]
                    mm(P, o_, Tst[:, h, :], AR[:, h, c, 128:256], True, False, [f'AR{h}', f'T{h}'], [pyk])
                    mm(P, o_, Usb[:, h, :], Gm[:, q, 128:256], False, False, ['U', f'Gm{sl}_{q}'], [pyk])
                    mm(P, o_, tok3[:, h, c, 0, :], Gm[:, q, 384:512], False, True, [f'tok{h}', f'Gm{sl}_{q}'], [pyk])
                P.op('act', lambda e, py_=py_, half=half: e.activation(
                    out=yT[:, half * 4:half * 4 + 4, c * 128:(c + 1) * 128], in_=py_[0:64, :].rearrange("p (h t) -> p h t", h=4), func=AF.Copy),
                    reads=[pyk], writes=[f'yT{half}'])
            for h in range(8):
                dve(lambda e, h=h: e.tensor_scalar(out=Tst[:, h, :], in0=pt_[0:64, h * 64:(h + 1) * 64], scalar1=pC[:, h, c:c + 1], scalar2=None, op0=ALU.mult),
                    [ptk, 'pC', f'T{h}'], [f'T{h}'])
            yield

        def chain(*gs):
            for g in gs:
                yield from g

        def rr(g1, g2):
            a = b = True
            while a or b:
                if a:
                    try:
                        next(g1)
                    except StopIteration:
                        a = False
                if b:
                    try:
                        next(g2)
                    except StopIteration:
                        b = False
        for _ in stage1([0, 1]):
            pass
        rr(stage1([2, 3]), chain(stage2(0), stage2(1)))
        for _ in chain(stage2(2), stage2(3)):
            pass
        for h in range(8):
            u1, u2 = hb['u1'], hb['t']
            p1, p1k = pl.next()
            mm(P, p1[:], onesm[:], yT[:, h, :], True, True, ['onesm', f'yT{h // 4}'], [p1k])
            dve(lambda e, p1=p1, h=h: e.tensor_tensor(out=u1[:], in0=yT[:, h, :], in1=p1[:], op=ALU.subtract), [p1k, f'yT{h // 4}', 'u1'], ['u1'])
            P.op('act', lambda e: e.activation(out=u2[:], in_=u1[:], func=AF.Square), reads=['u1', 't'], writes=['t'])
            p2, p2k = pl.next()
            mm(P, p2[:], onesm[:], u2[:], True, True, ['onesm', 't'], [p2k])
            P.op('act', lambda e, p2=p2: e.activation(out=u2[:], in_=p2[:], func=AF.Sqrt, bias=gneps[:, 0:1]), reads=[p2k, 'gneps', 't'], writes=['t'])
            dve(lambda e: e.reciprocal(out=u2[:], in_=u2[:]), ['t'], ['t'])
            dve(lambda e: e.tensor_tensor(out=u1[:], in0=u1[:], in1=u2[:], op=ALU.mult), ['u1', 't'], ['u1'])
            dve(lambda e, h=h: e.tensor_scalar(out=u1[:], in0=u1[:], scalar1=lgc[:, h:h + 1], scalar2=lbc[:, h:h + 1], op0=ALU.mult, op1=ALU.add),
                ['u1', 'par'], ['u1'])
            dve(lambda e, h=h: e.tensor_tensor(out=u1[:], in0=u1[:], in1=bon[:, h, :], op=ALU.add), ['u1', f'bon{h}'], ['u1'])
            dve(lambda e, h=h: e.tensor_tensor(out=RW[:, h, :], in0=u1[:], in1=gg[:, h, :], op=ALU.mult), ['u1', f'gg{h}'], ['RW'])
        P.dma('sp', lambda e, t0=t0: e.dma_start(out=rwT[:, :, t0:t0 + 512], in_=RW[:]), reads=['RW'])
        if tg == 0 and 'dbg_y' in T:
            P.dma('sp', lambda e: e.dma_start(out=T['dbg_y'], in_=yT[:]), reads=['yT0', 'yT1'])
            P.dma('sp', lambda e: e.dma_start(out=T['dbg_bon'], in_=bon[:]), reads=[f'bon{h}' for h in range(8)])
            P.dma('sp', lambda e: e.dma_start(out=T['dbg_g'], in_=gg[:]), reads=[f'gg{h}' for h in range(8)])
            P.dma('sp', lambda e: e.dma_start(out=T['dbg_AR'], in_=AR[:]), reads=[f'AR{h}' for h in range(8)])
            P.dma('sp', lambda e: e.dma_start(out=T['dbg_BK'], in_=BK[:]), reads=[f'BK{h}' for h in range(8)])


def phase_F(P, T, G):
    wa, wr, wo = P.sb([128, 4, D], BF16), P.sb([128, 4, D], BF16), P.sb([128, 8, D], BF16)
    P.dma('pool', lambda e: e.dma_start(out=wa[:], in_=T['w_attn_br'].rearrange("(j p) d -> p j d", p=128)), writes=['wa'])
    P.dma('pool', lambda e: e.dma_start(out=wr[:], in_=T['w_rwkv_br'].rearrange("(j p) d -> p j d", p=128)), writes=['wr'])
    P.dma('pool', lambda e: e.dma_start(out=wo[:], in_=T['w_out'].rearrange("(j p) d -> p j d", p=128)), writes=['wo'])
    W = dict(junk=P.sb([128, D], F32), ss=P.sb([128, 1], F32), rs=P.sb([128, 1], F32), t1=P.sb([128, D], F32),
             hb=P.sb([128, D], BF16), pt=P.ps([128, 8, 128], BF16))
    W['mod'] = P.sb([128, 3072], F32)
    P.dma('sp', lambda e: e.dma_start(out=W['mod'][:], in_=T['modd'][:, 2048:5120]), writes=['modl'])
    xrot = Rot(P, 2, [128, D], F32, 'x')
    atr, rtr = Rot(P, 2, [128, 4, 128], BF16, 'at'), Rot(P, 2, [128, 4, 128], BF16, 'rt')
    gar, grr = Rot(P, 2, [128, 8, 128], BF16, 'ga'), Rot(P, 2, [128, 8, 128], BF16, 'gr')
    sga, sgr = P.sb([128, 8, 128], F32), P.sb([128, 8, 128], F32)
    m1, m2 = P.sb([128, 4, 128], F32), P.sb([128, 4, 128], F32)
    mixT = P.sb([128, 8, 128], BF16)
    x1t = P.sb([128, D], F32)
    h2t = P.sb([128, 8, 128], BF16)
    pA = Rot(P, 1, [128, 4, 128], F32, 'pA', psum=True)
    pR = Rot(P, 1, [128, 4, 128], F32, 'pR', psum=True)
    po = Rot(P, 2, [128, 512], F32, 'po', psum=True)
    aT = T['attnT'].rearrange("(j p) t -> p j t", p=128)
    rT = T['rwT'].rearrange("(j p) t -> p j t", p=128)
    gaT = T['zga'].rearrange("(j p) t -> p j t", p=128)
    grT = T['zgr'].rearrange("(j p) t -> p j t", p=128)
    h2T = T['h2T'].rearrange("(k p) t -> p k t", p=128)
    R = router_setup(P, T, G) if G.get('sparse') else None
    zt = P.sb([128, D], BF16)
    P.op('pool', lambda e: e.memset(zt[:], 0.0), writes=['zt'])
    for i in range(NT):
        tsl = slice(i * 128, (i + 1) * 128)
        if R is not None:
            for bz in range(i * 10, i * 10 + 10):
                P.dma('act' if bz % 2 else 'sp', lambda e, bz=bz: e.dma_start(out=T['Xs'][bz * 128:(bz + 1) * 128, :], in_=zt[:]), reads=['zt'])
        xt, xk = xrot.next()
        at, atk = atr.next(); rt, rtk = rtr.next(); ga, gak = gar.next(); gr, grk = grr.next()
        P.dma('sp', lambda e, xt=xt, tsl=tsl: e.dma_start(out=xt[:], in_=T['x'][tsl, :]), writes=[xk])
        P.dma('act', lambda e, at=at, tsl=tsl: e.dma_start(out=at[:], in_=aT[:, :, tsl]), writes=[atk])
        P.dma('act', lambda e, rt=rt, tsl=tsl: e.dma_start(out=rt[:], in_=rT[:, :, tsl]), writes=[rtk])
        P.dma('sp', lambda e, ga=ga, tsl=tsl: e.dma_start(out=ga[:], in_=gaT[:, :, tsl]), writes=[gak])
        P.dma('sp', lambda e, gr=gr, tsl=tsl: e.dma_start(out=gr[:], in_=grT[:, :, tsl]), writes=[grk])
        P.op('act', lambda e, ga=ga: e.activation(out=sga[:], in_=ga[:], func=AF.Sigmoid), reads=[gak], writes=['sga'])
        P.op('act', lambda e, gr=gr: e.activation(out=sgr[:], in_=gr[:], func=AF.Sigmoid), reads=[grk], writes=['sgr'])
        for half in range(2):
            pa, pak = pA.next(); pr, prk = pR.next()
            for s_ in range(4):
                dt = half * 4 + s_
                for j in range(4):
                    mm(P, pa[:, s_, :], wa[:, j, dt * 128:(dt + 1) * 128], at[:, j, :], j == 0, j == 3, ['wa', atk], [pak])
            for s_ in range(4):
                dt = half * 4 + s_
                for j in range(4):
                    mm(P, pr[:, s_, :], wr[:, j, dt * 128:(dt + 1) * 128], rt[:, j, :], j == 0, j == 3, ['wr', rtk], [prk])
            hs = slice(half * 4, half * 4 + 4)
            P.op('dve', lambda e, pa=pa, hs=hs: e.tensor_tensor(out=m1[:], in0=pa[:], in1=sga[:, hs, :], op=ALU.mult), reads=[pak, 'sga'], writes=['m1'])
            P.op('dve', lambda e, pr=pr, hs=hs: e.tensor_tensor(out=m2[:], in0=pr[:], in1=sgr[:, hs, :], op=ALU.mult), reads=[prk, 'sgr'], writes=['m2'])
            P.op('pool', lambda e, hs=hs: e.tensor_tensor(out=mixT[:, hs, :], in0=m1[:], in1=m2[:], op=ALU.add), reads=['m1', 'm2'], writes=['mixT'])
        for half in range(2):
            p_, pk = po.next()
            cs_ = slice(half * 512, (half + 1) * 512)
            for dt in range(8):
                mm(P, p_[:], mixT[:, dt, :], wo[:, dt, cs_], dt == 0, dt == 7, ['mixT', 'wo'], [pk])
            P.op('dve', lambda e, p_=p_, cs_=cs_: e.tensor_tensor(out=x1t[:, cs_], in0=p_[:], in1=W['mod'][:, cs_], op=ALU.mult),
                 reads=[pk, 'modl'], writes=['x1t'])
        P.op('pool', lambda e, xt=xt: e.tensor_tensor(out=x1t[:], in0=x1t[:], in1=xt[:], op=ALU.add), reads=['x1t', xk], writes=['x1t'])
        P.dma('sp', lambda e, tsl=tsl: e.dma_start(out=T['x1'][tsl, :], in_=x1t[:]), reads=['x1t'])
        norm_mod_transpose(P, G, x1t, 'x1t', h2t, 0, slice(2048, 3072), slice(1024, 2048), W)
        P.dma('act', lambda e, tsl=tsl: e.dma_start(out=h2T[:, :, tsl], in_=h2t[:]), reads=['hT0'])
        if R is not None:
            P.dma('act', lambda e, tsl=tsl: e.dma_start(out=T['h2tok'][tsl, :], in_=W['hb'][:]), reads=['hb'])
            router_tile(P, T, R, h2t, 'hT0', i)
    if R is not None:
        P.dma('sp', lambda e: e.dma_start(out=T['cntd'], in_=R['cnt'][:]), reads=['cnt'])


def phase_G0(P, T, G):
    for e in range(64):
        for (src, dst) in (('exp_gate', 'wg16'), ('exp_up', 'wu16'), ('exp_down', 'wd16')):
            P.dma('pool', lambda e_, e=e, src=src, dst=dst: e_.dma_start(
                out=T[dst][e].rearrange("k p f -> (k p f)").rearrange("(a b) -> a b", b=2048), in_=T[src][e].rearrange("r c -> (r c)").rearrange("(a b) -> a b", b=2048)))
    for (src, dst) in (('sh_gate', 'wg16'), ('sh_up', 'wu16'), ('sh_down', 'wd16')):
        P.dma('pool', lambda e_, src=src, dst=dst: e_.dma_start(
            out=T[dst][64].rearrange("k p f -> (k p f)").rearrange("(a b) -> a b", b=2048), in_=T[src].rearrange("r c -> (r c)").rearrange("(a b) -> a b", b=2048)))


def phase_G(P, T, G):
    ident = G['identb']
    h2T = T['h2T'].rearrange("(k p) t -> p k t", p=128)
    yacc = G['yacc']
    gwT = P.sb([64, S], BF16)
    rwt = P.sb([128, 8, 64], BF16)
    rbias = P.sb([128, 64], F32)
    P.dma('pool', lambda e: e.dma_start(out=rwt[:], in_=T['router_w'].rearrange("(k p) n -> p k n", p=128)), writes=['rwt'])
    P.dma('sp', lambda e: e.dma_start(out=rbias[:], in_=T['router_bias'].partition_broadcast(128)), writes=['rbias'])
    ones128 = P.sb([64, 128], BF16)
    P.op('dve', lambda e: e.memset(ones128[:], 1.0), writes=['ones128'])
    hrot = Rot(P, 2, [128, 8, 256], BF16, 'h2g')
    pmisc = P.ps([128, 512], F32)
    ptb = P.ps([64, 128], BF16)
    emb = P.sb([128, 64], BF16)
    sc_, ch, tmp, cm, em = [P.sb([128, 64], F32) for _ in range(5)]
    m1, m2, grp, s8, gmask, pen, den = [P.sb([128, 8], F32) for _ in range(7)]
    dve = lambda fn, r, w: P.op('dve', fn, reads=r, writes=w)
    for tgp in range(16):
        hg, hk = hrot.next()
        P.dma('sp', lambda e, hg=hg, tgp=tgp: e.dma_start(out=hg[:], in_=h2T[:, :, tgp * 256:(tgp + 1) * 256]), writes=[hk])
        for tt in range(2):
            i = tgp * 2 + tt
            p_, pk = pmisc[:, 0:64], 'pm_a'
            for k in range(8):
                mm(P, p_, hg[:, k, tt * 128:(tt + 1) * 128], rwt[:, k, :], k == 0, k == 7, [hk, 'rwt'], [pk])
            P.op('act', lambda e, p_=p_: e.activation(out=sc_[:], in_=p_, func=AF.Sigmoid), reads=[pk], writes=['sc'])
            dve(lambda e: e.tensor_tensor(out=ch[:], in0=sc_[:], in1=rbias[:], op=ALU.add), ['sc', 'rbias'], ['ch'])
            ch3 = ch[:].rearrange("p (g e) -> p g e", g=8)
            dve(lambda e, ch3=ch3: e.tensor_reduce(out=m1[:], in_=ch3, axis=AX.X, op=ALU.max), ['ch'], ['m1'])
            for g in range(8):
                dve(lambda e, g=g: e.tensor_scalar(out=tmp[:, g * 8:(g + 1) * 8], in0=ch[:, g * 8:(g + 1) * 8], scalar1=m1[:, g:g + 1],
                                                  scalar2=-1e9, op0=ALU.is_equal, op1=ALU.mult), ['ch', 'm1', 'tmp'], ['tmp'])
            dve(lambda e: e.tensor_tensor(out=tmp[:], in0=tmp[:], in1=ch[:], op=ALU.add), ['tmp', 'ch'], ['tmp'])
            dve(lambda e: e.tensor_reduce(out=m2[:], in_=tmp[:].rearrange("p (g e) -> p g e", g=8), axis=AX.X, op=ALU.max), ['tmp'], ['m2'])
            dve(lambda e: e.tensor_tensor(out=grp[:], in0=m1[:], in1=m2[:], op=ALU.add), ['m1', 'm2'], ['grp'])
            dve(lambda e: e.max(out=s8[:], in_=grp[:]), ['grp'], ['s8'])
            dve(lambda e: e.tensor_scalar(out=gmask[:], in0=grp[:], scalar1=s8[:, 3:4], scalar2=None, op0=ALU.is_ge), ['grp', 's8'], ['gmask'])
            dve(lambda e: e.tensor_scalar(out=pen[:], in0=gmask[:], scalar1=-1.0, scalar2=1e9, op0=ALU.add, op1=ALU.mult), ['gmask'], ['pen'])
            for g in range(8):
                dve(lambda e, g=g: e.tensor_scalar(out=cm[:, g * 8:(g + 1) * 8], in0=ch[:, g * 8:(g + 1) * 8], scalar1=pen[:, g:g + 1],
                                                  scalar2=None, op0=ALU.add), ['ch', 'pen', 'cm'], ['cm'])
            dve(lambda e: e.max(out=s8[:], in_=cm[:]), ['cm', 's8'], ['s8'])
            dve(lambda e: e.tensor_scalar(out=em[:], in0=cm[:], scalar1=s8[:, 7:8], scalar2=None, op0=ALU.is_ge), ['cm', 's8'], ['em'])
            dve(lambda e: e.tensor_tensor(out=em[:], in0=em[:], in1=sc_[:], op=ALU.mult), ['em', 'sc'], ['em'])
            dve(lambda e: e.tensor_reduce(out=den[:, 0:1], in_=em[:], axis=AX.X, op=ALU.add), ['em'], ['den'])
            dve(lambda e: e.reciprocal(out=den[:, 1:2], in_=den[:, 0:1]), ['den'], ['den'])
            dve(lambda e: e.tensor_scalar(out=em[:], in0=em[:], scalar1=den[:, 1:2], scalar2=2.5, op0=ALU.mult, op1=ALU.mult), ['em', 'den'], ['em'])
            pt_, ptk = ptb[:], 'pm_b'
            dve(lambda e: e.tensor_copy(out=emb[:], in_=em[:]), ['em', 'emb'], ['emb'])
            P.op('pe', lambda e, pt_=pt_: e.transpose(out=pt_, in_=emb[:], identity=G['identb'][:]), reads=['emb'], writes=[ptk])
            P.op('act', lambda e, pt_=pt_, i=i: e.activation(out=gwT[:, i * 128:(i + 1) * 128], in_=pt_, func=AF.Copy), reads=[ptk], writes=['gwT'])
    if G.get('gstop') == 'router':
        return
    wgr = Rot(P, 2, [128, 8, 256], BF16, 'wg')
    wur = Rot(P, 2, [128, 8, 256], BF16, 'wu')
    wdr = Rot(P, 2, [128, 2, D], BF16, 'wd')
    selr = Rot(P, 2, [64, 128], BF16, 'sel')
    pgu = Rot(P, 2, [128, 4, 256], F32, 'pgu', psum=True)
    py = Rot(P, 2, [128, 512], F32, 'py', psum=True)
    sgr_ = Rot(P, 2, [128, 2, 256], F32, 'sg')
    tr_ = Rot(P, 2, [128, 2, 256], F32, 'tt')
    actr = Rot(P, 2, [128, 2, 256], BF16, 'act')
    for e_ in range(G.get('nexp', 65)):
        wg, wgk = wgr.next(); wu, wuk = wur.next(); wd, wdk = wdr.next()
        if e_ < 64:
            sg_, su_, sd_ = T['exp_gate'][e_], T['exp_up'][e_], T['exp_down'][e_]
        else:
            sg_, su_, sd_ = T['sh_gate'], T['sh_up'], T['sh_down']
        P.dma('pool', lambda e, wg=wg, sg_=sg_: e.dma_start(out=wg[:], in_=sg_.rearrange("(k p) f -> p k f", p=128)), writes=[wgk])
        P.dma('pool', lambda e, wu=wu, su_=su_: e.dma_start(out=wu[:], in_=su_.rearrange("(k p) f -> p k f", p=128)), writes=[wuk])
        P.dma('pool', lambda e, wd=wd, sd_=sd_: e.dma_start(out=wd[:], in_=sd_.rearrange("(k p) f -> p k f", p=128)), writes=[wdk])
        if e_ < 64:
            sel, selk = selr.next()
            P.op('pool', lambda e, sel=sel, e_=e_: e.tensor_scalar(out=sel[:], in0=ones128[:], scalar1=G['identf'][0:64, e_:e_ + 1], scalar2=0.0, op0=ALU.mult, op1=ALU.add),
                 reads=['ones128'], writes=[selk])
        for tgp in range(16):
            hg, hk = hrot.next()
            P.dma('sp' if tgp % 2 == 0 else 'act', lambda e, hg=hg, tgp=tgp: e.dma_start(out=hg[:], in_=h2T[:, :, tgp * 256:(tgp + 1) * 256]), writes=[hk])
            p_, pk = pgu.next()
            for s_, (w_, wk_) in enumerate(((wg, wgk), (wg, wgk), (wu, wuk), (wu, wuk))):
                ft = s_ % 2
                for k in range(8):
                    mm(P, p_[:, s_, :], w_[:, k, ft * 128:(ft + 1) * 128], hg[:, k, :], k == 0, k == 7, [wk_, hk], [pk])
            sg, sgk = sgr_.next(); t_, tk = tr_.next(); ac, ack = actr.next()
            P.op('act', lambda e, sg=sg, p_=p_: e.activation(out=sg[:], in_=p_[:, 0:2, :], func=AF.Silu), reads=[pk], writes=[sgk])
            P.op('dve', lambda e, sg=sg, p_=p_, t_=t_: e.tensor_tensor(out=t_[:], in0=p_[:, 2:4, :], in1=sg[:], op=ALU.mult), reads=[pk, sgk], writes=[tk])
            if e_ < 64:
                pw, pwk = pmisc[:, 256:512], 'pm_c'
                mm(P, pw, sel[:], gwT[:, tgp * 256:(tgp + 1) * 256], True, True, [selk, 'gwT'], [pwk])
                for ft in range(2):
                    P.op('dve', lambda e, ac=ac, t_=t_, pw=pw, ft=ft: e.tensor_tensor(out=ac[:, ft, :], in0=pw, in1=t_[:, ft, :], op=ALU.mult),
                         reads=[tk, pwk], writes=[ack])
            else:
                P.op('pool', lambda e, ac=ac, t_=t_: e.tensor_copy(out=ac[:], in_=t_[:]), reads=[tk], writes=[ack])
            for tt in range(2):
                i = tgp * 2 + tt
                for half in range(2):
                    q_, qk = py.next()
                    cs_ = slice(half * 512, (half + 1) * 512)
                    for ft in range(2):
                        mm(P, q_[:], ac[:, ft, tt * 128:(tt + 1) * 128], wd[:, ft, cs_], ft == 0, ft == 1, [ack, wdk], [qk])
                    if e_ == 0:
                        P.op('act', lambda e, q_=q_, i=i, cs_=cs_: e.activation(out=yacc[:, i, cs_], in_=q_[:], func=AF.Copy), reads=[qk], writes=[f'y{i}'])
                    else:
                        P.op('dve', lambda e, q_=q_, i=i, cs_=cs_: e.tensor_tensor(out=yacc[:, i, cs_], in0=q_[:], in1=yacc[:, i, cs_], op=ALU.add),
                             reads=[qk, f'y{i}'], writes=[f'y{i}'])


def phase_H(P, T, G):
    yacc = G['yacc']
    dve = lambda fn, r, w: P.op('dve', fn, reads=r, writes=w)
    g2b = P.sb([128, D], F32)
    fing = P.sb([128, D], F32)
    P.dma('sp', lambda e: e.dma_start(out=g2b[:], in_=T['modd'][:, 5120:6144]), writes=['g2b'])
    P.dma('sp', lambda e: e.dma_start(out=fing[:], in_=T['final_g'].partition_broadcast(128)), writes=['fing'])
    xr = Rot(P, 2, [128, D], F32, 'x1')
    junk, ss, rs = P.sb([128, D], F32), P.sb([128, 1], F32), P.sb([128, 1], F32)
    for i in range(NT):
        tsl = slice(i * 128, (i + 1) * 128)
        xt, xk = xr.next()
        P.dma('sp', lambda e, xt=xt, tsl=tsl: e.dma_start(out=xt[:], in_=T['x1'][tsl, :]), writes=[xk])
        dve(lambda e, i=i: e.tensor_tensor(out=yacc[:, i, :], in0=yacc[:, i, :], in1=g2b[:], op=ALU.mult), [f'y{i}', 'g2b'], [f'y{i}'])
        P.op('pool', lambda e, i=i, xt=xt: e.tensor_tensor(out=xt[:], in0=xt[:], in1=yacc[:, i, :], op=ALU.add), reads=[f'y{i}', xk], writes=[xk])
        rms_rstd(P, xt, xk, junk, ss, rs, 'f')
        dve(lambda e, xt=xt: e.scalar_tensor_tensor(out=xt[:], in0=xt[:], scalar=rs[:, 0:1], in1=fing[:], op0=ALU.mult, op1=ALU.mult),
            [xk, 'rsf', 'fing'], [xk])
        P.dma('sp', lambda e, xt=xt, tsl=tsl: e.dma_start(out=T['out'][tsl, :], in_=xt[:]), reads=[xk])


NBLK = 320


def router_setup(P, T, G):
    R = {}
    R['rwt'] = P.sb([128, 8, 64], BF16)
    R['rbias'] = P.sb([128, 64], F32)
    R['iota'] = P.sb([128, 64], F32)
    R['su'] = P.sb([128, 128], BF16)
    R['onesb'] = P.sb([128, 128], BF16)
    R['cnt'] = P.sb([128, 64], F32)
    R['kmf'] = P.sb([128, 128], F32)
    P.dma('pool', lambda e: e.dma_start(out=R['rwt'][:], in_=T['router_w'].rearrange("(k p) n -> p k n", p=128)), writes=['rwt'])
    P.dma('sp', lambda e: e.dma_start(out=R['rbias'][:], in_=T['router_bias'].partition_broadcast(128)), writes=['rbias'])
    P.dma('sp', lambda e: e.dma_start(out=R['iota'][:], in_=T['k_rel'][0:1, 0:64].rearrange("o f -> (o f)").partition_broadcast(128)), writes=['iota'])
    P.dma('sp', lambda e: e.dma_start(out=R['kmf'][:], in_=T['k_masks'][:, 0:128]), writes=['kmf'])
    P.op('dve', lambda e: e.tensor_copy(out=R['su'][:], in_=R['kmf'][:]), reads=['kmf'], writes=['su'])
    P.op('dve', lambda e: e.memset(R['onesb'][:], 1.0), writes=['onesb'])
    P.op('dve', lambda e: e.memset(R['cnt'][:], 0.0), writes=['cnt'])
    R['pm'] = P.ps([128, 512], F32)
    for n in ('sc', 'ch', 'tmp', 'cm', 'em', 'mk', 'oh', 'rk', 'jk'):
        R[n] = P.sb([128, 64], F32)
    R['mkb'] = P.sb([128, 64], BF16)
    for n in ('m1', 'm2', 'grp', 's8', 'gmask', 'pen', 'den', 'i8f'):
        R[n] = P.sb([128, 8], F32)
    R['i8u'] = P.sb([128, 8], U32)
    R['meta'] = P.sb([128, 24], F32)
    return R


def router_tile(P, T, R, h2t, h2k, i):
    dve = lambda fn, r, w: P.op('dve', fn, reads=r, writes=w)
    pm = R['pm']
    sc_, ch, tmp, cm, em, mk, oh, rk, jk, mkb = [R[n] for n in ('sc', 'ch', 'tmp', 'cm', 'em', 'mk', 'oh', 'rk', 'jk', 'mkb')]
    m1, m2, grp, s8, gmask, pen, den, i8f, i8u, meta = [R[n] for n in ('m1', 'm2', 'grp', 's8', 'gmask', 'pen', 'den', 'i8f', 'i8u', 'meta')]
    for k in range(8):
        mm(P, pm[:, 0:64], h2t[:, k, :], R['rwt'][:, k, :], k == 0, k == 7, [h2k, 'rwt'], ['pm_a'])
    P.op('act', lambda e: e.activation(out=sc_[:], in_=pm[:, 0:64], func=AF.Sigmoid), reads=['pm_a'], writes=['sc'])
    dve(lambda e: e.tensor_tensor(out=ch[:], in0=sc_[:], in1=R['rbias'][:], op=ALU.add), ['sc', 'rbias'], ['ch'])
    dve(lambda e: e.tensor_reduce(out=m1[:], in_=ch[:].rearrange("p (g e) -> p g e", g=8), axis=AX.X, op=ALU.max), ['ch'], ['m1'])
    for g in range(8):
        dve(lambda e, g=g: e.tensor_scalar(out=tmp[:, g * 8:(g + 1) * 8], in0=ch[:, g * 8:(g + 1) * 8], scalar1=m1[:, g:g + 1],
                                          scalar2=-1e9, op0=ALU.is_equal, op1=ALU.mult), ['ch', 'm1', 'tmp'], ['tmp'])
    dve(lambda e: e.tensor_tensor(out=tmp[:], in0=tmp[:], in1=ch[:], op=ALU.add), ['tmp', 'ch'], ['tmp'])
    dve(lambda e: e.tensor_reduce(out=m2[:], in_=tmp[:].rearrange("p (g e) -> p g e", g=8), axis=AX.X, op=ALU.max), ['tmp'], ['m2'])
    dve(lambda e: e.tensor_tensor(out=grp[:], in0=m1[:], in1=m2[:], op=ALU.add), ['m1', 'm2'], ['grp'])
    dve(lambda e: e.max(out=s8[:], in_=grp[:]), ['grp'], ['s8'])
    dve(lambda e: e.tensor_scalar(out=gmask[:], in0=grp[:], scalar1=s8[:, 3:4], scalar2=None, op0=ALU.is_ge), ['grp', 's8'], ['gmask'])
    dve(lambda e: e.tensor_scalar(out=pen[:], in0=gmask[:], scalar1=-1.0, scalar2=1e9, op0=ALU.add, op1=ALU.mult), ['gmask'], ['pen'])
    for g in range(8):
        dve(lambda e, g=g: e.tensor_scalar(out=cm[:, g * 8:(g + 1) * 8], in0=ch[:, g * 8:(g + 1) * 8], scalar1=pen[:, g:g + 1],
                                          scalar2=None, op0=ALU.add), ['ch', 'pen', 'cm'], ['cm'])
    dve(lambda e: e.max(out=s8[:], in_=cm[:]), ['cm', 's8'], ['s8'])
    dve(lambda e: e.max_index(out=i8u[:], in_max=s8[:], in_values=cm[:]), ['cm', 's8', 'i8u'], ['i8u'])
    dve(lambda e: e.tensor_copy(out=meta[:, 0:8], in_=i8u[:]), ['i8u', 'meta'], ['meta'])
    dve(lambda e: e.tensor_scalar(out=mk[:], in0=cm[:], scalar1=s8[:, 7:8], scalar2=None, op0=ALU.is_ge), ['cm', 's8'], ['mk'])
    dve(lambda e: e.tensor_copy(out=mkb[:], in_=mk[:]), ['mk', 'mkb'], ['mkb'])
    dve(lambda e: e.tensor_tensor(out=em[:], in0=mk[:], in1=sc_[:], op=ALU.mult), ['mk', 'sc'], ['em'])
    dve(lambda e: e.tensor_reduce(out=den[:, 0:1], in_=em[:], axis=AX.X, op=ALU.add), ['em'], ['den'])
    dve(lambda e: e.reciprocal(out=den[:, 1:2], in_=den[:, 0:1]), ['den'], ['den'])
    dve(lambda e: e.tensor_scalar(out=em[:], in0=em[:], scalar1=den[:, 1:2], scalar2=2.5, op0=ALU.mult, op1=ALU.mult), ['em', 'den'], ['em'])
    mm(P, pm[:, 64:128], R['su'][:], mkb[:], True, True, ['su', 'mkb'], ['pm_b'])
    mm(P, pm[:, 128:192], R['onesb'][:], mkb[:], True, True, ['onesb', 'mkb'], ['pm_c'])
    dve(lambda e: e.tensor_tensor(out=rk[:], in0=pm[:, 64:128], in1=R['cnt'][:], op=ALU.add), ['pm_b', 'cnt', 'rk'], ['rk'])
    dve(lambda e: e.tensor_tensor(out=R['cnt'][:], in0=pm[:, 128:192], in1=R['cnt'][:], op=ALU.add), ['pm_c', 'cnt'], ['cnt'])
    for k in range(8):
        dve(lambda e, k=k: e.tensor_scalar(out=oh[:], in0=R['iota'][:], scalar1=meta[:, k:k + 1], scalar2=None, op0=ALU.is_equal),
            ['iota', 'meta', 'oh'], ['oh'])
        dve(lambda e, k=k: e.scalar_tensor_tensor(out=jk[:], in0=oh[:], scalar=1.0, in1=rk[:], op0=ALU.mult, op1=ALU.mult, accum_out=meta[:, 8 + k:9 + k]), ['oh', 'rk', 'jk', 'meta'], ['jk', 'meta'])
        dve(lambda e, k=k: e.scalar_tensor_tensor(out=jk[:], in0=oh[:], scalar=1.0, in1=em[:], op0=ALU.mult, op1=ALU.mult, accum_out=meta[:, 16 + k:17 + k]), ['oh', 'em', 'jk', 'meta'], ['jk', 'meta'])
    P.dma('sp', lambda e: e.dma_start(out=T['meta'][i * 128:(i + 1) * 128, :], in_=meta[:]), reads=['meta'])


def phase_S0(P, T, G):
    st32 = Rot(P, 2, [128, 1024], F32, 's32')
    st16 = Rot(P, 1, [128, 1024], BF16, 's16')
    n = 0
    for e in range(65):
        for (src, shsrc, dst) in (('exp_gate', 'sh_gate', 'wg16'), ('exp_up', 'sh_up', 'wu16'), ('exp_down', 'sh_down', 'wd16')):
            s_ap = T[src][e] if e < 64 else T[shsrc]
            f = 256 if dst != 'wd16' else D
            rows = 512 if dst != 'wd16' else 128
            c0 = {'wg16': 0, 'wu16': 2048, 'wd16': 4096}[dst]
            for hh in range(2):
                a, ak = st32.next()
                c, ck = st16.next()
                src_ap = s_ap[hh * rows:(hh + 1) * rows, :].rearrange("(k p) f -> p k f", p=128)
                q = 'sp' if n % 2 == 0 else 'act'
                P.dma(q, lambda e_, a=a, src_ap=src_ap, f=f: e_.dma_start(out=a[:].rearrange("p (k f) -> p k f", f=f), in_=src_ap), writes=[ak])
                eng = ('dve', 'pool')[n % 2]
                P.op(eng, lambda e_, a=a, c=c: e_.tensor_copy(out=c[:], in_=a[:]), reads=[ak], writes=[ck])
                cc = c0 + hh * 1024
                P.dma('act' if n % 2 == 0 else 'sp', lambda e_, c=c, cc=cc, e=e: e_.dma_start(out=T['wall16'][e * 128:(e + 1) * 128, cc:cc + 1024], in_=c[:]), reads=[ck])
                n += 1
                yield


def phase_S(P, T, G):
    identb = G['identb']
    dve = lambda fn, r, w: P.op('dve', fn, reads=r, writes=w)
    cnt = P.sb([128, 64], F32)
    ci = P.sb([128, 64], I32)
    pad = P.sb([128, 64], F32)
    pend = P.sb([128, 64], F32)
    pst = P.sb([128, 64], F32)
    ones64f = P.sb([128, 64], F32)
    iota = P.sb([128, 64], F32)
    bst = P.sb([128, NBLK], F32)
    bef = P.sb([128, NBLK], F32)
    bei = P.sb([128, NBLK], I32)
    P.dma('sp', lambda e: e.dma_start(out=cnt[:], in_=T['cntd']), writes=['cnt'])
    P.dma('sp', lambda e: e.dma_start(out=iota[:], in_=T['k_rel'][0:1, 0:64].rearrange("o f -> (o f)").partition_broadcast(128)), writes=['iota'])
    P.dma('sp', lambda e: e.dma_start(out=bst[:], in_=T['k_bst'].partition_broadcast(128)), writes=['bst'])
    dve(lambda e: e.memset(ones64f[:], 1.0), [], ['ones64f'])
    dve(lambda e: e.tensor_scalar(out=pad[:], in0=cnt[:], scalar1=127.0, scalar2=None, op0=ALU.add), ['cnt'], ['pad'])
    dve(lambda e: e.tensor_copy(out=ci[:], in_=pad[:]), ['pad'], ['ci'])
    dve(lambda e: e.tensor_scalar(out=ci[:], in0=ci[:], scalar1=7, scalar2=None, op0=ALU.arith_shift_right), ['ci'], ['ci'])
    dve(lambda e: e.tensor_scalar(out=ci[:], in0=ci[:], scalar1=7, scalar2=None, op0=ALU.logical_shift_left), ['ci'], ['ci'])
    dve(lambda e: e.tensor_copy(out=pad[:], in_=ci[:]), ['ci', 'pad'], ['pad'])
    dve(lambda e: e.tensor_tensor_scan(out=pend[:], data0=ones64f[:], data1=pad[:], initial=0.0, op0=ALU.mult, op1=ALU.add),
        ['ones64f', 'pad'], ['pend'])
    dve(lambda e: e.tensor_tensor(out=pst[:], in0=pend[:], in1=pad[:], op=ALU.subtract), ['pend', 'pad'], ['pst'])
    for ex in range(64):
        if ex == 0:
            dve(lambda e: e.tensor_scalar(out=bef[:], in0=bst[:], scalar1=pend[:, 0:1], scalar2=None, op0=ALU.is_ge), ['bst', 'pend'], ['bef'])
        else:
            dve(lambda e, ex=ex: e.scalar_tensor_tensor(out=bef[:], in0=bst[:], scalar=pend[:, ex:ex + 1], in1=bef[:], op0=ALU.is_ge, op1=ALU.add),
                ['bst', 'pend', 'bef'], ['bef'])
    dve(lambda e: e.tensor_scalar(out=bef[:], in0=bef[:], scalar1=63.0, scalar2=None, op0=ALU.min), ['bef'], ['bef'])
    pcol = P.sb([128, 1], F32)
    widxf = P.sb([128, NBLK], F32)
    widx = P.sb([128, NBLK], I32)
    P.dma('sp', lambda e: e.dma_start(out=pcol[:], in_=T['k_rel'][:, 0:1], allow_slow_non_contiguous=True), writes=['pcol'])
    dve(lambda e: e.tensor_scalar(out=pcol[:], in0=pcol[:], scalar1=-1.0, scalar2=None, op0=ALU.mult), ['pcol'], ['pcol'])
    dve(lambda e: e.tensor_scalar(out=widxf[:], in0=bef[:], scalar1=128.0, scalar2=pcol[:, 0:1], op0=ALU.mult, op1=ALU.add), ['bef', 'pcol'], ['widxf'])
    chg = P.sb([128, NBLK], F32)
    dve(lambda e: e.memset(chg[:], 1.0), [], ['chg'])
    dve(lambda e: e.tensor_tensor(out=chg[:, 3:NBLK], in0=bef[:, 3:NBLK], in1=bef[:, 0:NBLK - 3], op=ALU.not_equal), ['bef', 'chg'], ['chg'])
    dve(lambda e: e.scalar_tensor_tensor(out=widxf[:], in0=widxf[:], scalar=-1.0e6, in1=chg[:], op0=ALU.add, op1=ALU.mult), ['widxf', 'chg'], ['widxf'])
    dve(lambda e: e.tensor_scalar(out=widxf[:], in0=widxf[:], scalar1=1.0e6, scalar2=None, op0=ALU.add), ['widxf'], ['widxf'])
    dve(lambda e: e.tensor_copy(out=widx[:], in_=widxf[:]), ['widxf'], ['widx'])
    d8i = P.sb([128, NT, 8], I32)
    gw8 = P.sb([128, NT, 8], F32)
    mrot = Rot(P, 2, [128, 24], F32, 'meta')
    hrot = Rot(P, 2, [128, D], BF16, 'htok')
    oh, jk = P.sb([128, 64], F32), P.sb([128, 64], F32)
    d8f = P.sb([128, 8], F32)
    for i in range(NT):
        tsl = slice(i * 128, (i + 1) * 128)
        mt, mtk = mrot.next()
        ht, htk = hrot.next()
        P.dma('sp', lambda e, mt=mt, tsl=tsl: e.dma_start(out=mt[:], in_=T['meta'][tsl, :]), writes=[mtk])
        P.dma('act', lambda e, ht=ht, tsl=tsl: e.dma_start(out=ht[:], in_=T['h2tok'][tsl, :]), writes=[htk])
        for k in range(8):
            dve(lambda e, mt=mt, k=k: e.tensor_scalar(out=oh[:], in0=iota[:], scalar1=mt[:, k:k + 1], scalar2=None, op0=ALU.is_equal),
                ['iota', mtk, 'oh'], ['oh'])
            dve(lambda e, k=k: e.scalar_tensor_tensor(out=jk[:], in0=oh[:], scalar=1.0, in1=pst[:], op0=ALU.mult, op1=ALU.mult, accum_out=d8f[:, k:k + 1]), ['oh', 'pst', 'jk', 'd8f'], ['jk', 'd8f'])
        dve(lambda e, mt=mt: e.tensor_tensor(out=d8f[:], in0=d8f[:], in1=mt[:, 8:16], op=ALU.add), ['d8f', mtk], ['d8f'])
        dve(lambda e, i=i: e.tensor_copy(out=d8i[:, i, :], in_=d8f[:]), ['d8f'], [f'd8i{i}'])
        dve(lambda e, mt=mt, i=i: e.tensor_copy(out=gw8[:, i, :], in_=mt[:, 16:24]), [mtk], [f'gw{i}'])
        for k in range(8):
            P.dma('pool', lambda e, ht=ht, i=i, k=k: e.indirect_dma_start(
                out=T['Xs'], out_offset=bass.IndirectOffsetOnAxis(ap=d8i[:, i, k:k + 1], axis=0), in_=ht[:], in_offset=None),
                reads=[htk, f'd8i{i}'])
    wgu = Rot(P, 3, [128, 6144], BF16, 'wgu')
    wsh = P.sb([128, 6144], BF16)
    P.dma('sp', lambda e: e.dma_start(out=wsh[:], in_=T['wall16'][64 * 128:65 * 128, :]), writes=['wsh'])
    xbr = Rot(P, 4, [128, D], BF16, 'xb')
    xTr = Rot(P, 2, [128, 8, 128], BF16, 'xT')
    sgr_ = Rot(P, 2, [128, 256], F32, 'sg')
    acr = Rot(P, 2, [128, 256], BF16, 'ac')
    aTr = Rot(P, 2, [128, 2, 128], BF16, 'aT')
    ybr = Rot(P, 2, [128, D], BF16, 'yb')
    ptx = Rot(P, 1, [128, 8, 128], BF16, 'ptx', psum=True)
    pgu = Rot(P, 2, [128, 512], F32, 'pgu', psum=True)
    pta = Rot(P, 1, [128, 2, 128], BF16, 'pta', psum=True)
    pyd = Rot(P, 3, [128, 512], F32, 'pyd', psum=True)
    regn = [0]

    P.emit(keep=True)
    hold = {}
    blocks = [('r', b) for b in range(NBLK)] + [('s', i) for i in range(NT)]
    st = {}

    ld = {}

    def stageL(kind, b):
        xb, xk = xbr.next()
        if kind == 'r':
            wl, wk = wgu.next()

            def gat(e):
                if 'bc' not in hold:
                    hold['bc'] = e.alloc_register("bc_reg")
                    e.reg_mov(hold['bc'], 65 * 128 - 1)
                return e.indirect_dma_start(out=wl[:], out_offset=None, in_=T['wall16'],
                                            in_offset=bass.IndirectOffsetOnAxis(ap=widx[:, b:b + 1], axis=0),
                                            bounds_check=hold['bc'], oob_is_err=False)
            P.dma('pool', gat, reads=['widx'], writes=[wk])
            P.dma('sp', lambda e: e.dma_start(out=xb[:], in_=T['Xs'][b * 128:(b + 1) * 128, :]), writes=[xk])
        else:
            wl, wk = wsh, 'wsh'
            P.dma('sp', lambda e: e.dma_start(out=xb[:], in_=T['h2tok'][b * 128:(b + 1) * 128, :]), writes=[xk])
        ld[(kind, b)] = (xb, xk, wl, wk)

    def stageA(kind, b):
        xb, xk, wl, wk = ld.pop((kind, b))
        wga, wua = wl[:, 0:2048], wl[:, 2048:4096]
        wd, wdk = wl[:, 4096:6144].rearrange("p (k f) -> p k f", f=D), wk
        px, pxk = ptx.next()
        for k in range(8):
            P.op('pe', lambda e, k=k: e.transpose(out=px[:, k, :], in_=xb[:, k * 128:(k + 1) * 128], identity=identb[:]), reads=[xk], writes=[pxk])
        xT, xTk = xTr.next()
        P.op('act', lambda e: e.activation(out=xT[:], in_=px[:], func=AF.Copy), reads=[pxk], writes=[xTk])
        pg_, pgk = pgu.next()
        for k in range(8):
            mm(P, pg_[:, 0:256], xT[:, k, :], wga[:, k * 256:(k + 1) * 256], k == 0, False, [xTk, wk], [pgk])
        for k in range(8):
            P.op('pe', lambda e, k=k: e.matmul(pg_[:, 256:512], lhsT=xT[:, k, :], rhs=wua[:, k * 256:(k + 1) * 256], start=False, stop=(k == 7), skip_group_check=True), reads=[xTk, wk], writes=[pgk])
        st[(kind, b)] = (pg_, pgk, wd, wdk)

    def stageB(kind, b):
        pg_, pgk, wd, wdk = st.pop((kind, b))
        sg, sgk = sgr_.next(); ac, ack = acr.next()
        P.op('act', lambda e: e.activation(out=sg[:], in_=pg_[:, 0:256], func=AF.Silu), reads=[pgk], writes=[sgk])
        dve(lambda e: e.tensor_tensor(out=ac[:], in0=pg_[:, 256:512], in1=sg[:], op=ALU.mult), [pgk, sgk], [ack])
        pa, pak = pta.next()
        for ft in range(2):
            P.op('pe', lambda e, ft=ft: e.transpose(out=pa[:, ft, :], in_=ac[:, ft * 128:(ft + 1) * 128], identity=identb[:]), reads=[ack], writes=[pak])
        aT, aTk = aTr.next()
        dve(lambda e: e.tensor_copy(out=aT[:], in_=pa[:]), [pak], [aTk])
        yb, ybk = ybr.next()
        for half in range(2):
            py_, pyk = pyd.next()
            cs_ = slice(half * 512, (half + 1) * 512)
            for ft in range(2):
                mm(P, py_[:], aT[:, ft, :], wd[:, ft, cs_], ft == 0, ft == 1, [aTk, wdk], [pyk])
            if half == 0:
                P.op('act', lambda e, py_=py_, cs_=cs_: e.activation(out=yb[:, cs_], in_=py_[:], func=AF.Copy), reads=[pyk], writes=[ybk])
            else:
                dve(lambda e, py_=py_, cs_=cs_: e.tensor_copy(out=yb[:, cs_], in_=py_[:]), [pyk], [ybk])
        dst = T['Ys'] if kind == 'r' else T['Ysh']
        P.dma('sp', lambda e: e.dma_start(out=dst[b * 128:(b + 1) * 128, :], in_=yb[:]), reads=[ybk])

    stageL(*blocks[0])
    stageL(*blocks[1])
    stageA(*blocks[0])
    for bi in range(len(blocks)):
        if bi + 2 < len(blocks):
            stageL(*blocks[bi + 2])
        if bi + 1 < len(blocks):
            stageA(*blocks[bi + 1])
        stageB(*blocks[bi])
    P.emit(keep=True)
    g2b = P.sb([128, D], F32)
    fing = P.sb([128, D], F32)
    P.dma('sp', lambda e: e.dma_start(out=g2b[:], in_=T['modd'][:, 5120:6144]), writes=['g2b'])
    P.dma('sp', lambda e: e.dma_start(out=fing[:], in_=T['final_g'].partition_broadcast(128)), writes=['fing'])
    xr = Rot(P, 2, [128, D], F32, 'x1')
    grot = Rot(P, 4, [128, D], BF16, 'gat')
    shr = Rot(P, 2, [128, D], BF16, 'shr')
    acc = P.sb([128, D], F32)
    junk, ss, rs = P.sb([128, D], F32), P.sb([128, 1], F32), P.sb([128, 1], F32)
    for i in range(NT):
        tsl = slice(i * 128, (i + 1) * 128)
        xt, xk = xr.next()
        sh, shk = shr.next()
        P.dma('sp', lambda e, xt=xt, tsl=tsl: e.dma_start(out=xt[:], in_=T['x1'][tsl, :]), writes=[xk])
        P.dma('act', lambda e, sh=sh, tsl=tsl: e.dma_start(out=sh[:], in_=T['Ysh'][tsl, :]), writes=[shk])
        for k in range(8):
            gt, gtk = grot.next()
            P.dma('pool', lambda e, gt=gt, i=i, k=k: e.indirect_dma_start(
                out=gt[:], out_offset=None, in_=T['Ys'], in_offset=bass.IndirectOffsetOnAxis(ap=d8i[:, i, k:k + 1], axis=0)),
                reads=[f'd8i{i}'], writes=[gtk])
            if k == 0:
                dve(lambda e, gt=gt, i=i, sh=sh: e.scalar_tensor_tensor(out=acc[:], in0=gt[:], scalar=gw8[:, i, 0:1], in1=sh[:], op0=ALU.mult, op1=ALU.add),
                    [gtk, f'gw{i}', shk, 'acc'], ['acc'])
            else:
                dve(lambda e, gt=gt, i=i, k=k: e.scalar_tensor_tensor(out=acc[:], in0=gt[:], scalar=gw8[:, i, k:k + 1], in1=acc[:], op0=ALU.mult, op1=ALU.add),
                    [gtk, f'gw{i}', 'acc'], ['acc'])
        dve(lambda e: e.tensor_tensor(out=acc[:], in0=acc[:], in1=g2b[:], op=ALU.mult), ['acc', 'g2b'], ['acc'])
        P.op('pool', lambda e, xt=xt: e.tensor_tensor(out=xt[:], in0=xt[:], in1=acc[:], op=ALU.add), reads=['acc', xk], writes=[xk])
        rms_rstd(P, xt, xk, junk, ss, rs, 'f')
        dve(lambda e, xt=xt: e.scalar_tensor_tensor(out=xt[:], in0=xt[:], scalar=rs[:, 0:1], in1=fing[:], op0=ALU.mult, op1=ALU.mult),
            [xk, 'rsf', 'fing'], [xk])
        P.dma('sp', lambda e, xt=xt, tsl=tsl: e.dma_start(out=T['out'][tsl, :], in_=xt[:]), reads=[xk])


SCRATCH = [
    ('zq', [512, S], BF16), ('zk', [512, S], BF16), ('zv', [S, 512], BF16), ('ziq', [512, S], BF16),
    ('zik', [32, S], BF16), ('ziw', [S, 16], F32), ('zr', [1792, S], F32), ('zga', [1024, S], BF16),
    ('zgr', [1024, S], BF16), ('modd', [128, 6 * D], F32), ('attnT', [512, S], BF16), ('rwT', [512, S], BF16),
    ('x1', [S, D], F32), ('h2T', [D, S], BF16), ('h2tok', [S, D], BF16), ('meta', [S, 24], F32), ('cntd', [128, 64], F32),
    ('Xs', [NBLK * 128, D], BF16), ('Ys', [NBLK * 128, D], BF16), ('Ysh', [S, D], BF16),
    ('wall16', [65 * 128, 6144], BF16),
]

INPUT_SHAPES = [
    ('x', [S, D]), ('c_col', [128, 8]), ('ada_w', [D, 6 * D]), ('ada_b', [1, 6 * D]), ('norm1_g', [D]),
    ('w_in', [D, NIN]), ('rel_bias', [256]), ('tshift_mu', [1792]), ('decay_w0', [512]), ('decay_up', [64, 512]),
    ('iclr_a0', [512]), ('iclr_up', [64, 512]), ('gate_up', [128, 512]), ('k_k', [512]), ('k_a', [512]),
    ('r_k', [512]), ('lnx_g', [512]), ('lnx_b', [512]), ('w_attn_br', [512, D]), ('w_rwkv_br', [512, D]),
    ('w_out', [D, D]), ('norm2_g', [D]), ('router_w', [D, 64]), ('router_bias', [64]),
    ('exp_gate', [64, D, 256]), ('exp_up', [64, D, 256]), ('exp_down', [64, 256, D]),
    ('sh_gate', [D, 256]), ('sh_up', [D, 256]), ('sh_down', [256, D]), ('final_g', [D]),
    ('k_ident', [128, 128]), ('k_rel', [128, 256]), ('k_masks', [128, 896]), ('k_reset', [64, 512]), ('k_bst', [NBLK]),
]


def build(debug_outs=(), stop_after=None):
    nc = bass.Bass("TRN2", target_bir_lowering=False)
    T = {}
    for name, shp in INPUT_SHAPES:
        T[name] = nc.dram_tensor(name, shp, F32, kind="ExternalInput").ap()
    for name, shp, dt in SCRATCH:
        kind = "ExternalOutput" if name in debug_outs else "Internal"
        T[name] = nc.dram_tensor(name, shp, dt, kind=kind).ap()
    T['out'] = nc.dram_tensor('out', [S, D], F32, kind="ExternalOutput").ap()
    if 'dbg_y' in debug_outs:
        T['dbg_y'] = nc.dram_tensor('dbg_y', [64, 8, 512], F32, kind="ExternalOutput").ap()
        T['dbg_bon'] = nc.dram_tensor('dbg_bon', [64, 8, 512], BF16, kind="ExternalOutput").ap()
        T['dbg_g'] = nc.dram_tensor('dbg_g', [64, 8, 512], BF16, kind="ExternalOutput").ap()
        T['dbg_AR'] = nc.dram_tensor('dbg_AR', [64, 8, 4, 256], BF16, kind="ExternalOutput").ap()
        T['dbg_BK'] = nc.dram_tensor('dbg_BK', [64, 8, 4, 256], BF16, kind="ExternalOutput").ap()
    P = Prog(nc)
    G = {}
    G['E'] = P.gsb([128, 8, 256], F32)
    G['b31'] = P.gsb([128, 8], F32)
    G['ones_row'] = P.gsb([1, 128], F32)
    G['identf'] = P.gsb([128, 128], F32)
    G['identb'] = P.gsb([128, 128], BF16)
    G['eps'] = P.gsb([128, 1], F32)
    G_EPS[0] = G['eps']
    P.op('dve', lambda e: e.memset(G['ones_row'][:], 1.0), writes=['ones_row'])
    P.op('dve', lambda e: e.memset(G['eps'][:], 1e-6), writes=['eps'])
    P.dma('sp', lambda e: e.dma_start(out=G['identf'][:], in_=T['k_ident']), writes=['identf'])
    P.op('dve', lambda e: e.tensor_copy(out=G['identb'][:], in_=G['identf'][:]), reads=['identf'], writes=['identb'])
    G['sparse'] = SPARSE
    phase_A(P, T, G)
    P.emit()
    if stop_after == 'A':
        P.emit(); P.finish(); return nc
    G['sparse'] = SPARSE
    phase_BC(P, T, G)
    P.emit()
    if stop_after == 'C':
        P.finish(); return nc
    phase_D(P, T, G)
    P.emit()
    if stop_after == 'D':
        P.finish(); return nc
    phase_E(P, T, G)
    P.emit()
    if stop_after == 'E':
        P.finish(); return nc
    G['sparse'] = SPARSE
    phase_F(P, T, G)
    P.emit()
    if stop_after == 'F':
        P.finish(); return nc
    if SPARSE:
        phase_S(P, T, G)
        P.emit()
        P.finish()
        return nc
    G['yacc'] = P.gsb([128, NT, D], F32)
    if stop_after in ('router', 'exp1'):
        G['gstop'] = stop_after
        G['nexp'] = 1
    if stop_after == 'router':
        phase_G(P, T, G); P.emit(); P.finish(); return nc
    if stop_after == 'exp1':
        phase_G(P, T, G); P.emit(); P.finish(); return nc
    phase_G(P, T, G)
    P.emit()
    phase_H(P, T, G)
    P.emit()
    P.finish()
    return nc


def host_inputs(inputs, b):
    m = {}
    f = lambda a: np.ascontiguousarray(np.asarray(a, dtype=np.float32))
    m['x'] = f(inputs['x'][b])
    m['c_col'] = f(np.asarray(inputs['c'][b]).reshape(8, 128).T)
    for name, shp in INPUT_SHAPES:
        if name in ('x', 'c_col', 'k_ident', 'k_rel', 'k_masks', 'k_reset', 'k_bst'):
            continue
        a = np.asarray(inputs[name])
        m[name] = f(a.reshape(shp))
    m['k_ident'] = np.eye(128, dtype=np.float32)
    ii = np.arange(128)
    su = (ii[:, None] < ii[None, :]).astype(np.float32)
    iu = (ii[:, None] <= ii[None, :]).astype(np.float32)
    sl = (ii[:, None] > ii[None, :]).astype(np.float32)
    m['k_masks'] = np.ascontiguousarray(np.concatenate([su, iu, su, iu, sl, np.zeros((128, 256), np.float32)], axis=1))
    rs_ = np.ones((64, 512), np.float32); rs_[:, ::128] = 0.0
    m['k_reset'] = rs_
    m['k_bst'] = (np.arange(NBLK, dtype=np.float32) * 128.0)
    m['k_rel'] = (np.arange(256, dtype=np.float32)[None, :] - np.arange(128, dtype=np.float32)[:, None])
    return m


def kernel(**inputs):
    nc = build()
    in_maps = [host_inputs(inputs, b) for b in range(8)]
    res = run_bass_kernel_spmd(nc, in_maps, core_ids=list(range(8)))
    return np.stack([np.asarray(r['out']) for r in res.results], axis=0).astype(np.float32)
```

```python
import contextlib
import numpy as np
import concourse.bass as bass
import concourse.mybir as mybir

F32 = mybir.dt.float32
BF16 = mybir.dt.bfloat16
I32 = mybir.dt.int32
U32 = mybir.dt.uint32
AF = mybir.ActivationFunctionType
ALU = mybir.AluOpType
AX = mybir.AxisListType

N_DMA_SEMS = 11


class Prog:
    ENGS = ('pe', 'act', 'dve', 'pool', 'sp')

    def __init__(self, nc):
        self.nc = nc
        self.ops = {e: [] for e in self.ENGS}
        self.cnt = {e: 0 for e in self.ENGS}
        self.waited = {e: {} for e in self.ENGS}
        self.res = {}
        self.dma_tot = [0] * N_DMA_SEMS
        self.dma_rr = 0
        self.stack = contextlib.ExitStack()
        self.gstack = contextlib.ExitStack()
        self.nsb = 0
        self.nphase = 0
        self.sems = None

    def _new_sems(self):
        nc = self.nc
        self.sems = {}
        for e in self.ENGS:
            self.sems[e] = self.gstack.enter_context(nc.semaphore(f"s_{e}_{self.nphase}"))
        for i in range(N_DMA_SEMS):
            self.sems[('dma', i)] = self.gstack.enter_context(nc.semaphore(f"s_dma{i}_{self.nphase}"))
        self.cnt = {e: 0 for e in self.ENGS}
        self.waited = {e: {} for e in self.ENGS}
        self.dma_tot = [0] * N_DMA_SEMS
        self.dma_rr = 0

    def gsb(self, shape, dt, name=None):
        self.nsb += 1
        return self.gstack.enter_context(self.nc.sbuf_tensor(name or f"gsb{self.nsb}", list(shape), dt))

    def sb(self, shape, dt, name=None):
        self.nsb += 1
        return self.stack.enter_context(self.nc.sbuf_tensor(name or f"sb{self.nsb}", list(shape), dt))

    def ps(self, shape, dt, name=None):
        self.nsb += 1
        return self.stack.enter_context(self.nc.psum_tensor(name or f"ps{self.nsb}", list(shape), dt))

    def _deps(self, reads, writes):
        deps = {}
        def add(d):
            if d is None:
                return
            k, v = d
            if deps.get(k, 0) < v:
                deps[k] = v
        for k in reads:
            r = self.res.get(k)
            if r:
                add(r['w'])
        for k in writes:
            r = self.res.get(k)
            if r:
                add(r['w'])
                for d in r['r']:
                    add(d)
        return deps

    def _emit_waits(self, eng, deps):
        w = self.waited[eng]
        for k, v in deps.items():
            if w.get(k, 0) < v:
                w[k] = v
                self.ops[eng].append(('wait', k, v))

    def _update(self, dep, reads, writes):
        for k in reads:
            r = self.res.setdefault(k, {'w': None, 'r': []})
            r['r'] = [d for d in r['r'] if d[0] != dep[0]] + [dep]
        for k in writes:
            self.res[k] = {'w': dep, 'r': []}

    def op(self, eng, fn, reads=(), writes=()):
        if self.sems is None:
            self._new_sems()
        deps = self._deps(reads, writes)
        if eng == 'pe':
            deps.pop('pe', None)
        self._emit_waits(eng, deps)
        self.cnt[eng] += 1
        dep = (eng, self.cnt[eng])
        self.ops[eng].append(('op', fn))
        self._update(dep, reads, writes)
        return dep

    def dma(self, q, fn, reads=(), writes=()):
        if self.sems is None:
            self._new_sems()
        deps = self._deps(reads, writes)
        s = self.dma_rr
        self.dma_rr = (self.dma_rr + 1) % N_DMA_SEMS
        key = ('dma', s)
        if self.dma_tot[s] > 0:
            if deps.get(key, 0) < self.dma_tot[s]:
                deps[key] = self.dma_tot[s]
        self._emit_waits(q, deps)
        self.dma_tot[s] += 16
        dep = (key, self.dma_tot[s])
        self.ops[q].append(('dma', fn, s))
        self._update(dep, reads, writes)
        return dep

    def emit(self, keep=False):
        nc = self.nc
        with contextlib.ExitStack() as st:
            sems = self.sems
            self.nphase += 1
            block = st.enter_context(nc.Block(f"ph{self.nphase}"))
            engobj = {'pe': nc.tensor, 'act': nc.scalar, 'dve': nc.vector, 'pool': nc.gpsimd, 'sp': nc.sync}
            fin = {}
            for e in self.ENGS:
                if e != 'sp' and self.cnt[e] > 0:
                    fin[e] = self.cnt[e]
            for i in range(N_DMA_SEMS):
                if self.dma_tot[i] > 0:
                    fin[('dma', i)] = self.dma_tot[i]
            self._emit_waits('sp', fin)

            def run(e):
                eo = engobj[e]
                for item in self.ops[e]:
                    if item[0] == 'wait':
                        eo.wait_ge(sems[item[1]], item[2])
                    elif item[0] == 'op':
                        item[1](eo).then_inc(sems[e], 1)
                    else:
                        item[1](eo).then_inc(sems[('dma', item[2])], 16)

            @block.tensor
            def _(t):
                run('pe')

            @block.scalar
            def _(t):
                run('act')

            @block.vector
            def _(t):
                run('dve')

            @block.gpsimd
            def _(t):
                run('pool')

            @block.sync
            def _(t):
                run('sp')
        self.ops = {e: [] for e in self.ENGS}
        self.res = {}
        if keep:
            return
        self.stack.close()
        self.stack = contextlib.ExitStack()
        self.sems = None

    def finish(self):
        self.gstack.close()

from concourse.bass_utils import run_bass_kernel_spmd
import ml_dtypes

SPARSE = True
S = 4096
D = 1024
NT = S // 128
NIN = 5936


class Rot:
    def __init__(self, P, n, shape, dt, name, psum=False):
        self.tiles = [(P.ps(shape, dt) if psum else P.sb(shape, dt)) for _ in range(n)]
        self.name = name
        self.i = 0

    def next(self):
        t = self.tiles[self.i % len(self.tiles)]
        k = f"{self.name}{self.i % len(self.tiles)}"
        self.i += 1
        return t, k


class Rot2(Rot):
    def __init__(self, P, n, shape, dt, name):
        self.tiles = [(P.sb(shape, dt), P.sb(shape, dt)) for _ in range(n)]
        self.name = name
        self.i = 0


def mm(P, out, lhsT, rhs, start, stop, reads, writes):
    P.op('pe', lambda e: e.matmul(out, lhsT=lhsT, rhs=rhs, start=start, stop=stop), reads=reads, writes=writes)


def phase_A(P, T, G):
    mod_bc = P.sb([128, 6 * D], F32)
    ccol = P.sb([128, 8], F32)
    scol = P.sb([128, 8], F32)
    adab = P.sb([1, 6144], F32)
    modrow = P.sb([1, 6144], F32)
    ngb = P.sb([128, 1024], F32)
    P.dma('sp', lambda e: e.dma_start(out=ccol[:], in_=T['c_col']), writes=['ccol'])
    P.dma('sp', lambda e: e.dma_start(out=adab[:], in_=T['ada_b']), writes=['adab'])
    P.op('act', lambda e: e.activation(out=scol[:], in_=ccol[:], func=AF.Silu), reads=['ccol'], writes=['scol'])
    wrot = Rot(P, 2, [128, 8, 512], F32, 'aw')
    psr = Rot(P, 2, [1, 512], F32, 'psr', psum=True)
    psb = Rot(P, 2, [128, 512], F32, 'psb', psum=True)
    adaw = T['ada_w'].rearrange("(k p) n -> p k n", p=128)
    for n in range(12):
        wb, wk = wrot.next()
        P.dma('sp' if n % 2 == 0 else 'act',
              lambda e, wb=wb, n=n: e.dma_start(out=wb[:], in_=adaw[:, :, n * 512:(n + 1) * 512]), writes=[wk])
        pr, pk = psr.next()
        for k in range(8):
            mm(P, pr[:], scol[:, k:k + 1], wb[:, k, :], k == 0, k == 7, [wk, 'scol'], [pk])
        sl = slice(n * 512, (n + 1) * 512)
        P.op('dve', lambda e, pr=pr, sl=sl: e.tensor_tensor(out=modrow[0:1, sl], in0=pr[:], in1=adab[0:1, sl], op=ALU.add),
             reads=[pk, 'adab'], writes=[f'modrow{n}'])
        pb, pbk = psb.next()
        mm(P, pb[:], G['ones_row'][:], modrow[0:1, sl], True, True, [f'modrow{n}'], [pbk])
        P.op('act', lambda e, pb=pb, sl=sl: e.activation(out=mod_bc[:, sl], in_=pb[:], func=AF.Copy),
             reads=[pbk], writes=[f'mod{n}'])
    for (gname, c0, deps) in (('norm1_g', 1024, ['mod2', 'mod3']), ('norm2_g', 4096, ['mod8', 'mod9'])):
        P.dma('sp', lambda e, gname=gname: e.dma_start(out=ngb[:], in_=T[gname].partition_broadcast(128)), writes=['ngb'])
        P.op('dve', lambda e, c0=c0: e.scalar_tensor_tensor(out=mod_bc[:, c0:c0 + 1024], in0=mod_bc[:, c0:c0 + 1024],
                                                            scalar=1.0, in1=ngb[:], op0=ALU.add, op1=ALU.mult),
             reads=deps + ['ngb'], writes=deps)
    P.dma('sp', lambda e: e.dma_start(out=T['modd'], in_=mod_bc[:]), reads=[f'mod{n}' for n in range(12)])


def rms_rstd(P, xt, xk, junk, ss, rs, tag):
    P.op('act', lambda e: e.activation(out=junk[:], in_=xt[:], func=AF.Square, accum_out=ss[:]),
         reads=[xk], writes=['junk' + tag, 'ss' + tag])
    P.op('act', lambda e: e.activation(out=ss[:], in_=ss[:], func=AF.Sqrt, scale=1.0 / D, bias=G_EPS[0][:, 0:1]),
         reads=['ss' + tag], writes=['ss' + tag])
    P.op('dve', lambda e: e.reciprocal(out=rs[:], in_=ss[:]), reads=['ss' + tag], writes=['rs' + tag])


G_EPS = [None]


def norm_mod_transpose(P, G, xt, xk, hT, i, g_sl, sh_sl, W):
    mod_bc = W['mod']
    junk, ss, rs, t1, hb, pt = W['junk'], W['ss'], W['rs'], W['t1'], W['hb'], W['pt']
    rms_rstd(P, xt, xk, junk, ss, rs, '')
    P.op('dve', lambda e: e.scalar_tensor_tensor(out=t1[:], in0=xt[:], scalar=rs[:, 0:1], in1=mod_bc[:, g_sl],
                                                 op0=ALU.mult, op1=ALU.mult), reads=[xk, 'rs', 'modl'], writes=['t1'])
    P.op('pool', lambda e: e.tensor_tensor(out=hb[:], in0=t1[:], in1=mod_bc[:, sh_sl], op=ALU.add),
         reads=['t1', 'modl'], writes=['hb'])
    for k in range(8):
        P.op('pe', lambda e, k=k: e.transpose(out=pt[:, k, :], in_=hb[:, k * 128:(k + 1) * 128], identity=G['identb'][:]),
             reads=['hb'], writes=['pt'])
    P.op('act', lambda e: e.activation(out=hT[:, :, i * 128:(i + 1) * 128], in_=pt[:], func=AF.Copy),
         reads=['pt'], writes=[f'hT{i // 4}'])


def phase_BC(P, T, G):
    hT = P.sb([128, 8, S], BF16)
    W = dict(junk=P.sb([128, D], F32), ss=P.sb([128, 1], F32), rs=P.sb([128, 1], F32), t1=P.sb([128, D], F32),
             hb=P.sb([128, D], BF16), pt=P.ps([128, 8, 128], BF16))
    W['mod'] = P.sb([128, 2048], F32)
    P.dma('sp', lambda e: e.dma_start(out=W['mod'][:], in_=T['modd'][:, 0:2048]), writes=['modl'])
    xrot = Rot(P, 2, [128, D], F32, 'x')
    s0 = phase_D0(P, T, G)

    def s0step(n=1):
        for _ in range(n):
            try:
                next(s0)
            except StopIteration:
                return
    for i in range(NT):
        xt, xk = xrot.next()
        P.dma('sp', lambda e, xt=xt, i=i: e.dma_start(out=xt[:], in_=T['x'][i * 128:(i + 1) * 128, :]), writes=[xk])
        norm_mod_transpose(P, G, xt, xk, hT, i, slice(1024, 2048), slice(0, 1024), W)
        s0step()
    win = T['w_in'].rearrange("(k p) n -> p k n", p=128)
    segs = [('zq', 0, 512, BF16), ('zk', 512, 512, BF16), ('ziq', 1536, 512, BF16), ('zik', 2048, 32, BF16),
            ('zr', 2096, 1792, F32), ('zga', 3888, 1024, BF16), ('zgr', 4912, 1024, BF16)]
    wrot = Rot(P, 2, [128, 8, 128], BF16, 'w')
    psrot = Rot(P, 3, [128, 512], F32, 'ps', psum=True)
    strot = {BF16: Rot(P, 3, [128, 512], BF16, 'stb'), F32: Rot(P, 3, [128, 512], F32, 'stf')}
    ev = 0
    for (name, c0, n, dt) in segs:
        for m0 in range(0, n, 128):
            M = min(128, n - m0)
            wt, wk = wrot.next()
            P.dma('pool', lambda e, wt=wt, M=M, a=c0 + m0: e.dma_start(out=wt[:, :, :M], in_=win[:, :, a:a + M]), writes=[wk])
            for tg in range(8):
                ps, pk = psrot.next()
                for k in range(8):
                    mm(P, ps[:M, :], wt[:, k, :M], hT[:, k, tg * 512:(tg + 1) * 512], k == 0, k == 7, [wk, f'hT{tg}'], [pk])
                st, sk = strot[dt].next()
                if ev % 2 == 0:
                    P.op('act', lambda e, st=st, ps=ps, M=M: e.activation(out=st[:M, :], in_=ps[:M, :], func=AF.Copy),
                         reads=[pk], writes=[sk])
                else:
                    P.op('dve', lambda e, st=st, ps=ps, M=M: e.tensor_copy(out=st[:M, :], in_=ps[:M, :]),
                         reads=[pk], writes=[sk])
                ev += 1
                if ev % 2 == 0:
                    s0step()
                P.dma('sp', lambda e, st=st, M=M, name=name, m0=m0, tg=tg:
                      e.dma_start(out=T[name][m0:m0 + M, tg * 512:(tg + 1) * 512], in_=st[:M, :]), reads=[sk])
    wv = P.sb([128, 8, 528], BF16)
    P.dma('pool', lambda e: e.dma_start(out=wv[:, :, 0:512], in_=win[:, :, 1024:1536]), writes=['wv'])
    P.dma('pool', lambda e: e.dma_start(out=wv[:, :, 512:528], in_=win[:, :, 2080:2096]), writes=['wv2'])
    ps2rot = Rot(P, 2, [128, 16], F32, 'ps2', psum=True)
    st2rot = Rot(P, 2, [128, 16], F32, 'st2')
    for i in range(NT):
        ps, pk = psrot.next()
        ps2, pk2 = ps2rot.next()
        for k in range(8):
            mm(P, ps[:], hT[:, k, i * 128:(i + 1) * 128], wv[:, k, 0:512], k == 0, k == 7, ['wv', f'hT{i // 4}'], [pk])
        for k in range(8):
            mm(P, ps2[:], hT[:, k, i * 128:(i + 1) * 128], wv[:, k, 512:528], k == 0, k == 7, ['wv2', f'hT{i // 4}'], [pk2])
        st, sk = strot[BF16].next()
        st2, sk2 = st2rot.next()
        P.op('act', lambda e, st=st, ps=ps: e.activation(out=st[:], in_=ps[:], func=AF.Copy), reads=[pk], writes=[sk])
        P.op('dve', lambda e, st2=st2, ps2=ps2: e.tensor_copy(out=st2[:], in_=ps2[:]), reads=[pk2], writes=[sk2])
        P.dma('sp', lambda e, st=st, i=i: e.dma_start(out=T['zv'][i * 128:(i + 1) * 128, :], in_=st[:]), reads=[sk])
        P.dma('sp', lambda e, st2=st2, i=i: e.dma_start(out=T['ziw'][i * 128:(i + 1) * 128, :], in_=st2[:]), reads=[sk2])
    s0step(1000)


def t5_lo_bounds():
    n = np.arange(256)
    nf = np.maximum(n, 1).astype(np.float32)
    large = 16 + (np.log(nf / np.float32(16)) / np.float32(np.log(8.0)) * np.float32(16)).astype(np.int32)
    large = np.minimum(large, 31)
    bk = np.where(n < 16, n, large)
    return [int(np.min(np.nonzero(bk >= b)[0])) for b in range(1, 32)]


def phase_D0(P, T, G):
    relb = P.sb([128, 256], F32)
    diff = P.sb([128, 248], F32)
    base = P.sb([128, 8], F32)
    relidx = P.sb([128, 256], F32)
    P.dma('sp', lambda e: e.dma_start(out=relb[:], in_=T['rel_bias'].partition_broadcast(128)), writes=['relb'])
    P.dma('sp', lambda e: e.dma_start(out=relidx[:], in_=T['k_rel']), writes=['relidx'])
    P.op('dve', lambda e: e.tensor_tensor(out=diff[:], in0=relb[:, 8:256], in1=relb[:, 0:248], op=ALU.subtract),
         reads=['relb'], writes=['diff'])
    P.op('dve', lambda e: e.tensor_tensor(out=base[:], in0=relb[:, 0:8], in1=relb[:, 248:256], op=ALU.subtract),
         reads=['relb'], writes=['base'])
    P.op('dve', lambda e: e.tensor_copy(out=G['b31'][:], in_=relb[:, 248:256]), reads=['relb'], writes=['b31'])
    E = G['E']
    irot = Rot(P, 2, [128, 256], F32, 'ind')
    los = t5_lo_bounds()
    for b in range(1, 32):
        ind, ik = irot.next()
        P.op('dve', lambda e, ind=ind, lo=float(los[b - 1]): e.tensor_scalar(out=ind[:], in0=relidx[:], scalar1=lo, scalar2=None,
                                                                              op0=ALU.is_ge), reads=['relidx'], writes=[ik])
        for h in range(8):
            if b == 1:
                P.op('dve', lambda e, ind=ind, h=h: e.tensor_scalar(out=E[:, h, :], in0=ind[:], scalar1=diff[:, h:h + 1],
                                                                    scalar2=base[:, h:h + 1], op0=ALU.mult, op1=ALU.add),
                     reads=[ik, 'diff', 'base'], writes=[f'E{h}'])
            else:
                c = (b - 1) * 8 + h
                P.op('dve', lambda e, ind=ind, h=h, c=c: e.scalar_tensor_tensor(out=E[:, h, :], in0=ind[:], scalar=diff[:, c:c + 1],
                                                                               in1=E[:, h, :], op0=ALU.mult, op1=ALU.add),
                     reads=[ik, 'diff', f'E{h}'], writes=[f'E{h}'])
        yield
    P.op('act', lambda e: e.activation(out=E[:], in_=E[:], func=AF.Exp), reads=[f'E{h}' for h in range(8)],
         writes=[f'E{h}' for h in range(8)])


def phase_D(P, T, G):
    NIT = 14
    KT = P.sb([64, 8, S], BF16)
    V = P.sb([128, NT, 512], BF16)
    ik4 = P.sb([128, S], BF16)
    iw = P.sb([128, NT, 16], F32)
    ones64 = P.sb([128, 64], BF16)
    scs = [P.sb([128, S], F32), P.sb([128, S], F32)]
    maskbs = [P.sb([128, S], BF16), P.sb([128, S], BF16)]
    maskT = P.sb([128, NT, 128], BF16)
    lo, hi, mid, cnt, tmp, thr = [P.sb([128, 1], F32) for _ in range(6)]
    cvec = P.sb([128, NIT + 1], F32)
    dk = P.sb([128, NIT + 1], F32)
    for k in range(NIT + 1):
        P.op('pool', lambda e, k=k: e.memset(cvec[:, k:k + 1], 2.0 ** -(k + 1)), reads=['cvec'], writes=['cvec'])
    P.dma('sp', lambda e: e.dma_start(out=KT[:], in_=T['zk'].rearrange("(h p) t -> p h t", p=64)), writes=['KT'])
    P.dma('act', lambda e: e.dma_start(out=V[:], in_=T['zv'].rearrange("(i p) f -> p i f", p=128)), writes=['V'])
    for i in range(3):
        P.dma('sp', lambda e, i=i: e.dma_start(out=ik4[32 * i:32 * i + 32, :], in_=T['zik']), writes=[f'ik4{i}'])
    P.dma('sp', lambda e: e.dma_start(out=iw[:], in_=T['ziw'].rearrange("(i p) f -> p i f", p=128)), writes=['iw'])
    P.op('dve', lambda e: e.memset(ones64[:], 1.0), writes=['ones64'])
    zq = T['zq'].rearrange("(h p) t -> p h t", p=64)
    ziq = T['ziq'][0:480, :].rearrange("(j p) t -> p j t", p=96)
    attnT = T['attnT'].rearrange("(h p) t -> p h t", p=64)
    qrot = Rot(P, 2, [64, 8, 128], BF16, 'q')
    iqrot = Rot(P, 2, [96, 6, 128], BF16, 'iq')
    dgrot = Rot(P, 2, [128, 16, 128], BF16, 'dg')
    rrot = Rot(P, 3, [128, 512], BF16, 'r')
    psi = Rot(P, 2, [128, 512], F32, 'psi', psum=True)
    pacc = Rot(P, 1, [128, 512], F32, 'pacc', psum=True)
    pss = Rot(P, 3, [128, 4, 128], F32, 'pss', psum=True)
    ptm = Rot(P, 1, [128, 4, 128], BF16, 'ptm', psum=True)
    pod = Rot(P, 1, [64, 512], F32, 'pod', psum=True)
    pTrot = Rot(P, 3, [128, 4, 128], BF16, 'pT')
    atrot = Rot(P, 2, [64, 8, 128], BF16, 'at')
    rdrot = Rot(P, 2, [64, 128], F32, 'rd')
    E, b31 = G['E'], G['b31']
    identb = G['identb']

    def indexer(qi):
        sc, sck = scs[qi % 2], f'sc{qi % 2}'
        n = 128 * (qi + 1)
        tsl = slice(qi * 128, (qi + 1) * 128)
        iqt, iqk = iqrot.next()
        P.dma('act', lambda e: e.dma_start(out=iqt[:, 0:5, :], in_=ziq[:, :, tsl]), writes=[iqk])
        P.dma('act', lambda e: e.dma_start(out=iqt[0:32, 5, :], in_=T['ziq'][480:512, tsl]), writes=[iqk + 'b'])
        dg, dgk = dgrot.next()
        for h in range(16):
            P.op('pool', lambda e, h=h: e.tensor_scalar(out=dg[:, h, :], in0=identb[:], scalar1=iw[:, qi, h:h + 1], scalar2=0.0, op0=ALU.mult, op1=ALU.add),
                 reads=['iw'], writes=[dgk])
        for ch in range((n + 511) // 512):
            c0 = ch * 512
            nc_ = min(512, n - c0)
            pa, pak = pacc.next()
            pend = []

            def acc(h, r, rk):
                mm(P, pa[:, :nc_], dg[:, h, :], r[:, :nc_], h == 0, h == 15, [dgk, rk], [pak])
            for h in range(16):
                j, i = divmod(h, 3)
                ps, pk = psi.next()
                mm(P, ps[:, :nc_], iqt[32 * i:32 * i + 32, j, :], ik4[32 * i:32 * i + 32, c0:c0 + nc_], True, True,
                   [iqk, iqk + 'b', f'ik4{i}'], [pk])
                r, rk = rrot.next()
                P.op('act', lambda e, r=r, ps=ps, nc_=nc_: e.activation(out=r[:, :nc_], in_=ps[:, :nc_], func=AF.Relu), reads=[pk], writes=[rk])
                pend.append((h, r, rk))
                if len(pend) > 1:
                    acc(*pend.pop(0))
                yield
            while pend:
                acc(*pend.pop(0))
            P.op('dve', lambda e, pa=pa, c0=c0, nc_=nc_: e.tensor_copy(out=sc[:, c0:c0 + nc_], in_=pa[:, :nc_]), reads=[pak], writes=[sck])
        P.op('pool', lambda e: e.affine_select(out=sc[:, tsl], in_=sc[:, tsl], pattern=[[-1, 128]], compare_op=ALU.is_ge, fill=-1e30,
                                               base=0, channel_multiplier=1), reads=[sck], writes=[sck])

    def threshold(qi):
        sc, sck = scs[qi % 2], f'sc{qi % 2}'
        n = 128 * (qi + 1)
        maskb, mbk = maskbs[qi % 2], f'maskb{qi % 2}'
        dve = lambda fn, r, w: P.op('dve', fn, reads=r, writes=w)
        if n <= 256:
            dve(lambda e: e.memset(thr[:], -1e29), ['thr'], ['thr'])
        else:
            nv = 128 * qi
            dve(lambda e: e.tensor_reduce(out=lo[:], in_=sc[:, :nv], axis=AX.X, op=ALU.min), [sck, 'lo'], ['lo'])
            dve(lambda e: e.tensor_reduce(out=hi[:], in_=sc[:, :n], axis=AX.X, op=ALU.max), [sck, 'hi'], ['hi'])
            dve(lambda e: e.tensor_tensor(out=hi[:], in0=hi[:], in1=lo[:], op=ALU.subtract), ['hi', 'lo'], ['hi'])
            dve(lambda e: e.tensor_scalar(out=dk[:], in0=cvec[:], scalar1=hi[:, 0:1], scalar2=None, op0=ALU.mult), ['cvec', 'hi', 'dk'], ['dk'])
            dve(lambda e: e.tensor_tensor(out=mid[:], in0=lo[:], in1=dk[:, 0:1], op=ALU.add), ['lo', 'dk', 'mid'], ['mid'])
            for k in range(NIT):
                dve(lambda e: e.tensor_scalar(out=maskb[:, :n], in0=sc[:, :n], scalar1=mid[:, 0:1], scalar2=None,
                                              op0=ALU.is_ge, op1=ALU.add, accum_out=cnt[:]), [sck, 'mid', 'cnt', mbk], [mbk, 'cnt'])
                dve(lambda e: e.tensor_scalar(out=tmp[:], in0=cnt[:], scalar1=255.5, scalar2=-0.5, op0=ALU.is_ge, op1=ALU.add),
                    ['cnt', 'tmp'], ['tmp'])
                dve(lambda e, k=k: e.scalar_tensor_tensor(out=mid[:], in0=tmp[:], scalar=dk[:, k:k + 1], in1=mid[:], op0=ALU.mult, op1=ALU.add),
                    ['tmp', 'dk', 'mid'], ['mid'])
                yield
            dve(lambda e: e.tensor_tensor(out=thr[:], in0=mid[:], in1=dk[:, NIT:NIT + 1], op=ALU.subtract), ['mid', 'dk', 'thr'], ['thr'])
        dve(lambda e: e.tensor_scalar(out=maskb[:, :n], in0=sc[:, :n], scalar1=thr[:, 0:1], scalar2=None, op0=ALU.is_ge),
            [sck, 'thr', mbk], [mbk])
        yield

    def attention(qi):
        LOOK = 2
        nkt = qi + 1
        nch = (nkt + 3) // 4
        tsl = slice(qi * 128, (qi + 1) * 128)
        mb = maskbs[qi % 2]
        mbk = f'maskb{qi % 2}'
        qt, qk = qrot.next()
        P.dma('sp', lambda e: e.dma_start(out=qt[:], in_=zq[:, :, tsl]), writes=[qk])
        for c4 in range(nch):
            kts = list(range(4 * c4, min(4 * c4 + 4, nkt)))
            pm, pmk = ptm.next()
            for kt in kts:
                P.op('pe', lambda e, pm=pm, kt=kt: e.transpose(out=pm[:, kt % 4, :], in_=mb[:, kt * 128:(kt + 1) * 128],
                                                                identity=identb[:]), reads=[mbk], writes=[pmk])
            P.op('act', lambda e, pm=pm, kts=kts: e.activation(out=maskT[:, kts[0]:kts[-1] + 1, :], in_=pm[:, :len(kts), :],
                                                               func=AF.Copy), reads=[pmk], writes=['maskT'])
        at, atk = atrot.next()
        items = [(h, c4) for h in range(8) for c4 in range(nch)]
        qkd = {}
        hst = {}

        def emit_qk(h, c4):
            kts = list(range(4 * c4, min(4 * c4 + 4, nkt)))
            ps, pk = pss.next()
            for kt in kts:
                mm(P, ps[:, kt % 4, :], KT[:, h, kt * 128:(kt + 1) * 128], qt[:, h, :], True, True, ['KT', qk], [pk])
            qkd[(h, c4)] = (ps, pk)

        def emit_rest(h, c4):
            kts = list(range(4 * c4, min(4 * c4 + 4, nkt)))
            nk = len(kts)
            ps, pk = qkd.pop((h, c4))
            if c4 == 0:
                hst[h] = pod.next()
            po, pok = hst[h]
            pT, pTk = pTrot.next()
            P.op('act', lambda e: e.activation(out=pT[:, :nk, :], in_=ps[:, :nk, :], func=AF.Exp, scale=0.125, bias=b31[:, h:h + 1]),
                 reads=[pk], writes=[pTk])
            P.op('dve', lambda e: e.tensor_tensor(out=pT[:, :nk, :], in0=pT[:, :nk, :], in1=maskT[:, kts[0]:kts[-1] + 1, :], op=ALU.mult),
                 reads=[pTk, 'maskT'], writes=[pTk])
            for kt in kts:
                dl = qi - kt
                if dl <= 1:
                    P.op('dve', lambda e, kt=kt, dl=dl: e.tensor_tensor(
                        out=pT[:, kt % 4, :], in0=pT[:, kt % 4, :], in1=E[:, h, dl * 128:(dl + 1) * 128], op=ALU.mult),
                        reads=[pTk], writes=[pTk])
            for kt in kts:
                P.op('pe', lambda e, kt=kt: e.matmul(po[:, 0:128], lhsT=V[:, kt, h * 64:(h + 1) * 64], rhs=pT[:, kt % 4, :],
                                                     start=(kt == 0), stop=(kt == nkt - 1), skip_group_check=True),
                     reads=['V', pTk], writes=[pok])
                P.op('pe', lambda e, kt=kt: e.matmul(po[:, 128:256], lhsT=ones64[:], rhs=pT[:, kt % 4, :],
                                                     start=False, stop=(kt == nkt - 1), skip_group_check=True),
                     reads=['ones64', pTk], writes=[pok])
            if c4 == nch - 1:
                rd, rdk = rdrot.next()
                P.op('dve', lambda e: e.reciprocal(out=rd[:], in_=po[:, 128:256]), reads=[pok], writes=[rdk])
                P.op('dve', lambda e: e.tensor_tensor(out=at[:, h, :], in0=po[:, 0:128], in1=rd[:], op=ALU.mult),
                     reads=[pok, rdk], writes=[atk])

        for idx in range(len(items) + LOOK):
            if idx < len(items):
                emit_qk(*items[idx])
            if idx >= LOOK:
                emit_rest(*items[idx - LOOK])
                yield
        P.dma('sp', lambda e: e.dma_start(out=attnT[:, :, tsl], in_=at[:]), reads=[atk])

    def drain(g):
        for _ in g:
            pass

    def merge(gens):
        gens = [[g, max(1, n), 0.0, True] for g, n in gens]
        total = max(n for _, n, _, _ in gens)
        for step in range(total + 1):
            for it in gens:
                it[2] += it[1] / total
                while it[3] and it[2] >= 1.0:
                    it[2] -= 1.0
                    try:
                        next(it[0])
                    except StopIteration:
                        it[3] = False
        for it in gens:
            if it[3]:
                drain(it[0])

    s0 = phase_S0(P, T, G) if G.get('sparse') else iter(())

    def s0gen(n):
        for _ in range(n):
            try:
                next(s0)
            except StopIteration:
                return
            yield
    drain(indexer(0))
    drain(threshold(0))
    for qi in range(NT):
        ns0 = -(-390 * (qi + 1) // 528)
        if qi + 1 < NT:
            drain(indexer(qi + 1))
            merge([(attention(qi), 8 * ((qi + 4) // 4)), (threshold(qi + 1), NIT + 1), (s0gen(ns0), ns0)])
        else:
            merge([(attention(qi), 8 * ((qi + 4) // 4)), (s0gen(1000), 100)])
    drain(s0gen(1000))


def phase_E(P, T, G):
    LD = 0.6065306597126334
    ident = G['identb']
    zr = T['zr']
    def colload(name, n, key):
        t = P.sb([64, n], F32)
        P.dma('sp', lambda e: e.dma_start(out=t[:], in_=T[name].rearrange("(h p) -> p h", p=64), allow_slow_non_contiguous=True), writes=[key])
        return t
    mu_rkv = P.sb([64, 24], F32)
    P.dma('sp', lambda e: e.dma_start(out=mu_rkv[:], in_=T['tshift_mu'][0:1536].rearrange("(h p) -> p h", p=64), allow_slow_non_contiguous=True), writes=['mu'])
    mu_wa = P.sb([64, 2], F32)
    P.dma('sp', lambda e: e.dma_start(out=mu_wa[:], in_=T['tshift_mu'][1536:1664].rearrange("(h p) -> p h", p=64), allow_slow_non_contiguous=True), writes=['mu'])
    mu_g = P.sb([128, 1], F32)
    P.dma('sp', lambda e: e.dma_start(out=mu_g[:], in_=T['tshift_mu'][1664:1792].rearrange("(h p) -> p h", p=128), allow_slow_non_contiguous=True), writes=['mu'])
    om_rkv, om_wa, om_g = P.sb([64, 24], F32), P.sb([64, 2], F32), P.sb([128, 1], F32)
    for (o, m) in ((om_rkv, mu_rkv), (om_wa, mu_wa), (om_g, mu_g)):
        P.op('dve', lambda e, o=o, m=m: e.tensor_scalar(out=o[:], in0=m[:], scalar1=-1.0, scalar2=1.0, op0=ALU.mult, op1=ALU.add),
             reads=['mu'], writes=['om'])
    w0c = colload('decay_w0', 8, 'par'); a0c = colload('iclr_a0', 8, 'par'); kkc = colload('k_k', 8, 'par')
    kac = colload('k_a', 8, 'par'); rkc = colload('r_k', 8, 'par'); lgc = colload('lnx_g', 8, 'par'); lbc = colload('lnx_b', 8, 'par')
    omka = P.sb([64, 8], F32)
    P.op('dve', lambda e: e.tensor_scalar(out=omka[:], in0=kac[:], scalar1=-1.0, scalar2=1.0, op0=ALU.mult, op1=ALU.add),
         reads=['par'], writes=['omka'])
    dup, iup, gup = P.sb([64, 512], BF16), P.sb([64, 512], BF16), P.sb([128, 512], BF16)
    P.dma('pool', lambda e: e.dma_start(out=dup[:], in_=T['decay_up']), writes=['wts'])
    P.dma('pool', lambda e: e.dma_start(out=iup[:], in_=T['iclr_up']), writes=['wts'])
    P.dma('pool', lambda e: e.dma_start(out=gup[:], in_=T['gate_up']), writes=['wts'])
    onesf = P.sb([64, 64], F32)
    onesm = P.sb([64, 64], F32)
    gneps = P.sb([64, 1], F32)
    P.op('dve', lambda e: e.memset(onesf[:], 1.0), writes=['onesf'])
    P.op('dve', lambda e: e.memset(onesm[:], 1.0 / 64), writes=['onesm'])
    P.op('dve', lambda e: e.memset(gneps[:], 64e-5), writes=['gneps'])
    km = P.sb([128, 896], F32)
    P.dma('sp', lambda e: e.dma_start(out=km[:], in_=T['k_masks']), writes=['km'])
    mask4 = P.sb([128, 512], BF16)
    maskL = P.sb([128, 128], BF16)
    P.op('dve', lambda e: e.tensor_copy(out=mask4[:], in_=km[:, 0:512]), reads=['km'], writes=['mask4'])
    P.op('dve', lambda e: e.tensor_copy(out=maskL[:], in_=km[:, 512:640]), reads=['km'], writes=['maskL'])
    rst = P.sb([64, 512], F32)
    P.dma('sp', lambda e: e.dma_start(out=rst[:], in_=T['k_reset']), writes=['rst'])
    Tst = P.sb([64, 8, 64], BF16)
    P.op('dve', lambda e: e.memset(Tst[:], 0.0), writes=[f'T{h}' for h in range(8)])
    zrot = Rot(P, 1, [64, 3, 513], F32, 'z')
    wa_in = P.sb([64, 2, 513], F32)
    gd_in = P.sb([128, 513], F32)
    tmpr = Rot(P, 1, [128, 512], F32, 'tmp')
    twb, adb, sgb = P.sb([64, 512], BF16), P.sb([64, 512], BF16), P.sb([128, 512], BF16)
    AR = P.sb([64, 8, 4, 256], BF16)
    BK = P.sb([64, 8, 4, 256], BF16)
    tok3 = P.sb([128, 8, 4, 3, 64], BF16)
    pC = P.sb([64, 8, 4], F32)
    bon = P.sb([64, 8, 512], BF16)
    gg = P.sb([64, 8, 512], BF16)
    yT = P.sb([64, 8, 512], F32)
    RW = P.sb([64, 8, 512], BF16)
    hb = {n: P.sb([64, 512], F32) for n in ('sig', 'cs', 'ep', 'em', 'epv', 'kk', 'kkn', 'a', 't', 'kp', 'b', 'u1')}
    vb = P.sb([64, 512], BF16)
    Gms = [P.sb([128, 16, 512], BF16) for _ in range(2)]
    XY = [P.sb([128, 16, 256], BF16) for _ in range(2)]
    Nms = [P.sb([128, 16, 128], BF16) for _ in range(2)]
    Wsb, Usb = P.sb([128, 8, 64], BF16), P.sb([128, 8, 64], BF16)
    pg = Rot(P, 5, [128, 512], F32, 'pg', psum=True)
    pl = Rot(P, 2, [64, 512], F32, 'pl', psum=True)
    ptr = Rot(P, 1, [128, 3, 64], BF16, 'ptr', psum=True)
    rwT = T['rwT'].rearrange("(h p) t -> p h t", p=64)

    def dve(fn, reads, writes):
        P.op('dve', fn, reads=reads, writes=writes)

    for tg in range(8):
        t0 = tg * 512
        def load_halo(dst, rows, key, q, tg=tg, t0=t0):
            if tg == 0:
                src = rows(t0, t0 + 512)
                P.op('pool', lambda e: e.memset(dst[:, 0:1] if len(dst.shape) == 2 else dst[:, :, 0:1], 0.0), reads=[key], writes=[key])
                P.dma(q, lambda e: e.dma_start(out=(dst[:, 1:513] if len(dst.shape) == 2 else dst[:, :, 1:513]), in_=src), writes=[key + 'b'])
            else:
                src = rows(t0 - 1, t0 + 512)
                P.dma(q, lambda e: e.dma_start(out=dst[:], in_=src), reads=[key + 'b'], writes=[key])
        load_halo(wa_in, lambda a, b: zr[1536:1664, a:b].rearrange("(h p) t -> p h t", p=64), 'wa', 'sp')
        load_halo(gd_in, lambda a, b: zr[1664:1792, a:b], 'gd', 'act')

        def tshift(src_prev, src_cur, mu_ap, om_ap, np_, keys):
            tm, tk = tmpr.next()
            P.op('pool', lambda e: e.tensor_scalar(out=tm[:np_, :], in0=src_prev, scalar1=mu_ap, scalar2=0.0, op0=ALU.mult, op1=ALU.add),
                 reads=keys + ['mu'], writes=[tk])
            dve(lambda e: e.scalar_tensor_tensor(out=src_cur, in0=src_cur, scalar=om_ap, in1=tm[:np_, :], op0=ALU.mult, op1=ALU.add),
                keys + [tk, 'om'], keys)
        for i in range(2):
            tshift(wa_in[:, i, 0:512], wa_in[:, i, 1:513], mu_wa[:, i:i + 1], om_wa[:, i:i + 1], 64, ['wa', 'wab'])
        tshift(gd_in[:, 0:512], gd_in[:, 1:513], mu_g[:, 0:1], om_g[:, 0:1], 128, ['gd', 'gdb'])
        P.op('act', lambda e: e.activation(out=twb[:], in_=wa_in[:, 0, 1:513], func=AF.Tanh), reads=['wa', 'wab'], writes=['twb'])
        P.op('act', lambda e: e.activation(out=sgb[:], in_=gd_in[:, 1:513], func=AF.Sigmoid), reads=['gd', 'gdb'], writes=['sgb'])
        dve(lambda e: e.tensor_copy(out=adb[:], in_=wa_in[:, 1, 1:513]), ['wa', 'wab'], ['adb'])
        def prep_head(h, z, zk):
            def zrows(a, b, h=h):
                return zr[0:1536, a:b].rearrange("(s hh p) t -> hh p s t", s=3, p=64)[h]
            load_halo(z, zrows, zk, 'sp' if h % 2 == 0 else 'act')
            for s_ in range(3):
                tshift(z[:, s_, 0:512], z[:, s_, 1:513], mu_rkv[:, s_ * 8 + h:s_ * 8 + h + 1], om_rkv[:, s_ * 8 + h:s_ * 8 + h + 1], 64, [zk, zk + 'b'])
            r_, k_, v_ = z[:, 0, 1:513], z[:, 1, 1:513], z[:, 2, 1:513]
            zkeys = [zk, zk + 'b']
            sig, cs, ep, em, epv, kk, kkn, a_, t_, kp, b_, u1 = [hb[n] for n in ('sig', 'cs', 'ep', 'em', 'epv', 'kk', 'kkn', 'a', 't', 'kp', 'b', 'u1')]
            hs = slice(h * 64, (h + 1) * 64)
            p1, p1k = pl.next()
            mm(P, p1[:], dup[:, hs], twb[:], True, True, ['wts', 'twb'], [p1k])
            P.op('act', lambda e, p1=p1, h=h: e.activation(out=sig[:], in_=p1[:], func=AF.Sigmoid, bias=w0c[:, h:h + 1]),
                 reads=[p1k, 'par'], writes=['sig'])
            dve(lambda e: e.tensor_tensor_scan(out=cs[:], data0=rst[:], data1=sig[:], initial=0.0, op0=ALU.mult, op1=ALU.add),
                ['rst', 'sig'], ['cs'])
            P.op('act', lambda e: e.activation(out=ep[:], in_=cs[:], func=AF.Exp, scale=-LD), reads=['cs'], writes=['ep'])
            P.op('act', lambda e: e.activation(out=em[:], in_=cs[:], func=AF.Exp, scale=LD), reads=['cs'], writes=['em'])
            dve(lambda e: e.tensor_tensor(out=u1[:], in0=cs[:], in1=sig[:], op=ALU.subtract), ['cs', 'sig'], ['u1'])
            P.op('act', lambda e: e.activation(out=epv[:], in_=u1[:], func=AF.Exp, scale=-LD), reads=['u1'], writes=['epv'])
            dve(lambda e, h=h: e.tensor_copy(out=pC[:, h, :], in_=ep[:, 127:512:128]), ['ep'], ['pC'])
            p2, p2k = pl.next()
            mm(P, p2[:], iup[:, hs], adb[:], True, True, ['wts', 'adb'], [p2k])
            P.op('act', lambda e, p2=p2, h=h: e.activation(out=a_[:], in_=p2[:], func=AF.Sigmoid, bias=a0c[:, h:h + 1]),
                 reads=[p2k, 'par'], writes=['a'])
            p3, p3k = pl.next()
            mm(P, p3[:], gup[:, hs], sgb[:], True, True, ['wts', 'sgb'], [p3k])
            P.op('act', lambda e, p3=p3, h=h: e.activation(out=gg[:, h, :], in_=p3[:], func=AF.Copy), reads=[p3k], writes=[f'gg{h}'])
            dve(lambda e, h=h: e.tensor_scalar(out=kk[:], in0=k_, scalar1=kkc[:, h:h + 1], scalar2=None, op0=ALU.mult), zkeys + ['par'], ['kk'])
            P.op('act', lambda e: e.activation(out=u1[:], in_=kk[:], func=AF.Square), reads=['kk', 'u1'], writes=['u1'])
            p4, p4k = pl.next()
            mm(P, p4[:], onesf[:], u1[:], True, True, ['onesf', 'u1'], [p4k])
            P.op('act', lambda e, p4=p4: e.activation(out=kkn[:], in_=p4[:], func=AF.Sqrt), reads=[p4k], writes=['kkn'])
            dve(lambda e: e.tensor_scalar(out=kkn[:], in0=kkn[:], scalar1=1e-12, scalar2=None, op0=ALU.max), ['kkn'], ['kkn'])
            dve(lambda e: e.reciprocal(out=kkn[:], in_=kkn[:]), ['kkn'], ['kkn'])
            dve(lambda e: e.tensor_tensor(out=kkn[:], in0=kkn[:], in1=kk[:], op=ALU.mult), ['kkn', 'kk'], ['kkn'])
            dve(lambda e, h=h: e.tensor_scalar(out=t_[:], in0=a_[:], scalar1=kac[:, h:h + 1], scalar2=omka[:, h:h + 1], op0=ALU.mult, op1=ALU.add),
                ['a', 'par', 'omka'], ['t'])
            dve(lambda e: e.tensor_tensor(out=kp[:], in0=t_[:], in1=k_, op=ALU.mult), ['t'] + zkeys, ['kp'])
            dve(lambda e: e.tensor_tensor(out=b_[:], in0=kkn[:], in1=a_[:], op=ALU.mult), ['kkn', 'a'], ['b'])
            c4 = lambda ap: ap.rearrange("p (c t) -> p c t", c=4)
            dve(lambda e, h=h: e.tensor_tensor(out=AR[:, h, :, 128:256], in0=c4(r_), in1=c4(ep[:]), op=ALU.mult), zkeys + ['ep'], [f'AR{h}'])
            dve(lambda e, h=h: e.scalar_tensor_tensor(out=AR[:, h, :, 0:128], in0=c4(kkn[:]), scalar=-1.0, in1=c4(epv[:]), op0=ALU.mult, op1=ALU.mult),
                ['kkn', 'epv'], [f'AR{h}'])
            dve(lambda e, h=h: e.tensor_tensor(out=BK[:, h, :, 0:128], in0=c4(b_[:]), in1=c4(em[:]), op=ALU.mult), ['b', 'em'], [f'BK{h}'])
            dve(lambda e, h=h: e.tensor_tensor(out=BK[:, h, :, 128:256], in0=c4(kp[:]), in1=c4(em[:]), op=ALU.mult), ['kp', 'em'], [f'BK{h}'])
            dve(lambda e, h=h: e.scalar_tensor_tensor(out=u1[:], in0=r_, scalar=rkc[:, h:h + 1], in1=kp[:], op0=ALU.mult, op1=ALU.mult),
                zkeys + ['kp', 'par', 'u1'], ['u1'])
            p5, p5k = pl.next()
            mm(P, p5[:], onesf[:], u1[:], True, True, ['onesf', 'u1'], [p5k])
            dve(lambda e, p5=p5, h=h: e.tensor_tensor(out=bon[:, h, :], in0=p5[:], in1=v_, op=ALU.mult), [p5k] + zkeys, [f'bon{h}'])
            P.op('pool', lambda e: e.tensor_copy(out=vb[:], in_=v_), reads=zkeys, writes=['vb'])
            for c in range(4):
                pt_, ptk = ptr.next()
                cs_ = slice(c * 128, (c + 1) * 128)
                P.op('pe', lambda e, pt_=pt_, cs_=cs_: e.transpose(out=pt_[:, 0, :], in_=vb[:, cs_], identity=ident[0:64, 0:64]), reads=['vb'], writes=[ptk])
                P.op('pe', lambda e, pt_=pt_, h=h, c=c: e.transpose(out=pt_[:, 1, :], in_=BK[:, h, c, 0:128], identity=ident[0:64, 0:64]), reads=[f'BK{h}'], writes=[ptk])
                P.op('pe', lambda e, pt_=pt_, h=h, c=c: e.transpose(out=pt_[:, 2, :], in_=BK[:, h, c, 128:256], identity=ident[0:64, 0:64]), reads=[f'BK{h}'], writes=[ptk])
                P.op('act', lambda e, pt_=pt_, h=h, c=c: e.activation(out=tok3[:, h, c, :, :], in_=pt_[:], func=AF.Copy), reads=[ptk], writes=[f'tok{h}'])
        for h in range(8):
            z, zk = zrot.next()
            prep_head(h, z, zk)
        def stage1(cs, tg=tg):
            sl = (cs[0] // 2) % 2
            Gm, Nm = Gms[sl], Nms[sl]
            probs = [(ci, c, h) for ci, c in enumerate(cs) for h in range(8)]
            for (ci, c, h) in probs:
                q = ci * 8 + h
                p_, pk = pg.next()
                mm(P, p_[:, 0:256], BK[:, h, c, 0:128], AR[:, h, c, :], True, True, [f'BK{h}', f'AR{h}'], [pk])
                mm(P, p_[:, 256:512], BK[:, h, c, 128:256], AR[:, h, c, :], True, True, [f'BK{h}', f'AR{h}'], [pk])
                dve(lambda e, p_=p_, q=q: e.tensor_tensor(out=Gm[:, q, :], in0=p_[:], in1=mask4[:], op=ALU.mult), [pk, 'mask4'], [f'Gm{sl}_{q}'])
                p2_, p2k = pg.next()
                mm(P, p2_[:, 0:128], AR[:, h, c, 0:128], BK[:, h, c, 0:128], True, True, [f'BK{h}', f'AR{h}'], [p2k])
                dve(lambda e, p2_=p2_, q=q: e.tensor_tensor(out=XY[0][:, q, 128:256], in0=p2_[:, 0:128], in1=maskL[:], op=ALU.mult),
                    [p2k, 'maskL'], [f'XY0{q}'])
                P.op('pool', lambda e, q=q: e.tensor_copy(out=XY[0][:, q, 0:128], in_=Gm[:, q, 0:128]), reads=[f'Gm{sl}_{q}'], writes=[f'XY0{q}x'])
                P.op('pool', lambda e, q=q: e.tensor_tensor(out=Nm[:, q, :], in0=Gm[:, q, 0:128], in1=ident[:], op=ALU.add),
                     reads=[f'Gm{sl}_{q}'], writes=[f'N{sl}_{q}'])
            yield
            for j in range(6):
                cur, nxt = XY[j % 2], XY[(j + 1) % 2]
                ck, nk_ = f'XY{j % 2}', f'XY{(j + 1) % 2}'
                for q in range(len(probs)):
                    p_, pk = pg.next()
                    rk_ = [ck + f'{q}', ck + f'{q}x']
                    mm(P, p_[:, 0:128], cur[:, q, 128:256], cur[:, q, 0:128], True, True, rk_, [pk])
                    mm(P, p_[:, 128:256], cur[:, q, 0:128], cur[:, q, 128:256], True, True, rk_, [pk])
                    P.op('act', lambda e, p_=p_, nxt=nxt, q=q: e.activation(out=nxt[:, q, :], in_=p_[:, 0:256], func=AF.Copy),
                         reads=[pk], writes=[nk_ + f'{q}', nk_ + f'{q}x'])
                    if q % 4 == 3:
                        yield
                for q in range(len(probs)):
                    p_, pk = pg.next()
                    mm(P, p_[:, 0:128], nxt[:, q, 128:256], Nm[:, q, :], True, True, [nk_ + f'{q}', nk_ + f'{q}x', f'N{sl}_{q}'], [pk])
                    dve(lambda e, p_=p_, q=q: e.tensor_tensor(out=Nm[:, q, :], in0=p_[:, 0:128], in1=Nm[:, q, :], op=ALU.add),
                        [pk, f'N{sl}_{q}'], [f'N{sl}_{q}'])
                    if q % 4 == 3:
                        yield

        def stage2(c, tg=tg):
            sl = (c // 2) % 2
            Gm, Nm = Gms[sl], Nms[sl]
            ci = c % 2
            pw, pwk = pg.next()
            for h in range(8):
                q = ci * 8 + h
                mm(P, pw[:, h * 64:(h + 1) * 64], AR[:, h, c, 0:128], Tst[:, h, :], True, False, [f'AR{h}', f'T{h}'], [pwk])
                mm(P, pw[:, h * 64:(h + 1) * 64], Gm[:, q, 256:384], tok3[:, h, c, 0, :], False, True, [f'Gm{sl}_{q}', f'tok{h}'], [pwk])
            P.op('act', lambda e: e.activation(out=Wsb[:].rearrange("p h v -> p (h v)"), in_=pw[:], func=AF.Copy), reads=[pwk], writes=['W'])
            yield
            pu, puk = pg.next()
            for h in range(8):
                q = ci * 8 + h
                mm(P, pu[:, h * 64:(h + 1) * 64], Nm[:, q, :], Wsb[:, h, :], True, True, [f'N{sl}_{q}', 'W'], [puk])
            P.op('act', lambda e: e.activation(out=Usb[:].rearrange("p h v -> p (h v)"), in_=pu[:], func=AF.Copy), reads=[puk], writes=['U'])
            yield
            pt_, ptk = pg.next()
            for h in range(8):
                q = ci * 8 + h
                o_ = pt_[0:64, h * 64:(h + 1) * 64]
                mm(P, o_, ident[0:64, 0:64], Tst[:, h, :], True, False, [f'T{h}'], [ptk])
                mm(P, o_, tok3[:, h, c, 1, :], Usb[:, h, :], False, False, [f'tok{h}', 'U'], [ptk])
                mm(P, o_, tok3[:, h, c, 2, :], tok3[:, h, c, 0, :], False, True, [f'tok{h}'], [ptk])
            for half in range(2):
                py_, pyk = pg.next()
                for hh in range(4):
                    h = half * 4 + hh
                    q = ci * 8 + h
                    o_ = py_[0:64, hh * 128:(hh + 1) * 128]
                    mm(P, o_, Tst[:, h, :], AR[:, h, c, 128:256], True, False, [f'AR{h}', f'T{h}'], [pyk])
                    mm(P, o_, Usb[:, h, :], Gm[:, q, 128:256], False, False, ['U', f'Gm{sl}_{q}'], [pyk])
                    mm(P, o_, tok3[:, h, c, 0, :], Gm[:, q, 384:512], False, True, [f'tok{h}', f'Gm{sl}_{q}'], [pyk])
                P.op('act', lambda e, py_=py_, half=half: e.activation(
                    out=yT[:, half * 4:half * 4 + 4, c * 128:(c + 1) * 128], in_=py_[0:64, :].rearrange("p (h t) -> p h t", h=4), func=AF.Copy),
                    reads=[pyk], writes=[f'yT{half}'])
            for h in range(8):
                dve(lambda e, h=h: e.tensor_scalar(out=Tst[:, h, :], in0=pt_[0:64, h * 64:(h + 1) * 64], scalar1=pC[:, h, c:c + 1], scalar2=None, op0=ALU.mult),
                    [ptk, 'pC', f'T{h}'], [f'T{h}'])
            yield

        def chain(*gs):
            for g in gs:
                yield from g

        def rr(g1, g2):
            a = b = True
            while a or b:
                if a:
                    try:
                        next(g1)
                    except StopIteration:
                        a = False
                if b:
                    try:
                        next(g2)
                    except StopIteration:
                        b = False
        for _ in stage1([0, 1]):
            pass
        rr(stage1([2, 3]), chain(stage2(0), stage2(1)))
        for _ in chain(stage2(2), stage2(3)):
            pass
        for h in range(8):
            u1, u2 = hb['u1'], hb['t']
            p1, p1k = pl.next()
            mm(P, p1[:], onesm[:], yT[:, h, :], True, True, ['onesm', f'yT{h // 4}'], [p1k])
            dve(lambda e, p1=p1, h=h: e.tensor_tensor(out=u1[:], in0=yT[:, h, :], in1=p1[:], op=ALU.subtract), [p1k, f'yT{h // 4}', 'u1'], ['u1'])
            P.op('act', lambda e: e.activation(out=u2[:], in_=u1[:], func=AF.Square), reads=['u1', 't'], writes=['t'])
            p2, p2k = pl.next()
            mm(P, p2[:], onesm[:], u2[:], True, True, ['onesm', 't'], [p2k])
            P.op('act', lambda e, p2=p2: e.activation(out=u2[:], in_=p2[:], func=AF.Sqrt, bias=gneps[:, 0:1]), reads=[p2k, 'gneps', 't'], writes=['t'])
            dve(lambda e: e.reciprocal(out=u2[:], in_=u2[:]), ['t'], ['t'])
            dve(lambda e: e.tensor_tensor(out=u1[:], in0=u1[:], in1=u2[:], op=ALU.mult), ['u1', 't'], ['u1'])
            dve(lambda e, h=h: e.tensor_scalar(out=u1[:], in0=u1[:], scalar1=lgc[:, h:h + 1], scalar2=lbc[:, h:h + 1], op0=ALU.mult, op1=ALU.add),
                ['u1', 'par'], ['u1'])
            dve(lambda e, h=h: e.tensor_tensor(out=u1[:], in0=u1[:], in1=bon[:, h, :], op=ALU.add), ['u1', f'bon{h}'], ['u1'])
            dve(lambda e, h=h: e.tensor_tensor(out=RW[:, h, :], in0=u1[:], in1=gg[:, h, :], op=ALU.mult), ['u1', f'gg{h}'], ['RW'])
        P.dma('sp', lambda e, t0=t0: e.dma_start(out=rwT[:, :, t0:t0 + 512], in_=RW[:]), reads=['RW'])
        if tg == 0 and 'dbg_y' in T:
            P.dma('sp', lambda e: e.dma_start(out=T['dbg_y'], in_=yT[:]), reads=['yT0', 'yT1'])
            P.dma('sp', lambda e: e.dma_start(out=T['dbg_bon'], in_=bon[:]), reads=[f'bon{h}' for h in range(8)])
            P.dma('sp', lambda e: e.dma_start(out=T['dbg_g'], in_=gg[:]), reads=[f'gg{h}' for h in range(8)])
            P.dma('sp', lambda e: e.dma_start(out=T['dbg_AR'], in_=AR[:]), reads=[f'AR{h}' for h in range(8)])
            P.dma('sp', lambda e: e.dma_start(out=T['dbg_BK'], in_=BK[:]), reads=[f'BK{h}' for h in range(8)])


def phase_F(P, T, G):
    wa, wr, wo = P.sb([128, 4, D], BF16), P.sb([128, 4, D], BF16), P.sb([128, 8, D], BF16)
    P.dma('pool', lambda e: e.dma_start(out=wa[:], in_=T['w_attn_br'].rearrange("(j p) d -> p j d", p=128)), writes=['wa'])
    P.dma('pool', lambda e: e.dma_start(out=wr[:], in_=T['w_rwkv_br'].rearrange("(j p) d -> p j d", p=128)), writes=['wr'])
    P.dma('pool', lambda e: e.dma_start(out=wo[:], in_=T['w_out'].rearrange("(j p) d -> p j d", p=128)), writes=['wo'])
    W = dict(junk=P.sb([128, D], F32), ss=P.sb([128, 1], F32), rs=P.sb([128, 1], F32), t1=P.sb([128, D], F32),
             hb=P.sb([128, D], BF16), pt=P.ps([128, 8, 128], BF16))
    W['mod'] = P.sb([128, 3072], F32)
    P.dma('sp', lambda e: e.dma_start(out=W['mod'][:], in_=T['modd'][:, 2048:5120]), writes=['modl'])
    xrot = Rot(P, 2, [128, D], F32, 'x')
    atr, rtr = Rot(P, 2, [128, 4, 128], BF16, 'at'), Rot(P, 2, [128, 4, 128], BF16, 'rt')
    gar, grr = Rot(P, 2, [128, 8, 128], BF16, 'ga'), Rot(P, 2, [128, 8, 128], BF16, 'gr')
    sga, sgr = P.sb([128, 8, 128], F32), P.sb([128, 8, 128], F32)
    m1, m2 = P.sb([128, 4, 128], F32), P.sb([128, 4, 128], F32)
    mixT = P.sb([128, 8, 128], BF16)
    x1t = P.sb([128, D], F32)
    h2t = P.sb([128, 8, 128], BF16)
    pA = Rot(P, 1, [128, 4, 128], F32, 'pA', psum=True)
    pR = Rot(P, 1, [128, 4, 128], F32, 'pR', psum=True)
    po = Rot(P, 2, [128, 512], F32, 'po', psum=True)
    aT = T['attnT'].rearrange("(j p) t -> p j t", p=128)
    rT = T['rwT'].rearrange("(j p) t -> p j t", p=128)
    gaT = T['zga'].rearrange("(j p) t -> p j t", p=128)
    grT = T['zgr'].rearrange("(j p) t -> p j t", p=128)
    h2T = T['h2T'].rearrange("(k p) t -> p k t", p=128)
    R = router_setup(P, T, G) if G.get('sparse') else None
    zt = P.sb([128, D], BF16)
    P.op('pool', lambda e: e.memset(zt[:], 0.0), writes=['zt'])
    for i in range(NT):
        tsl = slice(i * 128, (i + 1) * 128)
        if R is not None:
            for bz in range(i * 10, i * 10 + 10):
                P.dma('act' if bz % 2 else 'sp', lambda e, bz=bz: e.dma_start(out=T['Xs'][bz * 128:(bz + 1) * 128, :], in_=zt[:]), reads=['zt'])
        xt, xk = xrot.next()
        at, atk = atr.next(); rt, rtk = rtr.next(); ga, gak = gar.next(); gr, grk = grr.next()
        P.dma('sp', lambda e, xt=xt, tsl=tsl: e.dma_start(out=xt[:], in_=T['x'][tsl, :]), writes=[xk])
        P.dma('act', lambda e, at=at, tsl=tsl: e.dma_start(out=at[:], in_=aT[:, :, tsl]), writes=[atk])
        P.dma('act', lambda e, rt=rt, tsl=tsl: e.dma_start(out=rt[:], in_=rT[:, :, tsl]), writes=[rtk])
        P.dma('sp', lambda e, ga=ga, tsl=tsl: e.dma_start(out=ga[:], in_=gaT[:, :, tsl]), writes=[gak])
        P.dma('sp', lambda e, gr=gr, tsl=tsl: e.dma_start(out=gr[:], in_=grT[:, :, tsl]), writes=[grk])
        P.op('act', lambda e, ga=ga: e.activation(out=sga[:], in_=ga[:], func=AF.Sigmoid), reads=[gak], writes=['sga'])
        P.op('act', lambda e, gr=gr: e.activation(out=sgr[:], in_=gr[:], func=AF.Sigmoid), reads=[grk], writes=['sgr'])
        for half in range(2):
            pa, pak = pA.next(); pr, prk = pR.next()
            for s_ in range(4):
                dt = half * 4 + s_
                for j in range(4):
                    mm(P, pa[:, s_, :], wa[:, j, dt * 128:(dt + 1) * 128], at[:, j, :], j == 0, j == 3, ['wa', atk], [pak])
            for s_ in range(4):
                dt = half * 4 + s_
                for j in range(4):
                    mm(P, pr[:, s_, :], wr[:, j, dt * 128:(dt + 1) * 128], rt[:, j, :], j == 0, j == 3, ['wr', rtk], [prk])
            hs = slice(half * 4, half * 4 + 4)
            P.op('dve', lambda e, pa=pa, hs=hs: e.tensor_tensor(out=m1[:], in0=pa[:], in1=sga[:, hs, :], op=ALU.mult), reads=[pak, 'sga'], writes=['m1'])
            P.op('dve', lambda e, pr=pr, hs=hs: e.tensor_tensor(out=m2[:], in0=pr[:], in1=sgr[:, hs, :], op=ALU.mult), reads=[prk, 'sgr'], writes=['m2'])
            P.op('pool', lambda e, hs=hs: e.tensor_tensor(out=mixT[:, hs, :], in0=m1[:], in1=m2[:], op=ALU.add), reads=['m1', 'm2'], writes=['mixT'])
        for half in range(2):
            p_, pk = po.next()
            cs_ = slice(half * 512, (half + 1) * 512)
            for dt in range(8):
                mm(P, p_[:], mixT[:, dt, :], wo[:, dt, cs_], dt == 0, dt == 7, ['mixT', 'wo'], [pk])
            P.op('dve', lambda e, p_=p_, cs_=cs_: e.tensor_tensor(out=x1t[:, cs_], in0=p_[:], in1=W['mod'][:, cs_], op=ALU.mult),
                 reads=[pk, 'modl'], writes=['x1t'])
        P.op('pool', lambda e, xt=xt: e.tensor_tensor(out=x1t[:], in0=x1t[:], in1=xt[:], op=ALU.add), reads=['x1t', xk], writes=['x1t'])
        P.dma('sp', lambda e, tsl=tsl: e.dma_start(out=T['x1'][tsl, :], in_=x1t[:]), reads=['x1t'])
        norm_mod_transpose(P, G, x1t, 'x1t', h2t, 0, slice(2048, 3072), slice(1024, 2048), W)
        P.dma('act', lambda e, tsl=tsl: e.dma_start(out=h2T[:, :, tsl], in_=h2t[:]), reads=['hT0'])
        if R is not None:
            P.dma('act', lambda e, tsl=tsl: e.dma_start(out=T['h2tok'][tsl, :], in_=W['hb'][:]), reads=['hb'])
            router_tile(P, T, R, h2t, 'hT0', i)
    if R is not None:
        P.dma('sp', lambda e: e.dma_start(out=T['cntd'], in_=R['cnt'][:]), reads=['cnt'])


def phase_G0(P, T, G):
    for e in range(64):
        for (src, dst) in (('exp_gate', 'wg16'), ('exp_up', 'wu16'), ('exp_down', 'wd16')):
            P.dma('pool', lambda e_, e=e, src=src, dst=dst: e_.dma_start(
                out=T[dst][e].rearrange("k p f -> (k p f)").rearrange("(a b) -> a b", b=2048), in_=T[src][e].rearrange("r c -> (r c)").rearrange("(a b) -> a b", b=2048)))
    for (src, dst) in (('sh_gate', 'wg16'), ('sh_up', 'wu16'), ('sh_down', 'wd16')):
        P.dma('pool', lambda e_, src=src, dst=dst: e_.dma_start(
            out=T[dst][64].rearrange("k p f -> (k p f)").rearrange("(a b) -> a b", b=2048), in_=T[src].rearrange("r c -> (r c)").rearrange("(a b) -> a b", b=2048)))


def phase_G(P, T, G):
    ident = G['identb']
    h2T = T['h2T'].rearrange("(k p) t -> p k t", p=128)
    yacc = G['yacc']
    gwT = P.sb([64, S], BF16)
    rwt = P.sb([128, 8, 64], BF16)
    rbias = P.sb([128, 64], F32)
    P.dma('pool', lambda e: e.dma_start(out=rwt[:], in_=T['router_w'].rearrange("(k p) n -> p k n", p=128)), writes=['rwt'])
    P.dma('sp', lambda e: e.dma_start(out=rbias[:], in_=T['router_bias'].partition_broadcast(128)), writes=['rbias'])
    ones128 = P.sb([64, 128], BF16)
    P.op('dve', lambda e: e.memset(ones128[:], 1.0), writes=['ones128'])
    hrot = Rot(P, 2, [128, 8, 256], BF16, 'h2g')
    pmisc = P.ps([128, 512], F32)
    ptb = P.ps([64, 128], BF16)
    emb = P.sb([128, 64], BF16)
    sc_, ch, tmp, cm, em = [P.sb([128, 64], F32) for _ in range(5)]
    m1, m2, grp, s8, gmask, pen, den = [P.sb([128, 8], F32) for _ in range(7)]
    dve = lambda fn, r, w: P.op('dve', fn, reads=r, writes=w)
    for tgp in range(16):
        hg, hk = hrot.next()
        P.dma('sp', lambda e, hg=hg, tgp=tgp: e.dma_start(out=hg[:], in_=h2T[:, :, tgp * 256:(tgp + 1) * 256]), writes=[hk])
        for tt in range(2):
            i = tgp * 2 + tt
            p_, pk = pmisc[:, 0:64], 'pm_a'
            for k in range(8):
                mm(P, p_, hg[:, k, tt * 128:(tt + 1) * 128], rwt[:, k, :], k == 0, k == 7, [hk, 'rwt'], [pk])
            P.op('act', lambda e, p_=p_: e.activation(out=sc_[:], in_=p_, func=AF.Sigmoid), reads=[pk], writes=['sc'])
            dve(lambda e: e.tensor_tensor(out=ch[:], in0=sc_[:], in1=rbias[:], op=ALU.add), ['sc', 'rbias'], ['ch'])
            ch3 = ch[:].rearrange("p (g e) -> p g e", g=8)
            dve(lambda e, ch3=ch3: e.tensor_reduce(out=m1[:], in_=ch3, axis=AX.X, op=ALU.max), ['ch'], ['m1'])
            for g in range(8):
                dve(lambda e, g=g: e.tensor_scalar(out=tmp[:, g * 8:(g + 1) * 8], in0=ch[:, g * 8:(g + 1) * 8], scalar1=m1[:, g:g + 1],
                                                  scalar2=-1e9, op0=ALU.is_equal, op1=ALU.mult), ['ch', 'm1', 'tmp'], ['tmp'])
            dve(lambda e: e.tensor_tensor(out=tmp[:], in0=tmp[:], in1=ch[:], op=ALU.add), ['tmp', 'ch'], ['tmp'])
            dve(lambda e: e.tensor_reduce(out=m2[:], in_=tmp[:].rearrange("p (g e) -> p g e", g=8), axis=AX.X, op=ALU.max), ['tmp'], ['m2'])
            dve(lambda e: e.tensor_tensor(out=grp[:], in0=m1[:], in1=m2[:], op=ALU.add), ['m1', 'm2'], ['grp'])
            dve(lambda e: e.max(out=s8[:], in_=grp[:]), ['grp'], ['s8'])
            dve(lambda e: e.tensor_scalar(out=gmask[:], in0=grp[:], scalar1=s8[:, 3:4], scalar2=None, op0=ALU.is_ge), ['grp', 's8'], ['gmask'])
            dve(lambda e: e.tensor_scalar(out=pen[:], in0=gmask[:], scalar1=-1.0, scalar2=1e9, op0=ALU.add, op1=ALU.mult), ['gmask'], ['pen'])
            for g in range(8):
                dve(lambda e, g=g: e.tensor_scalar(out=cm[:, g * 8:(g + 1) * 8], in0=ch[:, g * 8:(g + 1) * 8], scalar1=pen[:, g:g + 1],
                                                  scalar2=None, op0=ALU.add), ['ch', 'pen', 'cm'], ['cm'])
            dve(lambda e: e.max(out=s8[:], in_=cm[:]), ['cm', 's8'], ['s8'])
            dve(lambda e: e.tensor_scalar(out=em[:], in0=cm[:], scalar1=s8[:, 7:8], scalar2=None, op0=ALU.is_ge), ['cm', 's8'], ['em'])
            dve(lambda e: e.tensor_tensor(out=em[:], in0=em[:], in1=sc_[:], op=ALU.mult), ['em', 'sc'], ['em'])
            dve(lambda e: e.tensor_reduce(out=den[:, 0:1], in_=em[:], axis=AX.X, op=ALU.add), ['em'], ['den'])
            dve(lambda e: e.reciprocal(out=den[:, 1:2], in_=den[:, 0:1]), ['den'], ['den'])
            dve(lambda e: e.tensor_scalar(out=em[:], in0=em[:], scalar1=den[:, 1:2], scalar2=2.5, op0=ALU.mult, op1=ALU.mult), ['em', 'den'], ['em'])
            pt_, ptk = ptb[:], 'pm_b'
            dve(lambda e: e.tensor_copy(out=emb[:], in_=em[:]), ['em', 'emb'], ['emb'])
            P.op('pe', lambda e, pt_=pt_: e.transpose(out=pt_, in_=emb[:], identity=G['identb'][:]), reads=['emb'], writes=[ptk])
            P.op('act', lambda e, pt_=pt_, i=i: e.activation(out=gwT[:, i * 128:(i + 1) * 128], in_=pt_, func=AF.Copy), reads=[ptk], writes=['gwT'])
    if G.get('gstop') == 'router':
        return
    wgr = Rot(P, 2, [128, 8, 256], BF16, 'wg')
    wur = Rot(P, 2, [128, 8, 256], BF16, 'wu')
    wdr = Rot(P, 2, [128, 2, D], BF16, 'wd')
    selr = Rot(P, 2, [64, 128], BF16, 'sel')
    pgu = Rot(P, 2, [128, 4, 256], F32, 'pgu', psum=True)
    py = Rot(P, 2, [128, 512], F32, 'py', psum=True)
    sgr_ = Rot(P, 2, [128, 2, 256], F32, 'sg')
    tr_ = Rot(P, 2, [128, 2, 256], F32, 'tt')
    actr = Rot(P, 2, [128, 2, 256], BF16, 'act')
    for e_ in range(G.get('nexp', 65)):
        wg, wgk = wgr.next(); wu, wuk = wur.next(); wd, wdk = wdr.next()
        if e_ < 64:
            sg_, su_, sd_ = T['exp_gate'][e_], T['exp_up'][e_], T['exp_down'][e_]
        else:
            sg_, su_, sd_ = T['sh_gate'], T['sh_up'], T['sh_down']
        P.dma('pool', lambda e, wg=wg, sg_=sg_: e.dma_start(out=wg[:], in_=sg_.rearrange("(k p) f -> p k f", p=128)), writes=[wgk])
        P.dma('pool', lambda e, wu=wu, su_=su_: e.dma_start(out=wu[:], in_=su_.rearrange("(k p) f -> p k f", p=128)), writes=[wuk])
        P.dma('pool', lambda e, wd=wd, sd_=sd_: e.dma_start(out=wd[:], in_=sd_.rearrange("(k p) f -> p k f", p=128)), writes=[wdk])
        if e_ < 64:
            sel, selk = selr.next()
            P.op('pool', lambda e, sel=sel, e_=e_: e.tensor_scalar(out=sel[:], in0=ones128[:], scalar1=G['identf'][0:64, e_:e_ + 1], scalar2=0.0, op0=ALU.mult, op1=ALU.add),
                 reads=['ones128'], writes=[selk])
        for tgp in range(16):
            hg, hk = hrot.next()
            P.dma('sp' if tgp % 2 == 0 else 'act', lambda e, hg=hg, tgp=tgp: e.dma_start(out=hg[:], in_=h2T[:, :, tgp * 256:(tgp + 1) * 256]), writes=[hk])
            p_, pk = pgu.next()
            for s_, (w_, wk_) in enumerate(((wg, wgk), (wg, wgk), (wu, wuk), (wu, wuk))):
                ft = s_ % 2
                for k in range(8):
                    mm(P, p_[:, s_, :], w_[:, k, ft * 128:(ft + 1) * 128], hg[:, k, :], k == 0, k == 7, [wk_, hk], [pk])
            sg, sgk = sgr_.next(); t_, tk = tr_.next(); ac, ack = actr.next()
            P.op('act', lambda e, sg=sg, p_=p_: e.activation(out=sg[:], in_=p_[:, 0:2, :], func=AF.Silu), reads=[pk], writes=[sgk])
            P.op('dve', lambda e, sg=sg, p_=p_, t_=t_: e.tensor_tensor(out=t_[:], in0=p_[:, 2:4, :], in1=sg[:], op=ALU.mult), reads=[pk, sgk], writes=[tk])
            if e_ < 64:
                pw, pwk = pmisc[:, 256:512], 'pm_c'
                mm(P, pw, sel[:], gwT[:, tgp * 256:(tgp + 1) * 256], True, True, [selk, 'gwT'], [pwk])
                for ft in range(2):
                    P.op('dve', lambda e, ac=ac, t_=t_, pw=pw, ft=ft: e.tensor_tensor(out=ac[:, ft, :], in0=pw, in1=t_[:, ft, :], op=ALU.mult),
                         reads=[tk, pwk], writes=[ack])
            else:
                P.op('pool', lambda e, ac=ac, t_=t_: e.tensor_copy(out=ac[:], in_=t_[:]), reads=[tk], writes=[ack])
            for tt in range(2):
                i = tgp * 2 + tt
                for half in range(2):
                    q_, qk = py.next()
                    cs_ = slice(half * 512, (half + 1) * 512)
                    for ft in range(2):
                        mm(P, q_[:], ac[:, ft, tt * 128:(tt + 1) * 128], wd[:, ft, cs_], ft == 0, ft == 1, [ack, wdk], [qk])
                    if e_ == 0:
                        P.op('act', lambda e, q_=q_, i=i, cs_=cs_: e.activation(out=yacc[:, i, cs_], in_=q_[:], func=AF.Copy), reads=[qk], writes=[f'y{i}'])
                    else:
                        P.op('dve', lambda e, q_=q_, i=i, cs_=cs_: e.tensor_tensor(out=yacc[:, i, cs_], in0=q_[:], in1=yacc[:, i, cs_], op=ALU.add),
                             reads=[qk, f'y{i}'], writes=[f'y{i}'])


def phase_H(P, T, G):
    yacc = G['yacc']
    dve = lambda fn, r, w: P.op('dve', fn, reads=r, writes=w)
    g2b = P.sb([128, D], F32)
    fing = P.sb([128, D], F32)
    P.dma('sp', lambda e: e.dma_start(out=g2b[:], in_=T['modd'][:, 5120:6144]), writes=['g2b'])
    P.dma('sp', lambda e: e.dma_start(out=fing[:], in_=T['final_g'].partition_broadcast(128)), writes=['fing'])
    xr = Rot(P, 2, [128, D], F32, 'x1')
    junk, ss, rs = P.sb([128, D], F32), P.sb([128, 1], F32), P.sb([128, 1], F32)
    for i in range(NT):
        tsl = slice(i * 128, (i + 1) * 128)
        xt, xk = xr.next()
        P.dma('sp', lambda e, xt=xt, tsl=tsl: e.dma_start(out=xt[:], in_=T['x1'][tsl, :]), writes=[xk])
        dve(lambda e, i=i: e.tensor_tensor(out=yacc[:, i, :], in0=yacc[:, i, :], in1=g2b[:], op=ALU.mult), [f'y{i}', 'g2b'], [f'y{i}'])
        P.op('pool', lambda e, i=i, xt=xt: e.tensor_tensor(out=xt[:], in0=xt[:], in1=yacc[:, i, :], op=ALU.add), reads=[f'y{i}', xk], writes=[xk])
        rms_rstd(P, xt, xk, junk, ss, rs, 'f')
        dve(lambda e, xt=xt: e.scalar_tensor_tensor(out=xt[:], in0=xt[:], scalar=rs[:, 0:1], in1=fing[:], op0=ALU.mult, op1=ALU.mult),
            [xk, 'rsf', 'fing'], [xk])
        P.dma('sp', lambda e, xt=xt, tsl=tsl: e.dma_start(out=T['out'][tsl, :], in_=xt[:]), reads=[xk])


NBLK = 320


def router_setup(P, T, G):
    R = {}
    R['rwt'] = P.sb([128, 8, 64], BF16)
    R['rbias'] = P.sb([128, 64], F32)
    R['iota'] = P.sb([128, 64], F32)
    R['su'] = P.sb([128, 128], BF16)
    R['onesb'] = P.sb([128, 128], BF16)
    R['cnt'] = P.sb([128, 64], F32)
    R['kmf'] = P.sb([128, 128], F32)
    P.dma('pool', lambda e: e.dma_start(out=R['rwt'][:], in_=T['router_w'].rearrange("(k p) n -> p k n", p=128)), writes=['rwt'])
    P.dma('sp', lambda e: e.dma_start(out=R['rbias'][:], in_=T['router_bias'].partition_broadcast(128)), writes=['rbias'])
    P.dma('sp', lambda e: e.dma_start(out=R['iota'][:], in_=T['k_rel'][0:1, 0:64].rearrange("o f -> (o f)").partition_broadcast(128)), writes=['iota'])
    P.dma('sp', lambda e: e.dma_start(out=R['kmf'][:], in_=T['k_masks'][:, 0:128]), writes=['kmf'])
    P.op('dve', lambda e: e.tensor_copy(out=R['su'][:], in_=R['kmf'][:]), reads=['kmf'], writes=['su'])
    P.op('dve', lambda e: e.memset(R['onesb'][:], 1.0), writes=['onesb'])
    P.op('dve', lambda e: e.memset(R['cnt'][:], 0.0), writes=['cnt'])
    R['pm'] = P.ps([128, 512], F32)
    for n in ('sc', 'ch', 'tmp', 'cm', 'em', 'mk', 'oh', 'rk', 'jk'):
        R[n] = P.sb([128, 64], F32)
    R['mkb'] = P.sb([128, 64], BF16)
    for n in ('m1', 'm2', 'grp', 's8', 'gmask', 'pen', 'den', 'i8f'):
        R[n] = P.sb([128, 8], F32)
    R['i8u'] = P.sb([128, 8], U32)
    R['meta'] = P.sb([128, 24], F32)
    return R


def router_tile(P, T, R, h2t, h2k, i):
    dve = lambda fn, r, w: P.op('dve', fn, reads=r, writes=w)
    pm = R['pm']
    sc_, ch, tmp, cm, em, mk, oh, rk, jk, mkb = [R[n] for n in ('sc', 'ch', 'tmp', 'cm', 'em', 'mk', 'oh', 'rk', 'jk', 'mkb')]
    m1, m2, grp, s8, gmask, pen, den, i8f, i8u, meta = [R[n] for n in ('m1', 'm2', 'grp', 's8', 'gmask', 'pen', 'den', 'i8f', 'i8u', 'meta')]
    for k in range(8):
        mm(P, pm[:, 0:64], h2t[:, k, :], R['rwt'][:, k, :], k == 0, k == 7, [h2k, 'rwt'], ['pm_a'])
    P.op('act', lambda e: e.activation(out=sc_[:], in_=pm[:, 0:64], func=AF.Sigmoid), reads=['pm_a'], writes=['sc'])
    dve(lambda e: e.tensor_tensor(out=ch[:], in0=sc_[:], in1=R['rbias'][:], op=ALU.add), ['sc', 'rbias'], ['ch'])
    dve(lambda e: e.tensor_reduce(out=m1[:], in_=ch[:].rearrange("p (g e) -> p g e", g=8), axis=AX.X, op=ALU.max), ['ch'], ['m1'])
    for g in range(8):
        dve(lambda e, g=g: e.tensor_scalar(out=tmp[:, g * 8:(g + 1) * 8], in0=ch[:, g * 8:(g + 1) * 8], scalar1=m1[:, g:g + 1],
                                          scalar2=-1e9, op0=ALU.is_equal, op1=ALU.mult), ['ch', 'm1', 'tmp'], ['tmp'])
    dve(lambda e: e.tensor_tensor(out=tmp[:], in0=tmp[:], in1=ch[:], op=ALU.add), ['tmp', 'ch'], ['tmp'])
    dve(lambda e: e.tensor_reduce(out=m2[:], in_=tmp[:].rearrange("p (g e) -> p g e", g=8), axis=AX.X, op=ALU.max), ['tmp'], ['m2'])
    dve(lambda e: e.tensor_tensor(out=grp[:], in0=m1[:], in1=m2[:], op=ALU.add), ['m1', 'm2'], ['grp'])
    dve(lambda e: e.max(out=s8[:], in_=grp[:]), ['grp'], ['s8'])
    dve(lambda e: e.tensor_scalar(out=gmask[:], in0=grp[:], scalar1=s8[:, 3:4], scalar2=None, op0=ALU.is_ge), ['grp', 's8'], ['gmask'])
    dve(lambda e: e.tensor_scalar(out=pen[:], in0=gmask[:], scalar1=-1.0, scalar2=1e9, op0=ALU.add, op1=ALU.mult), ['gmask'], ['pen'])
    for g in range(8):
        dve(lambda e, g=g: e.tensor_scalar(out=cm[:, g * 8:(g + 1) * 8], in0=ch[:, g * 8:(g + 1) * 8], scalar1=pen[:, g:g + 1],
                                          scalar2=None, op0=ALU.add), ['ch', 'pen', 'cm'], ['cm'])
    dve(lambda e: e.max(out=s8[:], in_=cm[:]), ['cm', 's8'], ['s8'])
    dve(lambda e: e.max_index(out=i8u[:], in_max=s8[:], in_values=cm[:]), ['cm', 's8', 'i8u'], ['i8u'])
    dve(lambda e: e.tensor_copy(out=meta[:, 0:8], in_=i8u[:]), ['i8u', 'meta'], ['meta'])
    dve(lambda e: e.tensor_scalar(out=mk[:], in0=cm[:], scalar1=s8[:, 7:8], scalar2=None, op0=ALU.is_ge), ['cm', 's8'], ['mk'])
    dve(lambda e: e.tensor_copy(out=mkb[:], in_=mk[:]), ['mk', 'mkb'], ['mkb'])
    dve(lambda e: e.tensor_tensor(out=em[:], in0=mk[:], in1=sc_[:], op=ALU.mult), ['mk', 'sc'], ['em'])
    dve(lambda e: e.tensor_reduce(out=den[:, 0:1], in_=em[:], axis=AX.X, op=ALU.add), ['em'], ['den'])
    dve(lambda e: e.reciprocal(out=den[:, 1:2], in_=den[:, 0:1]), ['den'], ['den'])
    dve(lambda e: e.tensor_scalar(out=em[:], in0=em[:], scalar1=den[:, 1:2], scalar2=2.5, op0=ALU.mult, op1=ALU.mult), ['em', 'den'], ['em'])
    mm(P, pm[:, 64:128], R['su'][:], mkb[:], True, True, ['su', 'mkb'], ['pm_b'])
    mm(P, pm[:, 128:192], R['onesb'][:], mkb[:], True, True, ['onesb', 'mkb'], ['pm_c'])
    dve(lambda e: e.tensor_tensor(out=rk[:], in0=pm[:, 64:128], in1=R['cnt'][:], op=ALU.add), ['pm_b', 'cnt', 'rk'], ['rk'])
    dve(lambda e: e.tensor_tensor(out=R['cnt'][:], in0=pm[:, 128:192], in1=R['cnt'][:], op=ALU.add), ['pm_c', 'cnt'], ['cnt'])
    for k in range(8):
        dve(lambda e, k=k: e.tensor_scalar(out=oh[:], in0=R['iota'][:], scalar1=meta[:, k:k + 1], scalar2=None, op0=ALU.is_equal),
            ['iota', 'meta', 'oh'], ['oh'])
        dve(lambda e, k=k: e.scalar_tensor_tensor(out=jk[:], in0=oh[:], scalar=1.0, in1=rk[:], op0=ALU.mult, op1=ALU.mult, accum_out=meta[:, 8 + k:9 + k]), ['oh', 'rk', 'jk', 'meta'], ['jk', 'meta'])
        dve(lambda e, k=k: e.scalar_tensor_tensor(out=jk[:], in0=oh[:], scalar=1.0, in1=em[:], op0=ALU.mult, op1=ALU.mult, accum_out=meta[:, 16 + k:17 + k]), ['oh', 'em', 'jk', 'meta'], ['jk', 'meta'])
    P.dma('sp', lambda e: e.dma_start(out=T['meta'][i * 128:(i + 1) * 128, :], in_=meta[:]), reads=['meta'])


def phase_S0(P, T, G):
    st32 = Rot(P, 2, [128, 1024], F32, 's32')
    st16 = Rot(P, 1, [128, 1024], BF16, 's16')
    n = 0
    for e in range(65):
        for (src, shsrc, dst) in (('exp_gate', 'sh_gate', 'wg16'), ('exp_up', 'sh_up', 'wu16'), ('exp_down', 'sh_down', 'wd16')):
            s_ap = T[src][e] if e < 64 else T[shsrc]
            f = 256 if dst != 'wd16' else D
            rows = 512 if dst != 'wd16' else 128
            c0 = {'wg16': 0, 'wu16': 2048, 'wd16': 4096}[dst]
            for hh in range(2):
                a, ak = st32.next()
                c, ck = st16.next()
                src_ap = s_ap[hh * rows:(hh + 1) * rows, :].rearrange("(k p) f -> p k f", p=128)
                q = 'sp' if n % 2 == 0 else 'act'
                P.dma(q, lambda e_, a=a, src_ap=src_ap, f=f: e_.dma_start(out=a[:].rearrange("p (k f) -> p k f", f=f), in_=src_ap), writes=[ak])
                eng = ('dve', 'pool')[n % 2]
                P.op(eng, lambda e_, a=a, c=c: e_.tensor_copy(out=c[:], in_=a[:]), reads=[ak], writes=[ck])
                cc = c0 + hh * 1024
                P.dma('act' if n % 2 == 0 else 'sp', lambda e_, c=c, cc=cc, e=e: e_.dma_start(out=T['wall16'][e * 128:(e + 1) * 128, cc:cc + 1024], in_=c[:]), reads=[ck])
                n += 1
                yield


def phase_S(P, T, G):
    identb = G['identb']
    dve = lambda fn, r, w: P.op('dve', fn, reads=r, writes=w)
    cnt = P.sb([128, 64], F32)
    ci = P.sb([128, 64], I32)
    pad = P.sb([128, 64], F32)
    pend = P.sb([128, 64], F32)
    pst = P.sb([128, 64], F32)
    ones64f = P.sb([128, 64], F32)
    iota = P.sb([128, 64], F32)
    bst = P.sb([128, NBLK], F32)
    bef = P.sb([128, NBLK], F32)
    bei = P.sb([128, NBLK], I32)
    P.dma('sp', lambda e: e.dma_start(out=cnt[:], in_=T['cntd']), writes=['cnt'])
    P.dma('sp', lambda e: e.dma_start(out=iota[:], in_=T['k_rel'][0:1, 0:64].rearrange("o f -> (o f)").partition_broadcast(128)), writes=['iota'])
    P.dma('sp', lambda e: e.dma_start(out=bst[:], in_=T['k_bst'].partition_broadcast(128)), writes=['bst'])
    dve(lambda e: e.memset(ones64f[:], 1.0), [], ['ones64f'])
    dve(lambda e: e.tensor_scalar(out=pad[:], in0=cnt[:], scalar1=127.0, scalar2=None, op0=ALU.add), ['cnt'], ['pad'])
    dve(lambda e: e.tensor_copy(out=ci[:], in_=pad[:]), ['pad'], ['ci'])
    dve(lambda e: e.tensor_scalar(out=ci[:], in0=ci[:], scalar1=7, scalar2=None, op0=ALU.arith_shift_right), ['ci'], ['ci'])
    dve(lambda e: e.tensor_scalar(out=ci[:], in0=ci[:], scalar1=7, scalar2=None, op0=ALU.logical_shift_left), ['ci'], ['ci'])
    dve(lambda e: e.tensor_copy(out=pad[:], in_=ci[:]), ['ci', 'pad'], ['pad'])
    dve(lambda e: e.tensor_tensor_scan(out=pend[:], data0=ones64f[:], data1=pad[:], initial=0.0, op0=ALU.mult, op1=ALU.add),
        ['ones64f', 'pad'], ['pend'])
    dve(lambda e: e.tensor_tensor(out=pst[:], in0=pend[:], in1=pad[:], op=ALU.subtract), ['pend', 'pad'], ['pst'])
    for ex in range(64):
        if ex == 0:
            dve(lambda e: e.tensor_scalar(out=bef[:], in0=bst[:], scalar1=pend[:, 0:1], scalar2=None, op0=ALU.is_ge), ['bst', 'pend'], ['bef'])
        else:
            dve(lambda e, ex=ex: e.scalar_tensor_tensor(out=bef[:], in0=bst[:], scalar=pend[:, ex:ex + 1], in1=bef[:], op0=ALU.is_ge, op1=ALU.add),
                ['bst', 'pend', 'bef'], ['bef'])
    dve(lambda e: e.tensor_scalar(out=bef[:], in0=bef[:], scalar1=63.0, scalar2=None, op0=ALU.min), ['bef'], ['bef'])
    pcol = P.sb([128, 1], F32)
    widxf = P.sb([128, NBLK], F32)
    widx = P.sb([128, NBLK], I32)
    P.dma('sp', lambda e: e.dma_start(out=pcol[:], in_=T['k_rel'][:, 0:1], allow_slow_non_contiguous=True), writes=['pcol'])
    dve(lambda e: e.tensor_scalar(out=pcol[:], in0=pcol[:], scalar1=-1.0, scalar2=None, op0=ALU.mult), ['pcol'], ['pcol'])
    dve(lambda e: e.tensor_scalar(out=widxf[:], in0=bef[:], scalar1=128.0, scalar2=pcol[:, 0:1], op0=ALU.mult, op1=ALU.add), ['bef', 'pcol'], ['widxf'])
    chg = P.sb([128, NBLK], F32)
    dve(lambda e: e.memset(chg[:], 1.0), [], ['chg'])
    dve(lambda e: e.tensor_tensor(out=chg[:, 3:NBLK], in0=bef[:, 3:NBLK], in1=bef[:, 0:NBLK - 3], op=ALU.not_equal), ['bef', 'chg'], ['chg'])
    dve(lambda e: e.scalar_tensor_tensor(out=widxf[:], in0=widxf[:], scalar=-1.0e6, in1=chg[:], op0=ALU.add, op1=ALU.mult), ['widxf', 'chg'], ['widxf'])
    dve(lambda e: e.tensor_scalar(out=widxf[:], in0=widxf[:], scalar1=1.0e6, scalar2=None, op0=ALU.add), ['widxf'], ['widxf'])
    dve(lambda e: e.tensor_copy(out=widx[:], in_=widxf[:]), ['widxf'], ['widx'])
    d8i = P.sb([128, NT, 8], I32)
    gw8 = P.sb([128, NT, 8], F32)
    mrot = Rot(P, 2, [128, 24], F32, 'meta')
    hrot = Rot(P, 2, [128, D], BF16, 'htok')
    oh, jk = P.sb([128, 64], F32), P.sb([128, 64], F32)
    d8f = P.sb([128, 8], F32)
    for i in range(NT):
        tsl = slice(i * 128, (i + 1) * 128)
        mt, mtk = mrot.next()
        ht, htk = hrot.next()
        P.dma('sp', lambda e, mt=mt, tsl=tsl: e.dma_start(out=mt[:], in_=T['meta'][tsl, :]), writes=[mtk])
        P.dma('act', lambda e, ht=ht, tsl=tsl: e.dma_start(out=ht[:], in_=T['h2tok'][tsl, :]), writes=[htk])
        for k in range(8):
            dve(lambda e, mt=mt, k=k: e.tensor_scalar(out=oh[:], in0=iota[:], scalar1=mt[:, k:k + 1], scalar2=None, op0=ALU.is_equal),
                ['iota', mtk, 'oh'], ['oh'])
            dve(lambda e, k=k: e.scalar_tensor_tensor(out=jk[:], in0=oh[:], scalar=1.0, in1=pst[:], op0=ALU.mult, op1=ALU.mult, accum_out=d8f[:, k:k + 1]), ['oh', 'pst', 'jk', 'd8f'], ['jk', 'd8f'])
        dve(lambda e, mt=mt: e.tensor_tensor(out=d8f[:], in0=d8f[:], in1=mt[:, 8:16], op=ALU.add), ['d8f', mtk], ['d8f'])
        dve(lambda e, i=i: e.tensor_copy(out=d8i[:, i, :], in_=d8f[:]), ['d8f'], [f'd8i{i}'])
        dve(lambda e, mt=mt, i=i: e.tensor_copy(out=gw8[:, i, :], in_=mt[:, 16:24]), [mtk], [f'gw{i}'])
        for k in range(8):
            P.dma('pool', lambda e, ht=ht, i=i, k=k: e.indirect_dma_start(
                out=T['Xs'], out_offset=bass.IndirectOffsetOnAxis(ap=d8i[:, i, k:k + 1], axis=0), in_=ht[:], in_offset=None),
                reads=[htk, f'd8i{i}'])
    wgu = Rot(P, 3, [128, 6144], BF16, 'wgu')
    wsh = P.sb([128, 6144], BF16)
    P.dma('sp', lambda e: e.dma_start(out=wsh[:], in_=T['wall16'][64 * 128:65 * 128, :]), writes=['wsh'])
    xbr = Rot(P, 4, [128, D], BF16, 'xb')
    xTr = Rot(P, 2, [128, 8, 128], BF16, 'xT')
    sgr_ = Rot(P, 2, [128, 256], F32, 'sg')
    acr = Rot(P, 2, [128, 256], BF16, 'ac')
    aTr = Rot(P, 2, [128, 2, 128], BF16, 'aT')
    ybr = Rot(P, 2, [128, D], BF16, 'yb')
    ptx = Rot(P, 1, [128, 8, 128], BF16, 'ptx', psum=True)
    pgu = Rot(P, 2, [128, 512], F32, 'pgu', psum=True)
    pta = Rot(P, 1, [128, 2, 128], BF16, 'pta', psum=True)
    pyd = Rot(P, 3, [128, 512], F32, 'pyd', psum=True)
    regn = [0]

    P.emit(keep=True)
    hold = {}
    blocks = [('r', b) for b in range(NBLK)] + [('s', i) for i in range(NT)]
    st = {}

    ld = {}

    def stageL(kind, b):
        xb, xk = xbr.next()
        if kind == 'r':
            wl, wk = wgu.next()

            def gat(e):
                if 'bc' not in hold:
                    hold['bc'] = e.alloc_register("bc_reg")
                    e.reg_mov(hold['bc'], 65 * 128 - 1)
                return e.indirect_dma_start(out=wl[:], out_offset=None, in_=T['wall16'],
                                            in_offset=bass.IndirectOffsetOnAxis(ap=widx[:, b:b + 1], axis=0),
                                            bounds_check=hold['bc'], oob_is_err=False)
            P.dma('pool', gat, reads=['widx'], writes=[wk])
            P.dma('sp', lambda e: e.dma_start(out=xb[:], in_=T['Xs'][b * 128:(b + 1) * 128, :]), writes=[xk])
        else:
            wl, wk = wsh, 'wsh'
            P.dma('sp', lambda e: e.dma_start(out=xb[:], in_=T['h2tok'][b * 128:(b + 1) * 128, :]), writes=[xk])
        ld[(kind, b)] = (xb, xk, wl, wk)

    def stageA(kind, b):
        xb, xk, wl, wk = ld.pop((kind, b))
        wga, wua = wl[:, 0:2048], wl[:, 2048:4096]
        wd, wdk = wl[:, 4096:6144].rearrange("p (k f) -> p k f", f=D), wk
        px, pxk = ptx.next()
        for k in range(8):
            P.op('pe', lambda e, k=k: e.transpose(out=px[:, k, :], in_=xb[:, k * 128:(k + 1) * 128], identity=identb[:]), reads=[xk], writes=[pxk])
        xT, xTk = xTr.next()
        P.op('act', lambda e: e.activation(out=xT[:], in_=px[:], func=AF.Copy), reads=[pxk], writes=[xTk])
        pg_, pgk = pgu.next()
        for k in range(8):
            mm(P, pg_[:, 0:256], xT[:, k, :], wga[:, k * 256:(k + 1) * 256], k == 0, False, [xTk, wk], [pgk])
        for k in range(8):
            P.op('pe', lambda e, k=k: e.matmul(pg_[:, 256:512], lhsT=xT[:, k, :], rhs=wua[:, k * 256:(k + 1) * 256], start=False, stop=(k == 7), skip_group_check=True), reads=[xTk, wk], writes=[pgk])
        st[(kind, b)] = (pg_, pgk, wd, wdk)

    def stageB(kind, b):
        pg_, pgk, wd, wdk = st.pop((kind, b))
        sg, sgk = sgr_.next(); ac, ack = acr.next()
        P.op('act', lambda e: e.activation(out=sg[:], in_=pg_[:, 0:256], func=AF.Silu), reads=[pgk], writes=[sgk])
        dve(lambda e: e.tensor_tensor(out=ac[:], in0=pg_[:, 256:512], in1=sg[:], op=ALU.mult), [pgk, sgk], [ack])
        pa, pak = pta.next()
        for ft in range(2):
            P.op('pe', lambda e, ft=ft: e.transpose(out=pa[:, ft, :], in_=ac[:, ft * 128:(ft + 1) * 128], identity=identb[:]), reads=[ack], writes=[pak])
        aT, aTk = aTr.next()
        dve(lambda e: e.tensor_copy(out=aT[:], in_=pa[:]), [pak], [aTk])
        yb, ybk = ybr.next()
        for half in range(2):
            py_, pyk = pyd.next()
            cs_ = slice(half * 512, (half + 1) * 512)
            for ft in range(2):
                mm(P, py_[:], aT[:, ft, :], wd[:, ft, cs_], ft == 0, ft == 1, [aTk, wdk], [pyk])
            if half == 0:
                P.op('act', lambda e, py_=py_, cs_=cs_: e.activation(out=yb[:, cs_], in_=py_[:], func=AF.Copy), reads=[pyk], writes=[ybk])
            else:
                dve(lambda e, py_=py_, cs_=cs_: e.tensor_copy(out=yb[:, cs_], in_=py_[:]), [pyk], [ybk])
        dst = T['Ys'] if kind == 'r' else T['Ysh']
        P.dma('sp', lambda e: e.dma_start(out=dst[b * 128:(b + 1) * 128, :], in_=yb[:]), reads=[ybk])

    stageL(*blocks[0])
    stageL(*blocks[1])
    stageA(*blocks[0])
    for bi in range(len(blocks)):
        if bi + 2 < len(blocks):
            stageL(*blocks[bi + 2])
        if bi + 1 < len(blocks):
            stageA(*blocks[bi + 1])
        stageB(*blocks[bi])
    P.emit(keep=True)
    g2b = P.sb([128, D], F32)
    fing = P.sb([128, D], F32)
    P.dma('sp', lambda e: e.dma_start(out=g2b[:], in_=T['modd'][:, 5120:6144]), writes=['g2b'])
    P.dma('sp', lambda e: e.dma_start(out=fing[:], in_=T['final_g'].partition_broadcast(128)), writes=['fing'])
    xr = Rot(P, 2, [128, D], F32, 'x1')
    grot = Rot(P, 4, [128, D], BF16, 'gat')
    shr = Rot(P, 2, [128, D], BF16, 'shr')
    acc = P.sb([128, D], F32)
    junk, ss, rs = P.sb([128, D], F32), P.sb([128, 1], F32), P.sb([128, 1], F32)
    for i in range(NT):
        tsl = slice(i * 128, (i + 1) * 128)
        xt, xk = xr.next()
        sh, shk = shr.next()
        P.dma('sp', lambda e, xt=xt, tsl=tsl: e.dma_start(out=xt[:], in_=T['x1'][tsl, :]), writes=[xk])
        P.dma('act', lambda e, sh=sh, tsl=tsl: e.dma_start(out=sh[:], in_=T['Ysh'][tsl, :]), writes=[shk])
        for k in range(8):
            gt, gtk = grot.next()
            P.dma('pool', lambda e, gt=gt, i=i, k=k: e.indirect_dma_start(
                out=gt[:], out_offset=None, in_=T['Ys'], in_offset=bass.IndirectOffsetOnAxis(ap=d8i[:, i, k:k + 1], axis=0)),
                reads=[f'd8i{i}'], writes=[gtk])
            if k == 0:
                dve(lambda e, gt=gt, i=i, sh=sh: e.scalar_tensor_tensor(out=acc[:], in0=gt[:], scalar=gw8[:, i, 0:1], in1=sh[:], op0=ALU.mult, op1=ALU.add),
                    [gtk, f'gw{i}', shk, 'acc'], ['acc'])
            else:
                dve(lambda e, gt=gt, i=i, k=k: e.scalar_tensor_tensor(out=acc[:], in0=gt[:], scalar=gw8[:, i, k:k + 1], in1=acc[:], op0=ALU.mult, op1=ALU.add),
                    [gtk, f'gw{i}', 'acc'], ['acc'])
        dve(lambda e: e.tensor_tensor(out=acc[:], in0=acc[:], in1=g2b[:], op=ALU.mult), ['acc', 'g2b'], ['acc'])
        P.op('pool', lambda e, xt=xt: e.tensor_tensor(out=xt[:], in0=xt[:], in1=acc[:], op=ALU.add), reads=['acc', xk], writes=[xk])
        rms_rstd(P, xt, xk, junk, ss, rs, 'f')
        dve(lambda e, xt=xt: e.scalar_tensor_tensor(out=xt[:], in0=xt[:], scalar=rs[:, 0:1], in1=fing[:], op0=ALU.mult, op1=ALU.mult),
            [xk, 'rsf', 'fing'], [xk])
        P.dma('sp', lambda e, xt=xt, tsl=tsl: e.dma_start(out=T['out'][tsl, :], in_=xt[:]), reads=[xk])


SCRATCH = [
    ('zq', [512, S], BF16), ('zk', [512, S], BF16), ('zv', [S, 512], BF16), ('ziq', [512, S], BF16),
    ('zik', [32, S], BF16), ('ziw', [S, 16], F32), ('zr', [1792, S], F32), ('zga', [1024, S], BF16),
    ('zgr', [1024, S], BF16), ('modd', [128, 6 * D], F32), ('attnT', [512, S], BF16), ('rwT', [512, S], BF16),
    ('x1', [S, D], F32), ('h2T', [D, S], BF16), ('h2tok', [S, D], BF16), ('meta', [S, 24], F32), ('cntd', [128, 64], F32),
    ('Xs', [NBLK * 128, D], BF16), ('Ys', [NBLK * 128, D], BF16), ('Ysh', [S, D], BF16),
    ('wall16', [65 * 128, 6144], BF16),
]

INPUT_SHAPES = [
    ('x', [S, D]), ('c_col', [128, 8]), ('ada_w', [D, 6 * D]), ('ada_b', [1, 6 * D]), ('norm1_g', [D]),
    ('w_in', [D, NIN]), ('rel_bias', [256]), ('tshift_mu', [1792]), ('decay_w0', [512]), ('decay_up', [64, 512]),
    ('iclr_a0', [512]), ('iclr_up', [64, 512]), ('gate_up', [128, 512]), ('k_k', [512]), ('k_a', [512]),
    ('r_k', [512]), ('lnx_g', [512]), ('lnx_b', [512]), ('w_attn_br', [512, D]), ('w_rwkv_br', [512, D]),
    ('w_out', [D, D]), ('norm2_g', [D]), ('router_w', [D, 64]), ('router_bias', [64]),
    ('exp_gate', [64, D, 256]), ('exp_up', [64, D, 256]), ('exp_down', [64, 256, D]),
    ('sh_gate', [D, 256]), ('sh_up', [D, 256]), ('sh_down', [256, D]), ('final_g', [D]),
    ('k_ident', [128, 128]), ('k_rel', [128, 256]), ('k_masks', [128, 896]), ('k_reset', [64, 512]), ('k_bst', [NBLK]),
]


def build(debug_outs=(), stop_after=None):
    nc = bass.Bass("TRN2", target_bir_lowering=False)
    T = {}
    for name, shp in INPUT_SHAPES:
        T[name] = nc.dram_tensor(name, shp, F32, kind="ExternalInput").ap()
    for name, shp, dt in SCRATCH:
        kind = "ExternalOutput" if name in debug_outs else "Internal"
        T[name] = nc.dram_tensor(name, shp, dt, kind=kind).ap()
    T['out'] = nc.dram_tensor('out', [S, D], F32, kind="ExternalOutput").ap()
    if 'dbg_y' in debug_outs:
        T['dbg_y'] = nc.dram_tensor('dbg_y', [64, 8, 512], F32, kind="ExternalOutput").ap()
        T['dbg_bon'] = nc.dram_tensor('dbg_bon', [64, 8, 512], BF16, kind="ExternalOutput").ap()
        T['dbg_g'] = nc.dram_tensor('dbg_g', [64, 8, 512], BF16, kind="ExternalOutput").ap()
        T['dbg_AR'] = nc.dram_tensor('dbg_AR', [64, 8, 4, 256], BF16, kind="ExternalOutput").ap()
        T['dbg_BK'] = nc.dram_tensor('dbg_BK', [64, 8, 4, 256], BF16, kind="ExternalOutput").ap()
    P = Prog(nc)
    G = {}
    G['E'] = P.gsb([128, 8, 256], F32)
    G['b31'] = P.gsb([128, 8], F32)
    G['ones_row'] = P.gsb([1, 128], F32)
    G['identf'] = P.gsb([128, 128], F32)
    G['identb'] = P.gsb([128, 128], BF16)
    G['eps'] = P.gsb([128, 1], F32)
    G_EPS[0] = G['eps']
    P.op('dve', lambda e: e.memset(G['ones_row'][:], 1.0), writes=['ones_row'])
    P.op('dve', lambda e: e.memset(G['eps'][:], 1e-6), writes=['eps'])
    P.dma('sp', lambda e: e.dma_start(out=G['identf'][:], in_=T['k_ident']), writes=['identf'])
    P.op('dve', lambda e: e.tensor_copy(out=G['identb'][:], in_=G['identf'][:]), reads=['identf'], writes=['identb'])
    G['sparse'] = SPARSE
    phase_A(P, T, G)
    P.emit()
    if stop_after == 'A':
        P.emit(); P.finish(); return nc
    G['sparse'] = SPARSE
    phase_BC(P, T, G)
    P.emit()
    if stop_after == 'C':
        P.finish(); return nc
    phase_D(P, T, G)
    P.emit()
    if stop_after == 'D':
        P.finish(); return nc
    phase_E(P, T, G)
    P.emit()
    if stop_after == 'E':
        P.finish(); return nc
    G['sparse'] = SPARSE
    phase_F(P, T, G)
    P.emit()
    if stop_after == 'F':
        P.finish(); return nc
    if SPARSE:
        phase_S(P, T, G)
        P.emit()
        P.finish()
        return nc
    G['yacc'] = P.gsb([128, NT, D], F32)
    if stop_after in ('router', 'exp1'):
        G['gstop'] = stop_after
        G['nexp'] = 1
    if stop_after == 'router':
        phase_G(P, T, G); P.emit(); P.finish(); return nc
    if stop_after == 'exp1':
        phase_G(P, T, G); P.emit(); P.finish(); return nc
    phase_G(P, T, G)
    P.emit()
    phase_H(P, T, G)
    P.emit()
    P.finish()
    return nc


def host_inputs(inputs, b):
    m = {}
    f = lambda a: np.ascontiguousarray(np.asarray(a, dtype=np.float32))
    m['x'] = f(inputs['x'][b])
    m['c_col'] = f(np.asarray(inputs['c'][b]).reshape(8, 128).T)
    for name, shp in INPUT_SHAPES:
        if name in ('x', 'c_col', 'k_ident', 'k_rel', 'k_masks', 'k_reset', 'k_bst'):
            continue
        a = np.asarray(inputs[name])
        m[name] = f(a.reshape(shp))
    m['k_ident'] = np.eye(128, dtype=np.float32)
    ii = np.arange(128)
    su = (ii[:, None] < ii[None, :]).astype(np.float32)
    iu = (ii[:, None] <= ii[None, :]).astype(np.float32)
    sl = (ii[:, None] > ii[None, :]).astype(np.float32)
    m['k_masks'] = np.ascontiguousarray(np.concatenate([su, iu, su, iu, sl, np.zeros((128, 256), np.float32)], axis=1))
    rs_ = np.ones((64, 512), np.float32); rs_[:, ::128] = 0.0
    m['k_reset'] = rs_
    m['k_bst'] = (np.arange(NBLK, dtype=np.float32) * 128.0)
    m['k_rel'] = (np.arange(256, dtype=np.float32)[None, :] - np.arange(128, dtype=np.float32)[:, None])
    return m


def kernel(**inputs):
    nc = build()
    in_maps = [host_inputs(inputs, b) for b in range(8)]
    res = run_bass_kernel_spmd(nc, in_maps, core_ids=list(range(8)))
    return np.stack([np.asarray(r['out']) for r in res.results], axis=0).astype(np.float32)
```

```python
import contextlib
import numpy as np
import concourse.bass as bass
import concourse.mybir as mybir

F32 = mybir.dt.float32
BF16 = mybir.dt.bfloat16
I32 = mybir.dt.int32
U32 = mybir.dt.uint32
AF = mybir.ActivationFunctionType
ALU = mybir.AluOpType
AX = mybir.AxisListType

N_DMA_SEMS = 8


class Prog:
    ENGS = ('pe', 'act', 'dve', 'pool', 'sp')

    def __init__(self, nc):
        self.nc = nc
        self.ops = {e: [] for e in self.ENGS}
        self.cnt = {e: 0 for e in self.ENGS}
        self.waited = {e: {} for e in self.ENGS}
        self.res = {}
        self.dma_tot = [0] * N_DMA_SEMS
        self.dma_rr = 0
        self.stack = contextlib.ExitStack()
        self.gstack = contextlib.ExitStack()
        self.nsb = 0
        self.nphase = 0
        self.sems = None

    def _new_sems(self):
        nc = self.nc
        self.sems = {}
        for e in self.ENGS:
            self.sems[e] = self.gstack.enter_context(nc.semaphore(f"s_{e}_{self.nphase}"))
        for i in range(N_DMA_SEMS):
            self.sems[('dma', i)] = self.gstack.enter_context(nc.semaphore(f"s_dma{i}_{self.nphase}"))
        self.cnt = {e: 0 for e in self.ENGS}
        self.waited = {e: {} for e in self.ENGS}
        self.dma_tot = [0] * N_DMA_SEMS
        self.dma_rr = 0

    def gsb(self, shape, dt, name=None):
        self.nsb += 1
        return self.gstack.enter_context(self.nc.sbuf_tensor(name or f"gsb{self.nsb}", list(shape), dt))

    def sb(self, shape, dt, name=None):
        self.nsb += 1
        return self.stack.enter_context(self.nc.sbuf_tensor(name or f"sb{self.nsb}", list(shape), dt))

    def ps(self, shape, dt, name=None):
        self.nsb += 1
        return self.stack.enter_context(self.nc.psum_tensor(name or f"ps{self.nsb}", list(shape), dt))

    def _deps(self, reads, writes):
        deps = {}
        def add(d):
            if d is None:
                return
            k, v = d
            if deps.get(k, 0) < v:
                deps[k] = v
        for k in reads:
            r = self.res.get(k)
            if r:
                add(r['w'])
        for k in writes:
            r = self.res.get(k)
            if r:
                add(r['w'])
                for d in r['r']:
                    add(d)
        return deps

    def _emit_waits(self, eng, deps):
        w = self.waited[eng]
        for k, v in deps.items():
            if w.get(k, 0) < v:
                w[k] = v
                self.ops[eng].append(('wait', k, v))

    def _update(self, dep, reads, writes):
        for k in reads:
            r = self.res.setdefault(k, {'w': None, 'r': []})
            r['r'] = [d for d in r['r'] if d[0] != dep[0]] + [dep]
        for k in writes:
            self.res[k] = {'w': dep, 'r': []}

    def op(self, eng, fn, reads=(), writes=()):
        if self.sems is None:
            self._new_sems()
        deps = self._deps(reads, writes)
        if eng == 'pe':
            deps.pop('pe', None)
        self._emit_waits(eng, deps)
        self.cnt[eng] += 1
        dep = (eng, self.cnt[eng])
        self.ops[eng].append(('op', fn))
        self._update(dep, reads, writes)
        return dep

    def dma(self, q, fn, reads=(), writes=()):
        if self.sems is None:
            self._new_sems()
        deps = self._deps(reads, writes)
        s = self.dma_rr
        self.dma_rr = (self.dma_rr + 1) % N_DMA_SEMS
        key = ('dma', s)
        if self.dma_tot[s] > 0:
            if deps.get(key, 0) < self.dma_tot[s]:
                deps[key] = self.dma_tot[s]
        self._emit_waits(q, deps)
        self.dma_tot[s] += 16
        dep = (key, self.dma_tot[s])
        self.ops[q].append(('dma', fn, s))
        self._update(dep, reads, writes)
        return dep

    def emit(self, keep=False):
        nc = self.nc
        with contextlib.ExitStack() as st:
            sems = self.sems
            self.nphase += 1
            block = st.enter_context(nc.Block(f"ph{self.nphase}"))
            engobj = {'pe': nc.tensor, 'act': nc.scalar, 'dve': nc.vector, 'pool': nc.gpsimd, 'sp': nc.sync}
            fin = {}
            for e in self.ENGS:
                if e != 'sp' and self.cnt[e] > 0:
                    fin[e] = self.cnt[e]
            for i in range(N_DMA_SEMS):
                if self.dma_tot[i] > 0:
                    fin[('dma', i)] = self.dma_tot[i]
            self._emit_waits('sp', fin)

            def run(e):
                eo = engobj[e]
                for item in self.ops[e]:
                    if item[0] == 'wait':
                        eo.wait_ge(sems[item[1]], item[2])
                    elif item[0] == 'op':
                        item[1](eo).then_inc(sems[e], 1)
                    else:
                        item[1](eo).then_inc(sems[('dma', item[2])], 16)

            @block.tensor
            def _(t):
                run('pe')

            @block.scalar
            def _(t):
                run('act')

            @block.vector
            def _(t):
                run('dve')

            @block.gpsimd
            def _(t):
                run('pool')

            @block.sync
            def _(t):
                run('sp')
        self.ops = {e: [] for e in self.ENGS}
        self.res = {}
        if keep:
            return
        self.stack.close()
        self.stack = contextlib.ExitStack()
        self.sems = None

    def finish(self):
        self.gstack.close()

from concourse.bass_utils import run_bass_kernel_spmd
import ml_dtypes

SPARSE = True
S = 4096
D = 1024
NT = S // 128
NIN = 5936


class Rot:
    def __init__(self, P, n, shape, dt, name, psum=False):
        self.tiles = [(P.ps(shape, dt) if psum else P.sb(shape, dt)) for _ in range(n)]
        self.name = name
        self.i = 0

    def next(self):
        t = self.tiles[self.i % len(self.tiles)]
        k = f"{self.name}{self.i % len(self.tiles)}"
        self.i += 1
        return t, k


class Rot2(Rot):
    def __init__(self, P, n, shape, dt, name):
        self.tiles = [(P.sb(shape, dt), P.sb(shape, dt)) for _ in range(n)]
        self.name = name
        self.i = 0


def mm(P, out, lhsT, rhs, start, stop, reads, writes):
    P.op('pe', lambda e: e.matmul(out, lhsT=lhsT, rhs=rhs, start=start, stop=stop), reads=reads, writes=writes)


def phase_A(P, T, G):
    mod_bc = P.sb([128, 6 * D], F32)
    ccol = P.sb([128, 8], F32)
    scol = P.sb([128, 8], F32)
    adab = P.sb([1, 6144], F32)
    modrow = P.sb([1, 6144], F32)
    ngb = P.sb([128, 1024], F32)
    P.dma('sp', lambda e: e.dma_start(out=ccol[:], in_=T['c_col']), writes=['ccol'])
    P.dma('sp', lambda e: e.dma_start(out=adab[:], in_=T['ada_b']), writes=['adab'])
    P.op('act', lambda e: e.activation(out=scol[:], in_=ccol[:], func=AF.Silu), reads=['ccol'], writes=['scol'])
    wrot = Rot(P, 2, [128, 8, 512], F32, 'aw')
    psr = Rot(P, 2, [1, 512], F32, 'psr', psum=True)
    psb = Rot(P, 2, [128, 512], F32, 'psb', psum=True)
    adaw = T['ada_w'].rearrange("(k p) n -> p k n", p=128)
    for n in range(12):
        wb, wk = wrot.next()
        P.dma('sp' if n % 2 == 0 else 'act',
              lambda e, wb=wb, n=n: e.dma_start(out=wb[:], in_=adaw[:, :, n * 512:(n + 1) * 512]), writes=[wk])
        pr, pk = psr.next()
        for k in range(8):
            mm(P, pr[:], scol[:, k:k + 1], wb[:, k, :], k == 0, k == 7, [wk, 'scol'], [pk])
        sl = slice(n * 512, (n + 1) * 512)
        P.op('dve', lambda e, pr=pr, sl=sl: e.tensor_tensor(out=modrow[0:1, sl], in0=pr[:], in1=adab[0:1, sl], op=ALU.add),
             reads=[pk, 'adab'], writes=[f'modrow{n}'])
        pb, pbk = psb.next()
        mm(P, pb[:], G['ones_row'][:], modrow[0:1, sl], True, True, [f'modrow{n}'], [pbk])
        P.op('act', lambda e, pb=pb, sl=sl: e.activation(out=mod_bc[:, sl], in_=pb[:], func=AF.Copy),
             reads=[pbk], writes=[f'mod{n}'])
    for (gname, c0, deps) in (('norm1_g', 1024, ['mod2', 'mod3']), ('norm2_g', 4096, ['mod8', 'mod9'])):
        P.dma('sp', lambda e, gname=gname: e.dma_start(out=ngb[:], in_=T[gname].partition_broadcast(128)), writes=['ngb'])
        P.op('dve', lambda e, c0=c0: e.scalar_tensor_tensor(out=mod_bc[:, c0:c0 + 1024], in0=mod_bc[:, c0:c0 + 1024],
                                                            scalar=1.0, in1=ngb[:], op0=ALU.add, op1=ALU.mult),
             reads=deps + ['ngb'], writes=deps)
    P.dma('sp', lambda e: e.dma_start(out=T['modd'], in_=mod_bc[:]), reads=[f'mod{n}' for n in range(12)])


def rms_rstd(P, xt, xk, junk, ss, rs, tag):
    P.op('act', lambda e: e.activation(out=junk[:], in_=xt[:], func=AF.Square, accum_out=ss[:]),
         reads=[xk], writes=['junk' + tag, 'ss' + tag])
    P.op('act', lambda e: e.activation(out=ss[:], in_=ss[:], func=AF.Sqrt, scale=1.0 / D, bias=G_EPS[0][:, 0:1]),
         reads=['ss' + tag], writes=['ss' + tag])
    P.op('dve', lambda e: e.reciprocal(out=rs[:], in_=ss[:]), reads=['ss' + tag], writes=['rs' + tag])


G_EPS = [None]


def norm_mod_transpose(P, G, xt, xk, hT, i, g_sl, sh_sl, W):
    mod_bc = W['mod']
    junk, ss, rs, t1, hb, pt = W['junk'], W['ss'], W['rs'], W['t1'], W['hb'], W['pt']
    rms_rstd(P, xt, xk, junk, ss, rs, '')
    P.op('dve', lambda e: e.scalar_tensor_tensor(out=t1[:], in0=xt[:], scalar=rs[:, 0:1], in1=mod_bc[:, g_sl],
                                                 op0=ALU.mult, op1=ALU.mult), reads=[xk, 'rs', 'modl'], writes=['t1'])
    P.op('pool', lambda e: e.tensor_tensor(out=hb[:], in0=t1[:], in1=mod_bc[:, sh_sl], op=ALU.add),
         reads=['t1', 'modl'], writes=['hb'])
    for k in range(8):
        P.op('pe', lambda e, k=k: e.transpose(out=pt[:, k, :], in_=hb[:, k * 128:(k + 1) * 128], identity=G['identb'][:]),
             reads=['hb'], writes=['pt'])
    P.op('act', lambda e: e.activation(out=hT[:, :, i * 128:(i + 1) * 128], in_=pt[:], func=AF.Copy),
         reads=['pt'], writes=[f'hT{i // 4}'])


def phase_BC(P, T, G):
    hT = P.sb([128, 8, S], BF16)
    W = dict(junk=P.sb([128, D], F32), ss=P.sb([128, 1], F32), rs=P.sb([128, 1], F32), t1=P.sb([128, D], F32),
             hb=P.sb([128, D], BF16), pt=P.ps([128, 8, 128], BF16))
    W['mod'] = P.sb([128, 2048], F32)
    P.dma('sp', lambda e: e.dma_start(out=W['mod'][:], in_=T['modd'][:, 0:2048]), writes=['modl'])
    xrot = Rot(P, 2, [128, D], F32, 'x')
    s0 = phase_D0(P, T, G)

    def s0step(n=1):
        for _ in range(n):
            try:
                next(s0)
            except StopIteration:
                return
    for i in range(NT):
        xt, xk = xrot.next()
        P.dma('sp', lambda e, xt=xt, i=i: e.dma_start(out=xt[:], in_=T['x'][i * 128:(i + 1) * 128, :]), writes=[xk])
        norm_mod_transpose(P, G, xt, xk, hT, i, slice(1024, 2048), slice(0, 1024), W)
        s0step()
    win = T['w_in'].rearrange("(k p) n -> p k n", p=128)
    segs = [('zq', 0, 512, BF16), ('zk', 512, 512, BF16), ('ziq', 1536, 512, BF16), ('zik', 2048, 32, BF16),
            ('zr', 2096, 1792, F32), ('zga', 3888, 1024, BF16), ('zgr', 4912, 1024, BF16)]
    wrot = Rot(P, 2, [128, 8, 128], BF16, 'w')
    psrot = Rot(P, 3, [128, 512], F32, 'ps', psum=True)
    strot = {BF16: Rot(P, 3, [128, 512], BF16, 'stb'), F32: Rot(P, 3, [128, 512], F32, 'stf')}
    ev = 0
    for (name, c0, n, dt) in segs:
        for m0 in range(0, n, 128):
            M = min(128, n - m0)
            wt, wk = wrot.next()
            P.dma('pool', lambda e, wt=wt, M=M, a=c0 + m0: e.dma_start(out=wt[:, :, :M], in_=win[:, :, a:a + M]), writes=[wk])
            for tg in range(8):
                ps, pk = psrot.next()
                for k in range(8):
                    mm(P, ps[:M, :], wt[:, k, :M], hT[:, k, tg * 512:(tg + 1) * 512], k == 0, k == 7, [wk, f'hT{tg}'], [pk])
                st, sk = strot[dt].next()
                if ev % 2 == 0:
                    P.op('act', lambda e, st=st, ps=ps, M=M: e.activation(out=st[:M, :], in_=ps[:M, :], func=AF.Copy),
                         reads=[pk], writes=[sk])
                else:
                    P.op('dve', lambda e, st=st, ps=ps, M=M: e.tensor_copy(out=st[:M, :], in_=ps[:M, :]),
                         reads=[pk], writes=[sk])
                ev += 1
                if ev % 2 == 0:
                    s0step()
                P.dma('sp', lambda e, st=st, M=M, name=name, m0=m0, tg=tg:
                      e.dma_start(out=T[name][m0:m0 + M, tg * 512:(tg + 1) * 512], in_=st[:M, :]), reads=[sk])
    wv = P.sb([128, 8, 528], BF16)
    P.dma('pool', lambda e: e.dma_start(out=wv[:, :, 0:512], in_=win[:, :, 1024:1536]), writes=['wv'])
    P.dma('pool', lambda e: e.dma_start(out=wv[:, :, 512:528], in_=win[:, :, 2080:2096]), writes=['wv2'])
    ps2rot = Rot(P, 2, [128, 16], F32, 'ps2', psum=True)
    st2rot = Rot(P, 2, [128, 16], F32, 'st2')
    for i in range(NT):
        ps, pk = psrot.next()
        ps2, pk2 = ps2rot.next()
        for k in range(8):
            mm(P, ps[:], hT[:, k, i * 128:(i + 1) * 128], wv[:, k, 0:512], k == 0, k == 7, ['wv', f'hT{i // 4}'], [pk])
        for k in range(8):
            mm(P, ps2[:], hT[:, k, i * 128:(i + 1) * 128], wv[:, k, 512:528], k == 0, k == 7, ['wv2', f'hT{i // 4}'], [pk2])
        st, sk = strot[BF16].next()
        st2, sk2 = st2rot.next()
        P.op('act', lambda e, st=st, ps=ps: e.activation(out=st[:], in_=ps[:], func=AF.Copy), reads=[pk], writes=[sk])
        P.op('dve', lambda e, st2=st2, ps2=ps2: e.tensor_copy(out=st2[:], in_=ps2[:]), reads=[pk2], writes=[sk2])
        P.dma('sp', lambda e, st=st, i=i: e.dma_start(out=T['zv'][i * 128:(i + 1) * 128, :], in_=st[:]), reads=[sk])
        P.dma('sp', lambda e, st2=st2, i=i: e.dma_start(out=T['ziw'][i * 128:(i + 1) * 128, :], in_=st2[:]), reads=[sk2])
    s0step(1000)


def t5_lo_bounds():
    n = np.arange(256)
    nf = np.maximum(n, 1).astype(np.float32)
    large = 16 + (np.log(nf / np.float32(16)) / np.float32(np.log(8.0)) * np.float32(16)).astype(np.int32)
    large = np.minimum(large, 31)
    bk = np.where(n < 16, n, large)
    return [int(np.min(np.nonzero(bk >= b)[0])) for b in range(1, 32)]


def phase_D0(P, T, G):
    relb = P.sb([128, 256], F32)
    diff = P.sb([128, 248], F32)
    base = P.sb([128, 8], F32)
    relidx = P.sb([128, 256], F32)
    P.dma('sp', lambda e: e.dma_start(out=relb[:], in_=T['rel_bias'].partition_broadcast(128)), writes=['relb'])
    P.dma('sp', lambda e: e.dma_start(out=relidx[:], in_=T['k_rel']), writes=['relidx'])
    P.op('dve', lambda e: e.tensor_tensor(out=diff[:], in0=relb[:, 8:256], in1=relb[:, 0:248], op=ALU.subtract),
         reads=['relb'], writes=['diff'])
    P.op('dve', lambda e: e.tensor_tensor(out=base[:], in0=relb[:, 0:8], in1=relb[:, 248:256], op=ALU.subtract),
         reads=['relb'], writes=['base'])
    P.op('dve', lambda e: e.tensor_copy(out=G['b31'][:], in_=relb[:, 248:256]), reads=['relb'], writes=['b31'])
    E = G['E']
    irot = Rot(P, 2, [128, 256], F32, 'ind')
    los = t5_lo_bounds()
    for b in range(1, 32):
        ind, ik = irot.next()
        P.op('dve', lambda e, ind=ind, lo=float(los[b - 1]): e.tensor_scalar(out=ind[:], in0=relidx[:], scalar1=lo, scalar2=None,
                                                                              op0=ALU.is_ge), reads=['relidx'], writes=[ik])
        for h in range(8):
            if b == 1:
                P.op('dve', lambda e, ind=ind, h=h: e.tensor_scalar(out=E[:, h, :], in0=ind[:], scalar1=diff[:, h:h + 1],
                                                                    scalar2=base[:, h:h + 1], op0=ALU.mult, op1=ALU.add),
                     reads=[ik, 'diff', 'base'], writes=[f'E{h}'])
            else:
                c = (b - 1) * 8 + h
                P.op('dve', lambda e, ind=ind, h=h, c=c: e.scalar_tensor_tensor(out=E[:, h, :], in0=ind[:], scalar=diff[:, c:c + 1],
                                                                               in1=E[:, h, :], op0=ALU.mult, op1=ALU.add),
                     reads=[ik, 'diff', f'E{h}'], writes=[f'E{h}'])
        yield
    P.op('act', lambda e: e.activation(out=E[:], in_=E[:], func=AF.Exp), reads=[f'E{h}' for h in range(8)],
         writes=[f'E{h}' for h in range(8)])


def phase_D(P, T, G):
    NIT = 12
    KT = P.sb([64, 8, S], BF16)
    V = P.sb([128, NT, 512], BF16)
    ik4 = P.sb([128, S], BF16)
    iw = P.sb([128, NT, 16], F32)
    ones64 = P.sb([128, 64], BF16)
    scs = [P.sb([128, S], F32), P.sb([128, S], F32)]
    maskbs = [P.sb([128, S], BF16), P.sb([128, S], BF16)]
    maskT = P.sb([128, NT, 128], BF16)
    lo, hi, mid, cnt, tmp, thr = [P.sb([128, 1], F32) for _ in range(6)]
    cvec = P.sb([128, NIT + 1], F32)
    dk = P.sb([128, NIT + 1], F32)
    for k in range(NIT + 1):
        P.op('pool', lambda e, k=k: e.memset(cvec[:, k:k + 1], 2.0 ** -(k + 1)), reads=['cvec'], writes=['cvec'])
    P.dma('sp', lambda e: e.dma_start(out=KT[:], in_=T['zk'].rearrange("(h p) t -> p h t", p=64)), writes=['KT'])
    P.dma('act', lambda e: e.dma_start(out=V[:], in_=T['zv'].rearrange("(i p) f -> p i f", p=128)), writes=['V'])
    for i in range(3):
        P.dma('sp', lambda e, i=i: e.dma_start(out=ik4[32 * i:32 * i + 32, :], in_=T['zik']), writes=[f'ik4{i}'])
    P.dma('sp', lambda e: e.dma_start(out=iw[:], in_=T['ziw'].rearrange("(i p) f -> p i f", p=128)), writes=['iw'])
    P.op('dve', lambda e: e.memset(ones64[:], 1.0), writes=['ones64'])
    zq = T['zq'].rearrange("(h p) t -> p h t", p=64)
    ziq = T['ziq'][0:480, :].rearrange("(j p) t -> p j t", p=96)
    attnT = T['attnT'].rearrange("(h p) t -> p h t", p=64)
    qrot = Rot(P, 2, [64, 8, 128], BF16, 'q')
    iqrot = Rot(P, 2, [96, 6, 128], BF16, 'iq')
    dgrot = Rot(P, 2, [128, 16, 128], BF16, 'dg')
    rrot = Rot(P, 3, [128, 512], BF16, 'r')
    psi = Rot(P, 2, [128, 512], F32, 'psi', psum=True)
    pacc = Rot(P, 1, [128, 512], F32, 'pacc', psum=True)
    pss = Rot(P, 3, [128, 4, 128], F32, 'pss', psum=True)
    ptm = Rot(P, 1, [128, 4, 128], BF16, 'ptm', psum=True)
    pod = Rot(P, 1, [64, 512], F32, 'pod', psum=True)
    pTrot = Rot(P, 3, [128, 4, 128], BF16, 'pT')
    atrot = Rot(P, 2, [64, 8, 128], BF16, 'at')
    rdrot = Rot(P, 2, [64, 128], F32, 'rd')
    E, b31 = G['E'], G['b31']
    identb = G['identb']

    def indexer(qi):
        sc, sck = scs[qi % 2], f'sc{qi % 2}'
        n = 128 * (qi + 1)
        tsl = slice(qi * 128, (qi + 1) * 128)
        iqt, iqk = iqrot.next()
        P.dma('act', lambda e: e.dma_start(out=iqt[:, 0:5, :], in_=ziq[:, :, tsl]), writes=[iqk])
        P.dma('act', lambda e: e.dma_start(out=iqt[0:32, 5, :], in_=T['ziq'][480:512, tsl]), writes=[iqk + 'b'])
        dg, dgk = dgrot.next()
        for h in range(16):
            P.op('pool', lambda e, h=h: e.tensor_scalar(out=dg[:, h, :], in0=identb[:], scalar1=iw[:, qi, h:h + 1], scalar2=0.0, op0=ALU.mult, op1=ALU.add),
                 reads=['iw'], writes=[dgk])
        for ch in range((n + 511) // 512):
            c0 = ch * 512
            nc_ = min(512, n - c0)
            pa, pak = pacc.next()
            pend = []

            def acc(h, r, rk):
                mm(P, pa[:, :nc_], dg[:, h, :], r[:, :nc_], h == 0, h == 15, [dgk, rk], [pak])
            for h in range(16):
                j, i = divmod(h, 3)
                ps, pk = psi.next()
                mm(P, ps[:, :nc_], iqt[32 * i:32 * i + 32, j, :], ik4[32 * i:32 * i + 32, c0:c0 + nc_], True, True,
                   [iqk, iqk + 'b', f'ik4{i}'], [pk])
                r, rk = rrot.next()
                P.op('act', lambda e, r=r, ps=ps, nc_=nc_: e.activation(out=r[:, :nc_], in_=ps[:, :nc_], func=AF.Relu), reads=[pk], writes=[rk])
                pend.append((h, r, rk))
                if len(pend) > 1:
                    acc(*pend.pop(0))
                yield
            while pend:
                acc(*pend.pop(0))
            P.op('dve', lambda e, pa=pa, c0=c0, nc_=nc_: e.tensor_copy(out=sc[:, c0:c0 + nc_], in_=pa[:, :nc_]), reads=[pak], writes=[sck])
        P.op('pool', lambda e: e.affine_select(out=sc[:, tsl], in_=sc[:, tsl], pattern=[[-1, 128]], compare_op=ALU.is_ge, fill=-1e30,
                                               base=0, channel_multiplier=1), reads=[sck], writes=[sck])

    def threshold(qi):
        sc, sck = scs[qi % 2], f'sc{qi % 2}'
        n = 128 * (qi + 1)
        maskb, mbk = maskbs[qi % 2], f'maskb{qi % 2}'
        dve = lambda fn, r, w: P.op('dve', fn, reads=r, writes=w)
        if n <= 256:
            dve(lambda e: e.memset(thr[:], -1e29), ['thr'], ['thr'])
        else:
            nv = 128 * qi
            dve(lambda e: e.tensor_reduce(out=lo[:], in_=sc[:, :nv], axis=AX.X, op=ALU.min), [sck, 'lo'], ['lo'])
            dve(lambda e: e.tensor_reduce(out=hi[:], in_=sc[:, :n], axis=AX.X, op=ALU.max), [sck, 'hi'], ['hi'])
            dve(lambda e: e.tensor_tensor(out=hi[:], in0=hi[:], in1=lo[:], op=ALU.subtract), ['hi', 'lo'], ['hi'])
            dve(lambda e: e.tensor_scalar(out=dk[:], in0=cvec[:], scalar1=hi[:, 0:1], scalar2=None, op0=ALU.mult), ['cvec', 'hi', 'dk'], ['dk'])
            dve(lambda e: e.tensor_tensor(out=mid[:], in0=lo[:], in1=dk[:, 0:1], op=ALU.add), ['lo', 'dk', 'mid'], ['mid'])
            for k in range(NIT):
                dve(lambda e: e.tensor_scalar(out=maskb[:, :n], in0=sc[:, :n], scalar1=mid[:, 0:1], scalar2=None,
                                              op0=ALU.is_ge, op1=ALU.add, accum_out=cnt[:]), [sck, 'mid', 'cnt', mbk], [mbk, 'cnt'])
                dve(lambda e: e.tensor_scalar(out=tmp[:], in0=cnt[:], scalar1=255.5, scalar2=-0.5, op0=ALU.is_ge, op1=ALU.add),
                    ['cnt', 'tmp'], ['tmp'])
                dve(lambda e, k=k: e.scalar_tensor_tensor(out=mid[:], in0=tmp[:], scalar=dk[:, k:k + 1], in1=mid[:], op0=ALU.mult, op1=ALU.add),
                    ['tmp', 'dk', 'mid'], ['mid'])
                yield
            dve(lambda e: e.tensor_tensor(out=thr[:], in0=mid[:], in1=dk[:, NIT:NIT + 1], op=ALU.subtract), ['mid', 'dk', 'thr'], ['thr'])
        dve(lambda e: e.tensor_scalar(out=maskb[:, :n], in0=sc[:, :n], scalar1=thr[:, 0:1], scalar2=None, op0=ALU.is_ge),
            [sck, 'thr', mbk], [mbk])
        yield

    def attention(qi):
        LOOK = 2
        nkt = qi + 1
        nch = (nkt + 3) // 4
        tsl = slice(qi * 128, (qi + 1) * 128)
        mb = maskbs[qi % 2]
        mbk = f'maskb{qi % 2}'
        qt, qk = qrot.next()
        P.dma('sp', lambda e: e.dma_start(out=qt[:], in_=zq[:, :, tsl]), writes=[qk])
        for c4 in range(nch):
            kts = list(range(4 * c4, min(4 * c4 + 4, nkt)))
            pm, pmk = ptm.next()
            for kt in kts:
                P.op('pe', lambda e, pm=pm, kt=kt: e.transpose(out=pm[:, kt % 4, :], in_=mb[:, kt * 128:(kt + 1) * 128],
                                                                identity=identb[:]), reads=[mbk], writes=[pmk])
            P.op('act', lambda e, pm=pm, kts=kts: e.activation(out=maskT[:, kts[0]:kts[-1] + 1, :], in_=pm[:, :len(kts), :],
                                                               func=AF.Copy), reads=[pmk], writes=['maskT'])
        at, atk = atrot.next()
        items = [(h, c4) for h in range(8) for c4 in range(nch)]
        qkd = {}
        hst = {}

        def emit_qk(h, c4):
            kts = list(range(4 * c4, min(4 * c4 + 4, nkt)))
            ps, pk = pss.next()
            for kt in kts:
                mm(P, ps[:, kt % 4, :], KT[:, h, kt * 128:(kt + 1) * 128], qt[:, h, :], True, True, ['KT', qk], [pk])
            qkd[(h, c4)] = (ps, pk)

        def emit_rest(h, c4):
            kts = list(range(4 * c4, min(4 * c4 + 4, nkt)))
            nk = len(kts)
            ps, pk = qkd.pop((h, c4))
            if c4 == 0:
                hst[h] = pod.next()
            po, pok = hst[h]
            pT, pTk = pTrot.next()
            P.op('act', lambda e: e.activation(out=pT[:, :nk, :], in_=ps[:, :nk, :], func=AF.Exp, scale=0.125, bias=b31[:, h:h + 1]),
                 reads=[pk], writes=[pTk])
            P.op('dve', lambda e: e.tensor_tensor(out=pT[:, :nk, :], in0=pT[:, :nk, :], in1=maskT[:, kts[0]:kts[-1] + 1, :], op=ALU.mult),
                 reads=[pTk, 'maskT'], writes=[pTk])
            for kt in kts:
                dl = qi - kt
                if dl <= 1:
                    P.op('dve', lambda e, kt=kt, dl=dl: e.tensor_tensor(
                        out=pT[:, kt % 4, :], in0=pT[:, kt % 4, :], in1=E[:, h, dl * 128:(dl + 1) * 128], op=ALU.mult),
                        reads=[pTk], writes=[pTk])
            for kt in kts:
                P.op('pe', lambda e, kt=kt: e.matmul(po[:, 0:128], lhsT=V[:, kt, h * 64:(h + 1) * 64], rhs=pT[:, kt % 4, :],
                                                     start=(kt == 0), stop=(kt == nkt - 1), skip_group_check=True),
                     reads=['V', pTk], writes=[pok])
                P.op('pe', lambda e, kt=kt: e.matmul(po[:, 128:256], lhsT=ones64[:], rhs=pT[:, kt % 4, :],
                                                     start=False, stop=(kt == nkt - 1), skip_group_check=True),
                     reads=['ones64', pTk], writes=[pok])
            if c4 == nch - 1:
                rd, rdk = rdrot.next()
                P.op('dve', lambda e: e.reciprocal(out=rd[:], in_=po[:, 128:256]), reads=[pok], writes=[rdk])
                P.op('dve', lambda e: e.tensor_tensor(out=at[:, h, :], in0=po[:, 0:128], in1=rd[:], op=ALU.mult),
                     reads=[pok, rdk], writes=[atk])

        for idx in range(len(items) + LOOK):
            if idx < len(items):
                emit_qk(*items[idx])
            if idx >= LOOK:
                emit_rest(*items[idx - LOOK])
                yield
        P.dma('sp', lambda e: e.dma_start(out=attnT[:, :, tsl], in_=at[:]), reads=[atk])

    def drain(g):
        for _ in g:
            pass

    def merge(gens):
        gens = [[g, max(1, n), 0.0, True] for g, n in gens]
        total = max(n for _, n, _, _ in gens)
        for step in range(total + 1):
            for it in gens:
                it[2] += it[1] / total
                while it[3] and it[2] >= 1.0:
                    it[2] -= 1.0
                    try:
                        next(it[0])
                    except StopIteration:
                        it[3] = False
        for it in gens:
            if it[3]:
                drain(it[0])

    s0 = phase_S0(P, T, G) if G.get('sparse') else iter(())

    def s0gen(n):
        for _ in range(n):
            try:
                next(s0)
            except StopIteration:
                return
            yield
    drain(indexer(0))
    drain(threshold(0))
    for qi in range(NT):
        ns0 = -(-390 * (qi + 1) // 528)
        if qi + 1 < NT:
            drain(indexer(qi + 1))
            merge([(attention(qi), 8 * ((qi + 4) // 4)), (threshold(qi + 1), NIT + 1), (s0gen(ns0), ns0)])
        else:
            merge([(attention(qi), 8 * ((qi + 4) // 4)), (s0gen(1000), 100)])
    drain(s0gen(1000))


def phase_E(P, T, G):
    LD = 0.6065306597126334
    ident = G['identb']
    zr = T['zr']
    def colload(name, n, key):
        t = P.sb([64, n], F32)
        P.dma('sp', lambda e: e.dma_start(out=t[:], in_=T[name].rearrange("(h p) -> p h", p=64), allow_slow_non_contiguous=True), writes=[key])
        return t
    mu_rkv = P.sb([64, 24], F32)
    P.dma('sp', lambda e: e.dma_start(out=mu_rkv[:], in_=T['tshift_mu'][0:1536].rearrange("(h p) -> p h", p=64), allow_slow_non_contiguous=True), writes=['mu'])
    mu_wa = P.sb([64, 2], F32)
    P.dma('sp', lambda e: e.dma_start(out=mu_wa[:], in_=T['tshift_mu'][1536:1664].rearrange("(h p) -> p h", p=64), allow_slow_non_contiguous=True), writes=['mu'])
    mu_g = P.sb([128, 1], F32)
    P.dma('sp', lambda e: e.dma_start(out=mu_g[:], in_=T['tshift_mu'][1664:1792].rearrange("(h p) -> p h", p=128), allow_slow_non_contiguous=True), writes=['mu'])
    om_rkv, om_wa, om_g = P.sb([64, 24], F32), P.sb([64, 2], F32), P.sb([128, 1], F32)
    for (o, m) in ((om_rkv, mu_rkv), (om_wa, mu_wa), (om_g, mu_g)):
        P.op('dve', lambda e, o=o, m=m: e.tensor_scalar(out=o[:], in0=m[:], scalar1=-1.0, scalar2=1.0, op0=ALU.mult, op1=ALU.add),
             reads=['mu'], writes=['om'])
    w0c = colload('decay_w0', 8, 'par'); a0c = colload('iclr_a0', 8, 'par'); kkc = colload('k_k', 8, 'par')
    kac = colload('k_a', 8, 'par'); rkc = colload('r_k', 8, 'par'); lgc = colload('lnx_g', 8, 'par'); lbc = colload('lnx_b', 8, 'par')
    omka = P.sb([64, 8], F32)
    P.op('dve', lambda e: e.tensor_scalar(out=omka[:], in0=kac[:], scalar1=-1.0, scalar2=1.0, op0=ALU.mult, op1=ALU.add),
         reads=['par'], writes=['omka'])
    dup, iup, gup = P.sb([64, 512], BF16), P.sb([64, 512], BF16), P.sb([128, 512], BF16)
    P.dma('pool', lambda e: e.dma_start(out=dup[:], in_=T['decay_up']), writes=['wts'])
    P.dma('pool', lambda e: e.dma_start(out=iup[:], in_=T['iclr_up']), writes=['wts'])
    P.dma('pool', lambda e: e.dma_start(out=gup[:], in_=T['gate_up']), writes=['wts'])
    onesf = P.sb([64, 64], F32)
    onesm = P.sb([64, 64], F32)
    gneps = P.sb([64, 1], F32)
    P.op('dve', lambda e: e.memset(onesf[:], 1.0), writes=['onesf'])
    P.op('dve', lambda e: e.memset(onesm[:], 1.0 / 64), writes=['onesm'])
    P.op('dve', lambda e: e.memset(gneps[:], 64e-5), writes=['gneps'])
    km = P.sb([128, 896], F32)
    P.dma('sp', lambda e: e.dma_start(out=km[:], in_=T['k_masks']), writes=['km'])
    mask4 = P.sb([128, 512], BF16)
    maskL = P.sb([128, 128], BF16)
    P.op('dve', lambda e: e.tensor_copy(out=mask4[:], in_=km[:, 0:512]), reads=['km'], writes=['mask4'])
    P.op('dve', lambda e: e.tensor_copy(out=maskL[:], in_=km[:, 512:640]), reads=['km'], writes=['maskL'])
    rst = P.sb([64, 512], F32)
    P.dma('sp', lambda e: e.dma_start(out=rst[:], in_=T['k_reset']), writes=['rst'])
    Tst = P.sb([64, 8, 64], BF16)
    P.op('dve', lambda e: e.memset(Tst[:], 0.0), writes=[f'T{h}' for h in range(8)])
    zrot = Rot(P, 1, [64, 3, 513], F32, 'z')
    wa_in = P.sb([64, 2, 513], F32)
    gd_in = P.sb([128, 513], F32)
    tmpr = Rot(P, 2, [128, 512], F32, 'tmp')
    twb, adb, sgb = P.sb([64, 512], BF16), P.sb([64, 512], BF16), P.sb([128, 512], BF16)
    AR = P.sb([64, 8, 4, 256], BF16)
    BK = P.sb([64, 8, 4, 256], BF16)
    tok3 = P.sb([128, 8, 4, 3, 64], BF16)
    pC = P.sb([64, 8, 4], F32)
    bon = P.sb([64, 8, 512], BF16)
    gg = P.sb([64, 8, 512], BF16)
    yT = P.sb([64, 8, 512], F32)
    RW = P.sb([64, 8, 512], BF16)
    hb = {n: P.sb([64, 512], F32) for n in ('sig', 'cs', 'ep', 'em', 'epv', 'kk', 'kkn', 'a', 't', 'kp', 'b', 'u1')}
    vb = P.sb([64, 512], BF16)
    Gms = [P.sb([128, 16, 512], BF16) for _ in range(2)]
    XY = [P.sb([128, 16, 256], BF16) for _ in range(2)]
    Nms = [P.sb([128, 16, 128], BF16) for _ in range(2)]
    Wsb, Usb = P.sb([128, 8, 64], BF16), P.sb([128, 8, 64], BF16)
    pg = Rot(P, 5, [128, 512], F32, 'pg', psum=True)
    pl = Rot(P, 2, [64, 512], F32, 'pl', psum=True)
    ptr = Rot(P, 1, [128, 3, 64], BF16, 'ptr', psum=True)
    rwT = T['rwT'].rearrange("(h p) t -> p h t", p=64)

    def dve(fn, reads, writes):
        P.op('dve', fn, reads=reads, writes=writes)

    for tg in range(8):
        t0 = tg * 512
        def load_halo(dst, rows, key, q, tg=tg, t0=t0):
            if tg == 0:
                src = rows(t0, t0 + 512)
                P.op('pool', lambda e: e.memset(dst[:, 0:1] if len(dst.shape) == 2 else dst[:, :, 0:1], 0.0), reads=[key], writes=[key])
                P.dma(q, lambda e: e.dma_start(out=(dst[:, 1:513] if len(dst.shape) == 2 else dst[:, :, 1:513]), in_=src), writes=[key + 'b'])
            else:
                src = rows(t0 - 1, t0 + 512)
                P.dma(q, lambda e: e.dma_start(out=dst[:], in_=src), reads=[key + 'b'], writes=[key])
        load_halo(wa_in, lambda a, b: zr[1536:1664, a:b].rearrange("(h p) t -> p h t", p=64), 'wa', 'sp')
        load_halo(gd_in, lambda a, b: zr[1664:1792, a:b], 'gd', 'act')

        def tshift(src_prev, src_cur, mu_ap, om_ap, np_, keys):
            tm, tk = tmpr.next()
            P.op('pool', lambda e: e.tensor_scalar(out=tm[:np_, :], in0=src_prev, scalar1=mu_ap, scalar2=0.0, op0=ALU.mult, op1=ALU.add),
                 reads=keys + ['mu'], writes=[tk])
            dve(lambda e: e.scalar_tensor_tensor(out=src_cur, in0=src_cur, scalar=om_ap, in1=tm[:np_, :], op0=ALU.mult, op1=ALU.add),
                keys + [tk, 'om'], keys)
        for i in range(2):
            tshift(wa_in[:, i, 0:512], wa_in[:, i, 1:513], mu_wa[:, i:i + 1], om_wa[:, i:i + 1], 64, ['wa', 'wab'])
        tshift(gd_in[:, 0:512], gd_in[:, 1:513], mu_g[:, 0:1], om_g[:, 0:1], 128, ['gd', 'gdb'])
        P.op('act', lambda e: e.activation(out=twb[:], in_=wa_in[:, 0, 1:513], func=AF.Tanh), reads=['wa', 'wab'], writes=['twb'])
        P.op('act', lambda e: e.activation(out=sgb[:], in_=gd_in[:, 1:513], func=AF.Sigmoid), reads=['gd', 'gdb'], writes=['sgb'])
        dve(lambda e: e.tensor_copy(out=adb[:], in_=wa_in[:, 1, 1:513]), ['wa', 'wab'], ['adb'])
        def prep_head(h, z, zk):
            def zrows(a, b, h=h):
                return zr[0:1536, a:b].rearrange("(s hh p) t -> hh p s t", s=3, p=64)[h]
            load_halo(z, zrows, zk, 'sp' if h % 2 == 0 else 'act')
            for s_ in range(3):
                tshift(z[:, s_, 0:512], z[:, s_, 1:513], mu_rkv[:, s_ * 8 + h:s_ * 8 + h + 1], om_rkv[:, s_ * 8 + h:s_ * 8 + h + 1], 64, [zk, zk + 'b'])
            r_, k_, v_ = z[:, 0, 1:513], z[:, 1, 1:513], z[:, 2, 1:513]
            zkeys = [zk, zk + 'b']
            sig, cs, ep, em, epv, kk, kkn, a_, t_, kp, b_, u1 = [hb[n] for n in ('sig', 'cs', 'ep', 'em', 'epv', 'kk', 'kkn', 'a', 't', 'kp', 'b', 'u1')]
            hs = slice(h * 64, (h + 1) * 64)
            p1, p1k = pl.next()
            mm(P, p1[:], dup[:, hs], twb[:], True, True, ['wts', 'twb'], [p1k])
            P.op('act', lambda e, p1=p1, h=h: e.activation(out=sig[:], in_=p1[:], func=AF.Sigmoid, bias=w0c[:, h:h + 1]),
                 reads=[p1k, 'par'], writes=['sig'])
            dve(lambda e: e.tensor_tensor_scan(out=cs[:], data0=rst[:], data1=sig[:], initial=0.0, op0=ALU.mult, op1=ALU.add),
                ['rst', 'sig'], ['cs'])
            P.op('act', lambda e: e.activation(out=ep[:], in_=cs[:], func=AF.Exp, scale=-LD), reads=['cs'], writes=['ep'])
            P.op('act', lambda e: e.activation(out=em[:], in_=cs[:], func=AF.Exp, scale=LD), reads=['cs'], writes=['em'])
            dve(lambda e: e.tensor_tensor(out=u1[:], in0=cs[:], in1=sig[:], op=ALU.subtract), ['cs', 'sig'], ['u1'])
            P.op('act', lambda e: e.activation(out=epv[:], in_=u1[:], func=AF.Exp, scale=-LD), reads=['u1'], writes=['epv'])
            dve(lambda e, h=h: e.tensor_copy(out=pC[:, h, :], in_=ep[:, 127:512:128]), ['ep'], ['pC'])
            p2, p2k = pl.next()
            mm(P, p2[:], iup[:, hs], adb[:], True, True, ['wts', 'adb'], [p2k])
            P.op('act', lambda e, p2=p2, h=h: e.activation(out=a_[:], in_=p2[:], func=AF.Sigmoid, bias=a0c[:, h:h + 1]),
                 reads=[p2k, 'par'], writes=['a'])
            p3, p3k = pl.next()
            mm(P, p3[:], gup[:, hs], sgb[:], True, True, ['wts', 'sgb'], [p3k])
            P.op('act', lambda e, p3=p3, h=h: e.activation(out=gg[:, h, :], in_=p3[:], func=AF.Copy), reads=[p3k], writes=[f'gg{h}'])
            dve(lambda e, h=h: e.tensor_scalar(out=kk[:], in0=k_, scalar1=kkc[:, h:h + 1], scalar2=None, op0=ALU.mult), zkeys + ['par'], ['kk'])
            P.op('act', lambda e: e.activation(out=u1[:], in_=kk[:], func=AF.Square), reads=['kk', 'u1'], writes=['u1'])
            p4, p4k = pl.next()
            mm(P, p4[:], onesf[:], u1[:], True, True, ['onesf', 'u1'], [p4k])
            P.op('act', lambda e, p4=p4: e.activation(out=kkn[:], in_=p4[:], func=AF.Sqrt), reads=[p4k], writes=['kkn'])
            dve(lambda e: e.tensor_scalar(out=kkn[:], in0=kkn[:], scalar1=1e-12, scalar2=None, op0=ALU.max), ['kkn'], ['kkn'])
            dve(lambda e: e.reciprocal(out=kkn[:], in_=kkn[:]), ['kkn'], ['kkn'])
            dve(lambda e: e.tensor_tensor(out=kkn[:], in0=kkn[:], in1=kk[:], op=ALU.mult), ['kkn', 'kk'], ['kkn'])
            dve(lambda e, h=h: e.tensor_scalar(out=t_[:], in0=a_[:], scalar1=kac[:, h:h + 1], scalar2=omka[:, h:h + 1], op0=ALU.mult, op1=ALU.add),
                ['a', 'par', 'omka'], ['t'])
            dve(lambda e: e.tensor_tensor(out=kp[:], in0=t_[:], in1=k_, op=ALU.mult), ['t'] + zkeys, ['kp'])
            dve(lambda e: e.tensor_tensor(out=b_[:], in0=kkn[:], in1=a_[:], op=ALU.mult), ['kkn', 'a'], ['b'])
            c4 = lambda ap: ap.rearrange("p (c t) -> p c t", c=4)
            dve(lambda e, h=h: e.tensor_tensor(out=AR[:, h, :, 128:256], in0=c4(r_), in1=c4(ep[:]), op=ALU.mult), zkeys + ['ep'], [f'AR{h}'])
            dve(lambda e, h=h: e.scalar_tensor_tensor(out=AR[:, h, :, 0:128], in0=c4(kkn[:]), scalar=-1.0, in1=c4(epv[:]), op0=ALU.mult, op1=ALU.mult),
                ['kkn', 'epv'], [f'AR{h}'])
            dve(lambda e, h=h: e.tensor_tensor(out=BK[:, h, :, 0:128], in0=c4(b_[:]), in1=c4(em[:]), op=ALU.mult), ['b', 'em'], [f'BK{h}'])
            dve(lambda e, h=h: e.tensor_tensor(out=BK[:, h, :, 128:256], in0=c4(kp[:]), in1=c4(em[:]), op=ALU.mult), ['kp', 'em'], [f'BK{h}'])
            dve(lambda e, h=h: e.scalar_tensor_tensor(out=u1[:], in0=r_, scalar=rkc[:, h:h + 1], in1=kp[:], op0=ALU.mult, op1=ALU.mult),
                zkeys + ['kp', 'par', 'u1'], ['u1'])
            p5, p5k = pl.next()
            mm(P, p5[:], onesf[:], u1[:], True, True, ['onesf', 'u1'], [p5k])
            dve(lambda e, p5=p5, h=h: e.tensor_tensor(out=bon[:, h, :], in0=p5[:], in1=v_, op=ALU.mult), [p5k] + zkeys, [f'bon{h}'])
            P.op('pool', lambda e: e.tensor_copy(out=vb[:], in_=v_), reads=zkeys, writes=['vb'])
            for c in range(4):
                pt_, ptk = ptr.next()
                cs_ = slice(c * 128, (c + 1) * 128)
                P.op('pe', lambda e, pt_=pt_, cs_=cs_: e.transpose(out=pt_[:, 0, :], in_=vb[:, cs_], identity=ident[0:64, 0:64]), reads=['vb'], writes=[ptk])
                P.op('pe', lambda e, pt_=pt_, h=h, c=c: e.transpose(out=pt_[:, 1, :], in_=BK[:, h, c, 0:128], identity=ident[0:64, 0:64]), reads=[f'BK{h}'], writes=[ptk])
                P.op('pe', lambda e, pt_=pt_, h=h, c=c: e.transpose(out=pt_[:, 2, :], in_=BK[:, h, c, 128:256], identity=ident[0:64, 0:64]), reads=[f'BK{h}'], writes=[ptk])
                P.op('act', lambda e, pt_=pt_, h=h, c=c: e.activation(out=tok3[:, h, c, :, :], in_=pt_[:], func=AF.Copy), reads=[ptk], writes=[f'tok{h}'])
        for h in range(8):
            z, zk = zrot.next()
            prep_head(h, z, zk)
        def stage1(cs, tg=tg):
            sl = (cs[0] // 2) % 2
            Gm, Nm = Gms[sl], Nms[sl]
            probs = [(ci, c, h) for ci, c in enumerate(cs) for h in range(8)]
            for (ci, c, h) in probs:
                q = ci * 8 + h
                p_, pk = pg.next()
                mm(P, p_[:, 0:256], BK[:, h, c, 0:128], AR[:, h, c, :], True, True, [f'BK{h}', f'AR{h}'], [pk])
                mm(P, p_[:, 256:512], BK[:, h, c, 128:256], AR[:, h, c, :], True, True, [f'BK{h}', f'AR{h}'], [pk])
                dve(lambda e, p_=p_, q=q: e.tensor_tensor(out=Gm[:, q, :], in0=p_[:], in1=mask4[:], op=ALU.mult), [pk, 'mask4'], [f'Gm{sl}_{q}'])
                p2_, p2k = pg.next()
                mm(P, p2_[:, 0:128], AR[:, h, c, 0:128], BK[:, h, c, 0:128], True, True, [f'BK{h}', f'AR{h}'], [p2k])
                dve(lambda e, p2_=p2_, q=q: e.tensor_tensor(out=XY[0][:, q, 128:256], in0=p2_[:, 0:128], in1=maskL[:], op=ALU.mult),
                    [p2k, 'maskL'], [f'XY0{q}'])
                P.op('pool', lambda e, q=q: e.tensor_copy(out=XY[0][:, q, 0:128], in_=Gm[:, q, 0:128]), reads=[f'Gm{sl}_{q}'], writes=[f'XY0{q}x'])
                P.op('pool', lambda e, q=q: e.tensor_tensor(out=Nm[:, q, :], in0=Gm[:, q, 0:128], in1=ident[:], op=ALU.add),
                     reads=[f'Gm{sl}_{q}'], writes=[f'N{sl}_{q}'])
            yield
            for j in range(6):
                cur, nxt = XY[j % 2], XY[(j + 1) % 2]
                ck, nk_ = f'XY{j % 2}', f'XY{(j + 1) % 2}'
                for q in range(len(probs)):
                    p_, pk = pg.next()
                    rk_ = [ck + f'{q}', ck + f'{q}x']
                    mm(P, p_[:, 0:128], cur[:, q, 128:256], cur[:, q, 0:128], True, True, rk_, [pk])
                    mm(P, p_[:, 128:256], cur[:, q, 0:128], cur[:, q, 128:256], True, True, rk_, [pk])
                    P.op('act', lambda e, p_=p_, nxt=nxt, q=q: e.activation(out=nxt[:, q, :], in_=p_[:, 0:256], func=AF.Copy),
                         reads=[pk], writes=[nk_ + f'{q}', nk_ + f'{q}x'])
                    if q % 4 == 3:
                        yield
                for q in range(len(probs)):
                    p_, pk = pg.next()
                    mm(P, p_[:, 0:128], nxt[:, q, 128:256], Nm[:, q, :], True, True, [nk_ + f'{q}', nk_ + f'{q}x', f'N{sl}_{q}'], [pk])
                    dve(lambda e, p_=p_, q=q: e.tensor_tensor(out=Nm[:, q, :], in0=p_[:, 0:128], in1=Nm[:, q, :], op=ALU.add),
                        [pk, f'N{sl}_{q}'], [f'N{sl}_{q}'])
                    if q % 4 == 3:
                        yield

        def stage2(c, tg=tg):
            sl = (c // 2) % 2
            Gm, Nm = Gms[sl], Nms[sl]
            ci = c % 2
            pw, pwk = pg.next()
            for h in range(8):
                q = ci * 8 + h
                mm(P, pw[:, h * 64:(h + 1) * 64], AR[:, h, c, 0:128], Tst[:, h, :], True, False, [f'AR{h}', f'T{h}'], [pwk])
                mm(P, pw[:, h * 64:(h + 1) * 64], Gm[:, q, 256:384], tok3[:, h, c, 0, :], False, True, [f'Gm{sl}_{q}', f'tok{h}'], [pwk])
            P.op('act', lambda e: e.activation(out=Wsb[:].rearrange("p h v -> p (h v)"), in_=pw[:], func=AF.Copy), reads=[pwk], writes=['W'])
            yield
            pu, puk = pg.next()
            for h in range(8):
                q = ci * 8 + h
                mm(P, pu[:, h * 64:(h + 1) * 64], Nm[:, q, :], Wsb[:, h, :], True, True, [f'N{sl}_{q}', 'W'], [puk])
            P.op('act', lambda e: e.activation(out=Usb[:].rearrange("p h v -> p (h v)"), in_=pu[:], func=AF.Copy), reads=[puk], writes=['U'])
            yield
            pt_, ptk = pg.next()
            for h in range(8):
                q = ci * 8 + h
                o_ = pt_[0:64, h * 64:(h + 1) * 64]
                mm(P, o_, ident[0:64, 0:64], Tst[:, h, :], True, False, [f'T{h}'], [ptk])
                mm(P, o_, tok3[:, h, c, 1, :], Usb[:, h, :], False, False, [f'tok{h}', 'U'], [ptk])
                mm(P, o_, tok3[:, h, c, 2, :], tok3[:, h, c, 0, :], False, True, [f'tok{h}'], [ptk])
            for half in range(2):
                py_, pyk = pg.next()
                for hh in range(4):
                    h = half * 4 + hh
                    q = ci * 8 + h
                    o_ = py_[0:64, hh * 128:(hh + 1) * 128]
                    mm(P, o_, Tst[:, h, :], AR[:, h, c, 128:256], True, False, [f'AR{h}', f'T{h}'], [pyk])
                    mm(P, o_, Usb[:, h, :], Gm[:, q, 128:256], False, False, ['U', f'Gm{sl}_{q}'], [pyk])
                    mm(P, o_, tok3[:, h, c, 0, :], Gm[:, q, 384:512], False, True, [f'tok{h}', f'Gm{sl}_{q}'], [pyk])
                P.op('act', lambda e, py_=py_, half=half: e.activation(
                    out=yT[:, half * 4:half * 4 + 4, c * 128:(c + 1) * 128], in_=py_[0:64, :].rearrange("p (h t) -> p h t", h=4), func=AF.Copy),
                    reads=[pyk], writes=[f'yT{half}'])
            for h in range(8):
                dve(lambda e, h=h: e.tensor_scalar(out=Tst[:, h, :], in0=pt_[0:64, h * 64:(h + 1) * 64], scalar1=pC[:, h, c:c + 1], scalar2=None, op0=ALU.mult),
                    [ptk, 'pC', f'T{h}'], [f'T{h}'])
            yield

        def chain(*gs):
            for g in gs:
                yield from g

        def rr(g1, g2):
            a = b = True
            while a or b:
                if a:
                    try:
                        next(g1)
                    except StopIteration:
                        a = False
                if b:
                    try:
                        next(g2)
                    except StopIteration:
                        b = False
        for _ in stage1([0, 1]):
            pass
        rr(stage1([2, 3]), chain(stage2(0), stage2(1)))
        for _ in chain(stage2(2), stage2(3)):
            pass
        for h in range(8):
            u1, u2 = hb['u1'], hb['t']
            p1, p1k = pl.next()
            mm(P, p1[:], onesm[:], yT[:, h, :], True, True, ['onesm', f'yT{h // 4}'], [p1k])
            dve(lambda e, p1=p1, h=h: e.tensor_tensor(out=u1[:], in0=yT[:, h, :], in1=p1[:], op=ALU.subtract), [p1k, f'yT{h // 4}', 'u1'], ['u1'])
            P.op('act', lambda e: e.activation(out=u2[:], in_=u1[:], func=AF.Square), reads=['u1', 't'], writes=['t'])
            p2, p2k = pl.next()
            mm(P, p2[:], onesm[:], u2[:], True, True, ['onesm', 't'], [p2k])
            P.op('act', lambda e, p2=p2: e.activation(out=u2[:], in_=p2[:], func=AF.Sqrt, bias=gneps[:, 0:1]), reads=[p2k, 'gneps', 't'], writes=['t'])
            dve(lambda e: e.reciprocal(out=u2[:], in_=u2[:]), ['t'], ['t'])
            dve(lambda e: e.tensor_tensor(out=u1[:], in0=u1[:], in1=u2[:], op=ALU.mult), ['u1', 't'], ['u1'])
            dve(lambda e, h=h: e.tensor_scalar(out=u1[:], in0=u1[:], scalar1=lgc[:, h:h + 1], scalar2=lbc[:, h:h + 1], op0=ALU.mult, op1=ALU.add),
                ['u1', 'par'], ['u1'])
            dve(lambda e, h=h: e.tensor_tensor(out=u1[:], in0=u1[:], in1=bon[:, h, :], op=ALU.add), ['u1', f'bon{h}'], ['u1'])
            dve(lambda e, h=h: e.tensor_tensor(out=RW[:, h, :], in0=u1[:], in1=gg[:, h, :], op=ALU.mult), ['u1', f'gg{h}'], ['RW'])
        P.dma('sp', lambda e, t0=t0: e.dma_start(out=rwT[:, :, t0:t0 + 512], in_=RW[:]), reads=['RW'])
        if tg == 0 and 'dbg_y' in T:
            P.dma('sp', lambda e: e.dma_start(out=T['dbg_y'], in_=yT[:]), reads=['yT0', 'yT1'])
            P.dma('sp', lambda e: e.dma_start(out=T['dbg_bon'], in_=bon[:]), reads=[f'bon{h}' for h in range(8)])
            P.dma('sp', lambda e: e.dma_start(out=T['dbg_g'], in_=gg[:]), reads=[f'gg{h}' for h in range(8)])
            P.dma('sp', lambda e: e.dma_start(out=T['dbg_AR'], in_=AR[:]), reads=[f'AR{h}' for h in range(8)])
            P.dma('sp', lambda e: e.dma_start(out=T['dbg_BK'], in_=BK[:]), reads=[f'BK{h}' for h in range(8)])


def phase_F(P, T, G):
    wa, wr, wo = P.sb([128, 4, D], BF16), P.sb([128, 4, D], BF16), P.sb([128, 8, D], BF16)
    P.dma('pool', lambda e: e.dma_start(out=wa[:], in_=T['w_attn_br'].rearrange("(j p) d -> p j d", p=128)), writes=['wa'])
    P.dma('pool', lambda e: e.dma_start(out=wr[:], in_=T['w_rwkv_br'].rearrange("(j p) d -> p j d", p=128)), writes=['wr'])
    P.dma('pool', lambda e: e.dma_start(out=wo[:], in_=T['w_out'].rearrange("(j p) d -> p j d", p=128)), writes=['wo'])
    W = dict(junk=P.sb([128, D], F32), ss=P.sb([128, 1], F32), rs=P.sb([128, 1], F32), t1=P.sb([128, D], F32),
             hb=P.sb([128, D], BF16), pt=P.ps([128, 8, 128], BF16))
    W['mod'] = P.sb([128, 3072], F32)
    P.dma('sp', lambda e: e.dma_start(out=W['mod'][:], in_=T['modd'][:, 2048:5120]), writes=['modl'])
    xrot = Rot(P, 2, [128, D], F32, 'x')
    atr, rtr = Rot(P, 2, [128, 4, 128], BF16, 'at'), Rot(P, 2, [128, 4, 128], BF16, 'rt')
    gar, grr = Rot(P, 2, [128, 8, 128], BF16, 'ga'), Rot(P, 2, [128, 8, 128], BF16, 'gr')
    sga, sgr = P.sb([128, 8, 128], F32), P.sb([128, 8, 128], F32)
    m1, m2 = P.sb([128, 4, 128], F32), P.sb([128, 4, 128], F32)
    mixT = P.sb([128, 8, 128], BF16)
    x1t = P.sb([128, D], F32)
    h2t = P.sb([128, 8, 128], BF16)
    pA = Rot(P, 1, [128, 4, 128], F32, 'pA', psum=True)
    pR = Rot(P, 1, [128, 4, 128], F32, 'pR', psum=True)
    po = Rot(P, 2, [128, 512], F32, 'po', psum=True)
    aT = T['attnT'].rearrange("(j p) t -> p j t", p=128)
    rT = T['rwT'].rearrange("(j p) t -> p j t", p=128)
    gaT = T['zga'].rearrange("(j p) t -> p j t", p=128)
    grT = T['zgr'].rearrange("(j p) t -> p j t", p=128)
    h2T = T['h2T'].rearrange("(k p) t -> p k t", p=128)
    R = router_setup(P, T, G) if G.get('sparse') else None
    zt = P.sb([128, D], BF16)
    P.op('pool', lambda e: e.memset(zt[:], 0.0), writes=['zt'])
    for i in range(NT):
        tsl = slice(i * 128, (i + 1) * 128)
        if R is not None:
            for bz in range(i * 10, i * 10 + 10):
                P.dma('act' if bz % 2 else 'sp', lambda e, bz=bz: e.dma_start(out=T['Xs'][bz * 128:(bz + 1) * 128, :], in_=zt[:]), reads=['zt'])
        xt, xk = xrot.next()
        at, atk = atr.next(); rt, rtk = rtr.next(); ga, gak = gar.next(); gr, grk = grr.next()
        P.dma('sp', lambda e, xt=xt, tsl=tsl: e.dma_start(out=xt[:], in_=T['x'][tsl, :]), writes=[xk])
        P.dma('act', lambda e, at=at, tsl=tsl: e.dma_start(out=at[:], in_=aT[:, :, tsl]), writes=[atk])
        P.dma('act', lambda e, rt=rt, tsl=tsl: e.dma_start(out=rt[:], in_=rT[:, :, tsl]), writes=[rtk])
        P.dma('sp', lambda e, ga=ga, tsl=tsl: e.dma_start(out=ga[:], in_=gaT[:, :, tsl]), writes=[gak])
        P.dma('sp', lambda e, gr=gr, tsl=tsl: e.dma_start(out=gr[:], in_=grT[:, :, tsl]), writes=[grk])
        P.op('act', lambda e, ga=ga: e.activation(out=sga[:], in_=ga[:], func=AF.Sigmoid), reads=[gak], writes=['sga'])
        P.op('act', lambda e, gr=gr: e.activation(out=sgr[:], in_=gr[:], func=AF.Sigmoid), reads=[grk], writes=['sgr'])
        for half in range(2):
            pa, pak = pA.next(); pr, prk = pR.next()
            for s_ in range(4):
                dt = half * 4 + s_
                for j in range(4):
                    mm(P, pa[:, s_, :], wa[:, j, dt * 128:(dt + 1) * 128], at[:, j, :], j == 0, j == 3, ['wa', atk], [pak])
            for s_ in range(4):
                dt = half * 4 + s_
                for j in range(4):
                    mm(P, pr[:, s_, :], wr[:, j, dt * 128:(dt + 1) * 128], rt[:, j, :], j == 0, j == 3, ['wr', rtk], [prk])
            hs = slice(half * 4, half * 4 + 4)
            P.op('dve', lambda e, pa=pa, hs=hs: e.tensor_tensor(out=m1[:], in0=pa[:], in1=sga[:, hs, :], op=ALU.mult), reads=[pak, 'sga'], writes=['m1'])
            P.op('dve', lambda e, pr=pr, hs=hs: e.tensor_tensor(out=m2[:], in0=pr[:], in1=sgr[:, hs, :], op=ALU.mult), reads=[prk, 'sgr'], writes=['m2'])
            P.op('pool', lambda e, hs=hs: e.tensor_tensor(out=mixT[:, hs, :], in0=m1[:], in1=m2[:], op=ALU.add), reads=['m1', 'm2'], writes=['mixT'])
        for half in range(2):
            p_, pk = po.next()
            cs_ = slice(half * 512, (half + 1) * 512)
            for dt in range(8):
                mm(P, p_[:], mixT[:, dt, :], wo[:, dt, cs_], dt == 0, dt == 7, ['mixT', 'wo'], [pk])
            P.op('dve', lambda e, p_=p_, cs_=cs_: e.tensor_tensor(out=x1t[:, cs_], in0=p_[:], in1=W['mod'][:, cs_], op=ALU.mult),
                 reads=[pk, 'modl'], writes=['x1t'])
        P.op('pool', lambda e, xt=xt: e.tensor_tensor(out=x1t[:], in0=x1t[:], in1=xt[:], op=ALU.add), reads=['x1t', xk], writes=['x1t'])
        P.dma('sp', lambda e, tsl=tsl: e.dma_start(out=T['x1'][tsl, :], in_=x1t[:]), reads=['x1t'])
        norm_mod_transpose(P, G, x1t, 'x1t', h2t, 0, slice(2048, 3072), slice(1024, 2048), W)
        P.dma('act', lambda e, tsl=tsl: e.dma_start(out=h2T[:, :, tsl], in_=h2t[:]), reads=['hT0'])
        if R is not None:
            P.dma('act', lambda e, tsl=tsl: e.dma_start(out=T['h2tok'][tsl, :], in_=W['hb'][:]), reads=['hb'])
            router_tile(P, T, R, h2t, 'hT0', i)
    if R is not None:
        P.dma('sp', lambda e: e.dma_start(out=T['cntd'], in_=R['cnt'][:]), reads=['cnt'])


def phase_G0(P, T, G):
    for e in range(64):
        for (src, dst) in (('exp_gate', 'wg16'), ('exp_up', 'wu16'), ('exp_down', 'wd16')):
            P.dma('pool', lambda e_, e=e, src=src, dst=dst: e_.dma_start(
                out=T[dst][e].rearrange("k p f -> (k p f)").rearrange("(a b) -> a b", b=2048), in_=T[src][e].rearrange("r c -> (r c)").rearrange("(a b) -> a b", b=2048)))
    for (src, dst) in (('sh_gate', 'wg16'), ('sh_up', 'wu16'), ('sh_down', 'wd16')):
        P.dma('pool', lambda e_, src=src, dst=dst: e_.dma_start(
            out=T[dst][64].rearrange("k p f -> (k p f)").rearrange("(a b) -> a b", b=2048), in_=T[src].rearrange("r c -> (r c)").rearrange("(a b) -> a b", b=2048)))


def phase_G(P, T, G):
    ident = G['identb']
    h2T = T['h2T'].rearrange("(k p) t -> p k t", p=128)
    yacc = G['yacc']
    gwT = P.sb([64, S], BF16)
    rwt = P.sb([128, 8, 64], BF16)
    rbias = P.sb([128, 64], F32)
    P.dma('pool', lambda e: e.dma_start(out=rwt[:], in_=T['router_w'].rearrange("(k p) n -> p k n", p=128)), writes=['rwt'])
    P.dma('sp', lambda e: e.dma_start(out=rbias[:], in_=T['router_bias'].partition_broadcast(128)), writes=['rbias'])
    ones128 = P.sb([64, 128], BF16)
    P.op('dve', lambda e: e.memset(ones128[:], 1.0), writes=['ones128'])
    hrot = Rot(P, 2, [128, 8, 256], BF16, 'h2g')
    pmisc = P.ps([128, 512], F32)
    ptb = P.ps([64, 128], BF16)
    emb = P.sb([128, 64], BF16)
    sc_, ch, tmp, cm, em = [P.sb([128, 64], F32) for _ in range(5)]
    m1, m2, grp, s8, gmask, pen, den = [P.sb([128, 8], F32) for _ in range(7)]
    dve = lambda fn, r, w: P.op('dve', fn, reads=r, writes=w)
    for tgp in range(16):
        hg, hk = hrot.next()
        P.dma('sp', lambda e, hg=hg, tgp=tgp: e.dma_start(out=hg[:], in_=h2T[:, :, tgp * 256:(tgp + 1) * 256]), writes=[hk])
        for tt in range(2):
            i = tgp * 2 + tt
            p_, pk = pmisc[:, 0:64], 'pm_a'
            for k in range(8):
                mm(P, p_, hg[:, k, tt * 128:(tt + 1) * 128], rwt[:, k, :], k == 0, k == 7, [hk, 'rwt'], [pk])
            P.op('act', lambda e, p_=p_: e.activation(out=sc_[:], in_=p_, func=AF.Sigmoid), reads=[pk], writes=['sc'])
            dve(lambda e: e.tensor_tensor(out=ch[:], in0=sc_[:], in1=rbias[:], op=ALU.add), ['sc', 'rbias'], ['ch'])
            ch3 = ch[:].rearrange("p (g e) -> p g e", g=8)
            dve(lambda e, ch3=ch3: e.tensor_reduce(out=m1[:], in_=ch3, axis=AX.X, op=ALU.max), ['ch'], ['m1'])
            for g in range(8):
                dve(lambda e, g=g: e.tensor_scalar(out=tmp[:, g * 8:(g + 1) * 8], in0=ch[:, g * 8:(g + 1) * 8], scalar1=m1[:, g:g + 1],
                                                  scalar2=-1e9, op0=ALU.is_equal, op1=ALU.mult), ['ch', 'm1', 'tmp'], ['tmp'])
            dve(lambda e: e.tensor_tensor(out=tmp[:], in0=tmp[:], in1=ch[:], op=ALU.add), ['tmp', 'ch'], ['tmp'])
            dve(lambda e: e.tensor_reduce(out=m2[:], in_=tmp[:].rearrange("p (g e) -> p g e", g=8), axis=AX.X, op=ALU.max), ['tmp'], ['m2'])
            dve(lambda e: e.tensor_tensor(out=grp[:], in0=m1[:], in1=m2[:], op=ALU.add), ['m1', 'm2'], ['grp'])
            dve(lambda e: e.max(out=s8[:], in_=grp[:]), ['grp'], ['s8'])
            dve(lambda e: e.tensor_scalar(out=gmask[:], in0=grp[:], scalar1=s8[:, 3:4], scalar2=None, op0=ALU.is_ge), ['grp', 's8'], ['gmask'])
            dve(lambda e: e.tensor_scalar(out=pen[:], in0=gmask[:], scalar1=-1.0, scalar2=1e9, op0=ALU.add, op1=ALU.mult), ['gmask'], ['pen'])
            for g in range(8):
                dve(lambda e, g=g: e.tensor_scalar(out=cm[:, g * 8:(g + 1) * 8], in0=ch[:, g * 8:(g + 1) * 8], scalar1=pen[:, g:g + 1],
                                                  scalar2=None, op0=ALU.add), ['ch', 'pen', 'cm'], ['cm'])
            dve(lambda e: e.max(out=s8[:], in_=cm[:]), ['cm', 's8'], ['s8'])
            dve(lambda e: e.tensor_scalar(out=em[:], in0=cm[:], scalar1=s8[:, 7:8], scalar2=None, op0=ALU.is_ge), ['cm', 's8'], ['em'])
            dve(lambda e: e.tensor_tensor(out=em[:], in0=em[:], in1=sc_[:], op=ALU.mult), ['em', 'sc'], ['em'])
            dve(lambda e: e.tensor_reduce(out=den[:, 0:1], in_=em[:], axis=AX.X, op=ALU.add), ['em'], ['den'])
            dve(lambda e: e.reciprocal(out=den[:, 1:2], in_=den[:, 0:1]), ['den'], ['den'])
            dve(lambda e: e.tensor_scalar(out=em[:], in0=em[:], scalar1=den[:, 1:2], scalar2=2.5, op0=ALU.mult, op1=ALU.mult), ['em', 'den'], ['em'])
            pt_, ptk = ptb[:], 'pm_b'
            dve(lambda e: e.tensor_copy(out=emb[:], in_=em[:]), ['em', 'emb'], ['emb'])
            P.op('pe', lambda e, pt_=pt_: e.transpose(out=pt_, in_=emb[:], identity=G['identb'][:]), reads=['emb'], writes=[ptk])
            P.op('act', lambda e, pt_=pt_, i=i: e.activation(out=gwT[:, i * 128:(i + 1) * 128], in_=pt_, func=AF.Copy), reads=[ptk], writes=['gwT'])
    if G.get('gstop') == 'router':
        return
    wgr = Rot(P, 2, [128, 8, 256], BF16, 'wg')
    wur = Rot(P, 2, [128, 8, 256], BF16, 'wu')
    wdr = Rot(P, 2, [128, 2, D], BF16, 'wd')
    selr = Rot(P, 2, [64, 128], BF16, 'sel')
    pgu = Rot(P, 2, [128, 4, 256], F32, 'pgu', psum=True)
    py = Rot(P, 2, [128, 512], F32, 'py', psum=True)
    sgr_ = Rot(P, 2, [128, 2, 256], F32, 'sg')
    tr_ = Rot(P, 2, [128, 2, 256], F32, 'tt')
    actr = Rot(P, 2, [128, 2, 256], BF16, 'act')
    for e_ in range(G.get('nexp', 65)):
        wg, wgk = wgr.next(); wu, wuk = wur.next(); wd, wdk = wdr.next()
        if e_ < 64:
            sg_, su_, sd_ = T['exp_gate'][e_], T['exp_up'][e_], T['exp_down'][e_]
        else:
            sg_, su_, sd_ = T['sh_gate'], T['sh_up'], T['sh_down']
        P.dma('pool', lambda e, wg=wg, sg_=sg_: e.dma_start(out=wg[:], in_=sg_.rearrange("(k p) f -> p k f", p=128)), writes=[wgk])
        P.dma('pool', lambda e, wu=wu, su_=su_: e.dma_start(out=wu[:], in_=su_.rearrange("(k p) f -> p k f", p=128)), writes=[wuk])
        P.dma('pool', lambda e, wd=wd, sd_=sd_: e.dma_start(out=wd[:], in_=sd_.rearrange("(k p) f -> p k f", p=128)), writes=[wdk])
        if e_ < 64:
            sel, selk = selr.next()
            P.op('pool', lambda e, sel=sel, e_=e_: e.tensor_scalar(out=sel[:], in0=ones128[:], scalar1=G['identf'][0:64, e_:e_ + 1], scalar2=0.0, op0=ALU.mult, op1=ALU.add),
                 reads=['ones128'], writes=[selk])
        for tgp in range(16):
            hg, hk = hrot.next()
            P.dma('sp' if tgp % 2 == 0 else 'act', lambda e, hg=hg, tgp=tgp: e.dma_start(out=hg[:], in_=h2T[:, :, tgp * 256:(tgp + 1) * 256]), writes=[hk])
            p_, pk = pgu.next()
            for s_, (w_, wk_) in enumerate(((wg, wgk), (wg, wgk), (wu, wuk), (wu, wuk))):
                ft = s_ % 2
                for k in range(8):
                    mm(P, p_[:, s_, :], w_[:, k, ft * 128:(ft + 1) * 128], hg[:, k, :], k == 0, k == 7, [wk_, hk], [pk])
            sg, sgk = sgr_.next(); t_, tk = tr_.next(); ac, ack = actr.next()
            P.op('act', lambda e, sg=sg, p_=p_: e.activation(out=sg[:], in_=p_[:, 0:2, :], func=AF.Silu), reads=[pk], writes=[sgk])
            P.op('dve', lambda e, sg=sg, p_=p_, t_=t_: e.tensor_tensor(out=t_[:], in0=p_[:, 2:4, :], in1=sg[:], op=ALU.mult), reads=[pk, sgk], writes=[tk])
            if e_ < 64:
                pw, pwk = pmisc[:, 256:512], 'pm_c'
                mm(P, pw, sel[:], gwT[:, tgp * 256:(tgp + 1) * 256], True, True, [selk, 'gwT'], [pwk])
                for ft in range(2):
                    P.op('dve', lambda e, ac=ac, t_=t_, pw=pw, ft=ft: e.tensor_tensor(out=ac[:, ft, :], in0=pw, in1=t_[:, ft, :], op=ALU.mult),
                         reads=[tk, pwk], writes=[ack])
            else:
                P.op('pool', lambda e, ac=ac, t_=t_: e.tensor_copy(out=ac[:], in_=t_[:]), reads=[tk], writes=[ack])
            for tt in range(2):
                i = tgp * 2 + tt
                for half in range(2):
                    q_, qk = py.next()
                    cs_ = slice(half * 512, (half + 1) * 512)
                    for ft in range(2):
                        mm(P, q_[:], ac[:, ft, tt * 128:(tt + 1) * 128], wd[:, ft, cs_], ft == 0, ft == 1, [ack, wdk], [qk])
                    if e_ == 0:
                        P.op('act', lambda e, q_=q_, i=i, cs_=cs_: e.activation(out=yacc[:, i, cs_], in_=q_[:], func=AF.Copy), reads=[qk], writes=[f'y{i}'])
                    else:
                        P.op('dve', lambda e, q_=q_, i=i, cs_=cs_: e.tensor_tensor(out=yacc[:, i, cs_], in0=q_[:], in1=yacc[:, i, cs_], op=ALU.add),
                             reads=[qk, f'y{i}'], writes=[f'y{i}'])


def phase_H(P, T, G):
    yacc = G['yacc']
    dve = lambda fn, r, w: P.op('dve', fn, reads=r, writes=w)
    g2b = P.sb([128, D], F32)
    fing = P.sb([128, D], F32)
    P.dma('sp', lambda e: e.dma_start(out=g2b[:], in_=T['modd'][:, 5120:6144]), writes=['g2b'])
    P.dma('sp', lambda e: e.dma_start(out=fing[:], in_=T['final_g'].partition_broadcast(128)), writes=['fing'])
    xr = Rot(P, 2, [128, D], F32, 'x1')
    junk, ss, rs = P.sb([128, D], F32), P.sb([128, 1], F32), P.sb([128, 1], F32)
    for i in range(NT):
        tsl = slice(i * 128, (i + 1) * 128)
        xt, xk = xr.next()
        P.dma('sp', lambda e, xt=xt, tsl=tsl: e.dma_start(out=xt[:], in_=T['x1'][tsl, :]), writes=[xk])
        dve(lambda e, i=i: e.tensor_tensor(out=yacc[:, i, :], in0=yacc[:, i, :], in1=g2b[:], op=ALU.mult), [f'y{i}', 'g2b'], [f'y{i}'])
        P.op('pool', lambda e, i=i, xt=xt: e.tensor_tensor(out=xt[:], in0=xt[:], in1=yacc[:, i, :], op=ALU.add), reads=[f'y{i}', xk], writes=[xk])
        rms_rstd(P, xt, xk, junk, ss, rs, 'f')
        dve(lambda e, xt=xt: e.scalar_tensor_tensor(out=xt[:], in0=xt[:], scalar=rs[:, 0:1], in1=fing[:], op0=ALU.mult, op1=ALU.mult),
            [xk, 'rsf', 'fing'], [xk])
        P.dma('sp', lambda e, xt=xt, tsl=tsl: e.dma_start(out=T['out'][tsl, :], in_=xt[:]), reads=[xk])


NBLK = 320


def router_setup(P, T, G):
    R = {}
    R['rwt'] = P.sb([128, 8, 64], BF16)
    R['rbias'] = P.sb([128, 64], F32)
    R['iota'] = P.sb([128, 64], F32)
    R['su'] = P.sb([128, 128], BF16)
    R['onesb'] = P.sb([128, 128], BF16)
    R['cnt'] = P.sb([128, 64], F32)
    R['kmf'] = P.sb([128, 128], F32)
    P.dma('pool', lambda e: e.dma_start(out=R['rwt'][:], in_=T['router_w'].rearrange("(k p) n -> p k n", p=128)), writes=['rwt'])
    P.dma('sp', lambda e: e.dma_start(out=R['rbias'][:], in_=T['router_bias'].partition_broadcast(128)), writes=['rbias'])
    P.dma('sp', lambda e: e.dma_start(out=R['iota'][:], in_=T['k_rel'][0:1, 0:64].rearrange("o f -> (o f)").partition_broadcast(128)), writes=['iota'])
    P.dma('sp', lambda e: e.dma_start(out=R['kmf'][:], in_=T['k_masks'][:, 0:128]), writes=['kmf'])
    P.op('dve', lambda e: e.tensor_copy(out=R['su'][:], in_=R['kmf'][:]), reads=['kmf'], writes=['su'])
    P.op('dve', lambda e: e.memset(R['onesb'][:], 1.0), writes=['onesb'])
    P.op('dve', lambda e: e.memset(R['cnt'][:], 0.0), writes=['cnt'])
    R['pm'] = P.ps([128, 512], F32)
    for n in ('sc', 'ch', 'tmp', 'cm', 'em', 'mk', 'oh', 'rk', 'jk'):
        R[n] = P.sb([128, 64], F32)
    R['mkb'] = P.sb([128, 64], BF16)
    for n in ('m1', 'm2', 'grp', 's8', 'gmask', 'pen', 'den', 'i8f'):
        R[n] = P.sb([128, 8], F32)
    R['i8u'] = P.sb([128, 8], U32)
    R['meta'] = P.sb([128, 24], F32)
    return R


def router_tile(P, T, R, h2t, h2k, i):
    dve = lambda fn, r, w: P.op('dve', fn, reads=r, writes=w)
    pm = R['pm']
    sc_, ch, tmp, cm, em, mk, oh, rk, jk, mkb = [R[n] for n in ('sc', 'ch', 'tmp', 'cm', 'em', 'mk', 'oh', 'rk', 'jk', 'mkb')]
    m1, m2, grp, s8, gmask, pen, den, i8f, i8u, meta = [R[n] for n in ('m1', 'm2', 'grp', 's8', 'gmask', 'pen', 'den', 'i8f', 'i8u', 'meta')]
    for k in range(8):
        mm(P, pm[:, 0:64], h2t[:, k, :], R['rwt'][:, k, :], k == 0, k == 7, [h2k, 'rwt'], ['pm_a'])
    P.op('act', lambda e: e.activation(out=sc_[:], in_=pm[:, 0:64], func=AF.Sigmoid), reads=['pm_a'], writes=['sc'])
    dve(lambda e: e.tensor_tensor(out=ch[:], in0=sc_[:], in1=R['rbias'][:], op=ALU.add), ['sc', 'rbias'], ['ch'])
    dve(lambda e: e.tensor_reduce(out=m1[:], in_=ch[:].rearrange("p (g e) -> p g e", g=8), axis=AX.X, op=ALU.max), ['ch'], ['m1'])
    for g in range(8):
        dve(lambda e, g=g: e.tensor_scalar(out=tmp[:, g * 8:(g + 1) * 8], in0=ch[:, g * 8:(g + 1) * 8], scalar1=m1[:, g:g + 1],
                                          scalar2=-1e9, op0=ALU.is_equal, op1=ALU.mult), ['ch', 'm1', 'tmp'], ['tmp'])
    dve(lambda e: e.tensor_tensor(out=tmp[:], in0=tmp[:], in1=ch[:], op=ALU.add), ['tmp', 'ch'], ['tmp'])
    dve(lambda e: e.tensor_reduce(out=m2[:], in_=tmp[:].rearrange("p (g e) -> p g e", g=8), axis=AX.X, op=ALU.max), ['tmp'], ['m2'])
    dve(lambda e: e.tensor_tensor(out=grp[:], in0=m1[:], in1=m2[:], op=ALU.add), ['m1', 'm2'], ['grp'])
    dve(lambda e: e.max(out=s8[:], in_=grp[:]), ['grp'], ['s8'])
    dve(lambda e: e.tensor_scalar(out=gmask[:], in0=grp[:], scalar1=s8[:, 3:4], scalar2=None, op0=ALU.is_ge), ['grp', 's8'], ['gmask'])
    dve(lambda e: e.tensor_scalar(out=pen[:], in0=gmask[:], scalar1=-1.0, scalar2=1e9, op0=ALU.add, op1=ALU.mult), ['gmask'], ['pen'])
    for g in range(8):
        dve(lambda e, g=g: e.tensor_scalar(out=cm[:, g * 8:(g + 1) * 8], in0=ch[:, g * 8:(g + 1) * 8], scalar1=pen[:, g:g + 1],
                                          scalar2=None, op0=ALU.add), ['ch', 'pen', 'cm'], ['cm'])
    dve(lambda e: e.max(out=s8[:], in_=cm[:]), ['cm', 's8'], ['s8'])
    dve(lambda e: e.max_index(out=i8u[:], in_max=s8[:], in_values=cm[:]), ['cm', 's8', 'i8u'], ['i8u'])
    dve(lambda e: e.tensor_copy(out=meta[:, 0:8], in_=i8u[:]), ['i8u', 'meta'], ['meta'])
    dve(lambda e: e.tensor_scalar(out=mk[:], in0=cm[:], scalar1=s8[:, 7:8], scalar2=None, op0=ALU.is_ge), ['cm', 's8'], ['mk'])
    dve(lambda e: e.tensor_copy(out=mkb[:], in_=mk[:]), ['mk', 'mkb'], ['mkb'])
    dve(lambda e: e.tensor_tensor(out=em[:], in0=mk[:], in1=sc_[:], op=ALU.mult), ['mk', 'sc'], ['em'])
    dve(lambda e: e.tensor_reduce(out=den[:, 0:1], in_=em[:], axis=AX.X, op=ALU.add), ['em'], ['den'])
    dve(lambda e: e.reciprocal(out=den[:, 1:2], in_=den[:, 0:1]), ['den'], ['den'])
    dve(lambda e: e.tensor_scalar(out=em[:], in0=em[:], scalar1=den[:, 1:2], scalar2=2.5, op0=ALU.mult, op1=ALU.mult), ['em', 'den'], ['em'])
    mm(P, pm[:, 64:128], R['su'][:], mkb[:], True, True, ['su', 'mkb'], ['pm_b'])
    mm(P, pm[:, 128:192], R['onesb'][:], mkb[:], True, True, ['onesb', 'mkb'], ['pm_c'])
    dve(lambda e: e.tensor_tensor(out=rk[:], in0=pm[:, 64:128], in1=R['cnt'][:], op=ALU.add), ['pm_b', 'cnt', 'rk'], ['rk'])
    dve(lambda e: e.tensor_tensor(out=R['cnt'][:], in0=pm[:, 128:192], in1=R['cnt'][:], op=ALU.add), ['pm_c', 'cnt'], ['cnt'])
    for k in range(8):
        dve(lambda e, k=k: e.tensor_scalar(out=oh[:], in0=R['iota'][:], scalar1=meta[:, k:k + 1], scalar2=None, op0=ALU.is_equal),
            ['iota', 'meta', 'oh'], ['oh'])
        dve(lambda e, k=k: e.scalar_tensor_tensor(out=jk[:], in0=oh[:], scalar=1.0, in1=rk[:], op0=ALU.mult, op1=ALU.mult, accum_out=meta[:, 8 + k:9 + k]), ['oh', 'rk', 'jk', 'meta'], ['jk', 'meta'])
        dve(lambda e, k=k: e.scalar_tensor_tensor(out=jk[:], in0=oh[:], scalar=1.0, in1=em[:], op0=ALU.mult, op1=ALU.mult, accum_out=meta[:, 16 + k:17 + k]), ['oh', 'em', 'jk', 'meta'], ['jk', 'meta'])
    P.dma('sp', lambda e: e.dma_start(out=T['meta'][i * 128:(i + 1) * 128, :], in_=meta[:]), reads=['meta'])


def phase_S0(P, T, G):
    st32 = Rot(P, 2, [128, 1024], F32, 's32')
    st16 = Rot(P, 1, [128, 1024], BF16, 's16')
    n = 0
    for e in range(65):
        for (src, shsrc, dst) in (('exp_gate', 'sh_gate', 'wg16'), ('exp_up', 'sh_up', 'wu16'), ('exp_down', 'sh_down', 'wd16')):
            s_ap = T[src][e] if e < 64 else T[shsrc]
            f = 256 if dst != 'wd16' else D
            rows = 512 if dst != 'wd16' else 128
            c0 = {'wg16': 0, 'wu16': 2048, 'wd16': 4096}[dst]
            for hh in range(2):
                a, ak = st32.next()
                c, ck = st16.next()
                src_ap = s_ap[hh * rows:(hh + 1) * rows, :].rearrange("(k p) f -> p k f", p=128)
                q = 'sp' if n % 2 == 0 else 'act'
                P.dma(q, lambda e_, a=a, src_ap=src_ap, f=f: e_.dma_start(out=a[:].rearrange("p (k f) -> p k f", f=f), in_=src_ap), writes=[ak])
                eng = ('dve', 'pool')[n % 2]
                P.op(eng, lambda e_, a=a, c=c: e_.tensor_copy(out=c[:], in_=a[:]), reads=[ak], writes=[ck])
                cc = c0 + hh * 1024
                P.dma('act' if n % 2 == 0 else 'sp', lambda e_, c=c, cc=cc, e=e: e_.dma_start(out=T['wall16'][e * 128:(e + 1) * 128, cc:cc + 1024], in_=c[:]), reads=[ck])
                n += 1
                yield


def phase_S(P, T, G):
    identb = G['identb']
    dve = lambda fn, r, w: P.op('dve', fn, reads=r, writes=w)
    cnt = P.sb([128, 64], F32)
    ci = P.sb([128, 64], I32)
    pad = P.sb([128, 64], F32)
    pend = P.sb([128, 64], F32)
    pst = P.sb([128, 64], F32)
    ones64f = P.sb([128, 64], F32)
    iota = P.sb([128, 64], F32)
    bst = P.sb([128, NBLK], F32)
    bef = P.sb([128, NBLK], F32)
    bei = P.sb([128, NBLK], I32)
    P.dma('sp', lambda e: e.dma_start(out=cnt[:], in_=T['cntd']), writes=['cnt'])
    P.dma('sp', lambda e: e.dma_start(out=iota[:], in_=T['k_rel'][0:1, 0:64].rearrange("o f -> (o f)").partition_broadcast(128)), writes=['iota'])
    P.dma('sp', lambda e: e.dma_start(out=bst[:], in_=T['k_bst'].partition_broadcast(128)), writes=['bst'])
    dve(lambda e: e.memset(ones64f[:], 1.0), [], ['ones64f'])
    dve(lambda e: e.tensor_scalar(out=pad[:], in0=cnt[:], scalar1=127.0, scalar2=None, op0=ALU.add), ['cnt'], ['pad'])
    dve(lambda e: e.tensor_copy(out=ci[:], in_=pad[:]), ['pad'], ['ci'])
    dve(lambda e: e.tensor_scalar(out=ci[:], in0=ci[:], scalar1=7, scalar2=None, op0=ALU.arith_shift_right), ['ci'], ['ci'])
    dve(lambda e: e.tensor_scalar(out=ci[:], in0=ci[:], scalar1=7, scalar2=None, op0=ALU.logical_shift_left), ['ci'], ['ci'])
    dve(lambda e: e.tensor_copy(out=pad[:], in_=ci[:]), ['ci', 'pad'], ['pad'])
    dve(lambda e: e.tensor_tensor_scan(out=pend[:], data0=ones64f[:], data1=pad[:], initial=0.0, op0=ALU.mult, op1=ALU.add),
        ['ones64f', 'pad'], ['pend'])
    dve(lambda e: e.tensor_tensor(out=pst[:], in0=pend[:], in1=pad[:], op=ALU.subtract), ['pend', 'pad'], ['pst'])
    for ex in range(64):
        if ex == 0:
            dve(lambda e: e.tensor_scalar(out=bef[:], in0=bst[:], scalar1=pend[:, 0:1], scalar2=None, op0=ALU.is_ge), ['bst', 'pend'], ['bef'])
        else:
            dve(lambda e, ex=ex: e.scalar_tensor_tensor(out=bef[:], in0=bst[:], scalar=pend[:, ex:ex + 1], in1=bef[:], op0=ALU.is_ge, op1=ALU.add),
                ['bst', 'pend', 'bef'], ['bef'])
    dve(lambda e: e.tensor_scalar(out=bef[:], in0=bef[:], scalar1=63.0, scalar2=None, op0=ALU.min), ['bef'], ['bef'])
    pcol = P.sb([128, 1], F32)
    widxf = P.sb([128, NBLK], F32)
    widx = P.sb([128, NBLK], I32)
    P.dma('sp', lambda e: e.dma_start(out=pcol[:], in_=T['k_rel'][:, 0:1], allow_slow_non_contiguous=True), writes=['pcol'])
    dve(lambda e: e.tensor_scalar(out=pcol[:], in0=pcol[:], scalar1=-1.0, scalar2=None, op0=ALU.mult), ['pcol'], ['pcol'])
    dve(lambda e: e.tensor_scalar(out=widxf[:], in0=bef[:], scalar1=128.0, scalar2=pcol[:, 0:1], op0=ALU.mult, op1=ALU.add), ['bef', 'pcol'], ['widxf'])
    chg = P.sb([128, NBLK], F32)
    dve(lambda e: e.memset(chg[:], 1.0), [], ['chg'])
    dve(lambda e: e.tensor_tensor(out=chg[:, 3:NBLK], in0=bef[:, 3:NBLK], in1=bef[:, 0:NBLK - 3], op=ALU.not_equal), ['bef', 'chg'], ['chg'])
    dve(lambda e: e.scalar_tensor_tensor(out=widxf[:], in0=widxf[:], scalar=-1.0e6, in1=chg[:], op0=ALU.add, op1=ALU.mult), ['widxf', 'chg'], ['widxf'])
    dve(lambda e: e.tensor_scalar(out=widxf[:], in0=widxf[:], scalar1=1.0e6, scalar2=None, op0=ALU.add), ['widxf'], ['widxf'])
    dve(lambda e: e.tensor_copy(out=widx[:], in_=widxf[:]), ['widxf'], ['widx'])
    d8i = P.sb([128, NT, 8], I32)
    gw8 = P.sb([128, NT, 8], F32)
    mrot = Rot(P, 2, [128, 24], F32, 'meta')
    hrot = Rot(P, 2, [128, D], BF16, 'htok')
    oh, jk = P.sb([128, 64], F32), P.sb([128, 64], F32)
    d8f = P.sb([128, 8], F32)
    for i in range(NT):
        tsl = slice(i * 128, (i + 1) * 128)
        mt, mtk = mrot.next()
        ht, htk = hrot.next()
        P.dma('sp', lambda e, mt=mt, tsl=tsl: e.dma_start(out=mt[:], in_=T['meta'][tsl, :]), writes=[mtk])
        P.dma('act', lambda e, ht=ht, tsl=tsl: e.dma_start(out=ht[:], in_=T['h2tok'][tsl, :]), writes=[htk])
        for k in range(8):
            dve(lambda e, mt=mt, k=k: e.tensor_scalar(out=oh[:], in0=iota[:], scalar1=mt[:, k:k + 1], scalar2=None, op0=ALU.is_equal),
                ['iota', mtk, 'oh'], ['oh'])
            dve(lambda e, k=k: e.scalar_tensor_tensor(out=jk[:], in0=oh[:], scalar=1.0, in1=pst[:], op0=ALU.mult, op1=ALU.mult, accum_out=d8f[:, k:k + 1]), ['oh', 'pst', 'jk', 'd8f'], ['jk', 'd8f'])
        dve(lambda e, mt=mt: e.tensor_tensor(out=d8f[:], in0=d8f[:], in1=mt[:, 8:16], op=ALU.add), ['d8f', mtk], ['d8f'])
        dve(lambda e, i=i: e.tensor_copy(out=d8i[:, i, :], in_=d8f[:]), ['d8f'], [f'd8i{i}'])
        dve(lambda e, mt=mt, i=i: e.tensor_copy(out=gw8[:, i, :], in_=mt[:, 16:24]), [mtk], [f'gw{i}'])
        for k in range(8):
            P.dma('pool', lambda e, ht=ht, i=i, k=k: e.indirect_dma_start(
                out=T['Xs'], out_offset=bass.IndirectOffsetOnAxis(ap=d8i[:, i, k:k + 1], axis=0), in_=ht[:], in_offset=None),
                reads=[htk, f'd8i{i}'])
    wgu = Rot(P, 3, [128, 6144], BF16, 'wgu')
    wsh = P.sb([128, 6144], BF16)
    P.dma('sp', lambda e: e.dma_start(out=wsh[:], in_=T['wall16'][64 * 128:65 * 128, :]), writes=['wsh'])
    xbr = Rot(P, 4, [128, D], BF16, 'xb')
    xTr = Rot(P, 2, [128, 8, 128], BF16, 'xT')
    sgr_ = Rot(P, 2, [128, 256], F32, 'sg')
    acr = Rot(P, 2, [128, 256], BF16, 'ac')
    aTr = Rot(P, 2, [128, 2, 128], BF16, 'aT')
    ybr = Rot(P, 2, [128, D], BF16, 'yb')
    ptx = Rot(P, 1, [128, 8, 128], BF16, 'ptx', psum=True)
    pgu = Rot(P, 2, [128, 512], F32, 'pgu', psum=True)
    pta = Rot(P, 1, [128, 2, 128], BF16, 'pta', psum=True)
    pyd = Rot(P, 3, [128, 512], F32, 'pyd', psum=True)
    regn = [0]

    P.emit(keep=True)
    hold = {}
    blocks = [('r', b) for b in range(NBLK)] + [('s', i) for i in range(NT)]
    st = {}

    ld = {}

    def stageL(kind, b):
        xb, xk = xbr.next()
        if kind == 'r':
            wl, wk = wgu.next()

            def gat(e):
                if 'bc' not in hold:
                    hold['bc'] = e.alloc_register("bc_reg")
                    e.reg_mov(hold['bc'], 65 * 128 - 1)
                return e.indirect_dma_start(out=wl[:], out_offset=None, in_=T['wall16'],
                                            in_offset=bass.IndirectOffsetOnAxis(ap=widx[:, b:b + 1], axis=0),
                                            bounds_check=hold['bc'], oob_is_err=False)
            P.dma('pool', gat, reads=['widx'], writes=[wk])
            P.dma('sp', lambda e: e.dma_start(out=xb[:], in_=T['Xs'][b * 128:(b + 1) * 128, :]), writes=[xk])
        else:
            wl, wk = wsh, 'wsh'
            P.dma('sp', lambda e: e.dma_start(out=xb[:], in_=T['h2tok'][b * 128:(b + 1) * 128, :]), writes=[xk])
        ld[(kind, b)] = (xb, xk, wl, wk)

    def stageA(kind, b):
        xb, xk, wl, wk = ld.pop((kind, b))
        wga, wua = wl[:, 0:2048], wl[:, 2048:4096]
        wd, wdk = wl[:, 4096:6144].rearrange("p (k f) -> p k f", f=D), wk
        px, pxk = ptx.next()
        for k in range(8):
            P.op('pe', lambda e, k=k: e.transpose(out=px[:, k, :], in_=xb[:, k * 128:(k + 1) * 128], identity=identb[:]), reads=[xk], writes=[pxk])
        xT, xTk = xTr.next()
        P.op('act', lambda e: e.activation(out=xT[:], in_=px[:], func=AF.Copy), reads=[pxk], writes=[xTk])
        pg_, pgk = pgu.next()
        for k in range(8):
            mm(P, pg_[:, 0:256], xT[:, k, :], wga[:, k * 256:(k + 1) * 256], k == 0, False, [xTk, wk], [pgk])
        for k in range(8):
            P.op('pe', lambda e, k=k: e.matmul(pg_[:, 256:512], lhsT=xT[:, k, :], rhs=wua[:, k * 256:(k + 1) * 256], start=False, stop=(k == 7), skip_group_check=True), reads=[xTk, wk], writes=[pgk])
        st[(kind, b)] = (pg_, pgk, wd, wdk)

    def stageB(kind, b):
        pg_, pgk, wd, wdk = st.pop((kind, b))
        sg, sgk = sgr_.next(); ac, ack = acr.next()
        P.op('act', lambda e: e.activation(out=sg[:], in_=pg_[:, 0:256], func=AF.Silu), reads=[pgk], writes=[sgk])
        dve(lambda e: e.tensor_tensor(out=ac[:], in0=pg_[:, 256:512], in1=sg[:], op=ALU.mult), [pgk, sgk], [ack])
        pa, pak = pta.next()
        for ft in range(2):
            P.op('pe', lambda e, ft=ft: e.transpose(out=pa[:, ft, :], in_=ac[:, ft * 128:(ft + 1) * 128], identity=identb[:]), reads=[ack], writes=[pak])
        aT, aTk = aTr.next()
        dve(lambda e: e.tensor_copy(out=aT[:], in_=pa[:]), [pak], [aTk])
        yb, ybk = ybr.next()
        for half in range(2):
            py_, pyk = pyd.next()
            cs_ = slice(half * 512, (half + 1) * 512)
            for ft in range(2):
                mm(P, py_[:], aT[:, ft, :], wd[:, ft, cs_], ft == 0, ft == 1, [aTk, wdk], [pyk])
            if half == 0:
                P.op('act', lambda e, py_=py_, cs_=cs_: e.activation(out=yb[:, cs_], in_=py_[:], func=AF.Copy), reads=[pyk], writes=[ybk])
            else:
                dve(lambda e, py_=py_, cs_=cs_: e.tensor_copy(out=yb[:, cs_], in_=py_[:]), [pyk], [ybk])
        dst = T['Ys'] if kind == 'r' else T['Ysh']
        P.dma('sp', lambda e: e.dma_start(out=dst[b * 128:(b + 1) * 128, :], in_=yb[:]), reads=[ybk])

    stageL(*blocks[0])
    stageL(*blocks[1])
    stageA(*blocks[0])
    for bi in range(len(blocks)):
        if bi + 2 < len(blocks):
            stageL(*blocks[bi + 2])
        if bi + 1 < len(blocks):
            stageA(*blocks[bi + 1])
        stageB(*blocks[bi])
    P.emit(keep=True)
    g2b = P.sb([128, D], F32)
    fing = P.sb([128, D], F32)
    P.dma('sp', lambda e: e.dma_start(out=g2b[:], in_=T['modd'][:, 5120:6144]), writes=['g2b'])
    P.dma('sp', lambda e: e.dma_start(out=fing[:], in_=T['final_g'].partition_broadcast(128)), writes=['fing'])
    xr = Rot(P, 2, [128, D], F32, 'x1')
    grot = Rot(P, 4, [128, D], BF16, 'gat')
    shr = Rot(P, 2, [128, D], BF16, 'shr')
    acc = P.sb([128, D], F32)
    junk, ss, rs = P.sb([128, D], F32), P.sb([128, 1], F32), P.sb([128, 1], F32)
    for i in range(NT):
        tsl = slice(i * 128, (i + 1) * 128)
        xt, xk = xr.next()
        sh, shk = shr.next()
        P.dma('sp', lambda e, xt=xt, tsl=tsl: e.dma_start(out=xt[:], in_=T['x1'][tsl, :]), writes=[xk])
        P.dma('act', lambda e, sh=sh, tsl=tsl: e.dma_start(out=sh[:], in_=T['Ysh'][tsl, :]), writes=[shk])
        for k in range(8):
            gt, gtk = grot.next()
            P.dma('pool', lambda e, gt=gt, i=i, k=k: e.indirect_dma_start(
                out=gt[:], out_offset=None, in_=T['Ys'], in_offset=bass.IndirectOffsetOnAxis(ap=d8i[:, i, k:k + 1], axis=0)),
                reads=[f'd8i{i}'], writes=[gtk])
            if k == 0:
                dve(lambda e, gt=gt, i=i, sh=sh: e.scalar_tensor_tensor(out=acc[:], in0=gt[:], scalar=gw8[:, i, 0:1], in1=sh[:], op0=ALU.mult, op1=ALU.add),
                    [gtk, f'gw{i}', shk, 'acc'], ['acc'])
            else:
                dve(lambda e, gt=gt, i=i, k=k: e.scalar_tensor_tensor(out=acc[:], in0=gt[:], scalar=gw8[:, i, k:k + 1], in1=acc[:], op0=ALU.mult, op1=ALU.add),
                    [gtk, f'gw{i}', 'acc'], ['acc'])
        dve(lambda e: e.tensor_tensor(out=acc[:], in0=acc[:], in1=g2b[:], op=ALU.mult), ['acc', 'g2b'], ['acc'])
        P.op('pool', lambda e, xt=xt: e.tensor_tensor(out=xt[:], in0=xt[:], in1=acc[:], op=ALU.add), reads=['acc', xk], writes=[xk])
        rms_rstd(P, xt, xk, junk, ss, rs, 'f')
        dve(lambda e, xt=xt: e.scalar_tensor_tensor(out=xt[:], in0=xt[:], scalar=rs[:, 0:1], in1=fing[:], op0=ALU.mult, op1=ALU.mult),
            [xk, 'rsf', 'fing'], [xk])
        P.dma('sp', lambda e, xt=xt, tsl=tsl: e.dma_start(out=T['out'][tsl, :], in_=xt[:]), reads=[xk])


SCRATCH = [
    ('zq', [512, S], BF16), ('zk', [512, S], BF16), ('zv', [S, 512], BF16), ('ziq', [512, S], BF16),
    ('zik', [32, S], BF16), ('ziw', [S, 16], F32), ('zr', [1792, S], F32), ('zga', [1024, S], BF16),
    ('zgr', [1024, S], BF16), ('modd', [128, 6 * D], F32), ('attnT', [512, S], BF16), ('rwT', [512, S], BF16),
    ('x1', [S, D], F32), ('h2T', [D, S], BF16), ('h2tok', [S, D], BF16), ('meta', [S, 24], F32), ('cntd', [128, 64], F32),
    ('Xs', [NBLK * 128, D], BF16), ('Ys', [NBLK * 128, D], BF16), ('Ysh', [S, D], BF16),
    ('wall16', [65 * 128, 6144], BF16),
]

INPUT_SHAPES = [
    ('x', [S, D]), ('c_col', [128, 8]), ('ada_w', [D, 6 * D]), ('ada_b', [1, 6 * D]), ('norm1_g', [D]),
    ('w_in', [D, NIN]), ('rel_bias', [256]), ('tshift_mu', [1792]), ('decay_w0', [512]), ('decay_up', [64, 512]),
    ('iclr_a0', [512]), ('iclr_up', [64, 512]), ('gate_up', [128, 512]), ('k_k', [512]), ('k_a', [512]),
    ('r_k', [512]), ('lnx_g', [512]), ('lnx_b', [512]), ('w_attn_br', [512, D]), ('w_rwkv_br', [512, D]),
    ('w_out', [D, D]), ('norm2_g', [D]), ('router_w', [D, 64]), ('router_bias', [64]),
    ('exp_gate', [64, D, 256]), ('exp_up', [64, D, 256]), ('exp_down', [64, 256, D]),
    ('sh_gate', [D, 256]), ('sh_up', [D, 256]), ('sh_down', [256, D]), ('final_g', [D]),
    ('k_ident', [128, 128]), ('k_rel', [128, 256]), ('k_masks', [128, 896]), ('k_reset', [64, 512]), ('k_bst', [NBLK]),
]


def build(debug_outs=(), stop_after=None):
    nc = bass.Bass("TRN2", target_bir_lowering=False)
    T = {}
    for name, shp in INPUT_SHAPES:
        T[name] = nc.dram_tensor(name, shp, F32, kind="ExternalInput").ap()
    for name, shp, dt in SCRATCH:
        kind = "ExternalOutput" if name in debug_outs else "Internal"
        T[name] = nc.dram_tensor(name, shp, dt, kind=kind).ap()
    T['out'] = nc.dram_tensor('out', [S, D], F32, kind="ExternalOutput").ap()
    if 'dbg_y' in debug_outs:
        T['dbg_y'] = nc.dram_tensor('dbg_y', [64, 8, 512], F32, kind="ExternalOutput").ap()
        T['dbg_bon'] = nc.dram_tensor('dbg_bon', [64, 8, 512], BF16, kind="ExternalOutput").ap()
        T['dbg_g'] = nc.dram_tensor('dbg_g', [64, 8, 512], BF16, kind="ExternalOutput").ap()
        T['dbg_AR'] = nc.dram_tensor('dbg_AR', [64, 8, 4, 256], BF16, kind="ExternalOutput").ap()
        T['dbg_BK'] = nc.dram_tensor('dbg_BK', [64, 8, 4, 256], BF16, kind="ExternalOutput").ap()
    P = Prog(nc)
    G = {}
    G['E'] = P.gsb([128, 8, 256], F32)
    G['b31'] = P.gsb([128, 8], F32)
    G['ones_row'] = P.gsb([1, 128], F32)
    G['identf'] = P.gsb([128, 128], F32)
    G['identb'] = P.gsb([128, 128], BF16)
    G['eps'] = P.gsb([128, 1], F32)
    G_EPS[0] = G['eps']
    P.op('dve', lambda e: e.memset(G['ones_row'][:], 1.0), writes=['ones_row'])
    P.op('dve', lambda e: e.memset(G['eps'][:], 1e-6), writes=['eps'])
    P.dma('sp', lambda e: e.dma_start(out=G['identf'][:], in_=T['k_ident']), writes=['identf'])
    P.op('dve', lambda e: e.tensor_copy(out=G['identb'][:], in_=G['identf'][:]), reads=['identf'], writes=['identb'])
    G['sparse'] = SPARSE
    phase_A(P, T, G)
    P.emit()
    if stop_after == 'A':
        P.emit(); P.finish(); return nc
    G['sparse'] = SPARSE
    phase_BC(P, T, G)
    P.emit()
    if stop_after == 'C':
        P.finish(); return nc
    phase_D(P, T, G)
    P.emit()
    if stop_after == 'D':
        P.finish(); return nc
    phase_E(P, T, G)
    P.emit()
    if stop_after == 'E':
        P.finish(); return nc
    G['sparse'] = SPARSE
    phase_F(P, T, G)
    P.emit()
    if stop_after == 'F':
        P.finish(); return nc
    if SPARSE:
        phase_S(P, T, G)
        P.emit()
        P.finish()
        return nc
    G['yacc'] = P.gsb([128, NT, D], F32)
    if stop_after in ('router', 'exp1'):
        G['gstop'] = stop_after
        G['nexp'] = 1
    if stop_after == 'router':
        phase_G(P, T, G); P.emit(); P.finish(); return nc
    if stop_after == 'exp1':
        phase_G(P, T, G); P.emit(); P.finish(); return nc
    phase_G(P, T, G)
    P.emit()
    phase_H(P, T, G)
    P.emit()
    P.finish()
    return nc


def host_inputs(inputs, b):
    m = {}
    f = lambda a: np.ascontiguousarray(np.asarray(a, dtype=np.float32))
    m['x'] = f(inputs['x'][b])
    m['c_col'] = f(np.asarray(inputs['c'][b]).reshape(8, 128).T)
    for name, shp in INPUT_SHAPES:
        if name in ('x', 'c_col', 'k_ident', 'k_rel', 'k_masks', 'k_reset', 'k_bst'):
            continue
        a = np.asarray(inputs[name])
        m[name] = f(a.reshape(shp))
    m['k_ident'] = np.eye(128, dtype=np.float32)
    ii = np.arange(128)
    su = (ii[:, None] < ii[None, :]).astype(np.float32)
    iu = (ii[:, None] <= ii[None, :]).astype(np.float32)
    sl = (ii[:, None] > ii[None, :]).astype(np.float32)
    m['k_masks'] = np.ascontiguousarray(np.concatenate([su, iu, su, iu, sl, np.zeros((128, 256), np.float32)], axis=1))
    rs_ = np.ones((64, 512), np.float32); rs_[:, ::128] = 0.0
    m['k_reset'] = rs_
    m['k_bst'] = (np.arange(NBLK, dtype=np.float32) * 128.0)
    m['k_rel'] = (np.arange(256, dtype=np.float32)[None, :] - np.arange(128, dtype=np.float32)[:, None])
    return m


def kernel(**inputs):
    nc = build()
    in_maps = [host_inputs(inputs, b) for b in range(8)]
    res = run_bass_kernel_spmd(nc, in_maps, core_ids=list(range(8)))
    return np.stack([np.asarray(r['out']) for r in res.results], axis=0).astype(np.float32)
```
